# Optimizing a Trainium2 kernel written in Bass

```python
import math
import jax, jax.numpy as jnp
from jax import lax
import numpy as np

D_MODEL = 1024
BATCH = 4
SEQ = 4096
DEPTH = 1
DEC_BATCH = 16
DEC_SEQ = 4096
PAST_LEN = 128

MIX_WIDTH = D_MODEL
MLA_HEADS = 8
QK_NOPE = 64
QK_ROPE = 32
V_HEAD = 64
Q_LORA = 256
KV_LORA = 128
MLA_WIDTH = MLA_HEADS * V_HEAD
Q_BLOCK = 128
ROPE_THETA = 10000.0
GMLP_GROUPS = 8
GMLP_WIDTH = MIX_WIDTH - MLA_WIDTH
GMLP_GROUP_DIM = GMLP_WIDTH // GMLP_GROUPS
CHUNK = 128
IN_COLS = Q_LORA + KV_LORA + QK_ROPE + 2 * GMLP_WIDTH
N_EXPERT_GROUPS = 4
EXPERTS_PER_GROUP = 8
N_EXPERTS = N_EXPERT_GROUPS * EXPERTS_PER_GROUP
TOP_K_IN_GROUP = 2
EXPERT_FF = 256
EPS = 1e-6

kernel_name = "hymba_style_mla_gmlp_hmoe_encoder"


def _rmsnorm(x, g):
    xf = x.astype(jnp.float32)
    y = xf * lax.rsqrt(jnp.mean(xf * xf, axis=-1, keepdims=True) + EPS)
    return (y * g.astype(jnp.float32)).astype(x.dtype)


def _rope(x, seq_len):
    half = x.shape[-1] // 2
    inv = ROPE_THETA ** (-jnp.arange(half, dtype=jnp.float32) / half)
    ang = jnp.arange(seq_len, dtype=jnp.float32)[:, None] * inv[None, :]
    cos = jnp.cos(ang)[None, :, None, :].astype(x.dtype)
    sin = jnp.sin(ang)[None, :, None, :].astype(x.dtype)
    x1, x2 = x[..., :half], x[..., half:]
    return jnp.concatenate([x1 * cos - x2 * sin, x1 * sin + x2 * cos], axis=-1)


def _block_attention(q, k, v):
    B, S, H, DQK = q.shape
    nb = S // Q_BLOCK
    qb = q.reshape(B, nb, Q_BLOCK, H, DQK).transpose(1, 0, 2, 3, 4)
    scale = DQK ** -0.5

    def one(qblk):
        s = jnp.einsum('bqhd,bkhd->bhqk', qblk, k).astype(jnp.float32) * scale
        p = jax.nn.softmax(s, axis=-1).astype(v.dtype)
        return jnp.einsum('bhqk,bkhd->bqhd', p, v)

    o = lax.map(one, qb)
    return o.transpose(1, 0, 2, 3, 4).reshape(B, S, H * v.shape[-1])


def _mla(cq, ckv, kr, g_q, w_uq, g_kv, w_ukv):
    B, S, _ = cq.shape
    q = (_rmsnorm(cq, g_q) @ w_uq).reshape(B, S, MLA_HEADS, QK_NOPE + QK_ROPE)
    q = jnp.concatenate([q[..., :QK_NOPE], _rope(q[..., QK_NOPE:], S)], axis=-1)
    kv = (_rmsnorm(ckv, g_kv) @ w_ukv).reshape(B, S, MLA_HEADS, QK_NOPE + V_HEAD)
    k_nope, v = kv[..., :QK_NOPE], kv[..., QK_NOPE:]
    k_rope = _rope(kr[:, :, None, :], S)
    k = jnp.concatenate([k_nope, jnp.broadcast_to(k_rope, (B, S, MLA_HEADS, QK_ROPE))], axis=-1)
    return _block_attention(q, k, v)


def _chunked_sgu(u, v, g_v, w_spatial, b_spatial):
    B, S, _ = u.shape
    u = jax.nn.gelu(u)
    v = _rmsnorm(jax.nn.gelu(v), g_v)
    vc = v.reshape(B, S // CHUNK, CHUNK, GMLP_GROUPS, GMLP_GROUP_DIM)
    vm = jnp.einsum('gts,bnsgc->bntgc', w_spatial, vc) + b_spatial.T[None, None, :, :, None]
    return u * vm.reshape(B, S, GMLP_WIDTH)


def _hier_moe(h, w_router_group, b_router_group, w_router_expert, b_router_expert,
              w_gate, w_up, w_down):
    B, S, D = h.shape
    t = h.reshape(B * S, D)
    tf = t.astype(jnp.float32)
    pg = jax.nn.softmax(tf @ w_router_group.astype(jnp.float32) + b_router_group.astype(jnp.float32), axis=-1)
    gi = jnp.argmax(pg, axis=-1)
    pg_sel = jnp.max(pg, axis=-1)
    le = (tf @ w_router_expert.astype(jnp.float32) + b_router_expert.astype(jnp.float32))
    le = le.reshape(-1, N_EXPERT_GROUPS, EXPERTS_PER_GROUP)
    le_sel = jnp.take_along_axis(le, gi[:, None, None], axis=1)[:, 0, :]
    pe = jax.nn.softmax(le_sel, axis=-1)
    vals, idx = lax.top_k(pe, TOP_K_IN_GROUP)
    w = pg_sel[:, None] * vals / jnp.sum(vals, axis=-1, keepdims=True)
    eid = gi[:, None] * EXPERTS_PER_GROUP + idx
    gates = jnp.einsum('tk,tke->te', w, jax.nn.one_hot(eid, N_EXPERTS, dtype=jnp.float32)).astype(t.dtype)

    def body(acc, xs):
        wg, wu, wd, g = xs
        hid = jax.nn.silu(t @ wg) * (t @ wu)
        return acc + g[:, None] * (hid @ wd), None

    out, _ = lax.scan(body, jnp.zeros_like(t), (w_gate, w_up, w_down, gates.T))
    return out.reshape(B, S, D)


def _layer(x, c, p):
    mod = jax.nn.silu(c) @ p['w_ada'] + p['b_ada']
    sh1, sc1, gt1, sh2, sc2, gt2 = [m[:, None, :] for m in jnp.split(mod, 6, axis=-1)]

    h = _rmsnorm(x, p['g_pre1']) * (1 + sc1) + sh1
    z = h @ p['w_in']
    o0 = Q_LORA
    o1 = o0 + KV_LORA
    o2 = o1 + QK_ROPE
    o3 = o2 + GMLP_WIDTH
    cq, ckv, kr, u, v = z[..., :o0], z[..., o0:o1], z[..., o1:o2], z[..., o2:o3], z[..., o3:]
    a = _mla(cq, ckv, kr, p['g_q'], p['w_uq'], p['g_kv'], p['w_ukv'])
    s = _chunked_sgu(u, v, p['g_v_gmlp'], p['w_spatial'], p['b_spatial'])
    merged = jnp.concatenate([_rmsnorm(a, p['g_attn_out']), _rmsnorm(s, p['g_gmlp_out'])], axis=-1)
    x = x + gt1 * _rmsnorm(merged @ p['w_out'], p['g_post1'])

    h2 = _rmsnorm(x, p['g_pre2']) * (1 + sc2) + sh2
    m = _hier_moe(h2, p['w_router_group'], p['b_router_group'], p['w_router_expert'],
                  p['b_router_expert'], p['w_gate'], p['w_up'], p['w_down'])
    return x + gt2 * _rmsnorm(m, p['g_post2'])


def setup_inputs(seed: int = 0) -> dict:
    key = jax.random.key(seed)
    ks = iter(jax.random.split(key, 40))

    def nrm(shape, scale):
        return jax.random.normal(next(ks), shape, jnp.float32) * scale

    def gain(n):
        return 1.0 + nrm((n,), 0.05)

    D = D_MODEL
    return {
        'x_prompt': nrm((BATCH, SEQ, D), 1.0),
        'x_sample': nrm((DEC_BATCH, DEC_SEQ, D), 1.0),
        'c_prompt': nrm((BATCH, D), 1.0),
        'c_sample': nrm((DEC_BATCH, D), 1.0),
        'w_ada': nrm((D, 6 * D), 0.1 * D ** -0.5),
        'b_ada': nrm((6 * D,), 0.02),
        'g_pre1': gain(D),
        'g_post1': gain(D),
        'g_pre2': gain(D),
        'g_post2': gain(D),
        'w_in': nrm((D, IN_COLS), D ** -0.5),
        'g_q': gain(Q_LORA),
        'w_uq': nrm((Q_LORA, MLA_HEADS * (QK_NOPE + QK_ROPE)), Q_LORA ** -0.5),
        'g_kv': gain(KV_LORA),
        'w_ukv': nrm((KV_LORA, MLA_HEADS * (QK_NOPE + V_HEAD)), KV_LORA ** -0.5),
        'g_v_gmlp': gain(GMLP_WIDTH),
        'w_spatial': nrm((GMLP_GROUPS, CHUNK, CHUNK), CHUNK ** -0.5),
        'b_spatial': 1.0 + nrm((GMLP_GROUPS, CHUNK), 0.05),
        'g_attn_out': gain(MLA_WIDTH),
        'g_gmlp_out': gain(GMLP_WIDTH),
        'w_out': nrm((MIX_WIDTH, D), MIX_WIDTH ** -0.5),
        'w_router_group': nrm((D, N_EXPERT_GROUPS), D ** -0.5),
        'b_router_group': nrm((N_EXPERT_GROUPS,), 0.01),
        'w_router_expert': nrm((D, N_EXPERTS), D ** -0.5),
        'b_router_expert': nrm((N_EXPERTS,), 0.01),
        'w_gate': nrm((N_EXPERTS, D, EXPERT_FF), D ** -0.5),
        'w_up': nrm((N_EXPERTS, D, EXPERT_FF), D ** -0.5),
        'w_down': nrm((N_EXPERTS, EXPERT_FF, D), EXPERT_FF ** -0.5),
    }


def reference(x_prompt, x_sample, c_prompt, c_sample, w_ada, b_ada, g_pre1, g_post1, g_pre2,
              g_post2, w_in, g_q, w_uq, g_kv, w_ukv, g_v_gmlp, w_spatial, b_spatial,
              g_attn_out, g_gmlp_out, w_out, w_router_group, b_router_group,
              w_router_expert, b_router_expert, w_gate, w_up, w_down):
    p = {
        'w_ada': w_ada, 'b_ada': b_ada, 'g_pre1': g_pre1, 'g_post1': g_post1,
        'g_pre2': g_pre2, 'g_post2': g_post2, 'w_in': w_in, 'g_q': g_q, 'w_uq': w_uq,
        'g_kv': g_kv, 'w_ukv': w_ukv, 'g_v_gmlp': g_v_gmlp, 'w_spatial': w_spatial,
        'b_spatial': b_spatial, 'g_attn_out': g_attn_out, 'g_gmlp_out': g_gmlp_out,
        'w_out': w_out, 'w_router_group': w_router_group, 'b_router_group': b_router_group,
        'w_router_expert': w_router_expert, 'b_router_expert': b_router_expert,
        'w_gate': w_gate, 'w_up': w_up, 'w_down': w_down,
    }
    y_prompt = x_prompt
    y_sample = x_sample
    for _ in range(DEPTH):
        y_prompt = _layer(y_prompt, c_prompt, p)
        y_sample = _layer(y_sample, c_sample, p)
    return (y_prompt, y_sample)
```

```python
import numpy as np
import concourse.bass as bass
import concourse.mybir as mybir
from concourse.bass_utils import run_bass_kernel_spmd
from contextlib import ExitStack

F32, BF16 = mybir.dt.float32, mybir.dt.bfloat16
I32 = mybir.dt.int32
AF = mybir.ActivationFunctionType
ALU = mybir.AluOpType
AX = mybir.AxisListType
D = 1024
EPS = 1e-6
NE = 32
QSCALE = 96.0 ** -0.5

FULL_CFG = dict(S=4096, NSEQ=3, NQG=[8, 8, 4])


class Dep:
    def __init__(self, nc, es):
        self.nc = nc
        self.es = es
        self.eng = {'pe': nc.tensor, 'act': nc.scalar, 'dve': nc.vector, 'pool': nc.gpsimd, 'sp': nc.sync}
        self.sem = {e: es.enter_context(nc.semaphore('s_' + e)) for e in self.eng}
        self.cnt = {e: 0 for e in self.eng}
        self.waited = {e: {} for e in self.eng}
        self.dsem = {}
        self.entries = {}
        self.free = []

    def semof(self, k):
        return self.sem[k] if k in self.sem else self.entries[k][0]

    def wait(self, e, deps):
        for d in _flat(deps):
            k, v = d
            if self.waited[e].get(k, 0) < v:
                self.eng[e].wait_ge(self.semof(k), v)
                self.waited[e][k] = v

    def op(self, e, deps, fn, sig=True):
        self.wait(e, deps)
        ins = fn()
        if sig:
            ins.then_inc(self.sem[e], 1)
            self.cnt[e] += 1
            return (e, self.cnt[e])
        return None

    def dma(self, q, name, deps, fn):
        if name not in self.dsem:
            if self.free:
                key = self.free.pop()
            else:
                key = 'D%d' % len(self.entries)
                self.entries[key] = [self.es.enter_context(self.nc.semaphore('d_' + key)), 0]
            self.dsem[name] = key
        key = self.dsem[name]
        self.wait(q, deps)
        ins = fn()
        ent = self.entries[key]
        ins.then_inc(ent[0], 16)
        ent[1] += 16
        return (key, ent[1])

    def mark(self):
        return set(self.dsem.keys())

    def retire_since(self, mark, keep=()):
        for n in list(self.dsem.keys()):
            if n in mark or n in keep:
                continue
            key = self.dsem[n]
            self.wait('sp', (key, self.entries[key][1]))
            del self.dsem[n]
            self.free.append(key)

    def last(self):
        return [(e, self.cnt[e]) for e in self.eng if self.cnt[e] > 0]


def _flat(deps):
    out = []
    if deps is None:
        return out
    if isinstance(deps, tuple) and len(deps) == 2 and isinstance(deps[0], str):
        return [deps]
    for d in deps:
        out.extend(_flat(d))
    return out


def interleave(gens, depth):
    active = []
    it = iter(gens)
    done = False
    while True:
        if len(active) < depth and not done:
            try:
                active.append(next(it))
            except StopIteration:
                done = True
        if not active:
            break
        nxt = []
        for g in active:
            try:
                next(g)
                nxt.append(g)
            except StopIteration:
                pass
        active = nxt


def build(cfg):
    S = cfg['S']
    NSEQ = cfg['NSEQ']
    NQG = cfg['NQG']
    NG = S // 512
    KB = S // 128
    NT = NSEQ * S
    NQT = sum(NQG) * 512
    NGB = sum(NQG)
    NSL = (2 * NQT + 32 * 127 + 127) // 128
    NSL = ((NSL + 7) // 8) * 8

    nc = bass.Bass("TRN2", target_bir_lowering=False)

    def din(name, shape, dt=F32):
        return nc.dram_tensor(name, list(shape), dt, kind="ExternalInput").ap()

    def dscr(name, shape, dt):
        return nc.dram_tensor(name, list(shape), dt, kind="Internal").ap()

    xs = din("xs", [NT, D])
    cvec = din("cvec", [NSEQ, D])
    rope_c = din("rope_c", [NSEQ, 32, S])
    rope_s = din("rope_s", [NSEQ, 32, S])
    w_ada = din("w_ada", [D, 6 * D])
    b_ada = din("b_ada", [6 * D])
    g_pre1 = din("g_pre1", [D]); g_post1 = din("g_post1", [D])
    g_pre2 = din("g_pre2", [D]); g_post2 = din("g_post2", [D])
    w_in = din("w_in", [D, 1440])
    g_q = din("g_q", [256]); w_uq = din("w_uq", [256, 768])
    g_kv = din("g_kv", [128]); w_ukv = din("w_ukv", [128, 1024])
    g_v_gmlp = din("g_v_gmlp", [512])
    w_spatial = din("w_spatial", [8, 128, 128]); b_spatial = din("b_spatial", [8, 128])
    g_attn_out = din("g_attn_out", [512]); g_gmlp_out = din("g_gmlp_out", [512])
    w_out = din("w_out", [D, D])
    w_rg = din("w_router_group", [D, 4]); b_rg = din("b_router_group", [4])
    w_re = din("w_router_expert", [D, 32]); b_re = din("b_router_expert", [32])
    w_gate = din("w_gate", [NE, D, 256]); w_up = din("w_up", [NE, D, 256]); w_down = din("w_down", [NE, 256, D])
    ident_in = din("ident", [128, 128])
    egrp_in = din("egrp", [8, 512])
    utri_in = din("utri", [128, 128])
    tri32_in = din("tri32", [32, 32])
    jv_in = din("jv", [128, NSL])
    pidx_in = din("pidx", [128, 1])
    y = nc.dram_tensor("y", [NQT, D], F32, kind="ExternalOutput").ap()

    mod_d = dscr("mod_d", [NSEQ, 6 * D], F32)
    sn_d = dscr("sn_d", [NT, 512], BF16)
    cq_d = dscr("cq_d", [NSEQ * NG, 128, 1024], BF16)
    x1_d = (nc.dram_tensor("x1_d", [NQT, D], F32, kind="ExternalOutput").ap() if cfg.get("dbg") else dscr("x1_d", [NQT, D], F32))
    wgu_r = dscr("wgu_r", [NE * 128, 8 * 512], BF16)
    wd_r = dscr("wd_r", [NE * 128, 2 * D], BF16)
    h2_d = dscr("h2_d", [NQT, D], BF16)
    xs_d = dscr("xs_d", [NSL * 128, D], BF16)
    ys_d = dscr("ys_d", [NSL * 128, D], F32)
    wkv_d = dscr("wkv_d", [128, 1024], BF16)
    wsp_d = dscr("wsp_d", [128, 1024], BF16)
    wq_d = dscr("wq_d", [128, 1536], BF16)
    wqsw_d = dscr("wqsw_d", [128, 1536], BF16)
    wo_d = dscr("wo_d", [128, 8192], BF16)

    _uid = [0]

    def U(name):
        _uid[0] += 1
        return "%s_u%d" % (name, _uid[0])

    top = ExitStack()
    with top:
        dp = Dep(nc, top)

        def PE(deps, fn, sig=True): return dp.op('pe', deps, fn, sig)
        def ACT(deps, fn, sig=True): return dp.op('act', deps, fn, sig)
        def DVE(deps, fn, sig=True): return dp.op('dve', deps, fn, sig)
        def POOL(deps, fn, sig=True): return dp.op('pool', deps, fn, sig)
        def DMA(name, deps, fn, q='sp'): return dp.dma(q, name, deps, fn)

        def mmg(out, pairs, deps, sig=True):
            n = len(pairs)
            tok = None
            for i, (l, r) in enumerate(pairs):
                tok = PE(deps if i == 0 else None,
                         lambda l=l, r=r, i=i: nc.tensor.matmul(out, lhsT=l, rhs=r, start=(i == 0), stop=(i == n - 1)),
                         sig=(sig and i == n - 1))
            return tok

        def rstd_chain(ss_ap, out_ap, inv_n, deps):
            t = ACT(deps, lambda: nc.scalar.activation(out=out_ap, in_=ss_ap, func=AF.Sqrt, bias=EPS, scale=inv_n))
            return DVE(t, lambda: nc.vector.reciprocal(out=out_ap, in_=out_ap))

        wcast = []
        for e in range(NE):
            wcast.append(DMA('wcast', None, lambda e=e: nc.gpsimd.dma_start(
                out=wgu_r[e * 128:(e + 1) * 128, :].rearrange("p (k c) -> p k c", k=8)[:, :, 0:256],
                in_=w_gate[e].rearrange("(k p) c -> p k c", p=128)), q='pool'))
            wcast.append(DMA('wcast', None, lambda e=e: nc.gpsimd.dma_start(
                out=wgu_r[e * 128:(e + 1) * 128, :].rearrange("p (k c) -> p k c", k=8)[:, :, 256:512],
                in_=w_up[e].rearrange("(k p) c -> p k c", p=128)), q='pool'))
            wcast.append(DMA('wcast', None, lambda e=e: nc.gpsimd.dma_start(
                out=wd_r[e * 128:(e + 1) * 128, :].rearrange("p (j c) -> p j c", j=2),
                in_=w_down[e].rearrange("(j p) c -> p j c", p=128)), q='pool'))
        wcast_tok = wcast[-1]

        ident_f = top.enter_context(nc.sbuf_tensor(U("ident_f"), [128, 128], F32))
        ident_b = top.enter_context(nc.sbuf_tensor(U("ident_b"), [128, 128], BF16))
        ones_f = top.enter_context(nc.sbuf_tensor(U("ones_f"), [128, 64], F32))
        t_id = DMA('c0', None, lambda: nc.sync.dma_start(out=ident_f[:], in_=ident_in))
        t_idb = DVE(t_id, lambda: nc.vector.tensor_copy(out=ident_b[:], in_=ident_f[:]))
        t_ones = DVE(None, lambda: nc.vector.memset(ones_f[:], 1.0))

        mk0 = dp.mark()
        with ExitStack() as pes:
            def sb(name, shape, dt): return pes.enter_context(nc.sbuf_tensor(U(name), shape, dt))
            def ps(name, shape, dt): return pes.enter_context(nc.psum_tensor(U(name), shape, dt))
            zt = sb("zt", [128, 8192], BF16)
            tz0 = POOL(None, lambda: nc.gpsimd.memset(zt[:], 0.0))
            zero_tok = []
            nz = (NSL * 128 * D) // (128 * 8192)
            xs_flat = xs_d.rearrange("(n p r) c -> n p (r c)", p=128, r=8)
            for zi in range(nz):
                zero_tok.append(DMA('zero', tz0, lambda zi=zi: nc.sync.dma_start(out=xs_flat[zi], in_=zt[:])))
            csT = sb("csT", [128, 8, NSEQ], F32)
            csS = sb("csS", [128, 8, NSEQ], F32)
            wblk = [sb("wblk%d" % i, [128, 8, 512], F32) for i in range(2)]
            brep = sb("brep", [NSEQ, 6 * D], F32)
            modsb = sb("modsb", [NSEQ, 6 * D], F32)
            pmod = [ps("pmod%d" % i, [128, 512], F32) for i in range(2)]
            t_c = [DMA('p0', None, lambda q=q: nc.sync.dma_start(out=csT[:, :, q], in_=cvec[q].rearrange("(k p) -> p k", p=128),
                                                                 allow_slow_non_contiguous=True)) for q in range(NSEQ)]
            t_b = DMA('p1', None, lambda: nc.sync.dma_start(out=brep[:], in_=b_ada.partition_broadcast(NSEQ)))
            t_cs = ACT(t_c, lambda: nc.scalar.activation(out=csS[:], in_=csT[:], func=AF.Silu))
            wfree = [None, None]
            pfree = [None, None]
            ev = None
            for blk in range(12):
                i = blk % 2
                t_w = DMA('pw%d' % i, wfree[i], lambda blk=blk, i=i: nc.sync.dma_start(
                    out=wblk[i][:], in_=w_ada[:, blk * 512:(blk + 1) * 512].rearrange("(k p) c -> p k c", p=128)))
                t_m = mmg(pmod[i][0:NSEQ, :], [(csS[:, k, :], wblk[i][:, k, :]) for k in range(8)], [t_w, t_cs, pfree[i]])
                wfree[i] = t_m
                ev = DVE([t_m, t_b], lambda blk=blk, i=i: nc.vector.tensor_tensor(
                    out=modsb[:, blk * 512:(blk + 1) * 512], in0=pmod[i][0:NSEQ, :],
                    in1=brep[:, blk * 512:(blk + 1) * 512], op=ALU.add))
                pfree[i] = ev
            t_mod = DMA('p2', ev, lambda: nc.sync.dma_start(out=mod_d, in_=modsb[:]))

            tmpq = sb("tmpq", [128, 2, 768], F32)
            gq = sb("gq", [128, 2], F32)
            wq_t = sb("wq_t", [128, 2, 768], BF16)
            wqsw_t = sb("wqsw_t", [128, 2, 768], BF16)
            t1 = DMA('p3', None, lambda: nc.sync.dma_start(out=tmpq[:], in_=w_uq.rearrange("(k p) c -> p k c", p=128)))
            t2 = DMA('p3', None, lambda: nc.sync.dma_start(out=gq[:], in_=g_q.rearrange("(k p) -> p k", p=128),
                                                          allow_slow_non_contiguous=True))
            tq = None
            for k in range(2):
                tq = DVE([t1, t2], lambda k=k: nc.vector.tensor_scalar(
                    out=wq_t[:, k, :], in0=tmpq[:, k, :], scalar1=gq[:, k:k + 1], scalar2=QSCALE,
                    op0=ALU.mult, op1=ALU.mult))
            tz = POOL(None, lambda: nc.gpsimd.memset(wqsw_t[:], 0.0))
            wq4 = wq_t[:].rearrange("p k (h c) -> p k h c", h=8)
            wqs4 = wqsw_t[:].rearrange("p k (h c) -> p k h c", h=8)
            ta = DVE([tq, tz], lambda: nc.vector.tensor_scalar(out=wqs4[:, :, :, 64:80], in0=wq4[:, :, :, 80:96],
                                                              scalar1=-1.0, scalar2=None, op0=ALU.mult))
            tb = DVE(None, lambda: nc.vector.tensor_copy(out=wqs4[:, :, :, 80:96], in_=wq4[:, :, :, 64:80]))
            t_wq = DMA('p4', tq, lambda: nc.sync.dma_start(out=wq_d, in_=wq_t[:].rearrange("p k c -> p (k c)")))
            t_wqsw = DMA('p4', [ta, tb], lambda: nc.sync.dma_start(out=wqsw_d, in_=wqsw_t[:].rearrange("p k c -> p (k c)")))

            tmpkv = sb("tmpkv", [128, 1024], F32)
            gkv = sb("gkv", [128, 1], F32)
            wkv_t = sb("wkv_t", [128, 1024], BF16)
            t1 = DMA('p5', None, lambda: nc.sync.dma_start(out=tmpkv[:], in_=w_ukv))
            t2 = DMA('p5', None, lambda: nc.sync.dma_start(out=gkv[:], in_=g_kv.rearrange("(p o) -> p o", o=1)))
            tk = DVE([t1, t2], lambda: nc.vector.tensor_scalar(out=wkv_t[:], in0=tmpkv[:], scalar1=gkv[:, 0:1],
                                                              scalar2=None, op0=ALU.mult))
            t_wkv = DMA('p6', tk, lambda: nc.sync.dma_start(out=wkv_d, in_=wkv_t[:]))

            tmpo = sb("tmpo", [128, 8, 1024], F32)
            gcat = sb("gcat", [128, 8], F32)
            wo_t = sb("wo_t", [128, 8, 1024], BF16)
            t1 = DMA('p7', None, lambda: nc.sync.dma_start(out=tmpo[:], in_=w_out.rearrange("(k p) c -> p k c", p=128)))
            t2 = DMA('p7', None, lambda: nc.sync.dma_start(out=gcat[:, 0:4], in_=g_attn_out.rearrange("(k p) -> p k", p=128),
                                                          allow_slow_non_contiguous=True))
            t3 = DMA('p7', None, lambda: nc.sync.dma_start(out=gcat[:, 4:8], in_=g_gmlp_out.rearrange("(k p) -> p k", p=128),
                                                          allow_slow_non_contiguous=True))
            two = None
            for k in range(8):
                two = DVE([t1, t2, t3], lambda k=k: nc.vector.tensor_scalar(
                    out=wo_t[:, k, :], in0=tmpo[:, k, :], scalar1=gcat[:, k:k + 1], scalar2=None, op0=ALU.mult))
            t_wo = DMA('p8', two, lambda: nc.sync.dma_start(out=wo_d, in_=wo_t[:].rearrange("p k c -> p (k c)")))

            tmps = sb("tmps", [128, 8, 128], F32)
            wsp_t = sb("wsp_t", [128, 8, 128], BF16)
            psp = ps("psp", [128, 1024], F32)
            t1 = DMA('p9', None, lambda: nc.sync.dma_start(out=tmps[:], in_=w_spatial.rearrange("g t s -> t g s")))
            tt = None
            for g in range(8):
                tt = PE([t1, t_id], lambda g=g: nc.tensor.transpose(psp[:, g * 128:(g + 1) * 128], tmps[:, g, :], ident_f[:]),
                        sig=(g == 7))
            tc_ = DVE(tt, lambda: nc.vector.tensor_copy(out=wsp_t[:].rearrange("p g t -> p (g t)"), in_=psp[:]))
            t_wsp = DMA('p10', tc_, lambda: nc.sync.dma_start(out=wsp_d, in_=wsp_t[:].rearrange("p g t -> p (g t)")))
            prep_done = [t_mod, t_wq, t_wqsw, t_wkv, t_wo, t_wsp, zero_tok]
            prep_bar = dp.last()
            dp.retire_since(mk0, keep=('wcast', 'zero', 'c0'))

        with ExitStack() as aes:
            def sbA(name, shape, dt): return aes.enter_context(nc.sbuf_tensor(U(name), shape, dt))
            KT = sbA("KT", [128, 8, S], BF16)
            VA = sbA("VA", [128, KB, 8, 65], BF16)
            geff1 = sbA("geff1", [128, D], F32)
            sh1r = sbA("sh1r", [128, D], F32)
            gvec1 = sbA("gvec1", [128, D], F32)
            gvrep = sbA("gvrep", [128, 512], F32)
            PA = aes.enter_context(nc.psum_tensor(U("PA"), [128, 1024], F32))
            PB = aes.enter_context(nc.psum_tensor(U("PB"), [128, 1024], F32))
            PC = aes.enter_context(nc.psum_tensor(U("PC"), [128, 1024], F32))
            PD = aes.enter_context(nc.psum_tensor(U("PD"), [128, 1024], F32))

            t_va1 = POOL(prep_bar, lambda: nc.gpsimd.memset(VA[:, :, :, 64:65], 1.0))
            t_gv = DMA('a0', prep_bar, lambda: nc.sync.dma_start(out=gvrep[:], in_=g_v_gmlp.partition_broadcast(128)))
            seq_bar = [prep_bar, prep_done, t_va1, t_gv, t_idb, t_ones]
            qbase = 0
            for s in range(NSEQ):
                mk1 = dp.mark()
                with ExitStack() as p1:
                    def sb1(name, shape, dt): return p1.enter_context(nc.sbuf_tensor(U(name), shape, dt))
                    wAs = sb1("wAs", [128, 8, 384], BF16)
                    wAuv = sb1("wAuv", [128, 8, 1024], BF16)
                    wAkr = sb1("wAkr", [128, 8, 96], BF16)
                    wAks = sb1("wAks", [128, 8, 96], BF16)
                    wkv = sb1("wkv", [128, 1024], BF16)
                    wsp = sb1("wsp", [128, 8, 128], BF16)
                    bsp = sb1("bsp", [8, 128], F32)
                    egrp = sb1("egrp", [8, 512], F32)
                    vt = [sb1("vt%d" % i, [128, D], F32) for i in range(2)]
                    xt = [sb1("xt%d" % i, [128, D], F32) for i in range(2)]
                    junk = sb1("junk", [128, D], BF16)
                    hm = sb1("hm", [128, D], F32)
                    hb = [sb1("hb%d" % i, [128, D], BF16) for i in range(2)]
                    hT = sb1("hT", [128, 8, 512], BF16)
                    zsb = [sb1("zsb%d" % i, [128, 384], BF16) for i in range(2)]
                    cqnT = [sb1("cqnT%d" % i, [128, 2, 512], BF16) for i in range(2)]
                    ckvnT = [sb1("ckvnT%d" % i, [128, 512], BF16) for i in range(2)]
                    gu = [sb1("gu%d" % i, [128, 512], BF16) for i in range(2)]
                    gv = [sb1("gv%d" % i, [128, 512], F32) for i in range(2)]
                    zraw = [sb1("zraw%d" % i, [128, 384], F32) for i in range(2)]
                    vn = [sb1("vn%d" % i, [128, 512], BF16) for i in range(2)]
                    sraw = [sb1("sraw%d" % i, [128, 512], F32) for i in range(2)]
                    sn = [sb1("sn%d" % i, [128, 512], BF16) for i in range(2)]
                    stt = [sb1("stt%d" % i, [128, 16], F32) for i in range(2)]
                    Ctt = [sb1("Ctt%d" % i, [128, 128], F32) for i in range(2)]
                    Stt = [sb1("Stt%d" % i, [128, 128], F32) for i in range(2)]
                    kt1 = [sb1("kt1_%d" % i, [128, 128], F32) for i in range(2)]
                    kt2 = [sb1("kt2_%d" % i, [128, 128], F32) for i in range(2)]
                    krr = [sb1("krr%d" % i, [128, 128], BF16) for i in range(2)]

                    pT = PA[:, 0:512].bitcast(BF16)
                    pT2 = PA[:, 512:1024].bitcast(BF16)
                    pzs = PB[:, 0:384]
                    pss = PB[:, 512:1024]
                    pu = PC[:, 0:512]
                    pv = PC[:, 512:1024]
                    pkr = PD[:, 0:512]
                    pks = PD[:, 512:1024]

                    sb_ = seq_bar
                    wl = []
                    wl.append(DMA('a1', sb_, lambda: nc.gpsimd.dma_start(
                        out=wAs[:], in_=w_in[:, 0:384].rearrange("(k p) c -> p k c", p=128)), q='pool'))
                    wl.append(DMA('a1', sb_, lambda: nc.gpsimd.dma_start(
                        out=wAuv[:], in_=w_in[:, 416:1440].rearrange("(k p) c -> p k c", p=128)), q='pool'))
                    tz1 = POOL(sb_, lambda: nc.gpsimd.memset(wAkr[:], 0.0))
                    tz2 = POOL(sb_, lambda: nc.gpsimd.memset(wAks[:], 0.0))
                    wl.append(DMA('a1', [tz1], lambda: nc.gpsimd.dma_start(
                        out=wAkr[:, :, 64:96], in_=w_in[:, 384:416].rearrange("(k p) c -> p k c", p=128)), q='pool'))
                    tn = DMA('a2', [tz2], lambda: nc.gpsimd.dma_start(
                        out=wAks[:, :, 64:80], in_=w_in[:, 400:416].rearrange("(k p) c -> p k c", p=128)), q='pool')
                    wl.append(DMA('a1', [tz2], lambda: nc.gpsimd.dma_start(
                        out=wAks[:, :, 80:96], in_=w_in[:, 384:400].rearrange("(k p) c -> p k c", p=128)), q='pool'))
                    wl.append(POOL(tn, lambda: nc.gpsimd.tensor_scalar(out=wAks[:, :, 64:80], in0=wAks[:, :, 64:80],
                                                                      scalar1=-1.0, scalar2=None, op0=ALU.mult)))
                    wl.append(DMA('a3', sb_, lambda: nc.sync.dma_start(out=wkv[:], in_=wkv_d)))
                    wl.append(DMA('a3', sb_, lambda: nc.sync.dma_start(out=wsp[:].rearrange("p g t -> p (g t)"), in_=wsp_d)))
                    wl.append(DMA('a3', sb_, lambda: nc.sync.dma_start(out=bsp[:], in_=b_spatial)))
                    wl.append(DMA('a3', sb_, lambda: nc.sync.dma_start(out=egrp[:], in_=egrp_in)))
                    l1 = DMA('a4_0', sb_, lambda: nc.sync.dma_start(out=vt[0][:], in_=mod_d[s, 1024:2048].partition_broadcast(128)))
                    l2 = DMA('a4_1', sb_, lambda: nc.sync.dma_start(out=vt[1][:], in_=g_pre1.partition_broadcast(128)))
                    l3 = DMA('a4_2', sb_, lambda: nc.sync.dma_start(out=sh1r[:], in_=mod_d[s, 0:1024].partition_broadcast(128)))
                    tg = DVE([l1, l2], lambda: nc.vector.scalar_tensor_tensor(out=geff1[:], in0=vt[0][:], scalar=1.0, in1=vt[1][:],
                                                                               op0=ALU.add, op1=ALU.mult))
                    l4 = DMA('a4_3', [tg], lambda: nc.sync.dma_start(out=vt[0][:], in_=mod_d[s, 2048:3072].partition_broadcast(128)))
                    l5 = DMA('a4_4', [tg], lambda: nc.sync.dma_start(out=vt[1][:], in_=g_post1.partition_broadcast(128)))
                    tg2 = DVE([l4, l5], lambda: nc.vector.tensor_tensor(out=gvec1[:], in0=vt[0][:], in1=vt[1][:], op=ALU.mult))
                    ready = [wl, l3, tg, tg2]

                    xt_free = [None, None]; hb_free = [None, None]
                    hT_free = [None] * 4
                    zraw_free = [None, None]; zsb_free = [None, None]; cq_free = [None, None]; ckv_free = [None, None]
                    gu_free = [None, None]; gv_free = [None, None]; vn_free = [None, None]; sn_free = [None, None]
                    sraw_free = [None, None]; ct_free = [None, None]; kt_free = [None, None]; krr_free = [None, None]
                    P = dict(hm_free=None, pT_free=None, pT2_free=None, pzs_free=None, pu_free=None, pv_free=None, pss_free=None,
                             pkr_free=None, pks_free=None)
                    grp = {}

                    def p1_tile(g, t):
                        gi = g % 2
                        ti = t % 2
                        if t == 0:
                            grp[g] = dict(cq_w=[], ckv_w=[])
                        G = grp[g]
                        tok0 = s * S + g * 512 + t * 128
                        ts_ = slice(t * 128, (t + 1) * 128)
                        gts = slice(g * 512 + t * 128, g * 512 + (t + 1) * 128)
                        st_ = stt[ti]
                        lx = DMA('x%d' % ti, [xt_free[ti], ready], lambda: nc.sync.dma_start(out=xt[ti][:], in_=xs[tok0:tok0 + 128, :]))
                        lc = DMA('rc%d' % ti, [ct_free[ti], ready], lambda: nc.sync.dma_start(out=Ctt[ti][64:96, :], in_=rope_c[s, :, gts]))
                        ls = DMA('rs%d' % ti, [ct_free[ti], ready], lambda: nc.sync.dma_start(out=Stt[ti][64:96, :], in_=rope_s[s, :, gts]))
                        a1 = ACT(lx, lambda: nc.scalar.activation(out=junk[:], in_=xt[ti][:], func=AF.Square, accum_out=st_[:, 0:1]))
                        yield
                        r1a = ACT(a1, lambda: nc.scalar.activation(out=st_[:, 1:2], in_=st_[:, 0:1], func=AF.Sqrt, bias=EPS, scale=1.0 / D))
                        yield
                        r1 = DVE(r1a, lambda: nc.vector.reciprocal(out=st_[:, 1:2], in_=st_[:, 1:2]))
                        d1 = DVE([r1, P['hm_free']], lambda: nc.vector.scalar_tensor_tensor(
                            out=hm[:], in0=xt[ti][:], scalar=st_[:, 1:2], in1=geff1[:], op0=ALU.mult, op1=ALU.mult))
                        xt_free[ti] = d1
                        p1_ = POOL([d1, hb_free[ti]], lambda: nc.gpsimd.tensor_tensor(out=hb[ti][:], in0=hm[:], in1=sh1r[:], op=ALU.add))
                        P['hm_free'] = p1_
                        yield
                        tp = None
                        for k in range(8):
                            tp = PE([p1_, P['pT_free']] if k == 0 else None,
                                    lambda k=k: nc.tensor.transpose(pT[:, k * 128:(k + 1) * 128], hb[ti][:, k * 128:(k + 1) * 128], ident_b[:]),
                                    sig=(k == 7))
                        hb_free[ti] = tp
                        yield
                        ev = ACT([tp, hT_free[t]], lambda: nc.scalar.copy(out=hT[:, :, ts_], in_=pT.rearrange("p (k c) -> p k c", k=8)))
                        P['pT_free'] = ev
                        yield
                        m_zs = mmg(pzs, [(hT[:, k, ts_], wAs[:, k, :]) for k in range(8)], [ev, P['pzs_free']])
                        m_u = mmg(pu, [(hT[:, k, ts_], wAuv[:, k, 0:512]) for k in range(8)], [P['pu_free']])
                        m_v = mmg(pv, [(hT[:, k, ts_], wAuv[:, k, 512:1024]) for k in range(8)], [P['pv_free']])
                        m_kr = mmg(pkr[0:96, 0:128], [(wAkr[:, k, :], hT[:, k, ts_]) for k in range(8)], [P['pkr_free']])
                        m_ks = mmg(pks[0:96, 0:128], [(wAks[:, k, :], hT[:, k, ts_]) for k in range(8)], [P['pks_free']])
                        hT_free[t] = m_ks
                        yield
                        zr = DVE([m_zs, zraw_free[ti]], lambda: nc.vector.tensor_copy(out=zraw[ti][:], in_=pzs))
                        P['pzs_free'] = zr
                        g1 = ACT([m_u, gu_free[ti]], lambda: nc.scalar.activation(out=gu[ti][:], in_=pu, func=AF.Gelu_apprx_tanh))
                        P['pu_free'] = g1
                        g2 = ACT([m_v, gv_free[ti]], lambda: nc.scalar.activation(out=gv[ti][:], in_=pv, func=AF.Gelu_apprx_tanh))
                        P['pv_free'] = g2
                        k1 = DVE([m_kr, lc, kt_free[ti]], lambda: nc.vector.tensor_tensor(out=kt1[ti][64:96, :], in0=pkr[64:96, 0:128], in1=Ctt[ti][64:96, :], op=ALU.mult))
                        P['pkr_free'] = k1
                        k2 = DVE([m_ks, ls], lambda: nc.vector.tensor_tensor(out=kt2[ti][64:96, :], in0=pks[64:96, 0:128], in1=Stt[ti][64:96, :], op=ALU.mult))
                        P['pks_free'] = k2
                        ct_free[ti] = k2
                        yield
                        a2 = ACT(zr, lambda: nc.scalar.activation(out=junk[:, 0:256], in_=zraw[ti][:, 0:256], func=AF.Square, accum_out=st_[:, 2:3]))
                        a3 = ACT(None, lambda: nc.scalar.activation(out=junk[:, 256:384], in_=zraw[ti][:, 256:384], func=AF.Square, accum_out=st_[:, 3:4]))
                        g3 = ACT(g2, lambda: nc.scalar.activation(out=junk[:, 0:512], in_=gv[ti][:], func=AF.Square, accum_out=st_[:, 6:7]))
                        k3 = DVE([k1, k2, krr_free[ti]], lambda: nc.vector.tensor_tensor(out=krr[ti][64:96, :], in0=kt1[ti][64:96, :], in1=kt2[ti][64:96, :], op=ALU.add))
                        kt_free[ti] = k3
                        kc = None
                        for h in range(8):
                            kc = POOL(k3, lambda h=h: nc.gpsimd.tensor_copy(out=KT[64:96, h, gts], in_=krr[ti][64:96, :]))
                        krr_free[ti] = kc
                        yield
                        q1 = ACT([a2, a3], lambda: nc.scalar.activation(out=st_[:, 4:5], in_=st_[:, 2:3], func=AF.Sqrt, bias=EPS, scale=1.0 / 256))
                        q2 = ACT(None, lambda: nc.scalar.activation(out=st_[:, 5:6], in_=st_[:, 3:4], func=AF.Sqrt, bias=EPS, scale=1.0 / 128))
                        q3 = ACT(g3, lambda: nc.scalar.activation(out=st_[:, 7:8], in_=st_[:, 6:7], func=AF.Sqrt, bias=EPS, scale=1.0 / 512))
                        yield
                        r2 = DVE([q1, q2], lambda: nc.vector.reciprocal(out=st_[:, 4:6], in_=st_[:, 4:6]))
                        r4 = DVE(q3, lambda: nc.vector.reciprocal(out=st_[:, 7:8], in_=st_[:, 7:8]))
                        d2 = DVE([r4, vn_free[ti]], lambda: nc.vector.scalar_tensor_tensor(
                            out=vn[ti][:], in0=gv[ti][:], scalar=st_[:, 7:8], in1=gvrep[:], op0=ALU.mult, op1=ALU.mult))
                        gv_free[ti] = d2
                        c1 = ACT([r2, zsb_free[ti]], lambda: nc.scalar.activation(
                            out=zsb[ti][:, 0:256], in_=zraw[ti][:, 0:256], func=AF.Copy, scale=st_[:, 4:5]))
                        c2 = ACT(None, lambda: nc.scalar.activation(
                            out=zsb[ti][:, 256:384], in_=zraw[ti][:, 256:384], func=AF.Copy, scale=st_[:, 5:6]))
                        zraw_free[ti] = c2
                        yield
                        tp2 = None
                        for k in range(3):
                            tp2 = PE([c1, c2, P['pT2_free']] if k == 0 else None,
                                     lambda k=k: nc.tensor.transpose(pT2[:, k * 128:(k + 1) * 128], zsb[ti][:, k * 128:(k + 1) * 128], ident_b[:]),
                                     sig=(k == 2))
                        zsb_free[ti] = tp2
                        PE([P['pss_free'], ready], lambda: nc.tensor.matmul(pss, lhsT=bsp[:, :], rhs=egrp[:, :], start=True, stop=False), sig=False)
                        m_s = None
                        for gg in range(8):
                            m_s = PE(d2 if gg == 0 else None,
                                     lambda gg=gg: nc.tensor.matmul(pss[:, gg * 64:(gg + 1) * 64], lhsT=wsp[:, gg, :],
                                                                    rhs=vn[ti][:, gg * 64:(gg + 1) * 64], start=False, stop=(gg == 7)),
                                     sig=(gg == 7))
                        vn_free[ti] = m_s
                        yield
                        e1 = DVE([tp2, cq_free[gi] if t == 0 else None], lambda: nc.vector.tensor_copy(
                            out=cqnT[gi][:, :, ts_], in_=pT2[:, 0:256].rearrange("p (k c) -> p k c", k=2)))
                        e2 = DVE([ckv_free[gi] if t == 0 else None], lambda: nc.vector.tensor_copy(
                            out=ckvnT[gi][:, ts_], in_=pT2[:, 256:384]))
                        P['pT2_free'] = e2
                        G['cq_w'].append(e1)
                        G['ckv_w'].append(e2)
                        d3 = DVE([m_s, g1, sraw_free[ti]], lambda: nc.vector.tensor_tensor(out=sraw[ti][:], in0=gu[ti][:], in1=pss, op=ALU.mult))
                        P['pss_free'] = d3
                        gu_free[ti] = d3
                        yield
                        a4 = ACT(d3, lambda: nc.scalar.activation(out=junk[:, 0:512], in_=sraw[ti][:], func=AF.Square, accum_out=st_[:, 8:9]))
                        yield
                        q4 = ACT(a4, lambda: nc.scalar.activation(out=st_[:, 9:10], in_=st_[:, 8:9], func=AF.Sqrt, bias=EPS, scale=1.0 / 512))
                        yield
                        r5 = DVE(q4, lambda: nc.vector.reciprocal(out=st_[:, 9:10], in_=st_[:, 9:10]))
                        yield
                        c3 = ACT([r5, sn_free[ti]], lambda: nc.scalar.activation(out=sn[ti][:], in_=sraw[ti][:], func=AF.Copy, scale=st_[:, 9:10]))
                        sraw_free[ti] = c3
                        sn_free[ti] = DMA('sn%d' % ti, c3, lambda: nc.sync.dma_start(out=sn_d[tok0:tok0 + 128, :], in_=sn[ti][:]))
                        if t != 3:
                            return
                        yield
                        gs = slice(g * 512, (g + 1) * 512)
                        cq_free[gi] = DMA('cq%d' % gi, G['cq_w'], lambda: nc.sync.dma_start(
                            out=cq_d[s * NG + g], in_=cqnT[gi][:].rearrange("p k c -> p (k c)")))
                        bank_free = [P['pkr_free'], P['pks_free']]
                        banks = [pkr, pks]
                        for h in range(8):
                            bi = h % 2
                            mk = mmg(banks[bi][0:64, :], [(wkv[:, h * 128:h * 128 + 64], ckvnT[gi][:, :])], [G['ckv_w'], bank_free[bi]])
                            if h % 2 == 0:
                                bank_free[bi] = ACT(mk, lambda h=h, bi=bi: nc.scalar.copy(out=KT[0:64, h, gs], in_=banks[bi][0:64, :]))
                            else:
                                bank_free[bi] = DVE(mk, lambda h=h, bi=bi: nc.vector.tensor_copy(out=KT[0:64, h, gs], in_=banks[bi][0:64, :]))
                        wkv3 = wkv[:].rearrange("p (h c) -> p h c", h=8)[:, :, 64:128]
                        mv = None
                        for tt in range(4):
                            bi = tt % 2
                            kb = g * 4 + tt
                            mv = mmg(banks[bi][:, :].rearrange("p (h c) -> p h c", h=8), [(ckvnT[gi][:, tt * 128:(tt + 1) * 128], wkv3)], [bank_free[bi]])
                            bank_free[bi] = DVE(mv, lambda kb=kb, bi=bi: nc.vector.tensor_copy(
                                out=VA[:, kb, :, 0:64], in_=banks[bi][:, :].rearrange("p (h c) -> p h c", h=8)))
                        ckv_free[gi] = mv
                        P['pkr_free'] = bank_free[0]
                        P['pks_free'] = bank_free[1]

                    interleave((p1_tile(g, t) for g in range(NG) for t in range(4)), 2)
                    p1_bar = dp.last() + [sn_free, cq_free]
                    dp.retire_since(mk1)

                mk2 = dp.mark()
                with ExitStack() as p2:
                    def sb2(name, shape, dt): return p2.enter_context(nc.sbuf_tensor(U(name), shape, dt))
                    wq = sb2("wq", [128, 2, 768], BF16)
                    wqs = sb2("wqs", [128, 2, 768], BF16)
                    wo = sb2("wo", [128, 8, 1024], BF16)
                    cqT = [sb2("cqT%d" % i, [128, 2, 512], BF16) for i in range(2)]
                    Ct = sb2("Ct2", [128, 512], F32)
                    St = sb2("St2", [128, 512], F32)
                    qt1 = sb2("qt1", [128, 512], F32)
                    qt2 = sb2("qt2", [128, 512], F32)
                    QT = sb2("QT", [128, 8, 512], BF16)
                    pTs = [sb2("pTs%d" % i, [128, 1024], BF16) for i in range(3)]
                    osb = [sb2("osb%d" % i, [128, 512], F32) for i in range(2)]
                    rinv = [sb2("rinv%d" % i, [128, 512], F32) for i in range(2)]
                    aT = [sb2("aT%d" % i, [128, 512], BF16) for i in range(2)]
                    merged = [sb2("merged%d" % i, [128, D], BF16) for i in range(2)]
                    mT = [sb2("mT%d" % i, [128, 8, 128], BF16) for i in range(2)]
                    otmp = sb2("otmp", [128, D], F32)
                    xr = [sb2("xr%d" % i, [128, D], F32) for i in range(2)]
                    x1 = [sb2("x1_%d" % i, [128, D], F32) for i in range(2)]
                    junk = sb2("junk2", [128, D], BF16)
                    stt = [sb2("stq%d" % i, [128, 16], F32) for i in range(2)]

                    po = PC[:, 0:512]
                    prb = PC[:, 512:1024]
                    pmT = PC[:, 512:1024].bitcast(BF16)
                    pa = PD[:, :].bitcast(BF16)
                    scT = [PA, PB]

                    wl2 = [DMA('b1', p1_bar, lambda: nc.sync.dma_start(out=wq[:].rearrange("p k c -> p (k c)"), in_=wq_d)),
                           DMA('b1', p1_bar, lambda: nc.sync.dma_start(out=wqs[:].rearrange("p k c -> p (k c)"), in_=wqsw_d)),
                           DMA('b1', p1_bar, lambda: nc.sync.dma_start(out=wo[:].rearrange("p k c -> p (k c)"), in_=wo_d))]
                    ready2 = [p1_bar, wl2]
                    cq_free2 = [None, None]; rope_free = None; QT_free = []
                    sc_free = [None, None]; pTs_free = [None, None, None]; po_free = None; osb_free = [None, None]
                    rinv_free = [None, None]; prb_free = None; aT_free = [None, None]; pa_free = []
                    merged_free = [None, None]; mT_free = [None, None]; pmT_free = None
                    otmp_free = None; xr_free = [None, None]; x1_free = [None, None]
                    step = 0
                    tcount = 0
                    for qg in range(NQG[s]):
                        gi = qg % 2
                        gs = slice(qg * 512, (qg + 1) * 512)
                        lq = DMA('cql%d' % gi, [cq_free2[gi], ready2], lambda gi=gi, qg=qg: nc.sync.dma_start(
                            out=cqT[gi][:].rearrange("p k c -> p (k c)"), in_=cq_d[s * NG + qg]))
                        lr1 = DMA('rp2c', [rope_free, ready2], lambda gs=gs: nc.sync.dma_start(out=Ct[64:96, :], in_=rope_c[s, :, gs]))
                        lr2 = DMA('rp2s', [rope_free, ready2], lambda gs=gs: nc.sync.dma_start(out=St[64:96, :], in_=rope_s[s, :, gs]))
                        wq4 = wq[:].rearrange("p k (h c) -> p k h c", h=8)
                        wqs4 = wqs[:].rearrange("p k (h c) -> p k h c", h=8)
                        QT_w = []
                        qd = None
                        for h in range(8):
                            T = scT[h % 2]
                            mq = mmg(T[0:96, 0:512], [(wq4[:, k, h, :], cqT[gi][:, k, :]) for k in range(2)], [lq, sc_free[h % 2]])
                            mqs = mmg(T[0:96, 512:1024], [(wqs4[:, k, h, :], cqT[gi][:, k, :]) for k in range(2)], None)
                            c0 = ACT([mq, QT_free if h == 0 else None], lambda h=h, T=T: nc.scalar.copy(out=QT[0:64, h, :], in_=T[0:64, 0:512]))
                            q1 = DVE([mq, lr1, qd], lambda T=T: nc.vector.tensor_tensor(out=qt1[64:96, :], in0=T[64:96, 0:512], in1=Ct[64:96, :], op=ALU.mult))
                            q2 = DVE([mqs, lr2], lambda T=T: nc.vector.tensor_tensor(out=qt2[64:96, :], in0=T[64:96, 512:1024], in1=St[64:96, :], op=ALU.mult))
                            qd = DVE([q1, q2, QT_free if h == 0 else None], lambda h=h: nc.vector.tensor_tensor(
                                out=QT[64:96, h, :], in0=qt1[64:96, :], in1=qt2[64:96, :], op=ALU.add))
                            sc_free[h % 2] = [c0, q2]
                            QT_w += [c0, qd]
                        cq_free2[gi] = mqs
                        rope_free = qd
                        NP = KB // 2
                        steps = [(h, j) for h in range(8) for j in range(NP)]
                        qk_tok = {}

                        def emit_qk(idx):
                            h, j = steps[idx]
                            T = scT[idx % 2]
                            tk = None
                            for u in range(2):
                                kb = 2 * j + u
                                tk = PE([sc_free[idx % 2], QT_w] if u == 0 else None,
                                        lambda h=h, kb=kb, u=u, T=T: nc.tensor.matmul(
                                            T[:, u * 512:(u + 1) * 512], lhsT=KT[0:96, h, kb * 128:(kb + 1) * 128], rhs=QT[0:96, h, :],
                                            start=True, stop=True), sig=(u == 1))
                            qk_tok[idx] = tk

                        emit_qk(0)
                        pa_w = []
                        QT_readers = []
                        pending = []
                        A = dict(prb_free=prb_free)
                        for idx, (h, j) in enumerate(steps):
                            if idx + 1 < len(steps):
                                emit_qk(idx + 1)
                            T = scT[idx % 2]
                            sl = step % 3
                            step += 1
                            ex = ACT([qk_tok[idx], pTs_free[sl]], lambda T=T, sl=sl: nc.scalar.activation(out=pTs[sl][:], in_=T[:, :], func=AF.Exp))
                            sc_free[idx % 2] = ex
                            pvt = None
                            for u in range(2):
                                kb = 2 * j + u
                                pvt = PE([ex, po_free if (j == 0 and u == 0) else None],
                                         lambda h=h, kb=kb, u=u, sl=sl: nc.tensor.matmul(
                                             po[0:65, :], lhsT=VA[:, kb, h, :], rhs=pTs[sl][:, u * 512:(u + 1) * 512],
                                             start=(kb == 0), stop=(kb == KB - 1)), sig=(u == 1))
                            pTs_free[sl] = pvt
                            for pend in list(pending):
                                pend[0] -= 1
                                if pend[0] <= 0:
                                    pend[1]()
                                    pending.remove(pend)
                            if j == NP - 1:
                                for pend in list(pending):
                                    pend[1]()
                                    pending.remove(pend)
                                oi = h % 2
                                QT_readers.append(pvt)
                                o1 = DVE([pvt, osb_free[oi]], lambda oi=oi: nc.vector.tensor_copy(out=osb[oi][0:65, :], in_=po[0:65, :]))
                                po_free = o1
                                o2 = DVE([o1, rinv_free[oi]], lambda oi=oi: nc.vector.reciprocal(out=rinv[oi][64:65, :], in_=osb[oi][64:65, :]))
                                hs = dict(o2=o2, oi=oi, h=h)

                                def part_a(hs=hs):
                                    oi = hs['oi']
                                    o3 = PE([hs['o2'], A['prb_free']], lambda oi=oi: nc.tensor.matmul(prb[0:64, :], lhsT=ones_f[64:65, 0:64], rhs=rinv[oi][64:65, :],
                                                                                                 start=True, stop=True))
                                    rinv_free[oi] = o3
                                    o4 = DVE([o3, aT_free[oi]], lambda oi=oi: nc.vector.tensor_tensor(out=aT[oi][0:64, :], in0=osb[oi][0:64, :],
                                                                                                       in1=prb[0:64, :], op=ALU.mult))
                                    A['prb_free'] = o4
                                    osb_free[oi] = o4
                                    hs['o4'] = o4

                                def part_b(hs=hs):
                                    oi = hs['oi']; h = hs['h']
                                    o5 = None
                                    for t in range(4):
                                        o5 = PE([hs['o4'], pa_free if h == 0 else None] if t == 0 else None,
                                                lambda t=t, h=h, oi=oi: nc.tensor.transpose(
                                                    pa[:, t * 512 + h * 64: t * 512 + (h + 1) * 64], aT[oi][0:64, t * 128:(t + 1) * 128], ident_b[0:64, 0:64]),
                                                sig=(t == 3))
                                    aT_free[oi] = o5
                                    pa_w.append(o5)
                                pending.append([2, part_a])
                                pending.append([4, part_b])
                        for pend in list(pending):
                            pend[1]()
                            pending.remove(pend)
                        prb_free = A['prb_free']
                        QT_free = QT_readers
                        pa_r = []
                        for t in range(4):
                            ti = tcount % 2
                            tcount += 1
                            tok0 = s * S + qg * 512 + t * 128
                            otok0 = qbase + qg * 512 + t * 128
                            st_ = stt[ti]
                            lsn = DMA('snl%d' % ti, [merged_free[ti], ready2], lambda ti=ti, tok0=tok0: nc.sync.dma_start(
                                out=merged[ti][:, 512:1024], in_=sn_d[tok0:tok0 + 128, :]))
                            lxr = DMA('xr%d' % ti, [xr_free[ti], ready2], lambda ti=ti, tok0=tok0: nc.sync.dma_start(
                                out=xr[ti][:], in_=xs[tok0:tok0 + 128, :]))
                            a1 = ACT(pa_w, lambda t=t, st_=st_: nc.scalar.activation(out=junk[:, 0:512], in_=pa[:, t * 512:(t + 1) * 512], func=AF.Square,
                                                                                     accum_out=st_[:, 0:1]))
                            r1 = rstd_chain(st_[:, 0:1], st_[:, 1:2], 1.0 / 512, a1)
                            c1 = ACT([r1, merged_free[ti]], lambda t=t, ti=ti, st_=st_: nc.scalar.activation(
                                out=merged[ti][:, 0:512], in_=pa[:, t * 512:(t + 1) * 512], func=AF.Copy, scale=st_[:, 1:2]))
                            pa_r.append(c1)
                            tp = None
                            for k in range(8):
                                tp = PE([c1, lsn, pmT_free, prb_free] if k == 0 else None,
                                        lambda k=k, ti=ti: nc.tensor.transpose(pmT[:, k * 128:(k + 1) * 128], merged[ti][:, k * 128:(k + 1) * 128], ident_b[:]),
                                        sig=(k == 7))
                            merged_free[ti] = tp
                            ev = DVE([tp, mT_free[ti]], lambda ti=ti: nc.vector.tensor_copy(out=mT[ti][:].rearrange("p k c -> p (k c)"), in_=pmT))
                            pmT_free = ev
                            prb_free = ev
                            T = scT[t % 2]
                            mo1 = mmg(T[:, 0:512], [(mT[ti][:, k, :], wo[:, k, 0:512]) for k in range(8)], [ev, sc_free[t % 2]])
                            mo2 = mmg(T[:, 512:1024], [(mT[ti][:, k, :], wo[:, k, 512:1024]) for k in range(8)], None)
                            mT_free[ti] = mo2
                            a2 = ACT(mo2, lambda T=T, st_=st_: nc.scalar.activation(out=junk[:], in_=T[:, :], func=AF.Square, accum_out=st_[:, 2:3]))
                            r2 = rstd_chain(st_[:, 2:3], st_[:, 3:4], 1.0 / D, a2)
                            d1 = DVE([r2, otmp_free], lambda T=T, st_=st_: nc.vector.scalar_tensor_tensor(
                                out=otmp[:], in0=T[:, :], scalar=st_[:, 3:4], in1=gvec1[:], op0=ALU.mult, op1=ALU.mult))
                            sc_free[t % 2] = d1
                            pp = POOL([d1, lxr, x1_free[ti]], lambda ti=ti: nc.gpsimd.tensor_tensor(out=x1[ti][:], in0=otmp[:], in1=xr[ti][:], op=ALU.add))
                            otmp_free = pp
                            xr_free[ti] = pp
                            x1_free[ti] = DMA('x1s%d' % ti, pp, lambda ti=ti, otok0=otok0: nc.sync.dma_start(
                                out=x1_d[otok0:otok0 + 128, :], in_=x1[ti][:]))
                        pa_free = pa_r
                    p2_bar = dp.last() + [x1_free]
                    dp.retire_since(mk2)
                seq_bar = p2_bar
                qbase += NQG[s] * 512
            stageA_bar = seq_bar

        NTT = NQT // 128
        gseq = []
        for s in range(NSEQ):
            gseq += [s] * (NQG[s] * 4)
        with ExitStack() as bes:
            def sbB(name, shape, dt): return bes.enter_context(nc.sbuf_tensor(U(name), shape, dt))
            geff2_ = [sbB("geff2_%d" % i, [128, D], F32) for i in range(2)]
            sh2r_ = [sbB("sh2r_%d" % i, [128, D], F32) for i in range(2)]
            gvec2_ = [sbB("gvec2_%d" % i, [128, D], F32) for i in range(2)]
            vt = [sbB("vtB%d" % i, [128, D], F32) for i in range(2)]
            M1a = sbB("M1a", [128, NTT, 32], F32)
            M2a = sbB("M2a", [128, NTT, 32], F32)
            W1a = sbB("W1a", [128, NTT], F32)
            W2a = sbB("W2a", [128, NTT], F32)
            R1a = sbB("R1a", [128, NTT], F32)
            R2a = sbB("R2a", [128, NTT], F32)
            slot0 = sbB("slot0", [128, NTT], I32)
            slot1 = sbB("slot1", [128, NTT], I32)
            idxw = sbB("idxw", [128, NSL], I32)
            carry = sbB("carry", [128, 32], F32)
            PS = [bes.enter_context(nc.psum_tensor(U("PS%d" % i), [128, 512], F32)) for i in range(8)]
            bb = stageA_bar
            readyB = [bb, wcast_tok, zero_tok]

            def load_vecs(s, deps):
                geff2 = geff2_[s % 2]; sh2r = sh2r_[s % 2]; gvec2 = gvec2_[s % 2]
                l1 = DMA('v0_0', deps, lambda s=s: nc.sync.dma_start(out=vt[0][:], in_=mod_d[s, 4096:5120].partition_broadcast(128)))
                l2 = DMA('v0_1', deps, lambda: nc.sync.dma_start(out=vt[1][:], in_=g_pre2.partition_broadcast(128)))
                l3 = DMA('v0_2', deps, lambda s=s: nc.sync.dma_start(out=sh2r[:], in_=mod_d[s, 3072:4096].partition_broadcast(128)))
                tg = DVE([l1, l2], lambda: nc.vector.scalar_tensor_tensor(out=geff2[:], in0=vt[0][:], scalar=1.0, in1=vt[1][:],
                                                                           op0=ALU.add, op1=ALU.mult))
                l4 = DMA('v0_3', [tg], lambda s=s: nc.sync.dma_start(out=vt[0][:], in_=mod_d[s, 5120:6144].partition_broadcast(128)))
                l5 = DMA('v0_4', [tg], lambda: nc.sync.dma_start(out=vt[1][:], in_=g_post2.partition_broadcast(128)))
                tg2 = DVE([l4, l5], lambda: nc.vector.tensor_tensor(out=gvec2[:], in0=vt[0][:], in1=vt[1][:], op=ALU.mult))
                return [l3, tg, tg2]

            mkb1 = dp.mark()
            with ExitStack() as b1:
                def sb1(name, shape, dt): return b1.enter_context(nc.sbuf_tensor(U(name), shape, dt))
                w_r = sb1("w_r", [128, 8, 36], F32)
                brr = sb1("brr", [128, 36], F32)
                utri = sb1("utri", [128, 128], BF16)
                onesb = sb1("onesb", [128, 128], BF16)
                x1t = [sb1("x1t%d" % i, [128, D], F32) for i in range(3)]
                junk = sb1("junkB", [128, D], BF16)
                hm = sb1("hmB", [128, D], F32)
                h2 = [sb1("h2_%d" % i, [128, D], F32) for i in range(3)]
                h2Tf = [sb1("h2Tf%d" % i, [128, 8, 128], F32) for i in range(3)]
                stt = [sb1("stB%d" % i, [128, 8], F32) for i in range(3)]
                lg = [sb1("lg%d" % i, [128, 36], F32) for i in range(3)]
                wk = [sb1("wk%d" % i, [128, 192], F32) for i in range(3)]
                ohb = [sb1("ohb%d" % i, [128, 32], BF16) for i in range(3)]
                PH = [PS[6], PS[7]]
                ld = [DMA('s0', bb, lambda: nc.sync.dma_start(out=w_r[:, :, 0:4], in_=w_rg.rearrange("(k p) c -> p k c", p=128))),
                      DMA('s0', bb, lambda: nc.sync.dma_start(out=w_r[:, :, 4:36], in_=w_re.rearrange("(k p) c -> p k c", p=128))),
                      DMA('s0', bb, lambda: nc.sync.dma_start(out=brr[:, 0:4], in_=b_rg.partition_broadcast(128))),
                      DMA('s0', bb, lambda: nc.sync.dma_start(out=brr[:, 4:36], in_=b_re.partition_broadcast(128))),
                      DMA('s1', bb, lambda: nc.gpsimd.dma_start(out=utri[:], in_=utri_in), q='pool'),
                      POOL(bb, lambda: nc.gpsimd.memset(onesb[:], 1.0)),
                      POOL(bb, lambda: nc.gpsimd.memset(carry[:], 0.0))]
                rdy1 = [readyB, ld]
                T = dict(cur_seq=-1, vec_ready=None, vec_readers=[], hm_free=None, PH_free=[None, None], plg_free=None,
                         pcum_free=None, carry_tok=ld[-1])
                x1t_free = [None] * 3; h2_free = [None] * 3
                h2Tf_free = [None] * 3
                h2d_w = []

                def p1_tile(i):
                    s = gseq[i]
                    if s != T['cur_seq']:
                        T['cur_seq'] = s
                        T['vec_ready'] = load_vecs(s, [rdy1, T['vec_readers']])
                        T['vec_readers'] = []
                    vec_ready = T['vec_ready']
                    geff2 = geff2_[s % 2]; sh2r = sh2r_[s % 2]
                    ti = i % 3
                    tok0 = i * 128
                    st_ = stt[ti]
                    lx = DMA('bx%d' % ti, [x1t_free[ti], rdy1], lambda ti=ti, tok0=tok0: nc.sync.dma_start(out=x1t[ti][:], in_=x1_d[tok0:tok0 + 128, :]))
                    a1 = ACT(lx, lambda ti=ti, st_=st_: nc.scalar.activation(out=junk[:], in_=x1t[ti][:], func=AF.Square, accum_out=st_[:, 0:1]))
                    yield
                    r1a = ACT(a1, lambda st_=st_: nc.scalar.activation(out=st_[:, 1:2], in_=st_[:, 0:1], func=AF.Sqrt, bias=EPS, scale=1.0 / D))
                    yield
                    r1 = DVE(r1a, lambda st_=st_: nc.vector.reciprocal(out=st_[:, 1:2], in_=st_[:, 1:2]))
                    d1 = DVE([r1, T['hm_free'], vec_ready], lambda ti=ti, st_=st_: nc.vector.scalar_tensor_tensor(
                        out=hm[:], in0=x1t[ti][:], scalar=st_[:, 1:2], in1=geff2[:], op0=ALU.mult, op1=ALU.mult))
                    x1t_free[ti] = d1
                    p1_ = POOL([d1, h2_free[ti], vec_ready], lambda ti=ti: nc.gpsimd.tensor_tensor(out=h2[ti][:], in0=hm[:], in1=sh2r[:], op=ALU.add))
                    T['hm_free'] = p1_
                    T['vec_readers'] = [p1_, d1]
                    wr = DMA('h2w%d' % ti, p1_, lambda ti=ti, tok0=tok0: nc.gpsimd.dma_start(out=h2_d[tok0:tok0 + 128, :], in_=h2[ti][:]), q='pool')
                    h2d_w.append(wr)
                    yield
                    tp = None
                    for k in range(8):
                        bank = PH[k // 4]
                        tp = PE([p1_, T['PH_free']] if k == 0 else None,
                                lambda k=k, ti=ti, bank=bank: nc.tensor.transpose(bank[:, (k % 4) * 128:(k % 4 + 1) * 128],
                                                                                  h2[ti][:, k * 128:(k + 1) * 128], ident_f[:]),
                                sig=(k == 7))
                    h2_free[ti] = [tp, wr]
                    yield
                    e1 = ACT([tp, h2Tf_free[ti]], lambda ti=ti: nc.scalar.copy(out=h2Tf[ti][:, 0:4, :], in_=PH[0][:, :].rearrange("p (k c) -> p k c", k=4)))
                    e2 = DVE([tp, h2Tf_free[ti]], lambda ti=ti: nc.vector.tensor_copy(out=h2Tf[ti][:, 4:8, :], in_=PH[1][:, :].rearrange("p (k c) -> p k c", k=4)))
                    T['PH_free'] = [e1, e2]
                    yield
                    plg = PS[4][:, 0:36]
                    m_l = mmg(plg, [(h2Tf[ti][:, k, :], w_r[:, k, :]) for k in range(8)], [e1, e2, T['plg_free'], rdy1])
                    h2Tf_free[ti] = m_l
                    yield
                    L = lg[ti]; W = wk[ti]
                    v1 = DVE([m_l], lambda L=L: nc.vector.tensor_tensor(out=L[:], in0=plg, in1=brr[:], op=ALU.add))
                    T['plg_free'] = v1
                    v2 = DVE(v1, lambda L=L, W=W: nc.vector.tensor_reduce(out=W[:, 0:1], in_=L[:, 0:4], axis=AX.X, op=ALU.max))
                    v3 = DVE(v2, lambda W=W: nc.vector.tensor_scalar(out=W[:, 1:2], in0=W[:, 0:1], scalar1=-1.0, scalar2=None, op0=ALU.mult))
                    v4 = DVE(v2, lambda L=L, W=W: nc.vector.tensor_scalar(out=W[:, 4:8], in0=L[:, 0:4], scalar1=W[:, 0:1], scalar2=None, op0=ALU.is_equal))
                    s1 = ACT([v3], lambda L=L, W=W: nc.scalar.activation(out=W[:, 8:12], in_=L[:, 0:4], func=AF.Exp, bias=W[:, 1:2], scale=1.0,
                                                                         accum_out=W[:, 2:3]))
                    yield
                    v5 = DVE(s1, lambda W=W: nc.vector.reciprocal(out=W[:, 3:4], in_=W[:, 2:3]))
                    v6 = DVE(v4, lambda L=L, W=W: nc.vector.tensor_tensor(
                        out=W[:, 16:48].rearrange("p (g e) -> p g e", g=4), in0=L[:, 4:36].rearrange("p (g e) -> p g e", g=4),
                        in1=W[:, 4:8].unsqueeze(2).to_broadcast([128, 4, 8]), op=ALU.mult))
                    v7 = DVE(v6, lambda W=W: nc.vector.tensor_reduce(out=W[:, 48:56], in_=W[:, 16:48].rearrange("p (g e) -> p e g", g=4),
                                                                    axis=AX.X, op=ALU.add))
                    v8 = DVE(v7, lambda W=W: nc.vector.tensor_reduce(out=W[:, 12:13], in_=W[:, 48:56], axis=AX.X, op=ALU.max))
                    v9 = DVE(v8, lambda W=W: nc.vector.tensor_scalar(out=W[:, 56:64], in0=W[:, 48:56], scalar1=W[:, 12:13], scalar2=None,
                                                                    op0=ALU.is_equal))
                    v10 = DVE(v9, lambda W=W: nc.vector.scalar_tensor_tensor(out=W[:, 64:72], in0=W[:, 56:64], scalar=-1e30, in1=W[:, 48:56],
                                                                            op0=ALU.mult, op1=ALU.add))
                    v11 = DVE(v10, lambda W=W: nc.vector.tensor_reduce(out=W[:, 13:14], in_=W[:, 64:72], axis=AX.X, op=ALU.max))
                    v12 = DVE(v11, lambda W=W: nc.vector.tensor_scalar(out=W[:, 72:80], in0=W[:, 64:72], scalar1=W[:, 13:14], scalar2=None,
                                                                      op0=ALU.is_equal))
                    v13 = DVE(v11, lambda W=W: nc.vector.tensor_scalar(out=W[:, 14:15], in0=W[:, 12:13], scalar1=-1.0, scalar2=None, op0=ALU.mult))
                    s2 = ACT([v13], lambda W=W: nc.scalar.activation(out=W[:, 15:16], in_=W[:, 13:14], func=AF.Exp, bias=W[:, 14:15], scale=1.0))
                    yield
                    v14 = DVE(s2, lambda W=W: nc.vector.tensor_scalar(out=W[:, 80:81], in0=W[:, 15:16], scalar1=1.0, scalar2=None, op0=ALU.add))
                    v15 = DVE(v14, lambda W=W: nc.vector.reciprocal(out=W[:, 81:82], in_=W[:, 80:81]))
                    v16 = DVE([v15, v5], lambda W=W, i=i: nc.vector.tensor_tensor(out=W1a[:, i:i + 1], in0=W[:, 81:82], in1=W[:, 3:4], op=ALU.mult))
                    v17 = DVE(v16, lambda W=W, i=i: nc.vector.tensor_tensor(out=W2a[:, i:i + 1], in0=W1a[:, i:i + 1], in1=W[:, 15:16], op=ALU.mult))
                    v18 = DVE([v9, v4], lambda W=W, i=i: nc.vector.tensor_tensor(
                        out=M1a[:, i, :].rearrange("p (g e) -> p g e", g=4), in0=W[:, 4:8].unsqueeze(2).to_broadcast([128, 4, 8]),
                        in1=W[:, 56:64].unsqueeze(1).to_broadcast([128, 4, 8]), op=ALU.mult))
                    v19 = DVE([v12], lambda W=W, i=i: nc.vector.tensor_tensor(
                        out=M2a[:, i, :].rearrange("p (g e) -> p g e", g=4), in0=W[:, 4:8].unsqueeze(2).to_broadcast([128, 4, 8]),
                        in1=W[:, 72:80].unsqueeze(1).to_broadcast([128, 4, 8]), op=ALU.mult))
                    OH = ohb[ti]
                    v20 = DVE([v18, v19, T['pcum_free']], lambda OH=OH, i=i: nc.vector.tensor_tensor(out=OH[:], in0=M1a[:, i, :], in1=M2a[:, i, :], op=ALU.add))
                    pcum = PS[5][:, 0:32]
                    ptot = PS[5][:, 32:64]
                    PE([v20, T['pcum_free'], rdy1], lambda OH=OH: nc.tensor.matmul(pcum, lhsT=utri[:], rhs=OH[:], start=True, stop=True), sig=False)
                    mc = PE(None, lambda OH=OH: nc.tensor.matmul(ptot, lhsT=onesb[:], rhs=OH[:], start=True, stop=True))
                    yield
                    v21 = DVE([mc, T['carry_tok']], lambda W=W: nc.vector.tensor_tensor(out=W[:, 96:128], in0=carry[:], in1=pcum, op=ALU.add))
                    v22 = DVE(v21, lambda: nc.vector.tensor_tensor(out=carry[:], in0=carry[:], in1=ptot, op=ALU.add))
                    T['carry_tok'] = v22
                    T['pcum_free'] = v22
                    v23 = DVE(v22, lambda W=W, i=i: nc.vector.tensor_tensor(out=W[:, 128:160], in0=W[:, 96:128], in1=M1a[:, i, :], op=ALU.mult))
                    v24 = DVE(v23, lambda W=W, i=i: nc.vector.tensor_reduce(out=R1a[:, i:i + 1], in_=W[:, 128:160], axis=AX.X, op=ALU.add))
                    v25 = DVE(v24, lambda W=W, i=i: nc.vector.tensor_tensor(out=W[:, 160:192], in0=W[:, 96:128], in1=M2a[:, i, :], op=ALU.mult))
                    v26 = DVE(v25, lambda W=W, i=i: nc.vector.tensor_reduce(out=R2a[:, i:i + 1], in_=W[:, 160:192], axis=AX.X, op=ALU.add))
                interleave((p1_tile(i) for i in range(NTT)), 3)
                b1_bar = dp.last() + [h2d_w]
                dp.retire_since(mkb1)

            with ExitStack() as b2:
                def sb2(name, shape, dt): return b2.enter_context(nc.sbuf_tensor(U(name), shape, dt))
                jv = sb2("jv", [128, NSL], F32)
                pidx = sb2("pidx", [128, 1], F32)
                tri32 = sb2("tri32", [32, 32], F32)
                cmp_ = sb2("cmp", [128, NSL * 32], F32)
                tmpM = sb2("tmpM", [128, NTT, 32], F32)
                nblk = sb2("nblk", [128, 32], F32)
                pc = sb2("pc", [128, 32], F32)
                pcT = sb2("pcT", [32, 128], F32)
                sst = sb2("sst", [128, 32], F32)
                send = sb2("send", [128, 32], F32)
                te = sb2("te", [128, NSL], F32)
                sf = sb2("sf", [128, NTT], F32)
                l = [DMA('i0', b1_bar, lambda: nc.sync.dma_start(out=jv[:], in_=jv_in)),
                     DMA('i0', b1_bar, lambda: nc.sync.dma_start(out=pidx[:], in_=pidx_in)),
                     DMA('i0', b1_bar, lambda: nc.sync.dma_start(out=tri32[:], in_=tri32_in))]
                c3 = cmp_[:].rearrange("p (e m) -> p e m", e=32)
                q1 = DVE([l, b1_bar], lambda: nc.vector.tensor_tensor(out=c3, in0=jv[:].unsqueeze(1).to_broadcast([128, 32, NSL]),
                                                                      in1=carry[:].unsqueeze(2).to_broadcast([128, 32, NSL]), op=ALU.is_lt))
                q2 = DVE(q1, lambda: nc.vector.tensor_reduce(out=nblk[:], in_=c3, axis=AX.X, op=ALU.add))
                q3 = DVE(q2, lambda: nc.vector.tensor_scalar(out=pc[:], in0=nblk[:], scalar1=128.0, scalar2=None, op0=ALU.mult))
                q4 = PE(q3, lambda: nc.tensor.transpose(PS[0][0:32, 0:128], pc[:, :], ident_f[:]))
                q5 = ACT(q4, lambda: nc.scalar.copy(out=pcT[:], in_=PS[0][0:32, 0:128]))
                q6 = PE([q5, l], lambda: nc.tensor.matmul(PS[1][:, 0:32], lhsT=pcT[:, :], rhs=tri32[:, :], start=True, stop=True))
                q7 = DVE(q6, lambda: nc.vector.tensor_copy(out=sst[:], in_=PS[1][:, 0:32]))
                q8 = DVE(q7, lambda: nc.vector.tensor_tensor(out=send[:], in0=sst[:], in1=pc[:], op=ALU.add))
                c4 = cmp_[:].rearrange("p (m e) -> p m e", e=32)
                q9 = DVE(q8, lambda: nc.vector.tensor_tensor(out=c4, in0=send[:].unsqueeze(1).to_broadcast([128, NSL, 32]),
                                                             in1=jv[:].unsqueeze(2).to_broadcast([128, NSL, 32]), op=ALU.is_le))
                q10 = DVE(q9, lambda: nc.vector.tensor_reduce(out=te[:], in_=c4, axis=AX.X, op=ALU.add))
                q11 = DVE(q10, lambda: nc.vector.tensor_scalar(out=te[:], in0=te[:], scalar1=31.0, scalar2=128.0, op0=ALU.min, op1=ALU.mult))
                q12 = DVE(q11, lambda: nc.vector.tensor_scalar(out=te[:], in0=te[:], scalar1=pidx[:, 0:1], scalar2=None, op0=ALU.add))
                q13 = DVE(q12, lambda: nc.vector.tensor_copy(out=idxw[:], in_=te[:]))
                q14 = DVE(q7, lambda: nc.vector.tensor_tensor(out=tmpM[:], in0=M1a[:], in1=sst[:].unsqueeze(1).to_broadcast([128, NTT, 32]), op=ALU.mult))
                q15 = DVE(q14, lambda: nc.vector.tensor_reduce(out=sf[:], in_=tmpM[:], axis=AX.X, op=ALU.add))
                q16 = DVE(q15, lambda: nc.vector.tensor_tensor(out=sf[:], in0=sf[:], in1=R1a[:], op=ALU.add))
                q17 = DVE(q16, lambda: nc.vector.tensor_copy(out=slot0[:], in_=sf[:]))
                q18 = DVE(q17, lambda: nc.vector.tensor_tensor(out=tmpM[:], in0=M2a[:], in1=sst[:].unsqueeze(1).to_broadcast([128, NTT, 32]), op=ALU.mult))
                q19 = DVE(q18, lambda: nc.vector.tensor_reduce(out=sf[:], in_=tmpM[:], axis=AX.X, op=ALU.add))
                q20 = DVE(q19, lambda: nc.vector.tensor_tensor(out=sf[:], in0=sf[:], in1=R2a[:], op=ALU.add))
                q21 = DVE(q20, lambda: nc.vector.tensor_copy(out=slot1[:], in_=sf[:]))
                b2_bar = dp.last()

            mkb3 = dp.mark()
            with ExitStack() as b3:
                def sb3(name, shape, dt): return b3.enter_context(nc.sbuf_tensor(U(name), shape, dt))
                hsc = [sb3("hsc%d" % i, [128, D], BF16) for i in range(3)]
                hsc_free = [None] * 3
                sc_toks = []
                for i in range(NTT):
                    si = i % 3
                    tok0 = i * 128
                    lh = DMA('hl%d' % si, [hsc_free[si], b2_bar], lambda si=si, tok0=tok0: nc.sync.dma_start(out=hsc[si][:], in_=h2_d[tok0:tok0 + 128, :]))
                    s0 = DMA('sc%d' % si, [lh, b2_bar], lambda si=si, i=i: nc.gpsimd.indirect_dma_start(
                        out=xs_d[:, :], out_offset=bass.IndirectOffsetOnAxis(ap=slot0[:, i:i + 1], axis=0), in_=hsc[si][:, :], in_offset=None), q='pool')
                    s1_ = DMA('sc%d' % si, [lh], lambda si=si, i=i: nc.gpsimd.indirect_dma_start(
                        out=xs_d[:, :], out_offset=bass.IndirectOffsetOnAxis(ap=slot1[:, i:i + 1], axis=0), in_=hsc[si][:, :], in_offset=None), q='pool')
                    hsc_free[si] = [s0, s1_]
                    sc_toks += [s0, s1_]
                scat_done = [sc_toks[-6:], b2_bar]

                PF = 3
                NW = PF + 3
                ND = PF + 5
                NX = PF + 2
                wgu = [sb3("wgu%d" % i, [128, 8, 512], BF16) for i in range(NW)]
                wdb = [sb3("wdb%d" % i, [128, 2, D], BF16) for i in range(ND)]
                xsb = [sb3("xsb%d" % i, [128, D], BF16) for i in range(NX)]
                xT = [sb3("xT%d" % i, [128, 8, 128], BF16) for i in range(2)]
                sgs = [sb3("sgs%d" % i, [128, 256], F32) for i in range(2)]
                hid = [sb3("hid%d" % i, [128, 256], BF16) for i in range(2)]
                hT = [sb3("hT%d" % i, [128, 2, 128], BF16) for i in range(2)]
                ysb = [sb3("ysb%d" % i, [128, D], F32) for i in range(2)]
                pX = [PS[0][:, :].bitcast(BF16), PS[1][:, :].bitcast(BF16)]
                pH = [PS[2], PS[3]]
                pHT = [PS[4][:, 0:128].bitcast(BF16), PS[5][:, 0:128].bitcast(BF16)]
                pY = [PS[6], PS[7]]
                wgu_free = [None] * NW; wdb_free = [None] * ND; xsb_free = [None] * NX
                pX_free = [None, None]; xT_free = [None, None]; pH_free = [None, None]; sgs_free = [None, None]
                hid_free = [None, None]; pHT_free = [None, None]; hT_free = [None, None]
                pY_free = [None, None]; ysb_free = [None, None]
                st0 = {}; st1 = {}; st2 = {}; ldt = {}
                ys_w = []

                def issue_loads(a):
                    wi = a % NW; di = a % ND; xj = a % NX
                    lw = DMA('wgl%d' % wi, [wgu_free[wi], scat_done], lambda wi=wi, a=a: nc.gpsimd.indirect_dma_start(
                        out=wgu[wi][:].rearrange("p k c -> p (k c)"), out_offset=None, in_=wgu_r[:, :],
                        in_offset=bass.IndirectOffsetOnAxis(ap=idxw[:, a:a + 1], axis=0)), q='pool')
                    lwd = DMA('wdl%d' % di, [wdb_free[di], scat_done], lambda di=di, a=a: nc.gpsimd.indirect_dma_start(
                        out=wdb[di][:].rearrange("p k c -> p (k c)"), out_offset=None, in_=wd_r[:, :],
                        in_offset=bass.IndirectOffsetOnAxis(ap=idxw[:, a:a + 1], axis=0)), q='pool')
                    lxs = DMA('xsl%d' % xj, [xsb_free[xj], scat_done, sc_toks], lambda xj=xj, a=a: nc.sync.dma_start(
                        out=xsb[xj][:], in_=xs_d[a * 128:(a + 1) * 128, :]))
                    ldt[a] = (lw, lwd, lxs)

                for a in range(min(PF, NSL)):
                    issue_loads(a)
                for it in range(NSL + 3):
                    if it + PF < NSL:
                        issue_loads(it + PF)
                    a = it
                    if a < NSL:
                        xi = a % 2; xj = a % NX
                        lw, lwd, lxs = ldt[a]
                        tp = None
                        for k in range(8):
                            tp = PE([lxs, pX_free[xi]] if k == 0 else None,
                                    lambda k=k, xi=xi, xj=xj: nc.tensor.transpose(pX[xi][:, k * 128:(k + 1) * 128], xsb[xj][:, k * 128:(k + 1) * 128], ident_b[:]),
                                    sig=(k == 7))
                        xsb_free[xj] = tp
                        if a % 2 == 0:
                            ev = ACT([tp, xT_free[xi]], lambda xi=xi: nc.scalar.copy(out=xT[xi][:].rearrange("p k c -> p (k c)"), in_=pX[xi]))
                        else:
                            ev = DVE([tp, xT_free[xi]], lambda xi=xi: nc.vector.tensor_copy(out=xT[xi][:].rearrange("p k c -> p (k c)"), in_=pX[xi]))
                        pX_free[xi] = ev
                        st0[a] = (ev, lw, lwd)
                    a = it - 1
                    if 0 <= a < NSL:
                        wi = a % NW; xi = a % 2
                        ev, lw, lwd = st0[a]
                        mh = mmg(pH[xi][:, :], [(xT[xi][:, k, :], wgu[wi][:, k, :]) for k in range(8)], [ev, lw, pH_free[xi]])
                        wgu_free[wi] = mh
                        xT_free[xi] = mh
                        a_s = ACT([mh, sgs_free[xi]], lambda xi=xi: nc.scalar.activation(out=sgs[xi][:], in_=pH[xi][:, 0:256], func=AF.Silu))
                        d_h = DVE([a_s, hid_free[xi]], lambda xi=xi: nc.vector.tensor_tensor(out=hid[xi][:], in0=sgs[xi][:], in1=pH[xi][:, 256:512], op=ALU.mult))
                        pH_free[xi] = d_h
                        sgs_free[xi] = d_h
                        st1[a] = (d_h, lwd)
                    a = it - 2
                    if 0 <= a < NSL:
                        xi = a % 2
                        d_h, lwd = st1[a]
                        tp2 = None
                        for j in range(2):
                            tp2 = PE([d_h, pHT_free[xi]] if j == 0 else None,
                                     lambda j=j, xi=xi: nc.tensor.transpose(pHT[xi][:, j * 128:(j + 1) * 128], hid[xi][:, j * 128:(j + 1) * 128], ident_b[:]),
                                     sig=(j == 1))
                        hid_free[xi] = tp2
                        ev2 = ACT([tp2, hT_free[xi]], lambda xi=xi: nc.scalar.copy(out=hT[xi][:].rearrange("p k c -> p (k c)"), in_=pHT[xi]))
                        pHT_free[xi] = ev2
                        st2[a] = (ev2, lwd)
                    a = it - 3
                    if 0 <= a < NSL:
                        xi = a % 2; di = a % ND
                        ev2, lwd = st2[a]
                        my0 = mmg(pY[0][:, :], [(hT[xi][:, j, :], wdb[di][:, j, 0:512]) for j in range(2)], [ev2, lwd, pY_free[0]])
                        my1 = mmg(pY[1][:, :], [(hT[xi][:, j, :], wdb[di][:, j, 512:1024]) for j in range(2)], [pY_free[1]])
                        wdb_free[di] = my1
                        hT_free[xi] = my1
                        c0 = ACT([my0, ysb_free[xi]], lambda xi=xi: nc.scalar.copy(out=ysb[xi][:, 0:512], in_=pY[0][:, :]))
                        c1 = DVE([my1, ysb_free[xi]], lambda xi=xi: nc.vector.tensor_copy(out=ysb[xi][:, 512:1024], in_=pY[1][:, :]))
                        pY_free = [c0, c1]
                        ysb_free[xi] = DMA('ysw%d' % xi, [c0, c1], lambda xi=xi, a=a: nc.sync.dma_start(out=ys_d[a * 128:(a + 1) * 128, :], in_=ysb[xi][:]))
                        ys_w.append(ysb_free[xi])
                b3_bar = dp.last() + [ys_w[-2:]]
                dp.retire_since(mkb3)

            with ExitStack() as b4:
                def sb4(name, shape, dt): return b4.enter_context(nc.sbuf_tensor(U(name), shape, dt))
                ya = [sb4("ya%d" % i, [128, D], F32) for i in range(3)]
                yb = [sb4("yb%d" % i, [128, D], F32) for i in range(3)]
                x1c = [sb4("x1c%d" % i, [128, D], F32) for i in range(3)]
                mm_ = [sb4("mm_%d" % i, [128, D], F32) for i in range(3)]
                ytmp = [sb4("ytmp%d" % i, [128, D], F32) for i in range(3)]
                yo = [sb4("yo%d" % i, [128, D], F32) for i in range(3)]
                junk = sb4("junkC", [128, D], BF16)
                stt = [sb4("stC%d" % i, [128, 8], F32) for i in range(3)]
                ya_free = [None] * 3; yb_free = [None] * 3; x1c_free = [None] * 3; mm_free = [None] * 3
                ytmp_free = [None] * 3; yo_free = [None] * 3
                T = dict(cur_seq=-1, vec_ready=None, vec_readers=[])
                out_toks = []

                def cmb_tile(i):
                    s = gseq[i]
                    if s != T['cur_seq']:
                        T['cur_seq'] = s
                        T['vec_ready'] = load_vecs(s, [b3_bar, T['vec_readers']])
                        T['vec_readers'] = []
                    vec_ready = T['vec_ready']
                    gvec2 = gvec2_[s % 2]
                    ti = i % 3
                    tok0 = i * 128
                    st_ = stt[ti]
                    ga = DMA('ga%d' % ti, [ya_free[ti], b3_bar, ys_w], lambda ti=ti, i=i: nc.gpsimd.indirect_dma_start(
                        out=ya[ti][:, :], out_offset=None, in_=ys_d[:, :], in_offset=bass.IndirectOffsetOnAxis(ap=slot0[:, i:i + 1], axis=0)), q='pool')
                    gb_ = DMA('gb%d' % ti, [yb_free[ti], b3_bar], lambda ti=ti, i=i: nc.gpsimd.indirect_dma_start(
                        out=yb[ti][:, :], out_offset=None, in_=ys_d[:, :], in_offset=bass.IndirectOffsetOnAxis(ap=slot1[:, i:i + 1], axis=0)), q='pool')
                    lx = DMA('cx%d' % ti, [x1c_free[ti], b3_bar], lambda ti=ti, tok0=tok0: nc.sync.dma_start(out=x1c[ti][:], in_=x1_d[tok0:tok0 + 128, :]))
                    yield
                    d1 = DVE([ga, mm_free[ti]], lambda ti=ti, i=i: nc.vector.tensor_scalar(out=mm_[ti][:], in0=ya[ti][:], scalar1=W1a[:, i:i + 1], scalar2=None, op0=ALU.mult))
                    ya_free[ti] = d1
                    d2 = DVE([gb_, d1], lambda ti=ti, i=i: nc.vector.scalar_tensor_tensor(out=mm_[ti][:], in0=yb[ti][:], scalar=W2a[:, i:i + 1], in1=mm_[ti][:],
                                                                                         op0=ALU.mult, op1=ALU.add))
                    yb_free[ti] = d2
                    a1 = ACT(d2, lambda ti=ti, st_=st_: nc.scalar.activation(out=junk[:], in_=mm_[ti][:], func=AF.Square, accum_out=st_[:, 0:1]))
                    yield
                    r1a = ACT(a1, lambda st_=st_: nc.scalar.activation(out=st_[:, 1:2], in_=st_[:, 0:1], func=AF.Sqrt, bias=EPS, scale=1.0 / D))
                    yield
                    r1 = DVE(r1a, lambda st_=st_: nc.vector.reciprocal(out=st_[:, 1:2], in_=st_[:, 1:2]))
                    d3 = DVE([r1, ytmp_free[ti], vec_ready], lambda ti=ti, st_=st_: nc.vector.scalar_tensor_tensor(
                        out=ytmp[ti][:], in0=mm_[ti][:], scalar=st_[:, 1:2], in1=gvec2[:], op0=ALU.mult, op1=ALU.mult))
                    mm_free[ti] = d3
                    T['vec_readers'] = [d3]
                    pp = POOL([d3, lx, yo_free[ti]], lambda ti=ti: nc.gpsimd.tensor_tensor(out=yo[ti][:], in0=ytmp[ti][:], in1=x1c[ti][:], op=ALU.add))
                    ytmp_free[ti] = pp
                    x1c_free[ti] = pp
                    yo_free[ti] = DMA('yo%d' % ti, pp, lambda ti=ti, tok0=tok0: nc.sync.dma_start(out=y[tok0:tok0 + 128, :], in_=yo[ti][:]))
                    out_toks.append(yo_free[ti])
                interleave((cmb_tile(i) for i in range(NTT)), 3)
            dp.wait('sp', [yo_free, out_toks[-3:]])
            for e in ('pe', 'act', 'dve', 'pool'):
                dp.wait('sp', [(e, dp.cnt[e])])
    return nc


def _rope_tables(pos):
    half = 16
    inv = (10000.0 ** (-np.arange(half, dtype=np.float32) / half)).astype(np.float32)
    ang = pos.astype(np.float32)[:, None] * inv[None, :]
    cos = np.cos(ang).astype(np.float32)
    sin = np.sin(ang).astype(np.float32)
    c = np.concatenate([cos, cos], axis=1).T
    s_ = np.concatenate([sin, sin], axis=1).T
    return np.ascontiguousarray(c), np.ascontiguousarray(s_)


def _consts(NSL):
    ident = np.eye(128, dtype=np.float32)
    egrp = np.zeros((8, 512), np.float32)
    for g in range(8):
        egrp[g, g * 64:(g + 1) * 64] = 1.0
    utri = np.triu(np.ones((128, 128), np.float32), k=1)
    tri32 = np.triu(np.ones((32, 32), np.float32), k=1)
    jv = np.tile((np.arange(NSL, dtype=np.float32) * 128.0)[None, :], (128, 1))
    pidx = np.arange(128, dtype=np.float32).reshape(128, 1)
    return dict(ident=ident, egrp=egrp, utri=utri, tri32=tri32, jv=np.ascontiguousarray(jv), pidx=pidx)


def _nt(cfg):
    nqt = sum(cfg['NQG']) * 512
    nt = (2 * nqt + 32 * 127 + 127) // 128
    return ((nt + 7) // 8) * 8


WEIGHT_KEYS = ['w_ada', 'b_ada', 'g_pre1', 'g_post1', 'g_pre2', 'g_post2', 'w_in', 'g_q', 'w_uq', 'g_kv', 'w_ukv',
               'g_v_gmlp', 'w_spatial', 'b_spatial', 'g_attn_out', 'g_gmlp_out', 'w_out', 'w_router_group',
               'b_router_group', 'w_router_expert', 'b_router_expert', 'w_gate', 'w_up', 'w_down']

_NC_CACHE = {}


def kernel(**inputs):
    S = 4096
    x_all = np.concatenate([np.asarray(inputs['x_prompt'], np.float32), np.asarray(inputs['x_sample'], np.float32)], axis=0)
    c_all = np.concatenate([np.asarray(inputs['c_prompt'], np.float32), np.asarray(inputs['c_sample'], np.float32)], axis=0)
    weights = {k: np.ascontiguousarray(np.asarray(inputs[k], np.float32)) for k in WEIGHT_KEYS}
    consts = _consts(_nt(FULL_CFG))
    pos_nat = np.arange(S)
    in_maps = []
    plans = []
    for c in range(8):
        if c % 2 == 0:
            s0 = (5 * c) // 2
            A, B, Cq, qhalf = s0, s0 + 1, s0 + 2, 0
        else:
            s0 = (5 * c - 1) // 2
            Cq, qhalf, A, B = s0, 1, s0 + 1, s0 + 2
        if qhalf == 0:
            posC = pos_nat
        else:
            posC = np.concatenate([pos_nat[S // 2:], pos_nat[:S // 2]])
        xs = np.concatenate([x_all[A], x_all[B], x_all[Cq][posC]], axis=0)
        cv = np.stack([c_all[A], c_all[B], c_all[Cq]], axis=0)
        rc = np.zeros((3, 32, S), np.float32)
        rs = np.zeros((3, 32, S), np.float32)
        for i, p in enumerate([pos_nat, pos_nat, posC]):
            rc[i], rs[i] = _rope_tables(p)
        m = dict(weights)
        m.update(xs=np.ascontiguousarray(xs), cvec=np.ascontiguousarray(cv), rope_c=rc, rope_s=rs)
        m.update(consts)
        in_maps.append(m)
        plans.append((A, B, Cq, qhalf))
    if 'full' not in _NC_CACHE:
        _NC_CACHE['full'] = build(FULL_CFG)
    nc = _NC_CACHE['full']
    res = run_bass_kernel_spmd(nc, in_maps, core_ids=list(range(8)))
    y_all = np.zeros((20, S, D), np.float32)
    for c in range(8):
        yc = res.results[c]['y']
        A, B, Cq, qhalf = plans[c]
        y_all[A] = yc[0:S]
        y_all[B] = yc[S:2 * S]
        if qhalf == 0:
            y_all[Cq, 0:S // 2] = yc[2 * S:2 * S + S // 2]
        else:
            y_all[Cq, S // 2:] = yc[2 * S:2 * S + S // 2]
    return (np.ascontiguousarray(y_all[0:4]), np.ascontiguousarray(y_all[4:20]))
```

```python
import numpy as np
import concourse.bass as bass
import concourse.mybir as mybir
from concourse.bass_utils import run_bass_kernel_spmd
from contextlib import ExitStack

F32, BF16 = mybir.dt.float32, mybir.dt.bfloat16
I32 = mybir.dt.int32
AF = mybir.ActivationFunctionType
ALU = mybir.AluOpType
AX = mybir.AxisListType
D = 1024
EPS = 1e-6
NE = 32
QSCALE = 96.0 ** -0.5

FULL_CFG = dict(S=4096, NSEQ=3, NQG=[8, 8, 4])


class Dep:
    def __init__(self, nc, es):
        self.nc = nc
        self.es = es
        self.eng = {'pe': nc.tensor, 'act': nc.scalar, 'dve': nc.vector, 'pool': nc.gpsimd, 'sp': nc.sync}
        self.sem = {e: es.enter_context(nc.semaphore('s_' + e)) for e in self.eng}
        self.cnt = {e: 0 for e in self.eng}
        self.waited = {e: {} for e in self.eng}
        self.dsem = {}
        self.entries = {}
        self.free = []

    def semof(self, k):
        return self.sem[k] if k in self.sem else self.entries[k][0]

    def wait(self, e, deps):
        for d in _flat(deps):
            k, v = d
            if self.waited[e].get(k, 0) < v:
                self.eng[e].wait_ge(self.semof(k), v)
                self.waited[e][k] = v

    def op(self, e, deps, fn, sig=True):
        self.wait(e, deps)
        ins = fn()
        if sig:
            ins.then_inc(self.sem[e], 1)
            self.cnt[e] += 1
            return (e, self.cnt[e])
        return None

    def dma(self, q, name, deps, fn):
        if name not in self.dsem:
            if self.free:
                key = self.free.pop()
            else:
                key = 'D%d' % len(self.entries)
                self.entries[key] = [self.es.enter_context(self.nc.semaphore('d_' + key)), 0]
            self.dsem[name] = key
        key = self.dsem[name]
        self.wait(q, deps)
        ins = fn()
        ent = self.entries[key]
        ins.then_inc(ent[0], 16)
        ent[1] += 16
        return (key, ent[1])

    def mark(self):
        return set(self.dsem.keys())

    def retire_since(self, mark, keep=()):
        for n in list(self.dsem.keys()):
            if n in mark or n in keep:
                continue
            key = self.dsem[n]
            self.wait('sp', (key, self.entries[key][1]))
            del self.dsem[n]
            self.free.append(key)

    def last(self):
        return [(e, self.cnt[e]) for e in self.eng if self.cnt[e] > 0]


def _flat(deps):
    out = []
    if deps is None:
        return out
    if isinstance(deps, tuple) and len(deps) == 2 and isinstance(deps[0], str):
        return [deps]
    for d in deps:
        out.extend(_flat(d))
    return out


def interleave(gens, depth):
    active = []
    it = iter(gens)
    done = False
    while True:
        if len(active) < depth and not done:
            try:
                active.append(next(it))
            except StopIteration:
                done = True
        if not active:
            break
        nxt = []
        for g in active:
            try:
                next(g)
                nxt.append(g)
            except StopIteration:
                pass
        active = nxt


def build(cfg):
    S = cfg['S']
    NSEQ = cfg['NSEQ']
    NQG = cfg['NQG']
    NG = S // 512
    KB = S // 128
    NT = NSEQ * S
    NQT = sum(NQG) * 512
    NGB = sum(NQG)
    NSL = (2 * NQT + 32 * 127 + 127) // 128
    NSL = ((NSL + 7) // 8) * 8

    nc = bass.Bass("TRN2", target_bir_lowering=False)

    def din(name, shape, dt=F32):
        return nc.dram_tensor(name, list(shape), dt, kind="ExternalInput").ap()

    def dscr(name, shape, dt):
        return nc.dram_tensor(name, list(shape), dt, kind="Internal").ap()

    xs = din("xs", [NT, D])
    cvec = din("cvec", [NSEQ, D])
    rope_c = din("rope_c", [NSEQ, 32, S])
    rope_s = din("rope_s", [NSEQ, 32, S])
    w_ada = din("w_ada", [D, 6 * D])
    b_ada = din("b_ada", [6 * D])
    g_pre1 = din("g_pre1", [D]); g_post1 = din("g_post1", [D])
    g_pre2 = din("g_pre2", [D]); g_post2 = din("g_post2", [D])
    w_in = din("w_in", [D, 1440])
    g_q = din("g_q", [256]); w_uq = din("w_uq", [256, 768])
    g_kv = din("g_kv", [128]); w_ukv = din("w_ukv", [128, 1024])
    g_v_gmlp = din("g_v_gmlp", [512])
    w_spatial = din("w_spatial", [8, 128, 128]); b_spatial = din("b_spatial", [8, 128])
    g_attn_out = din("g_attn_out", [512]); g_gmlp_out = din("g_gmlp_out", [512])
    w_out = din("w_out", [D, D])
    w_rg = din("w_router_group", [D, 4]); b_rg = din("b_router_group", [4])
    w_re = din("w_router_expert", [D, 32]); b_re = din("b_router_expert", [32])
    w_gate = din("w_gate", [NE, D, 256]); w_up = din("w_up", [NE, D, 256]); w_down = din("w_down", [NE, 256, D])
    ident_in = din("ident", [128, 128])
    egrp_in = din("egrp", [8, 512])
    utri_in = din("utri", [128, 128])
    tri32_in = din("tri32", [32, 32])
    jv_in = din("jv", [128, NSL])
    pidx_in = din("pidx", [128, 1])
    y = nc.dram_tensor("y", [NQT, D], F32, kind="ExternalOutput").ap()

    mod_d = dscr("mod_d", [NSEQ, 6 * D], F32)
    sn_d = dscr("sn_d", [NT, 512], BF16)
    cq_d = dscr("cq_d", [NSEQ * NG, 128, 1024], BF16)
    x1_d = (nc.dram_tensor("x1_d", [NQT, D], F32, kind="ExternalOutput").ap() if cfg.get("dbg") else dscr("x1_d", [NQT, D], F32))
    wgu_r = dscr("wgu_r", [NE * 128, 8 * 512], BF16)
    wd_r = dscr("wd_r", [NE * 128, 2 * D], BF16)
    h2_d = dscr("h2_d", [NQT, D], BF16)
    xs_d = dscr("xs_d", [NSL * 128, D], BF16)
    ys_d = dscr("ys_d", [NSL * 128, D], F32)
    wkv_d = dscr("wkv_d", [128, 1024], BF16)
    wsp_d = dscr("wsp_d", [128, 1024], BF16)
    wq_d = dscr("wq_d", [128, 1536], BF16)
    wqsw_d = dscr("wqsw_d", [128, 1536], BF16)
    wo_d = dscr("wo_d", [128, 8192], BF16)

    _uid = [0]

    def U(name):
        _uid[0] += 1
        return "%s_u%d" % (name, _uid[0])

    top = ExitStack()
    with top:
        dp = Dep(nc, top)

        def PE(deps, fn, sig=True): return dp.op('pe', deps, fn, sig)
        def ACT(deps, fn, sig=True): return dp.op('act', deps, fn, sig)
        def DVE(deps, fn, sig=True): return dp.op('dve', deps, fn, sig)
        def POOL(deps, fn, sig=True): return dp.op('pool', deps, fn, sig)
        def DMA(name, deps, fn, q='sp'): return dp.dma(q, name, deps, fn)

        def mmg(out, pairs, deps, sig=True):
            n = len(pairs)
            tok = None
            for i, (l, r) in enumerate(pairs):
                tok = PE(deps if i == 0 else None,
                         lambda l=l, r=r, i=i: nc.tensor.matmul(out, lhsT=l, rhs=r, start=(i == 0), stop=(i == n - 1)),
                         sig=(sig and i == n - 1))
            return tok

        def rstd_chain(ss_ap, out_ap, inv_n, deps):
            t = ACT(deps, lambda: nc.scalar.activation(out=out_ap, in_=ss_ap, func=AF.Sqrt, bias=EPS, scale=inv_n))
            return DVE(t, lambda: nc.vector.reciprocal(out=out_ap, in_=out_ap))

        wcast = []
        for e in range(NE):
            wcast.append(DMA('wcast', None, lambda e=e: nc.gpsimd.dma_start(
                out=wgu_r[e * 128:(e + 1) * 128, :].rearrange("p (k c) -> p k c", k=8)[:, :, 0:256],
                in_=w_gate[e].rearrange("(k p) c -> p k c", p=128)), q='pool'))
            wcast.append(DMA('wcast', None, lambda e=e: nc.gpsimd.dma_start(
                out=wgu_r[e * 128:(e + 1) * 128, :].rearrange("p (k c) -> p k c", k=8)[:, :, 256:512],
                in_=w_up[e].rearrange("(k p) c -> p k c", p=128)), q='pool'))
            wcast.append(DMA('wcast', None, lambda e=e: nc.gpsimd.dma_start(
                out=wd_r[e * 128:(e + 1) * 128, :].rearrange("p (j c) -> p j c", j=2),
                in_=w_down[e].rearrange("(j p) c -> p j c", p=128)), q='pool'))
        wcast_tok = wcast[-1]

        ident_f = top.enter_context(nc.sbuf_tensor(U("ident_f"), [128, 128], F32))
        ident_b = top.enter_context(nc.sbuf_tensor(U("ident_b"), [128, 128], BF16))
        ones_f = top.enter_context(nc.sbuf_tensor(U("ones_f"), [128, 64], F32))
        t_id = DMA('c0', None, lambda: nc.sync.dma_start(out=ident_f[:], in_=ident_in))
        t_idb = DVE(t_id, lambda: nc.vector.tensor_copy(out=ident_b[:], in_=ident_f[:]))
        t_ones = DVE(None, lambda: nc.vector.memset(ones_f[:], 1.0))

        mk0 = dp.mark()
        with ExitStack() as pes:
            def sb(name, shape, dt): return pes.enter_context(nc.sbuf_tensor(U(name), shape, dt))
            def ps(name, shape, dt): return pes.enter_context(nc.psum_tensor(U(name), shape, dt))
            zt = sb("zt", [128, 8192], BF16)
            tz0 = POOL(None, lambda: nc.gpsimd.memset(zt[:], 0.0))
            zero_tok = []
            nz = (NSL * 128 * D) // (128 * 8192)
            xs_flat = xs_d.rearrange("(n p r) c -> n p (r c)", p=128, r=8)
            for zi in range(nz):
                zero_tok.append(DMA('zero', tz0, lambda zi=zi: nc.sync.dma_start(out=xs_flat[zi], in_=zt[:])))
            csT = sb("csT", [128, 8, NSEQ], F32)
            csS = sb("csS", [128, 8, NSEQ], F32)
            wblk = [sb("wblk%d" % i, [128, 8, 512], F32) for i in range(2)]
            brep = sb("brep", [NSEQ, 6 * D], F32)
            modsb = sb("modsb", [NSEQ, 6 * D], F32)
            pmod = [ps("pmod%d" % i, [128, 512], F32) for i in range(2)]
            t_c = [DMA('p0', None, lambda q=q: nc.sync.dma_start(out=csT[:, :, q], in_=cvec[q].rearrange("(k p) -> p k", p=128),
                                                                 allow_slow_non_contiguous=True)) for q in range(NSEQ)]
            t_b = DMA('p1', None, lambda: nc.sync.dma_start(out=brep[:], in_=b_ada.partition_broadcast(NSEQ)))
            t_cs = ACT(t_c, lambda: nc.scalar.activation(out=csS[:], in_=csT[:], func=AF.Silu))
            wfree = [None, None]
            pfree = [None, None]
            ev = None
            for blk in range(12):
                i = blk % 2
                t_w = DMA('pw%d' % i, wfree[i], lambda blk=blk, i=i: nc.sync.dma_start(
                    out=wblk[i][:], in_=w_ada[:, blk * 512:(blk + 1) * 512].rearrange("(k p) c -> p k c", p=128)))
                t_m = mmg(pmod[i][0:NSEQ, :], [(csS[:, k, :], wblk[i][:, k, :]) for k in range(8)], [t_w, t_cs, pfree[i]])
                wfree[i] = t_m
                ev = DVE([t_m, t_b], lambda blk=blk, i=i: nc.vector.tensor_tensor(
                    out=modsb[:, blk * 512:(blk + 1) * 512], in0=pmod[i][0:NSEQ, :],
                    in1=brep[:, blk * 512:(blk + 1) * 512], op=ALU.add))
                pfree[i] = ev
            t_mod = DMA('p2', ev, lambda: nc.sync.dma_start(out=mod_d, in_=modsb[:]))

            tmpq = sb("tmpq", [128, 2, 768], F32)
            gq = sb("gq", [128, 2], F32)
            wq_t = sb("wq_t", [128, 2, 768], BF16)
            wqsw_t = sb("wqsw_t", [128, 2, 768], BF16)
            t1 = DMA('p3', None, lambda: nc.sync.dma_start(out=tmpq[:], in_=w_uq.rearrange("(k p) c -> p k c", p=128)))
            t2 = DMA('p3', None, lambda: nc.sync.dma_start(out=gq[:], in_=g_q.rearrange("(k p) -> p k", p=128),
                                                          allow_slow_non_contiguous=True))
            tq = None
            for k in range(2):
                tq = DVE([t1, t2], lambda k=k: nc.vector.tensor_scalar(
                    out=wq_t[:, k, :], in0=tmpq[:, k, :], scalar1=gq[:, k:k + 1], scalar2=QSCALE,
                    op0=ALU.mult, op1=ALU.mult))
            tz = POOL(None, lambda: nc.gpsimd.memset(wqsw_t[:], 0.0))
            wq4 = wq_t[:].rearrange("p k (h c) -> p k h c", h=8)
            wqs4 = wqsw_t[:].rearrange("p k (h c) -> p k h c", h=8)
            ta = DVE([tq, tz], lambda: nc.vector.tensor_scalar(out=wqs4[:, :, :, 64:80], in0=wq4[:, :, :, 80:96],
                                                              scalar1=-1.0, scalar2=None, op0=ALU.mult))
            tb = DVE(None, lambda: nc.vector.tensor_copy(out=wqs4[:, :, :, 80:96], in_=wq4[:, :, :, 64:80]))
            t_wq = DMA('p4', tq, lambda: nc.sync.dma_start(out=wq_d, in_=wq_t[:].rearrange("p k c -> p (k c)")))
            t_wqsw = DMA('p4', [ta, tb], lambda: nc.sync.dma_start(out=wqsw_d, in_=wqsw_t[:].rearrange("p k c -> p (k c)")))

            tmpkv = sb("tmpkv", [128, 1024], F32)
            gkv = sb("gkv", [128, 1], F32)
            wkv_t = sb("wkv_t", [128, 1024], BF16)
            t1 = DMA('p5', None, lambda: nc.sync.dma_start(out=tmpkv[:], in_=w_ukv))
            t2 = DMA('p5', None, lambda: nc.sync.dma_start(out=gkv[:], in_=g_kv.rearrange("(p o) -> p o", o=1)))
            tk = DVE([t1, t2], lambda: nc.vector.tensor_scalar(out=wkv_t[:], in0=tmpkv[:], scalar1=gkv[:, 0:1],
                                                              scalar2=None, op0=ALU.mult))
            t_wkv = DMA('p6', tk, lambda: nc.sync.dma_start(out=wkv_d, in_=wkv_t[:]))

            tmpo = sb("tmpo", [128, 8, 1024], F32)
            gcat = sb("gcat", [128, 8], F32)
            wo_t = sb("wo_t", [128, 8, 1024], BF16)
            t1 = DMA('p7', None, lambda: nc.sync.dma_start(out=tmpo[:], in_=w_out.rearrange("(k p) c -> p k c", p=128)))
            t2 = DMA('p7', None, lambda: nc.sync.dma_start(out=gcat[:, 0:4], in_=g_attn_out.rearrange("(k p) -> p k", p=128),
                                                          allow_slow_non_contiguous=True))
            t3 = DMA('p7', None, lambda: nc.sync.dma_start(out=gcat[:, 4:8], in_=g_gmlp_out.rearrange("(k p) -> p k", p=128),
                                                          allow_slow_non_contiguous=True))
            two = None
            for k in range(8):
                two = DVE([t1, t2, t3], lambda k=k: nc.vector.tensor_scalar(
                    out=wo_t[:, k, :], in0=tmpo[:, k, :], scalar1=gcat[:, k:k + 1], scalar2=None, op0=ALU.mult))
            t_wo = DMA('p8', two, lambda: nc.sync.dma_start(out=wo_d, in_=wo_t[:].rearrange("p k c -> p (k c)")))

            tmps = sb("tmps", [128, 8, 128], F32)
            wsp_t = sb("wsp_t", [128, 8, 128], BF16)
            psp = ps("psp", [128, 1024], F32)
            t1 = DMA('p9', None, lambda: nc.sync.dma_start(out=tmps[:], in_=w_spatial.rearrange("g t s -> t g s")))
            tt = None
            for g in range(8):
                tt = PE([t1, t_id], lambda g=g: nc.tensor.transpose(psp[:, g * 128:(g + 1) * 128], tmps[:, g, :], ident_f[:]),
                        sig=(g == 7))
            tc_ = DVE(tt, lambda: nc.vector.tensor_copy(out=wsp_t[:].rearrange("p g t -> p (g t)"), in_=psp[:]))
            t_wsp = DMA('p10', tc_, lambda: nc.sync.dma_start(out=wsp_d, in_=wsp_t[:].rearrange("p g t -> p (g t)")))
            prep_done = [t_mod, t_wq, t_wqsw, t_wkv, t_wo, t_wsp, zero_tok]
            prep_bar = dp.last()
            dp.retire_since(mk0, keep=('wcast', 'zero', 'c0'))

        with ExitStack() as aes:
            def sbA(name, shape, dt): return aes.enter_context(nc.sbuf_tensor(U(name), shape, dt))
            KT = sbA("KT", [128, 8, S], BF16)
            VA = sbA("VA", [128, KB, 8, 65], BF16)
            geff1 = sbA("geff1", [128, D], F32)
            sh1r = sbA("sh1r", [128, D], F32)
            gvec1 = sbA("gvec1", [128, D], F32)
            gvrep = sbA("gvrep", [128, 512], F32)
            PA = aes.enter_context(nc.psum_tensor(U("PA"), [128, 1024], F32))
            PB = aes.enter_context(nc.psum_tensor(U("PB"), [128, 1024], F32))
            PC = aes.enter_context(nc.psum_tensor(U("PC"), [128, 1024], F32))
            PD = aes.enter_context(nc.psum_tensor(U("PD"), [128, 1024], F32))

            t_va1 = POOL(prep_bar, lambda: nc.gpsimd.memset(VA[:, :, :, 64:65], 1.0))
            t_gv = DMA('a0', prep_bar, lambda: nc.sync.dma_start(out=gvrep[:], in_=g_v_gmlp.partition_broadcast(128)))
            seq_bar = [prep_bar, prep_done, t_va1, t_gv, t_idb, t_ones]
            qbase = 0
            for s in range(NSEQ):
                mk1 = dp.mark()
                with ExitStack() as p1:
                    def sb1(name, shape, dt): return p1.enter_context(nc.sbuf_tensor(U(name), shape, dt))
                    wAs = sb1("wAs", [128, 8, 384], BF16)
                    wAuv = sb1("wAuv", [128, 8, 1024], BF16)
                    wAkr = sb1("wAkr", [128, 8, 96], BF16)
                    wAks = sb1("wAks", [128, 8, 96], BF16)
                    wkv = sb1("wkv", [128, 1024], BF16)
                    wsp = sb1("wsp", [128, 8, 128], BF16)
                    bsp = sb1("bsp", [8, 128], F32)
                    egrp = sb1("egrp", [8, 512], F32)
                    vt = [sb1("vt%d" % i, [128, D], F32) for i in range(2)]
                    xt = [sb1("xt%d" % i, [128, D], F32) for i in range(2)]
                    junk = sb1("junk", [128, D], BF16)
                    hm = sb1("hm", [128, D], F32)
                    hb = [sb1("hb%d" % i, [128, D], BF16) for i in range(2)]
                    hT = sb1("hT", [128, 8, 512], BF16)
                    zsb = [sb1("zsb%d" % i, [128, 384], BF16) for i in range(2)]
                    cqnT = [sb1("cqnT%d" % i, [128, 2, 512], BF16) for i in range(2)]
                    ckvnT = [sb1("ckvnT%d" % i, [128, 512], BF16) for i in range(2)]
                    gu = [sb1("gu%d" % i, [128, 512], BF16) for i in range(2)]
                    gv = [sb1("gv%d" % i, [128, 512], F32) for i in range(2)]
                    zraw = [sb1("zraw%d" % i, [128, 384], F32) for i in range(2)]
                    vn = [sb1("vn%d" % i, [128, 512], BF16) for i in range(2)]
                    sraw = [sb1("sraw%d" % i, [128, 512], F32) for i in range(2)]
                    sn = [sb1("sn%d" % i, [128, 512], BF16) for i in range(2)]
                    stt = [sb1("stt%d" % i, [128, 16], F32) for i in range(2)]
                    Ctt = [sb1("Ctt%d" % i, [128, 128], F32) for i in range(2)]
                    Stt = [sb1("Stt%d" % i, [128, 128], F32) for i in range(2)]
                    kt1 = [sb1("kt1_%d" % i, [128, 128], F32) for i in range(2)]
                    kt2 = [sb1("kt2_%d" % i, [128, 128], F32) for i in range(2)]
                    krr = [sb1("krr%d" % i, [128, 128], BF16) for i in range(2)]

                    pT = PA[:, 0:512].bitcast(BF16)
                    pT2 = PA[:, 512:1024].bitcast(BF16)
                    pzs = PB[:, 0:384]
                    pss = PB[:, 512:1024]
                    pu = PC[:, 0:512]
                    pv = PC[:, 512:1024]
                    pkr = PD[:, 0:512]
                    pks = PD[:, 512:1024]

                    sb_ = seq_bar
                    wl = []
                    wl.append(DMA('a1', sb_, lambda: nc.gpsimd.dma_start(
                        out=wAs[:], in_=w_in[:, 0:384].rearrange("(k p) c -> p k c", p=128)), q='pool'))
                    wl.append(DMA('a1', sb_, lambda: nc.gpsimd.dma_start(
                        out=wAuv[:], in_=w_in[:, 416:1440].rearrange("(k p) c -> p k c", p=128)), q='pool'))
                    tz1 = POOL(sb_, lambda: nc.gpsimd.memset(wAkr[:], 0.0))
                    tz2 = POOL(sb_, lambda: nc.gpsimd.memset(wAks[:], 0.0))
                    wl.append(DMA('a1', [tz1], lambda: nc.gpsimd.dma_start(
                        out=wAkr[:, :, 64:96], in_=w_in[:, 384:416].rearrange("(k p) c -> p k c", p=128)), q='pool'))
                    tn = DMA('a2', [tz2], lambda: nc.gpsimd.dma_start(
                        out=wAks[:, :, 64:80], in_=w_in[:, 400:416].rearrange("(k p) c -> p k c", p=128)), q='pool')
                    wl.append(DMA('a1', [tz2], lambda: nc.gpsimd.dma_start(
                        out=wAks[:, :, 80:96], in_=w_in[:, 384:400].rearrange("(k p) c -> p k c", p=128)), q='pool'))
                    wl.append(POOL(tn, lambda: nc.gpsimd.tensor_scalar(out=wAks[:, :, 64:80], in0=wAks[:, :, 64:80],
                                                                      scalar1=-1.0, scalar2=None, op0=ALU.mult)))
                    wl.append(DMA('a3', sb_, lambda: nc.sync.dma_start(out=wkv[:], in_=wkv_d)))
                    wl.append(DMA('a3', sb_, lambda: nc.sync.dma_start(out=wsp[:].rearrange("p g t -> p (g t)"), in_=wsp_d)))
                    wl.append(DMA('a3', sb_, lambda: nc.sync.dma_start(out=bsp[:], in_=b_spatial)))
                    wl.append(DMA('a3', sb_, lambda: nc.sync.dma_start(out=egrp[:], in_=egrp_in)))
                    l1 = DMA('a4_0', sb_, lambda: nc.sync.dma_start(out=vt[0][:], in_=mod_d[s, 1024:2048].partition_broadcast(128)))
                    l2 = DMA('a4_1', sb_, lambda: nc.sync.dma_start(out=vt[1][:], in_=g_pre1.partition_broadcast(128)))
                    l3 = DMA('a4_2', sb_, lambda: nc.sync.dma_start(out=sh1r[:], in_=mod_d[s, 0:1024].partition_broadcast(128)))
                    tg = DVE([l1, l2], lambda: nc.vector.scalar_tensor_tensor(out=geff1[:], in0=vt[0][:], scalar=1.0, in1=vt[1][:],
                                                                               op0=ALU.add, op1=ALU.mult))
                    l4 = DMA('a4_3', [tg], lambda: nc.sync.dma_start(out=vt[0][:], in_=mod_d[s, 2048:3072].partition_broadcast(128)))
                    l5 = DMA('a4_4', [tg], lambda: nc.sync.dma_start(out=vt[1][:], in_=g_post1.partition_broadcast(128)))
                    tg2 = DVE([l4, l5], lambda: nc.vector.tensor_tensor(out=gvec1[:], in0=vt[0][:], in1=vt[1][:], op=ALU.mult))
                    ready = [wl, l3, tg, tg2]

                    xt_free = [None, None]; hb_free = [None, None]
                    hT_free = [None] * 4
                    zraw_free = [None, None]; zsb_free = [None, None]; cq_free = [None, None]; ckv_free = [None, None]
                    gu_free = [None, None]; gv_free = [None, None]; vn_free = [None, None]; sn_free = [None, None]
                    sraw_free = [None, None]; ct_free = [None, None]; kt_free = [None, None]; krr_free = [None, None]
                    P = dict(hm_free=None, pT_free=None, pT2_free=None, pzs_free=None, pu_free=None, pv_free=None, pss_free=None,
                             pkr_free=None, pks_free=None)
                    grp = {}

                    def p1_tile(g, t):
                        gi = g % 2
                        ti = t % 2
                        if t == 0:
                            grp[g] = dict(cq_w=[], ckv_w=[])
                        G = grp[g]
                        tok0 = s * S + g * 512 + t * 128
                        ts_ = slice(t * 128, (t + 1) * 128)
                        gts = slice(g * 512 + t * 128, g * 512 + (t + 1) * 128)
                        st_ = stt[ti]
                        lx = DMA('x%d' % ti, [xt_free[ti], ready], lambda: nc.sync.dma_start(out=xt[ti][:], in_=xs[tok0:tok0 + 128, :]))
                        lc = DMA('rc%d' % ti, [ct_free[ti], ready], lambda: nc.sync.dma_start(out=Ctt[ti][64:96, :], in_=rope_c[s, :, gts]))
                        ls = DMA('rs%d' % ti, [ct_free[ti], ready], lambda: nc.sync.dma_start(out=Stt[ti][64:96, :], in_=rope_s[s, :, gts]))
                        a1 = ACT(lx, lambda: nc.scalar.activation(out=junk[:], in_=xt[ti][:], func=AF.Square, accum_out=st_[:, 0:1]))
                        yield
                        r1a = ACT(a1, lambda: nc.scalar.activation(out=st_[:, 1:2], in_=st_[:, 0:1], func=AF.Sqrt, bias=EPS, scale=1.0 / D))
                        yield
                        r1 = DVE(r1a, lambda: nc.vector.reciprocal(out=st_[:, 1:2], in_=st_[:, 1:2]))
                        d1 = DVE([r1, P['hm_free']], lambda: nc.vector.scalar_tensor_tensor(
                            out=hm[:], in0=xt[ti][:], scalar=st_[:, 1:2], in1=geff1[:], op0=ALU.mult, op1=ALU.mult))
                        xt_free[ti] = d1
                        p1_ = POOL([d1, hb_free[ti]], lambda: nc.gpsimd.tensor_tensor(out=hb[ti][:], in0=hm[:], in1=sh1r[:], op=ALU.add))
                        P['hm_free'] = p1_
                        yield
                        tp = None
                        for k in range(8):
                            tp = PE([p1_, P['pT_free']] if k == 0 else None,
                                    lambda k=k: nc.tensor.transpose(pT[:, k * 128:(k + 1) * 128], hb[ti][:, k * 128:(k + 1) * 128], ident_b[:]),
                                    sig=(k == 7))
                        hb_free[ti] = tp
                        yield
                        ev = ACT([tp, hT_free[t]], lambda: nc.scalar.copy(out=hT[:, :, ts_], in_=pT.rearrange("p (k c) -> p k c", k=8)))
                        P['pT_free'] = ev
                        yield
                        m_zs = mmg(pzs, [(hT[:, k, ts_], wAs[:, k, :]) for k in range(8)], [ev, P['pzs_free']])
                        m_u = mmg(pu, [(hT[:, k, ts_], wAuv[:, k, 0:512]) for k in range(8)], [P['pu_free']])
                        m_v = mmg(pv, [(hT[:, k, ts_], wAuv[:, k, 512:1024]) for k in range(8)], [P['pv_free']])
                        m_kr = mmg(pkr[0:96, 0:128], [(wAkr[:, k, :], hT[:, k, ts_]) for k in range(8)], [P['pkr_free']])
                        m_ks = mmg(pks[0:96, 0:128], [(wAks[:, k, :], hT[:, k, ts_]) for k in range(8)], [P['pks_free']])
                        hT_free[t] = m_ks
                        yield
                        zr = DVE([m_zs, zraw_free[ti]], lambda: nc.vector.tensor_copy(out=zraw[ti][:], in_=pzs))
                        P['pzs_free'] = zr
                        g1 = ACT([m_u, gu_free[ti]], lambda: nc.scalar.activation(out=gu[ti][:], in_=pu, func=AF.Gelu_apprx_tanh))
                        P['pu_free'] = g1
                        g2 = ACT([m_v, gv_free[ti]], lambda: nc.scalar.activation(out=gv[ti][:], in_=pv, func=AF.Gelu_apprx_tanh))
                        P['pv_free'] = g2
                        k1 = DVE([m_kr, lc, kt_free[ti]], lambda: nc.vector.tensor_tensor(out=kt1[ti][64:96, :], in0=pkr[64:96, 0:128], in1=Ctt[ti][64:96, :], op=ALU.mult))
                        P['pkr_free'] = k1
                        k2 = DVE([m_ks, ls], lambda: nc.vector.tensor_tensor(out=kt2[ti][64:96, :], in0=pks[64:96, 0:128], in1=Stt[ti][64:96, :], op=ALU.mult))
                        P['pks_free'] = k2
                        ct_free[ti] = k2
                        yield
                        a2 = ACT(zr, lambda: nc.scalar.activation(out=junk[:, 0:256], in_=zraw[ti][:, 0:256], func=AF.Square, accum_out=st_[:, 2:3]))
                        a3 = ACT(None, lambda: nc.scalar.activation(out=junk[:, 256:384], in_=zraw[ti][:, 256:384], func=AF.Square, accum_out=st_[:, 3:4]))
                        g3 = ACT(g2, lambda: nc.scalar.activation(out=junk[:, 0:512], in_=gv[ti][:], func=AF.Square, accum_out=st_[:, 6:7]))
                        k3 = DVE([k1, k2, krr_free[ti]], lambda: nc.vector.tensor_tensor(out=krr[ti][64:96, :], in0=kt1[ti][64:96, :], in1=kt2[ti][64:96, :], op=ALU.add))
                        kt_free[ti] = k3
                        kc = None
                        for h in range(8):
                            kc = POOL(k3, lambda h=h: nc.gpsimd.tensor_copy(out=KT[64:96, h, gts], in_=krr[ti][64:96, :]))
                        krr_free[ti] = kc
                        yield
                        q1 = ACT([a2, a3], lambda: nc.scalar.activation(out=st_[:, 4:5], in_=st_[:, 2:3], func=AF.Sqrt, bias=EPS, scale=1.0 / 256))
                        q2 = ACT(None, lambda: nc.scalar.activation(out=st_[:, 5:6], in_=st_[:, 3:4], func=AF.Sqrt, bias=EPS, scale=1.0 / 128))
                        q3 = ACT(g3, lambda: nc.scalar.activation(out=st_[:, 7:8], in_=st_[:, 6:7], func=AF.Sqrt, bias=EPS, scale=1.0 / 512))
                        yield
                        r2 = DVE([q1, q2], lambda: nc.vector.reciprocal(out=st_[:, 4:6], in_=st_[:, 4:6]))
                        r4 = DVE(q3, lambda: nc.vector.reciprocal(out=st_[:, 7:8], in_=st_[:, 7:8]))
                        d2 = DVE([r4, vn_free[ti]], lambda: nc.vector.scalar_tensor_tensor(
                            out=vn[ti][:], in0=gv[ti][:], scalar=st_[:, 7:8], in1=gvrep[:], op0=ALU.mult, op1=ALU.mult))
                        gv_free[ti] = d2
                        c1 = ACT([r2, zsb_free[ti]], lambda: nc.scalar.activation(
                            out=zsb[ti][:, 0:256], in_=zraw[ti][:, 0:256], func=AF.Copy, scale=st_[:, 4:5]))
                        c2 = ACT(None, lambda: nc.scalar.activation(
                            out=zsb[ti][:, 256:384], in_=zraw[ti][:, 256:384], func=AF.Copy, scale=st_[:, 5:6]))
                        zraw_free[ti] = c2
                        yield
                        tp2 = None
                        for k in range(3):
                            tp2 = PE([c1, c2, P['pT2_free']] if k == 0 else None,
                                     lambda k=k: nc.tensor.transpose(pT2[:, k * 128:(k + 1) * 128], zsb[ti][:, k * 128:(k + 1) * 128], ident_b[:]),
                                     sig=(k == 2))
                        zsb_free[ti] = tp2
                        PE([P['pss_free'], ready], lambda: nc.tensor.matmul(pss, lhsT=bsp[:, :], rhs=egrp[:, :], start=True, stop=False), sig=False)
                        m_s = None
                        for gg in range(8):
                            m_s = PE(d2 if gg == 0 else None,
                                     lambda gg=gg: nc.tensor.matmul(pss[:, gg * 64:(gg + 1) * 64], lhsT=wsp[:, gg, :],
                                                                    rhs=vn[ti][:, gg * 64:(gg + 1) * 64], start=False, stop=(gg == 7)),
                                     sig=(gg == 7))
                        vn_free[ti] = m_s
                        yield
                        e1 = DVE([tp2, cq_free[gi] if t == 0 else None], lambda: nc.vector.tensor_copy(
                            out=cqnT[gi][:, :, ts_], in_=pT2[:, 0:256].rearrange("p (k c) -> p k c", k=2)))
                        e2 = DVE([ckv_free[gi] if t == 0 else None], lambda: nc.vector.tensor_copy(
                            out=ckvnT[gi][:, ts_], in_=pT2[:, 256:384]))
                        P['pT2_free'] = e2
                        G['cq_w'].append(e1)
                        G['ckv_w'].append(e2)
                        d3 = DVE([m_s, g1, sraw_free[ti]], lambda: nc.vector.tensor_tensor(out=sraw[ti][:], in0=gu[ti][:], in1=pss, op=ALU.mult))
                        P['pss_free'] = d3
                        gu_free[ti] = d3
                        yield
                        a4 = ACT(d3, lambda: nc.scalar.activation(out=junk[:, 0:512], in_=sraw[ti][:], func=AF.Square, accum_out=st_[:, 8:9]))
                        yield
                        q4 = ACT(a4, lambda: nc.scalar.activation(out=st_[:, 9:10], in_=st_[:, 8:9], func=AF.Sqrt, bias=EPS, scale=1.0 / 512))
                        yield
                        r5 = DVE(q4, lambda: nc.vector.reciprocal(out=st_[:, 9:10], in_=st_[:, 9:10]))
                        yield
                        c3 = ACT([r5, sn_free[ti]], lambda: nc.scalar.activation(out=sn[ti][:], in_=sraw[ti][:], func=AF.Copy, scale=st_[:, 9:10]))
                        sraw_free[ti] = c3
                        sn_free[ti] = DMA('sn%d' % ti, c3, lambda: nc.sync.dma_start(out=sn_d[tok0:tok0 + 128, :], in_=sn[ti][:]))
                        if t != 3:
                            return
                        yield
                        gs = slice(g * 512, (g + 1) * 512)
                        cq_free[gi] = DMA('cq%d' % gi, G['cq_w'], lambda: nc.sync.dma_start(
                            out=cq_d[s * NG + g], in_=cqnT[gi][:].rearrange("p k c -> p (k c)")))
                        bank_free = [P['pkr_free'], P['pks_free']]
                        banks = [pkr, pks]
                        for h in range(8):
                            bi = h % 2
                            mk = mmg(banks[bi][0:64, :], [(wkv[:, h * 128:h * 128 + 64], ckvnT[gi][:, :])], [G['ckv_w'], bank_free[bi]])
                            if h % 2 == 0:
                                bank_free[bi] = ACT(mk, lambda h=h, bi=bi: nc.scalar.copy(out=KT[0:64, h, gs], in_=banks[bi][0:64, :]))
                            else:
                                bank_free[bi] = DVE(mk, lambda h=h, bi=bi: nc.vector.tensor_copy(out=KT[0:64, h, gs], in_=banks[bi][0:64, :]))
                        wkv3 = wkv[:].rearrange("p (h c) -> p h c", h=8)[:, :, 64:128]
                        mv = None
                        for tt in range(4):
                            bi = tt % 2
                            kb = g * 4 + tt
                            mv = mmg(banks[bi][:, :].rearrange("p (h c) -> p h c", h=8), [(ckvnT[gi][:, tt * 128:(tt + 1) * 128], wkv3)], [bank_free[bi]])
                            bank_free[bi] = DVE(mv, lambda kb=kb, bi=bi: nc.vector.tensor_copy(
                                out=VA[:, kb, :, 0:64], in_=banks[bi][:, :].rearrange("p (h c) -> p h c", h=8)))
                        ckv_free[gi] = mv
                        P['pkr_free'] = bank_free[0]
                        P['pks_free'] = bank_free[1]

                    interleave((p1_tile(g, t) for g in range(NG) for t in range(4)), 2)
                    p1_bar = dp.last() + [sn_free, cq_free]
                    dp.retire_since(mk1)

                mk2 = dp.mark()
                with ExitStack() as p2:
                    def sb2(name, shape, dt): return p2.enter_context(nc.sbuf_tensor(U(name), shape, dt))
                    wq = sb2("wq", [128, 2, 768], BF16)
                    wqs = sb2("wqs", [128, 2, 768], BF16)
                    wo = sb2("wo", [128, 8, 1024], BF16)
                    cqT = [sb2("cqT%d" % i, [128, 2, 512], BF16) for i in range(2)]
                    Ct = sb2("Ct2", [128, 512], F32)
                    St = sb2("St2", [128, 512], F32)
                    qt1 = [sb2("qt1_%d" % i, [128, 512], F32) for i in range(2)]
                    qt2 = [sb2("qt2_%d" % i, [128, 512], F32) for i in range(2)]
                    qt_free = [None, None]
                    QT = sb2("QT", [128, 8, 512], BF16)
                    pTs = [sb2("pTs%d" % i, [128, 1024], BF16) for i in range(3)]
                    osb = [sb2("osb%d" % i, [128, 512], F32) for i in range(2)]
                    rinv = [sb2("rinv%d" % i, [128, 512], F32) for i in range(2)]
                    aT = [sb2("aT%d" % i, [128, 512], BF16) for i in range(2)]
                    merged = [sb2("merged%d" % i, [128, D], BF16) for i in range(2)]
                    mT = [sb2("mT%d" % i, [128, 8, 128], BF16) for i in range(2)]
                    otmp = sb2("otmp", [128, D], F32)
                    xr = [sb2("xr%d" % i, [128, D], F32) for i in range(2)]
                    x1 = [sb2("x1_%d" % i, [128, D], F32) for i in range(2)]
                    junk = sb2("junk2", [128, D], BF16)
                    stt = [sb2("stq%d" % i, [128, 16], F32) for i in range(2)]

                    po = PC[:, 0:512]
                    prb = PC[:, 512:1024]
                    pmT = PC[:, 512:1024].bitcast(BF16)
                    pa = PD[:, :].bitcast(BF16)
                    scT = [PA, PB]

                    wl2 = [DMA('b1', p1_bar, lambda: nc.sync.dma_start(out=wq[:].rearrange("p k c -> p (k c)"), in_=wq_d)),
                           DMA('b1', p1_bar, lambda: nc.sync.dma_start(out=wqs[:].rearrange("p k c -> p (k c)"), in_=wqsw_d)),
                           DMA('b1', p1_bar, lambda: nc.sync.dma_start(out=wo[:].rearrange("p k c -> p (k c)"), in_=wo_d))]
                    ready2 = [p1_bar, wl2]
                    cq_free2 = [None, None]; rope_free = None; QT_free = []
                    sc_free = [None, None]; pTs_free = [None, None, None]; po_free = None; osb_free = [None, None]
                    rinv_free = [None, None]; prb_free = None; aT_free = [None, None]; pa_free = []
                    merged_free = [None, None]; mT_free = [None, None]; pmT_free = None
                    otmp_free = None; xr_free = [None, None]; x1_free = [None, None]
                    step = 0
                    tcount = 0
                    for qg in range(NQG[s]):
                        gi = qg % 2
                        gs = slice(qg * 512, (qg + 1) * 512)
                        lq = DMA('cql%d' % gi, [cq_free2[gi], ready2], lambda gi=gi, qg=qg: nc.sync.dma_start(
                            out=cqT[gi][:].rearrange("p k c -> p (k c)"), in_=cq_d[s * NG + qg]))
                        lr1 = DMA('rp2c', [rope_free, ready2], lambda gs=gs: nc.sync.dma_start(out=Ct[64:96, :], in_=rope_c[s, :, gs]))
                        lr2 = DMA('rp2s', [rope_free, ready2], lambda gs=gs: nc.sync.dma_start(out=St[64:96, :], in_=rope_s[s, :, gs]))
                        wq4 = wq[:].rearrange("p k (h c) -> p k h c", h=8)
                        wqs4 = wqs[:].rearrange("p k (h c) -> p k h c", h=8)
                        QT_w = []
                        Qs = dict(mqs=None, qd=None)

                        def q_head(h):
                            T = scT[h % 2]
                            hi = h % 2
                            mq = mmg(T[0:96, 0:512], [(wq4[:, k, h, :], cqT[gi][:, k, :]) for k in range(2)], [lq, sc_free[h % 2]])
                            mqs = mmg(T[0:96, 512:1024], [(wqs4[:, k, h, :], cqT[gi][:, k, :]) for k in range(2)], None)
                            Qs['mqs'] = mqs
                            yield
                            c0 = ACT([mq, QT_free if h == 0 else None], lambda: nc.scalar.copy(out=QT[0:64, h, :], in_=T[0:64, 0:512]))
                            q1 = DVE([mq, lr1, qt_free[hi]], lambda: nc.vector.tensor_tensor(out=qt1[hi][64:96, :], in0=T[64:96, 0:512], in1=Ct[64:96, :], op=ALU.mult))
                            q2 = DVE([mqs, lr2], lambda: nc.vector.tensor_tensor(out=qt2[hi][64:96, :], in0=T[64:96, 512:1024], in1=St[64:96, :], op=ALU.mult))
                            sc_free[h % 2] = [c0, q2]
                            yield
                            qd = DVE([q1, q2, QT_free if h == 0 else None], lambda: nc.vector.tensor_tensor(
                                out=QT[64:96, h, :], in0=qt1[hi][64:96, :], in1=qt2[hi][64:96, :], op=ALU.add))
                            qt_free[hi] = qd
                            Qs['qd'] = qd
                            QT_w.extend([c0, qd])
                        interleave((q_head(h) for h in range(8)), 2)
                        mqs = Qs['mqs']
                        qd = Qs['qd']
                        cq_free2[gi] = mqs
                        rope_free = qd
                        NP = KB // 2
                        steps = [(h, j) for h in range(8) for j in range(NP)]
                        qk_tok = {}

                        def emit_qk(idx):
                            h, j = steps[idx]
                            T = scT[idx % 2]
                            tk = None
                            for u in range(2):
                                kb = 2 * j + u
                                tk = PE([sc_free[idx % 2], QT_w] if u == 0 else None,
                                        lambda h=h, kb=kb, u=u, T=T: nc.tensor.matmul(
                                            T[:, u * 512:(u + 1) * 512], lhsT=KT[0:96, h, kb * 128:(kb + 1) * 128], rhs=QT[0:96, h, :],
                                            start=True, stop=True), sig=(u == 1))
                            qk_tok[idx] = tk

                        emit_qk(0)
                        pa_w = []
                        QT_readers = []
                        pending = []
                        A = dict(prb_free=prb_free)
                        for idx, (h, j) in enumerate(steps):
                            if idx + 1 < len(steps):
                                emit_qk(idx + 1)
                            T = scT[idx % 2]
                            sl = step % 3
                            step += 1
                            ex = ACT([qk_tok[idx], pTs_free[sl]], lambda T=T, sl=sl: nc.scalar.activation(out=pTs[sl][:], in_=T[:, :], func=AF.Exp))
                            sc_free[idx % 2] = ex
                            pvt = None
                            for u in range(2):
                                kb = 2 * j + u
                                pvt = PE([ex, po_free if (j == 0 and u == 0) else None],
                                         lambda h=h, kb=kb, u=u, sl=sl: nc.tensor.matmul(
                                             po[0:65, :], lhsT=VA[:, kb, h, :], rhs=pTs[sl][:, u * 512:(u + 1) * 512],
                                             start=(kb == 0), stop=(kb == KB - 1)), sig=(u == 1))
                            pTs_free[sl] = pvt
                            for pend in list(pending):
                                pend[0] -= 1
                                if pend[0] <= 0:
                                    pend[1]()
                                    pending.remove(pend)
                            if j == NP - 1:
                                for pend in list(pending):
                                    pend[1]()
                                    pending.remove(pend)
                                oi = h % 2
                                QT_readers.append(pvt)
                                o1 = DVE([pvt, osb_free[oi]], lambda oi=oi: nc.vector.tensor_copy(out=osb[oi][0:65, :], in_=po[0:65, :]))
                                po_free = o1
                                o2 = DVE([o1, rinv_free[oi]], lambda oi=oi: nc.vector.reciprocal(out=rinv[oi][64:65, :], in_=osb[oi][64:65, :]))
                                hs = dict(o2=o2, oi=oi, h=h)

                                def part_a(hs=hs):
                                    oi = hs['oi']
                                    o3 = PE([hs['o2'], A['prb_free']], lambda oi=oi: nc.tensor.matmul(prb[0:64, :], lhsT=ones_f[64:65, 0:64], rhs=rinv[oi][64:65, :],
                                                                                                 start=True, stop=True))
                                    rinv_free[oi] = o3
                                    o4 = DVE([o3, aT_free[oi]], lambda oi=oi: nc.vector.tensor_tensor(out=aT[oi][0:64, :], in0=osb[oi][0:64, :],
                                                                                                       in1=prb[0:64, :], op=ALU.mult))
                                    A['prb_free'] = o4
                                    osb_free[oi] = o4
                                    hs['o4'] = o4

                                def part_b(hs=hs):
                                    oi = hs['oi']; h = hs['h']
                                    o5 = None
                                    for t in range(4):
                                        o5 = PE([hs['o4'], pa_free if h == 0 else None] if t == 0 else None,
                                                lambda t=t, h=h, oi=oi: nc.tensor.transpose(
                                                    pa[:, t * 512 + h * 64: t * 512 + (h + 1) * 64], aT[oi][0:64, t * 128:(t + 1) * 128], ident_b[0:64, 0:64]),
                                                sig=(t == 3))
                                    aT_free[oi] = o5
                                    pa_w.append(o5)
                                pending.append([2, part_a])
                                pending.append([4, part_b])
                        for pend in list(pending):
                            pend[1]()
                            pending.remove(pend)
                        prb_free = A['prb_free']
                        QT_free = QT_readers
                        pa_r = []
                        M = dict(pmT_free=pmT_free, prb_free=prb_free, otmp_free=otmp_free)

                        def mg_tile(t, ti):
                            tok0 = s * S + qg * 512 + t * 128
                            otok0 = qbase + qg * 512 + t * 128
                            st_ = stt[ti]
                            lsn = DMA('snl%d' % ti, [merged_free[ti], ready2], lambda: nc.sync.dma_start(
                                out=merged[ti][:, 512:1024], in_=sn_d[tok0:tok0 + 128, :]))
                            lxr = DMA('xr%d' % ti, [xr_free[ti], ready2], lambda: nc.sync.dma_start(
                                out=xr[ti][:], in_=xs[tok0:tok0 + 128, :]))
                            a1 = ACT(pa_w, lambda: nc.scalar.activation(out=junk[:, 0:512], in_=pa[:, t * 512:(t + 1) * 512], func=AF.Square,
                                                                        accum_out=st_[:, 0:1]))
                            yield
                            r1a = ACT(a1, lambda: nc.scalar.activation(out=st_[:, 1:2], in_=st_[:, 0:1], func=AF.Sqrt, bias=EPS, scale=1.0 / 512))
                            yield
                            r1 = DVE(r1a, lambda: nc.vector.reciprocal(out=st_[:, 1:2], in_=st_[:, 1:2]))
                            c1 = ACT([r1, merged_free[ti]], lambda: nc.scalar.activation(
                                out=merged[ti][:, 0:512], in_=pa[:, t * 512:(t + 1) * 512], func=AF.Copy, scale=st_[:, 1:2]))
                            pa_r.append(c1)
                            yield
                            tp = None
                            for k in range(8):
                                tp = PE([c1, lsn, M['pmT_free'], M['prb_free']] if k == 0 else None,
                                        lambda k=k: nc.tensor.transpose(pmT[:, k * 128:(k + 1) * 128], merged[ti][:, k * 128:(k + 1) * 128], ident_b[:]),
                                        sig=(k == 7))
                            merged_free[ti] = tp
                            yield
                            ev = DVE([tp, mT_free[ti]], lambda: nc.vector.tensor_copy(out=mT[ti][:].rearrange("p k c -> p (k c)"), in_=pmT))
                            M['pmT_free'] = ev
                            M['prb_free'] = ev
                            yield
                            T = scT[t % 2]
                            mo1 = mmg(T[:, 0:512], [(mT[ti][:, k, :], wo[:, k, 0:512]) for k in range(8)], [ev, sc_free[t % 2]])
                            mo2 = mmg(T[:, 512:1024], [(mT[ti][:, k, :], wo[:, k, 512:1024]) for k in range(8)], None)
                            mT_free[ti] = mo2
                            yield
                            a2 = ACT(mo2, lambda: nc.scalar.activation(out=junk[:], in_=T[:, :], func=AF.Square, accum_out=st_[:, 2:3]))
                            yield
                            r2a = ACT(a2, lambda: nc.scalar.activation(out=st_[:, 3:4], in_=st_[:, 2:3], func=AF.Sqrt, bias=EPS, scale=1.0 / D))
                            yield
                            r2 = DVE(r2a, lambda: nc.vector.reciprocal(out=st_[:, 3:4], in_=st_[:, 3:4]))
                            d1 = DVE([r2, M['otmp_free']], lambda: nc.vector.scalar_tensor_tensor(
                                out=otmp[:], in0=T[:, :], scalar=st_[:, 3:4], in1=gvec1[:], op0=ALU.mult, op1=ALU.mult))
                            sc_free[t % 2] = d1
                            pp = POOL([d1, lxr, x1_free[ti]], lambda: nc.gpsimd.tensor_tensor(out=x1[ti][:], in0=otmp[:], in1=xr[ti][:], op=ALU.add))
                            M['otmp_free'] = pp
                            xr_free[ti] = pp
                            x1_free[ti] = DMA('x1s%d' % ti, pp, lambda: nc.sync.dma_start(
                                out=x1_d[otok0:otok0 + 128, :], in_=x1[ti][:]))
                        interleave((mg_tile(t, (tcount + t) % 2) for t in range(4)), 2)
                        tcount += 4
                        pmT_free = M['pmT_free']; prb_free = M['prb_free']; otmp_free = M['otmp_free']
                        pa_free = pa_r
                    p2_bar = dp.last() + [x1_free]
                    dp.retire_since(mk2)
                seq_bar = p2_bar
                qbase += NQG[s] * 512
            stageA_bar = seq_bar

        NTT = NQT // 128
        gseq = []
        for s in range(NSEQ):
            gseq += [s] * (NQG[s] * 4)
        with ExitStack() as bes:
            def sbB(name, shape, dt): return bes.enter_context(nc.sbuf_tensor(U(name), shape, dt))
            geff2_ = [sbB("geff2_%d" % i, [128, D], F32) for i in range(2)]
            sh2r_ = [sbB("sh2r_%d" % i, [128, D], F32) for i in range(2)]
            gvec2_ = [sbB("gvec2_%d" % i, [128, D], F32) for i in range(2)]
            vt = [sbB("vtB%d" % i, [128, D], F32) for i in range(2)]
            M1a = sbB("M1a", [128, NTT, 32], F32)
            M2a = sbB("M2a", [128, NTT, 32], F32)
            W1a = sbB("W1a", [128, NTT], F32)
            W2a = sbB("W2a", [128, NTT], F32)
            R1a = sbB("R1a", [128, NTT], F32)
            R2a = sbB("R2a", [128, NTT], F32)
            slot0 = sbB("slot0", [128, NTT], I32)
            slot1 = sbB("slot1", [128, NTT], I32)
            idxw = sbB("idxw", [128, NSL], I32)
            carry = sbB("carry", [128, 32], F32)
            PS = [bes.enter_context(nc.psum_tensor(U("PS%d" % i), [128, 512], F32)) for i in range(8)]
            bb = stageA_bar
            readyB = [bb, wcast_tok, zero_tok]

            def load_vecs(s, deps):
                geff2 = geff2_[s % 2]; sh2r = sh2r_[s % 2]; gvec2 = gvec2_[s % 2]
                l1 = DMA('v0_0', deps, lambda s=s: nc.sync.dma_start(out=vt[0][:], in_=mod_d[s, 4096:5120].partition_broadcast(128)))
                l2 = DMA('v0_1', deps, lambda: nc.sync.dma_start(out=vt[1][:], in_=g_pre2.partition_broadcast(128)))
                l3 = DMA('v0_2', deps, lambda s=s: nc.sync.dma_start(out=sh2r[:], in_=mod_d[s, 3072:4096].partition_broadcast(128)))
                tg = DVE([l1, l2], lambda: nc.vector.scalar_tensor_tensor(out=geff2[:], in0=vt[0][:], scalar=1.0, in1=vt[1][:],
                                                                           op0=ALU.add, op1=ALU.mult))
                l4 = DMA('v0_3', [tg], lambda s=s: nc.sync.dma_start(out=vt[0][:], in_=mod_d[s, 5120:6144].partition_broadcast(128)))
                l5 = DMA('v0_4', [tg], lambda: nc.sync.dma_start(out=vt[1][:], in_=g_post2.partition_broadcast(128)))
                tg2 = DVE([l4, l5], lambda: nc.vector.tensor_tensor(out=gvec2[:], in0=vt[0][:], in1=vt[1][:], op=ALU.mult))
                return [l3, tg, tg2]

            mkb1 = dp.mark()
            with ExitStack() as b1:
                def sb1(name, shape, dt): return b1.enter_context(nc.sbuf_tensor(U(name), shape, dt))
                w_r = sb1("w_r", [128, 8, 36], F32)
                brr = sb1("brr", [128, 36], F32)
                utri = sb1("utri", [128, 128], BF16)
                onesb = sb1("onesb", [128, 128], BF16)
                x1t = [sb1("x1t%d" % i, [128, D], F32) for i in range(3)]
                junk = sb1("junkB", [128, D], BF16)
                hm = sb1("hmB", [128, D], F32)
                h2 = [sb1("h2_%d" % i, [128, D], F32) for i in range(3)]
                h2Tf = [sb1("h2Tf%d" % i, [128, 8, 128], F32) for i in range(3)]
                stt = [sb1("stB%d" % i, [128, 8], F32) for i in range(3)]
                lg = [sb1("lg%d" % i, [128, 36], F32) for i in range(3)]
                wk = [sb1("wk%d" % i, [128, 192], F32) for i in range(3)]
                ohb = [sb1("ohb%d" % i, [128, 32], BF16) for i in range(3)]
                PH = [PS[6], PS[7]]
                ld = [DMA('s0', bb, lambda: nc.sync.dma_start(out=w_r[:, :, 0:4], in_=w_rg.rearrange("(k p) c -> p k c", p=128))),
                      DMA('s0', bb, lambda: nc.sync.dma_start(out=w_r[:, :, 4:36], in_=w_re.rearrange("(k p) c -> p k c", p=128))),
                      DMA('s0', bb, lambda: nc.sync.dma_start(out=brr[:, 0:4], in_=b_rg.partition_broadcast(128))),
                      DMA('s0', bb, lambda: nc.sync.dma_start(out=brr[:, 4:36], in_=b_re.partition_broadcast(128))),
                      DMA('s1', bb, lambda: nc.gpsimd.dma_start(out=utri[:], in_=utri_in), q='pool'),
                      POOL(bb, lambda: nc.gpsimd.memset(onesb[:], 1.0)),
                      POOL(bb, lambda: nc.gpsimd.memset(carry[:], 0.0))]
                rdy1 = [readyB, ld]
                T = dict(cur_seq=-1, vec_ready=None, vec_readers=[], hm_free=None, PH_free=[None, None], plg_free=None,
                         pcum_free=None, carry_tok=ld[-1])
                x1t_free = [None] * 3; h2_free = [None] * 3
                h2Tf_free = [None] * 3
                h2d_w = []

                def p1_tile(i):
                    s = gseq[i]
                    if s != T['cur_seq']:
                        T['cur_seq'] = s
                        T['vec_ready'] = load_vecs(s, [rdy1, T['vec_readers']])
                        T['vec_readers'] = []
                    vec_ready = T['vec_ready']
                    geff2 = geff2_[s % 2]; sh2r = sh2r_[s % 2]
                    ti = i % 3
                    tok0 = i * 128
                    st_ = stt[ti]
                    lx = DMA('bx%d' % ti, [x1t_free[ti], rdy1], lambda ti=ti, tok0=tok0: nc.sync.dma_start(out=x1t[ti][:], in_=x1_d[tok0:tok0 + 128, :]))
                    a1 = ACT(lx, lambda ti=ti, st_=st_: nc.scalar.activation(out=junk[:], in_=x1t[ti][:], func=AF.Square, accum_out=st_[:, 0:1]))
                    yield
                    r1a = ACT(a1, lambda st_=st_: nc.scalar.activation(out=st_[:, 1:2], in_=st_[:, 0:1], func=AF.Sqrt, bias=EPS, scale=1.0 / D))
                    yield
                    r1 = DVE(r1a, lambda st_=st_: nc.vector.reciprocal(out=st_[:, 1:2], in_=st_[:, 1:2]))
                    d1 = DVE([r1, T['hm_free'], vec_ready], lambda ti=ti, st_=st_: nc.vector.scalar_tensor_tensor(
                        out=hm[:], in0=x1t[ti][:], scalar=st_[:, 1:2], in1=geff2[:], op0=ALU.mult, op1=ALU.mult))
                    x1t_free[ti] = d1
                    p1_ = POOL([d1, h2_free[ti], vec_ready], lambda ti=ti: nc.gpsimd.tensor_tensor(out=h2[ti][:], in0=hm[:], in1=sh2r[:], op=ALU.add))
                    T['hm_free'] = p1_
                    T['vec_readers'] = [p1_, d1]
                    wr = DMA('h2w%d' % ti, p1_, lambda ti=ti, tok0=tok0: nc.gpsimd.dma_start(out=h2_d[tok0:tok0 + 128, :], in_=h2[ti][:]), q='pool')
                    h2d_w.append(wr)
                    yield
                    tp = None
                    for k in range(8):
                        bank = PH[k // 4]
                        tp = PE([p1_, T['PH_free']] if k == 0 else None,
                                lambda k=k, ti=ti, bank=bank: nc.tensor.transpose(bank[:, (k % 4) * 128:(k % 4 + 1) * 128],
                                                                                  h2[ti][:, k * 128:(k + 1) * 128], ident_f[:]),
                                sig=(k == 7))
                    h2_free[ti] = [tp, wr]
                    yield
                    e1 = ACT([tp, h2Tf_free[ti]], lambda ti=ti: nc.scalar.copy(out=h2Tf[ti][:, 0:4, :], in_=PH[0][:, :].rearrange("p (k c) -> p k c", k=4)))
                    e2 = DVE([tp, h2Tf_free[ti]], lambda ti=ti: nc.vector.tensor_copy(out=h2Tf[ti][:, 4:8, :], in_=PH[1][:, :].rearrange("p (k c) -> p k c", k=4)))
                    T['PH_free'] = [e1, e2]
                    yield
                    plg = PS[4][:, 0:36]
                    m_l = mmg(plg, [(h2Tf[ti][:, k, :], w_r[:, k, :]) for k in range(8)], [e1, e2, T['plg_free'], rdy1])
                    h2Tf_free[ti] = m_l
                    yield
                    L = lg[ti]; W = wk[ti]
                    v1 = DVE([m_l], lambda L=L: nc.vector.tensor_tensor(out=L[:], in0=plg, in1=brr[:], op=ALU.add))
                    T['plg_free'] = v1
                    v2 = DVE(v1, lambda L=L, W=W: nc.vector.tensor_reduce(out=W[:, 0:1], in_=L[:, 0:4], axis=AX.X, op=ALU.max))
                    v3 = DVE(v2, lambda W=W: nc.vector.tensor_scalar(out=W[:, 1:2], in0=W[:, 0:1], scalar1=-1.0, scalar2=None, op0=ALU.mult))
                    v4 = DVE(v2, lambda L=L, W=W: nc.vector.tensor_scalar(out=W[:, 4:8], in0=L[:, 0:4], scalar1=W[:, 0:1], scalar2=None, op0=ALU.is_equal))
                    s1 = ACT([v3], lambda L=L, W=W: nc.scalar.activation(out=W[:, 8:12], in_=L[:, 0:4], func=AF.Exp, bias=W[:, 1:2], scale=1.0,
                                                                         accum_out=W[:, 2:3]))
                    yield
                    v5 = DVE(s1, lambda W=W: nc.vector.reciprocal(out=W[:, 3:4], in_=W[:, 2:3]))
                    v6 = DVE(v4, lambda L=L, W=W: nc.vector.tensor_tensor(
                        out=W[:, 16:48].rearrange("p (g e) -> p g e", g=4), in0=L[:, 4:36].rearrange("p (g e) -> p g e", g=4),
                        in1=W[:, 4:8].unsqueeze(2).to_broadcast([128, 4, 8]), op=ALU.mult))
                    v7 = DVE(v6, lambda W=W: nc.vector.tensor_reduce(out=W[:, 48:56], in_=W[:, 16:48].rearrange("p (g e) -> p e g", g=4),
                                                                    axis=AX.X, op=ALU.add))
                    v8 = DVE(v7, lambda W=W: nc.vector.tensor_reduce(out=W[:, 12:13], in_=W[:, 48:56], axis=AX.X, op=ALU.max))
                    v9 = DVE(v8, lambda W=W: nc.vector.tensor_scalar(out=W[:, 56:64], in0=W[:, 48:56], scalar1=W[:, 12:13], scalar2=None,
                                                                    op0=ALU.is_equal))
                    v10 = DVE(v9, lambda W=W: nc.vector.scalar_tensor_tensor(out=W[:, 64:72], in0=W[:, 56:64], scalar=-1e30, in1=W[:, 48:56],
                                                                            op0=ALU.mult, op1=ALU.add))
                    v11 = DVE(v10, lambda W=W: nc.vector.tensor_reduce(out=W[:, 13:14], in_=W[:, 64:72], axis=AX.X, op=ALU.max))
                    v12 = DVE(v11, lambda W=W: nc.vector.tensor_scalar(out=W[:, 72:80], in0=W[:, 64:72], scalar1=W[:, 13:14], scalar2=None,
                                                                      op0=ALU.is_equal))
                    v13 = DVE(v11, lambda W=W: nc.vector.tensor_scalar(out=W[:, 14:15], in0=W[:, 12:13], scalar1=-1.0, scalar2=None, op0=ALU.mult))
                    s2 = ACT([v13], lambda W=W: nc.scalar.activation(out=W[:, 15:16], in_=W[:, 13:14], func=AF.Exp, bias=W[:, 14:15], scale=1.0))
                    yield
                    v14 = DVE(s2, lambda W=W: nc.vector.tensor_scalar(out=W[:, 80:81], in0=W[:, 15:16], scalar1=1.0, scalar2=None, op0=ALU.add))
                    v15 = DVE(v14, lambda W=W: nc.vector.reciprocal(out=W[:, 81:82], in_=W[:, 80:81]))
                    v16 = DVE([v15, v5], lambda W=W, i=i: nc.vector.tensor_tensor(out=W1a[:, i:i + 1], in0=W[:, 81:82], in1=W[:, 3:4], op=ALU.mult))
                    v17 = DVE(v16, lambda W=W, i=i: nc.vector.tensor_tensor(out=W2a[:, i:i + 1], in0=W1a[:, i:i + 1], in1=W[:, 15:16], op=ALU.mult))
                    v18 = DVE([v9, v4], lambda W=W, i=i: nc.vector.tensor_tensor(
                        out=M1a[:, i, :].rearrange("p (g e) -> p g e", g=4), in0=W[:, 4:8].unsqueeze(2).to_broadcast([128, 4, 8]),
                        in1=W[:, 56:64].unsqueeze(1).to_broadcast([128, 4, 8]), op=ALU.mult))
                    v19 = DVE([v12], lambda W=W, i=i: nc.vector.tensor_tensor(
                        out=M2a[:, i, :].rearrange("p (g e) -> p g e", g=4), in0=W[:, 4:8].unsqueeze(2).to_broadcast([128, 4, 8]),
                        in1=W[:, 72:80].unsqueeze(1).to_broadcast([128, 4, 8]), op=ALU.mult))
                    OH = ohb[ti]
                    v20 = DVE([v18, v19, T['pcum_free']], lambda OH=OH, i=i: nc.vector.tensor_tensor(out=OH[:], in0=M1a[:, i, :], in1=M2a[:, i, :], op=ALU.add))
                    pcum = PS[5][:, 0:32]
                    ptot = PS[5][:, 32:64]
                    PE([v20, T['pcum_free'], rdy1], lambda OH=OH: nc.tensor.matmul(pcum, lhsT=utri[:], rhs=OH[:], start=True, stop=True), sig=False)
                    mc = PE(None, lambda OH=OH: nc.tensor.matmul(ptot, lhsT=onesb[:], rhs=OH[:], start=True, stop=True))
                    yield
                    v21 = DVE([mc, T['carry_tok']], lambda W=W: nc.vector.tensor_tensor(out=W[:, 96:128], in0=carry[:], in1=pcum, op=ALU.add))
                    v22 = DVE(v21, lambda: nc.vector.tensor_tensor(out=carry[:], in0=carry[:], in1=ptot, op=ALU.add))
                    T['carry_tok'] = v22
                    T['pcum_free'] = v22
                    v23 = DVE(v22, lambda W=W, i=i: nc.vector.tensor_tensor(out=W[:, 128:160], in0=W[:, 96:128], in1=M1a[:, i, :], op=ALU.mult))
                    v24 = DVE(v23, lambda W=W, i=i: nc.vector.tensor_reduce(out=R1a[:, i:i + 1], in_=W[:, 128:160], axis=AX.X, op=ALU.add))
                    v25 = DVE(v24, lambda W=W, i=i: nc.vector.tensor_tensor(out=W[:, 160:192], in0=W[:, 96:128], in1=M2a[:, i, :], op=ALU.mult))
                    v26 = DVE(v25, lambda W=W, i=i: nc.vector.tensor_reduce(out=R2a[:, i:i + 1], in_=W[:, 160:192], axis=AX.X, op=ALU.add))
                interleave((p1_tile(i) for i in range(NTT)), 3)
                b1_bar = dp.last() + [h2d_w]
                dp.retire_since(mkb1)

            with ExitStack() as b2:
                def sb2(name, shape, dt): return b2.enter_context(nc.sbuf_tensor(U(name), shape, dt))
                jv = sb2("jv", [128, NSL], F32)
                pidx = sb2("pidx", [128, 1], F32)
                tri32 = sb2("tri32", [32, 32], F32)
                cmp_ = sb2("cmp", [128, NSL * 32], F32)
                tmpM = sb2("tmpM", [128, NTT, 32], F32)
                nblk = sb2("nblk", [128, 32], F32)
                pc = sb2("pc", [128, 32], F32)
                pcT = sb2("pcT", [32, 128], F32)
                sst = sb2("sst", [128, 32], F32)
                send = sb2("send", [128, 32], F32)
                te = sb2("te", [128, NSL], F32)
                sf = sb2("sf", [128, NTT], F32)
                l = [DMA('i0', b1_bar, lambda: nc.sync.dma_start(out=jv[:], in_=jv_in)),
                     DMA('i0', b1_bar, lambda: nc.sync.dma_start(out=pidx[:], in_=pidx_in)),
                     DMA('i0', b1_bar, lambda: nc.sync.dma_start(out=tri32[:], in_=tri32_in))]
                c3 = cmp_[:].rearrange("p (e m) -> p e m", e=32)
                q1 = DVE([l, b1_bar], lambda: nc.vector.tensor_tensor(out=c3, in0=jv[:].unsqueeze(1).to_broadcast([128, 32, NSL]),
                                                                      in1=carry[:].unsqueeze(2).to_broadcast([128, 32, NSL]), op=ALU.is_lt))
                q2 = DVE(q1, lambda: nc.vector.tensor_reduce(out=nblk[:], in_=c3, axis=AX.X, op=ALU.add))
                q3 = DVE(q2, lambda: nc.vector.tensor_scalar(out=pc[:], in0=nblk[:], scalar1=128.0, scalar2=None, op0=ALU.mult))
                q4 = PE(q3, lambda: nc.tensor.transpose(PS[0][0:32, 0:128], pc[:, :], ident_f[:]))
                q5 = ACT(q4, lambda: nc.scalar.copy(out=pcT[:], in_=PS[0][0:32, 0:128]))
                q6 = PE([q5, l], lambda: nc.tensor.matmul(PS[1][:, 0:32], lhsT=pcT[:, :], rhs=tri32[:, :], start=True, stop=True))
                q7 = DVE(q6, lambda: nc.vector.tensor_copy(out=sst[:], in_=PS[1][:, 0:32]))
                q8 = DVE(q7, lambda: nc.vector.tensor_tensor(out=send[:], in0=sst[:], in1=pc[:], op=ALU.add))
                c4 = cmp_[:].rearrange("p (m e) -> p m e", e=32)
                q9 = DVE(q8, lambda: nc.vector.tensor_tensor(out=c4, in0=send[:].unsqueeze(1).to_broadcast([128, NSL, 32]),
                                                             in1=jv[:].unsqueeze(2).to_broadcast([128, NSL, 32]), op=ALU.is_le))
                q10 = DVE(q9, lambda: nc.vector.tensor_reduce(out=te[:], in_=c4, axis=AX.X, op=ALU.add))
                q11 = DVE(q10, lambda: nc.vector.tensor_scalar(out=te[:], in0=te[:], scalar1=31.0, scalar2=128.0, op0=ALU.min, op1=ALU.mult))
                q12 = DVE(q11, lambda: nc.vector.tensor_scalar(out=te[:], in0=te[:], scalar1=pidx[:, 0:1], scalar2=None, op0=ALU.add))
                q13 = DVE(q12, lambda: nc.vector.tensor_copy(out=idxw[:], in_=te[:]))
                q14 = DVE(q7, lambda: nc.vector.tensor_tensor(out=tmpM[:], in0=M1a[:], in1=sst[:].unsqueeze(1).to_broadcast([128, NTT, 32]), op=ALU.mult))
                q15 = DVE(q14, lambda: nc.vector.tensor_reduce(out=sf[:], in_=tmpM[:], axis=AX.X, op=ALU.add))
                q16 = DVE(q15, lambda: nc.vector.tensor_tensor(out=sf[:], in0=sf[:], in1=R1a[:], op=ALU.add))
                q17 = DVE(q16, lambda: nc.vector.tensor_copy(out=slot0[:], in_=sf[:]))
                q18 = DVE(q17, lambda: nc.vector.tensor_tensor(out=tmpM[:], in0=M2a[:], in1=sst[:].unsqueeze(1).to_broadcast([128, NTT, 32]), op=ALU.mult))
                q19 = DVE(q18, lambda: nc.vector.tensor_reduce(out=sf[:], in_=tmpM[:], axis=AX.X, op=ALU.add))
                q20 = DVE(q19, lambda: nc.vector.tensor_tensor(out=sf[:], in0=sf[:], in1=R2a[:], op=ALU.add))
                q21 = DVE(q20, lambda: nc.vector.tensor_copy(out=slot1[:], in_=sf[:]))
                b2_bar = dp.last()

            mkb3 = dp.mark()
            with ExitStack() as b3:
                def sb3(name, shape, dt): return b3.enter_context(nc.sbuf_tensor(U(name), shape, dt))
                hsc = [sb3("hsc%d" % i, [128, D], BF16) for i in range(3)]
                hsc_free = [None] * 3
                sc_toks = []
                for i in range(NTT):
                    si = i % 3
                    tok0 = i * 128
                    lh = DMA('hl%d' % si, [hsc_free[si], b2_bar], lambda si=si, tok0=tok0: nc.sync.dma_start(out=hsc[si][:], in_=h2_d[tok0:tok0 + 128, :]))
                    s0 = DMA('sc%d' % si, [lh, b2_bar], lambda si=si, i=i: nc.gpsimd.indirect_dma_start(
                        out=xs_d[:, :], out_offset=bass.IndirectOffsetOnAxis(ap=slot0[:, i:i + 1], axis=0), in_=hsc[si][:, :], in_offset=None), q='pool')
                    s1_ = DMA('sc%d' % si, [lh], lambda si=si, i=i: nc.gpsimd.indirect_dma_start(
                        out=xs_d[:, :], out_offset=bass.IndirectOffsetOnAxis(ap=slot1[:, i:i + 1], axis=0), in_=hsc[si][:, :], in_offset=None), q='pool')
                    hsc_free[si] = [s0, s1_]
                    sc_toks += [s0, s1_]
                scat_done = [sc_toks[-6:], b2_bar]

                PF = 3
                NW = PF + 3
                ND = PF + 5
                NX = PF + 2
                wgu = [sb3("wgu%d" % i, [128, 8, 512], BF16) for i in range(NW)]
                wdb = [sb3("wdb%d" % i, [128, 2, D], BF16) for i in range(ND)]
                xsb = [sb3("xsb%d" % i, [128, D], BF16) for i in range(NX)]
                xT = [sb3("xT%d" % i, [128, 8, 128], BF16) for i in range(2)]
                sgs = [sb3("sgs%d" % i, [128, 256], F32) for i in range(2)]
                hid = [sb3("hid%d" % i, [128, 256], BF16) for i in range(2)]
                hT = [sb3("hT%d" % i, [128, 2, 128], BF16) for i in range(2)]
                ysb = [sb3("ysb%d" % i, [128, D], F32) for i in range(2)]
                pX = [PS[0][:, :].bitcast(BF16), PS[1][:, :].bitcast(BF16)]
                pH = [PS[2], PS[3]]
                pHT = [PS[4][:, 0:128].bitcast(BF16), PS[5][:, 0:128].bitcast(BF16)]
                pY = [PS[6], PS[7]]
                wgu_free = [None] * NW; wdb_free = [None] * ND; xsb_free = [None] * NX
                pX_free = [None, None]; xT_free = [None, None]; pH_free = [None, None]; sgs_free = [None, None]
                hid_free = [None, None]; pHT_free = [None, None]; hT_free = [None, None]
                pY_free = [None, None]; ysb_free = [None, None]
                st0 = {}; st1 = {}; st2 = {}; ldt = {}
                ys_w = []

                def issue_loads(a):
                    wi = a % NW; di = a % ND; xj = a % NX
                    lw = DMA('wgl%d' % wi, [wgu_free[wi], scat_done], lambda wi=wi, a=a: nc.gpsimd.indirect_dma_start(
                        out=wgu[wi][:].rearrange("p k c -> p (k c)"), out_offset=None, in_=wgu_r[:, :],
                        in_offset=bass.IndirectOffsetOnAxis(ap=idxw[:, a:a + 1], axis=0)), q='pool')
                    lwd = DMA('wdl%d' % di, [wdb_free[di], scat_done], lambda di=di, a=a: nc.gpsimd.indirect_dma_start(
                        out=wdb[di][:].rearrange("p k c -> p (k c)"), out_offset=None, in_=wd_r[:, :],
                        in_offset=bass.IndirectOffsetOnAxis(ap=idxw[:, a:a + 1], axis=0)), q='pool')
                    lxs = DMA('xsl%d' % xj, [xsb_free[xj], scat_done, sc_toks], lambda xj=xj, a=a: nc.sync.dma_start(
                        out=xsb[xj][:], in_=xs_d[a * 128:(a + 1) * 128, :]))
                    ldt[a] = (lw, lwd, lxs)

                for a in range(min(PF, NSL)):
                    issue_loads(a)
                for it in range(NSL + 3):
                    if it + PF < NSL:
                        issue_loads(it + PF)
                    a = it
                    if a < NSL:
                        xi = a % 2; xj = a % NX
                        lw, lwd, lxs = ldt[a]
                        tp = None
                        for k in range(8):
                            tp = PE([lxs, pX_free[xi]] if k == 0 else None,
                                    lambda k=k, xi=xi, xj=xj: nc.tensor.transpose(pX[xi][:, k * 128:(k + 1) * 128], xsb[xj][:, k * 128:(k + 1) * 128], ident_b[:]),
                                    sig=(k == 7))
                        xsb_free[xj] = tp
                        if a % 2 == 0:
                            ev = ACT([tp, xT_free[xi]], lambda xi=xi: nc.scalar.copy(out=xT[xi][:].rearrange("p k c -> p (k c)"), in_=pX[xi]))
                        else:
                            ev = DVE([tp, xT_free[xi]], lambda xi=xi: nc.vector.tensor_copy(out=xT[xi][:].rearrange("p k c -> p (k c)"), in_=pX[xi]))
                        pX_free[xi] = ev
                        st0[a] = (ev, lw, lwd)
                    a = it - 1
                    if 0 <= a < NSL:
                        wi = a % NW; xi = a % 2
                        ev, lw, lwd = st0[a]
                        mh = mmg(pH[xi][:, :], [(xT[xi][:, k, :], wgu[wi][:, k, :]) for k in range(8)], [ev, lw, pH_free[xi]])
                        wgu_free[wi] = mh
                        xT_free[xi] = mh
                        a_s = ACT([mh, sgs_free[xi]], lambda xi=xi: nc.scalar.activation(out=sgs[xi][:], in_=pH[xi][:, 0:256], func=AF.Silu))
                        d_h = DVE([a_s, hid_free[xi]], lambda xi=xi: nc.vector.tensor_tensor(out=hid[xi][:], in0=sgs[xi][:], in1=pH[xi][:, 256:512], op=ALU.mult))
                        pH_free[xi] = d_h
                        sgs_free[xi] = d_h
                        st1[a] = (d_h, lwd)
                    a = it - 2
                    if 0 <= a < NSL:
                        xi = a % 2
                        d_h, lwd = st1[a]
                        tp2 = None
                        for j in range(2):
                            tp2 = PE([d_h, pHT_free[xi]] if j == 0 else None,
                                     lambda j=j, xi=xi: nc.tensor.transpose(pHT[xi][:, j * 128:(j + 1) * 128], hid[xi][:, j * 128:(j + 1) * 128], ident_b[:]),
                                     sig=(j == 1))
                        hid_free[xi] = tp2
                        ev2 = ACT([tp2, hT_free[xi]], lambda xi=xi: nc.scalar.copy(out=hT[xi][:].rearrange("p k c -> p (k c)"), in_=pHT[xi]))
                        pHT_free[xi] = ev2
                        st2[a] = (ev2, lwd)
                    a = it - 3
                    if 0 <= a < NSL:
                        xi = a % 2; di = a % ND
                        ev2, lwd = st2[a]
                        my0 = mmg(pY[0][:, :], [(hT[xi][:, j, :], wdb[di][:, j, 0:512]) for j in range(2)], [ev2, lwd, pY_free[0]])
                        my1 = mmg(pY[1][:, :], [(hT[xi][:, j, :], wdb[di][:, j, 512:1024]) for j in range(2)], [pY_free[1]])
                        wdb_free[di] = my1
                        hT_free[xi] = my1
                        c0 = ACT([my0, ysb_free[xi]], lambda xi=xi: nc.scalar.copy(out=ysb[xi][:, 0:512], in_=pY[0][:, :]))
                        c1 = DVE([my1, ysb_free[xi]], lambda xi=xi: nc.vector.tensor_copy(out=ysb[xi][:, 512:1024], in_=pY[1][:, :]))
                        pY_free = [c0, c1]
                        ysb_free[xi] = DMA('ysw%d' % xi, [c0, c1], lambda xi=xi, a=a: nc.sync.dma_start(out=ys_d[a * 128:(a + 1) * 128, :], in_=ysb[xi][:]))
                        ys_w.append(ysb_free[xi])
                b3_bar = dp.last() + [ys_w[-2:]]
                dp.retire_since(mkb3)

            with ExitStack() as b4:
                def sb4(name, shape, dt): return b4.enter_context(nc.sbuf_tensor(U(name), shape, dt))
                ya = [sb4("ya%d" % i, [128, D], F32) for i in range(3)]
                yb = [sb4("yb%d" % i, [128, D], F32) for i in range(3)]
                x1c = [sb4("x1c%d" % i, [128, D], F32) for i in range(3)]
                mm_ = [sb4("mm_%d" % i, [128, D], F32) for i in range(3)]
                ytmp = [sb4("ytmp%d" % i, [128, D], F32) for i in range(3)]
                yo = [sb4("yo%d" % i, [128, D], F32) for i in range(3)]
                junk = sb4("junkC", [128, D], BF16)
                stt = [sb4("stC%d" % i, [128, 8], F32) for i in range(3)]
                ya_free = [None] * 3; yb_free = [None] * 3; x1c_free = [None] * 3; mm_free = [None] * 3
                ytmp_free = [None] * 3; yo_free = [None] * 3
                T = dict(cur_seq=-1, vec_ready=None, vec_readers=[])
                out_toks = []

                def cmb_tile(i):
                    s = gseq[i]
                    if s != T['cur_seq']:
                        T['cur_seq'] = s
                        T['vec_ready'] = load_vecs(s, [b3_bar, T['vec_readers']])
                        T['vec_readers'] = []
                    vec_ready = T['vec_ready']
                    gvec2 = gvec2_[s % 2]
                    ti = i % 3
                    tok0 = i * 128
                    st_ = stt[ti]
                    ga = DMA('ga%d' % ti, [ya_free[ti], b3_bar, ys_w], lambda ti=ti, i=i: nc.gpsimd.indirect_dma_start(
                        out=ya[ti][:, :], out_offset=None, in_=ys_d[:, :], in_offset=bass.IndirectOffsetOnAxis(ap=slot0[:, i:i + 1], axis=0)), q='pool')
                    gb_ = DMA('gb%d' % ti, [yb_free[ti], b3_bar], lambda ti=ti, i=i: nc.gpsimd.indirect_dma_start(
                        out=yb[ti][:, :], out_offset=None, in_=ys_d[:, :], in_offset=bass.IndirectOffsetOnAxis(ap=slot1[:, i:i + 1], axis=0)), q='pool')
                    lx = DMA('cx%d' % ti, [x1c_free[ti], b3_bar], lambda ti=ti, tok0=tok0: nc.sync.dma_start(out=x1c[ti][:], in_=x1_d[tok0:tok0 + 128, :]))
                    yield
                    d1 = DVE([ga, mm_free[ti]], lambda ti=ti, i=i: nc.vector.tensor_scalar(out=mm_[ti][:], in0=ya[ti][:], scalar1=W1a[:, i:i + 1], scalar2=None, op0=ALU.mult))
                    ya_free[ti] = d1
                    d2 = DVE([gb_, d1], lambda ti=ti, i=i: nc.vector.scalar_tensor_tensor(out=mm_[ti][:], in0=yb[ti][:], scalar=W2a[:, i:i + 1], in1=mm_[ti][:],
                                                                                         op0=ALU.mult, op1=ALU.add))
                    yb_free[ti] = d2
                    a1 = ACT(d2, lambda ti=ti, st_=st_: nc.scalar.activation(out=junk[:], in_=mm_[ti][:], func=AF.Square, accum_out=st_[:, 0:1]))
                    yield
                    r1a = ACT(a1, lambda st_=st_: nc.scalar.activation(out=st_[:, 1:2], in_=st_[:, 0:1], func=AF.Sqrt, bias=EPS, scale=1.0 / D))
                    yield
                    r1 = DVE(r1a, lambda st_=st_: nc.vector.reciprocal(out=st_[:, 1:2], in_=st_[:, 1:2]))
                    d3 = DVE([r1, ytmp_free[ti], vec_ready], lambda ti=ti, st_=st_: nc.vector.scalar_tensor_tensor(
                        out=ytmp[ti][:], in0=mm_[ti][:], scalar=st_[:, 1:2], in1=gvec2[:], op0=ALU.mult, op1=ALU.mult))
                    mm_free[ti] = d3
                    T['vec_readers'] = [d3]
                    pp = POOL([d3, lx, yo_free[ti]], lambda ti=ti: nc.gpsimd.tensor_tensor(out=yo[ti][:], in0=ytmp[ti][:], in1=x1c[ti][:], op=ALU.add))
                    ytmp_free[ti] = pp
                    x1c_free[ti] = pp
                    yo_free[ti] = DMA('yo%d' % ti, pp, lambda ti=ti, tok0=tok0: nc.sync.dma_start(out=y[tok0:tok0 + 128, :], in_=yo[ti][:]))
                    out_toks.append(yo_free[ti])
                interleave((cmb_tile(i) for i in range(NTT)), 3)
            dp.wait('sp', [yo_free, out_toks[-3:]])
            for e in ('pe', 'act', 'dve', 'pool'):
                dp.wait('sp', [(e, dp.cnt[e])])
    return nc


def _rope_tables(pos):
    half = 16
    inv = (10000.0 ** (-np.arange(half, dtype=np.float32) / half)).astype(np.float32)
    ang = pos.astype(np.float32)[:, None] * inv[None, :]
    cos = np.cos(ang).astype(np.float32)
    sin = np.sin(ang).astype(np.float32)
    c = np.concatenate([cos, cos], axis=1).T
    s_ = np.concatenate([sin, sin], axis=1).T
    return np.ascontiguousarray(c), np.ascontiguousarray(s_)


def _consts(NSL):
    ident = np.eye(128, dtype=np.float32)
    egrp = np.zeros((8, 512), np.float32)
    for g in range(8):
        egrp[g, g * 64:(g + 1) * 64] = 1.0
    utri = np.triu(np.ones((128, 128), np.float32), k=1)
    tri32 = np.triu(np.ones((32, 32), np.float32), k=1)
    jv = np.tile((np.arange(NSL, dtype=np.float32) * 128.0)[None, :], (128, 1))
    pidx = np.arange(128, dtype=np.float32).reshape(128, 1)
    return dict(ident=ident, egrp=egrp, utri=utri, tri32=tri32, jv=np.ascontiguousarray(jv), pidx=pidx)


def _nt(cfg):
    nqt = sum(cfg['NQG']) * 512
    nt = (2 * nqt + 32 * 127 + 127) // 128
    return ((nt + 7) // 8) * 8


WEIGHT_KEYS = ['w_ada', 'b_ada', 'g_pre1', 'g_post1', 'g_pre2', 'g_post2', 'w_in', 'g_q', 'w_uq', 'g_kv', 'w_ukv',
               'g_v_gmlp', 'w_spatial', 'b_spatial', 'g_attn_out', 'g_gmlp_out', 'w_out', 'w_router_group',
               'b_router_group', 'w_router_expert', 'b_router_expert', 'w_gate', 'w_up', 'w_down']

_NC_CACHE = {}


def kernel(**inputs):
    S = 4096
    x_all = np.concatenate([np.asarray(inputs['x_prompt'], np.float32), np.asarray(inputs['x_sample'], np.float32)], axis=0)
    c_all = np.concatenate([np.asarray(inputs['c_prompt'], np.float32), np.asarray(inputs['c_sample'], np.float32)], axis=0)
    weights = {k: np.ascontiguousarray(np.asarray(inputs[k], np.float32)) for k in WEIGHT_KEYS}
    consts = _consts(_nt(FULL_CFG))
    pos_nat = np.arange(S)
    in_maps = []
    plans = []
    for c in range(8):
        if c % 2 == 0:
            s0 = (5 * c) // 2
            A, B, Cq, qhalf = s0, s0 + 1, s0 + 2, 0
        else:
            s0 = (5 * c - 1) // 2
            Cq, qhalf, A, B = s0, 1, s0 + 1, s0 + 2
        if qhalf == 0:
            posC = pos_nat
        else:
            posC = np.concatenate([pos_nat[S // 2:], pos_nat[:S // 2]])
        xs = np.concatenate([x_all[A], x_all[B], x_all[Cq][posC]], axis=0)
        cv = np.stack([c_all[A], c_all[B], c_all[Cq]], axis=0)
        rc = np.zeros((3, 32, S), np.float32)
        rs = np.zeros((3, 32, S), np.float32)
        for i, p in enumerate([pos_nat, pos_nat, posC]):
            rc[i], rs[i] = _rope_tables(p)
        m = dict(weights)
        m.update(xs=np.ascontiguousarray(xs), cvec=np.ascontiguousarray(cv), rope_c=rc, rope_s=rs)
        m.update(consts)
        in_maps.append(m)
        plans.append((A, B, Cq, qhalf))
    if 'full' not in _NC_CACHE:
        _NC_CACHE['full'] = build(FULL_CFG)
    nc = _NC_CACHE['full']
    res = run_bass_kernel_spmd(nc, in_maps, core_ids=list(range(8)))
    y_all = np.zeros((20, S, D), np.float32)
    for c in range(8):
        yc = res.results[c]['y']
        A, B, Cq, qhalf = plans[c]
        y_all[A] = yc[0:S]
        y_all[B] = yc[S:2 * S]
        if qhalf == 0:
            y_all[Cq, 0:S // 2] = yc[2 * S:2 * S + S // 2]
        else:
            y_all[Cq, S // 2:] = yc[2 * S:2 * S + S // 2]
    return (np.ascontiguousarray(y_all[0:4]), np.ascontiguousarray(y_all[4:20]))
```

```python
import numpy as np
import concourse.bass as bass
import concourse.mybir as mybir
from concourse.bass_utils import run_bass_kernel_spmd
from contextlib import ExitStack

F32, BF16 = mybir.dt.float32, mybir.dt.bfloat16
I32 = mybir.dt.int32
AF = mybir.ActivationFunctionType
ALU = mybir.AluOpType
AX = mybir.AxisListType
D = 1024
EPS = 1e-6
NE = 32
QSCALE = 96.0 ** -0.5

FULL_CFG = dict(S=4096, NSEQ=3, NQG=[8, 8, 4])


class Dep:
    def __init__(self, nc, es):
        self.nc = nc
        self.es = es
        self.eng = {'pe': nc.tensor, 'act': nc.scalar, 'dve': nc.vector, 'pool': nc.gpsimd, 'sp': nc.sync}
        self.sem = {e: es.enter_context(nc.semaphore('s_' + e)) for e in self.eng}
        self.cnt = {e: 0 for e in self.eng}
        self.waited = {e: {} for e in self.eng}
        self.dsem = {}
        self.entries = {}
        self.free = []

    def semof(self, k):
        return self.sem[k] if k in self.sem else self.entries[k][0]

    def wait(self, e, deps):
        for d in _flat(deps):
            k, v = d
            if self.waited[e].get(k, 0) < v:
                self.eng[e].wait_ge(self.semof(k), v)
                self.waited[e][k] = v

    def op(self, e, deps, fn, sig=True):
        self.wait(e, deps)
        ins = fn()
        if sig:
            ins.then_inc(self.sem[e], 1)
            self.cnt[e] += 1
            return (e, self.cnt[e])
        return None

    def dma(self, q, name, deps, fn):
        if name not in self.dsem:
            if self.free:
                key = self.free.pop()
            else:
                key = 'D%d' % len(self.entries)
                self.entries[key] = [self.es.enter_context(self.nc.semaphore('d_' + key)), 0]
            self.dsem[name] = key
        key = self.dsem[name]
        self.wait(q, deps)
        ins = fn()
        ent = self.entries[key]
        ins.then_inc(ent[0], 16)
        ent[1] += 16
        return (key, ent[1])

    def mark(self):
        return set(self.dsem.keys())

    def retire_since(self, mark, keep=()):
        for n in list(self.dsem.keys()):
            if n in mark or n in keep:
                continue
            key = self.dsem[n]
            self.wait('sp', (key, self.entries[key][1]))
            del self.dsem[n]
            self.free.append(key)

    def last(self):
        return [(e, self.cnt[e]) for e in self.eng if self.cnt[e] > 0]


def _flat(deps):
    out = []
    if deps is None:
        return out
    if isinstance(deps, tuple) and len(deps) == 2 and isinstance(deps[0], str):
        return [deps]
    for d in deps:
        out.extend(_flat(d))
    return out


def interleave(gens, depth):
    active = []
    it = iter(gens)
    done = False
    while True:
        if len(active) < depth and not done:
            try:
                active.append(next(it))
            except StopIteration:
                done = True
        if not active:
            break
        nxt = []
        for g in active:
            try:
                next(g)
                nxt.append(g)
            except StopIteration:
                pass
        active = nxt


def build(cfg):
    S = cfg['S']
    NSEQ = cfg['NSEQ']
    NQG = cfg['NQG']
    NG = S // 512
    KB = S // 128
    NT = NSEQ * S
    NQT = sum(NQG) * 512
    NGB = sum(NQG)
    NSL = (2 * NQT + 32 * 127 + 127) // 128
    NSL = ((NSL + 7) // 8) * 8

    nc = bass.Bass("TRN2", target_bir_lowering=False)

    def din(name, shape, dt=F32):
        return nc.dram_tensor(name, list(shape), dt, kind="ExternalInput").ap()

    def dscr(name, shape, dt):
        return nc.dram_tensor(name, list(shape), dt, kind="Internal").ap()

    xs = din("xs", [NT, D])
    cvec = din("cvec", [NSEQ, D])
    rope_c = din("rope_c", [NSEQ, 32, S])
    rope_s = din("rope_s", [NSEQ, 32, S])
    w_ada = din("w_ada", [D, 6 * D])
    b_ada = din("b_ada", [6 * D])
    g_pre1 = din("g_pre1", [D]); g_post1 = din("g_post1", [D])
    g_pre2 = din("g_pre2", [D]); g_post2 = din("g_post2", [D])
    w_in = din("w_in", [D, 1440])
    g_q = din("g_q", [256]); w_uq = din("w_uq", [256, 768])
    g_kv = din("g_kv", [128]); w_ukv = din("w_ukv", [128, 1024])
    g_v_gmlp = din("g_v_gmlp", [512])
    w_spatial = din("w_spatial", [8, 128, 128]); b_spatial = din("b_spatial", [8, 128])
    g_attn_out = din("g_attn_out", [512]); g_gmlp_out = din("g_gmlp_out", [512])
    w_out = din("w_out", [D, D])
    w_rg = din("w_router_group", [D, 4]); b_rg = din("b_router_group", [4])
    w_re = din("w_router_expert", [D, 32]); b_re = din("b_router_expert", [32])
    w_gate = din("w_gate", [NE, D, 256]); w_up = din("w_up", [NE, D, 256]); w_down = din("w_down", [NE, 256, D])
    ident_in = din("ident", [128, 128])
    egrp_in = din("egrp", [8, 512])
    utri_in = din("utri", [128, 128])
    tri32_in = din("tri32", [32, 32])
    jv_in = din("jv", [128, NSL])
    pidx_in = din("pidx", [128, 1])
    zeros_in = din("zeros", [128, 8192])
    y = nc.dram_tensor("y", [NQT, D], F32, kind="ExternalOutput").ap()

    mod_d = dscr("mod_d", [NSEQ, 6 * D], F32)
    sn_d = dscr("sn_d", [NT, 512], BF16)
    cq_d = dscr("cq_d", [NSEQ * NG, 128, 1024], BF16)
    x1_d = (nc.dram_tensor("x1_d", [NQT, D], F32, kind="ExternalOutput").ap() if cfg.get("dbg") else dscr("x1_d", [NQT, D], F32))
    wgu_r = dscr("wgu_r", [NE * 128, 8 * 512], BF16)
    wd_r = dscr("wd_r", [NE * 128, 2 * D], BF16)
    h2_d = dscr("h2_d", [NQT, D], BF16)
    xs_d = dscr("xs_d", [NSL * 128, D], BF16)
    ys_d = dscr("ys_d", [NSL * 128, D], F32)
    wkv_d = dscr("wkv_d", [128, 1024], BF16)
    wsp_d = dscr("wsp_d", [128, 1024], BF16)
    wq_d = dscr("wq_d", [128, 1536], BF16)
    wqsw_d = dscr("wqsw_d", [128, 1536], BF16)
    wo_d = dscr("wo_d", [128, 8192], BF16)

    _uid = [0]

    def U(name):
        _uid[0] += 1
        return "%s_u%d" % (name, _uid[0])

    top = ExitStack()
    with top:
        dp = Dep(nc, top)

        def PE(deps, fn, sig=True): return dp.op('pe', deps, fn, sig)
        def ACT(deps, fn, sig=True): return dp.op('act', deps, fn, sig)
        def DVE(deps, fn, sig=True): return dp.op('dve', deps, fn, sig)
        def POOL(deps, fn, sig=True): return dp.op('pool', deps, fn, sig)
        def DMA(name, deps, fn, q='sp'): return dp.dma(q, name, deps, fn)

        def mmg(out, pairs, deps, sig=True):
            n = len(pairs)
            tok = None
            for i, (l, r) in enumerate(pairs):
                tok = PE(deps if i == 0 else None,
                         lambda l=l, r=r, i=i: nc.tensor.matmul(out, lhsT=l, rhs=r, start=(i == 0), stop=(i == n - 1)),
                         sig=(sig and i == n - 1))
            return tok

        def rstd_chain(ss_ap, out_ap, inv_n, deps):
            t = ACT(deps, lambda: nc.scalar.activation(out=out_ap, in_=ss_ap, func=AF.Sqrt, bias=EPS, scale=inv_n))
            return DVE(t, lambda: nc.vector.reciprocal(out=out_ap, in_=out_ap))

        wcast = []
        for e in range(NE):
            wcast.append(DMA('wcast', None, lambda e=e: nc.gpsimd.dma_start(
                out=wgu_r[e * 128:(e + 1) * 128, :].rearrange("p (k c) -> p k c", k=8)[:, :, 0:256],
                in_=w_gate[e].rearrange("(k p) c -> p k c", p=128)), q='pool'))
            wcast.append(DMA('wcast', None, lambda e=e: nc.gpsimd.dma_start(
                out=wgu_r[e * 128:(e + 1) * 128, :].rearrange("p (k c) -> p k c", k=8)[:, :, 256:512],
                in_=w_up[e].rearrange("(k p) c -> p k c", p=128)), q='pool'))
            wcast.append(DMA('wcast', None, lambda e=e: nc.gpsimd.dma_start(
                out=wd_r[e * 128:(e + 1) * 128, :].rearrange("p (j c) -> p j c", j=2),
                in_=w_down[e].rearrange("(j p) c -> p j c", p=128)), q='pool'))
        wcast_tok = wcast[-1]
        zero_tok = []
        nz = (NSL * 128 * D) // (128 * 8192)
        xs_flat = xs_d.rearrange("(n p r) c -> n p (r c)", p=128, r=8)
        for zi in range(nz):
            zero_tok.append(DMA('zero', None, lambda zi=zi: nc.gpsimd.dma_start(out=xs_flat[zi], in_=zeros_in), q='pool'))

        ident_f = top.enter_context(nc.sbuf_tensor(U("ident_f"), [128, 128], F32))
        ident_b = top.enter_context(nc.sbuf_tensor(U("ident_b"), [128, 128], BF16))
        ones_f = top.enter_context(nc.sbuf_tensor(U("ones_f"), [128, 64], F32))
        t_id = DMA('c0', None, lambda: nc.sync.dma_start(out=ident_f[:], in_=ident_in))
        t_idb = DVE(t_id, lambda: nc.vector.tensor_copy(out=ident_b[:], in_=ident_f[:]))
        t_ones = DVE(None, lambda: nc.vector.memset(ones_f[:], 1.0))

        mk0 = dp.mark()
        with ExitStack() as pes:
            def sb(name, shape, dt): return pes.enter_context(nc.sbuf_tensor(U(name), shape, dt))
            def ps(name, shape, dt): return pes.enter_context(nc.psum_tensor(U(name), shape, dt))
            csT = sb("csT", [128, 8, NSEQ], F32)
            csS = sb("csS", [128, 8, NSEQ], F32)
            wblk = [sb("wblk%d" % i, [128, 8, 512], F32) for i in range(2)]
            brep = sb("brep", [NSEQ, 6 * D], F32)
            modsb = sb("modsb", [NSEQ, 6 * D], F32)
            pmod = [ps("pmod%d" % i, [128, 512], F32) for i in range(2)]
            t_c = [DMA('p0', None, lambda q=q: nc.sync.dma_start(out=csT[:, :, q], in_=cvec[q].rearrange("(k p) -> p k", p=128),
                                                                 allow_slow_non_contiguous=True)) for q in range(NSEQ)]
            t_b = DMA('p1', None, lambda: nc.sync.dma_start(out=brep[:], in_=b_ada.partition_broadcast(NSEQ)))
            t_cs = ACT(t_c, lambda: nc.scalar.activation(out=csS[:], in_=csT[:], func=AF.Silu))
            wfree = [None, None]
            pfree = [None, None]
            ev = None
            for blk in range(12):
                i = blk % 2
                t_w = DMA('pw%d' % i, wfree[i], lambda blk=blk, i=i: nc.sync.dma_start(
                    out=wblk[i][:], in_=w_ada[:, blk * 512:(blk + 1) * 512].rearrange("(k p) c -> p k c", p=128)))
                t_m = mmg(pmod[i][0:NSEQ, :], [(csS[:, k, :], wblk[i][:, k, :]) for k in range(8)], [t_w, t_cs, pfree[i]])
                wfree[i] = t_m
                ev = DVE([t_m, t_b], lambda blk=blk, i=i: nc.vector.tensor_tensor(
                    out=modsb[:, blk * 512:(blk + 1) * 512], in0=pmod[i][0:NSEQ, :],
                    in1=brep[:, blk * 512:(blk + 1) * 512], op=ALU.add))
                pfree[i] = ev
            t_mod = DMA('p2', ev, lambda: nc.sync.dma_start(out=mod_d, in_=modsb[:]))

            tmpq = sb("tmpq", [128, 2, 768], F32)
            gq = sb("gq", [128, 2], F32)
            wq_t = sb("wq_t", [128, 2, 768], BF16)
            wqsw_t = sb("wqsw_t", [128, 2, 768], BF16)
            t1 = DMA('p3', None, lambda: nc.sync.dma_start(out=tmpq[:], in_=w_uq.rearrange("(k p) c -> p k c", p=128)))
            t2 = DMA('p3', None, lambda: nc.sync.dma_start(out=gq[:], in_=g_q.rearrange("(k p) -> p k", p=128),
                                                          allow_slow_non_contiguous=True))
            tq = None
            for k in range(2):
                tq = DVE([t1, t2], lambda k=k: nc.vector.tensor_scalar(
                    out=wq_t[:, k, :], in0=tmpq[:, k, :], scalar1=gq[:, k:k + 1], scalar2=QSCALE,
                    op0=ALU.mult, op1=ALU.mult))
            tz = POOL(None, lambda: nc.gpsimd.memset(wqsw_t[:], 0.0))
            wq4 = wq_t[:].rearrange("p k (h c) -> p k h c", h=8)
            wqs4 = wqsw_t[:].rearrange("p k (h c) -> p k h c", h=8)
            ta = DVE([tq, tz], lambda: nc.vector.tensor_scalar(out=wqs4[:, :, :, 64:80], in0=wq4[:, :, :, 80:96],
                                                              scalar1=-1.0, scalar2=None, op0=ALU.mult))
            tb = DVE(None, lambda: nc.vector.tensor_copy(out=wqs4[:, :, :, 80:96], in_=wq4[:, :, :, 64:80]))
            t_wq = DMA('p4', tq, lambda: nc.sync.dma_start(out=wq_d, in_=wq_t[:].rearrange("p k c -> p (k c)")))
            t_wqsw = DMA('p4', [ta, tb], lambda: nc.sync.dma_start(out=wqsw_d, in_=wqsw_t[:].rearrange("p k c -> p (k c)")))

            tmpkv = sb("tmpkv", [128, 1024], F32)
            gkv = sb("gkv", [128, 1], F32)
            wkv_t = sb("wkv_t", [128, 1024], BF16)
            t1 = DMA('p5', None, lambda: nc.sync.dma_start(out=tmpkv[:], in_=w_ukv))
            t2 = DMA('p5', None, lambda: nc.sync.dma_start(out=gkv[:], in_=g_kv.rearrange("(p o) -> p o", o=1)))
            tk = DVE([t1, t2], lambda: nc.vector.tensor_scalar(out=wkv_t[:], in0=tmpkv[:], scalar1=gkv[:, 0:1],
                                                              scalar2=None, op0=ALU.mult))
            t_wkv = DMA('p6', tk, lambda: nc.sync.dma_start(out=wkv_d, in_=wkv_t[:]))

            tmpo = sb("tmpo", [128, 8, 1024], F32)
            gcat = sb("gcat", [128, 8], F32)
            wo_t = sb("wo_t", [128, 8, 1024], BF16)
            t1 = DMA('p7', None, lambda: nc.sync.dma_start(out=tmpo[:], in_=w_out.rearrange("(k p) c -> p k c", p=128)))
            t2 = DMA('p7', None, lambda: nc.sync.dma_start(out=gcat[:, 0:4], in_=g_attn_out.rearrange("(k p) -> p k", p=128),
                                                          allow_slow_non_contiguous=True))
            t3 = DMA('p7', None, lambda: nc.sync.dma_start(out=gcat[:, 4:8], in_=g_gmlp_out.rearrange("(k p) -> p k", p=128),
                                                          allow_slow_non_contiguous=True))
            two = None
            for k in range(8):
                two = DVE([t1, t2, t3], lambda k=k: nc.vector.tensor_scalar(
                    out=wo_t[:, k, :], in0=tmpo[:, k, :], scalar1=gcat[:, k:k + 1], scalar2=None, op0=ALU.mult))
            t_wo = DMA('p8', two, lambda: nc.sync.dma_start(out=wo_d, in_=wo_t[:].rearrange("p k c -> p (k c)")))

            tmps = sb("tmps", [128, 8, 128], F32)
            wsp_t = sb("wsp_t", [128, 8, 128], BF16)
            psp = ps("psp", [128, 1024], F32)
            t1 = DMA('p9', None, lambda: nc.sync.dma_start(out=tmps[:], in_=w_spatial.rearrange("g t s -> t g s")))
            tt = None
            for g in range(8):
                tt = PE([t1, t_id], lambda g=g: nc.tensor.transpose(psp[:, g * 128:(g + 1) * 128], tmps[:, g, :], ident_f[:]),
                        sig=(g == 7))
            tc_ = DVE(tt, lambda: nc.vector.tensor_copy(out=wsp_t[:].rearrange("p g t -> p (g t)"), in_=psp[:]))
            t_wsp = DMA('p10', tc_, lambda: nc.sync.dma_start(out=wsp_d, in_=wsp_t[:].rearrange("p g t -> p (g t)")))
            prep_done = [t_mod, t_wq, t_wqsw, t_wkv, t_wo, t_wsp]
            prep_bar = dp.last()
            dp.retire_since(mk0, keep=('wcast', 'zero', 'c0'))

        with ExitStack() as aes:
            def sbA(name, shape, dt): return aes.enter_context(nc.sbuf_tensor(U(name), shape, dt))
            KT = sbA("KT", [128, 8, S], BF16)
            VA = sbA("VA", [128, KB, 8, 65], BF16)
            geff1 = sbA("geff1", [128, D], F32)
            sh1r = sbA("sh1r", [128, D], F32)
            gvec1 = sbA("gvec1", [128, D], F32)
            gvrep = sbA("gvrep", [128, 512], F32)
            PA = aes.enter_context(nc.psum_tensor(U("PA"), [128, 1024], F32))
            PB = aes.enter_context(nc.psum_tensor(U("PB"), [128, 1024], F32))
            PC = aes.enter_context(nc.psum_tensor(U("PC"), [128, 1024], F32))
            PD = aes.enter_context(nc.psum_tensor(U("PD"), [128, 1024], F32))

            t_va1 = POOL(prep_bar, lambda: nc.gpsimd.memset(VA[:, :, :, 64:65], 1.0))
            t_gv = DMA('a0', prep_bar, lambda: nc.sync.dma_start(out=gvrep[:], in_=g_v_gmlp.partition_broadcast(128)))
            seq_bar = [prep_bar, prep_done, t_va1, t_gv, t_idb, t_ones]
            qbase = 0
            for s in range(NSEQ):
                mk1 = dp.mark()
                with ExitStack() as p1:
                    def sb1(name, shape, dt): return p1.enter_context(nc.sbuf_tensor(U(name), shape, dt))
                    wAs = sb1("wAs", [128, 8, 384], BF16)
                    wAuv = sb1("wAuv", [128, 8, 1024], BF16)
                    wAkr = sb1("wAkr", [128, 8, 96], BF16)
                    wAks = sb1("wAks", [128, 8, 96], BF16)
                    wkv = sb1("wkv", [128, 1024], BF16)
                    wsp = sb1("wsp", [128, 8, 128], BF16)
                    bsp = sb1("bsp", [8, 128], F32)
                    egrp = sb1("egrp", [8, 512], F32)
                    vt = [sb1("vt%d" % i, [128, D], F32) for i in range(2)]
                    xt = [sb1("xt%d" % i, [128, D], F32) for i in range(2)]
                    junk = sb1("junk", [128, D], BF16)
                    hm = sb1("hm", [128, D], F32)
                    hb = [sb1("hb%d" % i, [128, D], BF16) for i in range(2)]
                    hT = sb1("hT", [128, 8, 512], BF16)
                    zsb = [sb1("zsb%d" % i, [128, 384], BF16) for i in range(2)]
                    cqnT = [sb1("cqnT%d" % i, [128, 2, 512], BF16) for i in range(2)]
                    ckvnT = [sb1("ckvnT%d" % i, [128, 512], BF16) for i in range(2)]
                    gu = [sb1("gu%d" % i, [128, 512], BF16) for i in range(2)]
                    gv = [sb1("gv%d" % i, [128, 512], F32) for i in range(2)]
                    zraw = [sb1("zraw%d" % i, [128, 384], F32) for i in range(2)]
                    vn = [sb1("vn%d" % i, [128, 512], BF16) for i in range(2)]
                    sraw = [sb1("sraw%d" % i, [128, 512], F32) for i in range(2)]
                    sn = [sb1("sn%d" % i, [128, 512], BF16) for i in range(2)]
                    stt = [sb1("stt%d" % i, [128, 16], F32) for i in range(2)]
                    Ctt = [sb1("Ctt%d" % i, [128, 128], F32) for i in range(2)]
                    Stt = [sb1("Stt%d" % i, [128, 128], F32) for i in range(2)]
                    kt1 = [sb1("kt1_%d" % i, [128, 128], F32) for i in range(2)]
                    kt2 = [sb1("kt2_%d" % i, [128, 128], F32) for i in range(2)]
                    krr = [sb1("krr%d" % i, [128, 128], BF16) for i in range(2)]

                    pT = PA[:, 0:512].bitcast(BF16)
                    pT2 = PA[:, 512:1024].bitcast(BF16)
                    pzs = PB[:, 0:384]
                    pss = PB[:, 512:1024]
                    pu = PC[:, 0:512]
                    pv = PC[:, 512:1024]
                    pkr = PD[:, 0:512]
                    pks = PD[:, 512:1024]

                    sb_ = seq_bar
                    wl = []
                    wl.append(DMA('a1', sb_, lambda: nc.gpsimd.dma_start(
                        out=wAs[:], in_=w_in[:, 0:384].rearrange("(k p) c -> p k c", p=128)), q='pool'))
                    wl.append(DMA('a1', sb_, lambda: nc.gpsimd.dma_start(
                        out=wAuv[:], in_=w_in[:, 416:1440].rearrange("(k p) c -> p k c", p=128)), q='pool'))
                    tz1 = POOL(sb_, lambda: nc.gpsimd.memset(wAkr[:], 0.0))
                    tz2 = POOL(sb_, lambda: nc.gpsimd.memset(wAks[:], 0.0))
                    wl.append(DMA('a1', [tz1], lambda: nc.gpsimd.dma_start(
                        out=wAkr[:, :, 64:96], in_=w_in[:, 384:416].rearrange("(k p) c -> p k c", p=128)), q='pool'))
                    tn = DMA('a2', [tz2], lambda: nc.gpsimd.dma_start(
                        out=wAks[:, :, 64:80], in_=w_in[:, 400:416].rearrange("(k p) c -> p k c", p=128)), q='pool')
                    wl.append(DMA('a1', [tz2], lambda: nc.gpsimd.dma_start(
                        out=wAks[:, :, 80:96], in_=w_in[:, 384:400].rearrange("(k p) c -> p k c", p=128)), q='pool'))
                    wl.append(POOL(tn, lambda: nc.gpsimd.tensor_scalar(out=wAks[:, :, 64:80], in0=wAks[:, :, 64:80],
                                                                      scalar1=-1.0, scalar2=None, op0=ALU.mult)))
                    wl.append(DMA('a3', sb_, lambda: nc.sync.dma_start(out=wkv[:], in_=wkv_d)))
                    wl.append(DMA('a3', sb_, lambda: nc.sync.dma_start(out=wsp[:].rearrange("p g t -> p (g t)"), in_=wsp_d)))
                    wl.append(DMA('a3', sb_, lambda: nc.sync.dma_start(out=bsp[:], in_=b_spatial)))
                    wl.append(DMA('a3', sb_, lambda: nc.sync.dma_start(out=egrp[:], in_=egrp_in)))
                    l1 = DMA('a4_0', sb_, lambda: nc.sync.dma_start(out=vt[0][:], in_=mod_d[s, 1024:2048].partition_broadcast(128)))
                    l2 = DMA('a4_1', sb_, lambda: nc.sync.dma_start(out=vt[1][:], in_=g_pre1.partition_broadcast(128)))
                    l3 = DMA('a4_2', sb_, lambda: nc.sync.dma_start(out=sh1r[:], in_=mod_d[s, 0:1024].partition_broadcast(128)))
                    tg = DVE([l1, l2], lambda: nc.vector.scalar_tensor_tensor(out=geff1[:], in0=vt[0][:], scalar=1.0, in1=vt[1][:],
                                                                               op0=ALU.add, op1=ALU.mult))
                    l4 = DMA('a4_3', [tg], lambda: nc.sync.dma_start(out=vt[0][:], in_=mod_d[s, 2048:3072].partition_broadcast(128)))
                    l5 = DMA('a4_4', [tg], lambda: nc.sync.dma_start(out=vt[1][:], in_=g_post1.partition_broadcast(128)))
                    tg2 = DVE([l4, l5], lambda: nc.vector.tensor_tensor(out=gvec1[:], in0=vt[0][:], in1=vt[1][:], op=ALU.mult))
                    ready = [wl, l3, tg, tg2]

                    xt_free = [None, None]; hb_free = [None, None]
                    hT_free = [None] * 4
                    zraw_free = [None, None]; zsb_free = [None, None]; cq_free = [None, None]; ckv_free = [None, None]
                    gu_free = [None, None]; gv_free = [None, None]; vn_free = [None, None]; sn_free = [None, None]
                    sraw_free = [None, None]; ct_free = [None, None]; kt_free = [None, None]; krr_free = [None, None]
                    P = dict(hm_free=None, pT_free=None, pT2_free=None, pzs_free=None, pu_free=None, pv_free=None, pss_free=None,
                             pkr_free=None, pks_free=None)
                    grp = {}

                    def p1_tile(g, t):
                        gi = g % 2
                        ti = t % 2
                        if t == 0:
                            grp[g] = dict(cq_w=[], ckv_w=[])
                        G = grp[g]
                        tok0 = s * S + g * 512 + t * 128
                        ts_ = slice(t * 128, (t + 1) * 128)
                        gts = slice(g * 512 + t * 128, g * 512 + (t + 1) * 128)
                        st_ = stt[ti]
                        lx = DMA('x%d' % ti, [xt_free[ti], ready], lambda: nc.sync.dma_start(out=xt[ti][:], in_=xs[tok0:tok0 + 128, :]))
                        lc = DMA('rc%d' % ti, [ct_free[ti], ready], lambda: nc.sync.dma_start(out=Ctt[ti][64:96, :], in_=rope_c[s, :, gts]))
                        ls = DMA('rs%d' % ti, [ct_free[ti], ready], lambda: nc.sync.dma_start(out=Stt[ti][64:96, :], in_=rope_s[s, :, gts]))
                        a1 = ACT(lx, lambda: nc.scalar.activation(out=junk[:], in_=xt[ti][:], func=AF.Square, accum_out=st_[:, 0:1]))
                        yield
                        r1a = ACT(a1, lambda: nc.scalar.activation(out=st_[:, 1:2], in_=st_[:, 0:1], func=AF.Sqrt, bias=EPS, scale=1.0 / D))
                        yield
                        r1 = DVE(r1a, lambda: nc.vector.reciprocal(out=st_[:, 1:2], in_=st_[:, 1:2]))
                        d1 = DVE([r1, P['hm_free']], lambda: nc.vector.scalar_tensor_tensor(
                            out=hm[:], in0=xt[ti][:], scalar=st_[:, 1:2], in1=geff1[:], op0=ALU.mult, op1=ALU.mult))
                        xt_free[ti] = d1
                        p1_ = POOL([d1, hb_free[ti]], lambda: nc.gpsimd.tensor_tensor(out=hb[ti][:], in0=hm[:], in1=sh1r[:], op=ALU.add))
                        P['hm_free'] = p1_
                        yield
                        tp = None
                        for k in range(8):
                            tp = PE([p1_, P['pT_free']] if k == 0 else None,
                                    lambda k=k: nc.tensor.transpose(pT[:, k * 128:(k + 1) * 128], hb[ti][:, k * 128:(k + 1) * 128], ident_b[:]),
                                    sig=(k == 7))
                        hb_free[ti] = tp
                        yield
                        ev = ACT([tp, hT_free[t]], lambda: nc.scalar.copy(out=hT[:, :, ts_], in_=pT.rearrange("p (k c) -> p k c", k=8)))
                        P['pT_free'] = ev
                        yield
                        m_zs = mmg(pzs, [(hT[:, k, ts_], wAs[:, k, :]) for k in range(8)], [ev, P['pzs_free']])
                        m_u = mmg(pu, [(hT[:, k, ts_], wAuv[:, k, 0:512]) for k in range(8)], [P['pu_free']])
                        m_v = mmg(pv, [(hT[:, k, ts_], wAuv[:, k, 512:1024]) for k in range(8)], [P['pv_free']])
                        m_kr = mmg(pkr[0:96, 0:128], [(wAkr[:, k, :], hT[:, k, ts_]) for k in range(8)], [P['pkr_free']])
                        m_ks = mmg(pks[0:96, 0:128], [(wAks[:, k, :], hT[:, k, ts_]) for k in range(8)], [P['pks_free']])
                        hT_free[t] = m_ks
                        yield
                        zr = DVE([m_zs, zraw_free[ti]], lambda: nc.vector.tensor_copy(out=zraw[ti][:], in_=pzs))
                        P['pzs_free'] = zr
                        g1 = ACT([m_u, gu_free[ti]], lambda: nc.scalar.activation(out=gu[ti][:], in_=pu, func=AF.Gelu_apprx_tanh))
                        P['pu_free'] = g1
                        g2 = ACT([m_v, gv_free[ti]], lambda: nc.scalar.activation(out=gv[ti][:], in_=pv, func=AF.Gelu_apprx_tanh))
                        P['pv_free'] = g2
                        k1 = DVE([m_kr, lc, kt_free[ti]], lambda: nc.vector.tensor_tensor(out=kt1[ti][64:96, :], in0=pkr[64:96, 0:128], in1=Ctt[ti][64:96, :], op=ALU.mult))
                        P['pkr_free'] = k1
                        k2 = DVE([m_ks, ls], lambda: nc.vector.tensor_tensor(out=kt2[ti][64:96, :], in0=pks[64:96, 0:128], in1=Stt[ti][64:96, :], op=ALU.mult))
                        P['pks_free'] = k2
                        ct_free[ti] = k2
                        yield
                        a2 = ACT(zr, lambda: nc.scalar.activation(out=junk[:, 0:256], in_=zraw[ti][:, 0:256], func=AF.Square, accum_out=st_[:, 2:3]))
                        a3 = ACT(None, lambda: nc.scalar.activation(out=junk[:, 256:384], in_=zraw[ti][:, 256:384], func=AF.Square, accum_out=st_[:, 3:4]))
                        g3 = ACT(g2, lambda: nc.scalar.activation(out=junk[:, 0:512], in_=gv[ti][:], func=AF.Square, accum_out=st_[:, 6:7]))
                        k3 = DVE([k1, k2, krr_free[ti]], lambda: nc.vector.tensor_tensor(out=krr[ti][64:96, :], in0=kt1[ti][64:96, :], in1=kt2[ti][64:96, :], op=ALU.add))
                        kt_free[ti] = k3
                        kc = None
                        for h in range(8):
                            kc = POOL(k3, lambda h=h: nc.gpsimd.tensor_copy(out=KT[64:96, h, gts], in_=krr[ti][64:96, :]))
                        krr_free[ti] = kc
                        yield
                        q1 = ACT([a2, a3], lambda: nc.scalar.activation(out=st_[:, 4:5], in_=st_[:, 2:3], func=AF.Sqrt, bias=EPS, scale=1.0 / 256))
                        q2 = ACT(None, lambda: nc.scalar.activation(out=st_[:, 5:6], in_=st_[:, 3:4], func=AF.Sqrt, bias=EPS, scale=1.0 / 128))
                        q3 = ACT(g3, lambda: nc.scalar.activation(out=st_[:, 7:8], in_=st_[:, 6:7], func=AF.Sqrt, bias=EPS, scale=1.0 / 512))
                        yield
                        r2 = DVE([q1, q2], lambda: nc.vector.reciprocal(out=st_[:, 4:6], in_=st_[:, 4:6]))
                        r4 = DVE(q3, lambda: nc.vector.reciprocal(out=st_[:, 7:8], in_=st_[:, 7:8]))
                        d2 = DVE([r4, vn_free[ti]], lambda: nc.vector.scalar_tensor_tensor(
                            out=vn[ti][:], in0=gv[ti][:], scalar=st_[:, 7:8], in1=gvrep[:], op0=ALU.mult, op1=ALU.mult))
                        gv_free[ti] = d2
                        c1 = ACT([r2, zsb_free[ti]], lambda: nc.scalar.activation(
                            out=zsb[ti][:, 0:256], in_=zraw[ti][:, 0:256], func=AF.Copy, scale=st_[:, 4:5]))
                        c2 = ACT(None, lambda: nc.scalar.activation(
                            out=zsb[ti][:, 256:384], in_=zraw[ti][:, 256:384], func=AF.Copy, scale=st_[:, 5:6]))
                        zraw_free[ti] = c2
                        yield
                        tp2 = None
                        for k in range(3):
                            tp2 = PE([c1, c2, P['pT2_free']] if k == 0 else None,
                                     lambda k=k: nc.tensor.transpose(pT2[:, k * 128:(k + 1) * 128], zsb[ti][:, k * 128:(k + 1) * 128], ident_b[:]),
                                     sig=(k == 2))
                        zsb_free[ti] = tp2
                        PE([P['pss_free'], ready], lambda: nc.tensor.matmul(pss, lhsT=bsp[:, :], rhs=egrp[:, :], start=True, stop=False), sig=False)
                        m_s = None
                        for gg in range(8):
                            m_s = PE(d2 if gg == 0 else None,
                                     lambda gg=gg: nc.tensor.matmul(pss[:, gg * 64:(gg + 1) * 64], lhsT=wsp[:, gg, :],
                                                                    rhs=vn[ti][:, gg * 64:(gg + 1) * 64], start=False, stop=(gg == 7)),
                                     sig=(gg == 7))
                        vn_free[ti] = m_s
                        yield
                        e1 = DVE([tp2, cq_free[gi] if t == 0 else None], lambda: nc.vector.tensor_copy(
                            out=cqnT[gi][:, :, ts_], in_=pT2[:, 0:256].rearrange("p (k c) -> p k c", k=2)))
                        e2 = DVE([ckv_free[gi] if t == 0 else None], lambda: nc.vector.tensor_copy(
                            out=ckvnT[gi][:, ts_], in_=pT2[:, 256:384]))
                        P['pT2_free'] = e2
                        G['cq_w'].append(e1)
                        G['ckv_w'].append(e2)
                        d3 = DVE([m_s, g1, sraw_free[ti]], lambda: nc.vector.tensor_tensor(out=sraw[ti][:], in0=gu[ti][:], in1=pss, op=ALU.mult))
                        P['pss_free'] = d3
                        gu_free[ti] = d3
                        yield
                        a4 = ACT(d3, lambda: nc.scalar.activation(out=junk[:, 0:512], in_=sraw[ti][:], func=AF.Square, accum_out=st_[:, 8:9]))
                        yield
                        q4 = ACT(a4, lambda: nc.scalar.activation(out=st_[:, 9:10], in_=st_[:, 8:9], func=AF.Sqrt, bias=EPS, scale=1.0 / 512))
                        yield
                        r5 = DVE(q4, lambda: nc.vector.reciprocal(out=st_[:, 9:10], in_=st_[:, 9:10]))
                        yield
                        c3 = ACT([r5, sn_free[ti]], lambda: nc.scalar.activation(out=sn[ti][:], in_=sraw[ti][:], func=AF.Copy, scale=st_[:, 9:10]))
                        sraw_free[ti] = c3
                        sn_free[ti] = DMA('sn%d' % ti, c3, lambda: nc.sync.dma_start(out=sn_d[tok0:tok0 + 128, :], in_=sn[ti][:]))
                        if t != 3:
                            return
                        yield
                        gs = slice(g * 512, (g + 1) * 512)
                        cq_free[gi] = DMA('cq%d' % gi, G['cq_w'], lambda: nc.sync.dma_start(
                            out=cq_d[s * NG + g], in_=cqnT[gi][:].rearrange("p k c -> p (k c)")))
                        bank_free = [P['pkr_free'], P['pks_free']]
                        banks = [pkr, pks]
                        for h in range(8):
                            bi = h % 2
                            mk = mmg(banks[bi][0:64, :], [(wkv[:, h * 128:h * 128 + 64], ckvnT[gi][:, :])], [G['ckv_w'], bank_free[bi]])
                            if h % 2 == 0:
                                bank_free[bi] = ACT(mk, lambda h=h, bi=bi: nc.scalar.copy(out=KT[0:64, h, gs], in_=banks[bi][0:64, :]))
                            else:
                                bank_free[bi] = DVE(mk, lambda h=h, bi=bi: nc.vector.tensor_copy(out=KT[0:64, h, gs], in_=banks[bi][0:64, :]))
                        wkv3 = wkv[:].rearrange("p (h c) -> p h c", h=8)[:, :, 64:128]
                        mv = None
                        for tt in range(4):
                            bi = tt % 2
                            kb = g * 4 + tt
                            mv = mmg(banks[bi][:, :].rearrange("p (h c) -> p h c", h=8), [(ckvnT[gi][:, tt * 128:(tt + 1) * 128], wkv3)], [bank_free[bi]])
                            bank_free[bi] = DVE(mv, lambda kb=kb, bi=bi: nc.vector.tensor_copy(
                                out=VA[:, kb, :, 0:64], in_=banks[bi][:, :].rearrange("p (h c) -> p h c", h=8)))
                        ckv_free[gi] = mv
                        P['pkr_free'] = bank_free[0]
                        P['pks_free'] = bank_free[1]

                    interleave((p1_tile(g, t) for g in range(NG) for t in range(4)), 2)
                    p1_bar = dp.last() + [sn_free, cq_free]
                    dp.retire_since(mk1)

                mk2 = dp.mark()
                with ExitStack() as p2:
                    def sb2(name, shape, dt): return p2.enter_context(nc.sbuf_tensor(U(name), shape, dt))
                    wq = sb2("wq", [128, 2, 768], BF16)
                    wqs = sb2("wqs", [128, 2, 768], BF16)
                    wo = sb2("wo", [128, 8, 1024], BF16)
                    cqT = [sb2("cqT%d" % i, [128, 2, 512], BF16) for i in range(2)]
                    Ct = sb2("Ct2", [128, 512], F32)
                    St = sb2("St2", [128, 512], F32)
                    qt1 = [sb2("qt1_%d" % i, [128, 512], F32) for i in range(2)]
                    qt2 = [sb2("qt2_%d" % i, [128, 512], F32) for i in range(2)]
                    qt_free = [None, None]
                    QT = sb2("QT", [128, 8, 512], BF16)
                    pTs = [sb2("pTs%d" % i, [128, 1024], BF16) for i in range(3)]
                    osb = [sb2("osb%d" % i, [128, 512], F32) for i in range(2)]
                    rinv = [sb2("rinv%d" % i, [128, 512], F32) for i in range(2)]
                    aT = [sb2("aT%d" % i, [128, 512], BF16) for i in range(2)]
                    merged = [sb2("merged%d" % i, [128, D], BF16) for i in range(2)]
                    mT = [sb2("mT%d" % i, [128, 8, 128], BF16) for i in range(2)]
                    otmp = sb2("otmp", [128, D], F32)
                    xr = [sb2("xr%d" % i, [128, D], F32) for i in range(2)]
                    x1 = [sb2("x1_%d" % i, [128, D], F32) for i in range(2)]
                    junk = sb2("junk2", [128, D], BF16)
                    stt = [sb2("stq%d" % i, [128, 16], F32) for i in range(2)]

                    po = PC[:, 0:512]
                    prb = PC[:, 512:1024]
                    pmT = PC[:, 512:1024].bitcast(BF16)
                    pa = PD[:, :].bitcast(BF16)
                    scT = [PA, PB]

                    wl2 = [DMA('b1', p1_bar, lambda: nc.sync.dma_start(out=wq[:].rearrange("p k c -> p (k c)"), in_=wq_d)),
                           DMA('b1', p1_bar, lambda: nc.sync.dma_start(out=wqs[:].rearrange("p k c -> p (k c)"), in_=wqsw_d)),
                           DMA('b1', p1_bar, lambda: nc.sync.dma_start(out=wo[:].rearrange("p k c -> p (k c)"), in_=wo_d))]
                    ready2 = [p1_bar, wl2]
                    cq_free2 = [None, None]; rope_free = None; QT_free = []
                    sc_free = [None, None]; pTs_free = [None, None, None]; po_free = None; osb_free = [None, None]
                    rinv_free = [None, None]; prb_free = None; aT_free = [None, None]; pa_free = []
                    merged_free = [None, None]; mT_free = [None, None]; pmT_free = None
                    otmp_free = None; xr_free = [None, None]; x1_free = [None, None]
                    step = 0
                    tcount = 0
                    for qg in range(NQG[s]):
                        gi = qg % 2
                        gs = slice(qg * 512, (qg + 1) * 512)
                        lq = DMA('cql%d' % gi, [cq_free2[gi], ready2], lambda gi=gi, qg=qg: nc.sync.dma_start(
                            out=cqT[gi][:].rearrange("p k c -> p (k c)"), in_=cq_d[s * NG + qg]))
                        lr1 = DMA('rp2c', [rope_free, ready2], lambda gs=gs: nc.sync.dma_start(out=Ct[64:96, :], in_=rope_c[s, :, gs]))
                        lr2 = DMA('rp2s', [rope_free, ready2], lambda gs=gs: nc.sync.dma_start(out=St[64:96, :], in_=rope_s[s, :, gs]))
                        wq4 = wq[:].rearrange("p k (h c) -> p k h c", h=8)
                        wqs4 = wqs[:].rearrange("p k (h c) -> p k h c", h=8)
                        QT_w = []
                        Qs = dict(mqs=None, qd=None)

                        def q_head(h):
                            T = scT[h % 2]
                            hi = h % 2
                            mq = mmg(T[0:96, 0:512], [(wq4[:, k, h, :], cqT[gi][:, k, :]) for k in range(2)], [lq, sc_free[h % 2]])
                            mqs = mmg(T[0:96, 512:1024], [(wqs4[:, k, h, :], cqT[gi][:, k, :]) for k in range(2)], None)
                            Qs['mqs'] = mqs
                            yield
                            c0 = ACT([mq, QT_free if h == 0 else None], lambda: nc.scalar.copy(out=QT[0:64, h, :], in_=T[0:64, 0:512]))
                            q1 = DVE([mq, lr1, qt_free[hi]], lambda: nc.vector.tensor_tensor(out=qt1[hi][64:96, :], in0=T[64:96, 0:512], in1=Ct[64:96, :], op=ALU.mult))
                            q2 = DVE([mqs, lr2], lambda: nc.vector.tensor_tensor(out=qt2[hi][64:96, :], in0=T[64:96, 512:1024], in1=St[64:96, :], op=ALU.mult))
                            sc_free[h % 2] = [c0, q2]
                            yield
                            qd = DVE([q1, q2, QT_free if h == 0 else None], lambda: nc.vector.tensor_tensor(
                                out=QT[64:96, h, :], in0=qt1[hi][64:96, :], in1=qt2[hi][64:96, :], op=ALU.add))
                            qt_free[hi] = qd
                            Qs['qd'] = qd
                            QT_w.extend([c0, qd])
                        interleave((q_head(h) for h in range(8)), 2)
                        mqs = Qs['mqs']
                        qd = Qs['qd']
                        cq_free2[gi] = mqs
                        rope_free = qd
                        NP = KB // 2
                        steps = [(h, j) for h in range(8) for j in range(NP)]
                        qk_tok = {}

                        def emit_qk(idx):
                            h, j = steps[idx]
                            T = scT[idx % 2]
                            tk = None
                            for u in range(2):
                                kb = 2 * j + u
                                tk = PE([sc_free[idx % 2], QT_w] if u == 0 else None,
                                        lambda h=h, kb=kb, u=u, T=T: nc.tensor.matmul(
                                            T[:, u * 512:(u + 1) * 512], lhsT=KT[0:96, h, kb * 128:(kb + 1) * 128], rhs=QT[0:96, h, :],
                                            start=True, stop=True), sig=(u == 1))
                            qk_tok[idx] = tk

                        emit_qk(0)
                        pa_w = []
                        QT_readers = []
                        pending = []
                        A = dict(prb_free=prb_free)
                        for idx, (h, j) in enumerate(steps):
                            if idx + 1 < len(steps):
                                emit_qk(idx + 1)
                            T = scT[idx % 2]
                            sl = step % 3
                            step += 1
                            ex = ACT([qk_tok[idx], pTs_free[sl]], lambda T=T, sl=sl: nc.scalar.activation(out=pTs[sl][:], in_=T[:, :], func=AF.Exp))
                            sc_free[idx % 2] = ex
                            pvt = None
                            for u in range(2):
                                kb = 2 * j + u
                                pvt = PE([ex, po_free if (j == 0 and u == 0) else None],
                                         lambda h=h, kb=kb, u=u, sl=sl: nc.tensor.matmul(
                                             po[0:65, :], lhsT=VA[:, kb, h, :], rhs=pTs[sl][:, u * 512:(u + 1) * 512],
                                             start=(kb == 0), stop=(kb == KB - 1)), sig=(u == 1))
                            pTs_free[sl] = pvt
                            for pend in list(pending):
                                pend[0] -= 1
                                if pend[0] <= 0:
                                    pend[1]()
                                    pending.remove(pend)
                            if j == NP - 1:
                                for pend in list(pending):
                                    pend[1]()
                                    pending.remove(pend)
                                oi = h % 2
                                QT_readers.append(pvt)
                                o1 = DVE([pvt, osb_free[oi]], lambda oi=oi: nc.vector.tensor_copy(out=osb[oi][0:65, :], in_=po[0:65, :]))
                                po_free = o1
                                o2 = DVE([o1, rinv_free[oi]], lambda oi=oi: nc.vector.reciprocal(out=rinv[oi][64:65, :], in_=osb[oi][64:65, :]))
                                hs = dict(o2=o2, oi=oi, h=h)

                                def part_a(hs=hs):
                                    oi = hs['oi']
                                    o3 = PE([hs['o2'], A['prb_free']], lambda oi=oi: nc.tensor.matmul(prb[0:64, :], lhsT=ones_f[64:65, 0:64], rhs=rinv[oi][64:65, :],
                                                                                                 start=True, stop=True))
                                    rinv_free[oi] = o3
                                    o4 = DVE([o3, aT_free[oi]], lambda oi=oi: nc.vector.tensor_tensor(out=aT[oi][0:64, :], in0=osb[oi][0:64, :],
                                                                                                       in1=prb[0:64, :], op=ALU.mult))
                                    A['prb_free'] = o4
                                    osb_free[oi] = o4
                                    hs['o4'] = o4

                                def part_b(hs=hs):
                                    oi = hs['oi']; h = hs['h']
                                    o5 = None
                                    for t in range(4):
                                        o5 = PE([hs['o4'], pa_free if h == 0 else None] if t == 0 else None,
                                                lambda t=t, h=h, oi=oi: nc.tensor.transpose(
                                                    pa[:, t * 512 + h * 64: t * 512 + (h + 1) * 64], aT[oi][0:64, t * 128:(t + 1) * 128], ident_b[0:64, 0:64]),
                                                sig=(t == 3))
                                    aT_free[oi] = o5
                                    pa_w.append(o5)
                                pending.append([2, part_a])
                                pending.append([4, part_b])
                        for pend in list(pending):
                            pend[1]()
                            pending.remove(pend)
                        prb_free = A['prb_free']
                        QT_free = QT_readers
                        pa_r = []
                        M = dict(pmT_free=pmT_free, prb_free=prb_free, otmp_free=otmp_free)

                        def mg_tile(t, ti):
                            tok0 = s * S + qg * 512 + t * 128
                            otok0 = qbase + qg * 512 + t * 128
                            st_ = stt[ti]
                            lsn = DMA('snl%d' % ti, [merged_free[ti], ready2], lambda: nc.sync.dma_start(
                                out=merged[ti][:, 512:1024], in_=sn_d[tok0:tok0 + 128, :]))
                            lxr = DMA('xr%d' % ti, [xr_free[ti], ready2], lambda: nc.sync.dma_start(
                                out=xr[ti][:], in_=xs[tok0:tok0 + 128, :]))
                            a1 = ACT(pa_w, lambda: nc.scalar.activation(out=junk[:, 0:512], in_=pa[:, t * 512:(t + 1) * 512], func=AF.Square,
                                                                        accum_out=st_[:, 0:1]))
                            yield
                            r1a = ACT(a1, lambda: nc.scalar.activation(out=st_[:, 1:2], in_=st_[:, 0:1], func=AF.Sqrt, bias=EPS, scale=1.0 / 512))
                            yield
                            r1 = DVE(r1a, lambda: nc.vector.reciprocal(out=st_[:, 1:2], in_=st_[:, 1:2]))
                            c1 = ACT([r1, merged_free[ti]], lambda: nc.scalar.activation(
                                out=merged[ti][:, 0:512], in_=pa[:, t * 512:(t + 1) * 512], func=AF.Copy, scale=st_[:, 1:2]))
                            pa_r.append(c1)
                            yield
                            tp = None
                            for k in range(8):
                                tp = PE([c1, lsn, M['pmT_free'], M['prb_free']] if k == 0 else None,
                                        lambda k=k: nc.tensor.transpose(pmT[:, k * 128:(k + 1) * 128], merged[ti][:, k * 128:(k + 1) * 128], ident_b[:]),
                                        sig=(k == 7))
                            merged_free[ti] = tp
                            yield
                            ev = DVE([tp, mT_free[ti]], lambda: nc.vector.tensor_copy(out=mT[ti][:].rearrange("p k c -> p (k c)"), in_=pmT))
                            M['pmT_free'] = ev
                            M['prb_free'] = ev
                            yield
                            T = scT[t % 2]
                            mo1 = mmg(T[:, 0:512], [(mT[ti][:, k, :], wo[:, k, 0:512]) for k in range(8)], [ev, sc_free[t % 2]])
                            mo2 = mmg(T[:, 512:1024], [(mT[ti][:, k, :], wo[:, k, 512:1024]) for k in range(8)], None)
                            mT_free[ti] = mo2
                            yield
                            a2 = ACT(mo2, lambda: nc.scalar.activation(out=junk[:], in_=T[:, :], func=AF.Square, accum_out=st_[:, 2:3]))
                            yield
                            r2a = ACT(a2, lambda: nc.scalar.activation(out=st_[:, 3:4], in_=st_[:, 2:3], func=AF.Sqrt, bias=EPS, scale=1.0 / D))
                            yield
                            r2 = DVE(r2a, lambda: nc.vector.reciprocal(out=st_[:, 3:4], in_=st_[:, 3:4]))
                            d1 = DVE([r2, M['otmp_free']], lambda: nc.vector.scalar_tensor_tensor(
                                out=otmp[:], in0=T[:, :], scalar=st_[:, 3:4], in1=gvec1[:], op0=ALU.mult, op1=ALU.mult))
                            sc_free[t % 2] = d1
                            pp = POOL([d1, lxr, x1_free[ti]], lambda: nc.gpsimd.tensor_tensor(out=x1[ti][:], in0=otmp[:], in1=xr[ti][:], op=ALU.add))
                            M['otmp_free'] = pp
                            xr_free[ti] = pp
                            x1_free[ti] = DMA('x1s%d' % ti, pp, lambda: nc.sync.dma_start(
                                out=x1_d[otok0:otok0 + 128, :], in_=x1[ti][:]))
                        interleave((mg_tile(t, (tcount + t) % 2) for t in range(4)), 2)
                        tcount += 4
                        pmT_free = M['pmT_free']; prb_free = M['prb_free']; otmp_free = M['otmp_free']
                        pa_free = pa_r
                    p2_bar = dp.last() + [x1_free]
                    dp.retire_since(mk2)
                seq_bar = p2_bar
                qbase += NQG[s] * 512
            stageA_bar = seq_bar

        NTT = NQT // 128
        gseq = []
        for s in range(NSEQ):
            gseq += [s] * (NQG[s] * 4)
        with ExitStack() as bes:
            def sbB(name, shape, dt): return bes.enter_context(nc.sbuf_tensor(U(name), shape, dt))
            geff2_ = [sbB("geff2_%d" % i, [128, D], F32) for i in range(2)]
            sh2r_ = [sbB("sh2r_%d" % i, [128, D], F32) for i in range(2)]
            gvec2_ = [sbB("gvec2_%d" % i, [128, D], F32) for i in range(2)]
            vt = [sbB("vtB%d" % i, [128, D], F32) for i in range(2)]
            M1a = sbB("M1a", [128, NTT, 32], F32)
            M2a = sbB("M2a", [128, NTT, 32], F32)
            W1a = sbB("W1a", [128, NTT], F32)
            W2a = sbB("W2a", [128, NTT], F32)
            R1a = sbB("R1a", [128, NTT], F32)
            R2a = sbB("R2a", [128, NTT], F32)
            slot0 = sbB("slot0", [128, NTT], I32)
            slot1 = sbB("slot1", [128, NTT], I32)
            idxw = sbB("idxw", [128, NSL], I32)
            carry = sbB("carry", [128, 32], F32)
            PS = [bes.enter_context(nc.psum_tensor(U("PS%d" % i), [128, 512], F32)) for i in range(8)]
            bb = stageA_bar
            readyB = [bb, wcast_tok, zero_tok]

            def load_vecs(s, deps):
                geff2 = geff2_[s % 2]; sh2r = sh2r_[s % 2]; gvec2 = gvec2_[s % 2]
                l1 = DMA('v0_0', deps, lambda s=s: nc.sync.dma_start(out=vt[0][:], in_=mod_d[s, 4096:5120].partition_broadcast(128)))
                l2 = DMA('v0_1', deps, lambda: nc.sync.dma_start(out=vt[1][:], in_=g_pre2.partition_broadcast(128)))
                l3 = DMA('v0_2', deps, lambda s=s: nc.sync.dma_start(out=sh2r[:], in_=mod_d[s, 3072:4096].partition_broadcast(128)))
                tg = DVE([l1, l2], lambda: nc.vector.scalar_tensor_tensor(out=geff2[:], in0=vt[0][:], scalar=1.0, in1=vt[1][:],
                                                                           op0=ALU.add, op1=ALU.mult))
                l4 = DMA('v0_3', [tg], lambda s=s: nc.sync.dma_start(out=vt[0][:], in_=mod_d[s, 5120:6144].partition_broadcast(128)))
                l5 = DMA('v0_4', [tg], lambda: nc.sync.dma_start(out=vt[1][:], in_=g_post2.partition_broadcast(128)))
                tg2 = DVE([l4, l5], lambda: nc.vector.tensor_tensor(out=gvec2[:], in0=vt[0][:], in1=vt[1][:], op=ALU.mult))
                return [l3, tg, tg2]

            mkb1 = dp.mark()
            with ExitStack() as b1:
                def sb1(name, shape, dt): return b1.enter_context(nc.sbuf_tensor(U(name), shape, dt))
                w_r = sb1("w_r", [128, 8, 36], F32)
                brr = sb1("brr", [128, 36], F32)
                utri = sb1("utri", [128, 128], BF16)
                onesb = sb1("onesb", [128, 128], BF16)
                x1t = [sb1("x1t%d" % i, [128, D], F32) for i in range(5)]
                junk = sb1("junkB", [128, D], BF16)
                hm = sb1("hmB", [128, D], F32)
                h2 = [sb1("h2_%d" % i, [128, D], F32) for i in range(5)]
                h2Tf = [sb1("h2Tf%d" % i, [128, 8, 128], F32) for i in range(5)]
                stt = [sb1("stB%d" % i, [128, 8], F32) for i in range(5)]
                lg = [sb1("lg%d" % i, [128, 36], F32) for i in range(5)]
                wk = [sb1("wk%d" % i, [128, 192], F32) for i in range(5)]
                ohb = [sb1("ohb%d" % i, [128, 32], BF16) for i in range(5)]
                PH = [PS[6], PS[7]]
                ld = [DMA('s0', bb, lambda: nc.sync.dma_start(out=w_r[:, :, 0:4], in_=w_rg.rearrange("(k p) c -> p k c", p=128))),
                      DMA('s0', bb, lambda: nc.sync.dma_start(out=w_r[:, :, 4:36], in_=w_re.rearrange("(k p) c -> p k c", p=128))),
                      DMA('s0', bb, lambda: nc.sync.dma_start(out=brr[:, 0:4], in_=b_rg.partition_broadcast(128))),
                      DMA('s0', bb, lambda: nc.sync.dma_start(out=brr[:, 4:36], in_=b_re.partition_broadcast(128))),
                      DMA('s1', bb, lambda: nc.gpsimd.dma_start(out=utri[:], in_=utri_in), q='pool'),
                      POOL(bb, lambda: nc.gpsimd.memset(onesb[:], 1.0)),
                      POOL(bb, lambda: nc.gpsimd.memset(carry[:], 0.0))]
                rdy1 = [readyB, ld]
                T = dict(cur_seq=-1, vec_ready=None, vec_readers=[], hm_free=None, PH_free=[None, None], plg_free=None,
                         pcum_free=None, carry_tok=ld[-1])
                x1t_free = [None] * 5; h2_free = [None] * 5
                h2Tf_free = [None] * 5
                h2d_w = []

                def p1_tile(i):
                    s = gseq[i]
                    if s != T['cur_seq']:
                        T['cur_seq'] = s
                        T['vec_ready'] = load_vecs(s, [rdy1, T['vec_readers']])
                        T['vec_readers'] = []
                    vec_ready = T['vec_ready']
                    geff2 = geff2_[s % 2]; sh2r = sh2r_[s % 2]
                    ti = i % 5
                    tok0 = i * 128
                    st_ = stt[ti]
                    lx = DMA('bx%d' % ti, [x1t_free[ti], rdy1], lambda ti=ti, tok0=tok0: nc.sync.dma_start(out=x1t[ti][:], in_=x1_d[tok0:tok0 + 128, :]))
                    a1 = ACT(lx, lambda ti=ti, st_=st_: nc.scalar.activation(out=junk[:], in_=x1t[ti][:], func=AF.Square, accum_out=st_[:, 0:1]))
                    yield
                    r1a = ACT(a1, lambda st_=st_: nc.scalar.activation(out=st_[:, 1:2], in_=st_[:, 0:1], func=AF.Sqrt, bias=EPS, scale=1.0 / D))
                    yield
                    r1 = DVE(r1a, lambda st_=st_: nc.vector.reciprocal(out=st_[:, 1:2], in_=st_[:, 1:2]))
                    d1 = DVE([r1, T['hm_free'], vec_ready], lambda ti=ti, st_=st_: nc.vector.scalar_tensor_tensor(
                        out=hm[:], in0=x1t[ti][:], scalar=st_[:, 1:2], in1=geff2[:], op0=ALU.mult, op1=ALU.mult))
                    x1t_free[ti] = d1
                    p1_ = POOL([d1, h2_free[ti], vec_ready], lambda ti=ti: nc.gpsimd.tensor_tensor(out=h2[ti][:], in0=hm[:], in1=sh2r[:], op=ALU.add))
                    T['hm_free'] = p1_
                    T['vec_readers'] = [p1_, d1]
                    wr = DMA('h2w%d' % ti, p1_, lambda ti=ti, tok0=tok0: nc.gpsimd.dma_start(out=h2_d[tok0:tok0 + 128, :], in_=h2[ti][:]), q='pool')
                    h2d_w.append(wr)
                    yield
                    tp = None
                    for k in range(8):
                        bank = PH[k // 4]
                        tp = PE([p1_, T['PH_free']] if k == 0 else None,
                                lambda k=k, ti=ti, bank=bank: nc.tensor.transpose(bank[:, (k % 4) * 128:(k % 4 + 1) * 128],
                                                                                  h2[ti][:, k * 128:(k + 1) * 128], ident_f[:]),
                                sig=(k == 7))
                    h2_free[ti] = [tp, wr]
                    yield
                    e1 = ACT([tp, h2Tf_free[ti]], lambda ti=ti: nc.scalar.copy(out=h2Tf[ti][:, 0:4, :], in_=PH[0][:, :].rearrange("p (k c) -> p k c", k=4)))
                    e2 = DVE([tp, h2Tf_free[ti]], lambda ti=ti: nc.vector.tensor_copy(out=h2Tf[ti][:, 4:8, :], in_=PH[1][:, :].rearrange("p (k c) -> p k c", k=4)))
                    T['PH_free'] = [e1, e2]
                    yield
                    plg = PS[4][:, 0:36]
                    m_l = mmg(plg, [(h2Tf[ti][:, k, :], w_r[:, k, :]) for k in range(8)], [e1, e2, T['plg_free'], rdy1])
                    h2Tf_free[ti] = m_l
                    yield
                    L = lg[ti]; W = wk[ti]
                    v1 = DVE([m_l], lambda L=L: nc.vector.tensor_tensor(out=L[:], in0=plg, in1=brr[:], op=ALU.add))
                    T['plg_free'] = v1
                    v2 = DVE(v1, lambda L=L, W=W: nc.vector.tensor_reduce(out=W[:, 0:1], in_=L[:, 0:4], axis=AX.X, op=ALU.max))
                    v3 = DVE(v2, lambda W=W: nc.vector.tensor_scalar(out=W[:, 1:2], in0=W[:, 0:1], scalar1=-1.0, scalar2=None, op0=ALU.mult))
                    v4 = DVE(v2, lambda L=L, W=W: nc.vector.tensor_scalar(out=W[:, 4:8], in0=L[:, 0:4], scalar1=W[:, 0:1], scalar2=None, op0=ALU.is_equal))
                    s1 = ACT([v3], lambda L=L, W=W: nc.scalar.activation(out=W[:, 8:12], in_=L[:, 0:4], func=AF.Exp, bias=W[:, 1:2], scale=1.0,
                                                                         accum_out=W[:, 2:3]))
                    yield
                    v5 = DVE(s1, lambda W=W: nc.vector.reciprocal(out=W[:, 3:4], in_=W[:, 2:3]))
                    v6 = DVE(v4, lambda L=L, W=W: nc.vector.tensor_tensor(
                        out=W[:, 16:48].rearrange("p (g e) -> p g e", g=4), in0=L[:, 4:36].rearrange("p (g e) -> p g e", g=4),
                        in1=W[:, 4:8].unsqueeze(2).to_broadcast([128, 4, 8]), op=ALU.mult))
                    v7 = DVE(v6, lambda W=W: nc.vector.tensor_reduce(out=W[:, 48:56], in_=W[:, 16:48].rearrange("p (g e) -> p e g", g=4),
                                                                    axis=AX.X, op=ALU.add))
                    v8 = DVE(v7, lambda W=W: nc.vector.tensor_reduce(out=W[:, 12:13], in_=W[:, 48:56], axis=AX.X, op=ALU.max))
                    v9 = DVE(v8, lambda W=W: nc.vector.tensor_scalar(out=W[:, 56:64], in0=W[:, 48:56], scalar1=W[:, 12:13], scalar2=None,
                                                                    op0=ALU.is_equal))
                    v10 = DVE(v9, lambda W=W: nc.vector.scalar_tensor_tensor(out=W[:, 64:72], in0=W[:, 56:64], scalar=-1e30, in1=W[:, 48:56],
                                                                            op0=ALU.mult, op1=ALU.add))
                    v11 = DVE(v10, lambda W=W: nc.vector.tensor_reduce(out=W[:, 13:14], in_=W[:, 64:72], axis=AX.X, op=ALU.max))
                    v12 = DVE(v11, lambda W=W: nc.vector.tensor_scalar(out=W[:, 72:80], in0=W[:, 64:72], scalar1=W[:, 13:14], scalar2=None,
                                                                      op0=ALU.is_equal))
                    v13 = DVE(v11, lambda W=W: nc.vector.tensor_scalar(out=W[:, 14:15], in0=W[:, 12:13], scalar1=-1.0, scalar2=None, op0=ALU.mult))
                    s2 = ACT([v13], lambda W=W: nc.scalar.activation(out=W[:, 15:16], in_=W[:, 13:14], func=AF.Exp, bias=W[:, 14:15], scale=1.0))
                    yield
                    v14 = DVE(s2, lambda W=W: nc.vector.tensor_scalar(out=W[:, 80:81], in0=W[:, 15:16], scalar1=1.0, scalar2=None, op0=ALU.add))
                    v15 = DVE(v14, lambda W=W: nc.vector.reciprocal(out=W[:, 81:82], in_=W[:, 80:81]))
                    v16 = DVE([v15, v5], lambda W=W, i=i: nc.vector.tensor_tensor(out=W1a[:, i:i + 1], in0=W[:, 81:82], in1=W[:, 3:4], op=ALU.mult))
                    v17 = DVE(v16, lambda W=W, i=i: nc.vector.tensor_tensor(out=W2a[:, i:i + 1], in0=W1a[:, i:i + 1], in1=W[:, 15:16], op=ALU.mult))
                    v18 = DVE([v9, v4], lambda W=W, i=i: nc.vector.tensor_tensor(
                        out=M1a[:, i, :].rearrange("p (g e) -> p g e", g=4), in0=W[:, 4:8].unsqueeze(2).to_broadcast([128, 4, 8]),
                        in1=W[:, 56:64].unsqueeze(1).to_broadcast([128, 4, 8]), op=ALU.mult))
                    v19 = DVE([v12], lambda W=W, i=i: nc.vector.tensor_tensor(
                        out=M2a[:, i, :].rearrange("p (g e) -> p g e", g=4), in0=W[:, 4:8].unsqueeze(2).to_broadcast([128, 4, 8]),
                        in1=W[:, 72:80].unsqueeze(1).to_broadcast([128, 4, 8]), op=ALU.mult))
                    OH = ohb[ti]
                    v20 = DVE([v18, v19, T['pcum_free']], lambda OH=OH, i=i: nc.vector.tensor_tensor(out=OH[:], in0=M1a[:, i, :], in1=M2a[:, i, :], op=ALU.add))
                    pcum = PS[5][:, 0:32]
                    ptot = PS[5][:, 32:64]
                    PE([v20, T['pcum_free'], rdy1], lambda OH=OH: nc.tensor.matmul(pcum, lhsT=utri[:], rhs=OH[:], start=True, stop=True), sig=False)
                    mc = PE(None, lambda OH=OH: nc.tensor.matmul(ptot, lhsT=onesb[:], rhs=OH[:], start=True, stop=True))
                    yield
                    v21 = DVE([mc, T['carry_tok']], lambda W=W: nc.vector.tensor_tensor(out=W[:, 96:128], in0=carry[:], in1=pcum, op=ALU.add))
                    v22 = DVE(v21, lambda: nc.vector.tensor_tensor(out=carry[:], in0=carry[:], in1=ptot, op=ALU.add))
                    T['carry_tok'] = v22
                    T['pcum_free'] = v22
                    v23 = DVE(v22, lambda W=W, i=i: nc.vector.tensor_tensor(out=W[:, 128:160], in0=W[:, 96:128], in1=M1a[:, i, :], op=ALU.mult))
                    v24 = DVE(v23, lambda W=W, i=i: nc.vector.tensor_reduce(out=R1a[:, i:i + 1], in_=W[:, 128:160], axis=AX.X, op=ALU.add))
                    v25 = DVE(v24, lambda W=W, i=i: nc.vector.tensor_tensor(out=W[:, 160:192], in0=W[:, 96:128], in1=M2a[:, i, :], op=ALU.mult))
                    v26 = DVE(v25, lambda W=W, i=i: nc.vector.tensor_reduce(out=R2a[:, i:i + 1], in_=W[:, 160:192], axis=AX.X, op=ALU.add))
                interleave((p1_tile(i) for i in range(NTT)), 5)
                b1_bar = dp.last() + [h2d_w]
                dp.retire_since(mkb1)

            with ExitStack() as b2:
                def sb2(name, shape, dt): return b2.enter_context(nc.sbuf_tensor(U(name), shape, dt))
                jv = sb2("jv", [128, NSL], F32)
                pidx = sb2("pidx", [128, 1], F32)
                tri32 = sb2("tri32", [32, 32], F32)
                cmp_ = sb2("cmp", [128, NSL * 32], F32)
                tmpM = sb2("tmpM", [128, NTT, 32], F32)
                nblk = sb2("nblk", [128, 32], F32)
                pc = sb2("pc", [128, 32], F32)
                pcT = sb2("pcT", [32, 128], F32)
                sst = sb2("sst", [128, 32], F32)
                send = sb2("send", [128, 32], F32)
                te = sb2("te", [128, NSL], F32)
                sf = sb2("sf", [128, NTT], F32)
                l = [DMA('i0', b1_bar, lambda: nc.sync.dma_start(out=jv[:], in_=jv_in)),
                     DMA('i0', b1_bar, lambda: nc.sync.dma_start(out=pidx[:], in_=pidx_in)),
                     DMA('i0', b1_bar, lambda: nc.sync.dma_start(out=tri32[:], in_=tri32_in))]
                c3 = cmp_[:].rearrange("p (e m) -> p e m", e=32)
                q1 = DVE([l, b1_bar], lambda: nc.vector.tensor_tensor(out=c3, in0=jv[:].unsqueeze(1).to_broadcast([128, 32, NSL]),
                                                                      in1=carry[:].unsqueeze(2).to_broadcast([128, 32, NSL]), op=ALU.is_lt))
                q2 = DVE(q1, lambda: nc.vector.tensor_reduce(out=nblk[:], in_=c3, axis=AX.X, op=ALU.add))
                q3 = DVE(q2, lambda: nc.vector.tensor_scalar(out=pc[:], in0=nblk[:], scalar1=128.0, scalar2=None, op0=ALU.mult))
                q4 = PE(q3, lambda: nc.tensor.transpose(PS[0][0:32, 0:128], pc[:, :], ident_f[:]))
                q5 = ACT(q4, lambda: nc.scalar.copy(out=pcT[:], in_=PS[0][0:32, 0:128]))
                q6 = PE([q5, l], lambda: nc.tensor.matmul(PS[1][:, 0:32], lhsT=pcT[:, :], rhs=tri32[:, :], start=True, stop=True))
                q7 = DVE(q6, lambda: nc.vector.tensor_copy(out=sst[:], in_=PS[1][:, 0:32]))
                q8 = DVE(q7, lambda: nc.vector.tensor_tensor(out=send[:], in0=sst[:], in1=pc[:], op=ALU.add))
                c4 = cmp_[:].rearrange("p (m e) -> p m e", e=32)
                q9 = DVE(q8, lambda: nc.vector.tensor_tensor(out=c4, in0=send[:].unsqueeze(1).to_broadcast([128, NSL, 32]),
                                                             in1=jv[:].unsqueeze(2).to_broadcast([128, NSL, 32]), op=ALU.is_le))
                q10 = DVE(q9, lambda: nc.vector.tensor_reduce(out=te[:], in_=c4, axis=AX.X, op=ALU.add))
                q11 = DVE(q10, lambda: nc.vector.tensor_scalar(out=te[:], in0=te[:], scalar1=31.0, scalar2=128.0, op0=ALU.min, op1=ALU.mult))
                q12 = DVE(q11, lambda: nc.vector.tensor_scalar(out=te[:], in0=te[:], scalar1=pidx[:, 0:1], scalar2=None, op0=ALU.add))
                q13 = DVE(q12, lambda: nc.vector.tensor_copy(out=idxw[:], in_=te[:]))
                q14 = DVE(q7, lambda: nc.vector.tensor_tensor(out=tmpM[:], in0=M1a[:], in1=sst[:].unsqueeze(1).to_broadcast([128, NTT, 32]), op=ALU.mult))
                q15 = DVE(q14, lambda: nc.vector.tensor_reduce(out=sf[:], in_=tmpM[:], axis=AX.X, op=ALU.add))
                q16 = DVE(q15, lambda: nc.vector.tensor_tensor(out=sf[:], in0=sf[:], in1=R1a[:], op=ALU.add))
                q17 = DVE(q16, lambda: nc.vector.tensor_copy(out=slot0[:], in_=sf[:]))
                q18 = DVE(q17, lambda: nc.vector.tensor_tensor(out=tmpM[:], in0=M2a[:], in1=sst[:].unsqueeze(1).to_broadcast([128, NTT, 32]), op=ALU.mult))
                q19 = DVE(q18, lambda: nc.vector.tensor_reduce(out=sf[:], in_=tmpM[:], axis=AX.X, op=ALU.add))
                q20 = DVE(q19, lambda: nc.vector.tensor_tensor(out=sf[:], in0=sf[:], in1=R2a[:], op=ALU.add))
                q21 = DVE(q20, lambda: nc.vector.tensor_copy(out=slot1[:], in_=sf[:]))
                b2_bar = dp.last()

            mkb3 = dp.mark()
            with ExitStack() as b3:
                def sb3(name, shape, dt): return b3.enter_context(nc.sbuf_tensor(U(name), shape, dt))
                hsc = [sb3("hsc%d" % i, [128, D], BF16) for i in range(3)]
                hsc_free = [None] * 3
                sc_toks = []
                for i in range(NTT):
                    si = i % 3
                    tok0 = i * 128
                    lh = DMA('hl%d' % si, [hsc_free[si], b2_bar], lambda si=si, tok0=tok0: nc.sync.dma_start(out=hsc[si][:], in_=h2_d[tok0:tok0 + 128, :]))
                    s0 = DMA('sc%d' % si, [lh, b2_bar], lambda si=si, i=i: nc.gpsimd.indirect_dma_start(
                        out=xs_d[:, :], out_offset=bass.IndirectOffsetOnAxis(ap=slot0[:, i:i + 1], axis=0), in_=hsc[si][:, :], in_offset=None), q='pool')
                    s1_ = DMA('sc%d' % si, [lh], lambda si=si, i=i: nc.gpsimd.indirect_dma_start(
                        out=xs_d[:, :], out_offset=bass.IndirectOffsetOnAxis(ap=slot1[:, i:i + 1], axis=0), in_=hsc[si][:, :], in_offset=None), q='pool')
                    hsc_free[si] = [s0, s1_]
                    sc_toks += [s0, s1_]
                scat_done = [sc_toks[-6:], b2_bar]

                PF = 3
                NW = PF + 3
                ND = PF + 5
                NX = PF + 2
                wgu = [sb3("wgu%d" % i, [128, 8, 512], BF16) for i in range(NW)]
                wdb = [sb3("wdb%d" % i, [128, 2, D], BF16) for i in range(ND)]
                xsb = [sb3("xsb%d" % i, [128, D], BF16) for i in range(NX)]
                xT = [sb3("xT%d" % i, [128, 8, 128], BF16) for i in range(2)]
                sgs = [sb3("sgs%d" % i, [128, 256], F32) for i in range(2)]
                hid = [sb3("hid%d" % i, [128, 256], BF16) for i in range(2)]
                hT = [sb3("hT%d" % i, [128, 2, 128], BF16) for i in range(2)]
                ysb = [sb3("ysb%d" % i, [128, D], F32) for i in range(2)]
                pX = [PS[0][:, :].bitcast(BF16), PS[1][:, :].bitcast(BF16)]
                pH = [PS[2], PS[3]]
                pHT = [PS[4][:, 0:128].bitcast(BF16), PS[5][:, 0:128].bitcast(BF16)]
                pY = [PS[6], PS[7]]
                wgu_free = [None] * NW; wdb_free = [None] * ND; xsb_free = [None] * NX
                pX_free = [None, None]; xT_free = [None, None]; pH_free = [None, None]; sgs_free = [None, None]
                hid_free = [None, None]; pHT_free = [None, None]; hT_free = [None, None]
                pY_free = [None, None]; ysb_free = [None, None]
                st0 = {}; st1 = {}; st2 = {}; ldt = {}
                ys_w = []

                def issue_loads(a):
                    wi = a % NW; di = a % ND; xj = a % NX
                    lw = DMA('wgl%d' % wi, [wgu_free[wi], scat_done], lambda wi=wi, a=a: nc.gpsimd.indirect_dma_start(
                        out=wgu[wi][:].rearrange("p k c -> p (k c)"), out_offset=None, in_=wgu_r[:, :],
                        in_offset=bass.IndirectOffsetOnAxis(ap=idxw[:, a:a + 1], axis=0)), q='pool')
                    lwd = DMA('wdl%d' % di, [wdb_free[di], scat_done], lambda di=di, a=a: nc.gpsimd.indirect_dma_start(
                        out=wdb[di][:].rearrange("p k c -> p (k c)"), out_offset=None, in_=wd_r[:, :],
                        in_offset=bass.IndirectOffsetOnAxis(ap=idxw[:, a:a + 1], axis=0)), q='pool')
                    lxs = DMA('xsl%d' % xj, [xsb_free[xj], scat_done, sc_toks], lambda xj=xj, a=a: nc.sync.dma_start(
                        out=xsb[xj][:], in_=xs_d[a * 128:(a + 1) * 128, :]))
                    ldt[a] = (lw, lwd, lxs)

                for a in range(min(PF, NSL)):
                    issue_loads(a)
                for it in range(NSL + 3):
                    if it + PF < NSL:
                        issue_loads(it + PF)
                    a = it
                    if a < NSL:
                        xi = a % 2; xj = a % NX
                        lw, lwd, lxs = ldt[a]
                        tp = None
                        for k in range(8):
                            tp = PE([lxs, pX_free[xi]] if k == 0 else None,
                                    lambda k=k, xi=xi, xj=xj: nc.tensor.transpose(pX[xi][:, k * 128:(k + 1) * 128], xsb[xj][:, k * 128:(k + 1) * 128], ident_b[:]),
                                    sig=(k == 7))
                        xsb_free[xj] = tp
                        if a % 2 == 0:
                            ev = ACT([tp, xT_free[xi]], lambda xi=xi: nc.scalar.copy(out=xT[xi][:].rearrange("p k c -> p (k c)"), in_=pX[xi]))
                        else:
                            ev = DVE([tp, xT_free[xi]], lambda xi=xi: nc.vector.tensor_copy(out=xT[xi][:].rearrange("p k c -> p (k c)"), in_=pX[xi]))
                        pX_free[xi] = ev
                        st0[a] = (ev, lw, lwd)
                    a = it - 1
                    if 0 <= a < NSL:
                        wi = a % NW; xi = a % 2
                        ev, lw, lwd = st0[a]
                        mh = mmg(pH[xi][:, :], [(xT[xi][:, k, :], wgu[wi][:, k, :]) for k in range(8)], [ev, lw, pH_free[xi]])
                        wgu_free[wi] = mh
                        xT_free[xi] = mh
                        a_s = ACT([mh, sgs_free[xi]], lambda xi=xi: nc.scalar.activation(out=sgs[xi][:], in_=pH[xi][:, 0:256], func=AF.Silu))
                        d_h = DVE([a_s, hid_free[xi]], lambda xi=xi: nc.vector.tensor_tensor(out=hid[xi][:], in0=sgs[xi][:], in1=pH[xi][:, 256:512], op=ALU.mult))
                        pH_free[xi] = d_h
                        sgs_free[xi] = d_h
                        st1[a] = (d_h, lwd)
                    a = it - 2
                    if 0 <= a < NSL:
                        xi = a % 2
                        d_h, lwd = st1[a]
                        tp2 = None
                        for j in range(2):
                            tp2 = PE([d_h, pHT_free[xi]] if j == 0 else None,
                                     lambda j=j, xi=xi: nc.tensor.transpose(pHT[xi][:, j * 128:(j + 1) * 128], hid[xi][:, j * 128:(j + 1) * 128], ident_b[:]),
                                     sig=(j == 1))
                        hid_free[xi] = tp2
                        ev2 = ACT([tp2, hT_free[xi]], lambda xi=xi: nc.scalar.copy(out=hT[xi][:].rearrange("p k c -> p (k c)"), in_=pHT[xi]))
                        pHT_free[xi] = ev2
                        st2[a] = (ev2, lwd)
                    a = it - 3
                    if 0 <= a < NSL:
                        xi = a % 2; di = a % ND
                        ev2, lwd = st2[a]
                        my0 = mmg(pY[0][:, :], [(hT[xi][:, j, :], wdb[di][:, j, 0:512]) for j in range(2)], [ev2, lwd, pY_free[0]])
                        my1 = mmg(pY[1][:, :], [(hT[xi][:, j, :], wdb[di][:, j, 512:1024]) for j in range(2)], [pY_free[1]])
                        wdb_free[di] = my1
                        hT_free[xi] = my1
                        c0 = ACT([my0, ysb_free[xi]], lambda xi=xi: nc.scalar.copy(out=ysb[xi][:, 0:512], in_=pY[0][:, :]))
                        c1 = DVE([my1, ysb_free[xi]], lambda xi=xi: nc.vector.tensor_copy(out=ysb[xi][:, 512:1024], in_=pY[1][:, :]))
                        pY_free = [c0, c1]
                        ysb_free[xi] = DMA('ysw%d' % xi, [c0, c1], lambda xi=xi, a=a: nc.sync.dma_start(out=ys_d[a * 128:(a + 1) * 128, :], in_=ysb[xi][:]))
                        ys_w.append(ysb_free[xi])
                b3_bar = dp.last() + [ys_w[-2:]]
                dp.retire_since(mkb3)

            with ExitStack() as b4:
                def sb4(name, shape, dt): return b4.enter_context(nc.sbuf_tensor(U(name), shape, dt))
                ya = [sb4("ya%d" % i, [128, D], F32) for i in range(5)]
                yb = [sb4("yb%d" % i, [128, D], F32) for i in range(5)]
                x1c = [sb4("x1c%d" % i, [128, D], F32) for i in range(5)]
                mm_ = [sb4("mm_%d" % i, [128, D], F32) for i in range(5)]
                ytmp = [sb4("ytmp%d" % i, [128, D], F32) for i in range(5)]
                yo = [sb4("yo%d" % i, [128, D], F32) for i in range(5)]
                junk = sb4("junkC", [128, D], BF16)
                stt = [sb4("stC%d" % i, [128, 8], F32) for i in range(5)]
                ya_free = [None] * 5; yb_free = [None] * 5; x1c_free = [None] * 5; mm_free = [None] * 5
                ytmp_free = [None] * 5; yo_free = [None] * 5
                T = dict(cur_seq=-1, vec_ready=None, vec_readers=[])
                out_toks = []

                def cmb_tile(i):
                    s = gseq[i]
                    if s != T['cur_seq']:
                        T['cur_seq'] = s
                        T['vec_ready'] = load_vecs(s, [b3_bar, T['vec_readers']])
                        T['vec_readers'] = []
                    vec_ready = T['vec_ready']
                    gvec2 = gvec2_[s % 2]
                    ti = i % 5
                    tok0 = i * 128
                    st_ = stt[ti]
                    ga = DMA('ga%d' % ti, [ya_free[ti], b3_bar, ys_w], lambda ti=ti, i=i: nc.gpsimd.indirect_dma_start(
                        out=ya[ti][:, :], out_offset=None, in_=ys_d[:, :], in_offset=bass.IndirectOffsetOnAxis(ap=slot0[:, i:i + 1], axis=0)), q='pool')
                    gb_ = DMA('gb%d' % ti, [yb_free[ti], b3_bar], lambda ti=ti, i=i: nc.gpsimd.indirect_dma_start(
                        out=yb[ti][:, :], out_offset=None, in_=ys_d[:, :], in_offset=bass.IndirectOffsetOnAxis(ap=slot1[:, i:i + 1], axis=0)), q='pool')
                    lx = DMA('cx%d' % ti, [x1c_free[ti], b3_bar], lambda ti=ti, tok0=tok0: nc.sync.dma_start(out=x1c[ti][:], in_=x1_d[tok0:tok0 + 128, :]))
                    yield
                    d1 = DVE([ga, mm_free[ti]], lambda ti=ti, i=i: nc.vector.tensor_scalar(out=mm_[ti][:], in0=ya[ti][:], scalar1=W1a[:, i:i + 1], scalar2=None, op0=ALU.mult))
                    ya_free[ti] = d1
                    d2 = DVE([gb_, d1], lambda ti=ti, i=i: nc.vector.scalar_tensor_tensor(out=mm_[ti][:], in0=yb[ti][:], scalar=W2a[:, i:i + 1], in1=mm_[ti][:],
                                                                                         op0=ALU.mult, op1=ALU.add))
                    yb_free[ti] = d2
                    a1 = ACT(d2, lambda ti=ti, st_=st_: nc.scalar.activation(out=junk[:], in_=mm_[ti][:], func=AF.Square, accum_out=st_[:, 0:1]))
                    yield
                    r1a = ACT(a1, lambda st_=st_: nc.scalar.activation(out=st_[:, 1:2], in_=st_[:, 0:1], func=AF.Sqrt, bias=EPS, scale=1.0 / D))
                    yield
                    r1 = DVE(r1a, lambda st_=st_: nc.vector.reciprocal(out=st_[:, 1:2], in_=st_[:, 1:2]))
                    d3 = DVE([r1, ytmp_free[ti], vec_ready], lambda ti=ti, st_=st_: nc.vector.scalar_tensor_tensor(
                        out=ytmp[ti][:], in0=mm_[ti][:], scalar=st_[:, 1:2], in1=gvec2[:], op0=ALU.mult, op1=ALU.mult))
                    mm_free[ti] = d3
                    T['vec_readers'] = [d3]
                    pp = POOL([d3, lx, yo_free[ti]], lambda ti=ti: nc.gpsimd.tensor_tensor(out=yo[ti][:], in0=ytmp[ti][:], in1=x1c[ti][:], op=ALU.add))
                    ytmp_free[ti] = pp
                    x1c_free[ti] = pp
                    yo_free[ti] = DMA('yo%d' % ti, pp, lambda ti=ti, tok0=tok0: nc.sync.dma_start(out=y[tok0:tok0 + 128, :], in_=yo[ti][:]))
                    out_toks.append(yo_free[ti])
                interleave((cmb_tile(i) for i in range(NTT)), 5)
            dp.wait('sp', [yo_free, out_toks[-5:]])
            for e in ('pe', 'act', 'dve', 'pool'):
                dp.wait('sp', [(e, dp.cnt[e])])
    return nc


def _rope_tables(pos):
    half = 16
    inv = (10000.0 ** (-np.arange(half, dtype=np.float32) / half)).astype(np.float32)
    ang = pos.astype(np.float32)[:, None] * inv[None, :]
    cos = np.cos(ang).astype(np.float32)
    sin = np.sin(ang).astype(np.float32)
    c = np.concatenate([cos, cos], axis=1).T
    s_ = np.concatenate([sin, sin], axis=1).T
    return np.ascontiguousarray(c), np.ascontiguousarray(s_)


def _consts(NSL):
    ident = np.eye(128, dtype=np.float32)
    egrp = np.zeros((8, 512), np.float32)
    for g in range(8):
        egrp[g, g * 64:(g + 1) * 64] = 1.0
    utri = np.triu(np.ones((128, 128), np.float32), k=1)
    tri32 = np.triu(np.ones((32, 32), np.float32), k=1)
    jv = np.tile((np.arange(NSL, dtype=np.float32) * 128.0)[None, :], (128, 1))
    pidx = np.arange(128, dtype=np.float32).reshape(128, 1)
    return dict(ident=ident, egrp=egrp, utri=utri, tri32=tri32, jv=np.ascontiguousarray(jv), pidx=pidx,
                zeros=np.zeros((128, 8192), np.float32))


def _nt(cfg):
    nqt = sum(cfg['NQG']) * 512
    nt = (2 * nqt + 32 * 127 + 127) // 128
    return ((nt + 7) // 8) * 8


WEIGHT_KEYS = ['w_ada', 'b_ada', 'g_pre1', 'g_post1', 'g_pre2', 'g_post2', 'w_in', 'g_q', 'w_uq', 'g_kv', 'w_ukv',
               'g_v_gmlp', 'w_spatial', 'b_spatial', 'g_attn_out', 'g_gmlp_out', 'w_out', 'w_router_group',
               'b_router_group', 'w_router_expert', 'b_router_expert', 'w_gate', 'w_up', 'w_down']

_NC_CACHE = {}


def kernel(**inputs):
    S = 4096
    x_all = np.concatenate([np.asarray(inputs['x_prompt'], np.float32), np.asarray(inputs['x_sample'], np.float32)], axis=0)
    c_all = np.concatenate([np.asarray(inputs['c_prompt'], np.float32), np.asarray(inputs['c_sample'], np.float32)], axis=0)
    weights = {k: np.ascontiguousarray(np.asarray(inputs[k], np.float32)) for k in WEIGHT_KEYS}
    consts = _consts(_nt(FULL_CFG))
    pos_nat = np.arange(S)
    in_maps = []
    plans = []
    for c in range(8):
        if c % 2 == 0:
            s0 = (5 * c) // 2
            A, B, Cq, qhalf = s0, s0 + 1, s0 + 2, 0
        else:
            s0 = (5 * c - 1) // 2
            Cq, qhalf, A, B = s0, 1, s0 + 1, s0 + 2
        if qhalf == 0:
            posC = pos_nat
        else:
            posC = np.concatenate([pos_nat[S // 2:], pos_nat[:S // 2]])
        xs = np.concatenate([x_all[A], x_all[B], x_all[Cq][posC]], axis=0)
        cv = np.stack([c_all[A], c_all[B], c_all[Cq]], axis=0)
        rc = np.zeros((3, 32, S), np.float32)
        rs = np.zeros((3, 32, S), np.float32)
        for i, p in enumerate([pos_nat, pos_nat, posC]):
            rc[i], rs[i] = _rope_tables(p)
        m = dict(weights)
        m.update(xs=np.ascontiguousarray(xs), cvec=np.ascontiguousarray(cv), rope_c=rc, rope_s=rs)
        m.update(consts)
        in_maps.append(m)
        plans.append((A, B, Cq, qhalf))
    if 'full' not in _NC_CACHE:
        _NC_CACHE['full'] = build(FULL_CFG)
    nc = _NC_CACHE['full']
    res = run_bass_kernel_spmd(nc, in_maps, core_ids=list(range(8)))
    y_all = np.zeros((20, S, D), np.float32)
    for c in range(8):
        yc = res.results[c]['y']
        A, B, Cq, qhalf = plans[c]
        y_all[A] = yc[0:S]
        y_all[B] = yc[S:2 * S]
        if qhalf == 0:
            y_all[Cq, 0:S // 2] = yc[2 * S:2 * S + S // 2]
        else:
            y_all[Cq, S // 2:] = yc[2 * S:2 * S + S // 2]
    return (np.ascontiguousarray(y_all[0:4]), np.ascontiguousarray(y_all[4:20]))
```

```python
import numpy as np
import concourse.bass as bass
import concourse.mybir as mybir
from concourse.bass_utils import run_bass_kernel_spmd
from contextlib import ExitStack

F32, BF16 = mybir.dt.float32, mybir.dt.bfloat16
I32 = mybir.dt.int32
AF = mybir.ActivationFunctionType
ALU = mybir.AluOpType
AX = mybir.AxisListType
D = 1024
EPS = 1e-6
NE = 32
QSCALE = 96.0 ** -0.5

FULL_CFG = dict(S=4096, NSEQ=3, NQG=[8, 8, 4])


class Dep:
    def __init__(self, nc, es):
        self.nc = nc
        self.es = es
        self.eng = {'pe': nc.tensor, 'act': nc.scalar, 'dve': nc.vector, 'pool': nc.gpsimd, 'sp': nc.sync}
        self.sem = {e: es.enter_context(nc.semaphore('s_' + e)) for e in self.eng}
        self.cnt = {e: 0 for e in self.eng}
        self.waited = {e: {} for e in self.eng}
        self.dsem = {}
        self.entries = {}
        self.free = []

    def semof(self, k):
        return self.sem[k] if k in self.sem else self.entries[k][0]

    def wait(self, e, deps):
        mx = {}
        for k, v in _flat(deps):
            if v > mx.get(k, 0):
                mx[k] = v
        for k, v in mx.items():
            if self.waited[e].get(k, 0) < v:
                self.eng[e].wait_ge(self.semof(k), v)
                self.waited[e][k] = v

    def op(self, e, deps, fn, sig=True):
        self.wait(e, deps)
        ins = fn()
        if sig:
            ins.then_inc(self.sem[e], 1)
            self.cnt[e] += 1
            return (e, self.cnt[e])
        return None

    def dma(self, q, name, deps, fn):
        if name not in self.dsem:
            if self.free:
                key = self.free.pop()
            else:
                key = 'D%d' % len(self.entries)
                self.entries[key] = [self.es.enter_context(self.nc.semaphore('d_' + key)), 0]
            self.dsem[name] = key
        key = self.dsem[name]
        self.wait(q, deps)
        ins = fn()
        ent = self.entries[key]
        ins.then_inc(ent[0], 16)
        ent[1] += 16
        return (key, ent[1])

    def mark(self):
        return set(self.dsem.keys())

    def retire_since(self, mark, keep=()):
        for n in list(self.dsem.keys()):
            if n in mark or n in keep:
                continue
            key = self.dsem[n]
            self.wait('sp', (key, self.entries[key][1]))
            del self.dsem[n]
            self.free.append(key)

    def last(self):
        return [(e, self.cnt[e]) for e in self.eng if self.cnt[e] > 0]


def _flat(deps):
    out = []
    if deps is None:
        return out
    if isinstance(deps, tuple) and len(deps) == 2 and isinstance(deps[0], str):
        return [deps]
    for d in deps:
        out.extend(_flat(d))
    return out


def interleave(gens, depth):
    active = []
    it = iter(gens)
    done = False
    while True:
        if len(active) < depth and not done:
            try:
                active.append(next(it))
            except StopIteration:
                done = True
        if not active:
            break
        nxt = []
        for g in active:
            try:
                next(g)
                nxt.append(g)
            except StopIteration:
                pass
        active = nxt


def build(cfg):
    S = cfg['S']
    NSEQ = cfg['NSEQ']
    NQG = cfg['NQG']
    NG = S // 512
    KB = S // 128
    NT = NSEQ * S
    NQT = sum(NQG) * 512
    NGB = sum(NQG)
    NSL = (2 * NQT + 32 * 127 + 127) // 128
    NSL = ((NSL + 7) // 8) * 8

    nc = bass.Bass("TRN2", target_bir_lowering=False)

    def din(name, shape, dt=F32):
        return nc.dram_tensor(name, list(shape), dt, kind="ExternalInput").ap()

    def dscr(name, shape, dt):
        return nc.dram_tensor(name, list(shape), dt, kind="Internal").ap()

    xs = din("xs", [NT, D])
    cvec = din("cvec", [NSEQ, D])
    rope_c = din("rope_c", [NSEQ, 32, S])
    rope_s = din("rope_s", [NSEQ, 32, S])
    w_ada = din("w_ada", [D, 6 * D])
    b_ada = din("b_ada", [6 * D])
    g_pre1 = din("g_pre1", [D]); g_post1 = din("g_post1", [D])
    g_pre2 = din("g_pre2", [D]); g_post2 = din("g_post2", [D])
    w_in = din("w_in", [D, 1440])
    g_q = din("g_q", [256]); w_uq = din("w_uq", [256, 768])
    g_kv = din("g_kv", [128]); w_ukv = din("w_ukv", [128, 1024])
    g_v_gmlp = din("g_v_gmlp", [512])
    w_spatial = din("w_spatial", [8, 128, 128]); b_spatial = din("b_spatial", [8, 128])
    g_attn_out = din("g_attn_out", [512]); g_gmlp_out = din("g_gmlp_out", [512])
    w_out = din("w_out", [D, D])
    w_rg = din("w_router_group", [D, 4]); b_rg = din("b_router_group", [4])
    w_re = din("w_router_expert", [D, 32]); b_re = din("b_router_expert", [32])
    w_gate = din("w_gate", [NE, D, 256]); w_up = din("w_up", [NE, D, 256]); w_down = din("w_down", [NE, 256, D])
    ident_in = din("ident", [128, 128])
    egrp_in = din("egrp", [8, 512])
    utri_in = din("utri", [128, 128])
    tri32_in = din("tri32", [32, 32])
    jv_in = din("jv", [128, NSL])
    pidx_in = din("pidx", [128, 1])
    zeros_in = din("zeros", [128, 8192])
    y = nc.dram_tensor("y", [NQT, D], F32, kind="ExternalOutput").ap()

    mod_d = dscr("mod_d", [NSEQ, 6 * D], F32)
    sn_d = dscr("sn_d", [NT, 512], BF16)
    cq_d = dscr("cq_d", [NSEQ * NG, 128, 1024], BF16)
    x1_d = (nc.dram_tensor("x1_d", [NQT, D], F32, kind="ExternalOutput").ap() if cfg.get("dbg") else dscr("x1_d", [NQT, D], F32))
    wgu_r = dscr("wgu_r", [NE * 128, 8 * 512], BF16)
    wd_r = dscr("wd_r", [NE * 128, 2 * D], BF16)
    h2_d = dscr("h2_d", [NQT, D], BF16)
    xs_d = dscr("xs_d", [NSL * 128, D], BF16)
    ys_d = dscr("ys_d", [NSL * 128, D], F32)
    wkv_d = dscr("wkv_d", [128, 1024], BF16)
    wsp_d = dscr("wsp_d", [128, 1024], BF16)
    wq_d = dscr("wq_d", [128, 1536], BF16)
    wqsw_d = dscr("wqsw_d", [128, 1536], BF16)
    wo_d = dscr("wo_d", [128, 8192], BF16)

    _uid = [0]

    def U(name):
        _uid[0] += 1
        return "%s_u%d" % (name, _uid[0])

    top = ExitStack()
    with top:
        dp = Dep(nc, top)

        def PE(deps, fn, sig=True): return dp.op('pe', deps, fn, sig)
        def ACT(deps, fn, sig=True): return dp.op('act', deps, fn, sig)
        def DVE(deps, fn, sig=True): return dp.op('dve', deps, fn, sig)
        def POOL(deps, fn, sig=True): return dp.op('pool', deps, fn, sig)
        def DMA(name, deps, fn, q='sp'): return dp.dma(q, name, deps, fn)

        def mmg(out, pairs, deps, sig=True):
            n = len(pairs)
            tok = None
            for i, (l, r) in enumerate(pairs):
                tok = PE(deps if i == 0 else None,
                         lambda l=l, r=r, i=i: nc.tensor.matmul(out, lhsT=l, rhs=r, start=(i == 0), stop=(i == n - 1)),
                         sig=(sig and i == n - 1))
            return tok

        def rstd_chain(ss_ap, out_ap, inv_n, deps):
            t = ACT(deps, lambda: nc.scalar.activation(out=out_ap, in_=ss_ap, func=AF.Sqrt, bias=EPS, scale=inv_n))
            return DVE(t, lambda: nc.vector.reciprocal(out=out_ap, in_=out_ap))

        wcast = []
        for e in range(NE):
            wcast.append(DMA('wcast', None, lambda e=e: nc.gpsimd.dma_start(
                out=wgu_r[e * 128:(e + 1) * 128, :].rearrange("p (k c) -> p k c", k=8)[:, :, 0:256],
                in_=w_gate[e].rearrange("(k p) c -> p k c", p=128)), q='pool'))
            wcast.append(DMA('wcast', None, lambda e=e: nc.gpsimd.dma_start(
                out=wgu_r[e * 128:(e + 1) * 128, :].rearrange("p (k c) -> p k c", k=8)[:, :, 256:512],
                in_=w_up[e].rearrange("(k p) c -> p k c", p=128)), q='pool'))
            wcast.append(DMA('wcast', None, lambda e=e: nc.gpsimd.dma_start(
                out=wd_r[e * 128:(e + 1) * 128, :].rearrange("p (j c) -> p j c", j=2),
                in_=w_down[e].rearrange("(j p) c -> p j c", p=128)), q='pool'))
        wcast_tok = wcast[-1]
        zero_tok = []
        nz = (NSL * 128 * D) // (128 * 8192)
        xs_flat = xs_d.rearrange("(n p r) c -> n p (r c)", p=128, r=8)
        for zi in range(nz):
            zero_tok.append(DMA('zero', None, lambda zi=zi: nc.gpsimd.dma_start(out=xs_flat[zi], in_=zeros_in), q='pool'))

        ident_f = top.enter_context(nc.sbuf_tensor(U("ident_f"), [128, 128], F32))
        ident_b = top.enter_context(nc.sbuf_tensor(U("ident_b"), [128, 128], BF16))
        ones_f = top.enter_context(nc.sbuf_tensor(U("ones_f"), [128, 64], F32))
        t_id = DMA('c0', None, lambda: nc.sync.dma_start(out=ident_f[:], in_=ident_in))
        t_idb = DVE(t_id, lambda: nc.vector.tensor_copy(out=ident_b[:], in_=ident_f[:]))
        t_ones = DVE(None, lambda: nc.vector.memset(ones_f[:], 1.0))

        mk0 = dp.mark()
        with ExitStack() as pes:
            def sb(name, shape, dt): return pes.enter_context(nc.sbuf_tensor(U(name), shape, dt))
            def ps(name, shape, dt): return pes.enter_context(nc.psum_tensor(U(name), shape, dt))
            csT = sb("csT", [128, 8, NSEQ], F32)
            csS = sb("csS", [128, 8, NSEQ], F32)
            wblk = [sb("wblk%d" % i, [128, 8, 512], F32) for i in range(2)]
            brep = sb("brep", [NSEQ, 6 * D], F32)
            modsb = sb("modsb", [NSEQ, 6 * D], F32)
            pmod = [ps("pmod%d" % i, [128, 512], F32) for i in range(2)]
            t_c = [DMA('p0', None, lambda q=q: nc.sync.dma_start(out=csT[:, :, q], in_=cvec[q].rearrange("(k p) -> p k", p=128),
                                                                 allow_slow_non_contiguous=True)) for q in range(NSEQ)]
            t_b = DMA('p1', None, lambda: nc.sync.dma_start(out=brep[:], in_=b_ada.partition_broadcast(NSEQ)))
            t_cs = ACT(t_c, lambda: nc.scalar.activation(out=csS[:], in_=csT[:], func=AF.Silu))
            wfree = [None, None]
            pfree = [None, None]
            ev = None
            for blk in range(12):
                i = blk % 2
                t_w = DMA('pw%d' % i, wfree[i], lambda blk=blk, i=i: nc.sync.dma_start(
                    out=wblk[i][:], in_=w_ada[:, blk * 512:(blk + 1) * 512].rearrange("(k p) c -> p k c", p=128)))
                t_m = mmg(pmod[i][0:NSEQ, :], [(csS[:, k, :], wblk[i][:, k, :]) for k in range(8)], [t_w, t_cs, pfree[i]])
                wfree[i] = t_m
                ev = DVE([t_m, t_b], lambda blk=blk, i=i: nc.vector.tensor_tensor(
                    out=modsb[:, blk * 512:(blk + 1) * 512], in0=pmod[i][0:NSEQ, :],
                    in1=brep[:, blk * 512:(blk + 1) * 512], op=ALU.add))
                pfree[i] = ev
            t_mod = DMA('p2', ev, lambda: nc.sync.dma_start(out=mod_d, in_=modsb[:]))

            tmpq = sb("tmpq", [128, 2, 768], F32)
            gq = sb("gq", [128, 2], F32)
            wq_t = sb("wq_t", [128, 2, 768], BF16)
            wqsw_t = sb("wqsw_t", [128, 2, 768], BF16)
            t1 = DMA('p3', None, lambda: nc.sync.dma_start(out=tmpq[:], in_=w_uq.rearrange("(k p) c -> p k c", p=128)))
            t2 = DMA('p3', None, lambda: nc.sync.dma_start(out=gq[:], in_=g_q.rearrange("(k p) -> p k", p=128),
                                                          allow_slow_non_contiguous=True))
            tq = None
            for k in range(2):
                tq = DVE([t1, t2], lambda k=k: nc.vector.tensor_scalar(
                    out=wq_t[:, k, :], in0=tmpq[:, k, :], scalar1=gq[:, k:k + 1], scalar2=QSCALE,
                    op0=ALU.mult, op1=ALU.mult))
            tz = POOL(None, lambda: nc.gpsimd.memset(wqsw_t[:], 0.0))
            wq4 = wq_t[:].rearrange("p k (h c) -> p k h c", h=8)
            wqs4 = wqsw_t[:].rearrange("p k (h c) -> p k h c", h=8)
            ta = DVE([tq, tz], lambda: nc.vector.tensor_scalar(out=wqs4[:, :, :, 64:80], in0=wq4[:, :, :, 80:96],
                                                              scalar1=-1.0, scalar2=None, op0=ALU.mult))
            tb = DVE(None, lambda: nc.vector.tensor_copy(out=wqs4[:, :, :, 80:96], in_=wq4[:, :, :, 64:80]))
            t_wq = DMA('p4', tq, lambda: nc.sync.dma_start(out=wq_d, in_=wq_t[:].rearrange("p k c -> p (k c)")))
            t_wqsw = DMA('p4', [ta, tb], lambda: nc.sync.dma_start(out=wqsw_d, in_=wqsw_t[:].rearrange("p k c -> p (k c)")))

            tmpkv = sb("tmpkv", [128, 1024], F32)
            gkv = sb("gkv", [128, 1], F32)
            wkv_t = sb("wkv_t", [128, 1024], BF16)
            t1 = DMA('p5', None, lambda: nc.sync.dma_start(out=tmpkv[:], in_=w_ukv))
            t2 = DMA('p5', None, lambda: nc.sync.dma_start(out=gkv[:], in_=g_kv.rearrange("(p o) -> p o", o=1)))
            tk = DVE([t1, t2], lambda: nc.vector.tensor_scalar(out=wkv_t[:], in0=tmpkv[:], scalar1=gkv[:, 0:1],
                                                              scalar2=None, op0=ALU.mult))
            t_wkv = DMA('p6', tk, lambda: nc.sync.dma_start(out=wkv_d, in_=wkv_t[:]))

            tmpo = sb("tmpo", [128, 8, 1024], F32)
            gcat = sb("gcat", [128, 8], F32)
            wo_t = sb("wo_t", [128, 8, 1024], BF16)
            t1 = DMA('p7', None, lambda: nc.sync.dma_start(out=tmpo[:], in_=w_out.rearrange("(k p) c -> p k c", p=128)))
            t2 = DMA('p7', None, lambda: nc.sync.dma_start(out=gcat[:, 0:4], in_=g_attn_out.rearrange("(k p) -> p k", p=128),
                                                          allow_slow_non_contiguous=True))
            t3 = DMA('p7', None, lambda: nc.sync.dma_start(out=gcat[:, 4:8], in_=g_gmlp_out.rearrange("(k p) -> p k", p=128),
                                                          allow_slow_non_contiguous=True))
            two = None
            for k in range(8):
                two = DVE([t1, t2, t3], lambda k=k: nc.vector.tensor_scalar(
                    out=wo_t[:, k, :], in0=tmpo[:, k, :], scalar1=gcat[:, k:k + 1], scalar2=None, op0=ALU.mult))
            t_wo = DMA('p8', two, lambda: nc.sync.dma_start(out=wo_d, in_=wo_t[:].rearrange("p k c -> p (k c)")))

            tmps = sb("tmps", [128, 8, 128], F32)
            wsp_t = sb("wsp_t", [128, 8, 128], BF16)
            psp = ps("psp", [128, 1024], F32)
            t1 = DMA('p9', None, lambda: nc.sync.dma_start(out=tmps[:], in_=w_spatial.rearrange("g t s -> t g s")))
            tt = None
            for g in range(8):
                tt = PE([t1, t_id], lambda g=g: nc.tensor.transpose(psp[:, g * 128:(g + 1) * 128], tmps[:, g, :], ident_f[:]),
                        sig=(g == 7))
            tc_ = DVE(tt, lambda: nc.vector.tensor_copy(out=wsp_t[:].rearrange("p g t -> p (g t)"), in_=psp[:]))
            t_wsp = DMA('p10', tc_, lambda: nc.sync.dma_start(out=wsp_d, in_=wsp_t[:].rearrange("p g t -> p (g t)")))
            prep_done = [t_mod, t_wq, t_wqsw, t_wkv, t_wo, t_wsp]
            prep_bar = dp.last()
            dp.retire_since(mk0, keep=('wcast', 'zero', 'c0'))

        with ExitStack() as aes:
            def sbA(name, shape, dt): return aes.enter_context(nc.sbuf_tensor(U(name), shape, dt))
            KT = sbA("KT", [128, 8, S], BF16)
            VA = sbA("VA", [128, KB, 8, 65], BF16)
            geff1 = sbA("geff1", [128, D], F32)
            sh1r = sbA("sh1r", [128, D], F32)
            gvec1 = sbA("gvec1", [128, D], F32)
            gvrep = sbA("gvrep", [128, 512], F32)
            PA = aes.enter_context(nc.psum_tensor(U("PA"), [128, 1024], F32))
            PB = aes.enter_context(nc.psum_tensor(U("PB"), [128, 1024], F32))
            PC = aes.enter_context(nc.psum_tensor(U("PC"), [128, 1024], F32))
            PD = aes.enter_context(nc.psum_tensor(U("PD"), [128, 1024], F32))

            t_va1 = POOL(prep_bar, lambda: nc.gpsimd.memset(VA[:, :, :, 64:65], 1.0))
            t_gv = DMA('a0', prep_bar, lambda: nc.sync.dma_start(out=gvrep[:], in_=g_v_gmlp.partition_broadcast(128)))
            seq_bar = [prep_bar, prep_done, t_va1, t_gv, t_idb, t_ones]
            qbase = 0
            for s in range(NSEQ):
                mk1 = dp.mark()
                with ExitStack() as p1:
                    def sb1(name, shape, dt): return p1.enter_context(nc.sbuf_tensor(U(name), shape, dt))
                    wAs = sb1("wAs", [128, 8, 384], BF16)
                    wAuv = sb1("wAuv", [128, 8, 1024], BF16)
                    wAkr = sb1("wAkr", [128, 8, 96], BF16)
                    wAks = sb1("wAks", [128, 8, 96], BF16)
                    wkv = sb1("wkv", [128, 1024], BF16)
                    wsp = sb1("wsp", [128, 8, 128], BF16)
                    bsp = sb1("bsp", [8, 128], F32)
                    egrp = sb1("egrp", [8, 512], F32)
                    vt = [sb1("vt%d" % i, [128, D], F32) for i in range(2)]
                    xt = [sb1("xt%d" % i, [128, D], F32) for i in range(2)]
                    junk = sb1("junk", [128, D], BF16)
                    hm = sb1("hm", [128, D], F32)
                    hb = [sb1("hb%d" % i, [128, D], BF16) for i in range(2)]
                    hT = sb1("hT", [128, 8, 512], BF16)
                    zsb = [sb1("zsb%d" % i, [128, 384], BF16) for i in range(2)]
                    cqnT = [sb1("cqnT%d" % i, [128, 2, 512], BF16) for i in range(2)]
                    ckvnT = [sb1("ckvnT%d" % i, [128, 512], BF16) for i in range(2)]
                    gu = [sb1("gu%d" % i, [128, 512], BF16) for i in range(2)]
                    gv = [sb1("gv%d" % i, [128, 512], F32) for i in range(2)]
                    zraw = [sb1("zraw%d" % i, [128, 384], F32) for i in range(2)]
                    vn = [sb1("vn%d" % i, [128, 512], BF16) for i in range(2)]
                    sraw = [sb1("sraw%d" % i, [128, 512], F32) for i in range(2)]
                    sn = [sb1("sn%d" % i, [128, 512], BF16) for i in range(2)]
                    stt = [sb1("stt%d" % i, [128, 16], F32) for i in range(2)]
                    Ctt = [sb1("Ctt%d" % i, [128, 128], F32) for i in range(2)]
                    Stt = [sb1("Stt%d" % i, [128, 128], F32) for i in range(2)]
                    kt1 = [sb1("kt1_%d" % i, [128, 128], F32) for i in range(2)]
                    kt2 = [sb1("kt2_%d" % i, [128, 128], F32) for i in range(2)]
                    krr = [sb1("krr%d" % i, [128, 128], BF16) for i in range(2)]

                    pT = PA[:, 0:512].bitcast(BF16)
                    pT2 = PA[:, 512:1024].bitcast(BF16)
                    pzs = PB[:, 0:384]
                    pss = PB[:, 512:1024]
                    pu = PC[:, 0:512]
                    pv = PC[:, 512:1024]
                    pkr = PD[:, 0:512]
                    pks = PD[:, 512:1024]

                    sb_ = seq_bar
                    wl = []
                    wl.append(DMA('a1', sb_, lambda: nc.gpsimd.dma_start(
                        out=wAs[:], in_=w_in[:, 0:384].rearrange("(k p) c -> p k c", p=128)), q='pool'))
                    wl.append(DMA('a1', sb_, lambda: nc.gpsimd.dma_start(
                        out=wAuv[:], in_=w_in[:, 416:1440].rearrange("(k p) c -> p k c", p=128)), q='pool'))
                    tz1 = POOL(sb_, lambda: nc.gpsimd.memset(wAkr[:], 0.0))
                    tz2 = POOL(sb_, lambda: nc.gpsimd.memset(wAks[:], 0.0))
                    wl.append(DMA('a1', [tz1], lambda: nc.gpsimd.dma_start(
                        out=wAkr[:, :, 64:96], in_=w_in[:, 384:416].rearrange("(k p) c -> p k c", p=128)), q='pool'))
                    tn = DMA('a2', [tz2], lambda: nc.gpsimd.dma_start(
                        out=wAks[:, :, 64:80], in_=w_in[:, 400:416].rearrange("(k p) c -> p k c", p=128)), q='pool')
                    wl.append(DMA('a1', [tz2], lambda: nc.gpsimd.dma_start(
                        out=wAks[:, :, 80:96], in_=w_in[:, 384:400].rearrange("(k p) c -> p k c", p=128)), q='pool'))
                    wl.append(POOL(tn, lambda: nc.gpsimd.tensor_scalar(out=wAks[:, :, 64:80], in0=wAks[:, :, 64:80],
                                                                      scalar1=-1.0, scalar2=None, op0=ALU.mult)))
                    wl.append(DMA('a3', sb_, lambda: nc.sync.dma_start(out=wkv[:], in_=wkv_d)))
                    wl.append(DMA('a3', sb_, lambda: nc.sync.dma_start(out=wsp[:].rearrange("p g t -> p (g t)"), in_=wsp_d)))
                    wl.append(DMA('a3', sb_, lambda: nc.sync.dma_start(out=bsp[:], in_=b_spatial)))
                    wl.append(DMA('a3', sb_, lambda: nc.sync.dma_start(out=egrp[:], in_=egrp_in)))
                    l1 = DMA('a4_0', sb_, lambda: nc.sync.dma_start(out=vt[0][:], in_=mod_d[s, 1024:2048].partition_broadcast(128)))
                    l2 = DMA('a4_1', sb_, lambda: nc.sync.dma_start(out=vt[1][:], in_=g_pre1.partition_broadcast(128)))
                    l3 = DMA('a4_2', sb_, lambda: nc.sync.dma_start(out=sh1r[:], in_=mod_d[s, 0:1024].partition_broadcast(128)))
                    tg = DVE([l1, l2], lambda: nc.vector.scalar_tensor_tensor(out=geff1[:], in0=vt[0][:], scalar=1.0, in1=vt[1][:],
                                                                               op0=ALU.add, op1=ALU.mult))
                    l4 = DMA('a4_3', [tg], lambda: nc.sync.dma_start(out=vt[0][:], in_=mod_d[s, 2048:3072].partition_broadcast(128)))
                    l5 = DMA('a4_4', [tg], lambda: nc.sync.dma_start(out=vt[1][:], in_=g_post1.partition_broadcast(128)))
                    tg2 = DVE([l4, l5], lambda: nc.vector.tensor_tensor(out=gvec1[:], in0=vt[0][:], in1=vt[1][:], op=ALU.mult))
                    ready = [wl, l3, tg, tg2]

                    xt_free = [None, None]; hb_free = [None, None]
                    hT_free = [None] * 4
                    zraw_free = [None, None]; zsb_free = [None, None]; cq_free = [None, None]; ckv_free = [None, None]
                    gu_free = [None, None]; gv_free = [None, None]; vn_free = [None, None]; sn_free = [None, None]
                    sraw_free = [None, None]; ct_free = [None, None]; kt_free = [None, None]; krr_free = [None, None]
                    P = dict(hm_free=None, pT_free=None, pT2_free=None, pzs_free=None, pu_free=None, pv_free=None, pss_free=None,
                             pkr_free=None, pks_free=None)
                    grp = {}

                    def p1_tile(g, t):
                        gi = g % 2
                        ti = t % 2
                        if t == 0:
                            grp[g] = dict(cq_w=[], ckv_w=[])
                        G = grp[g]
                        tok0 = s * S + g * 512 + t * 128
                        ts_ = slice(t * 128, (t + 1) * 128)
                        gts = slice(g * 512 + t * 128, g * 512 + (t + 1) * 128)
                        st_ = stt[ti]
                        lx = DMA('x%d' % ti, [xt_free[ti], ready], lambda: nc.sync.dma_start(out=xt[ti][:], in_=xs[tok0:tok0 + 128, :]))
                        lc = DMA('rc%d' % ti, [ct_free[ti], ready], lambda: nc.sync.dma_start(out=Ctt[ti][64:96, :], in_=rope_c[s, :, gts]))
                        ls = DMA('rs%d' % ti, [ct_free[ti], ready], lambda: nc.sync.dma_start(out=Stt[ti][64:96, :], in_=rope_s[s, :, gts]))
                        a1 = ACT(lx, lambda: nc.scalar.activation(out=junk[:], in_=xt[ti][:], func=AF.Square, accum_out=st_[:, 0:1]))
                        yield
                        r1a = ACT(a1, lambda: nc.scalar.activation(out=st_[:, 1:2], in_=st_[:, 0:1], func=AF.Sqrt, bias=EPS, scale=1.0 / D))
                        yield
                        r1 = DVE(r1a, lambda: nc.vector.reciprocal(out=st_[:, 1:2], in_=st_[:, 1:2]))
                        d1 = DVE([r1, P['hm_free']], lambda: nc.vector.scalar_tensor_tensor(
                            out=hm[:], in0=xt[ti][:], scalar=st_[:, 1:2], in1=geff1[:], op0=ALU.mult, op1=ALU.mult))
                        xt_free[ti] = d1
                        p1_ = POOL([d1, hb_free[ti]], lambda: nc.gpsimd.tensor_tensor(out=hb[ti][:], in0=hm[:], in1=sh1r[:], op=ALU.add))
                        P['hm_free'] = p1_
                        yield
                        tp = None
                        for k in range(8):
                            tp = PE([p1_, P['pT_free']] if k == 0 else None,
                                    lambda k=k: nc.tensor.transpose(pT[:, k * 128:(k + 1) * 128], hb[ti][:, k * 128:(k + 1) * 128], ident_b[:]),
                                    sig=(k == 7))
                        hb_free[ti] = tp
                        yield
                        ev = ACT([tp, hT_free[t]], lambda: nc.scalar.copy(out=hT[:, :, ts_], in_=pT.rearrange("p (k c) -> p k c", k=8)))
                        P['pT_free'] = ev
                        yield
                        m_zs = mmg(pzs, [(hT[:, k, ts_], wAs[:, k, :]) for k in range(8)], [ev, P['pzs_free']])
                        m_u = mmg(pu, [(hT[:, k, ts_], wAuv[:, k, 0:512]) for k in range(8)], [P['pu_free']])
                        m_v = mmg(pv, [(hT[:, k, ts_], wAuv[:, k, 512:1024]) for k in range(8)], [P['pv_free']])
                        m_kr = mmg(pkr[0:96, 0:128], [(wAkr[:, k, :], hT[:, k, ts_]) for k in range(8)], [P['pkr_free']])
                        m_ks = mmg(pks[0:96, 0:128], [(wAks[:, k, :], hT[:, k, ts_]) for k in range(8)], [P['pks_free']])
                        hT_free[t] = m_ks
                        yield
                        zr = DVE([m_zs, zraw_free[ti]], lambda: nc.vector.tensor_copy(out=zraw[ti][:], in_=pzs))
                        P['pzs_free'] = zr
                        g1 = ACT([m_u, gu_free[ti]], lambda: nc.scalar.activation(out=gu[ti][:], in_=pu, func=AF.Gelu_apprx_tanh))
                        P['pu_free'] = g1
                        g2 = ACT([m_v, gv_free[ti]], lambda: nc.scalar.activation(out=gv[ti][:], in_=pv, func=AF.Gelu_apprx_tanh))
                        P['pv_free'] = g2
                        k1 = DVE([m_kr, lc, kt_free[ti]], lambda: nc.vector.tensor_tensor(out=kt1[ti][64:96, :], in0=pkr[64:96, 0:128], in1=Ctt[ti][64:96, :], op=ALU.mult))
                        P['pkr_free'] = k1
                        k2 = DVE([m_ks, ls], lambda: nc.vector.tensor_tensor(out=kt2[ti][64:96, :], in0=pks[64:96, 0:128], in1=Stt[ti][64:96, :], op=ALU.mult))
                        P['pks_free'] = k2
                        ct_free[ti] = k2
                        yield
                        a2 = ACT(zr, lambda: nc.scalar.activation(out=junk[:, 0:256], in_=zraw[ti][:, 0:256], func=AF.Square, accum_out=st_[:, 2:3]))
                        a3 = ACT(None, lambda: nc.scalar.activation(out=junk[:, 256:384], in_=zraw[ti][:, 256:384], func=AF.Square, accum_out=st_[:, 3:4]))
                        g3 = ACT(g2, lambda: nc.scalar.activation(out=junk[:, 0:512], in_=gv[ti][:], func=AF.Square, accum_out=st_[:, 6:7]))
                        k3 = DVE([k1, k2, krr_free[ti]], lambda: nc.vector.tensor_tensor(out=krr[ti][64:96, :], in0=kt1[ti][64:96, :], in1=kt2[ti][64:96, :], op=ALU.add))
                        kt_free[ti] = k3
                        kc = None
                        for h in range(8):
                            kc = POOL(k3, lambda h=h: nc.gpsimd.tensor_copy(out=KT[64:96, h, gts], in_=krr[ti][64:96, :]))
                        krr_free[ti] = kc
                        yield
                        q1 = ACT([a2, a3], lambda: nc.scalar.activation(out=st_[:, 4:5], in_=st_[:, 2:3], func=AF.Sqrt, bias=EPS, scale=1.0 / 256))
                        q2 = ACT(None, lambda: nc.scalar.activation(out=st_[:, 5:6], in_=st_[:, 3:4], func=AF.Sqrt, bias=EPS, scale=1.0 / 128))
                        q3 = ACT(g3, lambda: nc.scalar.activation(out=st_[:, 7:8], in_=st_[:, 6:7], func=AF.Sqrt, bias=EPS, scale=1.0 / 512))
                        yield
                        r2 = DVE([q1, q2], lambda: nc.vector.reciprocal(out=st_[:, 4:6], in_=st_[:, 4:6]))
                        r4 = DVE(q3, lambda: nc.vector.reciprocal(out=st_[:, 7:8], in_=st_[:, 7:8]))
                        d2 = DVE([r4, vn_free[ti]], lambda: nc.vector.scalar_tensor_tensor(
                            out=vn[ti][:], in0=gv[ti][:], scalar=st_[:, 7:8], in1=gvrep[:], op0=ALU.mult, op1=ALU.mult))
                        gv_free[ti] = d2
                        c1 = ACT([r2, zsb_free[ti]], lambda: nc.scalar.activation(
                            out=zsb[ti][:, 0:256], in_=zraw[ti][:, 0:256], func=AF.Copy, scale=st_[:, 4:5]))
                        c2 = ACT(None, lambda: nc.scalar.activation(
                            out=zsb[ti][:, 256:384], in_=zraw[ti][:, 256:384], func=AF.Copy, scale=st_[:, 5:6]))
                        zraw_free[ti] = c2
                        yield
                        tp2 = None
                        for k in range(3):
                            tp2 = PE([c1, c2, P['pT2_free']] if k == 0 else None,
                                     lambda k=k: nc.tensor.transpose(pT2[:, k * 128:(k + 1) * 128], zsb[ti][:, k * 128:(k + 1) * 128], ident_b[:]),
                                     sig=(k == 2))
                        zsb_free[ti] = tp2
                        PE([P['pss_free'], ready], lambda: nc.tensor.matmul(pss, lhsT=bsp[:, :], rhs=egrp[:, :], start=True, stop=False), sig=False)
                        m_s = None
                        for gg in range(8):
                            m_s = PE(d2 if gg == 0 else None,
                                     lambda gg=gg: nc.tensor.matmul(pss[:, gg * 64:(gg + 1) * 64], lhsT=wsp[:, gg, :],
                                                                    rhs=vn[ti][:, gg * 64:(gg + 1) * 64], start=False, stop=(gg == 7)),
                                     sig=(gg == 7))
                        vn_free[ti] = m_s
                        yield
                        e1 = DVE([tp2, cq_free[gi] if t == 0 else None], lambda: nc.vector.tensor_copy(
                            out=cqnT[gi][:, :, ts_], in_=pT2[:, 0:256].rearrange("p (k c) -> p k c", k=2)))
                        e2 = DVE([ckv_free[gi] if t == 0 else None], lambda: nc.vector.tensor_copy(
                            out=ckvnT[gi][:, ts_], in_=pT2[:, 256:384]))
                        P['pT2_free'] = e2
                        G['cq_w'].append(e1)
                        G['ckv_w'].append(e2)
                        d3 = DVE([m_s, g1, sraw_free[ti]], lambda: nc.vector.tensor_tensor(out=sraw[ti][:], in0=gu[ti][:], in1=pss, op=ALU.mult))
                        P['pss_free'] = d3
                        gu_free[ti] = d3
                        yield
                        a4 = ACT(d3, lambda: nc.scalar.activation(out=junk[:, 0:512], in_=sraw[ti][:], func=AF.Square, accum_out=st_[:, 8:9]))
                        yield
                        q4 = ACT(a4, lambda: nc.scalar.activation(out=st_[:, 9:10], in_=st_[:, 8:9], func=AF.Sqrt, bias=EPS, scale=1.0 / 512))
                        yield
                        r5 = DVE(q4, lambda: nc.vector.reciprocal(out=st_[:, 9:10], in_=st_[:, 9:10]))
                        yield
                        c3 = ACT([r5, sn_free[ti]], lambda: nc.scalar.activation(out=sn[ti][:], in_=sraw[ti][:], func=AF.Copy, scale=st_[:, 9:10]))
                        sraw_free[ti] = c3
                        sn_free[ti] = DMA('sn%d' % ti, c3, lambda: nc.sync.dma_start(out=sn_d[tok0:tok0 + 128, :], in_=sn[ti][:]))
                        if t != 3:
                            return
                        yield
                        gs = slice(g * 512, (g + 1) * 512)
                        cq_free[gi] = DMA('cq%d' % gi, G['cq_w'], lambda: nc.sync.dma_start(
                            out=cq_d[s * NG + g], in_=cqnT[gi][:].rearrange("p k c -> p (k c)")))
                        bank_free = [P['pkr_free'], P['pks_free']]
                        banks = [pkr, pks]
                        for h in range(8):
                            bi = h % 2
                            mk = mmg(banks[bi][0:64, :], [(wkv[:, h * 128:h * 128 + 64], ckvnT[gi][:, :])], [G['ckv_w'], bank_free[bi]])
                            if h % 2 == 0:
                                bank_free[bi] = ACT(mk, lambda h=h, bi=bi: nc.scalar.copy(out=KT[0:64, h, gs], in_=banks[bi][0:64, :]))
                            else:
                                bank_free[bi] = DVE(mk, lambda h=h, bi=bi: nc.vector.tensor_copy(out=KT[0:64, h, gs], in_=banks[bi][0:64, :]))
                        wkv3 = wkv[:].rearrange("p (h c) -> p h c", h=8)[:, :, 64:128]
                        mv = None
                        for tt in range(4):
                            bi = tt % 2
                            kb = g * 4 + tt
                            mv = mmg(banks[bi][:, :].rearrange("p (h c) -> p h c", h=8), [(ckvnT[gi][:, tt * 128:(tt + 1) * 128], wkv3)], [bank_free[bi]])
                            bank_free[bi] = DVE(mv, lambda kb=kb, bi=bi: nc.vector.tensor_copy(
                                out=VA[:, kb, :, 0:64], in_=banks[bi][:, :].rearrange("p (h c) -> p h c", h=8)))
                        ckv_free[gi] = mv
                        P['pkr_free'] = bank_free[0]
                        P['pks_free'] = bank_free[1]

                    interleave((p1_tile(g, t) for g in range(NG) for t in range(4)), 2)
                    p1_bar = dp.last() + [sn_free, cq_free]
                    dp.retire_since(mk1)

                mk2 = dp.mark()
                with ExitStack() as p2:
                    def sb2(name, shape, dt): return p2.enter_context(nc.sbuf_tensor(U(name), shape, dt))
                    wq = sb2("wq", [128, 2, 768], BF16)
                    wqs = sb2("wqs", [128, 2, 768], BF16)
                    wo = sb2("wo", [128, 8, 1024], BF16)
                    cqT = [sb2("cqT%d" % i, [128, 2, 512], BF16) for i in range(2)]
                    Ct = sb2("Ct2", [128, 512], F32)
                    St = sb2("St2", [128, 512], F32)
                    qt1 = [sb2("qt1_%d" % i, [128, 512], F32) for i in range(2)]
                    qt2 = [sb2("qt2_%d" % i, [128, 512], F32) for i in range(2)]
                    qt_free = [None, None]
                    QT = sb2("QT", [128, 8, 512], BF16)
                    pTs = [sb2("pTs%d" % i, [128, 1024], BF16) for i in range(3)]
                    osb = [sb2("osb%d" % i, [128, 512], F32) for i in range(2)]
                    rinv = [sb2("rinv%d" % i, [128, 512], F32) for i in range(2)]
                    aT = [sb2("aT%d" % i, [128, 512], BF16) for i in range(2)]
                    merged = [sb2("merged%d" % i, [128, D], BF16) for i in range(2)]
                    mT = [sb2("mT%d" % i, [128, 8, 128], BF16) for i in range(2)]
                    otmp = sb2("otmp", [128, D], F32)
                    xr = [sb2("xr%d" % i, [128, D], F32) for i in range(2)]
                    x1 = [sb2("x1_%d" % i, [128, D], F32) for i in range(2)]
                    junk = sb2("junk2", [128, D], BF16)
                    stt = [sb2("stq%d" % i, [128, 16], F32) for i in range(2)]

                    po = PC[:, 0:512]
                    prb = PC[:, 512:1024]
                    pmT = PC[:, 512:1024].bitcast(BF16)
                    pa = PD[:, :].bitcast(BF16)
                    scT = [PA, PB]

                    wl2 = [DMA('b1', p1_bar, lambda: nc.sync.dma_start(out=wq[:].rearrange("p k c -> p (k c)"), in_=wq_d)),
                           DMA('b1', p1_bar, lambda: nc.sync.dma_start(out=wqs[:].rearrange("p k c -> p (k c)"), in_=wqsw_d)),
                           DMA('b1', p1_bar, lambda: nc.sync.dma_start(out=wo[:].rearrange("p k c -> p (k c)"), in_=wo_d))]
                    ready2 = [p1_bar, wl2]
                    cq_free2 = [None, None]; rope_free = None; QT_free = []
                    sc_free = [None, None]; pTs_free = [None, None, None]; po_free = None; osb_free = [None, None]
                    rinv_free = [None, None]; prb_free = None; aT_free = [None, None]; pa_free = []
                    merged_free = [None, None]; mT_free = [None, None]; pmT_free = None
                    otmp_free = None; xr_free = [None, None]; x1_free = [None, None]
                    step = 0
                    tcount = 0
                    for qg in range(NQG[s]):
                        gi = qg % 2
                        gs = slice(qg * 512, (qg + 1) * 512)
                        lq = DMA('cql%d' % gi, [cq_free2[gi], ready2], lambda gi=gi, qg=qg: nc.sync.dma_start(
                            out=cqT[gi][:].rearrange("p k c -> p (k c)"), in_=cq_d[s * NG + qg]))
                        lr1 = DMA('rp2c', [rope_free, ready2], lambda gs=gs: nc.sync.dma_start(out=Ct[64:96, :], in_=rope_c[s, :, gs]))
                        lr2 = DMA('rp2s', [rope_free, ready2], lambda gs=gs: nc.sync.dma_start(out=St[64:96, :], in_=rope_s[s, :, gs]))
                        wq4 = wq[:].rearrange("p k (h c) -> p k h c", h=8)
                        wqs4 = wqs[:].rearrange("p k (h c) -> p k h c", h=8)
                        QT_w = []
                        Qs = dict(mqs=None, qd=None)

                        def q_head(h):
                            T = scT[h % 2]
                            hi = h % 2
                            mq = mmg(T[0:96, 0:512], [(wq4[:, k, h, :], cqT[gi][:, k, :]) for k in range(2)], [lq, sc_free[h % 2]])
                            mqs = mmg(T[0:96, 512:1024], [(wqs4[:, k, h, :], cqT[gi][:, k, :]) for k in range(2)], None)
                            Qs['mqs'] = mqs
                            yield
                            c0 = ACT([mq, QT_free if h == 0 else None], lambda: nc.scalar.copy(out=QT[0:64, h, :], in_=T[0:64, 0:512]))
                            q1 = DVE([mq, lr1, qt_free[hi]], lambda: nc.vector.tensor_tensor(out=qt1[hi][64:96, :], in0=T[64:96, 0:512], in1=Ct[64:96, :], op=ALU.mult))
                            q2 = DVE([mqs, lr2], lambda: nc.vector.tensor_tensor(out=qt2[hi][64:96, :], in0=T[64:96, 512:1024], in1=St[64:96, :], op=ALU.mult))
                            sc_free[h % 2] = [c0, q2]
                            yield
                            qd = DVE([q1, q2, QT_free if h == 0 else None], lambda: nc.vector.tensor_tensor(
                                out=QT[64:96, h, :], in0=qt1[hi][64:96, :], in1=qt2[hi][64:96, :], op=ALU.add))
                            qt_free[hi] = qd
                            Qs['qd'] = qd
                            QT_w.extend([c0, qd])
                        interleave((q_head(h) for h in range(8)), 2)
                        mqs = Qs['mqs']
                        qd = Qs['qd']
                        cq_free2[gi] = mqs
                        rope_free = qd
                        NP = KB // 2
                        steps = [(h, j) for h in range(8) for j in range(NP)]
                        qk_tok = {}

                        def emit_qk(idx):
                            h, j = steps[idx]
                            T = scT[idx % 2]
                            tk = None
                            for u in range(2):
                                kb = 2 * j + u
                                tk = PE([sc_free[idx % 2], QT_w] if u == 0 else None,
                                        lambda h=h, kb=kb, u=u, T=T: nc.tensor.matmul(
                                            T[:, u * 512:(u + 1) * 512], lhsT=KT[0:96, h, kb * 128:(kb + 1) * 128], rhs=QT[0:96, h, :],
                                            start=True, stop=True), sig=(u == 1))
                            qk_tok[idx] = tk

                        emit_qk(0)
                        pa_w = []
                        QT_readers = []
                        pending = []
                        A = dict(prb_free=prb_free)
                        for idx, (h, j) in enumerate(steps):
                            if idx + 1 < len(steps):
                                emit_qk(idx + 1)
                            T = scT[idx % 2]
                            sl = step % 3
                            step += 1
                            ex = ACT([qk_tok[idx], pTs_free[sl]], lambda T=T, sl=sl: nc.scalar.activation(out=pTs[sl][:], in_=T[:, :], func=AF.Exp))
                            sc_free[idx % 2] = ex
                            pvt = None
                            for u in range(2):
                                kb = 2 * j + u
                                pvt = PE([ex, po_free if (j == 0 and u == 0) else None],
                                         lambda h=h, kb=kb, u=u, sl=sl: nc.tensor.matmul(
                                             po[0:65, :], lhsT=VA[:, kb, h, :], rhs=pTs[sl][:, u * 512:(u + 1) * 512],
                                             start=(kb == 0), stop=(kb == KB - 1)), sig=(u == 1))
                            pTs_free[sl] = pvt
                            for pend in list(pending):
                                pend[0] -= 1
                                if pend[0] <= 0:
                                    pend[1]()
                                    pending.remove(pend)
                            if j == NP - 1:
                                for pend in list(pending):
                                    pend[1]()
                                    pending.remove(pend)
                                oi = h % 2
                                QT_readers.append(pvt)
                                o1 = DVE([pvt, osb_free[oi]], lambda oi=oi: nc.vector.tensor_copy(out=osb[oi][0:65, :], in_=po[0:65, :]))
                                po_free = o1
                                o2 = DVE([o1, rinv_free[oi]], lambda oi=oi: nc.vector.reciprocal(out=rinv[oi][64:65, :], in_=osb[oi][64:65, :]))
                                hs = dict(o2=o2, oi=oi, h=h)

                                def part_a(hs=hs):
                                    oi = hs['oi']
                                    o3 = PE([hs['o2'], A['prb_free']], lambda oi=oi: nc.tensor.matmul(prb[0:64, :], lhsT=ones_f[64:65, 0:64], rhs=rinv[oi][64:65, :],
                                                                                                 start=True, stop=True))
                                    rinv_free[oi] = o3
                                    o4 = DVE([o3, aT_free[oi]], lambda oi=oi: nc.vector.tensor_tensor(out=aT[oi][0:64, :], in0=osb[oi][0:64, :],
                                                                                                       in1=prb[0:64, :], op=ALU.mult))
                                    A['prb_free'] = o4
                                    osb_free[oi] = o4
                                    hs['o4'] = o4

                                def part_b(hs=hs):
                                    oi = hs['oi']; h = hs['h']
                                    o5 = None
                                    for t in range(4):
                                        o5 = PE([hs['o4'], pa_free if h == 0 else None] if t == 0 else None,
                                                lambda t=t, h=h, oi=oi: nc.tensor.transpose(
                                                    pa[:, t * 512 + h * 64: t * 512 + (h + 1) * 64], aT[oi][0:64, t * 128:(t + 1) * 128], ident_b[0:64, 0:64]),
                                                sig=(t == 3))
                                    aT_free[oi] = o5
                                    pa_w.append(o5)
                                pending.append([2, part_a])
                                pending.append([4, part_b])
                        for pend in list(pending):
                            pend[1]()
                            pending.remove(pend)
                        prb_free = A['prb_free']
                        QT_free = QT_readers
                        pa_r = []
                        M = dict(pmT_free=pmT_free, prb_free=prb_free, otmp_free=otmp_free)

                        def mg_tile(t, ti):
                            tok0 = s * S + qg * 512 + t * 128
                            otok0 = qbase + qg * 512 + t * 128
                            st_ = stt[ti]
                            lsn = DMA('snl%d' % ti, [merged_free[ti], ready2], lambda: nc.sync.dma_start(
                                out=merged[ti][:, 512:1024], in_=sn_d[tok0:tok0 + 128, :]))
                            lxr = DMA('xr%d' % ti, [xr_free[ti], ready2], lambda: nc.sync.dma_start(
                                out=xr[ti][:], in_=xs[tok0:tok0 + 128, :]))
                            a1 = ACT(pa_w, lambda: nc.scalar.activation(out=junk[:, 0:512], in_=pa[:, t * 512:(t + 1) * 512], func=AF.Square,
                                                                        accum_out=st_[:, 0:1]))
                            yield
                            r1a = ACT(a1, lambda: nc.scalar.activation(out=st_[:, 1:2], in_=st_[:, 0:1], func=AF.Sqrt, bias=EPS, scale=1.0 / 512))
                            yield
                            r1 = DVE(r1a, lambda: nc.vector.reciprocal(out=st_[:, 1:2], in_=st_[:, 1:2]))
                            c1 = ACT([r1, merged_free[ti]], lambda: nc.scalar.activation(
                                out=merged[ti][:, 0:512], in_=pa[:, t * 512:(t + 1) * 512], func=AF.Copy, scale=st_[:, 1:2]))
                            pa_r.append(c1)
                            yield
                            tp = None
                            for k in range(8):
                                tp = PE([c1, lsn, M['pmT_free'], M['prb_free']] if k == 0 else None,
                                        lambda k=k: nc.tensor.transpose(pmT[:, k * 128:(k + 1) * 128], merged[ti][:, k * 128:(k + 1) * 128], ident_b[:]),
                                        sig=(k == 7))
                            merged_free[ti] = tp
                            yield
                            ev = DVE([tp, mT_free[ti]], lambda: nc.vector.tensor_copy(out=mT[ti][:].rearrange("p k c -> p (k c)"), in_=pmT))
                            M['pmT_free'] = ev
                            M['prb_free'] = ev
                            yield
                            T = scT[t % 2]
                            mo1 = mmg(T[:, 0:512], [(mT[ti][:, k, :], wo[:, k, 0:512]) for k in range(8)], [ev, sc_free[t % 2]])
                            mo2 = mmg(T[:, 512:1024], [(mT[ti][:, k, :], wo[:, k, 512:1024]) for k in range(8)], None)
                            mT_free[ti] = mo2
                            yield
                            a2 = ACT(mo2, lambda: nc.scalar.activation(out=junk[:], in_=T[:, :], func=AF.Square, accum_out=st_[:, 2:3]))
                            yield
                            r2a = ACT(a2, lambda: nc.scalar.activation(out=st_[:, 3:4], in_=st_[:, 2:3], func=AF.Sqrt, bias=EPS, scale=1.0 / D))
                            yield
                            r2 = DVE(r2a, lambda: nc.vector.reciprocal(out=st_[:, 3:4], in_=st_[:, 3:4]))
                            d1 = DVE([r2, M['otmp_free']], lambda: nc.vector.scalar_tensor_tensor(
                                out=otmp[:], in0=T[:, :], scalar=st_[:, 3:4], in1=gvec1[:], op0=ALU.mult, op1=ALU.mult))
                            sc_free[t % 2] = d1
                            pp = POOL([d1, lxr, x1_free[ti]], lambda: nc.gpsimd.tensor_tensor(out=x1[ti][:], in0=otmp[:], in1=xr[ti][:], op=ALU.add))
                            M['otmp_free'] = pp
                            xr_free[ti] = pp
                            x1_free[ti] = DMA('x1s%d' % ti, pp, lambda: nc.sync.dma_start(
                                out=x1_d[otok0:otok0 + 128, :], in_=x1[ti][:]))
                        interleave((mg_tile(t, (tcount + t) % 2) for t in range(4)), 2)
                        tcount += 4
                        pmT_free = M['pmT_free']; prb_free = M['prb_free']; otmp_free = M['otmp_free']
                        pa_free = pa_r
                    p2_bar = dp.last() + [x1_free]
                    dp.retire_since(mk2)
                seq_bar = p2_bar
                qbase += NQG[s] * 512
            stageA_bar = seq_bar

        NTT = NQT // 128
        gseq = []
        for s in range(NSEQ):
            gseq += [s] * (NQG[s] * 4)
        with ExitStack() as bes:
            def sbB(name, shape, dt): return bes.enter_context(nc.sbuf_tensor(U(name), shape, dt))
            geff2_ = [sbB("geff2_%d" % i, [128, D], F32) for i in range(2)]
            sh2r_ = [sbB("sh2r_%d" % i, [128, D], F32) for i in range(2)]
            gvec2_ = [sbB("gvec2_%d" % i, [128, D], F32) for i in range(2)]
            vt = [sbB("vtB%d" % i, [128, D], F32) for i in range(2)]
            M1a = sbB("M1a", [128, NTT, 32], F32)
            M2a = sbB("M2a", [128, NTT, 32], F32)
            W1a = sbB("W1a", [128, NTT], F32)
            W2a = sbB("W2a", [128, NTT], F32)
            R1a = sbB("R1a", [128, NTT], F32)
            R2a = sbB("R2a", [128, NTT], F32)
            slot0 = sbB("slot0", [128, NTT], I32)
            slot1 = sbB("slot1", [128, NTT], I32)
            idxw = sbB("idxw", [128, NSL], I32)
            carry = sbB("carry", [128, 32], F32)
            PS = [bes.enter_context(nc.psum_tensor(U("PS%d" % i), [128, 512], F32)) for i in range(8)]
            bb = stageA_bar
            readyB = [bb, wcast_tok, zero_tok]

            def load_vecs(s, deps):
                geff2 = geff2_[s % 2]; sh2r = sh2r_[s % 2]; gvec2 = gvec2_[s % 2]
                l1 = DMA('v0_0', deps, lambda s=s: nc.sync.dma_start(out=vt[0][:], in_=mod_d[s, 4096:5120].partition_broadcast(128)))
                l2 = DMA('v0_1', deps, lambda: nc.sync.dma_start(out=vt[1][:], in_=g_pre2.partition_broadcast(128)))
                l3 = DMA('v0_2', deps, lambda s=s: nc.sync.dma_start(out=sh2r[:], in_=mod_d[s, 3072:4096].partition_broadcast(128)))
                tg = DVE([l1, l2], lambda: nc.vector.scalar_tensor_tensor(out=geff2[:], in0=vt[0][:], scalar=1.0, in1=vt[1][:],
                                                                           op0=ALU.add, op1=ALU.mult))
                l4 = DMA('v0_3', [tg], lambda s=s: nc.sync.dma_start(out=vt[0][:], in_=mod_d[s, 5120:6144].partition_broadcast(128)))
                l5 = DMA('v0_4', [tg], lambda: nc.sync.dma_start(out=vt[1][:], in_=g_post2.partition_broadcast(128)))
                tg2 = DVE([l4, l5], lambda: nc.vector.tensor_tensor(out=gvec2[:], in0=vt[0][:], in1=vt[1][:], op=ALU.mult))
                return [l3, tg, tg2]

            mkb1 = dp.mark()
            with ExitStack() as b1:
                def sb1(name, shape, dt): return b1.enter_context(nc.sbuf_tensor(U(name), shape, dt))
                w_r = sb1("w_r", [128, 8, 36], F32)
                brr = sb1("brr", [128, 36], F32)
                utri = sb1("utri", [128, 128], BF16)
                onesb = sb1("onesb", [128, 128], BF16)
                x1t = [sb1("x1t%d" % i, [128, D], F32) for i in range(5)]
                junk = sb1("junkB", [128, D], BF16)
                hm = sb1("hmB", [128, D], F32)
                h2 = [sb1("h2_%d" % i, [128, D], F32) for i in range(5)]
                h2Tf = [sb1("h2Tf%d" % i, [128, 8, 128], F32) for i in range(5)]
                stt = [sb1("stB%d" % i, [128, 8], F32) for i in range(5)]
                lg = [sb1("lg%d" % i, [128, 36], F32) for i in range(5)]
                wk = [sb1("wk%d" % i, [128, 192], F32) for i in range(5)]
                ohb = [sb1("ohb%d" % i, [128, 32], BF16) for i in range(5)]
                PH = [PS[6], PS[7]]
                ld = [DMA('s0', bb, lambda: nc.sync.dma_start(out=w_r[:, :, 0:4], in_=w_rg.rearrange("(k p) c -> p k c", p=128))),
                      DMA('s0', bb, lambda: nc.sync.dma_start(out=w_r[:, :, 4:36], in_=w_re.rearrange("(k p) c -> p k c", p=128))),
                      DMA('s0', bb, lambda: nc.sync.dma_start(out=brr[:, 0:4], in_=b_rg.partition_broadcast(128))),
                      DMA('s0', bb, lambda: nc.sync.dma_start(out=brr[:, 4:36], in_=b_re.partition_broadcast(128))),
                      DMA('s1', bb, lambda: nc.gpsimd.dma_start(out=utri[:], in_=utri_in), q='pool'),
                      POOL(bb, lambda: nc.gpsimd.memset(onesb[:], 1.0)),
                      POOL(bb, lambda: nc.gpsimd.memset(carry[:], 0.0))]
                rdy1 = [readyB, ld]
                T = dict(cur_seq=-1, vec_ready=None, vec_readers=[], hm_free=None, PH_free=[None, None], plg_free=None,
                         pcum_free=None, carry_tok=ld[-1])
                x1t_free = [None] * 5; h2_free = [None] * 5
                h2Tf_free = [None] * 5
                h2d_w = []

                def p1_tile(i):
                    s = gseq[i]
                    if s != T['cur_seq']:
                        T['cur_seq'] = s
                        T['vec_ready'] = load_vecs(s, [rdy1, T['vec_readers']])
                        T['vec_readers'] = []
                    vec_ready = T['vec_ready']
                    geff2 = geff2_[s % 2]; sh2r = sh2r_[s % 2]
                    ti = i % 5
                    tok0 = i * 128
                    st_ = stt[ti]
                    lx = DMA('bx%d' % ti, [x1t_free[ti], rdy1], lambda ti=ti, tok0=tok0: nc.sync.dma_start(out=x1t[ti][:], in_=x1_d[tok0:tok0 + 128, :]))
                    a1 = ACT(lx, lambda ti=ti, st_=st_: nc.scalar.activation(out=junk[:], in_=x1t[ti][:], func=AF.Square, accum_out=st_[:, 0:1]))
                    yield
                    r1a = ACT(a1, lambda st_=st_: nc.scalar.activation(out=st_[:, 1:2], in_=st_[:, 0:1], func=AF.Sqrt, bias=EPS, scale=1.0 / D))
                    yield
                    r1 = DVE(r1a, lambda st_=st_: nc.vector.reciprocal(out=st_[:, 1:2], in_=st_[:, 1:2]))
                    d1 = DVE([r1, T['hm_free'], vec_ready], lambda ti=ti, st_=st_: nc.vector.scalar_tensor_tensor(
                        out=hm[:], in0=x1t[ti][:], scalar=st_[:, 1:2], in1=geff2[:], op0=ALU.mult, op1=ALU.mult))
                    x1t_free[ti] = d1
                    p1_ = POOL([d1, h2_free[ti], vec_ready], lambda ti=ti: nc.gpsimd.tensor_tensor(out=h2[ti][:], in0=hm[:], in1=sh2r[:], op=ALU.add))
                    T['hm_free'] = p1_
                    T['vec_readers'] = [p1_, d1]
                    wr = DMA('h2w%d' % ti, p1_, lambda ti=ti, tok0=tok0: nc.gpsimd.dma_start(out=h2_d[tok0:tok0 + 128, :], in_=h2[ti][:]), q='pool')
                    h2d_w.append(wr)
                    yield
                    tp = None
                    for k in range(8):
                        bank = PH[k // 4]
                        tp = PE([p1_, T['PH_free']] if k == 0 else None,
                                lambda k=k, ti=ti, bank=bank: nc.tensor.transpose(bank[:, (k % 4) * 128:(k % 4 + 1) * 128],
                                                                                  h2[ti][:, k * 128:(k + 1) * 128], ident_f[:]),
                                sig=(k == 7))
                    h2_free[ti] = [tp, wr]
                    yield
                    e1 = ACT([tp, h2Tf_free[ti]], lambda ti=ti: nc.scalar.copy(out=h2Tf[ti][:, 0:4, :], in_=PH[0][:, :].rearrange("p (k c) -> p k c", k=4)))
                    e2 = DVE([tp, h2Tf_free[ti]], lambda ti=ti: nc.vector.tensor_copy(out=h2Tf[ti][:, 4:8, :], in_=PH[1][:, :].rearrange("p (k c) -> p k c", k=4)))
                    T['PH_free'] = [e1, e2]
                    yield
                    plg = PS[4][:, 0:36]
                    m_l = mmg(plg, [(h2Tf[ti][:, k, :], w_r[:, k, :]) for k in range(8)], [e1, e2, T['plg_free'], rdy1])
                    h2Tf_free[ti] = m_l
                    yield
                    L = lg[ti]; W = wk[ti]
                    v1 = DVE([m_l], lambda L=L: nc.vector.tensor_tensor(out=L[:], in0=plg, in1=brr[:], op=ALU.add))
                    T['plg_free'] = v1
                    v2 = DVE(v1, lambda L=L, W=W: nc.vector.tensor_reduce(out=W[:, 0:1], in_=L[:, 0:4], axis=AX.X, op=ALU.max))
                    v3 = DVE(v2, lambda W=W: nc.vector.tensor_scalar(out=W[:, 1:2], in0=W[:, 0:1], scalar1=-1.0, scalar2=None, op0=ALU.mult))
                    v4 = DVE(v2, lambda L=L, W=W: nc.vector.tensor_scalar(out=W[:, 4:8], in0=L[:, 0:4], scalar1=W[:, 0:1], scalar2=None, op0=ALU.is_equal))
                    s1 = ACT([v3], lambda L=L, W=W: nc.scalar.activation(out=W[:, 8:12], in_=L[:, 0:4], func=AF.Exp, bias=W[:, 1:2], scale=1.0,
                                                                         accum_out=W[:, 2:3]))
                    yield
                    v5 = DVE(s1, lambda W=W: nc.vector.reciprocal(out=W[:, 3:4], in_=W[:, 2:3]))
                    v6 = DVE(v4, lambda L=L, W=W: nc.vector.tensor_tensor(
                        out=W[:, 16:48].rearrange("p (g e) -> p g e", g=4), in0=L[:, 4:36].rearrange("p (g e) -> p g e", g=4),
                        in1=W[:, 4:8].unsqueeze(2).to_broadcast([128, 4, 8]), op=ALU.mult))
                    v7 = DVE(v6, lambda W=W: nc.vector.tensor_reduce(out=W[:, 48:56], in_=W[:, 16:48].rearrange("p (g e) -> p e g", g=4),
                                                                    axis=AX.X, op=ALU.add))
                    v8 = DVE(v7, lambda W=W: nc.vector.tensor_reduce(out=W[:, 12:13], in_=W[:, 48:56], axis=AX.X, op=ALU.max))
                    v9 = DVE(v8, lambda W=W: nc.vector.tensor_scalar(out=W[:, 56:64], in0=W[:, 48:56], scalar1=W[:, 12:13], scalar2=None,
                                                                    op0=ALU.is_equal))
                    v10 = DVE(v9, lambda W=W: nc.vector.scalar_tensor_tensor(out=W[:, 64:72], in0=W[:, 56:64], scalar=-1e30, in1=W[:, 48:56],
                                                                            op0=ALU.mult, op1=ALU.add))
                    v11 = DVE(v10, lambda W=W: nc.vector.tensor_reduce(out=W[:, 13:14], in_=W[:, 64:72], axis=AX.X, op=ALU.max))
                    v12 = DVE(v11, lambda W=W: nc.vector.tensor_scalar(out=W[:, 72:80], in0=W[:, 64:72], scalar1=W[:, 13:14], scalar2=None,
                                                                      op0=ALU.is_equal))
                    v13 = DVE(v11, lambda W=W: nc.vector.tensor_scalar(out=W[:, 14:15], in0=W[:, 12:13], scalar1=-1.0, scalar2=None, op0=ALU.mult))
                    s2 = ACT([v13], lambda W=W: nc.scalar.activation(out=W[:, 15:16], in_=W[:, 13:14], func=AF.Exp, bias=W[:, 14:15], scale=1.0))
                    yield
                    v14 = DVE(s2, lambda W=W: nc.vector.tensor_scalar(out=W[:, 80:81], in0=W[:, 15:16], scalar1=1.0, scalar2=None, op0=ALU.add))
                    v15 = DVE(v14, lambda W=W: nc.vector.reciprocal(out=W[:, 81:82], in_=W[:, 80:81]))
                    v16 = DVE([v15, v5], lambda W=W, i=i: nc.vector.tensor_tensor(out=W1a[:, i:i + 1], in0=W[:, 81:82], in1=W[:, 3:4], op=ALU.mult))
                    v17 = DVE(v16, lambda W=W, i=i: nc.vector.tensor_tensor(out=W2a[:, i:i + 1], in0=W1a[:, i:i + 1], in1=W[:, 15:16], op=ALU.mult))
                    v18 = DVE([v9, v4], lambda W=W, i=i: nc.vector.tensor_tensor(
                        out=M1a[:, i, :].rearrange("p (g e) -> p g e", g=4), in0=W[:, 4:8].unsqueeze(2).to_broadcast([128, 4, 8]),
                        in1=W[:, 56:64].unsqueeze(1).to_broadcast([128, 4, 8]), op=ALU.mult))
                    v19 = DVE([v12], lambda W=W, i=i: nc.vector.tensor_tensor(
                        out=M2a[:, i, :].rearrange("p (g e) -> p g e", g=4), in0=W[:, 4:8].unsqueeze(2).to_broadcast([128, 4, 8]),
                        in1=W[:, 72:80].unsqueeze(1).to_broadcast([128, 4, 8]), op=ALU.mult))
                    OH = ohb[ti]
                    v20 = DVE([v18, v19, T['pcum_free']], lambda OH=OH, i=i: nc.vector.tensor_tensor(out=OH[:], in0=M1a[:, i, :], in1=M2a[:, i, :], op=ALU.add))
                    pcum = PS[5][:, 0:32]
                    ptot = PS[5][:, 32:64]
                    PE([v20, T['pcum_free'], rdy1], lambda OH=OH: nc.tensor.matmul(pcum, lhsT=utri[:], rhs=OH[:], start=True, stop=True), sig=False)
                    mc = PE(None, lambda OH=OH: nc.tensor.matmul(ptot, lhsT=onesb[:], rhs=OH[:], start=True, stop=True))
                    yield
                    v21 = DVE([mc, T['carry_tok']], lambda W=W: nc.vector.tensor_tensor(out=W[:, 96:128], in0=carry[:], in1=pcum, op=ALU.add))
                    v22 = DVE(v21, lambda: nc.vector.tensor_tensor(out=carry[:], in0=carry[:], in1=ptot, op=ALU.add))
                    T['carry_tok'] = v22
                    T['pcum_free'] = v22
                    v23 = DVE(v22, lambda W=W, i=i: nc.vector.tensor_tensor(out=W[:, 128:160], in0=W[:, 96:128], in1=M1a[:, i, :], op=ALU.mult))
                    v24 = DVE(v23, lambda W=W, i=i: nc.vector.tensor_reduce(out=R1a[:, i:i + 1], in_=W[:, 128:160], axis=AX.X, op=ALU.add))
                    v25 = DVE(v24, lambda W=W, i=i: nc.vector.tensor_tensor(out=W[:, 160:192], in0=W[:, 96:128], in1=M2a[:, i, :], op=ALU.mult))
                    v26 = DVE(v25, lambda W=W, i=i: nc.vector.tensor_reduce(out=R2a[:, i:i + 1], in_=W[:, 160:192], axis=AX.X, op=ALU.add))
                interleave((p1_tile(i) for i in range(NTT)), 5)
                b1_bar = dp.last() + [h2d_w]
                dp.retire_since(mkb1)

            with ExitStack() as b2:
                def sb2(name, shape, dt): return b2.enter_context(nc.sbuf_tensor(U(name), shape, dt))
                jv = sb2("jv", [128, NSL], F32)
                pidx = sb2("pidx", [128, 1], F32)
                tri32 = sb2("tri32", [32, 32], F32)
                cmp_ = sb2("cmp", [128, NSL * 32], F32)
                tmpM = sb2("tmpM", [128, NTT, 32], F32)
                nblk = sb2("nblk", [128, 32], F32)
                pc = sb2("pc", [128, 32], F32)
                pcT = sb2("pcT", [32, 128], F32)
                sst = sb2("sst", [128, 32], F32)
                send = sb2("send", [128, 32], F32)
                te = sb2("te", [128, NSL], F32)
                sf = sb2("sf", [128, NTT], F32)
                l = [DMA('i0', b1_bar, lambda: nc.sync.dma_start(out=jv[:], in_=jv_in)),
                     DMA('i0', b1_bar, lambda: nc.sync.dma_start(out=pidx[:], in_=pidx_in)),
                     DMA('i0', b1_bar, lambda: nc.sync.dma_start(out=tri32[:], in_=tri32_in))]
                c3 = cmp_[:].rearrange("p (e m) -> p e m", e=32)
                q1 = DVE([l, b1_bar], lambda: nc.vector.tensor_tensor(out=c3, in0=jv[:].unsqueeze(1).to_broadcast([128, 32, NSL]),
                                                                      in1=carry[:].unsqueeze(2).to_broadcast([128, 32, NSL]), op=ALU.is_lt))
                q2 = DVE(q1, lambda: nc.vector.tensor_reduce(out=nblk[:], in_=c3, axis=AX.X, op=ALU.add))
                q3 = DVE(q2, lambda: nc.vector.tensor_scalar(out=pc[:], in0=nblk[:], scalar1=128.0, scalar2=None, op0=ALU.mult))
                q4 = PE(q3, lambda: nc.tensor.transpose(PS[0][0:32, 0:128], pc[:, :], ident_f[:]))
                q5 = ACT(q4, lambda: nc.scalar.copy(out=pcT[:], in_=PS[0][0:32, 0:128]))
                q6 = PE([q5, l], lambda: nc.tensor.matmul(PS[1][:, 0:32], lhsT=pcT[:, :], rhs=tri32[:, :], start=True, stop=True))
                q7 = DVE(q6, lambda: nc.vector.tensor_copy(out=sst[:], in_=PS[1][:, 0:32]))
                q8 = DVE(q7, lambda: nc.vector.tensor_tensor(out=send[:], in0=sst[:], in1=pc[:], op=ALU.add))
                c4 = cmp_[:].rearrange("p (m e) -> p m e", e=32)
                q9 = DVE(q8, lambda: nc.vector.tensor_tensor(out=c4, in0=send[:].unsqueeze(1).to_broadcast([128, NSL, 32]),
                                                             in1=jv[:].unsqueeze(2).to_broadcast([128, NSL, 32]), op=ALU.is_le))
                q10 = DVE(q9, lambda: nc.vector.tensor_reduce(out=te[:], in_=c4, axis=AX.X, op=ALU.add))
                q11 = DVE(q10, lambda: nc.vector.tensor_scalar(out=te[:], in0=te[:], scalar1=31.0, scalar2=128.0, op0=ALU.min, op1=ALU.mult))
                q12 = DVE(q11, lambda: nc.vector.tensor_scalar(out=te[:], in0=te[:], scalar1=pidx[:, 0:1], scalar2=None, op0=ALU.add))
                q13 = DVE(q12, lambda: nc.vector.tensor_copy(out=idxw[:], in_=te[:]))
                q14 = DVE(q7, lambda: nc.vector.tensor_tensor(out=tmpM[:], in0=M1a[:], in1=sst[:].unsqueeze(1).to_broadcast([128, NTT, 32]), op=ALU.mult))
                q15 = DVE(q14, lambda: nc.vector.tensor_reduce(out=sf[:], in_=tmpM[:], axis=AX.X, op=ALU.add))
                q16 = DVE(q15, lambda: nc.vector.tensor_tensor(out=sf[:], in0=sf[:], in1=R1a[:], op=ALU.add))
                q17 = DVE(q16, lambda: nc.vector.tensor_copy(out=slot0[:], in_=sf[:]))
                q18 = DVE(q17, lambda: nc.vector.tensor_tensor(out=tmpM[:], in0=M2a[:], in1=sst[:].unsqueeze(1).to_broadcast([128, NTT, 32]), op=ALU.mult))
                q19 = DVE(q18, lambda: nc.vector.tensor_reduce(out=sf[:], in_=tmpM[:], axis=AX.X, op=ALU.add))
                q20 = DVE(q19, lambda: nc.vector.tensor_tensor(out=sf[:], in0=sf[:], in1=R2a[:], op=ALU.add))
                q21 = DVE(q20, lambda: nc.vector.tensor_copy(out=slot1[:], in_=sf[:]))
                b2_bar = dp.last()

            mkb3 = dp.mark()
            with ExitStack() as b3:
                def sb3(name, shape, dt): return b3.enter_context(nc.sbuf_tensor(U(name), shape, dt))
                hsc = [sb3("hsc%d" % i, [128, D], BF16) for i in range(3)]
                hsc_free = [None] * 3
                sc_toks = []
                for i in range(NTT):
                    si = i % 3
                    tok0 = i * 128
                    lh = DMA('hl%d' % si, [hsc_free[si], b2_bar], lambda si=si, tok0=tok0: nc.sync.dma_start(out=hsc[si][:], in_=h2_d[tok0:tok0 + 128, :]))
                    s0 = DMA('sc%d' % si, [lh, b2_bar], lambda si=si, i=i: nc.gpsimd.indirect_dma_start(
                        out=xs_d[:, :], out_offset=bass.IndirectOffsetOnAxis(ap=slot0[:, i:i + 1], axis=0), in_=hsc[si][:, :], in_offset=None), q='pool')
                    s1_ = DMA('sc%d' % si, [lh], lambda si=si, i=i: nc.gpsimd.indirect_dma_start(
                        out=xs_d[:, :], out_offset=bass.IndirectOffsetOnAxis(ap=slot1[:, i:i + 1], axis=0), in_=hsc[si][:, :], in_offset=None), q='pool')
                    hsc_free[si] = [s0, s1_]
                    sc_toks += [s0, s1_]
                scat_done = [sc_toks[-6:], b2_bar]

                PF = 3
                NW = PF + 3
                ND = PF + 5
                NX = PF + 2
                wgu = [sb3("wgu%d" % i, [128, 8, 512], BF16) for i in range(NW)]
                wdb = [sb3("wdb%d" % i, [128, 2, D], BF16) for i in range(ND)]
                xsb = [sb3("xsb%d" % i, [128, D], BF16) for i in range(NX)]
                xT = [sb3("xT%d" % i, [128, 8, 128], BF16) for i in range(2)]
                sgs = [sb3("sgs%d" % i, [128, 256], F32) for i in range(2)]
                hid = [sb3("hid%d" % i, [128, 256], BF16) for i in range(2)]
                hT = [sb3("hT%d" % i, [128, 2, 128], BF16) for i in range(2)]
                ysb = [sb3("ysb%d" % i, [128, D], F32) for i in range(2)]
                pX = [PS[0][:, :].bitcast(BF16), PS[1][:, :].bitcast(BF16)]
                pH = [PS[2], PS[3]]
                pHT = [PS[4][:, 0:128].bitcast(BF16), PS[5][:, 0:128].bitcast(BF16)]
                pY = [PS[6], PS[7]]
                wgu_free = [None] * NW; wdb_free = [None] * ND; xsb_free = [None] * NX
                pX_free = [None, None]; xT_free = [None, None]; pH_free = [None, None]; sgs_free = [None, None]
                hid_free = [None, None]; pHT_free = [None, None]; hT_free = [None, None]
                pY_free = [None, None]; ysb_free = [None, None]
                st0 = {}; st1 = {}; st2 = {}; ldt = {}
                ys_w = []

                def issue_loads(a):
                    wi = a % NW; di = a % ND; xj = a % NX
                    lw = DMA('wgl%d' % wi, [wgu_free[wi], scat_done], lambda wi=wi, a=a: nc.gpsimd.indirect_dma_start(
                        out=wgu[wi][:].rearrange("p k c -> p (k c)"), out_offset=None, in_=wgu_r[:, :],
                        in_offset=bass.IndirectOffsetOnAxis(ap=idxw[:, a:a + 1], axis=0)), q='pool')
                    lwd = DMA('wdl%d' % di, [wdb_free[di], scat_done], lambda di=di, a=a: nc.gpsimd.indirect_dma_start(
                        out=wdb[di][:].rearrange("p k c -> p (k c)"), out_offset=None, in_=wd_r[:, :],
                        in_offset=bass.IndirectOffsetOnAxis(ap=idxw[:, a:a + 1], axis=0)), q='pool')
                    lxs = DMA('xsl%d' % xj, [xsb_free[xj], scat_done, sc_toks], lambda xj=xj, a=a: nc.sync.dma_start(
                        out=xsb[xj][:], in_=xs_d[a * 128:(a + 1) * 128, :]))
                    ldt[a] = (lw, lwd, lxs)

                for a in range(min(PF, NSL)):
                    issue_loads(a)
                for it in range(NSL + 3):
                    if it + PF < NSL:
                        issue_loads(it + PF)
                    a = it
                    if a < NSL:
                        xi = a % 2; xj = a % NX
                        lw, lwd, lxs = ldt[a]
                        tp = None
                        for k in range(8):
                            tp = PE([lxs, pX_free[xi]] if k == 0 else None,
                                    lambda k=k, xi=xi, xj=xj: nc.tensor.transpose(pX[xi][:, k * 128:(k + 1) * 128], xsb[xj][:, k * 128:(k + 1) * 128], ident_b[:]),
                                    sig=(k == 7))
                        xsb_free[xj] = tp
                        if a % 2 == 0:
                            ev = ACT([tp, xT_free[xi]], lambda xi=xi: nc.scalar.copy(out=xT[xi][:].rearrange("p k c -> p (k c)"), in_=pX[xi]))
                        else:
                            ev = DVE([tp, xT_free[xi]], lambda xi=xi: nc.vector.tensor_copy(out=xT[xi][:].rearrange("p k c -> p (k c)"), in_=pX[xi]))
                        pX_free[xi] = ev
                        st0[a] = (ev, lw, lwd)
                    a = it - 1
                    if 0 <= a < NSL:
                        wi = a % NW; xi = a % 2
                        ev, lw, lwd = st0[a]
                        mh = mmg(pH[xi][:, :], [(xT[xi][:, k, :], wgu[wi][:, k, :]) for k in range(8)], [ev, lw, pH_free[xi]])
                        wgu_free[wi] = mh
                        xT_free[xi] = mh
                        a_s = ACT([mh, sgs_free[xi]], lambda xi=xi: nc.scalar.activation(out=sgs[xi][:], in_=pH[xi][:, 0:256], func=AF.Silu))
                        d_h = DVE([a_s, hid_free[xi]], lambda xi=xi: nc.vector.tensor_tensor(out=hid[xi][:], in0=sgs[xi][:], in1=pH[xi][:, 256:512], op=ALU.mult))
                        pH_free[xi] = d_h
                        sgs_free[xi] = d_h
                        st1[a] = (d_h, lwd)
                    a = it - 2
                    if 0 <= a < NSL:
                        xi = a % 2
                        d_h, lwd = st1[a]
                        tp2 = None
                        for j in range(2):
                            tp2 = PE([d_h, pHT_free[xi]] if j == 0 else None,
                                     lambda j=j, xi=xi: nc.tensor.transpose(pHT[xi][:, j * 128:(j + 1) * 128], hid[xi][:, j * 128:(j + 1) * 128], ident_b[:]),
                                     sig=(j == 1))
                        hid_free[xi] = tp2
                        ev2 = ACT([tp2, hT_free[xi]], lambda xi=xi: nc.scalar.copy(out=hT[xi][:].rearrange("p k c -> p (k c)"), in_=pHT[xi]))
                        pHT_free[xi] = ev2
                        st2[a] = (ev2, lwd)
                    a = it - 3
                    if 0 <= a < NSL:
                        xi = a % 2; di = a % ND
                        ev2, lwd = st2[a]
                        my0 = mmg(pY[0][:, :], [(hT[xi][:, j, :], wdb[di][:, j, 0:512]) for j in range(2)], [ev2, lwd, pY_free[0]])
                        my1 = mmg(pY[1][:, :], [(hT[xi][:, j, :], wdb[di][:, j, 512:1024]) for j in range(2)], [pY_free[1]])
                        wdb_free[di] = my1
                        hT_free[xi] = my1
                        c0 = ACT([my0, ysb_free[xi]], lambda xi=xi: nc.scalar.copy(out=ysb[xi][:, 0:512], in_=pY[0][:, :]))
                        c1 = DVE([my1, ysb_free[xi]], lambda xi=xi: nc.vector.tensor_copy(out=ysb[xi][:, 512:1024], in_=pY[1][:, :]))
                        pY_free = [c0, c1]
                        ysb_free[xi] = DMA('ysw%d' % xi, [c0, c1], lambda xi=xi, a=a: nc.sync.dma_start(out=ys_d[a * 128:(a + 1) * 128, :], in_=ysb[xi][:]))
                        ys_w.append(ysb_free[xi])
                b3_bar = dp.last() + [ys_w[-2:]]
                dp.retire_since(mkb3)

            with ExitStack() as b4:
                def sb4(name, shape, dt): return b4.enter_context(nc.sbuf_tensor(U(name), shape, dt))
                ya = [sb4("ya%d" % i, [128, D], F32) for i in range(5)]
                yb = [sb4("yb%d" % i, [128, D], F32) for i in range(5)]
                x1c = [sb4("x1c%d" % i, [128, D], F32) for i in range(5)]
                mm_ = [sb4("mm_%d" % i, [128, D], F32) for i in range(5)]
                ytmp = [sb4("ytmp%d" % i, [128, D], F32) for i in range(5)]
                yo = [sb4("yo%d" % i, [128, D], F32) for i in range(5)]
                junk = sb4("junkC", [128, D], BF16)
                stt = [sb4("stC%d" % i, [128, 8], F32) for i in range(5)]
                ya_free = [None] * 5; yb_free = [None] * 5; x1c_free = [None] * 5; mm_free = [None] * 5
                ytmp_free = [None] * 5; yo_free = [None] * 5
                T = dict(cur_seq=-1, vec_ready=None, vec_readers=[])
                out_toks = []

                def cmb_tile(i):
                    s = gseq[i]
                    if s != T['cur_seq']:
                        T['cur_seq'] = s
                        T['vec_ready'] = load_vecs(s, [b3_bar, T['vec_readers']])
                        T['vec_readers'] = []
                    vec_ready = T['vec_ready']
                    gvec2 = gvec2_[s % 2]
                    ti = i % 5
                    tok0 = i * 128
                    st_ = stt[ti]
                    ga = DMA('ga%d' % ti, [ya_free[ti], b3_bar, ys_w], lambda ti=ti, i=i: nc.gpsimd.indirect_dma_start(
                        out=ya[ti][:, :], out_offset=None, in_=ys_d[:, :], in_offset=bass.IndirectOffsetOnAxis(ap=slot0[:, i:i + 1], axis=0)), q='pool')
                    gb_ = DMA('gb%d' % ti, [yb_free[ti], b3_bar], lambda ti=ti, i=i: nc.gpsimd.indirect_dma_start(
                        out=yb[ti][:, :], out_offset=None, in_=ys_d[:, :], in_offset=bass.IndirectOffsetOnAxis(ap=slot1[:, i:i + 1], axis=0)), q='pool')
                    lx = DMA('cx%d' % ti, [x1c_free[ti], b3_bar], lambda ti=ti, tok0=tok0: nc.sync.dma_start(out=x1c[ti][:], in_=x1_d[tok0:tok0 + 128, :]))
                    yield
                    d1 = DVE([ga, mm_free[ti]], lambda ti=ti, i=i: nc.vector.tensor_scalar(out=mm_[ti][:], in0=ya[ti][:], scalar1=W1a[:, i:i + 1], scalar2=None, op0=ALU.mult))
                    ya_free[ti] = d1
                    d2 = DVE([gb_, d1], lambda ti=ti, i=i: nc.vector.scalar_tensor_tensor(out=mm_[ti][:], in0=yb[ti][:], scalar=W2a[:, i:i + 1], in1=mm_[ti][:],
                                                                                         op0=ALU.mult, op1=ALU.add))
                    yb_free[ti] = d2
                    a1 = ACT(d2, lambda ti=ti, st_=st_: nc.scalar.activation(out=junk[:], in_=mm_[ti][:], func=AF.Square, accum_out=st_[:, 0:1]))
                    yield
                    r1a = ACT(a1, lambda st_=st_: nc.scalar.activation(out=st_[:, 1:2], in_=st_[:, 0:1], func=AF.Sqrt, bias=EPS, scale=1.0 / D))
                    yield
                    r1 = DVE(r1a, lambda st_=st_: nc.vector.reciprocal(out=st_[:, 1:2], in_=st_[:, 1:2]))
                    d3 = DVE([r1, ytmp_free[ti], vec_ready], lambda ti=ti, st_=st_: nc.vector.scalar_tensor_tensor(
                        out=ytmp[ti][:], in0=mm_[ti][:], scalar=st_[:, 1:2], in1=gvec2[:], op0=ALU.mult, op1=ALU.mult))
                    mm_free[ti] = d3
                    T['vec_readers'] = [d3]
                    pp = POOL([d3, lx, yo_free[ti]], lambda ti=ti: nc.gpsimd.tensor_tensor(out=yo[ti][:], in0=ytmp[ti][:], in1=x1c[ti][:], op=ALU.add))
                    ytmp_free[ti] = pp
                    x1c_free[ti] = pp
                    yo_free[ti] = DMA('yo%d' % ti, pp, lambda ti=ti, tok0=tok0: nc.sync.dma_start(out=y[tok0:tok0 + 128, :], in_=yo[ti][:]))
                    out_toks.append(yo_free[ti])
                interleave((cmb_tile(i) for i in range(NTT)), 5)
            dp.wait('sp', [yo_free, out_toks[-5:]])
            for e in ('pe', 'act', 'dve', 'pool'):
                dp.wait('sp', [(e, dp.cnt[e])])
    return nc


def _rope_tables(pos):
    half = 16
    inv = (10000.0 ** (-np.arange(half, dtype=np.float32) / half)).astype(np.float32)
    ang = pos.astype(np.float32)[:, None] * inv[None, :]
    cos = np.cos(ang).astype(np.float32)
    sin = np.sin(ang).astype(np.float32)
    c = np.concatenate([cos, cos], axis=1).T
    s_ = np.concatenate([sin, sin], axis=1).T
    return np.ascontiguousarray(c), np.ascontiguousarray(s_)


def _consts(NSL):
    ident = np.eye(128, dtype=np.float32)
    egrp = np.zeros((8, 512), np.float32)
    for g in range(8):
        egrp[g, g * 64:(g + 1) * 64] = 1.0
    utri = np.triu(np.ones((128, 128), np.float32), k=1)
    tri32 = np.triu(np.ones((32, 32), np.float32), k=1)
    jv = np.tile((np.arange(NSL, dtype=np.float32) * 128.0)[None, :], (128, 1))
    pidx = np.arange(128, dtype=np.float32).reshape(128, 1)
    return dict(ident=ident, egrp=egrp, utri=utri, tri32=tri32, jv=np.ascontiguousarray(jv), pidx=pidx,
                zeros=np.zeros((128, 8192), np.float32))


def _nt(cfg):
    nqt = sum(cfg['NQG']) * 512
    nt = (2 * nqt + 32 * 127 + 127) // 128
    return ((nt + 7) // 8) * 8


WEIGHT_KEYS = ['w_ada', 'b_ada', 'g_pre1', 'g_post1', 'g_pre2', 'g_post2', 'w_in', 'g_q', 'w_uq', 'g_kv', 'w_ukv',
               'g_v_gmlp', 'w_spatial', 'b_spatial', 'g_attn_out', 'g_gmlp_out', 'w_out', 'w_router_group',
               'b_router_group', 'w_router_expert', 'b_router_expert', 'w_gate', 'w_up', 'w_down']

_NC_CACHE = {}


def kernel(**inputs):
    S = 4096
    x_all = np.concatenate([np.asarray(inputs['x_prompt'], np.float32), np.asarray(inputs['x_sample'], np.float32)], axis=0)
    c_all = np.concatenate([np.asarray(inputs['c_prompt'], np.float32), np.asarray(inputs['c_sample'], np.float32)], axis=0)
    weights = {k: np.ascontiguousarray(np.asarray(inputs[k], np.float32)) for k in WEIGHT_KEYS}
    consts = _consts(_nt(FULL_CFG))
    pos_nat = np.arange(S)
    in_maps = []
    plans = []
    for c in range(8):
        if c % 2 == 0:
            s0 = (5 * c) // 2
            A, B, Cq, qhalf = s0, s0 + 1, s0 + 2, 0
        else:
            s0 = (5 * c - 1) // 2
            Cq, qhalf, A, B = s0, 1, s0 + 1, s0 + 2
        if qhalf == 0:
            posC = pos_nat
        else:
            posC = np.concatenate([pos_nat[S // 2:], pos_nat[:S // 2]])
        xs = np.concatenate([x_all[A], x_all[B], x_all[Cq][posC]], axis=0)
        cv = np.stack([c_all[A], c_all[B], c_all[Cq]], axis=0)
        rc = np.zeros((3, 32, S), np.float32)
        rs = np.zeros((3, 32, S), np.float32)
        for i, p in enumerate([pos_nat, pos_nat, posC]):
            rc[i], rs[i] = _rope_tables(p)
        m = dict(weights)
        m.update(xs=np.ascontiguousarray(xs), cvec=np.ascontiguousarray(cv), rope_c=rc, rope_s=rs)
        m.update(consts)
        in_maps.append(m)
        plans.append((A, B, Cq, qhalf))
    if 'full' not in _NC_CACHE:
        _NC_CACHE['full'] = build(FULL_CFG)
    nc = _NC_CACHE['full']
    res = run_bass_kernel_spmd(nc, in_maps, core_ids=list(range(8)))
    y_all = np.zeros((20, S, D), np.float32)
    for c in range(8):
        yc = res.results[c]['y']
        A, B, Cq, qhalf = plans[c]
        y_all[A] = yc[0:S]
        y_all[B] = yc[S:2 * S]
        if qhalf == 0:
            y_all[Cq, 0:S // 2] = yc[2 * S:2 * S + S // 2]
        else:
            y_all[Cq, S // 2:] = yc[2 * S:2 * S + S // 2]
    return (np.ascontiguousarray(y_all[0:4]), np.ascontiguousarray(y_all[4:20]))
```

```python
import numpy as np
import concourse.bass as bass
import concourse.mybir as mybir
from concourse.bass_utils import run_bass_kernel_spmd
from contextlib import ExitStack

F32, BF16 = mybir.dt.float32, mybir.dt.bfloat16
I32 = mybir.dt.int32
AF = mybir.ActivationFunctionType
ALU = mybir.AluOpType
AX = mybir.AxisListType
D = 1024
EPS = 1e-6
NE = 32
QSCALE = 96.0 ** -0.5

FULL_CFG = dict(S=4096, NSEQ=3, NQG=[8, 8, 4])


class Dep:
    def __init__(self, nc, es):
        self.nc = nc
        self.es = es
        self.eng = {'pe': nc.tensor, 'act': nc.scalar, 'dve': nc.vector, 'pool': nc.gpsimd, 'sp': nc.sync}
        self.sem = {e: es.enter_context(nc.semaphore('s_' + e)) for e in self.eng}
        self.cnt = {e: 0 for e in self.eng}
        self.waited = {e: {} for e in self.eng}
        self.dsem = {}
        self.entries = {}
        self.free = []

    def semof(self, k):
        return self.sem[k] if k in self.sem else self.entries[k][0]

    def wait(self, e, deps):
        mx = {}
        for k, v in _flat(deps):
            if v > mx.get(k, 0):
                mx[k] = v
        for k, v in mx.items():
            if self.waited[e].get(k, 0) < v:
                self.eng[e].wait_ge(self.semof(k), v)
                self.waited[e][k] = v

    def op(self, e, deps, fn, sig=True):
        self.wait(e, deps)
        ins = fn()
        if sig:
            ins.then_inc(self.sem[e], 1)
            self.cnt[e] += 1
            return (e, self.cnt[e])
        return None

    def dma(self, q, name, deps, fn):
        if name not in self.dsem:
            if self.free:
                key = self.free.pop()
            else:
                key = 'D%d' % len(self.entries)
                self.entries[key] = [self.es.enter_context(self.nc.semaphore('d_' + key)), 0]
            self.dsem[name] = key
        key = self.dsem[name]
        self.wait(q, deps)
        ins = fn()
        ent = self.entries[key]
        ins.then_inc(ent[0], 16)
        ent[1] += 16
        return (key, ent[1])

    def mark(self):
        return set(self.dsem.keys())

    def retire_since(self, mark, keep=()):
        for n in list(self.dsem.keys()):
            if n in mark or n in keep:
                continue
            key = self.dsem[n]
            self.wait('sp', (key, self.entries[key][1]))
            del self.dsem[n]
            self.free.append(key)

    def last(self):
        return [(e, self.cnt[e]) for e in self.eng if self.cnt[e] > 0]


def _flat(deps):
    out = []
    if deps is None:
        return out
    if isinstance(deps, tuple) and len(deps) == 2 and isinstance(deps[0], str):
        return [deps]
    for d in deps:
        out.extend(_flat(d))
    return out


def interleave(gens, depth):
    active = []
    it = iter(gens)
    done = False
    while True:
        if len(active) < depth and not done:
            try:
                active.append(next(it))
            except StopIteration:
                done = True
        if not active:
            break
        nxt = []
        for g in active:
            try:
                next(g)
                nxt.append(g)
            except StopIteration:
                pass
        active = nxt


def build(cfg):
    S = cfg['S']
    NSEQ = cfg['NSEQ']
    NQG = cfg['NQG']
    NG = S // 512
    KB = S // 128
    NT = NSEQ * S
    NQT = sum(NQG) * 512
    NGB = sum(NQG)
    NSL = (2 * NQT + 32 * 127 + 127) // 128
    NSL = ((NSL + 7) // 8) * 8

    nc = bass.Bass("TRN2", target_bir_lowering=False)

    def din(name, shape, dt=F32):
        return nc.dram_tensor(name, list(shape), dt, kind="ExternalInput").ap()

    def dscr(name, shape, dt):
        return nc.dram_tensor(name, list(shape), dt, kind="Internal").ap()

    xs = din("xs", [NT, D])
    cvec = din("cvec", [NSEQ, D])
    rope_c = din("rope_c", [NSEQ, 32, S])
    rope_s = din("rope_s", [NSEQ, 32, S])
    w_ada = din("w_ada", [D, 6 * D])
    b_ada = din("b_ada", [6 * D])
    g_pre1 = din("g_pre1", [D]); g_post1 = din("g_post1", [D])
    g_pre2 = din("g_pre2", [D]); g_post2 = din("g_post2", [D])
    w_in = din("w_in", [D, 1440])
    g_q = din("g_q", [256]); w_uq = din("w_uq", [256, 768])
    g_kv = din("g_kv", [128]); w_ukv = din("w_ukv", [128, 1024])
    g_v_gmlp = din("g_v_gmlp", [512])
    w_spatial = din("w_spatial", [8, 128, 128]); b_spatial = din("b_spatial", [8, 128])
    g_attn_out = din("g_attn_out", [512]); g_gmlp_out = din("g_gmlp_out", [512])
    w_out = din("w_out", [D, D])
    w_rg = din("w_router_group", [D, 4]); b_rg = din("b_router_group", [4])
    w_re = din("w_router_expert", [D, 32]); b_re = din("b_router_expert", [32])
    w_gate = din("w_gate", [NE, D, 256]); w_up = din("w_up", [NE, D, 256]); w_down = din("w_down", [NE, 256, D])
    ident_in = din("ident", [128, 128])
    egrp_in = din("egrp", [8, 512])
    utri_in = din("utri", [128, 128])
    tri32_in = din("tri32", [32, 32])
    jv_in = din("jv", [128, NSL])
    pidx_in = din("pidx", [128, 1])
    zeros_in = din("zeros", [128, 8192])
    y = nc.dram_tensor("y", [NQT, D], F32, kind="ExternalOutput").ap()

    mod_d = dscr("mod_d", [NSEQ, 6 * D], F32)
    sn_d = dscr("sn_d", [NT, 512], BF16)
    cq_d = dscr("cq_d", [NSEQ * NG, 128, 1024], BF16)
    x1_d = (nc.dram_tensor("x1_d", [NQT, D], F32, kind="ExternalOutput").ap() if cfg.get("dbg") else dscr("x1_d", [NQT, D], F32))
    wgu_r = dscr("wgu_r", [NE * 128, 8 * 512], BF16)
    wd_r = dscr("wd_r", [NE * 128, 2 * D], BF16)
    h2_d = dscr("h2_d", [NQT, D], BF16)
    xs_d = dscr("xs_d", [NSL * 128, D], BF16)
    ys_d = dscr("ys_d", [NSL * 128, D], F32)
    wkv_d = dscr("wkv_d", [128, 1024], BF16)
    wsp_d = dscr("wsp_d", [128, 1024], BF16)
    wq_d = dscr("wq_d", [128, 1536], BF16)
    wqsw_d = dscr("wqsw_d", [128, 1536], BF16)
    wo_d = dscr("wo_d", [128, 8192], BF16)

    _uid = [0]

    def U(name):
        _uid[0] += 1
        return "%s_u%d" % (name, _uid[0])

    top = ExitStack()
    with top:
        dp = Dep(nc, top)

        def PE(deps, fn, sig=True): return dp.op('pe', deps, fn, sig)
        def ACT(deps, fn, sig=True): return dp.op('act', deps, fn, sig)
        def DVE(deps, fn, sig=True): return dp.op('dve', deps, fn, sig)
        def POOL(deps, fn, sig=True): return dp.op('pool', deps, fn, sig)
        def DMA(name, deps, fn, q='sp'): return dp.dma(q, name, deps, fn)

        def mmg(out, pairs, deps, sig=True):
            n = len(pairs)
            tok = None
            for i, (l, r) in enumerate(pairs):
                tok = PE(deps if i == 0 else None,
                         lambda l=l, r=r, i=i: nc.tensor.matmul(out, lhsT=l, rhs=r, start=(i == 0), stop=(i == n - 1)),
                         sig=(sig and i == n - 1))
            return tok

        def rstd_chain(ss_ap, out_ap, inv_n, deps):
            t = ACT(deps, lambda: nc.scalar.activation(out=out_ap, in_=ss_ap, func=AF.Sqrt, bias=EPS, scale=inv_n))
            return DVE(t, lambda: nc.vector.reciprocal(out=out_ap, in_=out_ap))

        wcast = []
        for e in range(NE):
            wcast.append(DMA('wcast', None, lambda e=e: nc.gpsimd.dma_start(
                out=wgu_r[e * 128:(e + 1) * 128, :].rearrange("p (k c) -> p k c", k=8)[:, :, 0:256],
                in_=w_gate[e].rearrange("(k p) c -> p k c", p=128)), q='pool'))
            wcast.append(DMA('wcast', None, lambda e=e: nc.gpsimd.dma_start(
                out=wgu_r[e * 128:(e + 1) * 128, :].rearrange("p (k c) -> p k c", k=8)[:, :, 256:512],
                in_=w_up[e].rearrange("(k p) c -> p k c", p=128)), q='pool'))
            wcast.append(DMA('wcast', None, lambda e=e: nc.gpsimd.dma_start(
                out=wd_r[e * 128:(e + 1) * 128, :].rearrange("p (j c) -> p j c", j=2),
                in_=w_down[e].rearrange("(j p) c -> p j c", p=128)), q='pool'))
        wcast_tok = wcast[-1]
        zero_tok = []
        nz = (NSL * 128 * D) // (128 * 8192)
        xs_flat = xs_d.rearrange("(n p r) c -> n p (r c)", p=128, r=8)
        for zi in range(nz):
            zero_tok.append(DMA('zero', None, lambda zi=zi: nc.gpsimd.dma_start(out=xs_flat[zi], in_=zeros_in), q='pool'))

        ident_f = top.enter_context(nc.sbuf_tensor(U("ident_f"), [128, 128], F32))
        ident_b = top.enter_context(nc.sbuf_tensor(U("ident_b"), [128, 128], BF16))
        ones_f = top.enter_context(nc.sbuf_tensor(U("ones_f"), [128, 64], F32))
        t_id = DMA('c0', None, lambda: nc.sync.dma_start(out=ident_f[:], in_=ident_in))
        t_idb = DVE(t_id, lambda: nc.vector.tensor_copy(out=ident_b[:], in_=ident_f[:]))
        t_ones = DVE(None, lambda: nc.vector.memset(ones_f[:], 1.0))

        mk0 = dp.mark()
        with ExitStack() as pes:
            def sb(name, shape, dt): return pes.enter_context(nc.sbuf_tensor(U(name), shape, dt))
            def ps(name, shape, dt): return pes.enter_context(nc.psum_tensor(U(name), shape, dt))
            csT = sb("csT", [128, 8, NSEQ], F32)
            csS = sb("csS", [128, 8, NSEQ], F32)
            wblk = [sb("wblk%d" % i, [128, 8, 512], F32) for i in range(2)]
            brep = sb("brep", [NSEQ, 6 * D], F32)
            modsb = sb("modsb", [NSEQ, 6 * D], F32)
            pmod = [ps("pmod%d" % i, [128, 512], F32) for i in range(2)]
            t_c = [DMA('p0', None, lambda q=q: nc.sync.dma_start(out=csT[:, :, q], in_=cvec[q].rearrange("(k p) -> p k", p=128),
                                                                 allow_slow_non_contiguous=True)) for q in range(NSEQ)]
            t_b = DMA('p1', None, lambda: nc.sync.dma_start(out=brep[:], in_=b_ada.partition_broadcast(NSEQ)))
            t_cs = ACT(t_c, lambda: nc.scalar.activation(out=csS[:], in_=csT[:], func=AF.Silu))
            wfree = [None, None]
            pfree = [None, None]
            ev = None
            for blk in range(12):
                i = blk % 2
                t_w = DMA('pw%d' % i, wfree[i], lambda blk=blk, i=i: nc.sync.dma_start(
                    out=wblk[i][:], in_=w_ada[:, blk * 512:(blk + 1) * 512].rearrange("(k p) c -> p k c", p=128)))
                t_m = mmg(pmod[i][0:NSEQ, :], [(csS[:, k, :], wblk[i][:, k, :]) for k in range(8)], [t_w, t_cs, pfree[i]])
                wfree[i] = t_m
                ev = DVE([t_m, t_b], lambda blk=blk, i=i: nc.vector.tensor_tensor(
                    out=modsb[:, blk * 512:(blk + 1) * 512], in0=pmod[i][0:NSEQ, :],
                    in1=brep[:, blk * 512:(blk + 1) * 512], op=ALU.add))
                pfree[i] = ev
            t_mod = DMA('p2', ev, lambda: nc.sync.dma_start(out=mod_d, in_=modsb[:]))

            tmpq = sb("tmpq", [128, 2, 768], F32)
            gq = sb("gq", [128, 2], F32)
            wq_t = sb("wq_t", [128, 2, 768], BF16)
            wqsw_t = sb("wqsw_t", [128, 2, 768], BF16)
            t1 = DMA('p3', None, lambda: nc.sync.dma_start(out=tmpq[:], in_=w_uq.rearrange("(k p) c -> p k c", p=128)))
            t2 = DMA('p3', None, lambda: nc.sync.dma_start(out=gq[:], in_=g_q.rearrange("(k p) -> p k", p=128),
                                                          allow_slow_non_contiguous=True))
            tq = None
            for k in range(2):
                tq = DVE([t1, t2], lambda k=k: nc.vector.tensor_scalar(
                    out=wq_t[:, k, :], in0=tmpq[:, k, :], scalar1=gq[:, k:k + 1], scalar2=QSCALE,
                    op0=ALU.mult, op1=ALU.mult))
            tz = POOL(None, lambda: nc.gpsimd.memset(wqsw_t[:], 0.0))
            wq4 = wq_t[:].rearrange("p k (h c) -> p k h c", h=8)
            wqs4 = wqsw_t[:].rearrange("p k (h c) -> p k h c", h=8)
            ta = DVE([tq, tz], lambda: nc.vector.tensor_scalar(out=wqs4[:, :, :, 64:80], in0=wq4[:, :, :, 80:96],
                                                              scalar1=-1.0, scalar2=None, op0=ALU.mult))
            tb = DVE(None, lambda: nc.vector.tensor_copy(out=wqs4[:, :, :, 80:96], in_=wq4[:, :, :, 64:80]))
            t_wq = DMA('p4', tq, lambda: nc.sync.dma_start(out=wq_d, in_=wq_t[:].rearrange("p k c -> p (k c)")))
            t_wqsw = DMA('p4', [ta, tb], lambda: nc.sync.dma_start(out=wqsw_d, in_=wqsw_t[:].rearrange("p k c -> p (k c)")))

            tmpkv = sb("tmpkv", [128, 1024], F32)
            gkv = sb("gkv", [128, 1], F32)
            wkv_t = sb("wkv_t", [128, 1024], BF16)
            t1 = DMA('p5', None, lambda: nc.sync.dma_start(out=tmpkv[:], in_=w_ukv))
            t2 = DMA('p5', None, lambda: nc.sync.dma_start(out=gkv[:], in_=g_kv.rearrange("(p o) -> p o", o=1)))
            tk = DVE([t1, t2], lambda: nc.vector.tensor_scalar(out=wkv_t[:], in0=tmpkv[:], scalar1=gkv[:, 0:1],
                                                              scalar2=None, op0=ALU.mult))
            t_wkv = DMA('p6', tk, lambda: nc.sync.dma_start(out=wkv_d, in_=wkv_t[:]))

            tmpo = sb("tmpo", [128, 8, 1024], F32)
            gcat = sb("gcat", [128, 8], F32)
            wo_t = sb("wo_t", [128, 8, 1024], BF16)
            t1 = DMA('p7', None, lambda: nc.sync.dma_start(out=tmpo[:], in_=w_out.rearrange("(k p) c -> p k c", p=128)))
            t2 = DMA('p7', None, lambda: nc.sync.dma_start(out=gcat[:, 0:4], in_=g_attn_out.rearrange("(k p) -> p k", p=128),
                                                          allow_slow_non_contiguous=True))
            t3 = DMA('p7', None, lambda: nc.sync.dma_start(out=gcat[:, 4:8], in_=g_gmlp_out.rearrange("(k p) -> p k", p=128),
                                                          allow_slow_non_contiguous=True))
            two = None
            for k in range(8):
                two = DVE([t1, t2, t3], lambda k=k: nc.vector.tensor_scalar(
                    out=wo_t[:, k, :], in0=tmpo[:, k, :], scalar1=gcat[:, k:k + 1], scalar2=None, op0=ALU.mult))
            t_wo = DMA('p8', two, lambda: nc.sync.dma_start(out=wo_d, in_=wo_t[:].rearrange("p k c -> p (k c)")))

            tmps = sb("tmps", [128, 8, 128], F32)
            wsp_t = sb("wsp_t", [128, 8, 128], BF16)
            psp = ps("psp", [128, 1024], F32)
            t1 = DMA('p9', None, lambda: nc.sync.dma_start(out=tmps[:], in_=w_spatial.rearrange("g t s -> t g s")))
            tt = None
            for g in range(8):
                tt = PE([t1, t_id], lambda g=g: nc.tensor.transpose(psp[:, g * 128:(g + 1) * 128], tmps[:, g, :], ident_f[:]),
                        sig=(g == 7))
            tc_ = DVE(tt, lambda: nc.vector.tensor_copy(out=wsp_t[:].rearrange("p g t -> p (g t)"), in_=psp[:]))
            t_wsp = DMA('p10', tc_, lambda: nc.sync.dma_start(out=wsp_d, in_=wsp_t[:].rearrange("p g t -> p (g t)")))
            prep_done = [t_mod, t_wq, t_wqsw, t_wkv, t_wo, t_wsp]
            prep_bar = dp.last()
            dp.retire_since(mk0, keep=('wcast', 'zero', 'c0'))

        with ExitStack() as aes:
            def sbA(name, shape, dt): return aes.enter_context(nc.sbuf_tensor(U(name), shape, dt))
            KT = sbA("KT", [128, 8, S], BF16)
            VA = sbA("VA", [128, KB, 8, 65], BF16)
            geff1 = sbA("geff1", [128, D], F32)
            sh1r = sbA("sh1r", [128, D], F32)
            gvec1 = sbA("gvec1", [128, D], F32)
            gvrep = sbA("gvrep", [128, 512], F32)
            PA = aes.enter_context(nc.psum_tensor(U("PA"), [128, 1024], F32))
            PB = aes.enter_context(nc.psum_tensor(U("PB"), [128, 1024], F32))
            PC = aes.enter_context(nc.psum_tensor(U("PC"), [128, 1024], F32))
            PD = aes.enter_context(nc.psum_tensor(U("PD"), [128, 1024], F32))

            t_va1 = POOL(prep_bar, lambda: nc.gpsimd.memset(VA[:, :, :, 64:65], 1.0))
            t_gv = DMA('a0', prep_bar, lambda: nc.sync.dma_start(out=gvrep[:], in_=g_v_gmlp.partition_broadcast(128)))
            seq_bar = [prep_bar, prep_done, t_va1, t_gv, t_idb, t_ones]
            qbase = 0
            for s in range(NSEQ):
                mk1 = dp.mark()
                with ExitStack() as p1:
                    def sb1(name, shape, dt): return p1.enter_context(nc.sbuf_tensor(U(name), shape, dt))
                    wAs = sb1("wAs", [128, 8, 384], BF16)
                    wAuv = sb1("wAuv", [128, 8, 1024], BF16)
                    wAkr = sb1("wAkr", [128, 8, 96], BF16)
                    wAks = sb1("wAks", [128, 8, 96], BF16)
                    wkv = sb1("wkv", [128, 1024], BF16)
                    wsp = sb1("wsp", [128, 8, 128], BF16)
                    bsp = sb1("bsp", [8, 128], F32)
                    egrp = sb1("egrp", [8, 512], F32)
                    vt = [sb1("vt%d" % i, [128, D], F32) for i in range(2)]
                    xt = [sb1("xt%d" % i, [128, D], F32) for i in range(2)]
                    junk = sb1("junk", [128, D], BF16)
                    hm = sb1("hm", [128, D], F32)
                    hb = [sb1("hb%d" % i, [128, D], BF16) for i in range(2)]
                    hT = sb1("hT", [128, 8, 512], BF16)
                    zsb = [sb1("zsb%d" % i, [128, 384], BF16) for i in range(2)]
                    cqnT = [sb1("cqnT%d" % i, [128, 2, 512], BF16) for i in range(2)]
                    ckvnT = [sb1("ckvnT%d" % i, [128, 512], BF16) for i in range(2)]
                    gu = [sb1("gu%d" % i, [128, 512], BF16) for i in range(2)]
                    gv = [sb1("gv%d" % i, [128, 512], F32) for i in range(2)]
                    zraw = [sb1("zraw%d" % i, [128, 384], F32) for i in range(2)]
                    vn = [sb1("vn%d" % i, [128, 512], BF16) for i in range(2)]
                    sraw = [sb1("sraw%d" % i, [128, 512], F32) for i in range(2)]
                    sn = [sb1("sn%d" % i, [128, 512], BF16) for i in range(2)]
                    stt = [sb1("stt%d" % i, [128, 16], F32) for i in range(2)]
                    Ctt = [sb1("Ctt%d" % i, [128, 128], F32) for i in range(2)]
                    Stt = [sb1("Stt%d" % i, [128, 128], F32) for i in range(2)]
                    kt1 = [sb1("kt1_%d" % i, [128, 128], F32) for i in range(2)]
                    kt2 = [sb1("kt2_%d" % i, [128, 128], F32) for i in range(2)]
                    krr = [sb1("krr%d" % i, [128, 128], BF16) for i in range(2)]

                    pT = PA[:, 0:512].bitcast(BF16)
                    pT2 = PA[:, 512:1024].bitcast(BF16)
                    pzs = PB[:, 0:384]
                    pss = PB[:, 512:1024]
                    pu = PC[:, 0:512]
                    pv = PC[:, 512:1024]
                    pkr = PD[:, 0:512]
                    pks = PD[:, 512:1024]

                    sb_ = seq_bar
                    wl = []
                    wl.append(DMA('a1', sb_, lambda: nc.gpsimd.dma_start(
                        out=wAs[:], in_=w_in[:, 0:384].rearrange("(k p) c -> p k c", p=128)), q='pool'))
                    wl.append(DMA('a1', sb_, lambda: nc.gpsimd.dma_start(
                        out=wAuv[:], in_=w_in[:, 416:1440].rearrange("(k p) c -> p k c", p=128)), q='pool'))
                    tz1 = POOL(sb_, lambda: nc.gpsimd.memset(wAkr[:], 0.0))
                    tz2 = POOL(sb_, lambda: nc.gpsimd.memset(wAks[:], 0.0))
                    wl.append(DMA('a1', [tz1], lambda: nc.gpsimd.dma_start(
                        out=wAkr[:, :, 64:96], in_=w_in[:, 384:416].rearrange("(k p) c -> p k c", p=128)), q='pool'))
                    tn = DMA('a2', [tz2], lambda: nc.gpsimd.dma_start(
                        out=wAks[:, :, 64:80], in_=w_in[:, 400:416].rearrange("(k p) c -> p k c", p=128)), q='pool')
                    wl.append(DMA('a1', [tz2], lambda: nc.gpsimd.dma_start(
                        out=wAks[:, :, 80:96], in_=w_in[:, 384:400].rearrange("(k p) c -> p k c", p=128)), q='pool'))
                    wl.append(POOL(tn, lambda: nc.gpsimd.tensor_scalar(out=wAks[:, :, 64:80], in0=wAks[:, :, 64:80],
                                                                      scalar1=-1.0, scalar2=None, op0=ALU.mult)))
                    wl.append(DMA('a3', sb_, lambda: nc.sync.dma_start(out=wkv[:], in_=wkv_d)))
                    wl.append(DMA('a3', sb_, lambda: nc.sync.dma_start(out=wsp[:].rearrange("p g t -> p (g t)"), in_=wsp_d)))
                    wl.append(DMA('a3', sb_, lambda: nc.sync.dma_start(out=bsp[:], in_=b_spatial)))
                    wl.append(DMA('a3', sb_, lambda: nc.sync.dma_start(out=egrp[:], in_=egrp_in)))
                    l1 = DMA('a4_0', sb_, lambda: nc.sync.dma_start(out=vt[0][:], in_=mod_d[s, 1024:2048].partition_broadcast(128)))
                    l2 = DMA('a4_1', sb_, lambda: nc.sync.dma_start(out=vt[1][:], in_=g_pre1.partition_broadcast(128)))
                    l3 = DMA('a4_2', sb_, lambda: nc.sync.dma_start(out=sh1r[:], in_=mod_d[s, 0:1024].partition_broadcast(128)))
                    tg = DVE([l1, l2], lambda: nc.vector.scalar_tensor_tensor(out=geff1[:], in0=vt[0][:], scalar=1.0, in1=vt[1][:],
                                                                               op0=ALU.add, op1=ALU.mult))
                    l4 = DMA('a4_3', [tg], lambda: nc.sync.dma_start(out=vt[0][:], in_=mod_d[s, 2048:3072].partition_broadcast(128)))
                    l5 = DMA('a4_4', [tg], lambda: nc.sync.dma_start(out=vt[1][:], in_=g_post1.partition_broadcast(128)))
                    tg2 = DVE([l4, l5], lambda: nc.vector.tensor_tensor(out=gvec1[:], in0=vt[0][:], in1=vt[1][:], op=ALU.mult))
                    ready = [wl, l3, tg, tg2]

                    xt_free = [None, None]; hb_free = [None, None]
                    hT_free = [None] * 4
                    zraw_free = [None, None]; zsb_free = [None, None]; cq_free = [None, None]; ckv_free = [None, None]
                    gu_free = [None, None]; gv_free = [None, None]; vn_free = [None, None]; sn_free = [None, None]
                    sraw_free = [None, None]; ct_free = [None, None]; kt_free = [None, None]; krr_free = [None, None]
                    P = dict(hm_free=None, pT_free=None, pT2_free=None, pzs_free=None, pu_free=None, pv_free=None, pss_free=None,
                             pkr_free=None, pks_free=None)
                    grp = {}

                    def p1_tile(g, t):
                        gi = g % 2
                        ti = t % 2
                        if t == 0:
                            grp[g] = dict(cq_w=[], ckv_w=[])
                        G = grp[g]
                        tok0 = s * S + g * 512 + t * 128
                        ts_ = slice(t * 128, (t + 1) * 128)
                        gts = slice(g * 512 + t * 128, g * 512 + (t + 1) * 128)
                        st_ = stt[ti]
                        lx = DMA('x%d' % ti, [xt_free[ti], ready], lambda: nc.sync.dma_start(out=xt[ti][:], in_=xs[tok0:tok0 + 128, :]))
                        lc = DMA('rc%d' % ti, [ct_free[ti], ready], lambda: nc.sync.dma_start(out=Ctt[ti][64:96, :], in_=rope_c[s, :, gts]))
                        ls = DMA('rs%d' % ti, [ct_free[ti], ready], lambda: nc.sync.dma_start(out=Stt[ti][64:96, :], in_=rope_s[s, :, gts]))
                        a1 = ACT(lx, lambda: nc.scalar.activation(out=junk[:], in_=xt[ti][:], func=AF.Square, accum_out=st_[:, 0:1]))
                        yield
                        r1a = ACT(a1, lambda: nc.scalar.activation(out=st_[:, 1:2], in_=st_[:, 0:1], func=AF.Sqrt, bias=EPS, scale=1.0 / D))
                        yield
                        r1 = DVE(r1a, lambda: nc.vector.reciprocal(out=st_[:, 1:2], in_=st_[:, 1:2]))
                        d1 = DVE([r1, P['hm_free']], lambda: nc.vector.scalar_tensor_tensor(
                            out=hm[:], in0=xt[ti][:], scalar=st_[:, 1:2], in1=geff1[:], op0=ALU.mult, op1=ALU.mult))
                        xt_free[ti] = d1
                        p1_ = POOL([d1, hb_free[ti]], lambda: nc.gpsimd.tensor_tensor(out=hb[ti][:], in0=hm[:], in1=sh1r[:], op=ALU.add))
                        P['hm_free'] = p1_
                        yield
                        tp = None
                        for k in range(8):
                            tp = PE([p1_, P['pT_free']] if k == 0 else None,
                                    lambda k=k: nc.tensor.transpose(pT[:, k * 128:(k + 1) * 128], hb[ti][:, k * 128:(k + 1) * 128], ident_b[:]),
                                    sig=(k == 7))
                        hb_free[ti] = tp
                        yield
                        ev = ACT([tp, hT_free[t]], lambda: nc.scalar.copy(out=hT[:, :, ts_], in_=pT.rearrange("p (k c) -> p k c", k=8)))
                        P['pT_free'] = ev
                        yield
                        m_zs = mmg(pzs, [(hT[:, k, ts_], wAs[:, k, :]) for k in range(8)], [ev, P['pzs_free']])
                        m_u = mmg(pu, [(hT[:, k, ts_], wAuv[:, k, 0:512]) for k in range(8)], [P['pu_free']])
                        m_v = mmg(pv, [(hT[:, k, ts_], wAuv[:, k, 512:1024]) for k in range(8)], [P['pv_free']])
                        m_kr = mmg(pkr[0:96, 0:128], [(wAkr[:, k, :], hT[:, k, ts_]) for k in range(8)], [P['pkr_free']])
                        m_ks = mmg(pks[0:96, 0:128], [(wAks[:, k, :], hT[:, k, ts_]) for k in range(8)], [P['pks_free']])
                        hT_free[t] = m_ks
                        yield
                        zr = DVE([m_zs, zraw_free[ti]], lambda: nc.vector.tensor_copy(out=zraw[ti][:], in_=pzs))
                        P['pzs_free'] = zr
                        g1 = ACT([m_u, gu_free[ti]], lambda: nc.scalar.activation(out=gu[ti][:], in_=pu, func=AF.Gelu_apprx_tanh))
                        P['pu_free'] = g1
                        g2 = ACT([m_v, gv_free[ti]], lambda: nc.scalar.activation(out=gv[ti][:], in_=pv, func=AF.Gelu_apprx_tanh))
                        P['pv_free'] = g2
                        k1 = DVE([m_kr, lc, kt_free[ti]], lambda: nc.vector.tensor_tensor(out=kt1[ti][64:96, :], in0=pkr[64:96, 0:128], in1=Ctt[ti][64:96, :], op=ALU.mult))
                        P['pkr_free'] = k1
                        k2 = DVE([m_ks, ls], lambda: nc.vector.tensor_tensor(out=kt2[ti][64:96, :], in0=pks[64:96, 0:128], in1=Stt[ti][64:96, :], op=ALU.mult))
                        P['pks_free'] = k2
                        ct_free[ti] = k2
                        yield
                        a2 = ACT(zr, lambda: nc.scalar.activation(out=junk[:, 0:256], in_=zraw[ti][:, 0:256], func=AF.Square, accum_out=st_[:, 2:3]))
                        a3 = ACT(None, lambda: nc.scalar.activation(out=junk[:, 256:384], in_=zraw[ti][:, 256:384], func=AF.Square, accum_out=st_[:, 3:4]))
                        g3 = ACT(g2, lambda: nc.scalar.activation(out=junk[:, 0:512], in_=gv[ti][:], func=AF.Square, accum_out=st_[:, 6:7]))
                        k3 = DVE([k1, k2, krr_free[ti]], lambda: nc.vector.tensor_tensor(out=krr[ti][64:96, :], in0=kt1[ti][64:96, :], in1=kt2[ti][64:96, :], op=ALU.add))
                        kt_free[ti] = k3
                        kc = None
                        for h in range(8):
                            kc = POOL(k3, lambda h=h: nc.gpsimd.tensor_copy(out=KT[64:96, h, gts], in_=krr[ti][64:96, :]))
                        krr_free[ti] = kc
                        yield
                        q1 = ACT([a2, a3], lambda: nc.scalar.activation(out=st_[:, 4:5], in_=st_[:, 2:3], func=AF.Sqrt, bias=EPS, scale=1.0 / 256))
                        q2 = ACT(None, lambda: nc.scalar.activation(out=st_[:, 5:6], in_=st_[:, 3:4], func=AF.Sqrt, bias=EPS, scale=1.0 / 128))
                        q3 = ACT(g3, lambda: nc.scalar.activation(out=st_[:, 7:8], in_=st_[:, 6:7], func=AF.Sqrt, bias=EPS, scale=1.0 / 512))
                        yield
                        r2 = DVE([q1, q2], lambda: nc.vector.reciprocal(out=st_[:, 4:6], in_=st_[:, 4:6]))
                        r4 = DVE(q3, lambda: nc.vector.reciprocal(out=st_[:, 7:8], in_=st_[:, 7:8]))
                        d2 = DVE([r4, vn_free[ti]], lambda: nc.vector.scalar_tensor_tensor(
                            out=vn[ti][:], in0=gv[ti][:], scalar=st_[:, 7:8], in1=gvrep[:], op0=ALU.mult, op1=ALU.mult))
                        gv_free[ti] = d2
                        c1 = ACT([r2, zsb_free[ti]], lambda: nc.scalar.activation(
                            out=zsb[ti][:, 0:256], in_=zraw[ti][:, 0:256], func=AF.Copy, scale=st_[:, 4:5]))
                        c2 = ACT(None, lambda: nc.scalar.activation(
                            out=zsb[ti][:, 256:384], in_=zraw[ti][:, 256:384], func=AF.Copy, scale=st_[:, 5:6]))
                        zraw_free[ti] = c2
                        yield
                        tp2 = None
                        for k in range(3):
                            tp2 = PE([c1, c2, P['pT2_free']] if k == 0 else None,
                                     lambda k=k: nc.tensor.transpose(pT2[:, k * 128:(k + 1) * 128], zsb[ti][:, k * 128:(k + 1) * 128], ident_b[:]),
                                     sig=(k == 2))
                        zsb_free[ti] = tp2
                        PE([P['pss_free'], ready], lambda: nc.tensor.matmul(pss, lhsT=bsp[:, :], rhs=egrp[:, :], start=True, stop=False), sig=False)
                        m_s = None
                        for gg in range(8):
                            m_s = PE(d2 if gg == 0 else None,
                                     lambda gg=gg: nc.tensor.matmul(pss[:, gg * 64:(gg + 1) * 64], lhsT=wsp[:, gg, :],
                                                                    rhs=vn[ti][:, gg * 64:(gg + 1) * 64], start=False, stop=(gg == 7)),
                                     sig=(gg == 7))
                        vn_free[ti] = m_s
                        yield
                        e1 = DVE([tp2, cq_free[gi] if t == 0 else None], lambda: nc.vector.tensor_copy(
                            out=cqnT[gi][:, :, ts_], in_=pT2[:, 0:256].rearrange("p (k c) -> p k c", k=2)))
                        e2 = DVE([ckv_free[gi] if t == 0 else None], lambda: nc.vector.tensor_copy(
                            out=ckvnT[gi][:, ts_], in_=pT2[:, 256:384]))
                        P['pT2_free'] = e2
                        G['cq_w'].append(e1)
                        G['ckv_w'].append(e2)
                        d3 = DVE([m_s, g1, sraw_free[ti]], lambda: nc.vector.tensor_tensor(out=sraw[ti][:], in0=gu[ti][:], in1=pss, op=ALU.mult))
                        P['pss_free'] = d3
                        gu_free[ti] = d3
                        yield
                        a4 = ACT(d3, lambda: nc.scalar.activation(out=junk[:, 0:512], in_=sraw[ti][:], func=AF.Square, accum_out=st_[:, 8:9]))
                        yield
                        q4 = ACT(a4, lambda: nc.scalar.activation(out=st_[:, 9:10], in_=st_[:, 8:9], func=AF.Sqrt, bias=EPS, scale=1.0 / 512))
                        yield
                        r5 = DVE(q4, lambda: nc.vector.reciprocal(out=st_[:, 9:10], in_=st_[:, 9:10]))
                        yield
                        c3 = ACT([r5, sn_free[ti]], lambda: nc.scalar.activation(out=sn[ti][:], in_=sraw[ti][:], func=AF.Copy, scale=st_[:, 9:10]))
                        sraw_free[ti] = c3
                        sn_free[ti] = DMA('sn%d' % ti, c3, lambda: nc.sync.dma_start(out=sn_d[tok0:tok0 + 128, :], in_=sn[ti][:]))
                        if t != 3:
                            return
                        yield
                        gs = slice(g * 512, (g + 1) * 512)
                        cq_free[gi] = DMA('cq%d' % gi, G['cq_w'], lambda: nc.sync.dma_start(
                            out=cq_d[s * NG + g], in_=cqnT[gi][:].rearrange("p k c -> p (k c)")))
                        bank_free = [P['pkr_free'], P['pks_free']]
                        banks = [pkr, pks]
                        for h in range(8):
                            bi = h % 2
                            mk = mmg(banks[bi][0:64, :], [(wkv[:, h * 128:h * 128 + 64], ckvnT[gi][:, :])], [G['ckv_w'], bank_free[bi]])
                            if h % 2 == 0:
                                bank_free[bi] = ACT(mk, lambda h=h, bi=bi: nc.scalar.copy(out=KT[0:64, h, gs], in_=banks[bi][0:64, :]))
                            else:
                                bank_free[bi] = DVE(mk, lambda h=h, bi=bi: nc.vector.tensor_copy(out=KT[0:64, h, gs], in_=banks[bi][0:64, :]))
                        wkv3 = wkv[:].rearrange("p (h c) -> p h c", h=8)[:, :, 64:128]
                        mv = None
                        for tt in range(4):
                            bi = tt % 2
                            kb = g * 4 + tt
                            mv = mmg(banks[bi][:, :].rearrange("p (h c) -> p h c", h=8), [(ckvnT[gi][:, tt * 128:(tt + 1) * 128], wkv3)], [bank_free[bi]])
                            bank_free[bi] = DVE(mv, lambda kb=kb, bi=bi: nc.vector.tensor_copy(
                                out=VA[:, kb, :, 0:64], in_=banks[bi][:, :].rearrange("p (h c) -> p h c", h=8)))
                        ckv_free[gi] = mv
                        P['pkr_free'] = bank_free[0]
                        P['pks_free'] = bank_free[1]

                    interleave((p1_tile(g, t) for g in range(NG) for t in range(4)), 2)
                    p1_bar = dp.last() + [sn_free, cq_free]
                    dp.retire_since(mk1)

                mk2 = dp.mark()
                with ExitStack() as p2:
                    def sb2(name, shape, dt): return p2.enter_context(nc.sbuf_tensor(U(name), shape, dt))
                    wq = sb2("wq", [128, 2, 768], BF16)
                    wqs = sb2("wqs", [128, 2, 768], BF16)
                    wo = sb2("wo", [128, 8, 1024], BF16)
                    cqT = [sb2("cqT%d" % i, [128, 2, 512], BF16) for i in range(2)]
                    Ct = sb2("Ct2", [128, 512], F32)
                    St = sb2("St2", [128, 512], F32)
                    qt1 = [sb2("qt1_%d" % i, [128, 512], F32) for i in range(2)]
                    qt2 = [sb2("qt2_%d" % i, [128, 512], F32) for i in range(2)]
                    qt_free = [None, None]
                    QT = sb2("QT", [128, 8, 512], BF16)
                    pTs = [sb2("pTs%d" % i, [128, 1024], BF16) for i in range(3)]
                    osb = [sb2("osb%d" % i, [128, 512], F32) for i in range(2)]
                    rinv = [sb2("rinv0", [128, 512], F32)] * 2
                    aT = [sb2("aT%d" % i, [128, 512], BF16) for i in range(2)]
                    merged = [sb2("merged%d" % i, [128, D], BF16) for i in range(4)]
                    mT = [sb2("mT%d" % i, [128, 8, 128], BF16) for i in range(2)]
                    otmp = sb2("otmp", [128, D], F32)
                    xr = [sb2("xr%d" % i, [128, D], F32) for i in range(2)]
                    x1 = [sb2("x1_%d" % i, [128, D], F32) for i in range(2)]
                    junk = sb2("junk2", [128, D], BF16)
                    stt = [sb2("stq%d" % i, [128, 16], F32) for i in range(2)]

                    po = PC[:, 0:512]
                    prb = PC[:, 512:1024]
                    pmT = PC[:, 512:1024].bitcast(BF16)
                    pa = PD[:, :].bitcast(BF16)
                    scT = [PA, PB]

                    wl2 = [DMA('b1', p1_bar, lambda: nc.sync.dma_start(out=wq[:].rearrange("p k c -> p (k c)"), in_=wq_d)),
                           DMA('b1', p1_bar, lambda: nc.sync.dma_start(out=wqs[:].rearrange("p k c -> p (k c)"), in_=wqsw_d)),
                           DMA('b1', p1_bar, lambda: nc.sync.dma_start(out=wo[:].rearrange("p k c -> p (k c)"), in_=wo_d))]
                    ready2 = [p1_bar, wl2]
                    cq_free2 = [None, None]; rope_free = None; QT_free = []
                    sc_free = [None, None]; pTs_free = [None, None, None]; po_free = None; osb_free = [None, None]
                    rinv_free = [None]; prb_free = None; aT_free = [None, None]; pa_free = []
                    merged_free = [None] * 4; mT_free = [None, None]; pmT_free = None
                    otmp_free = None; xr_free = [None, None]; x1_free = [None, None]
                    step = 0
                    tcount = 0
                    for qg in range(NQG[s]):
                        gi = qg % 2
                        gs = slice(qg * 512, (qg + 1) * 512)
                        lq = DMA('cql%d' % gi, [cq_free2[gi], ready2], lambda gi=gi, qg=qg: nc.sync.dma_start(
                            out=cqT[gi][:].rearrange("p k c -> p (k c)"), in_=cq_d[s * NG + qg]))
                        lr1 = DMA('rp2c', [rope_free, ready2], lambda gs=gs: nc.sync.dma_start(out=Ct[64:96, :], in_=rope_c[s, :, gs]))
                        lr2 = DMA('rp2s', [rope_free, ready2], lambda gs=gs: nc.sync.dma_start(out=St[64:96, :], in_=rope_s[s, :, gs]))
                        pre_ld = []
                        for t in range(4):
                            tok0_ = s * S + qg * 512 + t * 128
                            l_sn = DMA('snl%d' % t, [merged_free[t], ready2], lambda t=t, tok0_=tok0_: nc.sync.dma_start(
                                out=merged[t][:, 512:1024], in_=sn_d[tok0_:tok0_ + 128, :]))
                            pre_ld.append(l_sn)
                        wq4 = wq[:].rearrange("p k (h c) -> p k h c", h=8)
                        wqs4 = wqs[:].rearrange("p k (h c) -> p k h c", h=8)
                        QT_w = []
                        Qs = dict(mqs=None, qd=None)

                        def q_head(h):
                            T = scT[h % 2]
                            hi = h % 2
                            mq = mmg(T[0:96, 0:512], [(wq4[:, k, h, :], cqT[gi][:, k, :]) for k in range(2)], [lq, sc_free[h % 2]])
                            mqs = mmg(T[0:96, 512:1024], [(wqs4[:, k, h, :], cqT[gi][:, k, :]) for k in range(2)], None)
                            Qs['mqs'] = mqs
                            yield
                            c0 = ACT([mq, QT_free if h == 0 else None], lambda: nc.scalar.copy(out=QT[0:64, h, :], in_=T[0:64, 0:512]))
                            q1 = DVE([mq, lr1, qt_free[hi]], lambda: nc.vector.tensor_tensor(out=qt1[hi][64:96, :], in0=T[64:96, 0:512], in1=Ct[64:96, :], op=ALU.mult))
                            q2 = DVE([mqs, lr2], lambda: nc.vector.tensor_tensor(out=qt2[hi][64:96, :], in0=T[64:96, 512:1024], in1=St[64:96, :], op=ALU.mult))
                            sc_free[h % 2] = [c0, q2]
                            yield
                            qd = DVE([q1, q2, QT_free if h == 0 else None], lambda: nc.vector.tensor_tensor(
                                out=QT[64:96, h, :], in0=qt1[hi][64:96, :], in1=qt2[hi][64:96, :], op=ALU.add))
                            qt_free[hi] = qd
                            Qs['qd'] = qd
                            QT_w.extend([c0, qd])
                        interleave((q_head(h) for h in range(8)), 2)
                        mqs = Qs['mqs']
                        qd = Qs['qd']
                        cq_free2[gi] = mqs
                        rope_free = qd
                        NP = KB // 2
                        steps = [(h, j) for h in range(8) for j in range(NP)]
                        qk_tok = {}

                        def emit_qk(idx):
                            h, j = steps[idx]
                            T = scT[idx % 2]
                            tk = None
                            for u in range(2):
                                kb = 2 * j + u
                                tk = PE([sc_free[idx % 2], QT_w] if u == 0 else None,
                                        lambda h=h, kb=kb, u=u, T=T: nc.tensor.matmul(
                                            T[:, u * 512:(u + 1) * 512], lhsT=KT[0:96, h, kb * 128:(kb + 1) * 128], rhs=QT[0:96, h, :],
                                            start=True, stop=True), sig=(u == 1))
                            qk_tok[idx] = tk

                        emit_qk(0)
                        pa_w = []
                        QT_readers = []
                        pending = []
                        A = dict(prb_free=prb_free)
                        for idx, (h, j) in enumerate(steps):
                            if idx + 1 < len(steps):
                                emit_qk(idx + 1)
                            T = scT[idx % 2]
                            sl = step % 3
                            step += 1
                            ex = ACT([qk_tok[idx], pTs_free[sl]], lambda T=T, sl=sl: nc.scalar.activation(out=pTs[sl][:], in_=T[:, :], func=AF.Exp))
                            sc_free[idx % 2] = ex
                            pvt = None
                            for u in range(2):
                                kb = 2 * j + u
                                pvt = PE([ex, po_free if (j == 0 and u == 0) else None],
                                         lambda h=h, kb=kb, u=u, sl=sl: nc.tensor.matmul(
                                             po[0:65, :], lhsT=VA[:, kb, h, :], rhs=pTs[sl][:, u * 512:(u + 1) * 512],
                                             start=(kb == 0), stop=(kb == KB - 1)), sig=(u == 1))
                            pTs_free[sl] = pvt
                            for pend in list(pending):
                                pend[0] -= 1
                                if pend[0] <= 0:
                                    pend[1]()
                                    pending.remove(pend)
                            if j == NP - 1:
                                for pend in list(pending):
                                    pend[1]()
                                    pending.remove(pend)
                                oi = h % 2
                                QT_readers.append(pvt)
                                o1 = DVE([pvt, osb_free[oi]], lambda oi=oi: nc.vector.tensor_copy(out=osb[oi][0:65, :], in_=po[0:65, :]))
                                po_free = o1
                                o2 = DVE([o1, rinv_free[0]], lambda oi=oi: nc.vector.reciprocal(out=rinv[oi][64:65, :], in_=osb[oi][64:65, :]))
                                hs = dict(o2=o2, oi=oi, h=h)

                                def part_a(hs=hs):
                                    oi = hs['oi']
                                    o3 = PE([hs['o2'], A['prb_free']], lambda oi=oi: nc.tensor.matmul(prb[0:64, :], lhsT=ones_f[64:65, 0:64], rhs=rinv[oi][64:65, :],
                                                                                                 start=True, stop=True))
                                    rinv_free[0] = o3
                                    o4 = DVE([o3, aT_free[oi]], lambda oi=oi: nc.vector.tensor_tensor(out=aT[oi][0:64, :], in0=osb[oi][0:64, :],
                                                                                                       in1=prb[0:64, :], op=ALU.mult))
                                    A['prb_free'] = o4
                                    osb_free[oi] = o4
                                    hs['o4'] = o4

                                def part_b(hs=hs):
                                    oi = hs['oi']; h = hs['h']
                                    o5 = None
                                    for t in range(4):
                                        o5 = PE([hs['o4'], pa_free if h == 0 else None] if t == 0 else None,
                                                lambda t=t, h=h, oi=oi: nc.tensor.transpose(
                                                    pa[:, t * 512 + h * 64: t * 512 + (h + 1) * 64], aT[oi][0:64, t * 128:(t + 1) * 128], ident_b[0:64, 0:64]),
                                                sig=(t == 3))
                                    aT_free[oi] = o5
                                    pa_w.append(o5)
                                pending.append([2, part_a])
                                pending.append([4, part_b])
                        for pend in list(pending):
                            pend[1]()
                            pending.remove(pend)
                        prb_free = A['prb_free']
                        QT_free = QT_readers
                        pa_r = []
                        M = dict(pmT_free=pmT_free, prb_free=prb_free, otmp_free=otmp_free)

                        def mg_tile(t, ti):
                            tok0 = s * S + qg * 512 + t * 128
                            otok0 = qbase + qg * 512 + t * 128
                            st_ = stt[ti]
                            lsn = pre_ld[t]
                            lxr = DMA('xr%d' % ti, [xr_free[ti], ready2], lambda: nc.sync.dma_start(
                                out=xr[ti][:], in_=xs[tok0:tok0 + 128, :]))
                            a1 = ACT(pa_w, lambda: nc.scalar.activation(out=junk[:, 0:512], in_=pa[:, t * 512:(t + 1) * 512], func=AF.Square,
                                                                        accum_out=st_[:, 0:1]))
                            yield
                            r1a = ACT(a1, lambda: nc.scalar.activation(out=st_[:, 1:2], in_=st_[:, 0:1], func=AF.Sqrt, bias=EPS, scale=1.0 / 512))
                            yield
                            r1 = DVE(r1a, lambda: nc.vector.reciprocal(out=st_[:, 1:2], in_=st_[:, 1:2]))
                            c1 = ACT([r1, merged_free[t]], lambda: nc.scalar.activation(
                                out=merged[t][:, 0:512], in_=pa[:, t * 512:(t + 1) * 512], func=AF.Copy, scale=st_[:, 1:2]))
                            pa_r.append(c1)
                            yield
                            tp = None
                            for k in range(8):
                                tp = PE([c1, lsn, M['pmT_free'], M['prb_free']] if k == 0 else None,
                                        lambda k=k: nc.tensor.transpose(pmT[:, k * 128:(k + 1) * 128], merged[t][:, k * 128:(k + 1) * 128], ident_b[:]),
                                        sig=(k == 7))
                            merged_free[t] = tp
                            yield
                            ev = DVE([tp, mT_free[ti]], lambda: nc.vector.tensor_copy(out=mT[ti][:].rearrange("p k c -> p (k c)"), in_=pmT))
                            M['pmT_free'] = ev
                            M['prb_free'] = ev
                            yield
                            T = scT[t % 2]
                            mo1 = mmg(T[:, 0:512], [(mT[ti][:, k, :], wo[:, k, 0:512]) for k in range(8)], [ev, sc_free[t % 2]])
                            mo2 = mmg(T[:, 512:1024], [(mT[ti][:, k, :], wo[:, k, 512:1024]) for k in range(8)], None)
                            mT_free[ti] = mo2
                            yield
                            a2 = ACT(mo2, lambda: nc.scalar.activation(out=junk[:], in_=T[:, :], func=AF.Square, accum_out=st_[:, 2:3]))
                            yield
                            r2a = ACT(a2, lambda: nc.scalar.activation(out=st_[:, 3:4], in_=st_[:, 2:3], func=AF.Sqrt, bias=EPS, scale=1.0 / D))
                            yield
                            r2 = DVE(r2a, lambda: nc.vector.reciprocal(out=st_[:, 3:4], in_=st_[:, 3:4]))
                            d1 = DVE([r2, M['otmp_free']], lambda: nc.vector.scalar_tensor_tensor(
                                out=otmp[:], in0=T[:, :], scalar=st_[:, 3:4], in1=gvec1[:], op0=ALU.mult, op1=ALU.mult))
                            sc_free[t % 2] = d1
                            pp = POOL([d1, lxr, x1_free[ti]], lambda: nc.gpsimd.tensor_tensor(out=x1[ti][:], in0=otmp[:], in1=xr[ti][:], op=ALU.add))
                            M['otmp_free'] = pp
                            xr_free[ti] = pp
                            x1_free[ti] = DMA('x1s%d' % ti, pp, lambda: nc.sync.dma_start(
                                out=x1_d[otok0:otok0 + 128, :], in_=x1[ti][:]))
                        interleave((mg_tile(t, (tcount + t) % 2) for t in range(4)), 2)
                        tcount += 4
                        pmT_free = M['pmT_free']; prb_free = M['prb_free']; otmp_free = M['otmp_free']
                        pa_free = pa_r
                    p2_bar = dp.last() + [x1_free]
                    dp.retire_since(mk2)
                seq_bar = p2_bar
                qbase += NQG[s] * 512
            stageA_bar = seq_bar

        NTT = NQT // 128
        gseq = []
        for s in range(NSEQ):
            gseq += [s] * (NQG[s] * 4)
        with ExitStack() as bes:
            def sbB(name, shape, dt): return bes.enter_context(nc.sbuf_tensor(U(name), shape, dt))
            geff2_ = [sbB("geff2_%d" % i, [128, D], F32) for i in range(2)]
            sh2r_ = [sbB("sh2r_%d" % i, [128, D], F32) for i in range(2)]
            gvec2_ = [sbB("gvec2_%d" % i, [128, D], F32) for i in range(2)]
            vt = [sbB("vtB%d" % i, [128, D], F32) for i in range(2)]
            M1a = sbB("M1a", [128, NTT, 32], F32)
            M2a = sbB("M2a", [128, NTT, 32], F32)
            W1a = sbB("W1a", [128, NTT], F32)
            W2a = sbB("W2a", [128, NTT], F32)
            R1a = sbB("R1a", [128, NTT], F32)
            R2a = sbB("R2a", [128, NTT], F32)
            slot0 = sbB("slot0", [128, NTT], I32)
            slot1 = sbB("slot1", [128, NTT], I32)
            idxw = sbB("idxw", [128, NSL], I32)
            carry = sbB("carry", [128, 32], F32)
            PS = [bes.enter_context(nc.psum_tensor(U("PS%d" % i), [128, 512], F32)) for i in range(8)]
            bb = stageA_bar
            readyB = [bb, wcast_tok, zero_tok]

            def load_vecs(s, deps):
                geff2 = geff2_[s % 2]; sh2r = sh2r_[s % 2]; gvec2 = gvec2_[s % 2]
                l1 = DMA('v0_0', deps, lambda s=s: nc.sync.dma_start(out=vt[0][:], in_=mod_d[s, 4096:5120].partition_broadcast(128)))
                l2 = DMA('v0_1', deps, lambda: nc.sync.dma_start(out=vt[1][:], in_=g_pre2.partition_broadcast(128)))
                l3 = DMA('v0_2', deps, lambda s=s: nc.sync.dma_start(out=sh2r[:], in_=mod_d[s, 3072:4096].partition_broadcast(128)))
                tg = DVE([l1, l2], lambda: nc.vector.scalar_tensor_tensor(out=geff2[:], in0=vt[0][:], scalar=1.0, in1=vt[1][:],
                                                                           op0=ALU.add, op1=ALU.mult))
                l4 = DMA('v0_3', [tg], lambda s=s: nc.sync.dma_start(out=vt[0][:], in_=mod_d[s, 5120:6144].partition_broadcast(128)))
                l5 = DMA('v0_4', [tg], lambda: nc.sync.dma_start(out=vt[1][:], in_=g_post2.partition_broadcast(128)))
                tg2 = DVE([l4, l5], lambda: nc.vector.tensor_tensor(out=gvec2[:], in0=vt[0][:], in1=vt[1][:], op=ALU.mult))
                return [l3, tg, tg2]

            mkb1 = dp.mark()
            with ExitStack() as b1:
                def sb1(name, shape, dt): return b1.enter_context(nc.sbuf_tensor(U(name), shape, dt))
                w_r = sb1("w_r", [128, 8, 36], F32)
                brr = sb1("brr", [128, 36], F32)
                utri = sb1("utri", [128, 128], BF16)
                onesb = sb1("onesb", [128, 128], BF16)
                x1t = [sb1("x1t%d" % i, [128, D], F32) for i in range(5)]
                junk = sb1("junkB", [128, D], BF16)
                hm = sb1("hmB", [128, D], F32)
                h2 = [sb1("h2_%d" % i, [128, D], F32) for i in range(5)]
                h2Tf = [sb1("h2Tf%d" % i, [128, 8, 128], F32) for i in range(5)]
                stt = [sb1("stB%d" % i, [128, 8], F32) for i in range(5)]
                lg = [sb1("lg%d" % i, [128, 36], F32) for i in range(5)]
                wk = [sb1("wk%d" % i, [128, 192], F32) for i in range(5)]
                ohb = [sb1("ohb%d" % i, [128, 32], BF16) for i in range(5)]
                PH = [PS[6], PS[7]]
                ld = [DMA('s0', bb, lambda: nc.sync.dma_start(out=w_r[:, :, 0:4], in_=w_rg.rearrange("(k p) c -> p k c", p=128))),
                      DMA('s0', bb, lambda: nc.sync.dma_start(out=w_r[:, :, 4:36], in_=w_re.rearrange("(k p) c -> p k c", p=128))),
                      DMA('s0', bb, lambda: nc.sync.dma_start(out=brr[:, 0:4], in_=b_rg.partition_broadcast(128))),
                      DMA('s0', bb, lambda: nc.sync.dma_start(out=brr[:, 4:36], in_=b_re.partition_broadcast(128))),
                      DMA('s1', bb, lambda: nc.gpsimd.dma_start(out=utri[:], in_=utri_in), q='pool'),
                      POOL(bb, lambda: nc.gpsimd.memset(onesb[:], 1.0)),
                      POOL(bb, lambda: nc.gpsimd.memset(carry[:], 0.0))]
                rdy1 = [readyB, ld]
                T = dict(cur_seq=-1, vec_ready=None, vec_readers=[], hm_free=None, PH_free=[None, None], plg_free=None,
                         pcum_free=None, carry_tok=ld[-1])
                x1t_free = [None] * 5; h2_free = [None] * 5
                h2Tf_free = [None] * 5
                h2d_w = []

                def p1_tile(i):
                    s = gseq[i]
                    if s != T['cur_seq']:
                        T['cur_seq'] = s
                        T['vec_ready'] = load_vecs(s, [rdy1, T['vec_readers']])
                        T['vec_readers'] = []
                    vec_ready = T['vec_ready']
                    geff2 = geff2_[s % 2]; sh2r = sh2r_[s % 2]
                    ti = i % 5
                    tok0 = i * 128
                    st_ = stt[ti]
                    lx = DMA('bx%d' % ti, [x1t_free[ti], rdy1], lambda ti=ti, tok0=tok0: nc.sync.dma_start(out=x1t[ti][:], in_=x1_d[tok0:tok0 + 128, :]))
                    a1 = ACT(lx, lambda ti=ti, st_=st_: nc.scalar.activation(out=junk[:], in_=x1t[ti][:], func=AF.Square, accum_out=st_[:, 0:1]))
                    yield
                    r1a = ACT(a1, lambda st_=st_: nc.scalar.activation(out=st_[:, 1:2], in_=st_[:, 0:1], func=AF.Sqrt, bias=EPS, scale=1.0 / D))
                    yield
                    r1 = DVE(r1a, lambda st_=st_: nc.vector.reciprocal(out=st_[:, 1:2], in_=st_[:, 1:2]))
                    d1 = DVE([r1, T['hm_free'], vec_ready], lambda ti=ti, st_=st_: nc.vector.scalar_tensor_tensor(
                        out=hm[:], in0=x1t[ti][:], scalar=st_[:, 1:2], in1=geff2[:], op0=ALU.mult, op1=ALU.mult))
                    x1t_free[ti] = d1
                    p1_ = POOL([d1, h2_free[ti], vec_ready], lambda ti=ti: nc.gpsimd.tensor_tensor(out=h2[ti][:], in0=hm[:], in1=sh2r[:], op=ALU.add))
                    T['hm_free'] = p1_
                    T['vec_readers'] = [p1_, d1]
                    wr = DMA('h2w%d' % ti, p1_, lambda ti=ti, tok0=tok0: nc.gpsimd.dma_start(out=h2_d[tok0:tok0 + 128, :], in_=h2[ti][:]), q='pool')
                    h2d_w.append(wr)
                    yield
                    tp = None
                    for k in range(8):
                        bank = PH[k // 4]
                        tp = PE([p1_, T['PH_free']] if k == 0 else None,
                                lambda k=k, ti=ti, bank=bank: nc.tensor.transpose(bank[:, (k % 4) * 128:(k % 4 + 1) * 128],
                                                                                  h2[ti][:, k * 128:(k + 1) * 128], ident_f[:]),
                                sig=(k == 7))
                    h2_free[ti] = [tp, wr]
                    yield
                    e1 = ACT([tp, h2Tf_free[ti]], lambda ti=ti: nc.scalar.copy(out=h2Tf[ti][:, 0:4, :], in_=PH[0][:, :].rearrange("p (k c) -> p k c", k=4)))
                    e2 = DVE([tp, h2Tf_free[ti]], lambda ti=ti: nc.vector.tensor_copy(out=h2Tf[ti][:, 4:8, :], in_=PH[1][:, :].rearrange("p (k c) -> p k c", k=4)))
                    T['PH_free'] = [e1, e2]
                    yield
                    plg = PS[4][:, 0:36]
                    m_l = mmg(plg, [(h2Tf[ti][:, k, :], w_r[:, k, :]) for k in range(8)], [e1, e2, T['plg_free'], rdy1])
                    h2Tf_free[ti] = m_l
                    yield
                    L = lg[ti]; W = wk[ti]
                    v1 = DVE([m_l], lambda L=L: nc.vector.tensor_tensor(out=L[:], in0=plg, in1=brr[:], op=ALU.add))
                    T['plg_free'] = v1
                    v2 = DVE(v1, lambda L=L, W=W: nc.vector.tensor_reduce(out=W[:, 0:1], in_=L[:, 0:4], axis=AX.X, op=ALU.max))
                    v3 = DVE(v2, lambda W=W: nc.vector.tensor_scalar(out=W[:, 1:2], in0=W[:, 0:1], scalar1=-1.0, scalar2=None, op0=ALU.mult))
                    v4 = DVE(v2, lambda L=L, W=W: nc.vector.tensor_scalar(out=W[:, 4:8], in0=L[:, 0:4], scalar1=W[:, 0:1], scalar2=None, op0=ALU.is_equal))
                    s1 = ACT([v3], lambda L=L, W=W: nc.scalar.activation(out=W[:, 8:12], in_=L[:, 0:4], func=AF.Exp, bias=W[:, 1:2], scale=1.0,
                                                                         accum_out=W[:, 2:3]))
                    yield
                    v5 = DVE(s1, lambda W=W: nc.vector.reciprocal(out=W[:, 3:4], in_=W[:, 2:3]))
                    v6 = DVE(v4, lambda L=L, W=W: nc.vector.tensor_tensor(
                        out=W[:, 16:48].rearrange("p (g e) -> p g e", g=4), in0=L[:, 4:36].rearrange("p (g e) -> p g e", g=4),
                        in1=W[:, 4:8].unsqueeze(2).to_broadcast([128, 4, 8]), op=ALU.mult))
                    v7 = DVE(v6, lambda W=W: nc.vector.tensor_reduce(out=W[:, 48:56], in_=W[:, 16:48].rearrange("p (g e) -> p e g", g=4),
                                                                    axis=AX.X, op=ALU.add))
                    v8 = DVE(v7, lambda W=W: nc.vector.tensor_reduce(out=W[:, 12:13], in_=W[:, 48:56], axis=AX.X, op=ALU.max))
                    v9 = DVE(v8, lambda W=W: nc.vector.tensor_scalar(out=W[:, 56:64], in0=W[:, 48:56], scalar1=W[:, 12:13], scalar2=None,
                                                                    op0=ALU.is_equal))
                    v10 = DVE(v9, lambda W=W: nc.vector.scalar_tensor_tensor(out=W[:, 64:72], in0=W[:, 56:64], scalar=-1e30, in1=W[:, 48:56],
                                                                            op0=ALU.mult, op1=ALU.add))
                    v11 = DVE(v10, lambda W=W: nc.vector.tensor_reduce(out=W[:, 13:14], in_=W[:, 64:72], axis=AX.X, op=ALU.max))
                    v12 = DVE(v11, lambda W=W: nc.vector.tensor_scalar(out=W[:, 72:80], in0=W[:, 64:72], scalar1=W[:, 13:14], scalar2=None,
                                                                      op0=ALU.is_equal))
                    v13 = DVE(v11, lambda W=W: nc.vector.tensor_scalar(out=W[:, 14:15], in0=W[:, 12:13], scalar1=-1.0, scalar2=None, op0=ALU.mult))
                    s2 = ACT([v13], lambda W=W: nc.scalar.activation(out=W[:, 15:16], in_=W[:, 13:14], func=AF.Exp, bias=W[:, 14:15], scale=1.0))
                    yield
                    v14 = DVE(s2, lambda W=W: nc.vector.tensor_scalar(out=W[:, 80:81], in0=W[:, 15:16], scalar1=1.0, scalar2=None, op0=ALU.add))
                    v15 = DVE(v14, lambda W=W: nc.vector.reciprocal(out=W[:, 81:82], in_=W[:, 80:81]))
                    v16 = DVE([v15, v5], lambda W=W, i=i: nc.vector.tensor_tensor(out=W1a[:, i:i + 1], in0=W[:, 81:82], in1=W[:, 3:4], op=ALU.mult))
                    v17 = DVE(v16, lambda W=W, i=i: nc.vector.tensor_tensor(out=W2a[:, i:i + 1], in0=W1a[:, i:i + 1], in1=W[:, 15:16], op=ALU.mult))
                    v18 = DVE([v9, v4], lambda W=W, i=i: nc.vector.tensor_tensor(
                        out=M1a[:, i, :].rearrange("p (g e) -> p g e", g=4), in0=W[:, 4:8].unsqueeze(2).to_broadcast([128, 4, 8]),
                        in1=W[:, 56:64].unsqueeze(1).to_broadcast([128, 4, 8]), op=ALU.mult))
                    v19 = DVE([v12], lambda W=W, i=i: nc.vector.tensor_tensor(
                        out=M2a[:, i, :].rearrange("p (g e) -> p g e", g=4), in0=W[:, 4:8].unsqueeze(2).to_broadcast([128, 4, 8]),
                        in1=W[:, 72:80].unsqueeze(1).to_broadcast([128, 4, 8]), op=ALU.mult))
                    OH = ohb[ti]
                    v20 = DVE([v18, v19, T['pcum_free']], lambda OH=OH, i=i: nc.vector.tensor_tensor(out=OH[:], in0=M1a[:, i, :], in1=M2a[:, i, :], op=ALU.add))
                    pcum = PS[5][:, 0:32]
                    ptot = PS[5][:, 32:64]
                    PE([v20, T['pcum_free'], rdy1], lambda OH=OH: nc.tensor.matmul(pcum, lhsT=utri[:], rhs=OH[:], start=True, stop=True), sig=False)
                    mc = PE(None, lambda OH=OH: nc.tensor.matmul(ptot, lhsT=onesb[:], rhs=OH[:], start=True, stop=True))
                    yield
                    v21 = DVE([mc, T['carry_tok']], lambda W=W: nc.vector.tensor_tensor(out=W[:, 96:128], in0=carry[:], in1=pcum, op=ALU.add))
                    v22 = DVE(v21, lambda: nc.vector.tensor_tensor(out=carry[:], in0=carry[:], in1=ptot, op=ALU.add))
                    T['carry_tok'] = v22
                    T['pcum_free'] = v22
                    v23 = DVE(v22, lambda W=W, i=i: nc.vector.tensor_tensor(out=W[:, 128:160], in0=W[:, 96:128], in1=M1a[:, i, :], op=ALU.mult))
                    v24 = DVE(v23, lambda W=W, i=i: nc.vector.tensor_reduce(out=R1a[:, i:i + 1], in_=W[:, 128:160], axis=AX.X, op=ALU.add))
                    v25 = DVE(v24, lambda W=W, i=i: nc.vector.tensor_tensor(out=W[:, 160:192], in0=W[:, 96:128], in1=M2a[:, i, :], op=ALU.mult))
                    v26 = DVE(v25, lambda W=W, i=i: nc.vector.tensor_reduce(out=R2a[:, i:i + 1], in_=W[:, 160:192], axis=AX.X, op=ALU.add))
                interleave((p1_tile(i) for i in range(NTT)), 5)
                b1_bar = dp.last() + [h2d_w]
                dp.retire_since(mkb1)

            with ExitStack() as b2:
                def sb2(name, shape, dt): return b2.enter_context(nc.sbuf_tensor(U(name), shape, dt))
                jv = sb2("jv", [128, NSL], F32)
                pidx = sb2("pidx", [128, 1], F32)
                tri32 = sb2("tri32", [32, 32], F32)
                cmp_ = sb2("cmp", [128, NSL * 32], F32)
                tmpM = sb2("tmpM", [128, NTT, 32], F32)
                nblk = sb2("nblk", [128, 32], F32)
                pc = sb2("pc", [128, 32], F32)
                pcT = sb2("pcT", [32, 128], F32)
                sst = sb2("sst", [128, 32], F32)
                send = sb2("send", [128, 32], F32)
                te = sb2("te", [128, NSL], F32)
                sf = sb2("sf", [128, NTT], F32)
                l = [DMA('i0', b1_bar, lambda: nc.sync.dma_start(out=jv[:], in_=jv_in)),
                     DMA('i0', b1_bar, lambda: nc.sync.dma_start(out=pidx[:], in_=pidx_in)),
                     DMA('i0', b1_bar, lambda: nc.sync.dma_start(out=tri32[:], in_=tri32_in))]
                c3 = cmp_[:].rearrange("p (e m) -> p e m", e=32)
                q1 = DVE([l, b1_bar], lambda: nc.vector.tensor_tensor(out=c3, in0=jv[:].unsqueeze(1).to_broadcast([128, 32, NSL]),
                                                                      in1=carry[:].unsqueeze(2).to_broadcast([128, 32, NSL]), op=ALU.is_lt))
                q2 = DVE(q1, lambda: nc.vector.tensor_reduce(out=nblk[:], in_=c3, axis=AX.X, op=ALU.add))
                q3 = DVE(q2, lambda: nc.vector.tensor_scalar(out=pc[:], in0=nblk[:], scalar1=128.0, scalar2=None, op0=ALU.mult))
                q4 = PE(q3, lambda: nc.tensor.transpose(PS[0][0:32, 0:128], pc[:, :], ident_f[:]))
                q5 = ACT(q4, lambda: nc.scalar.copy(out=pcT[:], in_=PS[0][0:32, 0:128]))
                q6 = PE([q5, l], lambda: nc.tensor.matmul(PS[1][:, 0:32], lhsT=pcT[:, :], rhs=tri32[:, :], start=True, stop=True))
                q7 = DVE(q6, lambda: nc.vector.tensor_copy(out=sst[:], in_=PS[1][:, 0:32]))
                q8 = DVE(q7, lambda: nc.vector.tensor_tensor(out=send[:], in0=sst[:], in1=pc[:], op=ALU.add))
                c4 = cmp_[:].rearrange("p (m e) -> p m e", e=32)
                q9 = DVE(q8, lambda: nc.vector.tensor_tensor(out=c4, in0=send[:].unsqueeze(1).to_broadcast([128, NSL, 32]),
                                                             in1=jv[:].unsqueeze(2).to_broadcast([128, NSL, 32]), op=ALU.is_le))
                q10 = DVE(q9, lambda: nc.vector.tensor_reduce(out=te[:], in_=c4, axis=AX.X, op=ALU.add))
                q11 = DVE(q10, lambda: nc.vector.tensor_scalar(out=te[:], in0=te[:], scalar1=31.0, scalar2=128.0, op0=ALU.min, op1=ALU.mult))
                q12 = DVE(q11, lambda: nc.vector.tensor_scalar(out=te[:], in0=te[:], scalar1=pidx[:, 0:1], scalar2=None, op0=ALU.add))
                q13 = DVE(q12, lambda: nc.vector.tensor_copy(out=idxw[:], in_=te[:]))
                q14 = DVE(q7, lambda: nc.vector.tensor_tensor(out=tmpM[:], in0=M1a[:], in1=sst[:].unsqueeze(1).to_broadcast([128, NTT, 32]), op=ALU.mult))
                q15 = DVE(q14, lambda: nc.vector.tensor_reduce(out=sf[:], in_=tmpM[:], axis=AX.X, op=ALU.add))
                q16 = DVE(q15, lambda: nc.vector.tensor_tensor(out=sf[:], in0=sf[:], in1=R1a[:], op=ALU.add))
                q17 = DVE(q16, lambda: nc.vector.tensor_copy(out=slot0[:], in_=sf[:]))
                q18 = DVE(q17, lambda: nc.vector.tensor_tensor(out=tmpM[:], in0=M2a[:], in1=sst[:].unsqueeze(1).to_broadcast([128, NTT, 32]), op=ALU.mult))
                q19 = DVE(q18, lambda: nc.vector.tensor_reduce(out=sf[:], in_=tmpM[:], axis=AX.X, op=ALU.add))
                q20 = DVE(q19, lambda: nc.vector.tensor_tensor(out=sf[:], in0=sf[:], in1=R2a[:], op=ALU.add))
                q21 = DVE(q20, lambda: nc.vector.tensor_copy(out=slot1[:], in_=sf[:]))
                b2_bar = dp.last()

            mkb3 = dp.mark()
            with ExitStack() as b3:
                def sb3(name, shape, dt): return b3.enter_context(nc.sbuf_tensor(U(name), shape, dt))
                hsc = [sb3("hsc%d" % i, [128, D], BF16) for i in range(3)]
                hsc_free = [None] * 3
                sc_toks = []
                for i in range(NTT):
                    si = i % 3
                    tok0 = i * 128
                    lh = DMA('hl%d' % si, [hsc_free[si], b2_bar], lambda si=si, tok0=tok0: nc.sync.dma_start(out=hsc[si][:], in_=h2_d[tok0:tok0 + 128, :]))
                    s0 = DMA('sc%d' % si, [lh, b2_bar], lambda si=si, i=i: nc.gpsimd.indirect_dma_start(
                        out=xs_d[:, :], out_offset=bass.IndirectOffsetOnAxis(ap=slot0[:, i:i + 1], axis=0), in_=hsc[si][:, :], in_offset=None), q='pool')
                    s1_ = DMA('sc%d' % si, [lh], lambda si=si, i=i: nc.gpsimd.indirect_dma_start(
                        out=xs_d[:, :], out_offset=bass.IndirectOffsetOnAxis(ap=slot1[:, i:i + 1], axis=0), in_=hsc[si][:, :], in_offset=None), q='pool')
                    hsc_free[si] = [s0, s1_]
                    sc_toks += [s0, s1_]
                scat_done = [sc_toks[-6:], b2_bar]

                PF = 3
                NW = PF + 3
                ND = PF + 5
                NX = PF + 2
                wgu = [sb3("wgu%d" % i, [128, 8, 512], BF16) for i in range(NW)]
                wdb = [sb3("wdb%d" % i, [128, 2, D], BF16) for i in range(ND)]
                xsb = [sb3("xsb%d" % i, [128, D], BF16) for i in range(NX)]
                xT = [sb3("xT%d" % i, [128, 8, 128], BF16) for i in range(2)]
                sgs = [sb3("sgs%d" % i, [128, 256], F32) for i in range(2)]
                hid = [sb3("hid%d" % i, [128, 256], BF16) for i in range(2)]
                hT = [sb3("hT%d" % i, [128, 2, 128], BF16) for i in range(2)]
                ysb = [sb3("ysb%d" % i, [128, D], F32) for i in range(2)]
                pX = [PS[0][:, :].bitcast(BF16), PS[1][:, :].bitcast(BF16)]
                pH = [PS[2], PS[3]]
                pHT = [PS[4][:, 0:128].bitcast(BF16), PS[5][:, 0:128].bitcast(BF16)]
                pY = [PS[6], PS[7]]
                wgu_free = [None] * NW; wdb_free = [None] * ND; xsb_free = [None] * NX
                pX_free = [None, None]; xT_free = [None, None]; pH_free = [None, None]; sgs_free = [None, None]
                hid_free = [None, None]; pHT_free = [None, None]; hT_free = [None, None]
                pY_free = [None, None]; ysb_free = [None, None]
                st0 = {}; st1 = {}; st2 = {}; ldt = {}
                ys_w = []

                def issue_loads(a):
                    wi = a % NW; di = a % ND; xj = a % NX
                    lw = DMA('wgl%d' % wi, [wgu_free[wi], scat_done], lambda wi=wi, a=a: nc.gpsimd.indirect_dma_start(
                        out=wgu[wi][:].rearrange("p k c -> p (k c)"), out_offset=None, in_=wgu_r[:, :],
                        in_offset=bass.IndirectOffsetOnAxis(ap=idxw[:, a:a + 1], axis=0)), q='pool')
                    lwd = DMA('wdl%d' % di, [wdb_free[di], scat_done], lambda di=di, a=a: nc.gpsimd.indirect_dma_start(
                        out=wdb[di][:].rearrange("p k c -> p (k c)"), out_offset=None, in_=wd_r[:, :],
                        in_offset=bass.IndirectOffsetOnAxis(ap=idxw[:, a:a + 1], axis=0)), q='pool')
                    lxs = DMA('xsl%d' % xj, [xsb_free[xj], scat_done, sc_toks], lambda xj=xj, a=a: nc.sync.dma_start(
                        out=xsb[xj][:], in_=xs_d[a * 128:(a + 1) * 128, :]))
                    ldt[a] = (lw, lwd, lxs)

                for a in range(min(PF, NSL)):
                    issue_loads(a)
                for it in range(NSL + 3):
                    if it + PF < NSL:
                        issue_loads(it + PF)
                    a = it
                    if a < NSL:
                        xi = a % 2; xj = a % NX
                        lw, lwd, lxs = ldt[a]
                        tp = None
                        for k in range(8):
                            tp = PE([lxs, pX_free[xi]] if k == 0 else None,
                                    lambda k=k, xi=xi, xj=xj: nc.tensor.transpose(pX[xi][:, k * 128:(k + 1) * 128], xsb[xj][:, k * 128:(k + 1) * 128], ident_b[:]),
                                    sig=(k == 7))
                        xsb_free[xj] = tp
                        if a % 2 == 0:
                            ev = ACT([tp, xT_free[xi]], lambda xi=xi: nc.scalar.copy(out=xT[xi][:].rearrange("p k c -> p (k c)"), in_=pX[xi]))
                        else:
                            ev = DVE([tp, xT_free[xi]], lambda xi=xi: nc.vector.tensor_copy(out=xT[xi][:].rearrange("p k c -> p (k c)"), in_=pX[xi]))
                        pX_free[xi] = ev
                        st0[a] = (ev, lw, lwd)
                    a = it - 1
                    if 0 <= a < NSL:
                        wi = a % NW; xi = a % 2
                        ev, lw, lwd = st0[a]
                        mh = mmg(pH[xi][:, :], [(xT[xi][:, k, :], wgu[wi][:, k, :]) for k in range(8)], [ev, lw, pH_free[xi]])
                        wgu_free[wi] = mh
                        xT_free[xi] = mh
                        a_s = ACT([mh, sgs_free[xi]], lambda xi=xi: nc.scalar.activation(out=sgs[xi][:], in_=pH[xi][:, 0:256], func=AF.Silu))
                        d_h = DVE([a_s, hid_free[xi]], lambda xi=xi: nc.vector.tensor_tensor(out=hid[xi][:], in0=sgs[xi][:], in1=pH[xi][:, 256:512], op=ALU.mult))
                        pH_free[xi] = d_h
                        sgs_free[xi] = d_h
                        st1[a] = (d_h, lwd)
                    a = it - 2
                    if 0 <= a < NSL:
                        xi = a % 2
                        d_h, lwd = st1[a]
                        tp2 = None
                        for j in range(2):
                            tp2 = PE([d_h, pHT_free[xi]] if j == 0 else None,
                                     lambda j=j, xi=xi: nc.tensor.transpose(pHT[xi][:, j * 128:(j + 1) * 128], hid[xi][:, j * 128:(j + 1) * 128], ident_b[:]),
                                     sig=(j == 1))
                        hid_free[xi] = tp2
                        ev2 = ACT([tp2, hT_free[xi]], lambda xi=xi: nc.scalar.copy(out=hT[xi][:].rearrange("p k c -> p (k c)"), in_=pHT[xi]))
                        pHT_free[xi] = ev2
                        st2[a] = (ev2, lwd)
                    a = it - 3
                    if 0 <= a < NSL:
                        xi = a % 2; di = a % ND
                        ev2, lwd = st2[a]
                        my0 = mmg(pY[0][:, :], [(hT[xi][:, j, :], wdb[di][:, j, 0:512]) for j in range(2)], [ev2, lwd, pY_free[0]])
                        my1 = mmg(pY[1][:, :], [(hT[xi][:, j, :], wdb[di][:, j, 512:1024]) for j in range(2)], [pY_free[1]])
                        wdb_free[di] = my1
                        hT_free[xi] = my1
                        c0 = ACT([my0, ysb_free[xi]], lambda xi=xi: nc.scalar.copy(out=ysb[xi][:, 0:512], in_=pY[0][:, :]))
                        c1 = DVE([my1, ysb_free[xi]], lambda xi=xi: nc.vector.tensor_copy(out=ysb[xi][:, 512:1024], in_=pY[1][:, :]))
                        pY_free = [c0, c1]
                        ysb_free[xi] = DMA('ysw%d' % xi, [c0, c1], lambda xi=xi, a=a: nc.sync.dma_start(out=ys_d[a * 128:(a + 1) * 128, :], in_=ysb[xi][:]))
                        ys_w.append(ysb_free[xi])
                b3_bar = dp.last() + [ys_w[-2:]]
                dp.retire_since(mkb3)

            with ExitStack() as b4:
                def sb4(name, shape, dt): return b4.enter_context(nc.sbuf_tensor(U(name), shape, dt))
                ya = [sb4("ya%d" % i, [128, D], F32) for i in range(5)]
                yb = [sb4("yb%d" % i, [128, D], F32) for i in range(5)]
                x1c = [sb4("x1c%d" % i, [128, D], F32) for i in range(5)]
                mm_ = [sb4("mm_%d" % i, [128, D], F32) for i in range(5)]
                ytmp = [sb4("ytmp%d" % i, [128, D], F32) for i in range(5)]
                yo = [sb4("yo%d" % i, [128, D], F32) for i in range(5)]
                junk = sb4("junkC", [128, D], BF16)
                stt = [sb4("stC%d" % i, [128, 8], F32) for i in range(5)]
                ya_free = [None] * 5; yb_free = [None] * 5; x1c_free = [None] * 5; mm_free = [None] * 5
                ytmp_free = [None] * 5; yo_free = [None] * 5
                T = dict(cur_seq=-1, vec_ready=None, vec_readers=[])
                out_toks = []

                def cmb_tile(i):
                    s = gseq[i]
                    if s != T['cur_seq']:
                        T['cur_seq'] = s
                        T['vec_ready'] = load_vecs(s, [b3_bar, T['vec_readers']])
                        T['vec_readers'] = []
                    vec_ready = T['vec_ready']
                    gvec2 = gvec2_[s % 2]
                    ti = i % 5
                    tok0 = i * 128
                    st_ = stt[ti]
                    ga = DMA('ga%d' % ti, [ya_free[ti], b3_bar, ys_w], lambda ti=ti, i=i: nc.gpsimd.indirect_dma_start(
                        out=ya[ti][:, :], out_offset=None, in_=ys_d[:, :], in_offset=bass.IndirectOffsetOnAxis(ap=slot0[:, i:i + 1], axis=0)), q='pool')
                    gb_ = DMA('gb%d' % ti, [yb_free[ti], b3_bar], lambda ti=ti, i=i: nc.gpsimd.indirect_dma_start(
                        out=yb[ti][:, :], out_offset=None, in_=ys_d[:, :], in_offset=bass.IndirectOffsetOnAxis(ap=slot1[:, i:i + 1], axis=0)), q='pool')
                    lx = DMA('cx%d' % ti, [x1c_free[ti], b3_bar], lambda ti=ti, tok0=tok0: nc.sync.dma_start(out=x1c[ti][:], in_=x1_d[tok0:tok0 + 128, :]))
                    yield
                    d1 = DVE([ga, mm_free[ti]], lambda ti=ti, i=i: nc.vector.tensor_scalar(out=mm_[ti][:], in0=ya[ti][:], scalar1=W1a[:, i:i + 1], scalar2=None, op0=ALU.mult))
                    ya_free[ti] = d1
                    d2 = DVE([gb_, d1], lambda ti=ti, i=i: nc.vector.scalar_tensor_tensor(out=mm_[ti][:], in0=yb[ti][:], scalar=W2a[:, i:i + 1], in1=mm_[ti][:],
                                                                                         op0=ALU.mult, op1=ALU.add))
                    yb_free[ti] = d2
                    a1 = ACT(d2, lambda ti=ti, st_=st_: nc.scalar.activation(out=junk[:], in_=mm_[ti][:], func=AF.Square, accum_out=st_[:, 0:1]))
                    yield
                    r1a = ACT(a1, lambda st_=st_: nc.scalar.activation(out=st_[:, 1:2], in_=st_[:, 0:1], func=AF.Sqrt, bias=EPS, scale=1.0 / D))
                    yield
                    r1 = DVE(r1a, lambda st_=st_: nc.vector.reciprocal(out=st_[:, 1:2], in_=st_[:, 1:2]))
                    d3 = DVE([r1, ytmp_free[ti], vec_ready], lambda ti=ti, st_=st_: nc.vector.scalar_tensor_tensor(
                        out=ytmp[ti][:], in0=mm_[ti][:], scalar=st_[:, 1:2], in1=gvec2[:], op0=ALU.mult, op1=ALU.mult))
                    mm_free[ti] = d3
                    T['vec_readers'] = [d3]
                    pp = POOL([d3, lx, yo_free[ti]], lambda ti=ti: nc.gpsimd.tensor_tensor(out=yo[ti][:], in0=ytmp[ti][:], in1=x1c[ti][:], op=ALU.add))
                    ytmp_free[ti] = pp
                    x1c_free[ti] = pp
                    yo_free[ti] = DMA('yo%d' % ti, pp, lambda ti=ti, tok0=tok0: nc.sync.dma_start(out=y[tok0:tok0 + 128, :], in_=yo[ti][:]))
                    out_toks.append(yo_free[ti])
                interleave((cmb_tile(i) for i in range(NTT)), 5)
            dp.wait('sp', [yo_free, out_toks[-5:]])
            for e in ('pe', 'act', 'dve', 'pool'):
                dp.wait('sp', [(e, dp.cnt[e])])
    return nc


def _rope_tables(pos):
    half = 16
    inv = (10000.0 ** (-np.arange(half, dtype=np.float32) / half)).astype(np.float32)
    ang = pos.astype(np.float32)[:, None] * inv[None, :]
    cos = np.cos(ang).astype(np.float32)
    sin = np.sin(ang).astype(np.float32)
    c = np.concatenate([cos, cos], axis=1).T
    s_ = np.concatenate([sin, sin], axis=1).T
    return np.ascontiguousarray(c), np.ascontiguousarray(s_)


def _consts(NSL):
    ident = np.eye(128, dtype=np.float32)
    egrp = np.zeros((8, 512), np.float32)
    for g in range(8):
        egrp[g, g * 64:(g + 1) * 64] = 1.0
    utri = np.triu(np.ones((128, 128), np.float32), k=1)
    tri32 = np.triu(np.ones((32, 32), np.float32), k=1)
    jv = np.tile((np.arange(NSL, dtype=np.float32) * 128.0)[None, :], (128, 1))
    pidx = np.arange(128, dtype=np.float32).reshape(128, 1)
    return dict(ident=ident, egrp=egrp, utri=utri, tri32=tri32, jv=np.ascontiguousarray(jv), pidx=pidx,
                zeros=np.zeros((128, 8192), np.float32))


def _nt(cfg):
    nqt = sum(cfg['NQG']) * 512
    nt = (2 * nqt + 32 * 127 + 127) // 128
    return ((nt + 7) // 8) * 8


WEIGHT_KEYS = ['w_ada', 'b_ada', 'g_pre1', 'g_post1', 'g_pre2', 'g_post2', 'w_in', 'g_q', 'w_uq', 'g_kv', 'w_ukv',
               'g_v_gmlp', 'w_spatial', 'b_spatial', 'g_attn_out', 'g_gmlp_out', 'w_out', 'w_router_group',
               'b_router_group', 'w_router_expert', 'b_router_expert', 'w_gate', 'w_up', 'w_down']

_NC_CACHE = {}


def kernel(**inputs):
    S = 4096
    x_all = np.concatenate([np.asarray(inputs['x_prompt'], np.float32), np.asarray(inputs['x_sample'], np.float32)], axis=0)
    c_all = np.concatenate([np.asarray(inputs['c_prompt'], np.float32), np.asarray(inputs['c_sample'], np.float32)], axis=0)
    weights = {k: np.ascontiguousarray(np.asarray(inputs[k], np.float32)) for k in WEIGHT_KEYS}
    consts = _consts(_nt(FULL_CFG))
    pos_nat = np.arange(S)
    in_maps = []
    plans = []
    for c in range(8):
        if c % 2 == 0:
            s0 = (5 * c) // 2
            A, B, Cq, qhalf = s0, s0 + 1, s0 + 2, 0
        else:
            s0 = (5 * c - 1) // 2
            Cq, qhalf, A, B = s0, 1, s0 + 1, s0 + 2
        if qhalf == 0:
            posC = pos_nat
        else:
            posC = np.concatenate([pos_nat[S // 2:], pos_nat[:S // 2]])
        xs = np.concatenate([x_all[A], x_all[B], x_all[Cq][posC]], axis=0)
        cv = np.stack([c_all[A], c_all[B], c_all[Cq]], axis=0)
        rc = np.zeros((3, 32, S), np.float32)
        rs = np.zeros((3, 32, S), np.float32)
        for i, p in enumerate([pos_nat, pos_nat, posC]):
            rc[i], rs[i] = _rope_tables(p)
        m = dict(weights)
        m.update(xs=np.ascontiguousarray(xs), cvec=np.ascontiguousarray(cv), rope_c=rc, rope_s=rs)
        m.update(consts)
        in_maps.append(m)
        plans.append((A, B, Cq, qhalf))
    if 'full' not in _NC_CACHE:
        _NC_CACHE['full'] = build(FULL_CFG)
    nc = _NC_CACHE['full']
    res = run_bass_kernel_spmd(nc, in_maps, core_ids=list(range(8)))
    y_all = np.zeros((20, S, D), np.float32)
    for c in range(8):
        yc = res.results[c]['y']
        A, B, Cq, qhalf = plans[c]
        y_all[A] = yc[0:S]
        y_all[B] = yc[S:2 * S]
        if qhalf == 0:
            y_all[Cq, 0:S // 2] = yc[2 * S:2 * S + S // 2]
        else:
            y_all[Cq, S // 2:] = yc[2 * S:2 * S + S // 2]
    return (np.ascontiguousarray(y_all[0:4]), np.ascontiguousarray(y_all[4:20]))
```

```python
import numpy as np
import concourse.bass as bass
import concourse.mybir as mybir
from concourse.bass_utils import run_bass_kernel_spmd
from contextlib import ExitStack

F32, BF16 = mybir.dt.float32, mybir.dt.bfloat16
I32 = mybir.dt.int32
AF = mybir.ActivationFunctionType
ALU = mybir.AluOpType
AX = mybir.AxisListType
D = 1024
EPS = 1e-6
NE = 32
QSCALE = 96.0 ** -0.5

FULL_CFG = dict(S=4096, NSEQ=3, NQG=[8, 8, 4])


class Dep:
    def __init__(self, nc, es):
        self.nc = nc
        self.es = es
        self.eng = {'pe': nc.tensor, 'act': nc.scalar, 'dve': nc.vector, 'pool': nc.gpsimd, 'sp': nc.sync}
        self.sem = {e: es.enter_context(nc.semaphore('s_' + e)) for e in self.eng}
        self.cnt = {e: 0 for e in self.eng}
        self.waited = {e: {} for e in self.eng}
        self.dsem = {}
        self.entries = {}
        self.free = []

    def semof(self, k):
        return self.sem[k] if k in self.sem else self.entries[k][0]

    def wait(self, e, deps):
        mx = {}
        for k, v in _flat(deps):
            if v > mx.get(k, 0):
                mx[k] = v
        for k, v in mx.items():
            if self.waited[e].get(k, 0) < v:
                self.eng[e].wait_ge(self.semof(k), v)
                self.waited[e][k] = v

    def op(self, e, deps, fn, sig=True):
        self.wait(e, deps)
        ins = fn()
        if sig:
            ins.then_inc(self.sem[e], 1)
            self.cnt[e] += 1
            return (e, self.cnt[e])
        return None

    def dma(self, q, name, deps, fn):
        if name not in self.dsem:
            if self.free:
                key = self.free.pop()
            else:
                key = 'D%d' % len(self.entries)
                self.entries[key] = [self.es.enter_context(self.nc.semaphore('d_' + key)), 0]
            self.dsem[name] = key
        key = self.dsem[name]
        self.wait(q, deps)
        ins = fn()
        ent = self.entries[key]
        ins.then_inc(ent[0], 16)
        ent[1] += 16
        return (key, ent[1])

    def mark(self):
        return set(self.dsem.keys())

    def retire_since(self, mark, keep=()):
        for n in list(self.dsem.keys()):
            if n in mark or n in keep:
                continue
            key = self.dsem[n]
            self.wait('sp', (key, self.entries[key][1]))
            del self.dsem[n]
            self.free.append(key)
        return self.op('sp', None, lambda: self.nc.sync.nop())

    def last(self):
        return [(e, self.cnt[e]) for e in self.eng if self.cnt[e] > 0]


def _flat(deps):
    out = []
    if deps is None:
        return out
    if isinstance(deps, tuple) and len(deps) == 2 and isinstance(deps[0], str):
        return [deps]
    for d in deps:
        out.extend(_flat(d))
    return out


def interleave(gens, depth):
    active = []
    it = iter(gens)
    done = False
    while True:
        if len(active) < depth and not done:
            try:
                active.append(next(it))
            except StopIteration:
                done = True
        if not active:
            break
        nxt = []
        for g in active:
            try:
                next(g)
                nxt.append(g)
            except StopIteration:
                pass
        active = nxt


def build(cfg):
    S = cfg['S']
    NSEQ = cfg['NSEQ']
    NQG = cfg['NQG']
    NG = S // 512
    KB = S // 128
    NT = NSEQ * S
    NQT = sum(NQG) * 512
    NGB = sum(NQG)
    NSL = (2 * NQT + 32 * 127 + 127) // 128
    NSL = ((NSL + 7) // 8) * 8

    nc = bass.Bass("TRN2", target_bir_lowering=False)

    def din(name, shape, dt=F32):
        return nc.dram_tensor(name, list(shape), dt, kind="ExternalInput").ap()

    def dscr(name, shape, dt):
        return nc.dram_tensor(name, list(shape), dt, kind="Internal").ap()

    xs = din("xs", [NT, D])
    cvec = din("cvec", [NSEQ, D])
    rope_c = din("rope_c", [NSEQ, 32, S])
    rope_s = din("rope_s", [NSEQ, 32, S])
    w_ada = din("w_ada", [D, 6 * D])
    b_ada = din("b_ada", [6 * D])
    g_pre1 = din("g_pre1", [D]); g_post1 = din("g_post1", [D])
    g_pre2 = din("g_pre2", [D]); g_post2 = din("g_post2", [D])
    w_in = din("w_in", [D, 1440])
    g_q = din("g_q", [256]); w_uq = din("w_uq", [256, 768])
    g_kv = din("g_kv", [128]); w_ukv = din("w_ukv", [128, 1024])
    g_v_gmlp = din("g_v_gmlp", [512])
    w_spatial = din("w_spatial", [8, 128, 128]); b_spatial = din("b_spatial", [8, 128])
    g_attn_out = din("g_attn_out", [512]); g_gmlp_out = din("g_gmlp_out", [512])
    w_out = din("w_out", [D, D])
    w_rg = din("w_router_group", [D, 4]); b_rg = din("b_router_group", [4])
    w_re = din("w_router_expert", [D, 32]); b_re = din("b_router_expert", [32])
    w_gate = din("w_gate", [NE, D, 256]); w_up = din("w_up", [NE, D, 256]); w_down = din("w_down", [NE, 256, D])
    ident_in = din("ident", [128, 128])
    egrp_in = din("egrp", [8, 512])
    utri_in = din("utri", [128, 128])
    tri32_in = din("tri32", [32, 32])
    jv_in = din("jv", [128, NSL])
    pidx_in = din("pidx", [128, 1])
    zeros_in = din("zeros", [128, 8192])
    y = nc.dram_tensor("y", [NQT, D], F32, kind="ExternalOutput").ap()

    mod_d = dscr("mod_d", [NSEQ, 6 * D], F32)
    sn_d = dscr("sn_d", [NT, 512], BF16)
    cq_d = dscr("cq_d", [NSEQ * NG, 128, 1024], BF16)
    x1_d = (nc.dram_tensor("x1_d", [NQT, D], F32, kind="ExternalOutput").ap() if cfg.get("dbg") else dscr("x1_d", [NQT, D], F32))
    wgu_r = dscr("wgu_r", [NE * 128, 8 * 512], BF16)
    wd_r = dscr("wd_r", [NE * 128, 2 * D], BF16)
    h2_d = dscr("h2_d", [NQT, D], BF16)
    xs_d = dscr("xs_d", [NSL * 128, D], BF16)
    ys_d = dscr("ys_d", [NSL * 128, D], F32)
    wkv_d = dscr("wkv_d", [128, 1024], BF16)
    wsp_d = dscr("wsp_d", [128, 1024], BF16)
    wq_d = dscr("wq_d", [128, 1536], BF16)
    wqsw_d = dscr("wqsw_d", [128, 1536], BF16)
    wo_d = dscr("wo_d", [128, 8192], BF16)

    _uid = [0]

    def U(name):
        _uid[0] += 1
        return "%s_u%d" % (name, _uid[0])

    top = ExitStack()
    with top:
        dp = Dep(nc, top)

        def PE(deps, fn, sig=True): return dp.op('pe', deps, fn, sig)
        def ACT(deps, fn, sig=True): return dp.op('act', deps, fn, sig)
        def DVE(deps, fn, sig=True): return dp.op('dve', deps, fn, sig)
        def POOL(deps, fn, sig=True): return dp.op('pool', deps, fn, sig)
        def DMA(name, deps, fn, q='sp'): return dp.dma(q, name, deps, fn)

        def mmg(out, pairs, deps, sig=True):
            n = len(pairs)
            tok = None
            for i, (l, r) in enumerate(pairs):
                tok = PE(deps if i == 0 else None,
                         lambda l=l, r=r, i=i: nc.tensor.matmul(out, lhsT=l, rhs=r, start=(i == 0), stop=(i == n - 1)),
                         sig=(sig and i == n - 1))
            return tok

        def rstd_chain(ss_ap, out_ap, inv_n, deps):
            t = ACT(deps, lambda: nc.scalar.activation(out=out_ap, in_=ss_ap, func=AF.Sqrt, bias=EPS, scale=inv_n))
            return DVE(t, lambda: nc.vector.reciprocal(out=out_ap, in_=out_ap))

        wcast = []
        for e in range(NE):
            wcast.append(DMA('wcast', None, lambda e=e: nc.gpsimd.dma_start(
                out=wgu_r[e * 128:(e + 1) * 128, :].rearrange("p (k c) -> p k c", k=8)[:, :, 0:256],
                in_=w_gate[e].rearrange("(k p) c -> p k c", p=128)), q='pool'))
            wcast.append(DMA('wcast', None, lambda e=e: nc.gpsimd.dma_start(
                out=wgu_r[e * 128:(e + 1) * 128, :].rearrange("p (k c) -> p k c", k=8)[:, :, 256:512],
                in_=w_up[e].rearrange("(k p) c -> p k c", p=128)), q='pool'))
            wcast.append(DMA('wcast', None, lambda e=e: nc.gpsimd.dma_start(
                out=wd_r[e * 128:(e + 1) * 128, :].rearrange("p (j c) -> p j c", j=2),
                in_=w_down[e].rearrange("(j p) c -> p j c", p=128)), q='pool'))
        wcast_tok = wcast[-1]
        zero_tok = []
        nz = (NSL * 128 * D) // (128 * 8192)
        xs_flat = xs_d.rearrange("(n p r) c -> n p (r c)", p=128, r=8)
        for zi in range(nz):
            zero_tok.append(DMA('zero', None, lambda zi=zi: nc.gpsimd.dma_start(out=xs_flat[zi], in_=zeros_in), q='pool'))

        ident_f = top.enter_context(nc.sbuf_tensor(U("ident_f"), [128, 128], F32))
        ident_b = top.enter_context(nc.sbuf_tensor(U("ident_b"), [128, 128], BF16))
        ones_f = top.enter_context(nc.sbuf_tensor(U("ones_f"), [128, 64], F32))
        t_id = DMA('c0', None, lambda: nc.sync.dma_start(out=ident_f[:], in_=ident_in))
        t_idb = DVE(t_id, lambda: nc.vector.tensor_copy(out=ident_b[:], in_=ident_f[:]))
        t_ones = DVE(None, lambda: nc.vector.memset(ones_f[:], 1.0))

        mk0 = dp.mark()
        with ExitStack() as pes:
            def sb(name, shape, dt): return pes.enter_context(nc.sbuf_tensor(U(name), shape, dt))
            def ps(name, shape, dt): return pes.enter_context(nc.psum_tensor(U(name), shape, dt))
            csT = sb("csT", [128, 8, NSEQ], F32)
            csS = sb("csS", [128, 8, NSEQ], F32)
            wblk = [sb("wblk%d" % i, [128, 8, 512], F32) for i in range(2)]
            brep = sb("brep", [NSEQ, 6 * D], F32)
            modsb = sb("modsb", [NSEQ, 6 * D], F32)
            pmod = [ps("pmod%d" % i, [128, 512], F32) for i in range(2)]
            t_c = [DMA('p0', None, lambda q=q: nc.sync.dma_start(out=csT[:, :, q], in_=cvec[q].rearrange("(k p) -> p k", p=128),
                                                                 allow_slow_non_contiguous=True)) for q in range(NSEQ)]
            t_b = DMA('p1', None, lambda: nc.sync.dma_start(out=brep[:], in_=b_ada.partition_broadcast(NSEQ)))
            t_cs = ACT(t_c, lambda: nc.scalar.activation(out=csS[:], in_=csT[:], func=AF.Silu))
            wfree = [None, None]
            pfree = [None, None]
            ev = None
            for blk in range(12):
                i = blk % 2
                t_w = DMA('pw%d' % i, wfree[i], lambda blk=blk, i=i: nc.sync.dma_start(
                    out=wblk[i][:], in_=w_ada[:, blk * 512:(blk + 1) * 512].rearrange("(k p) c -> p k c", p=128)))
                t_m = mmg(pmod[i][0:NSEQ, :], [(csS[:, k, :], wblk[i][:, k, :]) for k in range(8)], [t_w, t_cs, pfree[i]])
                wfree[i] = t_m
                ev = DVE([t_m, t_b], lambda blk=blk, i=i: nc.vector.tensor_tensor(
                    out=modsb[:, blk * 512:(blk + 1) * 512], in0=pmod[i][0:NSEQ, :],
                    in1=brep[:, blk * 512:(blk + 1) * 512], op=ALU.add))
                pfree[i] = ev
            t_mod = DMA('p2', ev, lambda: nc.sync.dma_start(out=mod_d, in_=modsb[:]))

            tmpq = sb("tmpq", [128, 2, 768], F32)
            gq = sb("gq", [128, 2], F32)
            wq_t = sb("wq_t", [128, 2, 768], BF16)
            wqsw_t = sb("wqsw_t", [128, 2, 768], BF16)
            t1 = DMA('p3', None, lambda: nc.sync.dma_start(out=tmpq[:], in_=w_uq.rearrange("(k p) c -> p k c", p=128)))
            t2 = DMA('p3', None, lambda: nc.sync.dma_start(out=gq[:], in_=g_q.rearrange("(k p) -> p k", p=128),
                                                          allow_slow_non_contiguous=True))
            tq = None
            for k in range(2):
                tq = DVE([t1, t2], lambda k=k: nc.vector.tensor_scalar(
                    out=wq_t[:, k, :], in0=tmpq[:, k, :], scalar1=gq[:, k:k + 1], scalar2=QSCALE,
                    op0=ALU.mult, op1=ALU.mult))
            tz = POOL(None, lambda: nc.gpsimd.memset(wqsw_t[:], 0.0))
            wq4 = wq_t[:].rearrange("p k (h c) -> p k h c", h=8)
            wqs4 = wqsw_t[:].rearrange("p k (h c) -> p k h c", h=8)
            ta = DVE([tq, tz], lambda: nc.vector.tensor_scalar(out=wqs4[:, :, :, 64:80], in0=wq4[:, :, :, 80:96],
                                                              scalar1=-1.0, scalar2=None, op0=ALU.mult))
            tb = DVE(None, lambda: nc.vector.tensor_copy(out=wqs4[:, :, :, 80:96], in_=wq4[:, :, :, 64:80]))
            t_wq = DMA('p4', tq, lambda: nc.sync.dma_start(out=wq_d, in_=wq_t[:].rearrange("p k c -> p (k c)")))
            t_wqsw = DMA('p4', [ta, tb], lambda: nc.sync.dma_start(out=wqsw_d, in_=wqsw_t[:].rearrange("p k c -> p (k c)")))

            tmpkv = sb("tmpkv", [128, 1024], F32)
            gkv = sb("gkv", [128, 1], F32)
            wkv_t = sb("wkv_t", [128, 1024], BF16)
            t1 = DMA('p5', None, lambda: nc.sync.dma_start(out=tmpkv[:], in_=w_ukv))
            t2 = DMA('p5', None, lambda: nc.sync.dma_start(out=gkv[:], in_=g_kv.rearrange("(p o) -> p o", o=1)))
            tk = DVE([t1, t2], lambda: nc.vector.tensor_scalar(out=wkv_t[:], in0=tmpkv[:], scalar1=gkv[:, 0:1],
                                                              scalar2=None, op0=ALU.mult))
            t_wkv = DMA('p6', tk, lambda: nc.sync.dma_start(out=wkv_d, in_=wkv_t[:]))

            tmpo = sb("tmpo", [128, 8, 1024], F32)
            gcat = sb("gcat", [128, 8], F32)
            wo_t = sb("wo_t", [128, 8, 1024], BF16)
            t1 = DMA('p7', None, lambda: nc.sync.dma_start(out=tmpo[:], in_=w_out.rearrange("(k p) c -> p k c", p=128)))
            t2 = DMA('p7', None, lambda: nc.sync.dma_start(out=gcat[:, 0:4], in_=g_attn_out.rearrange("(k p) -> p k", p=128),
                                                          allow_slow_non_contiguous=True))
            t3 = DMA('p7', None, lambda: nc.sync.dma_start(out=gcat[:, 4:8], in_=g_gmlp_out.rearrange("(k p) -> p k", p=128),
                                                          allow_slow_non_contiguous=True))
            two = None
            for k in range(8):
                two = DVE([t1, t2, t3], lambda k=k: nc.vector.tensor_scalar(
                    out=wo_t[:, k, :], in0=tmpo[:, k, :], scalar1=gcat[:, k:k + 1], scalar2=None, op0=ALU.mult))
            t_wo = DMA('p8', two, lambda: nc.sync.dma_start(out=wo_d, in_=wo_t[:].rearrange("p k c -> p (k c)")))

            tmps = sb("tmps", [128, 8, 128], F32)
            wsp_t = sb("wsp_t", [128, 8, 128], BF16)
            psp = ps("psp", [128, 1024], F32)
            t1 = DMA('p9', None, lambda: nc.sync.dma_start(out=tmps[:], in_=w_spatial.rearrange("g t s -> t g s")))
            tt = None
            for g in range(8):
                tt = PE([t1, t_id], lambda g=g: nc.tensor.transpose(psp[:, g * 128:(g + 1) * 128], tmps[:, g, :], ident_f[:]),
                        sig=(g == 7))
            tc_ = DVE(tt, lambda: nc.vector.tensor_copy(out=wsp_t[:].rearrange("p g t -> p (g t)"), in_=psp[:]))
            t_wsp = DMA('p10', tc_, lambda: nc.sync.dma_start(out=wsp_d, in_=wsp_t[:].rearrange("p g t -> p (g t)")))
            prep_done = [t_mod, t_wq, t_wqsw, t_wkv, t_wo, t_wsp]
            dp.retire_since(mk0, keep=('wcast', 'zero', 'c0'))
            prep_bar = dp.last()

        with ExitStack() as aes:
            def sbA(name, shape, dt): return aes.enter_context(nc.sbuf_tensor(U(name), shape, dt))
            KT = sbA("KT", [128, 8, S], BF16)
            VA = sbA("VA", [128, KB, 8, 65], BF16)
            geff1 = sbA("geff1", [128, D], F32)
            sh1r = sbA("sh1r", [128, D], F32)
            gvec1 = sbA("gvec1", [128, D], F32)
            gvrep = sbA("gvrep", [128, 512], F32)
            PA = aes.enter_context(nc.psum_tensor(U("PA"), [128, 1024], F32))
            PB = aes.enter_context(nc.psum_tensor(U("PB"), [128, 1024], F32))
            PC = aes.enter_context(nc.psum_tensor(U("PC"), [128, 1024], F32))
            PD = aes.enter_context(nc.psum_tensor(U("PD"), [128, 1024], F32))

            t_va1 = POOL(prep_bar, lambda: nc.gpsimd.memset(VA[:, :, :, 64:65], 1.0))
            t_gv = DMA('a0', prep_bar, lambda: nc.sync.dma_start(out=gvrep[:], in_=g_v_gmlp.partition_broadcast(128)))
            seq_bar = [prep_bar, prep_done, t_va1, t_gv, t_idb, t_ones]
            qbase = 0
            for s in range(NSEQ):
                mk1 = dp.mark()
                with ExitStack() as p1:
                    def sb1(name, shape, dt): return p1.enter_context(nc.sbuf_tensor(U(name), shape, dt))
                    wAs = sb1("wAs", [128, 8, 384], BF16)
                    wAuv = sb1("wAuv", [128, 8, 1024], BF16)
                    wAkr = sb1("wAkr", [128, 8, 96], BF16)
                    wAks = sb1("wAks", [128, 8, 96], BF16)
                    wkv = sb1("wkv", [128, 1024], BF16)
                    wsp = sb1("wsp", [128, 8, 128], BF16)
                    bsp = sb1("bsp", [8, 128], F32)
                    egrp = sb1("egrp", [8, 512], F32)
                    vt = [sb1("vt%d" % i, [128, D], F32) for i in range(2)]
                    xt = [sb1("xt%d" % i, [128, D], F32) for i in range(2)]
                    junk = sb1("junk", [128, D], BF16)
                    hm = sb1("hm", [128, D], F32)
                    hb = [sb1("hb%d" % i, [128, D], BF16) for i in range(2)]
                    hT = sb1("hT", [128, 8, 512], BF16)
                    zsb = [sb1("zsb%d" % i, [128, 384], BF16) for i in range(2)]
                    cqnT = [sb1("cqnT%d" % i, [128, 2, 512], BF16) for i in range(2)]
                    ckvnT = [sb1("ckvnT%d" % i, [128, 512], BF16) for i in range(2)]
                    gu = [sb1("gu%d" % i, [128, 512], BF16) for i in range(2)]
                    gv = [sb1("gv%d" % i, [128, 512], F32) for i in range(2)]
                    zraw = [sb1("zraw%d" % i, [128, 384], F32) for i in range(2)]
                    vn = [sb1("vn%d" % i, [128, 512], BF16) for i in range(2)]
                    sraw = [sb1("sraw%d" % i, [128, 512], F32) for i in range(2)]
                    sn = [sb1("sn%d" % i, [128, 512], BF16) for i in range(2)]
                    stt = [sb1("stt%d" % i, [128, 16], F32) for i in range(2)]
                    Ctt = [sb1("Ctt%d" % i, [128, 128], F32) for i in range(2)]
                    Stt = [sb1("Stt%d" % i, [128, 128], F32) for i in range(2)]
                    kt1 = [sb1("kt1_%d" % i, [128, 128], F32) for i in range(2)]
                    kt2 = [sb1("kt2_%d" % i, [128, 128], F32) for i in range(2)]
                    krr = [sb1("krr%d" % i, [128, 128], BF16) for i in range(2)]

                    pT = PA[:, 0:512].bitcast(BF16)
                    pT2 = PA[:, 512:1024].bitcast(BF16)
                    pzs = PB[:, 0:384]
                    pss = PB[:, 512:1024]
                    pu = PC[:, 0:512]
                    pv = PC[:, 512:1024]
                    pkr = PD[:, 0:512]
                    pks = PD[:, 512:1024]

                    sb_ = seq_bar
                    wl = []
                    wl.append(DMA('a1', sb_, lambda: nc.gpsimd.dma_start(
                        out=wAs[:], in_=w_in[:, 0:384].rearrange("(k p) c -> p k c", p=128)), q='pool'))
                    wl.append(DMA('a1', sb_, lambda: nc.gpsimd.dma_start(
                        out=wAuv[:], in_=w_in[:, 416:1440].rearrange("(k p) c -> p k c", p=128)), q='pool'))
                    tz1 = POOL(sb_, lambda: nc.gpsimd.memset(wAkr[:], 0.0))
                    tz2 = POOL(sb_, lambda: nc.gpsimd.memset(wAks[:], 0.0))
                    wl.append(DMA('a1', [tz1], lambda: nc.gpsimd.dma_start(
                        out=wAkr[:, :, 64:96], in_=w_in[:, 384:416].rearrange("(k p) c -> p k c", p=128)), q='pool'))
                    tn = DMA('a2', [tz2], lambda: nc.gpsimd.dma_start(
                        out=wAks[:, :, 64:80], in_=w_in[:, 400:416].rearrange("(k p) c -> p k c", p=128)), q='pool')
                    wl.append(DMA('a1', [tz2], lambda: nc.gpsimd.dma_start(
                        out=wAks[:, :, 80:96], in_=w_in[:, 384:400].rearrange("(k p) c -> p k c", p=128)), q='pool'))
                    wl.append(POOL(tn, lambda: nc.gpsimd.tensor_scalar(out=wAks[:, :, 64:80], in0=wAks[:, :, 64:80],
                                                                      scalar1=-1.0, scalar2=None, op0=ALU.mult)))
                    wl.append(DMA('a3', sb_, lambda: nc.sync.dma_start(out=wkv[:], in_=wkv_d)))
                    wl.append(DMA('a3', sb_, lambda: nc.sync.dma_start(out=wsp[:].rearrange("p g t -> p (g t)"), in_=wsp_d)))
                    wl.append(DMA('a3', sb_, lambda: nc.sync.dma_start(out=bsp[:], in_=b_spatial)))
                    wl.append(DMA('a3', sb_, lambda: nc.sync.dma_start(out=egrp[:], in_=egrp_in)))
                    l1 = DMA('a4_0', sb_, lambda: nc.sync.dma_start(out=vt[0][:], in_=mod_d[s, 1024:2048].partition_broadcast(128)))
                    l2 = DMA('a4_1', sb_, lambda: nc.sync.dma_start(out=vt[1][:], in_=g_pre1.partition_broadcast(128)))
                    l3 = DMA('a4_2', sb_, lambda: nc.sync.dma_start(out=sh1r[:], in_=mod_d[s, 0:1024].partition_broadcast(128)))
                    tg = DVE([l1, l2], lambda: nc.vector.scalar_tensor_tensor(out=geff1[:], in0=vt[0][:], scalar=1.0, in1=vt[1][:],
                                                                               op0=ALU.add, op1=ALU.mult))
                    l4 = DMA('a4_3', [tg], lambda: nc.sync.dma_start(out=vt[0][:], in_=mod_d[s, 2048:3072].partition_broadcast(128)))
                    l5 = DMA('a4_4', [tg], lambda: nc.sync.dma_start(out=vt[1][:], in_=g_post1.partition_broadcast(128)))
                    tg2 = DVE([l4, l5], lambda: nc.vector.tensor_tensor(out=gvec1[:], in0=vt[0][:], in1=vt[1][:], op=ALU.mult))
                    ready = [wl, l3, tg, tg2]

                    xt_free = [None, None]; hb_free = [None, None]
                    hT_free = [None] * 4
                    zraw_free = [None, None]; zsb_free = [None, None]; cq_free = [None, None]; ckv_free = [None, None]
                    gu_free = [None, None]; gv_free = [None, None]; vn_free = [None, None]; sn_free = [None, None]
                    sraw_free = [None, None]; ct_free = [None, None]; kt_free = [None, None]; krr_free = [None, None]
                    P = dict(hm_free=None, pT_free=None, pT2_free=None, pzs_free=None, pu_free=None, pv_free=None, pss_free=None,
                             pkr_free=None, pks_free=None)
                    grp = {}

                    def p1_tile(g, t):
                        gi = g % 2
                        ti = t % 2
                        if t == 0:
                            grp[g] = dict(cq_w=[], ckv_w=[])
                        G = grp[g]
                        tok0 = s * S + g * 512 + t * 128
                        ts_ = slice(t * 128, (t + 1) * 128)
                        gts = slice(g * 512 + t * 128, g * 512 + (t + 1) * 128)
                        st_ = stt[ti]
                        lx = DMA('x%d' % ti, [xt_free[ti], ready], lambda: nc.sync.dma_start(out=xt[ti][:], in_=xs[tok0:tok0 + 128, :]))
                        lc = DMA('rc%d' % ti, [ct_free[ti], ready], lambda: nc.sync.dma_start(out=Ctt[ti][64:96, :], in_=rope_c[s, :, gts]))
                        ls = DMA('rs%d' % ti, [ct_free[ti], ready], lambda: nc.sync.dma_start(out=Stt[ti][64:96, :], in_=rope_s[s, :, gts]))
                        a1 = ACT(lx, lambda: nc.scalar.activation(out=junk[:], in_=xt[ti][:], func=AF.Square, accum_out=st_[:, 0:1]))
                        yield
                        r1a = ACT(a1, lambda: nc.scalar.activation(out=st_[:, 1:2], in_=st_[:, 0:1], func=AF.Sqrt, bias=EPS, scale=1.0 / D))
                        yield
                        r1 = DVE(r1a, lambda: nc.vector.reciprocal(out=st_[:, 1:2], in_=st_[:, 1:2]))
                        d1 = DVE([r1, P['hm_free']], lambda: nc.vector.scalar_tensor_tensor(
                            out=hm[:], in0=xt[ti][:], scalar=st_[:, 1:2], in1=geff1[:], op0=ALU.mult, op1=ALU.mult))
                        xt_free[ti] = d1
                        p1_ = POOL([d1, hb_free[ti]], lambda: nc.gpsimd.tensor_tensor(out=hb[ti][:], in0=hm[:], in1=sh1r[:], op=ALU.add))
                        P['hm_free'] = p1_
                        yield
                        tp = None
                        for k in range(8):
                            tp = PE([p1_, P['pT_free']] if k == 0 else None,
                                    lambda k=k: nc.tensor.transpose(pT[:, k * 128:(k + 1) * 128], hb[ti][:, k * 128:(k + 1) * 128], ident_b[:]),
                                    sig=(k == 7))
                        hb_free[ti] = tp
                        yield
                        ev = ACT([tp, hT_free[t]], lambda: nc.scalar.copy(out=hT[:, :, ts_], in_=pT.rearrange("p (k c) -> p k c", k=8)))
                        P['pT_free'] = ev
                        yield
                        m_zs = mmg(pzs, [(hT[:, k, ts_], wAs[:, k, :]) for k in range(8)], [ev, P['pzs_free']])
                        m_u = mmg(pu, [(hT[:, k, ts_], wAuv[:, k, 0:512]) for k in range(8)], [P['pu_free']])
                        m_v = mmg(pv, [(hT[:, k, ts_], wAuv[:, k, 512:1024]) for k in range(8)], [P['pv_free']])
                        m_kr = mmg(pkr[0:96, 0:128], [(wAkr[:, k, :], hT[:, k, ts_]) for k in range(8)], [P['pkr_free']])
                        m_ks = mmg(pks[0:96, 0:128], [(wAks[:, k, :], hT[:, k, ts_]) for k in range(8)], [P['pks_free']])
                        hT_free[t] = m_ks
                        yield
                        zr = DVE([m_zs, zraw_free[ti]], lambda: nc.vector.tensor_copy(out=zraw[ti][:], in_=pzs))
                        P['pzs_free'] = zr
                        g1 = ACT([m_u, gu_free[ti]], lambda: nc.scalar.activation(out=gu[ti][:], in_=pu, func=AF.Gelu_apprx_tanh))
                        P['pu_free'] = g1
                        g2 = ACT([m_v, gv_free[ti]], lambda: nc.scalar.activation(out=gv[ti][:], in_=pv, func=AF.Gelu_apprx_tanh))
                        P['pv_free'] = g2
                        k1 = DVE([m_kr, lc, kt_free[ti]], lambda: nc.vector.tensor_tensor(out=kt1[ti][64:96, :], in0=pkr[64:96, 0:128], in1=Ctt[ti][64:96, :], op=ALU.mult))
                        P['pkr_free'] = k1
                        k2 = DVE([m_ks, ls], lambda: nc.vector.tensor_tensor(out=kt2[ti][64:96, :], in0=pks[64:96, 0:128], in1=Stt[ti][64:96, :], op=ALU.mult))
                        P['pks_free'] = k2
                        ct_free[ti] = k2
                        yield
                        a2 = ACT(zr, lambda: nc.scalar.activation(out=junk[:, 0:256], in_=zraw[ti][:, 0:256], func=AF.Square, accum_out=st_[:, 2:3]))
                        a3 = ACT(None, lambda: nc.scalar.activation(out=junk[:, 256:384], in_=zraw[ti][:, 256:384], func=AF.Square, accum_out=st_[:, 3:4]))
                        g3 = ACT(g2, lambda: nc.scalar.activation(out=junk[:, 0:512], in_=gv[ti][:], func=AF.Square, accum_out=st_[:, 6:7]))
                        k3 = DVE([k1, k2, krr_free[ti]], lambda: nc.vector.tensor_tensor(out=krr[ti][64:96, :], in0=kt1[ti][64:96, :], in1=kt2[ti][64:96, :], op=ALU.add))
                        kt_free[ti] = k3
                        kc = None
                        for h in range(8):
                            kc = POOL(k3, lambda h=h: nc.gpsimd.tensor_copy(out=KT[64:96, h, gts], in_=krr[ti][64:96, :]))
                        krr_free[ti] = kc
                        yield
                        q1 = ACT([a2, a3], lambda: nc.scalar.activation(out=st_[:, 4:5], in_=st_[:, 2:3], func=AF.Sqrt, bias=EPS, scale=1.0 / 256))
                        q2 = ACT(None, lambda: nc.scalar.activation(out=st_[:, 5:6], in_=st_[:, 3:4], func=AF.Sqrt, bias=EPS, scale=1.0 / 128))
                        q3 = ACT(g3, lambda: nc.scalar.activation(out=st_[:, 7:8], in_=st_[:, 6:7], func=AF.Sqrt, bias=EPS, scale=1.0 / 512))
                        yield
                        r2 = DVE([q1, q2], lambda: nc.vector.reciprocal(out=st_[:, 4:6], in_=st_[:, 4:6]))
                        r4 = DVE(q3, lambda: nc.vector.reciprocal(out=st_[:, 7:8], in_=st_[:, 7:8]))
                        d2 = DVE([r4, vn_free[ti]], lambda: nc.vector.scalar_tensor_tensor(
                            out=vn[ti][:], in0=gv[ti][:], scalar=st_[:, 7:8], in1=gvrep[:], op0=ALU.mult, op1=ALU.mult))
                        gv_free[ti] = d2
                        c1 = ACT([r2, zsb_free[ti]], lambda: nc.scalar.activation(
                            out=zsb[ti][:, 0:256], in_=zraw[ti][:, 0:256], func=AF.Copy, scale=st_[:, 4:5]))
                        c2 = ACT(None, lambda: nc.scalar.activation(
                            out=zsb[ti][:, 256:384], in_=zraw[ti][:, 256:384], func=AF.Copy, scale=st_[:, 5:6]))
                        zraw_free[ti] = c2
                        yield
                        tp2 = None
                        for k in range(3):
                            tp2 = PE([c1, c2, P['pT2_free']] if k == 0 else None,
                                     lambda k=k: nc.tensor.transpose(pT2[:, k * 128:(k + 1) * 128], zsb[ti][:, k * 128:(k + 1) * 128], ident_b[:]),
                                     sig=(k == 2))
                        zsb_free[ti] = tp2
                        PE([P['pss_free'], ready], lambda: nc.tensor.matmul(pss, lhsT=bsp[:, :], rhs=egrp[:, :], start=True, stop=False), sig=False)
                        m_s = None
                        for gg in range(8):
                            m_s = PE(d2 if gg == 0 else None,
                                     lambda gg=gg: nc.tensor.matmul(pss[:, gg * 64:(gg + 1) * 64], lhsT=wsp[:, gg, :],
                                                                    rhs=vn[ti][:, gg * 64:(gg + 1) * 64], start=False, stop=(gg == 7)),
                                     sig=(gg == 7))
                        vn_free[ti] = m_s
                        yield
                        e1 = DVE([tp2, cq_free[gi] if t == 0 else None], lambda: nc.vector.tensor_copy(
                            out=cqnT[gi][:, :, ts_], in_=pT2[:, 0:256].rearrange("p (k c) -> p k c", k=2)))
                        e2 = DVE([ckv_free[gi] if t == 0 else None], lambda: nc.vector.tensor_copy(
                            out=ckvnT[gi][:, ts_], in_=pT2[:, 256:384]))
                        P['pT2_free'] = e2
                        G['cq_w'].append(e1)
                        G['ckv_w'].append(e2)
                        d3 = DVE([m_s, g1, sraw_free[ti]], lambda: nc.vector.tensor_tensor(out=sraw[ti][:], in0=gu[ti][:], in1=pss, op=ALU.mult))
                        P['pss_free'] = d3
                        gu_free[ti] = d3
                        yield
                        a4 = ACT(d3, lambda: nc.scalar.activation(out=junk[:, 0:512], in_=sraw[ti][:], func=AF.Square, accum_out=st_[:, 8:9]))
                        yield
                        q4 = ACT(a4, lambda: nc.scalar.activation(out=st_[:, 9:10], in_=st_[:, 8:9], func=AF.Sqrt, bias=EPS, scale=1.0 / 512))
                        yield
                        r5 = DVE(q4, lambda: nc.vector.reciprocal(out=st_[:, 9:10], in_=st_[:, 9:10]))
                        yield
                        c3 = ACT([r5, sn_free[ti]], lambda: nc.scalar.activation(out=sn[ti][:], in_=sraw[ti][:], func=AF.Copy, scale=st_[:, 9:10]))
                        sraw_free[ti] = c3
                        sn_free[ti] = DMA('sn%d' % ti, c3, lambda: nc.sync.dma_start(out=sn_d[tok0:tok0 + 128, :], in_=sn[ti][:]))
                        if t != 3:
                            return
                        yield
                        gs = slice(g * 512, (g + 1) * 512)
                        cq_free[gi] = DMA('cq%d' % gi, G['cq_w'], lambda: nc.sync.dma_start(
                            out=cq_d[s * NG + g], in_=cqnT[gi][:].rearrange("p k c -> p (k c)")))
                        bank_free = [P['pkr_free'], P['pks_free']]
                        banks = [pkr, pks]
                        for h in range(8):
                            bi = h % 2
                            mk = mmg(banks[bi][0:64, :], [(wkv[:, h * 128:h * 128 + 64], ckvnT[gi][:, :])], [G['ckv_w'], bank_free[bi]])
                            if h % 2 == 0:
                                bank_free[bi] = ACT(mk, lambda h=h, bi=bi: nc.scalar.copy(out=KT[0:64, h, gs], in_=banks[bi][0:64, :]))
                            else:
                                bank_free[bi] = DVE(mk, lambda h=h, bi=bi: nc.vector.tensor_copy(out=KT[0:64, h, gs], in_=banks[bi][0:64, :]))
                        wkv3 = wkv[:].rearrange("p (h c) -> p h c", h=8)[:, :, 64:128]
                        mv = None
                        for tt in range(4):
                            bi = tt % 2
                            kb = g * 4 + tt
                            mv = mmg(banks[bi][:, :].rearrange("p (h c) -> p h c", h=8), [(ckvnT[gi][:, tt * 128:(tt + 1) * 128], wkv3)], [bank_free[bi]])
                            bank_free[bi] = DVE(mv, lambda kb=kb, bi=bi: nc.vector.tensor_copy(
                                out=VA[:, kb, :, 0:64], in_=banks[bi][:, :].rearrange("p (h c) -> p h c", h=8)))
                        ckv_free[gi] = mv
                        P['pkr_free'] = bank_free[0]
                        P['pks_free'] = bank_free[1]

                    interleave((p1_tile(g, t) for g in range(NG) for t in range(4)), 2)
                    dp.retire_since(mk1)
                    p1_bar = dp.last() + [sn_free, cq_free]

                mk2 = dp.mark()
                with ExitStack() as p2:
                    def sb2(name, shape, dt): return p2.enter_context(nc.sbuf_tensor(U(name), shape, dt))
                    wq = sb2("wq", [128, 2, 768], BF16)
                    wqs = sb2("wqs", [128, 2, 768], BF16)
                    wo = sb2("wo", [128, 8, 1024], BF16)
                    cqT = [sb2("cqT%d" % i, [128, 2, 512], BF16) for i in range(2)]
                    Ct = sb2("Ct2", [128, 512], F32)
                    St = sb2("St2", [128, 512], F32)
                    qt1 = [sb2("qt1_%d" % i, [128, 512], F32) for i in range(2)]
                    qt2 = [sb2("qt2_%d" % i, [128, 512], F32) for i in range(2)]
                    qt_free = [None, None]
                    QT = sb2("QT", [128, 8, 512], BF16)
                    pTs = [sb2("pTs%d" % i, [128, 1024], BF16) for i in range(3)]
                    osb = [sb2("osb%d" % i, [128, 512], F32) for i in range(2)]
                    rinv = [sb2("rinv0", [128, 512], F32)] * 2
                    aT = [sb2("aT%d" % i, [128, 512], BF16) for i in range(2)]
                    merged = [sb2("merged%d" % i, [128, D], BF16) for i in range(4)]
                    mT = [sb2("mT%d" % i, [128, 8, 128], BF16) for i in range(2)]
                    otmp = sb2("otmp", [128, D], F32)
                    xr = [sb2("xr%d" % i, [128, D], F32) for i in range(2)]
                    x1 = [sb2("x1_%d" % i, [128, D], F32) for i in range(2)]
                    junk = sb2("junk2", [128, D], BF16)
                    stt = [sb2("stq%d" % i, [128, 16], F32) for i in range(2)]

                    po = PC[:, 0:512]
                    prb = PC[:, 512:1024]
                    pmT = PC[:, 512:1024].bitcast(BF16)
                    pa = PD[:, :].bitcast(BF16)
                    scT = [PA, PB]

                    wl2 = [DMA('b1', p1_bar, lambda: nc.sync.dma_start(out=wq[:].rearrange("p k c -> p (k c)"), in_=wq_d)),
                           DMA('b1', p1_bar, lambda: nc.sync.dma_start(out=wqs[:].rearrange("p k c -> p (k c)"), in_=wqsw_d)),
                           DMA('b1', p1_bar, lambda: nc.sync.dma_start(out=wo[:].rearrange("p k c -> p (k c)"), in_=wo_d))]
                    ready2 = [p1_bar, wl2]
                    cq_free2 = [None, None]; rope_free = None; QT_free = []
                    sc_free = [None, None]; pTs_free = [None, None, None]; po_free = None; osb_free = [None, None]
                    rinv_free = [None]; prb_free = None; aT_free = [None, None]; pa_free = []
                    merged_free = [None] * 4; mT_free = [None, None]; pmT_free = None
                    otmp_free = None; xr_free = [None, None]; x1_free = [None, None]
                    step = 0
                    tcount = 0
                    for qg in range(NQG[s]):
                        gi = qg % 2
                        gs = slice(qg * 512, (qg + 1) * 512)
                        lq = DMA('cql%d' % gi, [cq_free2[gi], ready2], lambda gi=gi, qg=qg: nc.sync.dma_start(
                            out=cqT[gi][:].rearrange("p k c -> p (k c)"), in_=cq_d[s * NG + qg]))
                        lr1 = DMA('rp2c', [rope_free, ready2], lambda gs=gs: nc.sync.dma_start(out=Ct[64:96, :], in_=rope_c[s, :, gs]))
                        lr2 = DMA('rp2s', [rope_free, ready2], lambda gs=gs: nc.sync.dma_start(out=St[64:96, :], in_=rope_s[s, :, gs]))
                        pre_ld = []
                        for t in range(4):
                            tok0_ = s * S + qg * 512 + t * 128
                            l_sn = DMA('snl%d' % t, [merged_free[t], ready2], lambda t=t, tok0_=tok0_: nc.sync.dma_start(
                                out=merged[t][:, 512:1024], in_=sn_d[tok0_:tok0_ + 128, :]))
                            pre_ld.append(l_sn)
                        wq4 = wq[:].rearrange("p k (h c) -> p k h c", h=8)
                        wqs4 = wqs[:].rearrange("p k (h c) -> p k h c", h=8)
                        QT_w = []
                        Qs = dict(mqs=None, qd=None)

                        def q_head(h):
                            T = scT[h % 2]
                            hi = h % 2
                            mq = mmg(T[0:96, 0:512], [(wq4[:, k, h, :], cqT[gi][:, k, :]) for k in range(2)], [lq, sc_free[h % 2]])
                            mqs = mmg(T[0:96, 512:1024], [(wqs4[:, k, h, :], cqT[gi][:, k, :]) for k in range(2)], None)
                            Qs['mqs'] = mqs
                            yield
                            c0 = ACT([mq, QT_free if h == 0 else None], lambda: nc.scalar.copy(out=QT[0:64, h, :], in_=T[0:64, 0:512]))
                            q1 = DVE([mq, lr1, qt_free[hi]], lambda: nc.vector.tensor_tensor(out=qt1[hi][64:96, :], in0=T[64:96, 0:512], in1=Ct[64:96, :], op=ALU.mult))
                            q2 = DVE([mqs, lr2], lambda: nc.vector.tensor_tensor(out=qt2[hi][64:96, :], in0=T[64:96, 512:1024], in1=St[64:96, :], op=ALU.mult))
                            sc_free[h % 2] = [c0, q2]
                            yield
                            qd = DVE([q1, q2, QT_free if h == 0 else None], lambda: nc.vector.tensor_tensor(
                                out=QT[64:96, h, :], in0=qt1[hi][64:96, :], in1=qt2[hi][64:96, :], op=ALU.add))
                            qt_free[hi] = qd
                            Qs['qd'] = qd
                            QT_w.extend([c0, qd])
                        interleave((q_head(h) for h in range(8)), 2)
                        mqs = Qs['mqs']
                        qd = Qs['qd']
                        cq_free2[gi] = mqs
                        rope_free = qd
                        NP = KB // 2
                        steps = [(h, j) for h in range(8) for j in range(NP)]
                        qk_tok = {}

                        def emit_qk(idx):
                            h, j = steps[idx]
                            T = scT[idx % 2]
                            tk = None
                            for u in range(2):
                                kb = 2 * j + u
                                tk = PE([sc_free[idx % 2], QT_w] if u == 0 else None,
                                        lambda h=h, kb=kb, u=u, T=T: nc.tensor.matmul(
                                            T[:, u * 512:(u + 1) * 512], lhsT=KT[0:96, h, kb * 128:(kb + 1) * 128], rhs=QT[0:96, h, :],
                                            start=True, stop=True), sig=(u == 1))
                            qk_tok[idx] = tk

                        emit_qk(0)
                        pa_w = []
                        QT_readers = []
                        pending = []
                        A = dict(prb_free=prb_free)
                        for idx, (h, j) in enumerate(steps):
                            if idx + 1 < len(steps):
                                emit_qk(idx + 1)
                            T = scT[idx % 2]
                            sl = step % 3
                            step += 1
                            ex = ACT([qk_tok[idx], pTs_free[sl]], lambda T=T, sl=sl: nc.scalar.activation(out=pTs[sl][:], in_=T[:, :], func=AF.Exp))
                            sc_free[idx % 2] = ex
                            pvt = None
                            for u in range(2):
                                kb = 2 * j + u
                                pvt = PE([ex, po_free if (j == 0 and u == 0) else None],
                                         lambda h=h, kb=kb, u=u, sl=sl: nc.tensor.matmul(
                                             po[0:65, :], lhsT=VA[:, kb, h, :], rhs=pTs[sl][:, u * 512:(u + 1) * 512],
                                             start=(kb == 0), stop=(kb == KB - 1)), sig=(u == 1))
                            pTs_free[sl] = pvt
                            for pend in list(pending):
                                pend[0] -= 1
                                if pend[0] <= 0:
                                    pend[1]()
                                    pending.remove(pend)
                            if j == NP - 1:
                                for pend in list(pending):
                                    pend[1]()
                                    pending.remove(pend)
                                oi = h % 2
                                QT_readers.append(pvt)
                                o1 = DVE([pvt, osb_free[oi]], lambda oi=oi: nc.vector.tensor_copy(out=osb[oi][0:65, :], in_=po[0:65, :]))
                                po_free = o1
                                o2 = DVE([o1, rinv_free[0]], lambda oi=oi: nc.vector.reciprocal(out=rinv[oi][64:65, :], in_=osb[oi][64:65, :]))
                                hs = dict(o2=o2, oi=oi, h=h)

                                def part_a(hs=hs):
                                    oi = hs['oi']
                                    o3 = PE([hs['o2'], A['prb_free']], lambda oi=oi: nc.tensor.matmul(prb[0:64, :], lhsT=ones_f[64:65, 0:64], rhs=rinv[oi][64:65, :],
                                                                                                 start=True, stop=True))
                                    rinv_free[0] = o3
                                    o4 = DVE([o3, aT_free[oi]], lambda oi=oi: nc.vector.tensor_tensor(out=aT[oi][0:64, :], in0=osb[oi][0:64, :],
                                                                                                       in1=prb[0:64, :], op=ALU.mult))
                                    A['prb_free'] = o4
                                    osb_free[oi] = o4
                                    hs['o4'] = o4

                                def part_b(hs=hs):
                                    oi = hs['oi']; h = hs['h']
                                    o5 = None
                                    for t in range(4):
                                        o5 = PE([hs['o4'], pa_free if h == 0 else None] if t == 0 else None,
                                                lambda t=t, h=h, oi=oi: nc.tensor.transpose(
                                                    pa[:, t * 512 + h * 64: t * 512 + (h + 1) * 64], aT[oi][0:64, t * 128:(t + 1) * 128], ident_b[0:64, 0:64]),
                                                sig=(t == 3))
                                    aT_free[oi] = o5
                                    pa_w.append(o5)
                                pending.append([2, part_a])
                                pending.append([4, part_b])
                        for pend in list(pending):
                            pend[1]()
                            pending.remove(pend)
                        prb_free = A['prb_free']
                        QT_free = QT_readers
                        pa_r = []
                        M = dict(pmT_free=pmT_free, prb_free=prb_free, otmp_free=otmp_free)

                        def mg_tile(t, ti):
                            tok0 = s * S + qg * 512 + t * 128
                            otok0 = qbase + qg * 512 + t * 128
                            st_ = stt[ti]
                            lsn = pre_ld[t]
                            lxr = DMA('xr%d' % ti, [xr_free[ti], ready2], lambda: nc.sync.dma_start(
                                out=xr[ti][:], in_=xs[tok0:tok0 + 128, :]))
                            a1 = ACT(pa_w, lambda: nc.scalar.activation(out=junk[:, 0:512], in_=pa[:, t * 512:(t + 1) * 512], func=AF.Square,
                                                                        accum_out=st_[:, 0:1]))
                            yield
                            r1a = ACT(a1, lambda: nc.scalar.activation(out=st_[:, 1:2], in_=st_[:, 0:1], func=AF.Sqrt, bias=EPS, scale=1.0 / 512))
                            yield
                            r1 = DVE(r1a, lambda: nc.vector.reciprocal(out=st_[:, 1:2], in_=st_[:, 1:2]))
                            c1 = ACT([r1, merged_free[t]], lambda: nc.scalar.activation(
                                out=merged[t][:, 0:512], in_=pa[:, t * 512:(t + 1) * 512], func=AF.Copy, scale=st_[:, 1:2]))
                            pa_r.append(c1)
                            yield
                            tp = None
                            for k in range(8):
                                tp = PE([c1, lsn, M['pmT_free'], M['prb_free']] if k == 0 else None,
                                        lambda k=k: nc.tensor.transpose(pmT[:, k * 128:(k + 1) * 128], merged[t][:, k * 128:(k + 1) * 128], ident_b[:]),
                                        sig=(k == 7))
                            merged_free[t] = tp
                            yield
                            ev = DVE([tp, mT_free[ti]], lambda: nc.vector.tensor_copy(out=mT[ti][:].rearrange("p k c -> p (k c)"), in_=pmT))
                            M['pmT_free'] = ev
                            M['prb_free'] = ev
                            yield
                            T = scT[t % 2]
                            mo1 = mmg(T[:, 0:512], [(mT[ti][:, k, :], wo[:, k, 0:512]) for k in range(8)], [ev, sc_free[t % 2]])
                            mo2 = mmg(T[:, 512:1024], [(mT[ti][:, k, :], wo[:, k, 512:1024]) for k in range(8)], None)
                            mT_free[ti] = mo2
                            yield
                            a2 = ACT(mo2, lambda: nc.scalar.activation(out=junk[:], in_=T[:, :], func=AF.Square, accum_out=st_[:, 2:3]))
                            yield
                            r2a = ACT(a2, lambda: nc.scalar.activation(out=st_[:, 3:4], in_=st_[:, 2:3], func=AF.Sqrt, bias=EPS, scale=1.0 / D))
                            yield
                            r2 = DVE(r2a, lambda: nc.vector.reciprocal(out=st_[:, 3:4], in_=st_[:, 3:4]))
                            d1 = DVE([r2, M['otmp_free']], lambda: nc.vector.scalar_tensor_tensor(
                                out=otmp[:], in0=T[:, :], scalar=st_[:, 3:4], in1=gvec1[:], op0=ALU.mult, op1=ALU.mult))
                            sc_free[t % 2] = d1
                            pp = POOL([d1, lxr, x1_free[ti]], lambda: nc.gpsimd.tensor_tensor(out=x1[ti][:], in0=otmp[:], in1=xr[ti][:], op=ALU.add))
                            M['otmp_free'] = pp
                            xr_free[ti] = pp
                            x1_free[ti] = DMA('x1s%d' % ti, pp, lambda: nc.sync.dma_start(
                                out=x1_d[otok0:otok0 + 128, :], in_=x1[ti][:]))
                        interleave((mg_tile(t, (tcount + t) % 2) for t in range(4)), 2)
                        tcount += 4
                        pmT_free = M['pmT_free']; prb_free = M['prb_free']; otmp_free = M['otmp_free']
                        pa_free = pa_r
                    dp.retire_since(mk2)
                    p2_bar = dp.last() + [x1_free]
                seq_bar = p2_bar
                qbase += NQG[s] * 512
            stageA_bar = seq_bar

        NTT = NQT // 128
        gseq = []
        for s in range(NSEQ):
            gseq += [s] * (NQG[s] * 4)
        with ExitStack() as bes:
            def sbB(name, shape, dt): return bes.enter_context(nc.sbuf_tensor(U(name), shape, dt))
            geff2_ = [sbB("geff2_%d" % i, [128, D], F32) for i in range(2)]
            sh2r_ = [sbB("sh2r_%d" % i, [128, D], F32) for i in range(2)]
            gvec2_ = [sbB("gvec2_%d" % i, [128, D], F32) for i in range(2)]
            vt = [sbB("vtB%d" % i, [128, D], F32) for i in range(2)]
            M1a = sbB("M1a", [128, NTT, 32], F32)
            M2a = sbB("M2a", [128, NTT, 32], F32)
            W1a = sbB("W1a", [128, NTT], F32)
            W2a = sbB("W2a", [128, NTT], F32)
            R1a = sbB("R1a", [128, NTT], F32)
            R2a = sbB("R2a", [128, NTT], F32)
            slot0 = sbB("slot0", [128, NTT], I32)
            slot1 = sbB("slot1", [128, NTT], I32)
            idxw = sbB("idxw", [128, NSL], I32)
            carry = sbB("carry", [128, 32], F32)
            PS = [bes.enter_context(nc.psum_tensor(U("PS%d" % i), [128, 512], F32)) for i in range(8)]
            bb = stageA_bar
            readyB = [bb, wcast_tok, zero_tok]

            def load_vecs(s, deps):
                geff2 = geff2_[s % 2]; sh2r = sh2r_[s % 2]; gvec2 = gvec2_[s % 2]
                l1 = DMA('v0_0', deps, lambda s=s: nc.sync.dma_start(out=vt[0][:], in_=mod_d[s, 4096:5120].partition_broadcast(128)))
                l2 = DMA('v0_1', deps, lambda: nc.sync.dma_start(out=vt[1][:], in_=g_pre2.partition_broadcast(128)))
                l3 = DMA('v0_2', deps, lambda s=s: nc.sync.dma_start(out=sh2r[:], in_=mod_d[s, 3072:4096].partition_broadcast(128)))
                tg = DVE([l1, l2], lambda: nc.vector.scalar_tensor_tensor(out=geff2[:], in0=vt[0][:], scalar=1.0, in1=vt[1][:],
                                                                           op0=ALU.add, op1=ALU.mult))
                l4 = DMA('v0_3', [tg], lambda s=s: nc.sync.dma_start(out=vt[0][:], in_=mod_d[s, 5120:6144].partition_broadcast(128)))
                l5 = DMA('v0_4', [tg], lambda: nc.sync.dma_start(out=vt[1][:], in_=g_post2.partition_broadcast(128)))
                tg2 = DVE([l4, l5], lambda: nc.vector.tensor_tensor(out=gvec2[:], in0=vt[0][:], in1=vt[1][:], op=ALU.mult))
                return [l3, tg, tg2]

            mkb1 = dp.mark()
            with ExitStack() as b1:
                def sb1(name, shape, dt): return b1.enter_context(nc.sbuf_tensor(U(name), shape, dt))
                w_r = sb1("w_r", [128, 8, 36], F32)
                brr = sb1("brr", [128, 36], F32)
                utri = sb1("utri", [128, 128], BF16)
                onesb = sb1("onesb", [128, 128], BF16)
                x1t = [sb1("x1t%d" % i, [128, D], F32) for i in range(5)]
                junk = sb1("junkB", [128, D], BF16)
                hm = sb1("hmB", [128, D], F32)
                h2 = [sb1("h2_%d" % i, [128, D], F32) for i in range(5)]
                h2Tf = [sb1("h2Tf%d" % i, [128, 8, 128], F32) for i in range(5)]
                stt = [sb1("stB%d" % i, [128, 8], F32) for i in range(5)]
                lg = [sb1("lg%d" % i, [128, 36], F32) for i in range(5)]
                wk = [sb1("wk%d" % i, [128, 192], F32) for i in range(5)]
                ohb = [sb1("ohb%d" % i, [128, 32], BF16) for i in range(5)]
                PH = [PS[6], PS[7]]
                ld = [DMA('s0', bb, lambda: nc.sync.dma_start(out=w_r[:, :, 0:4], in_=w_rg.rearrange("(k p) c -> p k c", p=128))),
                      DMA('s0', bb, lambda: nc.sync.dma_start(out=w_r[:, :, 4:36], in_=w_re.rearrange("(k p) c -> p k c", p=128))),
                      DMA('s0', bb, lambda: nc.sync.dma_start(out=brr[:, 0:4], in_=b_rg.partition_broadcast(128))),
                      DMA('s0', bb, lambda: nc.sync.dma_start(out=brr[:, 4:36], in_=b_re.partition_broadcast(128))),
                      DMA('s1', bb, lambda: nc.gpsimd.dma_start(out=utri[:], in_=utri_in), q='pool'),
                      POOL(bb, lambda: nc.gpsimd.memset(onesb[:], 1.0)),
                      POOL(bb, lambda: nc.gpsimd.memset(carry[:], 0.0))]
                rdy1 = [readyB, ld]
                T = dict(cur_seq=-1, vec_ready=None, vec_readers=[], hm_free=None, PH_free=[None, None], plg_free=None,
                         pcum_free=None, carry_tok=ld[-1])
                x1t_free = [None] * 5; h2_free = [None] * 5
                h2Tf_free = [None] * 5
                h2d_w = []

                def p1_tile(i):
                    s = gseq[i]
                    if s != T['cur_seq']:
                        T['cur_seq'] = s
                        T['vec_ready'] = load_vecs(s, [rdy1, T['vec_readers']])
                        T['vec_readers'] = []
                    vec_ready = T['vec_ready']
                    geff2 = geff2_[s % 2]; sh2r = sh2r_[s % 2]
                    ti = i % 5
                    tok0 = i * 128
                    st_ = stt[ti]
                    lx = DMA('bx%d' % ti, [x1t_free[ti], rdy1], lambda ti=ti, tok0=tok0: nc.sync.dma_start(out=x1t[ti][:], in_=x1_d[tok0:tok0 + 128, :]))
                    a1 = ACT(lx, lambda ti=ti, st_=st_: nc.scalar.activation(out=junk[:], in_=x1t[ti][:], func=AF.Square, accum_out=st_[:, 0:1]))
                    yield
                    r1a = ACT(a1, lambda st_=st_: nc.scalar.activation(out=st_[:, 1:2], in_=st_[:, 0:1], func=AF.Sqrt, bias=EPS, scale=1.0 / D))
                    yield
                    r1 = DVE(r1a, lambda st_=st_: nc.vector.reciprocal(out=st_[:, 1:2], in_=st_[:, 1:2]))
                    d1 = DVE([r1, T['hm_free'], vec_ready], lambda ti=ti, st_=st_: nc.vector.scalar_tensor_tensor(
                        out=hm[:], in0=x1t[ti][:], scalar=st_[:, 1:2], in1=geff2[:], op0=ALU.mult, op1=ALU.mult))
                    x1t_free[ti] = d1
                    p1_ = POOL([d1, h2_free[ti], vec_ready], lambda ti=ti: nc.gpsimd.tensor_tensor(out=h2[ti][:], in0=hm[:], in1=sh2r[:], op=ALU.add))
                    T['hm_free'] = p1_
                    T['vec_readers'] = [p1_, d1]
                    wr = DMA('h2w%d' % ti, p1_, lambda ti=ti, tok0=tok0: nc.gpsimd.dma_start(out=h2_d[tok0:tok0 + 128, :], in_=h2[ti][:]), q='pool')
                    h2d_w.append(wr)
                    yield
                    tp = None
                    for k in range(8):
                        bank = PH[k // 4]
                        tp = PE([p1_, T['PH_free']] if k == 0 else None,
                                lambda k=k, ti=ti, bank=bank: nc.tensor.transpose(bank[:, (k % 4) * 128:(k % 4 + 1) * 128],
                                                                                  h2[ti][:, k * 128:(k + 1) * 128], ident_f[:]),
                                sig=(k == 7))
                    h2_free[ti] = [tp, wr]
                    yield
                    e1 = ACT([tp, h2Tf_free[ti]], lambda ti=ti: nc.scalar.copy(out=h2Tf[ti][:, 0:4, :], in_=PH[0][:, :].rearrange("p (k c) -> p k c", k=4)))
                    e2 = DVE([tp, h2Tf_free[ti]], lambda ti=ti: nc.vector.tensor_copy(out=h2Tf[ti][:, 4:8, :], in_=PH[1][:, :].rearrange("p (k c) -> p k c", k=4)))
                    T['PH_free'] = [e1, e2]
                    yield
                    plg = PS[4][:, 0:36]
                    m_l = mmg(plg, [(h2Tf[ti][:, k, :], w_r[:, k, :]) for k in range(8)], [e1, e2, T['plg_free'], rdy1])
                    h2Tf_free[ti] = m_l
                    yield
                    L = lg[ti]; W = wk[ti]
                    v1 = DVE([m_l], lambda L=L: nc.vector.tensor_tensor(out=L[:], in0=plg, in1=brr[:], op=ALU.add))
                    T['plg_free'] = v1
                    v2 = DVE(v1, lambda L=L, W=W: nc.vector.tensor_reduce(out=W[:, 0:1], in_=L[:, 0:4], axis=AX.X, op=ALU.max))
                    v3 = DVE(v2, lambda W=W: nc.vector.tensor_scalar(out=W[:, 1:2], in0=W[:, 0:1], scalar1=-1.0, scalar2=None, op0=ALU.mult))
                    v4 = DVE(v2, lambda L=L, W=W: nc.vector.tensor_scalar(out=W[:, 4:8], in0=L[:, 0:4], scalar1=W[:, 0:1], scalar2=None, op0=ALU.is_equal))
                    s1 = ACT([v3], lambda L=L, W=W: nc.scalar.activation(out=W[:, 8:12], in_=L[:, 0:4], func=AF.Exp, bias=W[:, 1:2], scale=1.0,
                                                                         accum_out=W[:, 2:3]))
                    yield
                    v5 = DVE(s1, lambda W=W: nc.vector.reciprocal(out=W[:, 3:4], in_=W[:, 2:3]))
                    v6 = DVE(v4, lambda L=L, W=W: nc.vector.tensor_tensor(
                        out=W[:, 16:48].rearrange("p (g e) -> p g e", g=4), in0=L[:, 4:36].rearrange("p (g e) -> p g e", g=4),
                        in1=W[:, 4:8].unsqueeze(2).to_broadcast([128, 4, 8]), op=ALU.mult))
                    v7 = DVE(v6, lambda W=W: nc.vector.tensor_reduce(out=W[:, 48:56], in_=W[:, 16:48].rearrange("p (g e) -> p e g", g=4),
                                                                    axis=AX.X, op=ALU.add))
                    v8 = DVE(v7, lambda W=W: nc.vector.tensor_reduce(out=W[:, 12:13], in_=W[:, 48:56], axis=AX.X, op=ALU.max))
                    v9 = DVE(v8, lambda W=W: nc.vector.tensor_scalar(out=W[:, 56:64], in0=W[:, 48:56], scalar1=W[:, 12:13], scalar2=None,
                                                                    op0=ALU.is_equal))
                    v10 = DVE(v9, lambda W=W: nc.vector.scalar_tensor_tensor(out=W[:, 64:72], in0=W[:, 56:64], scalar=-1e30, in1=W[:, 48:56],
                                                                            op0=ALU.mult, op1=ALU.add))
                    v11 = DVE(v10, lambda W=W: nc.vector.tensor_reduce(out=W[:, 13:14], in_=W[:, 64:72], axis=AX.X, op=ALU.max))
                    v12 = DVE(v11, lambda W=W: nc.vector.tensor_scalar(out=W[:, 72:80], in0=W[:, 64:72], scalar1=W[:, 13:14], scalar2=None,
                                                                      op0=ALU.is_equal))
                    v13 = DVE(v11, lambda W=W: nc.vector.tensor_scalar(out=W[:, 14:15], in0=W[:, 12:13], scalar1=-1.0, scalar2=None, op0=ALU.mult))
                    s2 = ACT([v13], lambda W=W: nc.scalar.activation(out=W[:, 15:16], in_=W[:, 13:14], func=AF.Exp, bias=W[:, 14:15], scale=1.0))
                    yield
                    v14 = DVE(s2, lambda W=W: nc.vector.tensor_scalar(out=W[:, 80:81], in0=W[:, 15:16], scalar1=1.0, scalar2=None, op0=ALU.add))
                    v15 = DVE(v14, lambda W=W: nc.vector.reciprocal(out=W[:, 81:82], in_=W[:, 80:81]))
                    v16 = DVE([v15, v5], lambda W=W, i=i: nc.vector.tensor_tensor(out=W1a[:, i:i + 1], in0=W[:, 81:82], in1=W[:, 3:4], op=ALU.mult))
                    v17 = DVE(v16, lambda W=W, i=i: nc.vector.tensor_tensor(out=W2a[:, i:i + 1], in0=W1a[:, i:i + 1], in1=W[:, 15:16], op=ALU.mult))
                    v18 = DVE([v9, v4], lambda W=W, i=i: nc.vector.tensor_tensor(
                        out=M1a[:, i, :].rearrange("p (g e) -> p g e", g=4), in0=W[:, 4:8].unsqueeze(2).to_broadcast([128, 4, 8]),
                        in1=W[:, 56:64].unsqueeze(1).to_broadcast([128, 4, 8]), op=ALU.mult))
                    v19 = DVE([v12], lambda W=W, i=i: nc.vector.tensor_tensor(
                        out=M2a[:, i, :].rearrange("p (g e) -> p g e", g=4), in0=W[:, 4:8].unsqueeze(2).to_broadcast([128, 4, 8]),
                        in1=W[:, 72:80].unsqueeze(1).to_broadcast([128, 4, 8]), op=ALU.mult))
                    OH = ohb[ti]
                    v20 = DVE([v18, v19, T['pcum_free']], lambda OH=OH, i=i: nc.vector.tensor_tensor(out=OH[:], in0=M1a[:, i, :], in1=M2a[:, i, :], op=ALU.add))
                    pcum = PS[5][:, 0:32]
                    ptot = PS[5][:, 32:64]
                    PE([v20, T['pcum_free'], rdy1], lambda OH=OH: nc.tensor.matmul(pcum, lhsT=utri[:], rhs=OH[:], start=True, stop=True), sig=False)
                    mc = PE(None, lambda OH=OH: nc.tensor.matmul(ptot, lhsT=onesb[:], rhs=OH[:], start=True, stop=True))
                    yield
                    v21 = DVE([mc, T['carry_tok']], lambda W=W: nc.vector.tensor_tensor(out=W[:, 96:128], in0=carry[:], in1=pcum, op=ALU.add))
                    v22 = DVE(v21, lambda: nc.vector.tensor_tensor(out=carry[:], in0=carry[:], in1=ptot, op=ALU.add))
                    T['carry_tok'] = v22
                    T['pcum_free'] = v22
                    v23 = DVE(v22, lambda W=W, i=i: nc.vector.tensor_tensor(out=W[:, 128:160], in0=W[:, 96:128], in1=M1a[:, i, :], op=ALU.mult))
                    v24 = DVE(v23, lambda W=W, i=i: nc.vector.tensor_reduce(out=R1a[:, i:i + 1], in_=W[:, 128:160], axis=AX.X, op=ALU.add))
                    v25 = DVE(v24, lambda W=W, i=i: nc.vector.tensor_tensor(out=W[:, 160:192], in0=W[:, 96:128], in1=M2a[:, i, :], op=ALU.mult))
                    v26 = DVE(v25, lambda W=W, i=i: nc.vector.tensor_reduce(out=R2a[:, i:i + 1], in_=W[:, 160:192], axis=AX.X, op=ALU.add))
                interleave((p1_tile(i) for i in range(NTT)), 5)
                dp.retire_since(mkb1)
                b1_bar = dp.last() + [h2d_w]

            with ExitStack() as b2:
                def sb2(name, shape, dt): return b2.enter_context(nc.sbuf_tensor(U(name), shape, dt))
                jv = sb2("jv", [128, NSL], F32)
                pidx = sb2("pidx", [128, 1], F32)
                tri32 = sb2("tri32", [32, 32], F32)
                cmp_ = sb2("cmp", [128, NSL * 32], F32)
                tmpM = sb2("tmpM", [128, NTT, 32], F32)
                nblk = sb2("nblk", [128, 32], F32)
                pc = sb2("pc", [128, 32], F32)
                pcT = sb2("pcT", [32, 128], F32)
                sst = sb2("sst", [128, 32], F32)
                send = sb2("send", [128, 32], F32)
                te = sb2("te", [128, NSL], F32)
                sf = sb2("sf", [128, NTT], F32)
                l = [DMA('i0', b1_bar, lambda: nc.sync.dma_start(out=jv[:], in_=jv_in)),
                     DMA('i0', b1_bar, lambda: nc.sync.dma_start(out=pidx[:], in_=pidx_in)),
                     DMA('i0', b1_bar, lambda: nc.sync.dma_start(out=tri32[:], in_=tri32_in))]
                c3 = cmp_[:].rearrange("p (e m) -> p e m", e=32)
                q1 = DVE([l, b1_bar], lambda: nc.vector.tensor_tensor(out=c3, in0=jv[:].unsqueeze(1).to_broadcast([128, 32, NSL]),
                                                                      in1=carry[:].unsqueeze(2).to_broadcast([128, 32, NSL]), op=ALU.is_lt))
                q2 = DVE(q1, lambda: nc.vector.tensor_reduce(out=nblk[:], in_=c3, axis=AX.X, op=ALU.add))
                q3 = DVE(q2, lambda: nc.vector.tensor_scalar(out=pc[:], in0=nblk[:], scalar1=128.0, scalar2=None, op0=ALU.mult))
                q4 = PE(q3, lambda: nc.tensor.transpose(PS[0][0:32, 0:128], pc[:, :], ident_f[:]))
                q5 = ACT(q4, lambda: nc.scalar.copy(out=pcT[:], in_=PS[0][0:32, 0:128]))
                q6 = PE([q5, l], lambda: nc.tensor.matmul(PS[1][:, 0:32], lhsT=pcT[:, :], rhs=tri32[:, :], start=True, stop=True))
                q7 = DVE(q6, lambda: nc.vector.tensor_copy(out=sst[:], in_=PS[1][:, 0:32]))
                q8 = DVE(q7, lambda: nc.vector.tensor_tensor(out=send[:], in0=sst[:], in1=pc[:], op=ALU.add))
                c4 = cmp_[:].rearrange("p (m e) -> p m e", e=32)
                q9 = DVE(q8, lambda: nc.vector.tensor_tensor(out=c4, in0=send[:].unsqueeze(1).to_broadcast([128, NSL, 32]),
                                                             in1=jv[:].unsqueeze(2).to_broadcast([128, NSL, 32]), op=ALU.is_le))
                q10 = DVE(q9, lambda: nc.vector.tensor_reduce(out=te[:], in_=c4, axis=AX.X, op=ALU.add))
                q11 = DVE(q10, lambda: nc.vector.tensor_scalar(out=te[:], in0=te[:], scalar1=31.0, scalar2=128.0, op0=ALU.min, op1=ALU.mult))
                q12 = DVE(q11, lambda: nc.vector.tensor_scalar(out=te[:], in0=te[:], scalar1=pidx[:, 0:1], scalar2=None, op0=ALU.add))
                q13 = DVE(q12, lambda: nc.vector.tensor_copy(out=idxw[:], in_=te[:]))
                q14 = DVE(q7, lambda: nc.vector.tensor_tensor(out=tmpM[:], in0=M1a[:], in1=sst[:].unsqueeze(1).to_broadcast([128, NTT, 32]), op=ALU.mult))
                q15 = DVE(q14, lambda: nc.vector.tensor_reduce(out=sf[:], in_=tmpM[:], axis=AX.X, op=ALU.add))
                q16 = DVE(q15, lambda: nc.vector.tensor_tensor(out=sf[:], in0=sf[:], in1=R1a[:], op=ALU.add))
                q17 = DVE(q16, lambda: nc.vector.tensor_copy(out=slot0[:], in_=sf[:]))
                q18 = DVE(q17, lambda: nc.vector.tensor_tensor(out=tmpM[:], in0=M2a[:], in1=sst[:].unsqueeze(1).to_broadcast([128, NTT, 32]), op=ALU.mult))
                q19 = DVE(q18, lambda: nc.vector.tensor_reduce(out=sf[:], in_=tmpM[:], axis=AX.X, op=ALU.add))
                q20 = DVE(q19, lambda: nc.vector.tensor_tensor(out=sf[:], in0=sf[:], in1=R2a[:], op=ALU.add))
                q21 = DVE(q20, lambda: nc.vector.tensor_copy(out=slot1[:], in_=sf[:]))
                b2_bar = dp.last()

            mkb3 = dp.mark()
            with ExitStack() as b3:
                def sb3(name, shape, dt): return b3.enter_context(nc.sbuf_tensor(U(name), shape, dt))
                hsc = [sb3("hsc%d" % i, [128, D], BF16) for i in range(3)]
                hsc_free = [None] * 3
                sc_toks = []
                for i in range(NTT):
                    si = i % 3
                    tok0 = i * 128
                    lh = DMA('hl%d' % si, [hsc_free[si], b2_bar], lambda si=si, tok0=tok0: nc.sync.dma_start(out=hsc[si][:], in_=h2_d[tok0:tok0 + 128, :]))
                    s0 = DMA('sc%d' % si, [lh, b2_bar], lambda si=si, i=i: nc.gpsimd.indirect_dma_start(
                        out=xs_d[:, :], out_offset=bass.IndirectOffsetOnAxis(ap=slot0[:, i:i + 1], axis=0), in_=hsc[si][:, :], in_offset=None), q='pool')
                    s1_ = DMA('sc%d' % si, [lh], lambda si=si, i=i: nc.gpsimd.indirect_dma_start(
                        out=xs_d[:, :], out_offset=bass.IndirectOffsetOnAxis(ap=slot1[:, i:i + 1], axis=0), in_=hsc[si][:, :], in_offset=None), q='pool')
                    hsc_free[si] = [s0, s1_]
                    sc_toks += [s0, s1_]
                scat_done = [sc_toks[-6:], b2_bar]

                PF = 3
                NW = PF + 3
                ND = PF + 5
                NX = PF + 2
                wgu = [sb3("wgu%d" % i, [128, 8, 512], BF16) for i in range(NW)]
                wdb = [sb3("wdb%d" % i, [128, 2, D], BF16) for i in range(ND)]
                xsb = [sb3("xsb%d" % i, [128, D], BF16) for i in range(NX)]
                xT = [sb3("xT%d" % i, [128, 8, 128], BF16) for i in range(2)]
                sgs = [sb3("sgs%d" % i, [128, 256], F32) for i in range(2)]
                hid = [sb3("hid%d" % i, [128, 256], BF16) for i in range(2)]
                hT = [sb3("hT%d" % i, [128, 2, 128], BF16) for i in range(2)]
                ysb = [sb3("ysb%d" % i, [128, D], F32) for i in range(2)]
                pX = [PS[0][:, :].bitcast(BF16), PS[1][:, :].bitcast(BF16)]
                pH = [PS[2], PS[3]]
                pHT = [PS[4][:, 0:128].bitcast(BF16), PS[5][:, 0:128].bitcast(BF16)]
                pY = [PS[6], PS[7]]
                wgu_free = [None] * NW; wdb_free = [None] * ND; xsb_free = [None] * NX
                pX_free = [None, None]; xT_free = [None, None]; pH_free = [None, None]; sgs_free = [None, None]
                hid_free = [None, None]; pHT_free = [None, None]; hT_free = [None, None]
                pY_free = [None, None]; ysb_free = [None, None]
                st0 = {}; st1 = {}; st2 = {}; ldt = {}
                ys_w = []

                def issue_loads(a):
                    wi = a % NW; di = a % ND; xj = a % NX
                    lw = DMA('wgl%d' % wi, [wgu_free[wi], scat_done], lambda wi=wi, a=a: nc.gpsimd.indirect_dma_start(
                        out=wgu[wi][:].rearrange("p k c -> p (k c)"), out_offset=None, in_=wgu_r[:, :],
                        in_offset=bass.IndirectOffsetOnAxis(ap=idxw[:, a:a + 1], axis=0)), q='pool')
                    lwd = DMA('wdl%d' % di, [wdb_free[di], scat_done], lambda di=di, a=a: nc.gpsimd.indirect_dma_start(
                        out=wdb[di][:].rearrange("p k c -> p (k c)"), out_offset=None, in_=wd_r[:, :],
                        in_offset=bass.IndirectOffsetOnAxis(ap=idxw[:, a:a + 1], axis=0)), q='pool')
                    lxs = DMA('xsl%d' % xj, [xsb_free[xj], scat_done, sc_toks], lambda xj=xj, a=a: nc.sync.dma_start(
                        out=xsb[xj][:], in_=xs_d[a * 128:(a + 1) * 128, :]))
                    ldt[a] = (lw, lwd, lxs)

                for a in range(min(PF, NSL)):
                    issue_loads(a)
                for it in range(NSL + 3):
                    if it + PF < NSL:
                        issue_loads(it + PF)
                    a = it
                    if a < NSL:
                        xi = a % 2; xj = a % NX
                        lw, lwd, lxs = ldt[a]
                        tp = None
                        for k in range(8):
                            tp = PE([lxs, pX_free[xi]] if k == 0 else None,
                                    lambda k=k, xi=xi, xj=xj: nc.tensor.transpose(pX[xi][:, k * 128:(k + 1) * 128], xsb[xj][:, k * 128:(k + 1) * 128], ident_b[:]),
                                    sig=(k == 7))
                        xsb_free[xj] = tp
                        if a % 2 == 0:
                            ev = ACT([tp, xT_free[xi]], lambda xi=xi: nc.scalar.copy(out=xT[xi][:].rearrange("p k c -> p (k c)"), in_=pX[xi]))
                        else:
                            ev = DVE([tp, xT_free[xi]], lambda xi=xi: nc.vector.tensor_copy(out=xT[xi][:].rearrange("p k c -> p (k c)"), in_=pX[xi]))
                        pX_free[xi] = ev
                        st0[a] = (ev, lw, lwd)
                    a = it - 1
                    if 0 <= a < NSL:
                        wi = a % NW; xi = a % 2
                        ev, lw, lwd = st0[a]
                        mh = mmg(pH[xi][:, :], [(xT[xi][:, k, :], wgu[wi][:, k, :]) for k in range(8)], [ev, lw, pH_free[xi]])
                        wgu_free[wi] = mh
                        xT_free[xi] = mh
                        a_s = ACT([mh, sgs_free[xi]], lambda xi=xi: nc.scalar.activation(out=sgs[xi][:], in_=pH[xi][:, 0:256], func=AF.Silu))
                        d_h = DVE([a_s, hid_free[xi]], lambda xi=xi: nc.vector.tensor_tensor(out=hid[xi][:], in0=sgs[xi][:], in1=pH[xi][:, 256:512], op=ALU.mult))
                        pH_free[xi] = d_h
                        sgs_free[xi] = d_h
                        st1[a] = (d_h, lwd)
                    a = it - 2
                    if 0 <= a < NSL:
                        xi = a % 2
                        d_h, lwd = st1[a]
                        tp2 = None
                        for j in range(2):
                            tp2 = PE([d_h, pHT_free[xi]] if j == 0 else None,
                                     lambda j=j, xi=xi: nc.tensor.transpose(pHT[xi][:, j * 128:(j + 1) * 128], hid[xi][:, j * 128:(j + 1) * 128], ident_b[:]),
                                     sig=(j == 1))
                        hid_free[xi] = tp2
                        ev2 = ACT([tp2, hT_free[xi]], lambda xi=xi: nc.scalar.copy(out=hT[xi][:].rearrange("p k c -> p (k c)"), in_=pHT[xi]))
                        pHT_free[xi] = ev2
                        st2[a] = (ev2, lwd)
                    a = it - 3
                    if 0 <= a < NSL:
                        xi = a % 2; di = a % ND
                        ev2, lwd = st2[a]
                        my0 = mmg(pY[0][:, :], [(hT[xi][:, j, :], wdb[di][:, j, 0:512]) for j in range(2)], [ev2, lwd, pY_free[0]])
                        my1 = mmg(pY[1][:, :], [(hT[xi][:, j, :], wdb[di][:, j, 512:1024]) for j in range(2)], [pY_free[1]])
                        wdb_free[di] = my1
                        hT_free[xi] = my1
                        c0 = ACT([my0, ysb_free[xi]], lambda xi=xi: nc.scalar.copy(out=ysb[xi][:, 0:512], in_=pY[0][:, :]))
                        c1 = DVE([my1, ysb_free[xi]], lambda xi=xi: nc.vector.tensor_copy(out=ysb[xi][:, 512:1024], in_=pY[1][:, :]))
                        pY_free = [c0, c1]
                        ysb_free[xi] = DMA('ysw%d' % xi, [c0, c1], lambda xi=xi, a=a: nc.sync.dma_start(out=ys_d[a * 128:(a + 1) * 128, :], in_=ysb[xi][:]))
                        ys_w.append(ysb_free[xi])
                dp.retire_since(mkb3)
                b3_bar = dp.last() + [ys_w[-2:]]

            with ExitStack() as b4:
                def sb4(name, shape, dt): return b4.enter_context(nc.sbuf_tensor(U(name), shape, dt))
                ya = [sb4("ya%d" % i, [128, D], F32) for i in range(5)]
                yb = [sb4("yb%d" % i, [128, D], F32) for i in range(5)]
                x1c = [sb4("x1c%d" % i, [128, D], F32) for i in range(5)]
                mm_ = [sb4("mm_%d" % i, [128, D], F32) for i in range(5)]
                ytmp = [sb4("ytmp%d" % i, [128, D], F32) for i in range(5)]
                yo = [sb4("yo%d" % i, [128, D], F32) for i in range(5)]
                junk = sb4("junkC", [128, D], BF16)
                stt = [sb4("stC%d" % i, [128, 8], F32) for i in range(5)]
                ya_free = [None] * 5; yb_free = [None] * 5; x1c_free = [None] * 5; mm_free = [None] * 5
                ytmp_free = [None] * 5; yo_free = [None] * 5
                T = dict(cur_seq=-1, vec_ready=None, vec_readers=[])
                out_toks = []

                def cmb_tile(i):
                    s = gseq[i]
                    if s != T['cur_seq']:
                        T['cur_seq'] = s
                        T['vec_ready'] = load_vecs(s, [b3_bar, T['vec_readers']])
                        T['vec_readers'] = []
                    vec_ready = T['vec_ready']
                    gvec2 = gvec2_[s % 2]
                    ti = i % 5
                    tok0 = i * 128
                    st_ = stt[ti]
                    ga = DMA('ga%d' % ti, [ya_free[ti], b3_bar, ys_w], lambda ti=ti, i=i: nc.gpsimd.indirect_dma_start(
                        out=ya[ti][:, :], out_offset=None, in_=ys_d[:, :], in_offset=bass.IndirectOffsetOnAxis(ap=slot0[:, i:i + 1], axis=0)), q='pool')
                    gb_ = DMA('gb%d' % ti, [yb_free[ti], b3_bar], lambda ti=ti, i=i: nc.gpsimd.indirect_dma_start(
                        out=yb[ti][:, :], out_offset=None, in_=ys_d[:, :], in_offset=bass.IndirectOffsetOnAxis(ap=slot1[:, i:i + 1], axis=0)), q='pool')
                    lx = DMA('cx%d' % ti, [x1c_free[ti], b3_bar], lambda ti=ti, tok0=tok0: nc.sync.dma_start(out=x1c[ti][:], in_=x1_d[tok0:tok0 + 128, :]))
                    yield
                    d1 = DVE([ga, mm_free[ti]], lambda ti=ti, i=i: nc.vector.tensor_scalar(out=mm_[ti][:], in0=ya[ti][:], scalar1=W1a[:, i:i + 1], scalar2=None, op0=ALU.mult))
                    ya_free[ti] = d1
                    d2 = DVE([gb_, d1], lambda ti=ti, i=i: nc.vector.scalar_tensor_tensor(out=mm_[ti][:], in0=yb[ti][:], scalar=W2a[:, i:i + 1], in1=mm_[ti][:],
                                                                                         op0=ALU.mult, op1=ALU.add))
                    yb_free[ti] = d2
                    a1 = ACT(d2, lambda ti=ti, st_=st_: nc.scalar.activation(out=junk[:], in_=mm_[ti][:], func=AF.Square, accum_out=st_[:, 0:1]))
                    yield
                    r1a = ACT(a1, lambda st_=st_: nc.scalar.activation(out=st_[:, 1:2], in_=st_[:, 0:1], func=AF.Sqrt, bias=EPS, scale=1.0 / D))
                    yield
                    r1 = DVE(r1a, lambda st_=st_: nc.vector.reciprocal(out=st_[:, 1:2], in_=st_[:, 1:2]))
                    d3 = DVE([r1, ytmp_free[ti], vec_ready], lambda ti=ti, st_=st_: nc.vector.scalar_tensor_tensor(
                        out=ytmp[ti][:], in0=mm_[ti][:], scalar=st_[:, 1:2], in1=gvec2[:], op0=ALU.mult, op1=ALU.mult))
                    mm_free[ti] = d3
                    T['vec_readers'] = [d3]
                    pp = POOL([d3, lx, yo_free[ti]], lambda ti=ti: nc.gpsimd.tensor_tensor(out=yo[ti][:], in0=ytmp[ti][:], in1=x1c[ti][:], op=ALU.add))
                    ytmp_free[ti] = pp
                    x1c_free[ti] = pp
                    yo_free[ti] = DMA('yo%d' % ti, pp, lambda ti=ti, tok0=tok0: nc.sync.dma_start(out=y[tok0:tok0 + 128, :], in_=yo[ti][:]))
                    out_toks.append(yo_free[ti])
                interleave((cmb_tile(i) for i in range(NTT)), 5)
            dp.wait('sp', [yo_free, out_toks[-5:]])
            for e in ('pe', 'act', 'dve', 'pool'):
                dp.wait('sp', [(e, dp.cnt[e])])
    return nc


def _rope_tables(pos):
    half = 16
    inv = (10000.0 ** (-np.arange(half, dtype=np.float32) / half)).astype(np.float32)
    ang = pos.astype(np.float32)[:, None] * inv[None, :]
    cos = np.cos(ang).astype(np.float32)
    sin = np.sin(ang).astype(np.float32)
    c = np.concatenate([cos, cos], axis=1).T
    s_ = np.concatenate([sin, sin], axis=1).T
    return np.ascontiguousarray(c), np.ascontiguousarray(s_)


def _consts(NSL):
    ident = np.eye(128, dtype=np.float32)
    egrp = np.zeros((8, 512), np.float32)
    for g in range(8):
        egrp[g, g * 64:(g + 1) * 64] = 1.0
    utri = np.triu(np.ones((128, 128), np.float32), k=1)
    tri32 = np.triu(np.ones((32, 32), np.float32), k=1)
    jv = np.tile((np.arange(NSL, dtype=np.float32) * 128.0)[None, :], (128, 1))
    pidx = np.arange(128, dtype=np.float32).reshape(128, 1)
    return dict(ident=ident, egrp=egrp, utri=utri, tri32=tri32, jv=np.ascontiguousarray(jv), pidx=pidx,
                zeros=np.zeros((128, 8192), np.float32))


def _nt(cfg):
    nqt = sum(cfg['NQG']) * 512
    nt = (2 * nqt + 32 * 127 + 127) // 128
    return ((nt + 7) // 8) * 8


WEIGHT_KEYS = ['w_ada', 'b_ada', 'g_pre1', 'g_post1', 'g_pre2', 'g_post2', 'w_in', 'g_q', 'w_uq', 'g_kv', 'w_ukv',
               'g_v_gmlp', 'w_spatial', 'b_spatial', 'g_attn_out', 'g_gmlp_out', 'w_out', 'w_router_group',
               'b_router_group', 'w_router_expert', 'b_router_expert', 'w_gate', 'w_up', 'w_down']

_NC_CACHE = {}


def kernel(**inputs):
    S = 4096
    x_all = np.concatenate([np.asarray(inputs['x_prompt'], np.float32), np.asarray(inputs['x_sample'], np.float32)], axis=0)
    c_all = np.concatenate([np.asarray(inputs['c_prompt'], np.float32), np.asarray(inputs['c_sample'], np.float32)], axis=0)
    weights = {k: np.ascontiguousarray(np.asarray(inputs[k], np.float32)) for k in WEIGHT_KEYS}
    consts = _consts(_nt(FULL_CFG))
    pos_nat = np.arange(S)
    in_maps = []
    plans = []
    for c in range(8):
        if c % 2 == 0:
            s0 = (5 * c) // 2
            A, B, Cq, qhalf = s0, s0 + 1, s0 + 2, 0
        else:
            s0 = (5 * c - 1) // 2
            Cq, qhalf, A, B = s0, 1, s0 + 1, s0 + 2
        if qhalf == 0:
            posC = pos_nat
        else:
            posC = np.concatenate([pos_nat[S // 2:], pos_nat[:S // 2]])
        xs = np.concatenate([x_all[A], x_all[B], x_all[Cq][posC]], axis=0)
        cv = np.stack([c_all[A], c_all[B], c_all[Cq]], axis=0)
        rc = np.zeros((3, 32, S), np.float32)
        rs = np.zeros((3, 32, S), np.float32)
        for i, p in enumerate([pos_nat, pos_nat, posC]):
            rc[i], rs[i] = _rope_tables(p)
        m = dict(weights)
        m.update(xs=np.ascontiguousarray(xs), cvec=np.ascontiguousarray(cv), rope_c=rc, rope_s=rs)
        m.update(consts)
        in_maps.append(m)
        plans.append((A, B, Cq, qhalf))
    if 'full' not in _NC_CACHE:
        _NC_CACHE['full'] = build(FULL_CFG)
    nc = _NC_CACHE['full']
    res = run_bass_kernel_spmd(nc, in_maps, core_ids=list(range(8)))
    y_all = np.zeros((20, S, D), np.float32)
    for c in range(8):
        yc = res.results[c]['y']
        A, B, Cq, qhalf = plans[c]
        y_all[A] = yc[0:S]
        y_all[B] = yc[S:2 * S]
        if qhalf == 0:
            y_all[Cq, 0:S // 2] = yc[2 * S:2 * S + S // 2]
        else:
            y_all[Cq, S // 2:] = yc[2 * S:2 * S + S // 2]
    return (np.ascontiguousarray(y_all[0:4]), np.ascontiguousarray(y_all[4:20]))
```

```python
import numpy as np
import concourse.bass as bass
import concourse.mybir as mybir
from concourse.bass_utils import run_bass_kernel_spmd
from contextlib import ExitStack

F32, BF16 = mybir.dt.float32, mybir.dt.bfloat16
I32 = mybir.dt.int32
AF = mybir.ActivationFunctionType
ALU = mybir.AluOpType
AX = mybir.AxisListType
D = 1024
EPS = 1e-6
NE = 32
QSCALE = 96.0 ** -0.5

FULL_CFG = dict(S=4096, NSEQ=3, NQG=[8, 8, 4])


class Dep:
    def __init__(self, nc, es):
        self.nc = nc
        self.es = es
        self.eng = {'pe': nc.tensor, 'act': nc.scalar, 'dve': nc.vector, 'pool': nc.gpsimd, 'sp': nc.sync}
        self.sem = {e: es.enter_context(nc.semaphore('s_' + e)) for e in self.eng}
        self.cnt = {e: 0 for e in self.eng}
        self.waited = {e: {} for e in self.eng}
        self.dsem = {}
        self.entries = {}
        self.free = []
        self.sw_names = set()

    def semof(self, k):
        return self.sem[k] if k in self.sem else self.entries[k][0]

    def wait(self, e, deps):
        mx = {}
        for k, v in _flat(deps):
            if v > mx.get(k, 0):
                mx[k] = v
        for k, v in mx.items():
            if self.waited[e].get(k, 0) < v:
                self.eng[e].wait_ge(self.semof(k), v)
                self.waited[e][k] = v

    def op(self, e, deps, fn, sig=True):
        self.wait(e, deps)
        ins = fn()
        if sig:
            ins.then_inc(self.sem[e], 1)
            self.cnt[e] += 1
            return (e, self.cnt[e])
        return None

    def dma(self, q, name, deps, fn):
        if name not in self.dsem:
            if q != 'pool' and self.free:
                key = self.free.pop()
            else:
                key = 'D%d' % len(self.entries)
                self.entries[key] = [self.es.enter_context(self.nc.semaphore('d_' + key)), 0]
            self.dsem[name] = key
            if q == 'pool':
                self.sw_names.add(name)
        assert (name in self.sw_names) == (q == 'pool'), name
        key = self.dsem[name]
        self.wait(q, deps)
        ins = fn()
        ent = self.entries[key]
        ins.then_inc(ent[0], 16)
        ent[1] += 16
        return (key, ent[1])

    def reserve(self, name):
        key = 'D%d' % len(self.entries)
        self.entries[key] = [self.es.enter_context(self.nc.semaphore('d_' + key)), 0]
        self.dsem[name] = key
        self.sw_names.add(name)

    def mark(self):
        return set(self.dsem.keys())

    def retire_since(self, mark, keep=()):
        for n in list(self.dsem.keys()):
            if n in mark or n in keep:
                continue
            key = self.dsem[n]
            self.wait('sp', (key, self.entries[key][1]))
            if n in self.sw_names:
                continue
            del self.dsem[n]
            self.free.append(key)
        return self.op('sp', None, lambda: self.nc.sync.nop())

    def last(self):
        return [(e, self.cnt[e]) for e in self.eng if self.cnt[e] > 0]


def _flat(deps):
    out = []
    if deps is None:
        return out
    if isinstance(deps, tuple) and len(deps) == 2 and isinstance(deps[0], str):
        return [deps]
    for d in deps:
        out.extend(_flat(d))
    return out


def interleave(gens, depth):
    active = []
    it = iter(gens)
    done = False
    while True:
        if len(active) < depth and not done:
            try:
                active.append(next(it))
            except StopIteration:
                done = True
        if not active:
            break
        nxt = []
        for g in active:
            try:
                next(g)
                nxt.append(g)
            except StopIteration:
                pass
        active = nxt


def build(cfg):
    S = cfg['S']
    NSEQ = cfg['NSEQ']
    NQG = cfg['NQG']
    NG = S // 512
    KB = S // 128
    NT = NSEQ * S
    NQT = sum(NQG) * 512
    NGB = sum(NQG)
    NSL = (2 * NQT + 32 * 127 + 127) // 128
    NSL = ((NSL + 7) // 8) * 8

    nc = bass.Bass("TRN2", target_bir_lowering=False)

    def din(name, shape, dt=F32):
        return nc.dram_tensor(name, list(shape), dt, kind="ExternalInput").ap()

    def dscr(name, shape, dt):
        return nc.dram_tensor(name, list(shape), dt, kind="Internal").ap()

    xs = din("xs", [NT, D])
    cvec = din("cvec", [NSEQ, D])
    rope_c = din("rope_c", [NSEQ, 32, S])
    rope_s = din("rope_s", [NSEQ, 32, S])
    w_ada = din("w_ada", [D, 6 * D])
    b_ada = din("b_ada", [6 * D])
    g_pre1 = din("g_pre1", [D]); g_post1 = din("g_post1", [D])
    g_pre2 = din("g_pre2", [D]); g_post2 = din("g_post2", [D])
    w_in = din("w_in", [D, 1440])
    g_q = din("g_q", [256]); w_uq = din("w_uq", [256, 768])
    g_kv = din("g_kv", [128]); w_ukv = din("w_ukv", [128, 1024])
    g_v_gmlp = din("g_v_gmlp", [512])
    w_spatial = din("w_spatial", [8, 128, 128]); b_spatial = din("b_spatial", [8, 128])
    g_attn_out = din("g_attn_out", [512]); g_gmlp_out = din("g_gmlp_out", [512])
    w_out = din("w_out", [D, D])
    w_rg = din("w_router_group", [D, 4]); b_rg = din("b_router_group", [4])
    w_re = din("w_router_expert", [D, 32]); b_re = din("b_router_expert", [32])
    w_gate = din("w_gate", [NE, D, 256]); w_up = din("w_up", [NE, D, 256]); w_down = din("w_down", [NE, 256, D])
    ident_in = din("ident", [128, 128])
    egrp_in = din("egrp", [8, 512])
    utri_in = din("utri", [128, 128])
    tri32_in = din("tri32", [32, 32])
    jv_in = din("jv", [128, NSL])
    pidx_in = din("pidx", [128, 1])
    zeros_in = din("zeros", [128, 8192])
    y = nc.dram_tensor("y", [NQT, D], F32, kind="ExternalOutput").ap()

    mod_d = dscr("mod_d", [NSEQ, 6 * D], F32)
    sn_d = dscr("sn_d", [NT, 512], BF16)
    cq_d = dscr("cq_d", [NSEQ * NG, 128, 1024], BF16)
    x1_d = (nc.dram_tensor("x1_d", [NQT, D], F32, kind="ExternalOutput").ap() if cfg.get("dbg") else dscr("x1_d", [NQT, D], F32))
    wgu_r = dscr("wgu_r", [NE * 128, 8 * 512], BF16)
    wd_r = dscr("wd_r", [NE * 128, 2 * D], BF16)
    h2_d = dscr("h2_d", [NQT, D], BF16)
    xs_d = dscr("xs_d", [NSL * 128, D], BF16)
    ys_d = dscr("ys_d", [NSL * 128, D], F32)
    wkv_d = dscr("wkv_d", [128, 1024], BF16)
    wsp_d = dscr("wsp_d", [128, 1024], BF16)
    wq_d = dscr("wq_d", [128, 1536], BF16)
    wqsw_d = dscr("wqsw_d", [128, 1536], BF16)
    wo_d = dscr("wo_d", [128, 8192], BF16)

    _uid = [0]

    def U(name):
        _uid[0] += 1
        return "%s_u%d" % (name, _uid[0])

    top = ExitStack()
    with top:
        dp = Dep(nc, top)

        def PE(deps, fn, sig=True): return dp.op('pe', deps, fn, sig)
        def ACT(deps, fn, sig=True): return dp.op('act', deps, fn, sig)
        def DVE(deps, fn, sig=True): return dp.op('dve', deps, fn, sig)
        def POOL(deps, fn, sig=True): return dp.op('pool', deps, fn, sig)
        def DMA(name, deps, fn, q='sp'): return dp.dma(q, name, deps, fn)

        def mmg(out, pairs, deps, sig=True):
            n = len(pairs)
            tok = None
            for i, (l, r) in enumerate(pairs):
                tok = PE(deps if i == 0 else None,
                         lambda l=l, r=r, i=i: nc.tensor.matmul(out, lhsT=l, rhs=r, start=(i == 0), stop=(i == n - 1)),
                         sig=(sig and i == n - 1))
            return tok

        def rstd_chain(ss_ap, out_ap, inv_n, deps):
            t = ACT(deps, lambda: nc.scalar.activation(out=out_ap, in_=ss_ap, func=AF.Sqrt, bias=EPS, scale=inv_n))
            return DVE(t, lambda: nc.vector.reciprocal(out=out_ap, in_=out_ap))

        bg = []
        wcast = []
        zero_tok = []
        dp.reserve('wcast')
        dp.reserve('zero')
        for e in range(NE):
            bg.append(lambda e=e: wcast.append(DMA('wcast', None, lambda: nc.gpsimd.dma_start(
                out=wgu_r[e * 128:(e + 1) * 128, :].rearrange("p (k c) -> p k c", k=8)[:, :, 0:256],
                in_=w_gate[e].rearrange("(k p) c -> p k c", p=128)), q='pool')))
            bg.append(lambda e=e: wcast.append(DMA('wcast', None, lambda: nc.gpsimd.dma_start(
                out=wgu_r[e * 128:(e + 1) * 128, :].rearrange("p (k c) -> p k c", k=8)[:, :, 256:512],
                in_=w_up[e].rearrange("(k p) c -> p k c", p=128)), q='pool')))
            bg.append(lambda e=e: wcast.append(DMA('wcast', None, lambda: nc.gpsimd.dma_start(
                out=wd_r[e * 128:(e + 1) * 128, :].rearrange("p (j c) -> p j c", j=2),
                in_=w_down[e].rearrange("(j p) c -> p j c", p=128)), q='pool')))
        nz = (NSL * 128 * D) // (128 * 8192)
        xs_flat = xs_d.rearrange("(n p r) c -> n p (r c)", p=128, r=8)
        for zi in range(nz):
            bg.append(lambda zi=zi: zero_tok.append(DMA('zero', None, lambda: nc.gpsimd.dma_start(out=xs_flat[zi], in_=zeros_in), q='pool')))

        ident_f = top.enter_context(nc.sbuf_tensor(U("ident_f"), [128, 128], F32))
        ident_b = top.enter_context(nc.sbuf_tensor(U("ident_b"), [128, 128], BF16))
        ones_f = top.enter_context(nc.sbuf_tensor(U("ones_f"), [128, 64], F32))
        t_id = DMA('c0', None, lambda: nc.sync.dma_start(out=ident_f[:], in_=ident_in))
        t_idb = DVE(t_id, lambda: nc.vector.tensor_copy(out=ident_b[:], in_=ident_f[:]))
        t_ones = DVE(None, lambda: nc.vector.memset(ones_f[:], 1.0))

        mk0 = dp.mark()
        with ExitStack() as pes:
            def sb(name, shape, dt): return pes.enter_context(nc.sbuf_tensor(U(name), shape, dt))
            def ps(name, shape, dt): return pes.enter_context(nc.psum_tensor(U(name), shape, dt))
            csT = sb("csT", [128, 8, NSEQ], F32)
            csS = sb("csS", [128, 8, NSEQ], F32)
            wblk = [sb("wblk%d" % i, [128, 8, 512], F32) for i in range(2)]
            brep = sb("brep", [NSEQ, 6 * D], F32)
            modsb = sb("modsb", [NSEQ, 6 * D], F32)
            pmod = [ps("pmod%d" % i, [128, 512], F32) for i in range(2)]
            t_c = [DMA('p0', None, lambda q=q: nc.sync.dma_start(out=csT[:, :, q], in_=cvec[q].rearrange("(k p) -> p k", p=128),
                                                                 allow_slow_non_contiguous=True)) for q in range(NSEQ)]
            t_b = DMA('p1', None, lambda: nc.sync.dma_start(out=brep[:], in_=b_ada.partition_broadcast(NSEQ)))
            t_cs = ACT(t_c, lambda: nc.scalar.activation(out=csS[:], in_=csT[:], func=AF.Silu))
            wfree = [None, None]
            pfree = [None, None]
            ev = None
            for blk in range(12):
                i = blk % 2
                t_w = DMA('pw%d' % i, wfree[i], lambda blk=blk, i=i: nc.sync.dma_start(
                    out=wblk[i][:], in_=w_ada[:, blk * 512:(blk + 1) * 512].rearrange("(k p) c -> p k c", p=128)))
                t_m = mmg(pmod[i][0:NSEQ, :], [(csS[:, k, :], wblk[i][:, k, :]) for k in range(8)], [t_w, t_cs, pfree[i]])
                wfree[i] = t_m
                ev = DVE([t_m, t_b], lambda blk=blk, i=i: nc.vector.tensor_tensor(
                    out=modsb[:, blk * 512:(blk + 1) * 512], in0=pmod[i][0:NSEQ, :],
                    in1=brep[:, blk * 512:(blk + 1) * 512], op=ALU.add))
                pfree[i] = ev
            t_mod = DMA('p2', ev, lambda: nc.sync.dma_start(out=mod_d, in_=modsb[:]))

            tmpq = sb("tmpq", [128, 2, 768], F32)
            gq = sb("gq", [128, 2], F32)
            wq_t = sb("wq_t", [128, 2, 768], BF16)
            wqsw_t = sb("wqsw_t", [128, 2, 768], BF16)
            t1 = DMA('p3', None, lambda: nc.sync.dma_start(out=tmpq[:], in_=w_uq.rearrange("(k p) c -> p k c", p=128)))
            t2 = DMA('p3', None, lambda: nc.sync.dma_start(out=gq[:], in_=g_q.rearrange("(k p) -> p k", p=128),
                                                          allow_slow_non_contiguous=True))
            tq = None
            for k in range(2):
                tq = DVE([t1, t2], lambda k=k: nc.vector.tensor_scalar(
                    out=wq_t[:, k, :], in0=tmpq[:, k, :], scalar1=gq[:, k:k + 1], scalar2=QSCALE,
                    op0=ALU.mult, op1=ALU.mult))
            tz = POOL(None, lambda: nc.gpsimd.memset(wqsw_t[:], 0.0))
            wq4 = wq_t[:].rearrange("p k (h c) -> p k h c", h=8)
            wqs4 = wqsw_t[:].rearrange("p k (h c) -> p k h c", h=8)
            ta = DVE([tq, tz], lambda: nc.vector.tensor_scalar(out=wqs4[:, :, :, 64:80], in0=wq4[:, :, :, 80:96],
                                                              scalar1=-1.0, scalar2=None, op0=ALU.mult))
            tb = DVE(None, lambda: nc.vector.tensor_copy(out=wqs4[:, :, :, 80:96], in_=wq4[:, :, :, 64:80]))
            t_wq = DMA('p4', tq, lambda: nc.sync.dma_start(out=wq_d, in_=wq_t[:].rearrange("p k c -> p (k c)")))
            t_wqsw = DMA('p4', [ta, tb], lambda: nc.sync.dma_start(out=wqsw_d, in_=wqsw_t[:].rearrange("p k c -> p (k c)")))

            tmpkv = sb("tmpkv", [128, 1024], F32)
            gkv = sb("gkv", [128, 1], F32)
            wkv_t = sb("wkv_t", [128, 1024], BF16)
            t1 = DMA('p5', None, lambda: nc.sync.dma_start(out=tmpkv[:], in_=w_ukv))
            t2 = DMA('p5', None, lambda: nc.sync.dma_start(out=gkv[:], in_=g_kv.rearrange("(p o) -> p o", o=1)))
            tk = DVE([t1, t2], lambda: nc.vector.tensor_scalar(out=wkv_t[:], in0=tmpkv[:], scalar1=gkv[:, 0:1],
                                                              scalar2=None, op0=ALU.mult))
            t_wkv = DMA('p6', tk, lambda: nc.sync.dma_start(out=wkv_d, in_=wkv_t[:]))

            tmpo = sb("tmpo", [128, 8, 1024], F32)
            gcat = sb("gcat", [128, 8], F32)
            wo_t = sb("wo_t", [128, 8, 1024], BF16)
            t1 = DMA('p7', None, lambda: nc.sync.dma_start(out=tmpo[:], in_=w_out.rearrange("(k p) c -> p k c", p=128)))
            t2 = DMA('p7', None, lambda: nc.sync.dma_start(out=gcat[:, 0:4], in_=g_attn_out.rearrange("(k p) -> p k", p=128),
                                                          allow_slow_non_contiguous=True))
            t3 = DMA('p7', None, lambda: nc.sync.dma_start(out=gcat[:, 4:8], in_=g_gmlp_out.rearrange("(k p) -> p k", p=128),
                                                          allow_slow_non_contiguous=True))
            two = None
            for k in range(8):
                two = DVE([t1, t2, t3], lambda k=k: nc.vector.tensor_scalar(
                    out=wo_t[:, k, :], in0=tmpo[:, k, :], scalar1=gcat[:, k:k + 1], scalar2=None, op0=ALU.mult))
            t_wo = DMA('p8', two, lambda: nc.sync.dma_start(out=wo_d, in_=wo_t[:].rearrange("p k c -> p (k c)")))

            tmps = sb("tmps", [128, 8, 128], F32)
            wsp_t = sb("wsp_t", [128, 8, 128], BF16)
            psp = ps("psp", [128, 1024], F32)
            t1 = DMA('p9', None, lambda: nc.sync.dma_start(out=tmps[:], in_=w_spatial.rearrange("g t s -> t g s")))
            tt = None
            for g in range(8):
                tt = PE([t1, t_id], lambda g=g: nc.tensor.transpose(psp[:, g * 128:(g + 1) * 128], tmps[:, g, :], ident_f[:]),
                        sig=(g == 7))
            tc_ = DVE(tt, lambda: nc.vector.tensor_copy(out=wsp_t[:].rearrange("p g t -> p (g t)"), in_=psp[:]))
            t_wsp = DMA('p10', tc_, lambda: nc.sync.dma_start(out=wsp_d, in_=wsp_t[:].rearrange("p g t -> p (g t)")))
            prep_done = [t_mod, t_wq, t_wqsw, t_wkv, t_wo, t_wsp]
            dp.retire_since(mk0, keep=('wcast', 'zero', 'c0'))
            prep_bar = dp.last()

        with ExitStack() as aes:
            def sbA(name, shape, dt): return aes.enter_context(nc.sbuf_tensor(U(name), shape, dt))
            KT = sbA("KT", [128, 8, S], BF16)
            VA = sbA("VA", [128, KB, 8, 65], BF16)
            geff1 = sbA("geff1", [128, D], F32)
            sh1r = sbA("sh1r", [128, D], F32)
            gvec1 = sbA("gvec1", [128, D], F32)
            gvrep = sbA("gvrep", [128, 512], F32)
            PA = aes.enter_context(nc.psum_tensor(U("PA"), [128, 1024], F32))
            PB = aes.enter_context(nc.psum_tensor(U("PB"), [128, 1024], F32))
            PC = aes.enter_context(nc.psum_tensor(U("PC"), [128, 1024], F32))
            PD = aes.enter_context(nc.psum_tensor(U("PD"), [128, 1024], F32))

            t_va1 = POOL(prep_bar, lambda: nc.gpsimd.memset(VA[:, :, :, 64:65], 1.0))
            t_gv = DMA('a0', prep_bar, lambda: nc.sync.dma_start(out=gvrep[:], in_=g_v_gmlp.partition_broadcast(128)))
            seq_bar = [prep_bar, prep_done, t_va1, t_gv, t_idb, t_ones]
            qbase = 0
            for s in range(NSEQ):
                mk1 = dp.mark()
                with ExitStack() as p1:
                    def sb1(name, shape, dt): return p1.enter_context(nc.sbuf_tensor(U(name), shape, dt))
                    wAs = sb1("wAs", [128, 8, 384], BF16)
                    wAuv = sb1("wAuv", [128, 8, 1024], BF16)
                    wAkr = sb1("wAkr", [128, 8, 96], BF16)
                    wAks = sb1("wAks", [128, 8, 96], BF16)
                    wkv = sb1("wkv", [128, 1024], BF16)
                    wsp = sb1("wsp", [128, 8, 128], BF16)
                    bsp = sb1("bsp", [8, 128], F32)
                    egrp = sb1("egrp", [8, 512], F32)
                    vt = [sb1("vt%d" % i, [128, D], F32) for i in range(2)]
                    xt = [sb1("xt%d" % i, [128, D], F32) for i in range(2)]
                    junk = sb1("junk", [128, D], BF16)
                    hm = sb1("hm", [128, D], F32)
                    hb = [sb1("hb%d" % i, [128, D], BF16) for i in range(2)]
                    hT = sb1("hT", [128, 8, 512], BF16)
                    zsb = [sb1("zsb%d" % i, [128, 384], BF16) for i in range(2)]
                    cqnT = [sb1("cqnT%d" % i, [128, 2, 512], BF16) for i in range(2)]
                    ckvnT = [sb1("ckvnT%d" % i, [128, 512], BF16) for i in range(2)]
                    gu = [sb1("gu%d" % i, [128, 512], BF16) for i in range(2)]
                    gv = [sb1("gv%d" % i, [128, 512], F32) for i in range(2)]
                    zraw = [sb1("zraw%d" % i, [128, 384], F32) for i in range(2)]
                    vn = [sb1("vn%d" % i, [128, 512], BF16) for i in range(2)]
                    sraw = [sb1("sraw%d" % i, [128, 512], F32) for i in range(2)]
                    sn = [sb1("sn%d" % i, [128, 512], BF16) for i in range(2)]
                    stt = [sb1("stt%d" % i, [128, 16], F32) for i in range(2)]
                    Ctt = [sb1("Ctt%d" % i, [128, 128], F32) for i in range(2)]
                    Stt = [sb1("Stt%d" % i, [128, 128], F32) for i in range(2)]
                    kt1 = [sb1("kt1_%d" % i, [128, 128], F32) for i in range(2)]
                    kt2 = [sb1("kt2_%d" % i, [128, 128], F32) for i in range(2)]
                    krr = [sb1("krr%d" % i, [128, 128], BF16) for i in range(2)]

                    pT = PA[:, 0:512].bitcast(BF16)
                    pT2 = PA[:, 512:1024].bitcast(BF16)
                    pzs = PB[:, 0:384]
                    pss = PB[:, 512:1024]
                    pu = PC[:, 0:512]
                    pv = PC[:, 512:1024]
                    pkr = PD[:, 0:512]
                    pks = PD[:, 512:1024]

                    sb_ = seq_bar
                    wl = []
                    wl.append(DMA('a1', sb_, lambda: nc.gpsimd.dma_start(
                        out=wAs[:], in_=w_in[:, 0:384].rearrange("(k p) c -> p k c", p=128)), q='pool'))
                    wl.append(DMA('a1', sb_, lambda: nc.gpsimd.dma_start(
                        out=wAuv[:], in_=w_in[:, 416:1440].rearrange("(k p) c -> p k c", p=128)), q='pool'))
                    tz1 = POOL(sb_, lambda: nc.gpsimd.memset(wAkr[:], 0.0))
                    tz2 = POOL(sb_, lambda: nc.gpsimd.memset(wAks[:], 0.0))
                    wl.append(DMA('a1', [tz1], lambda: nc.gpsimd.dma_start(
                        out=wAkr[:, :, 64:96], in_=w_in[:, 384:416].rearrange("(k p) c -> p k c", p=128)), q='pool'))
                    tn = DMA('a2', [tz2], lambda: nc.gpsimd.dma_start(
                        out=wAks[:, :, 64:80], in_=w_in[:, 400:416].rearrange("(k p) c -> p k c", p=128)), q='pool')
                    wl.append(DMA('a1', [tz2], lambda: nc.gpsimd.dma_start(
                        out=wAks[:, :, 80:96], in_=w_in[:, 384:400].rearrange("(k p) c -> p k c", p=128)), q='pool'))
                    wl.append(POOL(tn, lambda: nc.gpsimd.tensor_scalar(out=wAks[:, :, 64:80], in0=wAks[:, :, 64:80],
                                                                      scalar1=-1.0, scalar2=None, op0=ALU.mult)))
                    wl.append(DMA('a3', sb_, lambda: nc.sync.dma_start(out=wkv[:], in_=wkv_d)))
                    wl.append(DMA('a3', sb_, lambda: nc.sync.dma_start(out=wsp[:].rearrange("p g t -> p (g t)"), in_=wsp_d)))
                    wl.append(DMA('a3', sb_, lambda: nc.sync.dma_start(out=bsp[:], in_=b_spatial)))
                    wl.append(DMA('a3', sb_, lambda: nc.sync.dma_start(out=egrp[:], in_=egrp_in)))
                    l1 = DMA('a4_0', sb_, lambda: nc.sync.dma_start(out=vt[0][:], in_=mod_d[s, 1024:2048].partition_broadcast(128)))
                    l2 = DMA('a4_1', sb_, lambda: nc.sync.dma_start(out=vt[1][:], in_=g_pre1.partition_broadcast(128)))
                    l3 = DMA('a4_2', sb_, lambda: nc.sync.dma_start(out=sh1r[:], in_=mod_d[s, 0:1024].partition_broadcast(128)))
                    tg = DVE([l1, l2], lambda: nc.vector.scalar_tensor_tensor(out=geff1[:], in0=vt[0][:], scalar=1.0, in1=vt[1][:],
                                                                               op0=ALU.add, op1=ALU.mult))
                    l4 = DMA('a4_3', [tg], lambda: nc.sync.dma_start(out=vt[0][:], in_=mod_d[s, 2048:3072].partition_broadcast(128)))
                    l5 = DMA('a4_4', [tg], lambda: nc.sync.dma_start(out=vt[1][:], in_=g_post1.partition_broadcast(128)))
                    tg2 = DVE([l4, l5], lambda: nc.vector.tensor_tensor(out=gvec1[:], in0=vt[0][:], in1=vt[1][:], op=ALU.mult))
                    ready = [wl, l3, tg, tg2]

                    xt_free = [None, None]; hb_free = [None, None]
                    hT_free = [None] * 4
                    zraw_free = [None, None]; zsb_free = [None, None]; cq_free = [None, None]; ckv_free = [None, None]
                    gu_free = [None, None]; gv_free = [None, None]; vn_free = [None, None]; sn_free = [None, None]
                    sraw_free = [None, None]; ct_free = [None, None]; kt_free = [None, None]; krr_free = [None, None]
                    P = dict(hm_free=None, pT_free=None, pT2_free=None, pzs_free=None, pu_free=None, pv_free=None, pss_free=None,
                             pkr_free=None, pks_free=None)
                    grp = {}

                    def p1_tile(g, t):
                        gi = g % 2
                        ti = t % 2
                        if t == 0:
                            grp[g] = dict(cq_w=[], ckv_w=[])
                        G = grp[g]
                        tok0 = s * S + g * 512 + t * 128
                        ts_ = slice(t * 128, (t + 1) * 128)
                        gts = slice(g * 512 + t * 128, g * 512 + (t + 1) * 128)
                        st_ = stt[ti]
                        lx = DMA('x%d' % ti, [xt_free[ti], ready], lambda: nc.sync.dma_start(out=xt[ti][:], in_=xs[tok0:tok0 + 128, :]))
                        lc = DMA('rc%d' % ti, [ct_free[ti], ready], lambda: nc.sync.dma_start(out=Ctt[ti][64:96, :], in_=rope_c[s, :, gts]))
                        ls = DMA('rs%d' % ti, [ct_free[ti], ready], lambda: nc.sync.dma_start(out=Stt[ti][64:96, :], in_=rope_s[s, :, gts]))
                        a1 = ACT(lx, lambda: nc.scalar.activation(out=junk[:], in_=xt[ti][:], func=AF.Square, accum_out=st_[:, 0:1]))
                        yield
                        r1a = ACT(a1, lambda: nc.scalar.activation(out=st_[:, 1:2], in_=st_[:, 0:1], func=AF.Sqrt, bias=EPS, scale=1.0 / D))
                        yield
                        r1 = DVE(r1a, lambda: nc.vector.reciprocal(out=st_[:, 1:2], in_=st_[:, 1:2]))
                        d1 = DVE([r1, P['hm_free']], lambda: nc.vector.scalar_tensor_tensor(
                            out=hm[:], in0=xt[ti][:], scalar=st_[:, 1:2], in1=geff1[:], op0=ALU.mult, op1=ALU.mult))
                        xt_free[ti] = d1
                        p1_ = POOL([d1, hb_free[ti]], lambda: nc.gpsimd.tensor_tensor(out=hb[ti][:], in0=hm[:], in1=sh1r[:], op=ALU.add))
                        P['hm_free'] = p1_
                        yield
                        tp = None
                        for k in range(8):
                            tp = PE([p1_, P['pT_free']] if k == 0 else None,
                                    lambda k=k: nc.tensor.transpose(pT[:, k * 128:(k + 1) * 128], hb[ti][:, k * 128:(k + 1) * 128], ident_b[:]),
                                    sig=(k == 7))
                        hb_free[ti] = tp
                        yield
                        ev = ACT([tp, hT_free[t]], lambda: nc.scalar.copy(out=hT[:, :, ts_], in_=pT.rearrange("p (k c) -> p k c", k=8)))
                        P['pT_free'] = ev
                        yield
                        m_zs = mmg(pzs, [(hT[:, k, ts_], wAs[:, k, :]) for k in range(8)], [ev, P['pzs_free']])
                        m_u = mmg(pu, [(hT[:, k, ts_], wAuv[:, k, 0:512]) for k in range(8)], [P['pu_free']])
                        m_v = mmg(pv, [(hT[:, k, ts_], wAuv[:, k, 512:1024]) for k in range(8)], [P['pv_free']])
                        m_kr = mmg(pkr[0:96, 0:128], [(wAkr[:, k, :], hT[:, k, ts_]) for k in range(8)], [P['pkr_free']])
                        m_ks = mmg(pks[0:96, 0:128], [(wAks[:, k, :], hT[:, k, ts_]) for k in range(8)], [P['pks_free']])
                        hT_free[t] = m_ks
                        yield
                        zr = DVE([m_zs, zraw_free[ti]], lambda: nc.vector.tensor_copy(out=zraw[ti][:], in_=pzs))
                        P['pzs_free'] = zr
                        g1 = ACT([m_u, gu_free[ti]], lambda: nc.scalar.activation(out=gu[ti][:], in_=pu, func=AF.Gelu_apprx_tanh))
                        P['pu_free'] = g1
                        g2 = ACT([m_v, gv_free[ti]], lambda: nc.scalar.activation(out=gv[ti][:], in_=pv, func=AF.Gelu_apprx_tanh))
                        P['pv_free'] = g2
                        k1 = DVE([m_kr, lc, kt_free[ti]], lambda: nc.vector.tensor_tensor(out=kt1[ti][64:96, :], in0=pkr[64:96, 0:128], in1=Ctt[ti][64:96, :], op=ALU.mult))
                        P['pkr_free'] = k1
                        k2 = DVE([m_ks, ls], lambda: nc.vector.tensor_tensor(out=kt2[ti][64:96, :], in0=pks[64:96, 0:128], in1=Stt[ti][64:96, :], op=ALU.mult))
                        P['pks_free'] = k2
                        ct_free[ti] = k2
                        yield
                        a2 = ACT(zr, lambda: nc.scalar.activation(out=junk[:, 0:256], in_=zraw[ti][:, 0:256], func=AF.Square, accum_out=st_[:, 2:3]))
                        a3 = ACT(None, lambda: nc.scalar.activation(out=junk[:, 256:384], in_=zraw[ti][:, 256:384], func=AF.Square, accum_out=st_[:, 3:4]))
                        g3 = ACT(g2, lambda: nc.scalar.activation(out=junk[:, 0:512], in_=gv[ti][:], func=AF.Square, accum_out=st_[:, 6:7]))
                        k3 = DVE([k1, k2, krr_free[ti]], lambda: nc.vector.tensor_tensor(out=krr[ti][64:96, :], in0=kt1[ti][64:96, :], in1=kt2[ti][64:96, :], op=ALU.add))
                        kt_free[ti] = k3
                        kc = None
                        for h in range(8):
                            kc = POOL(k3, lambda h=h: nc.gpsimd.tensor_copy(out=KT[64:96, h, gts], in_=krr[ti][64:96, :]))
                        krr_free[ti] = kc
                        yield
                        q1 = ACT([a2, a3], lambda: nc.scalar.activation(out=st_[:, 4:5], in_=st_[:, 2:3], func=AF.Sqrt, bias=EPS, scale=1.0 / 256))
                        q2 = ACT(None, lambda: nc.scalar.activation(out=st_[:, 5:6], in_=st_[:, 3:4], func=AF.Sqrt, bias=EPS, scale=1.0 / 128))
                        q3 = ACT(g3, lambda: nc.scalar.activation(out=st_[:, 7:8], in_=st_[:, 6:7], func=AF.Sqrt, bias=EPS, scale=1.0 / 512))
                        yield
                        r2 = DVE([q1, q2], lambda: nc.vector.reciprocal(out=st_[:, 4:6], in_=st_[:, 4:6]))
                        r4 = DVE(q3, lambda: nc.vector.reciprocal(out=st_[:, 7:8], in_=st_[:, 7:8]))
                        d2 = DVE([r4, vn_free[ti]], lambda: nc.vector.scalar_tensor_tensor(
                            out=vn[ti][:], in0=gv[ti][:], scalar=st_[:, 7:8], in1=gvrep[:], op0=ALU.mult, op1=ALU.mult))
                        gv_free[ti] = d2
                        c1 = ACT([r2, zsb_free[ti]], lambda: nc.scalar.activation(
                            out=zsb[ti][:, 0:256], in_=zraw[ti][:, 0:256], func=AF.Copy, scale=st_[:, 4:5]))
                        c2 = ACT(None, lambda: nc.scalar.activation(
                            out=zsb[ti][:, 256:384], in_=zraw[ti][:, 256:384], func=AF.Copy, scale=st_[:, 5:6]))
                        zraw_free[ti] = c2
                        yield
                        tp2 = None
                        for k in range(3):
                            tp2 = PE([c1, c2, P['pT2_free']] if k == 0 else None,
                                     lambda k=k: nc.tensor.transpose(pT2[:, k * 128:(k + 1) * 128], zsb[ti][:, k * 128:(k + 1) * 128], ident_b[:]),
                                     sig=(k == 2))
                        zsb_free[ti] = tp2
                        PE([P['pss_free'], ready], lambda: nc.tensor.matmul(pss, lhsT=bsp[:, :], rhs=egrp[:, :], start=True, stop=False), sig=False)
                        m_s = None
                        for gg in range(8):
                            m_s = PE(d2 if gg == 0 else None,
                                     lambda gg=gg: nc.tensor.matmul(pss[:, gg * 64:(gg + 1) * 64], lhsT=wsp[:, gg, :],
                                                                    rhs=vn[ti][:, gg * 64:(gg + 1) * 64], start=False, stop=(gg == 7)),
                                     sig=(gg == 7))
                        vn_free[ti] = m_s
                        yield
                        e1 = DVE([tp2, cq_free[gi] if t == 0 else None], lambda: nc.vector.tensor_copy(
                            out=cqnT[gi][:, :, ts_], in_=pT2[:, 0:256].rearrange("p (k c) -> p k c", k=2)))
                        e2 = DVE([ckv_free[gi] if t == 0 else None], lambda: nc.vector.tensor_copy(
                            out=ckvnT[gi][:, ts_], in_=pT2[:, 256:384]))
                        P['pT2_free'] = e2
                        G['cq_w'].append(e1)
                        G['ckv_w'].append(e2)
                        d3 = DVE([m_s, g1, sraw_free[ti]], lambda: nc.vector.tensor_tensor(out=sraw[ti][:], in0=gu[ti][:], in1=pss, op=ALU.mult))
                        P['pss_free'] = d3
                        gu_free[ti] = d3
                        yield
                        a4 = ACT(d3, lambda: nc.scalar.activation(out=junk[:, 0:512], in_=sraw[ti][:], func=AF.Square, accum_out=st_[:, 8:9]))
                        yield
                        q4 = ACT(a4, lambda: nc.scalar.activation(out=st_[:, 9:10], in_=st_[:, 8:9], func=AF.Sqrt, bias=EPS, scale=1.0 / 512))
                        yield
                        r5 = DVE(q4, lambda: nc.vector.reciprocal(out=st_[:, 9:10], in_=st_[:, 9:10]))
                        yield
                        c3 = ACT([r5, sn_free[ti]], lambda: nc.scalar.activation(out=sn[ti][:], in_=sraw[ti][:], func=AF.Copy, scale=st_[:, 9:10]))
                        sraw_free[ti] = c3
                        sn_free[ti] = DMA('sn%d' % ti, c3, lambda: nc.sync.dma_start(out=sn_d[tok0:tok0 + 128, :], in_=sn[ti][:]))
                        if t != 3:
                            return
                        yield
                        gs = slice(g * 512, (g + 1) * 512)
                        cq_free[gi] = DMA('cq%d' % gi, G['cq_w'], lambda: nc.sync.dma_start(
                            out=cq_d[s * NG + g], in_=cqnT[gi][:].rearrange("p k c -> p (k c)")))
                        bank_free = [P['pkr_free'], P['pks_free']]
                        banks = [pkr, pks]
                        for h in range(8):
                            bi = h % 2
                            mk = mmg(banks[bi][0:64, :], [(wkv[:, h * 128:h * 128 + 64], ckvnT[gi][:, :])], [G['ckv_w'], bank_free[bi]])
                            if h % 2 == 0:
                                bank_free[bi] = ACT(mk, lambda h=h, bi=bi: nc.scalar.copy(out=KT[0:64, h, gs], in_=banks[bi][0:64, :]))
                            else:
                                bank_free[bi] = DVE(mk, lambda h=h, bi=bi: nc.vector.tensor_copy(out=KT[0:64, h, gs], in_=banks[bi][0:64, :]))
                        wkv3 = wkv[:].rearrange("p (h c) -> p h c", h=8)[:, :, 64:128]
                        mv = None
                        for tt in range(4):
                            bi = tt % 2
                            kb = g * 4 + tt
                            mv = mmg(banks[bi][:, :].rearrange("p (h c) -> p h c", h=8), [(ckvnT[gi][:, tt * 128:(tt + 1) * 128], wkv3)], [bank_free[bi]])
                            bank_free[bi] = DVE(mv, lambda kb=kb, bi=bi: nc.vector.tensor_copy(
                                out=VA[:, kb, :, 0:64], in_=banks[bi][:, :].rearrange("p (h c) -> p h c", h=8)))
                        ckv_free[gi] = mv
                        P['pkr_free'] = bank_free[0]
                        P['pks_free'] = bank_free[1]

                    interleave((p1_tile(g, t) for g in range(NG) for t in range(4)), 2)
                    dp.retire_since(mk1)
                    p1_bar = dp.last() + [sn_free, cq_free]

                mk2 = dp.mark()
                with ExitStack() as p2:
                    def sb2(name, shape, dt): return p2.enter_context(nc.sbuf_tensor(U(name), shape, dt))
                    wq = sb2("wq", [128, 2, 768], BF16)
                    wqs = sb2("wqs", [128, 2, 768], BF16)
                    wo = sb2("wo", [128, 8, 1024], BF16)
                    cqT = [sb2("cqT%d" % i, [128, 2, 512], BF16) for i in range(2)]
                    Ct = sb2("Ct2", [128, 512], F32)
                    St = sb2("St2", [128, 512], F32)
                    qt1 = [sb2("qt1_%d" % i, [128, 512], F32) for i in range(2)]
                    qt2 = [sb2("qt2_%d" % i, [128, 512], F32) for i in range(2)]
                    qt_free = [None, None]
                    QT = sb2("QT", [128, 8, 512], BF16)
                    pTs = [sb2("pTs%d" % i, [128, 1024], BF16) for i in range(3)]
                    osb = [sb2("osb%d" % i, [128, 512], F32) for i in range(2)]
                    rinv = [sb2("rinv0", [128, 512], F32)] * 2
                    aT = [sb2("aT%d" % i, [128, 512], BF16) for i in range(2)]
                    merged = [sb2("merged%d" % i, [128, D], BF16) for i in range(4)]
                    mT = [sb2("mT%d" % i, [128, 8, 128], BF16) for i in range(2)]
                    otmp = sb2("otmp", [128, D], F32)
                    xr = [sb2("xr%d" % i, [128, D], F32) for i in range(2)]
                    x1 = [sb2("x1_%d" % i, [128, D], F32) for i in range(2)]
                    junk = sb2("junk2", [128, D], BF16)
                    stt = [sb2("stq%d" % i, [128, 16], F32) for i in range(2)]

                    po = PC[:, 0:512]
                    prb = PC[:, 512:1024]
                    pmT = PC[:, 512:1024].bitcast(BF16)
                    pa = PD[:, :].bitcast(BF16)
                    scT = [PA, PB]

                    wl2 = [DMA('b1', p1_bar, lambda: nc.sync.dma_start(out=wq[:].rearrange("p k c -> p (k c)"), in_=wq_d)),
                           DMA('b1', p1_bar, lambda: nc.sync.dma_start(out=wqs[:].rearrange("p k c -> p (k c)"), in_=wqsw_d)),
                           DMA('b1', p1_bar, lambda: nc.sync.dma_start(out=wo[:].rearrange("p k c -> p (k c)"), in_=wo_d))]
                    ready2 = [p1_bar, wl2]
                    cq_free2 = [None, None]; rope_free = None; QT_free = []
                    sc_free = [None, None]; pTs_free = [None, None, None]; po_free = None; osb_free = [None, None]
                    rinv_free = [None]; prb_free = None; aT_free = [None, None]; pa_free = []
                    merged_free = [None] * 4; mT_free = [None, None]; pmT_free = None
                    otmp_free = None; xr_free = [None, None]; x1_free = [None, None]
                    step = 0
                    tcount = 0
                    for qg in range(NQG[s]):
                        gi = qg % 2
                        gs = slice(qg * 512, (qg + 1) * 512)
                        lq = DMA('cql%d' % gi, [cq_free2[gi], ready2], lambda gi=gi, qg=qg: nc.sync.dma_start(
                            out=cqT[gi][:].rearrange("p k c -> p (k c)"), in_=cq_d[s * NG + qg]))
                        lr1 = DMA('rp2c', [rope_free, ready2], lambda gs=gs: nc.sync.dma_start(out=Ct[64:96, :], in_=rope_c[s, :, gs]))
                        lr2 = DMA('rp2s', [rope_free, ready2], lambda gs=gs: nc.sync.dma_start(out=St[64:96, :], in_=rope_s[s, :, gs]))
                        pre_ld = []
                        for t in range(4):
                            tok0_ = s * S + qg * 512 + t * 128
                            l_sn = DMA('snl%d' % t, [merged_free[t], ready2], lambda t=t, tok0_=tok0_: nc.sync.dma_start(
                                out=merged[t][:, 512:1024], in_=sn_d[tok0_:tok0_ + 128, :]))
                            pre_ld.append(l_sn)
                        wq4 = wq[:].rearrange("p k (h c) -> p k h c", h=8)
                        wqs4 = wqs[:].rearrange("p k (h c) -> p k h c", h=8)
                        QT_w = []
                        Qs = dict(mqs=None, qd=None)

                        def q_head(h):
                            T = scT[h % 2]
                            hi = h % 2
                            mq = mmg(T[0:96, 0:512], [(wq4[:, k, h, :], cqT[gi][:, k, :]) for k in range(2)], [lq, sc_free[h % 2]])
                            mqs = mmg(T[0:96, 512:1024], [(wqs4[:, k, h, :], cqT[gi][:, k, :]) for k in range(2)], None)
                            Qs['mqs'] = mqs
                            yield
                            c0 = ACT([mq, QT_free if h == 0 else None], lambda: nc.scalar.copy(out=QT[0:64, h, :], in_=T[0:64, 0:512]))
                            q1 = DVE([mq, lr1, qt_free[hi]], lambda: nc.vector.tensor_tensor(out=qt1[hi][64:96, :], in0=T[64:96, 0:512], in1=Ct[64:96, :], op=ALU.mult))
                            q2 = DVE([mqs, lr2], lambda: nc.vector.tensor_tensor(out=qt2[hi][64:96, :], in0=T[64:96, 512:1024], in1=St[64:96, :], op=ALU.mult))
                            sc_free[h % 2] = [c0, q2]
                            yield
                            qd = DVE([q1, q2, QT_free if h == 0 else None], lambda: nc.vector.tensor_tensor(
                                out=QT[64:96, h, :], in0=qt1[hi][64:96, :], in1=qt2[hi][64:96, :], op=ALU.add))
                            qt_free[hi] = qd
                            Qs['qd'] = qd
                            QT_w.extend([c0, qd])
                        interleave((q_head(h) for h in range(8)), 2)
                        mqs = Qs['mqs']
                        qd = Qs['qd']
                        cq_free2[gi] = mqs
                        rope_free = qd
                        NP = KB // 2
                        steps = [(h, j) for h in range(8) for j in range(NP)]
                        qk_tok = {}

                        def emit_qk(idx):
                            h, j = steps[idx]
                            T = scT[idx % 2]
                            tk = None
                            for u in range(2):
                                kb = 2 * j + u
                                tk = PE([sc_free[idx % 2], QT_w] if u == 0 else None,
                                        lambda h=h, kb=kb, u=u, T=T: nc.tensor.matmul(
                                            T[:, u * 512:(u + 1) * 512], lhsT=KT[0:96, h, kb * 128:(kb + 1) * 128], rhs=QT[0:96, h, :],
                                            start=True, stop=True), sig=(u == 1))
                            qk_tok[idx] = tk

                        emit_qk(0)
                        pa_w = []
                        QT_readers = []
                        pending = []
                        A = dict(prb_free=prb_free)
                        for idx, (h, j) in enumerate(steps):
                            if idx + 1 < len(steps):
                                emit_qk(idx + 1)
                            T = scT[idx % 2]
                            sl = step % 3
                            step += 1
                            ex = ACT([qk_tok[idx], pTs_free[sl]], lambda T=T, sl=sl: nc.scalar.activation(out=pTs[sl][:], in_=T[:, :], func=AF.Exp))
                            sc_free[idx % 2] = ex
                            pvt = None
                            for u in range(2):
                                kb = 2 * j + u
                                pvt = PE([ex, po_free if (j == 0 and u == 0) else None],
                                         lambda h=h, kb=kb, u=u, sl=sl: nc.tensor.matmul(
                                             po[0:65, :], lhsT=VA[:, kb, h, :], rhs=pTs[sl][:, u * 512:(u + 1) * 512],
                                             start=(kb == 0), stop=(kb == KB - 1)), sig=(u == 1))
                            pTs_free[sl] = pvt
                            for pend in list(pending):
                                pend[0] -= 1
                                if pend[0] <= 0:
                                    pend[1]()
                                    pending.remove(pend)
                            if j == min(1, NP - 1) and bg:
                                bg.pop(0)()
                            if j == NP - 1:
                                for pend in list(pending):
                                    pend[1]()
                                    pending.remove(pend)
                                oi = h % 2
                                QT_readers.append(pvt)
                                o1 = DVE([pvt, osb_free[oi]], lambda oi=oi: nc.vector.tensor_copy(out=osb[oi][0:65, :], in_=po[0:65, :]))
                                po_free = o1
                                o2 = DVE([o1, rinv_free[0]], lambda oi=oi: nc.vector.reciprocal(out=rinv[oi][64:65, :], in_=osb[oi][64:65, :]))
                                hs = dict(o2=o2, oi=oi, h=h)

                                def part_a(hs=hs):
                                    oi = hs['oi']
                                    o3 = PE([hs['o2'], A['prb_free']], lambda oi=oi: nc.tensor.matmul(prb[0:64, :], lhsT=ones_f[64:65, 0:64], rhs=rinv[oi][64:65, :],
                                                                                                 start=True, stop=True))
                                    rinv_free[0] = o3
                                    o4 = DVE([o3, aT_free[oi]], lambda oi=oi: nc.vector.tensor_tensor(out=aT[oi][0:64, :], in0=osb[oi][0:64, :],
                                                                                                       in1=prb[0:64, :], op=ALU.mult))
                                    A['prb_free'] = o4
                                    osb_free[oi] = o4
                                    hs['o4'] = o4

                                def part_b(hs=hs):
                                    oi = hs['oi']; h = hs['h']
                                    o5 = None
                                    for t in range(4):
                                        o5 = PE([hs['o4'], pa_free if h == 0 else None] if t == 0 else None,
                                                lambda t=t, h=h, oi=oi: nc.tensor.transpose(
                                                    pa[:, t * 512 + h * 64: t * 512 + (h + 1) * 64], aT[oi][0:64, t * 128:(t + 1) * 128], ident_b[0:64, 0:64]),
                                                sig=(t == 3))
                                    aT_free[oi] = o5
                                    pa_w.append(o5)
                                pending.append([2, part_a])
                                pending.append([4, part_b])
                        for pend in list(pending):
                            pend[1]()
                            pending.remove(pend)
                        prb_free = A['prb_free']
                        QT_free = QT_readers
                        pa_r = []
                        M = dict(pmT_free=pmT_free, prb_free=prb_free, otmp_free=otmp_free)

                        def mg_tile(t, ti):
                            tok0 = s * S + qg * 512 + t * 128
                            otok0 = qbase + qg * 512 + t * 128
                            st_ = stt[ti]
                            lsn = pre_ld[t]
                            lxr = DMA('xr%d' % ti, [xr_free[ti], ready2], lambda: nc.sync.dma_start(
                                out=xr[ti][:], in_=xs[tok0:tok0 + 128, :]))
                            a1 = ACT(pa_w, lambda: nc.scalar.activation(out=junk[:, 0:512], in_=pa[:, t * 512:(t + 1) * 512], func=AF.Square,
                                                                        accum_out=st_[:, 0:1]))
                            yield
                            r1a = ACT(a1, lambda: nc.scalar.activation(out=st_[:, 1:2], in_=st_[:, 0:1], func=AF.Sqrt, bias=EPS, scale=1.0 / 512))
                            yield
                            r1 = DVE(r1a, lambda: nc.vector.reciprocal(out=st_[:, 1:2], in_=st_[:, 1:2]))
                            c1 = ACT([r1, merged_free[t]], lambda: nc.scalar.activation(
                                out=merged[t][:, 0:512], in_=pa[:, t * 512:(t + 1) * 512], func=AF.Copy, scale=st_[:, 1:2]))
                            pa_r.append(c1)
                            yield
                            tp = None
                            for k in range(8):
                                tp = PE([c1, lsn, M['pmT_free'], M['prb_free']] if k == 0 else None,
                                        lambda k=k: nc.tensor.transpose(pmT[:, k * 128:(k + 1) * 128], merged[t][:, k * 128:(k + 1) * 128], ident_b[:]),
                                        sig=(k == 7))
                            merged_free[t] = tp
                            yield
                            ev = DVE([tp, mT_free[ti]], lambda: nc.vector.tensor_copy(out=mT[ti][:].rearrange("p k c -> p (k c)"), in_=pmT))
                            M['pmT_free'] = ev
                            M['prb_free'] = ev
                            yield
                            T = scT[t % 2]
                            mo1 = mmg(T[:, 0:512], [(mT[ti][:, k, :], wo[:, k, 0:512]) for k in range(8)], [ev, sc_free[t % 2]])
                            mo2 = mmg(T[:, 512:1024], [(mT[ti][:, k, :], wo[:, k, 512:1024]) for k in range(8)], None)
                            mT_free[ti] = mo2
                            yield
                            a2 = ACT(mo2, lambda: nc.scalar.activation(out=junk[:], in_=T[:, :], func=AF.Square, accum_out=st_[:, 2:3]))
                            yield
                            r2a = ACT(a2, lambda: nc.scalar.activation(out=st_[:, 3:4], in_=st_[:, 2:3], func=AF.Sqrt, bias=EPS, scale=1.0 / D))
                            yield
                            r2 = DVE(r2a, lambda: nc.vector.reciprocal(out=st_[:, 3:4], in_=st_[:, 3:4]))
                            d1 = DVE([r2, M['otmp_free']], lambda: nc.vector.scalar_tensor_tensor(
                                out=otmp[:], in0=T[:, :], scalar=st_[:, 3:4], in1=gvec1[:], op0=ALU.mult, op1=ALU.mult))
                            sc_free[t % 2] = d1
                            pp = POOL([d1, lxr, x1_free[ti]], lambda: nc.gpsimd.tensor_tensor(out=x1[ti][:], in0=otmp[:], in1=xr[ti][:], op=ALU.add))
                            M['otmp_free'] = pp
                            xr_free[ti] = pp
                            x1_free[ti] = DMA('x1s%d' % ti, pp, lambda: nc.sync.dma_start(
                                out=x1_d[otok0:otok0 + 128, :], in_=x1[ti][:]))
                        interleave((mg_tile(t, (tcount + t) % 2) for t in range(4)), 2)
                        tcount += 4
                        pmT_free = M['pmT_free']; prb_free = M['prb_free']; otmp_free = M['otmp_free']
                        pa_free = pa_r
                    dp.retire_since(mk2)
                    p2_bar = dp.last() + [x1_free]
                seq_bar = p2_bar
                qbase += NQG[s] * 512
            stageA_bar = seq_bar
        while bg:
            bg.pop(0)()
        wcast_tok = wcast

        NTT = NQT // 128
        gseq = []
        for s in range(NSEQ):
            gseq += [s] * (NQG[s] * 4)
        with ExitStack() as bes:
            def sbB(name, shape, dt): return bes.enter_context(nc.sbuf_tensor(U(name), shape, dt))
            geff2_ = [sbB("geff2_%d" % i, [128, D], F32) for i in range(2)]
            sh2r_ = [sbB("sh2r_%d" % i, [128, D], F32) for i in range(2)]
            gvec2_ = [sbB("gvec2_%d" % i, [128, D], F32) for i in range(2)]
            vt = [sbB("vtB%d" % i, [128, D], F32) for i in range(2)]
            M1a = sbB("M1a", [128, NTT, 32], F32)
            M2a = sbB("M2a", [128, NTT, 32], F32)
            W1a = sbB("W1a", [128, NTT], F32)
            W2a = sbB("W2a", [128, NTT], F32)
            R1a = sbB("R1a", [128, NTT], F32)
            R2a = sbB("R2a", [128, NTT], F32)
            slot0 = sbB("slot0", [128, NTT], I32)
            slot1 = sbB("slot1", [128, NTT], I32)
            idxw = sbB("idxw", [128, NSL], I32)
            carry = sbB("carry", [128, 32], F32)
            PS = [bes.enter_context(nc.psum_tensor(U("PS%d" % i), [128, 512], F32)) for i in range(8)]
            bb = stageA_bar
            readyB = [bb, wcast_tok, zero_tok]

            def load_vecs(s, deps):
                geff2 = geff2_[s % 2]; sh2r = sh2r_[s % 2]; gvec2 = gvec2_[s % 2]
                l1 = DMA('v0_0', deps, lambda s=s: nc.sync.dma_start(out=vt[0][:], in_=mod_d[s, 4096:5120].partition_broadcast(128)))
                l2 = DMA('v0_1', deps, lambda: nc.sync.dma_start(out=vt[1][:], in_=g_pre2.partition_broadcast(128)))
                l3 = DMA('v0_2', deps, lambda s=s: nc.sync.dma_start(out=sh2r[:], in_=mod_d[s, 3072:4096].partition_broadcast(128)))
                tg = DVE([l1, l2], lambda: nc.vector.scalar_tensor_tensor(out=geff2[:], in0=vt[0][:], scalar=1.0, in1=vt[1][:],
                                                                           op0=ALU.add, op1=ALU.mult))
                l4 = DMA('v0_3', [tg], lambda s=s: nc.sync.dma_start(out=vt[0][:], in_=mod_d[s, 5120:6144].partition_broadcast(128)))
                l5 = DMA('v0_4', [tg], lambda: nc.sync.dma_start(out=vt[1][:], in_=g_post2.partition_broadcast(128)))
                tg2 = DVE([l4, l5], lambda: nc.vector.tensor_tensor(out=gvec2[:], in0=vt[0][:], in1=vt[1][:], op=ALU.mult))
                return [l3, tg, tg2]

            mkb1 = dp.mark()
            with ExitStack() as b1:
                def sb1(name, shape, dt): return b1.enter_context(nc.sbuf_tensor(U(name), shape, dt))
                w_r = sb1("w_r", [128, 8, 36], F32)
                brr = sb1("brr", [128, 36], F32)
                utri = sb1("utri", [128, 128], BF16)
                onesb = sb1("onesb", [128, 128], BF16)
                x1t = [sb1("x1t%d" % i, [128, D], F32) for i in range(5)]
                junk = sb1("junkB", [128, D], BF16)
                hm = sb1("hmB", [128, D], F32)
                h2 = [sb1("h2_%d" % i, [128, D], F32) for i in range(5)]
                h2Tf = [sb1("h2Tf%d" % i, [128, 8, 128], F32) for i in range(5)]
                stt = [sb1("stB%d" % i, [128, 8], F32) for i in range(5)]
                lg = [sb1("lg%d" % i, [128, 36], F32) for i in range(5)]
                wk = [sb1("wk%d" % i, [128, 192], F32) for i in range(5)]
                ohb = [sb1("ohb%d" % i, [128, 32], BF16) for i in range(5)]
                PH = [PS[6], PS[7]]
                ld = [DMA('s0', bb, lambda: nc.sync.dma_start(out=w_r[:, :, 0:4], in_=w_rg.rearrange("(k p) c -> p k c", p=128))),
                      DMA('s0', bb, lambda: nc.sync.dma_start(out=w_r[:, :, 4:36], in_=w_re.rearrange("(k p) c -> p k c", p=128))),
                      DMA('s0', bb, lambda: nc.sync.dma_start(out=brr[:, 0:4], in_=b_rg.partition_broadcast(128))),
                      DMA('s0', bb, lambda: nc.sync.dma_start(out=brr[:, 4:36], in_=b_re.partition_broadcast(128))),
                      DMA('s1', bb, lambda: nc.gpsimd.dma_start(out=utri[:], in_=utri_in), q='pool'),
                      POOL(bb, lambda: nc.gpsimd.memset(onesb[:], 1.0)),
                      POOL(bb, lambda: nc.gpsimd.memset(carry[:], 0.0))]
                rdy1 = [readyB, ld]
                T = dict(cur_seq=-1, vec_ready=None, vec_readers=[], hm_free=None, PH_free=[None, None], plg_free=None,
                         pcum_free=None, carry_tok=ld[-1])
                x1t_free = [None] * 5; h2_free = [None] * 5
                h2Tf_free = [None] * 5
                h2d_w = []

                def p1_tile(i):
                    s = gseq[i]
                    if s != T['cur_seq']:
                        T['cur_seq'] = s
                        T['vec_ready'] = load_vecs(s, [rdy1, T['vec_readers']])
                        T['vec_readers'] = []
                    vec_ready = T['vec_ready']
                    geff2 = geff2_[s % 2]; sh2r = sh2r_[s % 2]
                    ti = i % 5
                    tok0 = i * 128
                    st_ = stt[ti]
                    lx = DMA('bx%d' % ti, [x1t_free[ti], rdy1], lambda ti=ti, tok0=tok0: nc.sync.dma_start(out=x1t[ti][:], in_=x1_d[tok0:tok0 + 128, :]))
                    a1 = ACT(lx, lambda ti=ti, st_=st_: nc.scalar.activation(out=junk[:], in_=x1t[ti][:], func=AF.Square, accum_out=st_[:, 0:1]))
                    yield
                    r1a = ACT(a1, lambda st_=st_: nc.scalar.activation(out=st_[:, 1:2], in_=st_[:, 0:1], func=AF.Sqrt, bias=EPS, scale=1.0 / D))
                    yield
                    r1 = DVE(r1a, lambda st_=st_: nc.vector.reciprocal(out=st_[:, 1:2], in_=st_[:, 1:2]))
                    d1 = DVE([r1, T['hm_free'], vec_ready], lambda ti=ti, st_=st_: nc.vector.scalar_tensor_tensor(
                        out=hm[:], in0=x1t[ti][:], scalar=st_[:, 1:2], in1=geff2[:], op0=ALU.mult, op1=ALU.mult))
                    x1t_free[ti] = d1
                    p1_ = POOL([d1, h2_free[ti], vec_ready], lambda ti=ti: nc.gpsimd.tensor_tensor(out=h2[ti][:], in0=hm[:], in1=sh2r[:], op=ALU.add))
                    T['hm_free'] = p1_
                    T['vec_readers'] = [p1_, d1]
                    wr = DMA('h2w%d' % ti, p1_, lambda ti=ti, tok0=tok0: nc.gpsimd.dma_start(out=h2_d[tok0:tok0 + 128, :], in_=h2[ti][:]), q='pool')
                    h2d_w.append(wr)
                    yield
                    tp = None
                    for k in range(8):
                        bank = PH[k // 4]
                        tp = PE([p1_, T['PH_free']] if k == 0 else None,
                                lambda k=k, ti=ti, bank=bank: nc.tensor.transpose(bank[:, (k % 4) * 128:(k % 4 + 1) * 128],
                                                                                  h2[ti][:, k * 128:(k + 1) * 128], ident_f[:]),
                                sig=(k == 7))
                    h2_free[ti] = [tp, wr]
                    yield
                    e1 = ACT([tp, h2Tf_free[ti]], lambda ti=ti: nc.scalar.copy(out=h2Tf[ti][:, 0:4, :], in_=PH[0][:, :].rearrange("p (k c) -> p k c", k=4)))
                    e2 = DVE([tp, h2Tf_free[ti]], lambda ti=ti: nc.vector.tensor_copy(out=h2Tf[ti][:, 4:8, :], in_=PH[1][:, :].rearrange("p (k c) -> p k c", k=4)))
                    T['PH_free'] = [e1, e2]
                    yield
                    plg = PS[4][:, 0:36]
                    m_l = mmg(plg, [(h2Tf[ti][:, k, :], w_r[:, k, :]) for k in range(8)], [e1, e2, T['plg_free'], rdy1])
                    h2Tf_free[ti] = m_l
                    yield
                    L = lg[ti]; W = wk[ti]
                    v1 = DVE([m_l], lambda L=L: nc.vector.tensor_tensor(out=L[:], in0=plg, in1=brr[:], op=ALU.add))
                    T['plg_free'] = v1
                    v2 = DVE(v1, lambda L=L, W=W: nc.vector.tensor_reduce(out=W[:, 0:1], in_=L[:, 0:4], axis=AX.X, op=ALU.max))
                    v3 = DVE(v2, lambda W=W: nc.vector.tensor_scalar(out=W[:, 1:2], in0=W[:, 0:1], scalar1=-1.0, scalar2=None, op0=ALU.mult))
                    v4 = DVE(v2, lambda L=L, W=W: nc.vector.tensor_scalar(out=W[:, 4:8], in0=L[:, 0:4], scalar1=W[:, 0:1], scalar2=None, op0=ALU.is_equal))
                    s1 = ACT([v3], lambda L=L, W=W: nc.scalar.activation(out=W[:, 8:12], in_=L[:, 0:4], func=AF.Exp, bias=W[:, 1:2], scale=1.0,
                                                                         accum_out=W[:, 2:3]))
                    yield
                    v5 = DVE(s1, lambda W=W: nc.vector.reciprocal(out=W[:, 3:4], in_=W[:, 2:3]))
                    v6 = DVE(v4, lambda L=L, W=W: nc.vector.tensor_tensor(
                        out=W[:, 16:48].rearrange("p (g e) -> p g e", g=4), in0=L[:, 4:36].rearrange("p (g e) -> p g e", g=4),
                        in1=W[:, 4:8].unsqueeze(2).to_broadcast([128, 4, 8]), op=ALU.mult))
                    v7 = DVE(v6, lambda W=W: nc.vector.tensor_reduce(out=W[:, 48:56], in_=W[:, 16:48].rearrange("p (g e) -> p e g", g=4),
                                                                    axis=AX.X, op=ALU.add))
                    v8 = DVE(v7, lambda W=W: nc.vector.tensor_reduce(out=W[:, 12:13], in_=W[:, 48:56], axis=AX.X, op=ALU.max))
                    v9 = DVE(v8, lambda W=W: nc.vector.tensor_scalar(out=W[:, 56:64], in0=W[:, 48:56], scalar1=W[:, 12:13], scalar2=None,
                                                                    op0=ALU.is_equal))
                    v10 = DVE(v9, lambda W=W: nc.vector.scalar_tensor_tensor(out=W[:, 64:72], in0=W[:, 56:64], scalar=-1e30, in1=W[:, 48:56],
                                                                            op0=ALU.mult, op1=ALU.add))
                    v11 = DVE(v10, lambda W=W: nc.vector.tensor_reduce(out=W[:, 13:14], in_=W[:, 64:72], axis=AX.X, op=ALU.max))
                    v12 = DVE(v11, lambda W=W: nc.vector.tensor_scalar(out=W[:, 72:80], in0=W[:, 64:72], scalar1=W[:, 13:14], scalar2=None,
                                                                      op0=ALU.is_equal))
                    v13 = DVE(v11, lambda W=W: nc.vector.tensor_scalar(out=W[:, 14:15], in0=W[:, 12:13], scalar1=-1.0, scalar2=None, op0=ALU.mult))
                    s2 = ACT([v13], lambda W=W: nc.scalar.activation(out=W[:, 15:16], in_=W[:, 13:14], func=AF.Exp, bias=W[:, 14:15], scale=1.0))
                    yield
                    v14 = DVE(s2, lambda W=W: nc.vector.tensor_scalar(out=W[:, 80:81], in0=W[:, 15:16], scalar1=1.0, scalar2=None, op0=ALU.add))
                    v15 = DVE(v14, lambda W=W: nc.vector.reciprocal(out=W[:, 81:82], in_=W[:, 80:81]))
                    v16 = DVE([v15, v5], lambda W=W, i=i: nc.vector.tensor_tensor(out=W1a[:, i:i + 1], in0=W[:, 81:82], in1=W[:, 3:4], op=ALU.mult))
                    v17 = DVE(v16, lambda W=W, i=i: nc.vector.tensor_tensor(out=W2a[:, i:i + 1], in0=W1a[:, i:i + 1], in1=W[:, 15:16], op=ALU.mult))
                    v18 = DVE([v9, v4], lambda W=W, i=i: nc.vector.tensor_tensor(
                        out=M1a[:, i, :].rearrange("p (g e) -> p g e", g=4), in0=W[:, 4:8].unsqueeze(2).to_broadcast([128, 4, 8]),
                        in1=W[:, 56:64].unsqueeze(1).to_broadcast([128, 4, 8]), op=ALU.mult))
                    v19 = DVE([v12], lambda W=W, i=i: nc.vector.tensor_tensor(
                        out=M2a[:, i, :].rearrange("p (g e) -> p g e", g=4), in0=W[:, 4:8].unsqueeze(2).to_broadcast([128, 4, 8]),
                        in1=W[:, 72:80].unsqueeze(1).to_broadcast([128, 4, 8]), op=ALU.mult))
                    OH = ohb[ti]
                    v20 = DVE([v18, v19, T['pcum_free']], lambda OH=OH, i=i: nc.vector.tensor_tensor(out=OH[:], in0=M1a[:, i, :], in1=M2a[:, i, :], op=ALU.add))
                    pcum = PS[5][:, 0:32]
                    ptot = PS[5][:, 32:64]
                    PE([v20, T['pcum_free'], rdy1], lambda OH=OH: nc.tensor.matmul(pcum, lhsT=utri[:], rhs=OH[:], start=True, stop=True), sig=False)
                    mc = PE(None, lambda OH=OH: nc.tensor.matmul(ptot, lhsT=onesb[:], rhs=OH[:], start=True, stop=True))
                    yield
                    v21 = DVE([mc, T['carry_tok']], lambda W=W: nc.vector.tensor_tensor(out=W[:, 96:128], in0=carry[:], in1=pcum, op=ALU.add))
                    v22 = DVE(v21, lambda: nc.vector.tensor_tensor(out=carry[:], in0=carry[:], in1=ptot, op=ALU.add))
                    T['carry_tok'] = v22
                    T['pcum_free'] = v22
                    v23 = DVE(v22, lambda W=W, i=i: nc.vector.tensor_tensor(out=W[:, 128:160], in0=W[:, 96:128], in1=M1a[:, i, :], op=ALU.mult))
                    v24 = DVE(v23, lambda W=W, i=i: nc.vector.tensor_reduce(out=R1a[:, i:i + 1], in_=W[:, 128:160], axis=AX.X, op=ALU.add))
                    v25 = DVE(v24, lambda W=W, i=i: nc.vector.tensor_tensor(out=W[:, 160:192], in0=W[:, 96:128], in1=M2a[:, i, :], op=ALU.mult))
                    v26 = DVE(v25, lambda W=W, i=i: nc.vector.tensor_reduce(out=R2a[:, i:i + 1], in_=W[:, 160:192], axis=AX.X, op=ALU.add))
                interleave((p1_tile(i) for i in range(NTT)), 5)
                dp.retire_since(mkb1)
                b1_bar = dp.last() + [h2d_w]

            with ExitStack() as b2:
                def sb2(name, shape, dt): return b2.enter_context(nc.sbuf_tensor(U(name), shape, dt))
                jv = sb2("jv", [128, NSL], F32)
                pidx = sb2("pidx", [128, 1], F32)
                tri32 = sb2("tri32", [32, 32], F32)
                cmp_ = sb2("cmp", [128, NSL * 32], F32)
                tmpM = sb2("tmpM", [128, NTT, 32], F32)
                nblk = sb2("nblk", [128, 32], F32)
                pc = sb2("pc", [128, 32], F32)
                pcT = sb2("pcT", [32, 128], F32)
                sst = sb2("sst", [128, 32], F32)
                send = sb2("send", [128, 32], F32)
                te = sb2("te", [128, NSL], F32)
                sf = sb2("sf", [128, NTT], F32)
                l = [DMA('i0', b1_bar, lambda: nc.sync.dma_start(out=jv[:], in_=jv_in)),
                     DMA('i0', b1_bar, lambda: nc.sync.dma_start(out=pidx[:], in_=pidx_in)),
                     DMA('i0', b1_bar, lambda: nc.sync.dma_start(out=tri32[:], in_=tri32_in))]
                c3 = cmp_[:].rearrange("p (e m) -> p e m", e=32)
                q1 = DVE([l, b1_bar], lambda: nc.vector.tensor_tensor(out=c3, in0=jv[:].unsqueeze(1).to_broadcast([128, 32, NSL]),
                                                                      in1=carry[:].unsqueeze(2).to_broadcast([128, 32, NSL]), op=ALU.is_lt))
                q2 = DVE(q1, lambda: nc.vector.tensor_reduce(out=nblk[:], in_=c3, axis=AX.X, op=ALU.add))
                q3 = DVE(q2, lambda: nc.vector.tensor_scalar(out=pc[:], in0=nblk[:], scalar1=128.0, scalar2=None, op0=ALU.mult))
                q4 = PE(q3, lambda: nc.tensor.transpose(PS[0][0:32, 0:128], pc[:, :], ident_f[:]))
                q5 = ACT(q4, lambda: nc.scalar.copy(out=pcT[:], in_=PS[0][0:32, 0:128]))
                q6 = PE([q5, l], lambda: nc.tensor.matmul(PS[1][:, 0:32], lhsT=pcT[:, :], rhs=tri32[:, :], start=True, stop=True))
                q7 = DVE(q6, lambda: nc.vector.tensor_copy(out=sst[:], in_=PS[1][:, 0:32]))
                q8 = DVE(q7, lambda: nc.vector.tensor_tensor(out=send[:], in0=sst[:], in1=pc[:], op=ALU.add))
                c4 = cmp_[:].rearrange("p (m e) -> p m e", e=32)
                q9 = DVE(q8, lambda: nc.vector.tensor_tensor(out=c4, in0=send[:].unsqueeze(1).to_broadcast([128, NSL, 32]),
                                                             in1=jv[:].unsqueeze(2).to_broadcast([128, NSL, 32]), op=ALU.is_le))
                q10 = DVE(q9, lambda: nc.vector.tensor_reduce(out=te[:], in_=c4, axis=AX.X, op=ALU.add))
                q11 = DVE(q10, lambda: nc.vector.tensor_scalar(out=te[:], in0=te[:], scalar1=31.0, scalar2=128.0, op0=ALU.min, op1=ALU.mult))
                q12 = DVE(q11, lambda: nc.vector.tensor_scalar(out=te[:], in0=te[:], scalar1=pidx[:, 0:1], scalar2=None, op0=ALU.add))
                q13 = DVE(q12, lambda: nc.vector.tensor_copy(out=idxw[:], in_=te[:]))
                q14 = DVE(q7, lambda: nc.vector.tensor_tensor(out=tmpM[:], in0=M1a[:], in1=sst[:].unsqueeze(1).to_broadcast([128, NTT, 32]), op=ALU.mult))
                q15 = DVE(q14, lambda: nc.vector.tensor_reduce(out=sf[:], in_=tmpM[:], axis=AX.X, op=ALU.add))
                q16 = DVE(q15, lambda: nc.vector.tensor_tensor(out=sf[:], in0=sf[:], in1=R1a[:], op=ALU.add))
                q17 = DVE(q16, lambda: nc.vector.tensor_copy(out=slot0[:], in_=sf[:]))
                q18 = DVE(q17, lambda: nc.vector.tensor_tensor(out=tmpM[:], in0=M2a[:], in1=sst[:].unsqueeze(1).to_broadcast([128, NTT, 32]), op=ALU.mult))
                q19 = DVE(q18, lambda: nc.vector.tensor_reduce(out=sf[:], in_=tmpM[:], axis=AX.X, op=ALU.add))
                q20 = DVE(q19, lambda: nc.vector.tensor_tensor(out=sf[:], in0=sf[:], in1=R2a[:], op=ALU.add))
                q21 = DVE(q20, lambda: nc.vector.tensor_copy(out=slot1[:], in_=sf[:]))
                b2_bar = dp.last()

            mkb3 = dp.mark()
            with ExitStack() as b3:
                def sb3(name, shape, dt): return b3.enter_context(nc.sbuf_tensor(U(name), shape, dt))
                hsc = [sb3("hsc%d" % i, [128, D], BF16) for i in range(3)]
                hsc_free = [None] * 3
                sc_toks = []
                for i in range(NTT):
                    si = i % 3
                    tok0 = i * 128
                    lh = DMA('hl%d' % si, [hsc_free[si], b2_bar], lambda si=si, tok0=tok0: nc.sync.dma_start(out=hsc[si][:], in_=h2_d[tok0:tok0 + 128, :]))
                    s0 = DMA('sc%d' % si, [lh, b2_bar], lambda si=si, i=i: nc.gpsimd.indirect_dma_start(
                        out=xs_d[:, :], out_offset=bass.IndirectOffsetOnAxis(ap=slot0[:, i:i + 1], axis=0), in_=hsc[si][:, :], in_offset=None), q='pool')
                    s1_ = DMA('sc%d' % si, [lh], lambda si=si, i=i: nc.gpsimd.indirect_dma_start(
                        out=xs_d[:, :], out_offset=bass.IndirectOffsetOnAxis(ap=slot1[:, i:i + 1], axis=0), in_=hsc[si][:, :], in_offset=None), q='pool')
                    hsc_free[si] = [s0, s1_]
                    sc_toks += [s0, s1_]
                scat_done = [sc_toks[-6:], b2_bar]

                PF = 3
                NW = PF + 3
                ND = PF + 5
                NX = PF + 2
                wgu = [sb3("wgu%d" % i, [128, 8, 512], BF16) for i in range(NW)]
                wdb = [sb3("wdb%d" % i, [128, 2, D], BF16) for i in range(ND)]
                xsb = [sb3("xsb%d" % i, [128, D], BF16) for i in range(NX)]
                xT = [sb3("xT%d" % i, [128, 8, 128], BF16) for i in range(2)]
                sgs = [sb3("sgs%d" % i, [128, 256], F32) for i in range(2)]
                hid = [sb3("hid%d" % i, [128, 256], BF16) for i in range(2)]
                hT = [sb3("hT%d" % i, [128, 2, 128], BF16) for i in range(2)]
                ysb = [sb3("ysb%d" % i, [128, D], F32) for i in range(2)]
                pX = [PS[0][:, :].bitcast(BF16), PS[1][:, :].bitcast(BF16)]
                pH = [PS[2], PS[3]]
                pHT = [PS[4][:, 0:128].bitcast(BF16), PS[5][:, 0:128].bitcast(BF16)]
                pY = [PS[6], PS[7]]
                wgu_free = [None] * NW; wdb_free = [None] * ND; xsb_free = [None] * NX
                pX_free = [None, None]; xT_free = [None, None]; pH_free = [None, None]; sgs_free = [None, None]
                hid_free = [None, None]; pHT_free = [None, None]; hT_free = [None, None]
                pY_free = [None, None]; ysb_free = [None, None]
                st0 = {}; st1 = {}; st2 = {}; ldt = {}
                ys_w = []

                def issue_loads(a):
                    wi = a % NW; di = a % ND; xj = a % NX
                    lw = DMA('wgl%d' % wi, [wgu_free[wi], scat_done], lambda wi=wi, a=a: nc.gpsimd.indirect_dma_start(
                        out=wgu[wi][:].rearrange("p k c -> p (k c)"), out_offset=None, in_=wgu_r[:, :],
                        in_offset=bass.IndirectOffsetOnAxis(ap=idxw[:, a:a + 1], axis=0)), q='pool')
                    lwd = DMA('wdl%d' % di, [wdb_free[di], scat_done], lambda di=di, a=a: nc.gpsimd.indirect_dma_start(
                        out=wdb[di][:].rearrange("p k c -> p (k c)"), out_offset=None, in_=wd_r[:, :],
                        in_offset=bass.IndirectOffsetOnAxis(ap=idxw[:, a:a + 1], axis=0)), q='pool')
                    lxs = DMA('xsl%d' % xj, [xsb_free[xj], scat_done, sc_toks], lambda xj=xj, a=a: nc.sync.dma_start(
                        out=xsb[xj][:], in_=xs_d[a * 128:(a + 1) * 128, :]))
                    ldt[a] = (lw, lwd, lxs)

                for a in range(min(PF, NSL)):
                    issue_loads(a)
                for it in range(NSL + 3):
                    if it + PF < NSL:
                        issue_loads(it + PF)
                    a = it
                    if a < NSL:
                        xi = a % 2; xj = a % NX
                        lw, lwd, lxs = ldt[a]
                        tp = None
                        for k in range(8):
                            tp = PE([lxs, pX_free[xi]] if k == 0 else None,
                                    lambda k=k, xi=xi, xj=xj: nc.tensor.transpose(pX[xi][:, k * 128:(k + 1) * 128], xsb[xj][:, k * 128:(k + 1) * 128], ident_b[:]),
                                    sig=(k == 7))
                        xsb_free[xj] = tp
                        if a % 2 == 0:
                            ev = ACT([tp, xT_free[xi]], lambda xi=xi: nc.scalar.copy(out=xT[xi][:].rearrange("p k c -> p (k c)"), in_=pX[xi]))
                        else:
                            ev = DVE([tp, xT_free[xi]], lambda xi=xi: nc.vector.tensor_copy(out=xT[xi][:].rearrange("p k c -> p (k c)"), in_=pX[xi]))
                        pX_free[xi] = ev
                        st0[a] = (ev, lw, lwd)
                    a = it - 1
                    if 0 <= a < NSL:
                        wi = a % NW; xi = a % 2
                        ev, lw, lwd = st0[a]
                        mh = mmg(pH[xi][:, :], [(xT[xi][:, k, :], wgu[wi][:, k, :]) for k in range(8)], [ev, lw, pH_free[xi]])
                        wgu_free[wi] = mh
                        xT_free[xi] = mh
                        a_s = ACT([mh, sgs_free[xi]], lambda xi=xi: nc.scalar.activation(out=sgs[xi][:], in_=pH[xi][:, 0:256], func=AF.Silu))
                        d_h = DVE([a_s, hid_free[xi]], lambda xi=xi: nc.vector.tensor_tensor(out=hid[xi][:], in0=sgs[xi][:], in1=pH[xi][:, 256:512], op=ALU.mult))
                        pH_free[xi] = d_h
                        sgs_free[xi] = d_h
                        st1[a] = (d_h, lwd)
                    a = it - 2
                    if 0 <= a < NSL:
                        xi = a % 2
                        d_h, lwd = st1[a]
                        tp2 = None
                        for j in range(2):
                            tp2 = PE([d_h, pHT_free[xi]] if j == 0 else None,
                                     lambda j=j, xi=xi: nc.tensor.transpose(pHT[xi][:, j * 128:(j + 1) * 128], hid[xi][:, j * 128:(j + 1) * 128], ident_b[:]),
                                     sig=(j == 1))
                        hid_free[xi] = tp2
                        ev2 = ACT([tp2, hT_free[xi]], lambda xi=xi: nc.scalar.copy(out=hT[xi][:].rearrange("p k c -> p (k c)"), in_=pHT[xi]))
                        pHT_free[xi] = ev2
                        st2[a] = (ev2, lwd)
                    a = it - 3
                    if 0 <= a < NSL:
                        xi = a % 2; di = a % ND
                        ev2, lwd = st2[a]
                        my0 = mmg(pY[0][:, :], [(hT[xi][:, j, :], wdb[di][:, j, 0:512]) for j in range(2)], [ev2, lwd, pY_free[0]])
                        my1 = mmg(pY[1][:, :], [(hT[xi][:, j, :], wdb[di][:, j, 512:1024]) for j in range(2)], [pY_free[1]])
                        wdb_free[di] = my1
                        hT_free[xi] = my1
                        c0 = ACT([my0, ysb_free[xi]], lambda xi=xi: nc.scalar.copy(out=ysb[xi][:, 0:512], in_=pY[0][:, :]))
                        c1 = DVE([my1, ysb_free[xi]], lambda xi=xi: nc.vector.tensor_copy(out=ysb[xi][:, 512:1024], in_=pY[1][:, :]))
                        pY_free = [c0, c1]
                        ysb_free[xi] = DMA('ysw%d' % xi, [c0, c1], lambda xi=xi, a=a: nc.sync.dma_start(out=ys_d[a * 128:(a + 1) * 128, :], in_=ysb[xi][:]))
                        ys_w.append(ysb_free[xi])
                dp.retire_since(mkb3)
                b3_bar = dp.last() + [ys_w[-2:]]

            with ExitStack() as b4:
                def sb4(name, shape, dt): return b4.enter_context(nc.sbuf_tensor(U(name), shape, dt))
                ya = [sb4("ya%d" % i, [128, D], F32) for i in range(5)]
                yb = [sb4("yb%d" % i, [128, D], F32) for i in range(5)]
                x1c = [sb4("x1c%d" % i, [128, D], F32) for i in range(5)]
                mm_ = [sb4("mm_%d" % i, [128, D], F32) for i in range(5)]
                ytmp = [sb4("ytmp%d" % i, [128, D], F32) for i in range(5)]
                yo = [sb4("yo%d" % i, [128, D], F32) for i in range(5)]
                junk = sb4("junkC", [128, D], BF16)
                stt = [sb4("stC%d" % i, [128, 8], F32) for i in range(5)]
                ya_free = [None] * 5; yb_free = [None] * 5; x1c_free = [None] * 5; mm_free = [None] * 5
                ytmp_free = [None] * 5; yo_free = [None] * 5
                T = dict(cur_seq=-1, vec_ready=None, vec_readers=[])
                out_toks = []

                def cmb_tile(i):
                    s = gseq[i]
                    if s != T['cur_seq']:
                        T['cur_seq'] = s
                        T['vec_ready'] = load_vecs(s, [b3_bar, T['vec_readers']])
                        T['vec_readers'] = []
                    vec_ready = T['vec_ready']
                    gvec2 = gvec2_[s % 2]
                    ti = i % 5
                    tok0 = i * 128
                    st_ = stt[ti]
                    ga = DMA('ga%d' % ti, [ya_free[ti], b3_bar, ys_w], lambda ti=ti, i=i: nc.gpsimd.indirect_dma_start(
                        out=ya[ti][:, :], out_offset=None, in_=ys_d[:, :], in_offset=bass.IndirectOffsetOnAxis(ap=slot0[:, i:i + 1], axis=0)), q='pool')
                    gb_ = DMA('gb%d' % ti, [yb_free[ti], b3_bar], lambda ti=ti, i=i: nc.gpsimd.indirect_dma_start(
                        out=yb[ti][:, :], out_offset=None, in_=ys_d[:, :], in_offset=bass.IndirectOffsetOnAxis(ap=slot1[:, i:i + 1], axis=0)), q='pool')
                    lx = DMA('cx%d' % ti, [x1c_free[ti], b3_bar], lambda ti=ti, tok0=tok0: nc.sync.dma_start(out=x1c[ti][:], in_=x1_d[tok0:tok0 + 128, :]))
                    yield
                    d1 = DVE([ga, mm_free[ti]], lambda ti=ti, i=i: nc.vector.tensor_scalar(out=mm_[ti][:], in0=ya[ti][:], scalar1=W1a[:, i:i + 1], scalar2=None, op0=ALU.mult))
                    ya_free[ti] = d1
                    d2 = DVE([gb_, d1], lambda ti=ti, i=i: nc.vector.scalar_tensor_tensor(out=mm_[ti][:], in0=yb[ti][:], scalar=W2a[:, i:i + 1], in1=mm_[ti][:],
                                                                                         op0=ALU.mult, op1=ALU.add))
                    yb_free[ti] = d2
                    a1 = ACT(d2, lambda ti=ti, st_=st_: nc.scalar.activation(out=junk[:], in_=mm_[ti][:], func=AF.Square, accum_out=st_[:, 0:1]))
                    yield
                    r1a = ACT(a1, lambda st_=st_: nc.scalar.activation(out=st_[:, 1:2], in_=st_[:, 0:1], func=AF.Sqrt, bias=EPS, scale=1.0 / D))
                    yield
                    r1 = DVE(r1a, lambda st_=st_: nc.vector.reciprocal(out=st_[:, 1:2], in_=st_[:, 1:2]))
                    d3 = DVE([r1, ytmp_free[ti], vec_ready], lambda ti=ti, st_=st_: nc.vector.scalar_tensor_tensor(
                        out=ytmp[ti][:], in0=mm_[ti][:], scalar=st_[:, 1:2], in1=gvec2[:], op0=ALU.mult, op1=ALU.mult))
                    mm_free[ti] = d3
                    T['vec_readers'] = [d3]
                    pp = POOL([d3, lx, yo_free[ti]], lambda ti=ti: nc.gpsimd.tensor_tensor(out=yo[ti][:], in0=ytmp[ti][:], in1=x1c[ti][:], op=ALU.add))
                    ytmp_free[ti] = pp
                    x1c_free[ti] = pp
                    yo_free[ti] = DMA('yo%d' % ti, pp, lambda ti=ti, tok0=tok0: nc.sync.dma_start(out=y[tok0:tok0 + 128, :], in_=yo[ti][:]))
                    out_toks.append(yo_free[ti])
                interleave((cmb_tile(i) for i in range(NTT)), 5)
            dp.wait('sp', [yo_free, out_toks[-5:]])
            for e in ('pe', 'act', 'dve', 'pool'):
                dp.wait('sp', [(e, dp.cnt[e])])
    return nc


def _rope_tables(pos):
    half = 16
    inv = (10000.0 ** (-np.arange(half, dtype=np.float32) / half)).astype(np.float32)
    ang = pos.astype(np.float32)[:, None] * inv[None, :]
    cos = np.cos(ang).astype(np.float32)
    sin = np.sin(ang).astype(np.float32)
    c = np.concatenate([cos, cos], axis=1).T
    s_ = np.concatenate([sin, sin], axis=1).T
    return np.ascontiguousarray(c), np.ascontiguousarray(s_)


def _consts(NSL):
    ident = np.eye(128, dtype=np.float32)
    egrp = np.zeros((8, 512), np.float32)
    for g in range(8):
        egrp[g, g * 64:(g + 1) * 64] = 1.0
    utri = np.triu(np.ones((128, 128), np.float32), k=1)
    tri32 = np.triu(np.ones((32, 32), np.float32), k=1)
    jv = np.tile((np.arange(NSL, dtype=np.float32) * 128.0)[None, :], (128, 1))
    pidx = np.arange(128, dtype=np.float32).reshape(128, 1)
    return dict(ident=ident, egrp=egrp, utri=utri, tri32=tri32, jv=np.ascontiguousarray(jv), pidx=pidx,
                zeros=np.zeros((128, 8192), np.float32))


def _nt(cfg):
    nqt = sum(cfg['NQG']) * 512
    nt = (2 * nqt + 32 * 127 + 127) // 128
    return ((nt + 7) // 8) * 8


WEIGHT_KEYS = ['w_ada', 'b_ada', 'g_pre1', 'g_post1', 'g_pre2', 'g_post2', 'w_in', 'g_q', 'w_uq', 'g_kv', 'w_ukv',
               'g_v_gmlp', 'w_spatial', 'b_spatial', 'g_attn_out', 'g_gmlp_out', 'w_out', 'w_router_group',
               'b_router_group', 'w_router_expert', 'b_router_expert', 'w_gate', 'w_up', 'w_down']

_NC_CACHE = {}


def kernel(**inputs):
    S = 4096
    x_all = np.concatenate([np.asarray(inputs['x_prompt'], np.float32), np.asarray(inputs['x_sample'], np.float32)], axis=0)
    c_all = np.concatenate([np.asarray(inputs['c_prompt'], np.float32), np.asarray(inputs['c_sample'], np.float32)], axis=0)
    weights = {k: np.ascontiguousarray(np.asarray(inputs[k], np.float32)) for k in WEIGHT_KEYS}
    consts = _consts(_nt(FULL_CFG))
    pos_nat = np.arange(S)
    in_maps = []
    plans = []
    for c in range(8):
        if c % 2 == 0:
            s0 = (5 * c) // 2
            A, B, Cq, qhalf = s0, s0 + 1, s0 + 2, 0
        else:
            s0 = (5 * c - 1) // 2
            Cq, qhalf, A, B = s0, 1, s0 + 1, s0 + 2
        if qhalf == 0:
            posC = pos_nat
        else:
            posC = np.concatenate([pos_nat[S // 2:], pos_nat[:S // 2]])
        xs = np.concatenate([x_all[A], x_all[B], x_all[Cq][posC]], axis=0)
        cv = np.stack([c_all[A], c_all[B], c_all[Cq]], axis=0)
        rc = np.zeros((3, 32, S), np.float32)
        rs = np.zeros((3, 32, S), np.float32)
        for i, p in enumerate([pos_nat, pos_nat, posC]):
            rc[i], rs[i] = _rope_tables(p)
        m = dict(weights)
        m.update(xs=np.ascontiguousarray(xs), cvec=np.ascontiguousarray(cv), rope_c=rc, rope_s=rs)
        m.update(consts)
        in_maps.append(m)
        plans.append((A, B, Cq, qhalf))
    if 'full' not in _NC_CACHE:
        _NC_CACHE['full'] = build(FULL_CFG)
    nc = _NC_CACHE['full']
    res = run_bass_kernel_spmd(nc, in_maps, core_ids=list(range(8)))
    y_all = np.zeros((20, S, D), np.float32)
    for c in range(8):
        yc = res.results[c]['y']
        A, B, Cq, qhalf = plans[c]
        y_all[A] = yc[0:S]
        y_all[B] = yc[S:2 * S]
        if qhalf == 0:
            y_all[Cq, 0:S // 2] = yc[2 * S:2 * S + S // 2]
        else:
            y_all[Cq, S // 2:] = yc[2 * S:2 * S + S // 2]
    return (np.ascontiguousarray(y_all[0:4]), np.ascontiguousarray(y_all[4:20]))
```

```python
import numpy as np
import concourse.bass as bass
import concourse.mybir as mybir
from concourse.bass_utils import run_bass_kernel_spmd
from contextlib import ExitStack

F32, BF16 = mybir.dt.float32, mybir.dt.bfloat16
I32 = mybir.dt.int32
AF = mybir.ActivationFunctionType
ALU = mybir.AluOpType
AX = mybir.AxisListType
D = 1024
EPS = 1e-6
NE = 32
QSCALE = 96.0 ** -0.5

FULL_CFG = dict(S=4096, NSEQ=3, NQG=[8, 8, 4])


class Dep:
    def __init__(self, nc, es):
        self.nc = nc
        self.es = es
        self.eng = {'pe': nc.tensor, 'act': nc.scalar, 'dve': nc.vector, 'pool': nc.gpsimd, 'sp': nc.sync}
        self.sem = {e: es.enter_context(nc.semaphore('s_' + e)) for e in self.eng}
        self.cnt = {e: 0 for e in self.eng}
        self.waited = {e: {} for e in self.eng}
        self.dsem = {}
        self.entries = {}
        self.free = []
        self.sw_names = set()

    def semof(self, k):
        return self.sem[k] if k in self.sem else self.entries[k][0]

    def wait(self, e, deps):
        mx = {}
        for k, v in _flat(deps):
            if v > mx.get(k, 0):
                mx[k] = v
        for k, v in mx.items():
            if self.waited[e].get(k, 0) < v:
                self.eng[e].wait_ge(self.semof(k), v)
                self.waited[e][k] = v

    def op(self, e, deps, fn, sig=True):
        self.wait(e, deps)
        ins = fn()
        if sig:
            ins.then_inc(self.sem[e], 1)
            self.cnt[e] += 1
            return (e, self.cnt[e])
        return None

    def dma(self, q, name, deps, fn):
        if name not in self.dsem:
            if q != 'pool' and self.free:
                key = self.free.pop()
            else:
                key = 'D%d' % len(self.entries)
                self.entries[key] = [self.es.enter_context(self.nc.semaphore('d_' + key)), 0]
            self.dsem[name] = key
            if q == 'pool':
                self.sw_names.add(name)
        assert (name in self.sw_names) == (q == 'pool'), name
        key = self.dsem[name]
        self.wait(q, deps)
        ins = fn()
        ent = self.entries[key]
        ins.then_inc(ent[0], 16)
        ent[1] += 16
        return (key, ent[1])

    def reserve(self, name):
        key = 'D%d' % len(self.entries)
        self.entries[key] = [self.es.enter_context(self.nc.semaphore('d_' + key)), 0]
        self.dsem[name] = key
        self.sw_names.add(name)

    def mark(self):
        return set(self.dsem.keys())

    def retire_since(self, mark, keep=()):
        for n in list(self.dsem.keys()):
            if n in mark or n in keep:
                continue
            key = self.dsem[n]
            self.wait('sp', (key, self.entries[key][1]))
            if n in self.sw_names:
                continue
            del self.dsem[n]
            self.free.append(key)
        return self.op('sp', None, lambda: self.nc.sync.nop())

    def last(self):
        return [(e, self.cnt[e]) for e in self.eng if self.cnt[e] > 0]


def _flat(deps):
    out = []
    if deps is None:
        return out
    if isinstance(deps, tuple) and len(deps) == 2 and isinstance(deps[0], str):
        return [deps]
    for d in deps:
        out.extend(_flat(d))
    return out


def interleave(gens, depth):
    active = []
    it = iter(gens)
    done = False
    while True:
        if len(active) < depth and not done:
            try:
                active.append(next(it))
            except StopIteration:
                done = True
        if not active:
            break
        nxt = []
        for g in active:
            try:
                next(g)
                nxt.append(g)
            except StopIteration:
                pass
        active = nxt


def build(cfg):
    S = cfg['S']
    NSEQ = cfg['NSEQ']
    NQG = cfg['NQG']
    NG = S // 512
    KB = S // 128
    NT = NSEQ * S
    NQT = sum(NQG) * 512
    NGB = sum(NQG)
    NSL = (2 * NQT + 32 * 127 + 127) // 128
    NSL = ((NSL + 7) // 8) * 8

    nc = bass.Bass("TRN2", target_bir_lowering=False)

    def din(name, shape, dt=F32):
        return nc.dram_tensor(name, list(shape), dt, kind="ExternalInput").ap()

    def dscr(name, shape, dt):
        return nc.dram_tensor(name, list(shape), dt, kind="Internal").ap()

    xs = din("xs", [NT, D])
    cvec = din("cvec", [NSEQ, D])
    rope_c = din("rope_c", [NSEQ, 32, S])
    rope_s = din("rope_s", [NSEQ, 32, S])
    w_ada = din("w_ada", [D, 6 * D])
    b_ada = din("b_ada", [6 * D])
    g_pre1 = din("g_pre1", [D]); g_post1 = din("g_post1", [D])
    g_pre2 = din("g_pre2", [D]); g_post2 = din("g_post2", [D])
    w_in = din("w_in", [D, 1440])
    g_q = din("g_q", [256]); w_uq = din("w_uq", [256, 768])
    g_kv = din("g_kv", [128]); w_ukv = din("w_ukv", [128, 1024])
    g_v_gmlp = din("g_v_gmlp", [512])
    w_spatial = din("w_spatial", [8, 128, 128]); b_spatial = din("b_spatial", [8, 128])
    g_attn_out = din("g_attn_out", [512]); g_gmlp_out = din("g_gmlp_out", [512])
    w_out = din("w_out", [D, D])
    w_rg = din("w_router_group", [D, 4]); b_rg = din("b_router_group", [4])
    w_re = din("w_router_expert", [D, 32]); b_re = din("b_router_expert", [32])
    w_gate = din("w_gate", [NE, D, 256]); w_up = din("w_up", [NE, D, 256]); w_down = din("w_down", [NE, 256, D])
    ident_in = din("ident", [128, 128])
    egrp_in = din("egrp", [8, 512])
    utri_in = din("utri", [128, 128])
    tri32_in = din("tri32", [32, 32])
    jv_in = din("jv", [128, NSL])
    pidx_in = din("pidx", [128, 1])
    zeros_in = din("zeros", [128, 8192])
    y = nc.dram_tensor("y", [NQT, D], F32, kind="ExternalOutput").ap()

    mod_d = dscr("mod_d", [NSEQ, 6 * D], F32)
    sn_d = dscr("sn_d", [NT, 512], BF16)
    cq_d = dscr("cq_d", [NSEQ * NG, 128, 1024], BF16)
    x1_d = (nc.dram_tensor("x1_d", [NQT, D], F32, kind="ExternalOutput").ap() if cfg.get("dbg") else dscr("x1_d", [NQT, D], F32))
    wgu_r = dscr("wgu_r", [NE * 128, 8 * 512], BF16)
    wd_r = dscr("wd_r", [NE * 128, 2 * D], BF16)
    h2_d = dscr("h2_d", [NQT, D], BF16)
    xs_d = dscr("xs_d", [NSL * 128, D], BF16)
    ys_d = dscr("ys_d", [NSL * 128, D], F32)
    wkv_d = dscr("wkv_d", [128, 1024], BF16)
    wsp_d = dscr("wsp_d", [128, 1024], BF16)
    wq_d = dscr("wq_d", [128, 1536], BF16)
    wqsw_d = dscr("wqsw_d", [128, 1536], BF16)
    wo_d = dscr("wo_d", [128, 8192], BF16)

    _uid = [0]

    def U(name):
        _uid[0] += 1
        return "%s_u%d" % (name, _uid[0])

    top = ExitStack()
    with top:
        dp = Dep(nc, top)

        def PE(deps, fn, sig=True): return dp.op('pe', deps, fn, sig)
        def ACT(deps, fn, sig=True): return dp.op('act', deps, fn, sig)
        def DVE(deps, fn, sig=True): return dp.op('dve', deps, fn, sig)
        def POOL(deps, fn, sig=True): return dp.op('pool', deps, fn, sig)
        def DMA(name, deps, fn, q='sp'): return dp.dma(q, name, deps, fn)

        _jt = [None]

        def SQ(deps, fn):
            tok = ACT([deps, _jt[0]], fn)
            _jt[0] = tok
            return tok

        def mmg(out, pairs, deps, sig=True):
            n = len(pairs)
            tok = None
            for i, (l, r) in enumerate(pairs):
                tok = PE(deps if i == 0 else None,
                         lambda l=l, r=r, i=i: nc.tensor.matmul(out, lhsT=l, rhs=r, start=(i == 0), stop=(i == n - 1)),
                         sig=(sig and i == n - 1))
            return tok

        def rstd_chain(ss_ap, out_ap, inv_n, deps):
            t = ACT(deps, lambda: nc.scalar.activation(out=out_ap, in_=ss_ap, func=AF.Sqrt, bias=EPS, scale=inv_n))
            return DVE(t, lambda: nc.vector.reciprocal(out=out_ap, in_=out_ap))

        bg = []
        wcast = []
        zero_tok = []
        dp.reserve('wcast')
        dp.reserve('zero')
        for e in range(NE):
            bg.append(lambda e=e: wcast.append(DMA('wcast', None, lambda: nc.gpsimd.dma_start(
                out=wgu_r[e * 128:(e + 1) * 128, :].rearrange("p (k c) -> p k c", k=8)[:, :, 0:256],
                in_=w_gate[e].rearrange("(k p) c -> p k c", p=128)), q='pool')))
            bg.append(lambda e=e: wcast.append(DMA('wcast', None, lambda: nc.gpsimd.dma_start(
                out=wgu_r[e * 128:(e + 1) * 128, :].rearrange("p (k c) -> p k c", k=8)[:, :, 256:512],
                in_=w_up[e].rearrange("(k p) c -> p k c", p=128)), q='pool')))
            bg.append(lambda e=e: wcast.append(DMA('wcast', None, lambda: nc.gpsimd.dma_start(
                out=wd_r[e * 128:(e + 1) * 128, :].rearrange("p (j c) -> p j c", j=2),
                in_=w_down[e].rearrange("(j p) c -> p j c", p=128)), q='pool')))
        nz = (NSL * 128 * D) // (128 * 8192)
        xs_flat = xs_d.rearrange("(n p r) c -> n p (r c)", p=128, r=8)
        for zi in range(nz):
            bg.append(lambda zi=zi: zero_tok.append(DMA('zero', None, lambda: nc.gpsimd.dma_start(out=xs_flat[zi], in_=zeros_in), q='pool')))

        ident_f = top.enter_context(nc.sbuf_tensor(U("ident_f"), [128, 128], F32))
        ident_b = top.enter_context(nc.sbuf_tensor(U("ident_b"), [128, 128], BF16))
        ones_f = top.enter_context(nc.sbuf_tensor(U("ones_f"), [128, 64], F32))
        t_id = DMA('c0', None, lambda: nc.sync.dma_start(out=ident_f[:], in_=ident_in))
        t_idb = DVE(t_id, lambda: nc.vector.tensor_copy(out=ident_b[:], in_=ident_f[:]))
        t_ones = DVE(None, lambda: nc.vector.memset(ones_f[:], 1.0))

        mk0 = dp.mark()
        with ExitStack() as pes:
            def sb(name, shape, dt): return pes.enter_context(nc.sbuf_tensor(U(name), shape, dt))
            def ps(name, shape, dt): return pes.enter_context(nc.psum_tensor(U(name), shape, dt))
            csT = sb("csT", [128, 8, NSEQ], F32)
            csS = sb("csS", [128, 8, NSEQ], F32)
            wblk = [sb("wblk%d" % i, [128, 8, 512], F32) for i in range(2)]
            brep = sb("brep", [NSEQ, 6 * D], F32)
            modsb = sb("modsb", [NSEQ, 6 * D], F32)
            pmod = [ps("pmod%d" % i, [128, 512], F32) for i in range(2)]
            t_c = [DMA('p0', None, lambda q=q: nc.sync.dma_start(out=csT[:, :, q], in_=cvec[q].rearrange("(k p) -> p k", p=128),
                                                                 allow_slow_non_contiguous=True)) for q in range(NSEQ)]
            t_b = DMA('p1', None, lambda: nc.sync.dma_start(out=brep[:], in_=b_ada.partition_broadcast(NSEQ)))
            t_cs = ACT(t_c, lambda: nc.scalar.activation(out=csS[:], in_=csT[:], func=AF.Silu))
            wfree = [None, None]
            pfree = [None, None]
            ev = None
            for blk in range(12):
                i = blk % 2
                t_w = DMA('pw%d' % i, wfree[i], lambda blk=blk, i=i: nc.sync.dma_start(
                    out=wblk[i][:], in_=w_ada[:, blk * 512:(blk + 1) * 512].rearrange("(k p) c -> p k c", p=128)))
                t_m = mmg(pmod[i][0:NSEQ, :], [(csS[:, k, :], wblk[i][:, k, :]) for k in range(8)], [t_w, t_cs, pfree[i]])
                wfree[i] = t_m
                ev = DVE([t_m, t_b], lambda blk=blk, i=i: nc.vector.tensor_tensor(
                    out=modsb[:, blk * 512:(blk + 1) * 512], in0=pmod[i][0:NSEQ, :],
                    in1=brep[:, blk * 512:(blk + 1) * 512], op=ALU.add))
                pfree[i] = ev
            t_mod = DMA('p2', ev, lambda: nc.sync.dma_start(out=mod_d, in_=modsb[:]))

            tmpq = sb("tmpq", [128, 2, 768], F32)
            gq = sb("gq", [128, 2], F32)
            wq_t = sb("wq_t", [128, 2, 768], BF16)
            wqsw_t = sb("wqsw_t", [128, 2, 768], BF16)
            t1 = DMA('p3', None, lambda: nc.sync.dma_start(out=tmpq[:], in_=w_uq.rearrange("(k p) c -> p k c", p=128)))
            t2 = DMA('p3', None, lambda: nc.sync.dma_start(out=gq[:], in_=g_q.rearrange("(k p) -> p k", p=128),
                                                          allow_slow_non_contiguous=True))
            tq = None
            for k in range(2):
                tq = DVE([t1, t2], lambda k=k: nc.vector.tensor_scalar(
                    out=wq_t[:, k, :], in0=tmpq[:, k, :], scalar1=gq[:, k:k + 1], scalar2=QSCALE,
                    op0=ALU.mult, op1=ALU.mult))
            tz = POOL(None, lambda: nc.gpsimd.memset(wqsw_t[:], 0.0))
            wq4 = wq_t[:].rearrange("p k (h c) -> p k h c", h=8)
            wqs4 = wqsw_t[:].rearrange("p k (h c) -> p k h c", h=8)
            ta = DVE([tq, tz], lambda: nc.vector.tensor_scalar(out=wqs4[:, :, :, 64:80], in0=wq4[:, :, :, 80:96],
                                                              scalar1=-1.0, scalar2=None, op0=ALU.mult))
            tb = DVE(None, lambda: nc.vector.tensor_copy(out=wqs4[:, :, :, 80:96], in_=wq4[:, :, :, 64:80]))
            t_wq = DMA('p4', tq, lambda: nc.sync.dma_start(out=wq_d, in_=wq_t[:].rearrange("p k c -> p (k c)")))
            t_wqsw = DMA('p4', [ta, tb], lambda: nc.sync.dma_start(out=wqsw_d, in_=wqsw_t[:].rearrange("p k c -> p (k c)")))

            tmpkv = sb("tmpkv", [128, 1024], F32)
            gkv = sb("gkv", [128, 1], F32)
            wkv_t = sb("wkv_t", [128, 1024], BF16)
            t1 = DMA('p5', None, lambda: nc.sync.dma_start(out=tmpkv[:], in_=w_ukv))
            t2 = DMA('p5', None, lambda: nc.sync.dma_start(out=gkv[:], in_=g_kv.rearrange("(p o) -> p o", o=1)))
            tk = DVE([t1, t2], lambda: nc.vector.tensor_scalar(out=wkv_t[:], in0=tmpkv[:], scalar1=gkv[:, 0:1],
                                                              scalar2=None, op0=ALU.mult))
            t_wkv = DMA('p6', tk, lambda: nc.sync.dma_start(out=wkv_d, in_=wkv_t[:]))

            tmpo = sb("tmpo", [128, 8, 1024], F32)
            gcat = sb("gcat", [128, 8], F32)
            wo_t = sb("wo_t", [128, 8, 1024], BF16)
            t1 = DMA('p7', None, lambda: nc.sync.dma_start(out=tmpo[:], in_=w_out.rearrange("(k p) c -> p k c", p=128)))
            t2 = DMA('p7', None, lambda: nc.sync.dma_start(out=gcat[:, 0:4], in_=g_attn_out.rearrange("(k p) -> p k", p=128),
                                                          allow_slow_non_contiguous=True))
            t3 = DMA('p7', None, lambda: nc.sync.dma_start(out=gcat[:, 4:8], in_=g_gmlp_out.rearrange("(k p) -> p k", p=128),
                                                          allow_slow_non_contiguous=True))
            two = None
            for k in range(8):
                two = DVE([t1, t2, t3], lambda k=k: nc.vector.tensor_scalar(
                    out=wo_t[:, k, :], in0=tmpo[:, k, :], scalar1=gcat[:, k:k + 1], scalar2=None, op0=ALU.mult))
            t_wo = DMA('p8', two, lambda: nc.sync.dma_start(out=wo_d, in_=wo_t[:].rearrange("p k c -> p (k c)")))

            tmps = sb("tmps", [128, 8, 128], F32)
            wsp_t = sb("wsp_t", [128, 8, 128], BF16)
            psp = ps("psp", [128, 1024], F32)
            t1 = DMA('p9', None, lambda: nc.sync.dma_start(out=tmps[:], in_=w_spatial.rearrange("g t s -> t g s")))
            tt = None
            for g in range(8):
                tt = PE([t1, t_id], lambda g=g: nc.tensor.transpose(psp[:, g * 128:(g + 1) * 128], tmps[:, g, :], ident_f[:]),
                        sig=(g == 7))
            tc_ = DVE(tt, lambda: nc.vector.tensor_copy(out=wsp_t[:].rearrange("p g t -> p (g t)"), in_=psp[:]))
            t_wsp = DMA('p10', tc_, lambda: nc.sync.dma_start(out=wsp_d, in_=wsp_t[:].rearrange("p g t -> p (g t)")))
            prep_done = [t_mod, t_wq, t_wqsw, t_wkv, t_wo, t_wsp]
            dp.retire_since(mk0, keep=('wcast', 'zero', 'c0'))
            prep_bar = dp.last()

        with ExitStack() as aes:
            def sbA(name, shape, dt): return aes.enter_context(nc.sbuf_tensor(U(name), shape, dt))
            KT = sbA("KT", [128, 8, S], BF16)
            VA = sbA("VA", [128, KB, 8, 65], BF16)
            geff1 = sbA("geff1", [128, D], F32)
            sh1r = sbA("sh1r", [128, D], F32)
            gvec1 = sbA("gvec1", [128, D], F32)
            gvrep = sbA("gvrep", [128, 512], F32)
            PA = aes.enter_context(nc.psum_tensor(U("PA"), [128, 1024], F32))
            PB = aes.enter_context(nc.psum_tensor(U("PB"), [128, 1024], F32))
            PC = aes.enter_context(nc.psum_tensor(U("PC"), [128, 1024], F32))
            PD = aes.enter_context(nc.psum_tensor(U("PD"), [128, 1024], F32))

            t_va1 = POOL(prep_bar, lambda: nc.gpsimd.memset(VA[:, :, :, 64:65], 1.0))
            t_gv = DMA('a0', prep_bar, lambda: nc.sync.dma_start(out=gvrep[:], in_=g_v_gmlp.partition_broadcast(128)))
            seq_bar = [prep_bar, prep_done, t_va1, t_gv, t_idb, t_ones]
            qbase = 0
            for s in range(NSEQ):
                mk1 = dp.mark()
                with ExitStack() as p1:
                    def sb1(name, shape, dt): return p1.enter_context(nc.sbuf_tensor(U(name), shape, dt))
                    wAs = sb1("wAs", [128, 8, 384], BF16)
                    wAuv = sb1("wAuv", [128, 8, 1024], BF16)
                    wAkr = sb1("wAkr", [128, 8, 96], BF16)
                    wAks = sb1("wAks", [128, 8, 96], BF16)
                    wkv = sb1("wkv", [128, 1024], BF16)
                    wsp = sb1("wsp", [128, 8, 128], BF16)
                    bsp = sb1("bsp", [8, 128], F32)
                    egrp = sb1("egrp", [8, 512], F32)
                    vt = [sb1("vt%d" % i, [128, D], F32) for i in range(2)]
                    xt = [sb1("xt%d" % i, [128, D], F32) for i in range(2)]
                    junk = sb1("junk", [128, D], BF16)
                    hm = sb1("hm", [128, D], F32)
                    hb = [sb1("hb%d" % i, [128, D], BF16) for i in range(2)]
                    hT = sb1("hT", [128, 8, 512], BF16)
                    zsb = [sb1("zsb%d" % i, [128, 384], BF16) for i in range(2)]
                    cqnT = [sb1("cqnT%d" % i, [128, 2, 512], BF16) for i in range(2)]
                    ckvnT = [sb1("ckvnT%d" % i, [128, 512], BF16) for i in range(2)]
                    gu = [sb1("gu%d" % i, [128, 512], BF16) for i in range(2)]
                    gv = [sb1("gv%d" % i, [128, 512], F32) for i in range(2)]
                    zraw = [sb1("zraw%d" % i, [128, 384], F32) for i in range(2)]
                    vn = [sb1("vn%d" % i, [128, 512], BF16) for i in range(2)]
                    sraw = [sb1("sraw%d" % i, [128, 512], F32) for i in range(2)]
                    sn = [sb1("sn%d" % i, [128, 512], BF16) for i in range(2)]
                    stt = [sb1("stt%d" % i, [128, 16], F32) for i in range(2)]
                    Ctt = [sb1("Ctt%d" % i, [128, 128], F32) for i in range(2)]
                    Stt = [sb1("Stt%d" % i, [128, 128], F32) for i in range(2)]
                    kt1 = [sb1("kt1_%d" % i, [128, 128], F32) for i in range(2)]
                    kt2 = [sb1("kt2_%d" % i, [128, 128], F32) for i in range(2)]
                    krr = [sb1("krr%d" % i, [128, 128], BF16) for i in range(2)]

                    pT = PA[:, 0:512].bitcast(BF16)
                    pT2 = PA[:, 512:1024].bitcast(BF16)
                    pzs = PB[:, 0:384]
                    pss = PB[:, 512:1024]
                    pu = PC[:, 0:512]
                    pv = PC[:, 512:1024]
                    pkr = PD[:, 0:512]
                    pks = PD[:, 512:1024]

                    sb_ = seq_bar
                    wl = []
                    wl.append(DMA('a1', sb_, lambda: nc.gpsimd.dma_start(
                        out=wAs[:], in_=w_in[:, 0:384].rearrange("(k p) c -> p k c", p=128)), q='pool'))
                    wl.append(DMA('a1', sb_, lambda: nc.gpsimd.dma_start(
                        out=wAuv[:], in_=w_in[:, 416:1440].rearrange("(k p) c -> p k c", p=128)), q='pool'))
                    tz1 = POOL(sb_, lambda: nc.gpsimd.memset(wAkr[:], 0.0))
                    tz2 = POOL(sb_, lambda: nc.gpsimd.memset(wAks[:], 0.0))
                    wl.append(DMA('a1', [tz1], lambda: nc.gpsimd.dma_start(
                        out=wAkr[:, :, 64:96], in_=w_in[:, 384:416].rearrange("(k p) c -> p k c", p=128)), q='pool'))
                    tn = DMA('a2', [tz2], lambda: nc.gpsimd.dma_start(
                        out=wAks[:, :, 64:80], in_=w_in[:, 400:416].rearrange("(k p) c -> p k c", p=128)), q='pool')
                    wl.append(DMA('a1', [tz2], lambda: nc.gpsimd.dma_start(
                        out=wAks[:, :, 80:96], in_=w_in[:, 384:400].rearrange("(k p) c -> p k c", p=128)), q='pool'))
                    wl.append(POOL(tn, lambda: nc.gpsimd.tensor_scalar(out=wAks[:, :, 64:80], in0=wAks[:, :, 64:80],
                                                                      scalar1=-1.0, scalar2=None, op0=ALU.mult)))
                    wl.append(DMA('a3', sb_, lambda: nc.sync.dma_start(out=wkv[:], in_=wkv_d)))
                    wl.append(DMA('a3', sb_, lambda: nc.sync.dma_start(out=wsp[:].rearrange("p g t -> p (g t)"), in_=wsp_d)))
                    wl.append(DMA('a3', sb_, lambda: nc.sync.dma_start(out=bsp[:], in_=b_spatial)))
                    wl.append(DMA('a3', sb_, lambda: nc.sync.dma_start(out=egrp[:], in_=egrp_in)))
                    l1 = DMA('a4_0', sb_, lambda: nc.sync.dma_start(out=vt[0][:], in_=mod_d[s, 1024:2048].partition_broadcast(128)))
                    l2 = DMA('a4_1', sb_, lambda: nc.sync.dma_start(out=vt[1][:], in_=g_pre1.partition_broadcast(128)))
                    l3 = DMA('a4_2', sb_, lambda: nc.sync.dma_start(out=sh1r[:], in_=mod_d[s, 0:1024].partition_broadcast(128)))
                    tg = DVE([l1, l2], lambda: nc.vector.scalar_tensor_tensor(out=geff1[:], in0=vt[0][:], scalar=1.0, in1=vt[1][:],
                                                                               op0=ALU.add, op1=ALU.mult))
                    l4 = DMA('a4_3', [tg], lambda: nc.sync.dma_start(out=vt[0][:], in_=mod_d[s, 2048:3072].partition_broadcast(128)))
                    l5 = DMA('a4_4', [tg], lambda: nc.sync.dma_start(out=vt[1][:], in_=g_post1.partition_broadcast(128)))
                    tg2 = DVE([l4, l5], lambda: nc.vector.tensor_tensor(out=gvec1[:], in0=vt[0][:], in1=vt[1][:], op=ALU.mult))
                    ready = [wl, l3, tg, tg2]

                    xt_free = [None, None]; hb_free = [None, None]
                    hT_free = [None] * 4
                    zraw_free = [None, None]; zsb_free = [None, None]; cq_free = [None, None]; ckv_free = [None, None]
                    gu_free = [None, None]; gv_free = [None, None]; vn_free = [None, None]; sn_free = [None, None]
                    sraw_free = [None, None]; ct_free = [None, None]; kt_free = [None, None]; krr_free = [None, None]
                    P = dict(hm_free=None, pT_free=None, pT2_free=None, pzs_free=None, pu_free=None, pv_free=None, pss_free=None,
                             pkr_free=None, pks_free=None)
                    grp = {}

                    def p1_tile(g, t):
                        gi = g % 2
                        ti = t % 2
                        if t == 0:
                            grp[g] = dict(cq_w=[], ckv_w=[])
                        G = grp[g]
                        tok0 = s * S + g * 512 + t * 128
                        ts_ = slice(t * 128, (t + 1) * 128)
                        gts = slice(g * 512 + t * 128, g * 512 + (t + 1) * 128)
                        st_ = stt[ti]
                        lx = DMA('x%d' % ti, [xt_free[ti], ready], lambda: nc.sync.dma_start(out=xt[ti][:], in_=xs[tok0:tok0 + 128, :]))
                        lc = DMA('rc%d' % ti, [ct_free[ti], ready], lambda: nc.sync.dma_start(out=Ctt[ti][64:96, :], in_=rope_c[s, :, gts]))
                        ls = DMA('rs%d' % ti, [ct_free[ti], ready], lambda: nc.sync.dma_start(out=Stt[ti][64:96, :], in_=rope_s[s, :, gts]))
                        a1 = SQ(lx, lambda: nc.scalar.activation(out=junk[:], in_=xt[ti][:], func=AF.Square, accum_out=st_[:, 0:1]))
                        yield
                        r1a = ACT(a1, lambda: nc.scalar.activation(out=st_[:, 1:2], in_=st_[:, 0:1], func=AF.Sqrt, bias=EPS, scale=1.0 / D))
                        yield
                        r1 = DVE(r1a, lambda: nc.vector.reciprocal(out=st_[:, 1:2], in_=st_[:, 1:2]))
                        d1 = DVE([r1, P['hm_free']], lambda: nc.vector.scalar_tensor_tensor(
                            out=hm[:], in0=xt[ti][:], scalar=st_[:, 1:2], in1=geff1[:], op0=ALU.mult, op1=ALU.mult))
                        xt_free[ti] = d1
                        p1_ = POOL([d1, hb_free[ti]], lambda: nc.gpsimd.tensor_tensor(out=hb[ti][:], in0=hm[:], in1=sh1r[:], op=ALU.add))
                        P['hm_free'] = p1_
                        yield
                        tp = None
                        for k in range(8):
                            tp = PE([p1_, P['pT_free']] if k == 0 else None,
                                    lambda k=k: nc.tensor.transpose(pT[:, k * 128:(k + 1) * 128], hb[ti][:, k * 128:(k + 1) * 128], ident_b[:]),
                                    sig=(k == 7))
                        hb_free[ti] = tp
                        yield
                        ev = ACT([tp, hT_free[t]], lambda: nc.scalar.copy(out=hT[:, :, ts_], in_=pT.rearrange("p (k c) -> p k c", k=8)))
                        P['pT_free'] = ev
                        yield
                        m_zs = mmg(pzs, [(hT[:, k, ts_], wAs[:, k, :]) for k in range(8)], [ev, P['pzs_free']])
                        m_u = mmg(pu, [(hT[:, k, ts_], wAuv[:, k, 0:512]) for k in range(8)], [P['pu_free']])
                        m_v = mmg(pv, [(hT[:, k, ts_], wAuv[:, k, 512:1024]) for k in range(8)], [P['pv_free']])
                        m_kr = mmg(pkr[0:96, 0:128], [(wAkr[:, k, :], hT[:, k, ts_]) for k in range(8)], [P['pkr_free']])
                        m_ks = mmg(pks[0:96, 0:128], [(wAks[:, k, :], hT[:, k, ts_]) for k in range(8)], [P['pks_free']])
                        hT_free[t] = m_ks
                        yield
                        zr = DVE([m_zs, zraw_free[ti]], lambda: nc.vector.tensor_copy(out=zraw[ti][:], in_=pzs))
                        P['pzs_free'] = zr
                        g1 = ACT([m_u, gu_free[ti]], lambda: nc.scalar.activation(out=gu[ti][:], in_=pu, func=AF.Gelu_apprx_tanh))
                        P['pu_free'] = g1
                        g2 = ACT([m_v, gv_free[ti]], lambda: nc.scalar.activation(out=gv[ti][:], in_=pv, func=AF.Gelu_apprx_tanh))
                        P['pv_free'] = g2
                        k1 = DVE([m_kr, lc, kt_free[ti]], lambda: nc.vector.tensor_tensor(out=kt1[ti][64:96, :], in0=pkr[64:96, 0:128], in1=Ctt[ti][64:96, :], op=ALU.mult))
                        P['pkr_free'] = k1
                        k2 = DVE([m_ks, ls], lambda: nc.vector.tensor_tensor(out=kt2[ti][64:96, :], in0=pks[64:96, 0:128], in1=Stt[ti][64:96, :], op=ALU.mult))
                        P['pks_free'] = k2
                        ct_free[ti] = k2
                        yield
                        a2 = SQ(zr, lambda: nc.scalar.activation(out=junk[:, 0:256], in_=zraw[ti][:, 0:256], func=AF.Square, accum_out=st_[:, 2:3]))
                        a3 = SQ(None, lambda: nc.scalar.activation(out=junk[:, 256:384], in_=zraw[ti][:, 256:384], func=AF.Square, accum_out=st_[:, 3:4]))
                        g3 = SQ(g2, lambda: nc.scalar.activation(out=junk[:, 0:512], in_=gv[ti][:], func=AF.Square, accum_out=st_[:, 6:7]))
                        k3 = DVE([k1, k2, krr_free[ti]], lambda: nc.vector.tensor_tensor(out=krr[ti][64:96, :], in0=kt1[ti][64:96, :], in1=kt2[ti][64:96, :], op=ALU.add))
                        kt_free[ti] = k3
                        kc = None
                        for h in range(8):
                            kc = POOL(k3, lambda h=h: nc.gpsimd.tensor_copy(out=KT[64:96, h, gts], in_=krr[ti][64:96, :]))
                        krr_free[ti] = kc
                        yield
                        q1 = ACT([a2, a3], lambda: nc.scalar.activation(out=st_[:, 4:5], in_=st_[:, 2:3], func=AF.Sqrt, bias=EPS, scale=1.0 / 256))
                        q2 = ACT(None, lambda: nc.scalar.activation(out=st_[:, 5:6], in_=st_[:, 3:4], func=AF.Sqrt, bias=EPS, scale=1.0 / 128))
                        q3 = ACT(g3, lambda: nc.scalar.activation(out=st_[:, 7:8], in_=st_[:, 6:7], func=AF.Sqrt, bias=EPS, scale=1.0 / 512))
                        yield
                        r2 = DVE([q1, q2], lambda: nc.vector.reciprocal(out=st_[:, 4:6], in_=st_[:, 4:6]))
                        r4 = DVE(q3, lambda: nc.vector.reciprocal(out=st_[:, 7:8], in_=st_[:, 7:8]))
                        d2 = DVE([r4, vn_free[ti]], lambda: nc.vector.scalar_tensor_tensor(
                            out=vn[ti][:], in0=gv[ti][:], scalar=st_[:, 7:8], in1=gvrep[:], op0=ALU.mult, op1=ALU.mult))
                        gv_free[ti] = d2
                        c1 = ACT([r2, zsb_free[ti]], lambda: nc.scalar.activation(
                            out=zsb[ti][:, 0:256], in_=zraw[ti][:, 0:256], func=AF.Copy, scale=st_[:, 4:5]))
                        c2 = ACT(None, lambda: nc.scalar.activation(
                            out=zsb[ti][:, 256:384], in_=zraw[ti][:, 256:384], func=AF.Copy, scale=st_[:, 5:6]))
                        zraw_free[ti] = c2
                        yield
                        tp2 = None
                        for k in range(3):
                            tp2 = PE([c1, c2, P['pT2_free']] if k == 0 else None,
                                     lambda k=k: nc.tensor.transpose(pT2[:, k * 128:(k + 1) * 128], zsb[ti][:, k * 128:(k + 1) * 128], ident_b[:]),
                                     sig=(k == 2))
                        zsb_free[ti] = tp2
                        PE([P['pss_free'], ready], lambda: nc.tensor.matmul(pss, lhsT=bsp[:, :], rhs=egrp[:, :], start=True, stop=False), sig=False)
                        m_s = None
                        for gg in range(8):
                            m_s = PE(d2 if gg == 0 else None,
                                     lambda gg=gg: nc.tensor.matmul(pss[:, gg * 64:(gg + 1) * 64], lhsT=wsp[:, gg, :],
                                                                    rhs=vn[ti][:, gg * 64:(gg + 1) * 64], start=False, stop=(gg == 7)),
                                     sig=(gg == 7))
                        vn_free[ti] = m_s
                        yield
                        e1 = DVE([tp2, cq_free[gi] if t == 0 else None], lambda: nc.vector.tensor_copy(
                            out=cqnT[gi][:, :, ts_], in_=pT2[:, 0:256].rearrange("p (k c) -> p k c", k=2)))
                        e2 = DVE([ckv_free[gi] if t == 0 else None], lambda: nc.vector.tensor_copy(
                            out=ckvnT[gi][:, ts_], in_=pT2[:, 256:384]))
                        P['pT2_free'] = e2
                        G['cq_w'].append(e1)
                        G['ckv_w'].append(e2)
                        d3 = DVE([m_s, g1, sraw_free[ti]], lambda: nc.vector.tensor_tensor(out=sraw[ti][:], in0=gu[ti][:], in1=pss, op=ALU.mult))
                        P['pss_free'] = d3
                        gu_free[ti] = d3
                        yield
                        a4 = SQ(d3, lambda: nc.scalar.activation(out=junk[:, 0:512], in_=sraw[ti][:], func=AF.Square, accum_out=st_[:, 8:9]))
                        yield
                        q4 = ACT(a4, lambda: nc.scalar.activation(out=st_[:, 9:10], in_=st_[:, 8:9], func=AF.Sqrt, bias=EPS, scale=1.0 / 512))
                        yield
                        r5 = DVE(q4, lambda: nc.vector.reciprocal(out=st_[:, 9:10], in_=st_[:, 9:10]))
                        yield
                        c3 = ACT([r5, sn_free[ti]], lambda: nc.scalar.activation(out=sn[ti][:], in_=sraw[ti][:], func=AF.Copy, scale=st_[:, 9:10]))
                        sraw_free[ti] = c3
                        sn_free[ti] = DMA('sn%d' % ti, c3, lambda: nc.sync.dma_start(out=sn_d[tok0:tok0 + 128, :], in_=sn[ti][:]))
                        if t != 3:
                            return
                        yield
                        gs = slice(g * 512, (g + 1) * 512)
                        cq_free[gi] = DMA('cq%d' % gi, G['cq_w'], lambda: nc.sync.dma_start(
                            out=cq_d[s * NG + g], in_=cqnT[gi][:].rearrange("p k c -> p (k c)")))
                        bank_free = [P['pkr_free'], P['pks_free']]
                        banks = [pkr, pks]
                        for h in range(8):
                            bi = h % 2
                            mk = mmg(banks[bi][0:64, :], [(wkv[:, h * 128:h * 128 + 64], ckvnT[gi][:, :])], [G['ckv_w'], bank_free[bi]])
                            if h % 2 == 0:
                                bank_free[bi] = ACT(mk, lambda h=h, bi=bi: nc.scalar.copy(out=KT[0:64, h, gs], in_=banks[bi][0:64, :]))
                            else:
                                bank_free[bi] = DVE(mk, lambda h=h, bi=bi: nc.vector.tensor_copy(out=KT[0:64, h, gs], in_=banks[bi][0:64, :]))
                        wkv3 = wkv[:].rearrange("p (h c) -> p h c", h=8)[:, :, 64:128]
                        mv = None
                        for tt in range(4):
                            bi = tt % 2
                            kb = g * 4 + tt
                            mv = mmg(banks[bi][:, :].rearrange("p (h c) -> p h c", h=8), [(ckvnT[gi][:, tt * 128:(tt + 1) * 128], wkv3)], [bank_free[bi]])
                            bank_free[bi] = DVE(mv, lambda kb=kb, bi=bi: nc.vector.tensor_copy(
                                out=VA[:, kb, :, 0:64], in_=banks[bi][:, :].rearrange("p (h c) -> p h c", h=8)))
                        ckv_free[gi] = mv
                        P['pkr_free'] = bank_free[0]
                        P['pks_free'] = bank_free[1]

                    interleave((p1_tile(g, t) for g in range(NG) for t in range(4)), 2)
                    dp.retire_since(mk1)
                    p1_bar = dp.last() + [sn_free, cq_free]

                mk2 = dp.mark()
                with ExitStack() as p2:
                    def sb2(name, shape, dt): return p2.enter_context(nc.sbuf_tensor(U(name), shape, dt))
                    wq = sb2("wq", [128, 2, 768], BF16)
                    wqs = sb2("wqs", [128, 2, 768], BF16)
                    wo = sb2("wo", [128, 8, 1024], BF16)
                    cqT = [sb2("cqT%d" % i, [128, 2, 512], BF16) for i in range(2)]
                    Ct = sb2("Ct2", [128, 512], F32)
                    St = sb2("St2", [128, 512], F32)
                    qt1 = [sb2("qt1_%d" % i, [128, 512], F32) for i in range(2)]
                    qt2 = [sb2("qt2_%d" % i, [128, 512], F32) for i in range(2)]
                    qt_free = [None, None]
                    QT = sb2("QT", [128, 8, 512], BF16)
                    pTs = [sb2("pTs%d" % i, [128, 1024], BF16) for i in range(3)]
                    osb = [sb2("osb%d" % i, [128, 512], F32) for i in range(2)]
                    rcp = [sb2("rcp%d" % i, [128, 4], F32) for i in range(2)]
                    a_tok = sb2("a_tok", [128, 4, 512], BF16)
                    merged = [sb2("merged%d" % i, [128, D], BF16) for i in range(4)]
                    mT = [sb2("mT%d" % i, [128, 8, 128], BF16) for i in range(2)]
                    otmp = sb2("otmp", [128, D], F32)
                    xr = [sb2("xr%d" % i, [128, D], F32) for i in range(2)]
                    x1 = [sb2("x1_%d" % i, [128, D], F32) for i in range(2)]
                    junk = sb2("junk2", [128, D], BF16)
                    stt = [sb2("stq%d" % i, [128, 16], F32) for i in range(2)]

                    po = [PC[:, 0:512], PD[:, 0:512]]
                    ptr3 = PC[:, 512:512 + 260].rearrange("p (t c) -> p t c", t=4)
                    pmT = PC[:, 512:1024].bitcast(BF16)
                    scT = [PA, PB]

                    wl2 = [DMA('b1', p1_bar, lambda: nc.sync.dma_start(out=wq[:].rearrange("p k c -> p (k c)"), in_=wq_d)),
                           DMA('b1', p1_bar, lambda: nc.sync.dma_start(out=wqs[:].rearrange("p k c -> p (k c)"), in_=wqsw_d)),
                           DMA('b1', p1_bar, lambda: nc.sync.dma_start(out=wo[:].rearrange("p k c -> p (k c)"), in_=wo_d))]
                    ready2 = [p1_bar, wl2]
                    cq_free2 = [None, None]; rope_free = None; QT_free = []
                    sc_free = [None, None]; pTs_free = [None, None, None]; po_free = [None, None]; osb_free = [None, None]
                    rinv_free = [None]; prb_free = None; aT_free = [None, None]; pa_free = []
                    merged_free = [None] * 4; mT_free = [None, None]; pmT_free = None
                    otmp_free = None; xr_free = [None, None]; x1_free = [None, None]
                    step = 0
                    tcount = 0
                    for qg in range(NQG[s]):
                        gi = qg % 2
                        gs = slice(qg * 512, (qg + 1) * 512)
                        lq = DMA('cql%d' % gi, [cq_free2[gi], ready2], lambda gi=gi, qg=qg: nc.sync.dma_start(
                            out=cqT[gi][:].rearrange("p k c -> p (k c)"), in_=cq_d[s * NG + qg]))
                        lr1 = DMA('rp2c', [rope_free, ready2], lambda gs=gs: nc.sync.dma_start(out=Ct[64:96, :], in_=rope_c[s, :, gs]))
                        lr2 = DMA('rp2s', [rope_free, ready2], lambda gs=gs: nc.sync.dma_start(out=St[64:96, :], in_=rope_s[s, :, gs]))
                        pre_ld = []
                        for t in range(4):
                            tok0_ = s * S + qg * 512 + t * 128
                            l_sn = DMA('snl%d' % t, [merged_free[t], ready2], lambda t=t, tok0_=tok0_: nc.sync.dma_start(
                                out=merged[t][:, 512:1024], in_=sn_d[tok0_:tok0_ + 128, :]))
                            pre_ld.append(l_sn)
                        wq4 = wq[:].rearrange("p k (h c) -> p k h c", h=8)
                        wqs4 = wqs[:].rearrange("p k (h c) -> p k h c", h=8)
                        QT_w = []
                        Qs = dict(mqs=None, qd=None)

                        def q_head(h):
                            T = scT[h % 2]
                            hi = h % 2
                            mq = mmg(T[0:96, 0:512], [(wq4[:, k, h, :], cqT[gi][:, k, :]) for k in range(2)], [lq, sc_free[h % 2]])
                            mqs = mmg(T[0:96, 512:1024], [(wqs4[:, k, h, :], cqT[gi][:, k, :]) for k in range(2)], None)
                            Qs['mqs'] = mqs
                            yield
                            c0 = ACT([mq, QT_free if h == 0 else None], lambda: nc.scalar.copy(out=QT[0:64, h, :], in_=T[0:64, 0:512]))
                            q1 = DVE([mq, lr1, qt_free[hi]], lambda: nc.vector.tensor_tensor(out=qt1[hi][64:96, :], in0=T[64:96, 0:512], in1=Ct[64:96, :], op=ALU.mult))
                            q2 = DVE([mqs, lr2], lambda: nc.vector.tensor_tensor(out=qt2[hi][64:96, :], in0=T[64:96, 512:1024], in1=St[64:96, :], op=ALU.mult))
                            sc_free[h % 2] = [c0, q2]
                            yield
                            qd = DVE([q1, q2, QT_free if h == 0 else None], lambda: nc.vector.tensor_tensor(
                                out=QT[64:96, h, :], in0=qt1[hi][64:96, :], in1=qt2[hi][64:96, :], op=ALU.add))
                            qt_free[hi] = qd
                            Qs['qd'] = qd
                            QT_w.extend([c0, qd])
                        interleave((q_head(h) for h in range(8)), 2)
                        mqs = Qs['mqs']
                        qd = Qs['qd']
                        cq_free2[gi] = mqs
                        rope_free = qd
                        NP = KB // 2
                        steps = [(h, j) for h in range(8) for j in range(NP)]
                        qk_tok = {}

                        def emit_qk(idx):
                            h, j = steps[idx]
                            T = scT[idx % 2]
                            tk = None
                            for u in range(2):
                                kb = 2 * j + u
                                tk = PE([sc_free[idx % 2], QT_w] if u == 0 else None,
                                        lambda h=h, kb=kb, u=u, T=T: nc.tensor.matmul(
                                            T[:, u * 512:(u + 1) * 512], lhsT=KT[0:96, h, kb * 128:(kb + 1) * 128], rhs=QT[0:96, h, :],
                                            start=True, stop=True), sig=(u == 1))
                            qk_tok[idx] = tk

                        emit_qk(0)
                        pa_w = []
                        QT_readers = []
                        pending = []
                        A = dict(prb_free=prb_free)
                        for idx, (h, j) in enumerate(steps):
                            if idx + 1 < len(steps):
                                emit_qk(idx + 1)
                            T = scT[idx % 2]
                            sl = step % 3
                            step += 1
                            ex = ACT([qk_tok[idx], pTs_free[sl]], lambda T=T, sl=sl: nc.scalar.activation(out=pTs[sl][:], in_=T[:, :], func=AF.Exp))
                            sc_free[idx % 2] = ex
                            pvt = None
                            for u in range(2):
                                kb = 2 * j + u
                                pvt = PE([ex, po_free[h % 2] if (j == 0 and u == 0) else None],
                                         lambda h=h, kb=kb, u=u, sl=sl: nc.tensor.matmul(
                                             po[h % 2][0:65, :], lhsT=VA[:, kb, h, :], rhs=pTs[sl][:, u * 512:(u + 1) * 512],
                                             start=(kb == 0), stop=(kb == KB - 1)), sig=(u == 1))
                            pTs_free[sl] = pvt
                            for pend in list(pending):
                                pend[0] -= 1
                                if pend[0] <= 0:
                                    pend[1]()
                                    pending.remove(pend)
                            if j == min(1, NP - 1) and bg:
                                bg.pop(0)()
                            if j == NP - 1:
                                for pend in list(pending):
                                    pend[1]()
                                    pending.remove(pend)
                                oi = h % 2
                                QT_readers.append(pvt)
                                o1 = DVE([pvt, osb_free[oi]], lambda oi=oi: nc.vector.tensor_copy(out=osb[oi][0:65, :], in_=po[oi][0:65, :]))
                                po_free[oi] = o1
                                hs = dict(o1=o1, oi=oi, h=h)

                                def part_a(hs=hs):
                                    oi = hs['oi']
                                    o3 = None
                                    for t in range(4):
                                        o3 = PE([hs['o1'], A['prb_free']] if t == 0 else None,
                                                lambda t=t, oi=oi: nc.tensor.transpose(ptr3[:, t, :], osb[oi][0:65, t * 128:(t + 1) * 128], ident_f[0:65, 0:65]),
                                                sig=(t == 3))
                                    osb_free[oi] = o3
                                    hs['o3'] = o3

                                def part_b(hs=hs):
                                    oi = hs['oi']; h = hs['h']
                                    rv = DVE([hs['o3']], lambda oi=oi: nc.vector.reciprocal(out=rcp[oi][:, :], in_=ptr3[:, :, 64]))
                                    o4 = DVE([rv, pa_free if h == 0 else None], lambda oi=oi, h=h: nc.vector.tensor_tensor(
                                        out=a_tok[:, :, h * 64:(h + 1) * 64], in0=ptr3[:, :, 0:64],
                                        in1=rcp[oi][:, :].unsqueeze(2).to_broadcast([128, 4, 64]), op=ALU.mult))
                                    A['prb_free'] = o4
                                    pa_w.append(o4)
                                pending.append([2, part_a])
                                pending.append([3, part_b])
                        for pend in list(pending):
                            pend[1]()
                            pending.remove(pend)
                        prb_free = A['prb_free']
                        QT_free = QT_readers
                        pa_r = []
                        M = dict(pmT_free=pmT_free, prb_free=prb_free, otmp_free=otmp_free)

                        def mg_tile(t, ti):
                            tok0 = s * S + qg * 512 + t * 128
                            otok0 = qbase + qg * 512 + t * 128
                            st_ = stt[ti]
                            lsn = pre_ld[t]
                            lxr = DMA('xr%d' % ti, [xr_free[ti], ready2], lambda: nc.sync.dma_start(
                                out=xr[ti][:], in_=xs[tok0:tok0 + 128, :]))
                            a1 = SQ(pa_w, lambda: nc.scalar.activation(out=junk[:, 0:512], in_=a_tok[:, t, :], func=AF.Square,
                                                                        accum_out=st_[:, 0:1]))
                            yield
                            r1a = ACT(a1, lambda: nc.scalar.activation(out=st_[:, 1:2], in_=st_[:, 0:1], func=AF.Sqrt, bias=EPS, scale=1.0 / 512))
                            yield
                            r1 = DVE(r1a, lambda: nc.vector.reciprocal(out=st_[:, 1:2], in_=st_[:, 1:2]))
                            c1 = ACT([r1, merged_free[t]], lambda: nc.scalar.activation(
                                out=merged[t][:, 0:512], in_=a_tok[:, t, :], func=AF.Copy, scale=st_[:, 1:2]))
                            pa_r.append(c1)
                            yield
                            tp = None
                            for k in range(8):
                                tp = PE([c1, lsn, M['pmT_free'], M['prb_free']] if k == 0 else None,
                                        lambda k=k: nc.tensor.transpose(pmT[:, k * 128:(k + 1) * 128], merged[t][:, k * 128:(k + 1) * 128], ident_b[:]),
                                        sig=(k == 7))
                            merged_free[t] = tp
                            yield
                            ev = DVE([tp, mT_free[ti]], lambda: nc.vector.tensor_copy(out=mT[ti][:].rearrange("p k c -> p (k c)"), in_=pmT))
                            M['pmT_free'] = ev
                            M['prb_free'] = ev
                            yield
                            T = scT[t % 2]
                            mo1 = mmg(T[:, 0:512], [(mT[ti][:, k, :], wo[:, k, 0:512]) for k in range(8)], [ev, sc_free[t % 2]])
                            mo2 = mmg(T[:, 512:1024], [(mT[ti][:, k, :], wo[:, k, 512:1024]) for k in range(8)], None)
                            mT_free[ti] = mo2
                            yield
                            a2 = SQ(mo2, lambda: nc.scalar.activation(out=junk[:], in_=T[:, :], func=AF.Square, accum_out=st_[:, 2:3]))
                            yield
                            r2a = ACT(a2, lambda: nc.scalar.activation(out=st_[:, 3:4], in_=st_[:, 2:3], func=AF.Sqrt, bias=EPS, scale=1.0 / D))
                            yield
                            r2 = DVE(r2a, lambda: nc.vector.reciprocal(out=st_[:, 3:4], in_=st_[:, 3:4]))
                            d1 = DVE([r2, M['otmp_free']], lambda: nc.vector.scalar_tensor_tensor(
                                out=otmp[:], in0=T[:, :], scalar=st_[:, 3:4], in1=gvec1[:], op0=ALU.mult, op1=ALU.mult))
                            sc_free[t % 2] = d1
                            pp = POOL([d1, lxr, x1_free[ti]], lambda: nc.gpsimd.tensor_tensor(out=x1[ti][:], in0=otmp[:], in1=xr[ti][:], op=ALU.add))
                            M['otmp_free'] = pp
                            xr_free[ti] = pp
                            x1_free[ti] = DMA('x1s%d' % ti, pp, lambda: nc.sync.dma_start(
                                out=x1_d[otok0:otok0 + 128, :], in_=x1[ti][:]))
                        interleave((mg_tile(t, (tcount + t) % 2) for t in range(4)), 2)
                        tcount += 4
                        pmT_free = M['pmT_free']; prb_free = M['prb_free']; otmp_free = M['otmp_free']
                        pa_free = pa_r
                    dp.retire_since(mk2)
                    p2_bar = dp.last() + [x1_free]
                seq_bar = p2_bar
                qbase += NQG[s] * 512
            stageA_bar = seq_bar
        while bg:
            bg.pop(0)()
        wcast_tok = wcast

        NTT = NQT // 128
        gseq = []
        for s in range(NSEQ):
            gseq += [s] * (NQG[s] * 4)
        with ExitStack() as bes:
            def sbB(name, shape, dt): return bes.enter_context(nc.sbuf_tensor(U(name), shape, dt))
            geff2_ = [sbB("geff2_%d" % i, [128, D], F32) for i in range(2)]
            sh2r_ = [sbB("sh2r_%d" % i, [128, D], F32) for i in range(2)]
            gvec2_ = [sbB("gvec2_%d" % i, [128, D], F32) for i in range(2)]
            vt = [sbB("vtB%d" % i, [128, D], F32) for i in range(2)]
            M1a = sbB("M1a", [128, NTT, 32], F32)
            M2a = sbB("M2a", [128, NTT, 32], F32)
            W1a = sbB("W1a", [128, NTT], F32)
            W2a = sbB("W2a", [128, NTT], F32)
            R1a = sbB("R1a", [128, NTT], F32)
            R2a = sbB("R2a", [128, NTT], F32)
            slot0 = sbB("slot0", [128, NTT], I32)
            slot1 = sbB("slot1", [128, NTT], I32)
            idxw = sbB("idxw", [128, NSL], I32)
            carry = sbB("carry", [128, 32], F32)
            PS = [bes.enter_context(nc.psum_tensor(U("PS%d" % i), [128, 512], F32)) for i in range(8)]
            bb = stageA_bar
            readyB = [bb, wcast_tok, zero_tok]

            def load_vecs(s, deps):
                geff2 = geff2_[s % 2]; sh2r = sh2r_[s % 2]; gvec2 = gvec2_[s % 2]
                l1 = DMA('v0_0', deps, lambda s=s: nc.sync.dma_start(out=vt[0][:], in_=mod_d[s, 4096:5120].partition_broadcast(128)))
                l2 = DMA('v0_1', deps, lambda: nc.sync.dma_start(out=vt[1][:], in_=g_pre2.partition_broadcast(128)))
                l3 = DMA('v0_2', deps, lambda s=s: nc.sync.dma_start(out=sh2r[:], in_=mod_d[s, 3072:4096].partition_broadcast(128)))
                tg = DVE([l1, l2], lambda: nc.vector.scalar_tensor_tensor(out=geff2[:], in0=vt[0][:], scalar=1.0, in1=vt[1][:],
                                                                           op0=ALU.add, op1=ALU.mult))
                l4 = DMA('v0_3', [tg], lambda s=s: nc.sync.dma_start(out=vt[0][:], in_=mod_d[s, 5120:6144].partition_broadcast(128)))
                l5 = DMA('v0_4', [tg], lambda: nc.sync.dma_start(out=vt[1][:], in_=g_post2.partition_broadcast(128)))
                tg2 = DVE([l4, l5], lambda: nc.vector.tensor_tensor(out=gvec2[:], in0=vt[0][:], in1=vt[1][:], op=ALU.mult))
                return [l3, tg, tg2]

            mkb1 = dp.mark()
            with ExitStack() as b1:
                def sb1(name, shape, dt): return b1.enter_context(nc.sbuf_tensor(U(name), shape, dt))
                w_r = sb1("w_r", [128, 8, 36], F32)
                brr = sb1("brr", [128, 36], F32)
                utri = sb1("utri", [128, 128], BF16)
                onesb = sb1("onesb", [128, 128], BF16)
                x1t = [sb1("x1t%d" % i, [128, D], F32) for i in range(5)]
                junk = sb1("junkB", [128, D], BF16)
                hm = sb1("hmB", [128, D], F32)
                h2 = [sb1("h2_%d" % i, [128, D], F32) for i in range(5)]
                h2Tf = [sb1("h2Tf%d" % i, [128, 8, 128], F32) for i in range(5)]
                stt = [sb1("stB%d" % i, [128, 8], F32) for i in range(5)]
                lg = [sb1("lg%d" % i, [128, 36], F32) for i in range(5)]
                wk = [sb1("wk%d" % i, [128, 192], F32) for i in range(5)]
                ohb = [sb1("ohb%d" % i, [128, 32], BF16) for i in range(5)]
                PH = [PS[6], PS[7]]
                ld = [DMA('s0', bb, lambda: nc.sync.dma_start(out=w_r[:, :, 0:4], in_=w_rg.rearrange("(k p) c -> p k c", p=128))),
                      DMA('s0', bb, lambda: nc.sync.dma_start(out=w_r[:, :, 4:36], in_=w_re.rearrange("(k p) c -> p k c", p=128))),
                      DMA('s0', bb, lambda: nc.sync.dma_start(out=brr[:, 0:4], in_=b_rg.partition_broadcast(128))),
                      DMA('s0', bb, lambda: nc.sync.dma_start(out=brr[:, 4:36], in_=b_re.partition_broadcast(128))),
                      DMA('s1', bb, lambda: nc.gpsimd.dma_start(out=utri[:], in_=utri_in), q='pool'),
                      POOL(bb, lambda: nc.gpsimd.memset(onesb[:], 1.0)),
                      POOL(bb, lambda: nc.gpsimd.memset(carry[:], 0.0))]
                rdy1 = [readyB, ld]
                T = dict(cur_seq=-1, vec_ready=None, vec_readers=[], hm_free=None, PH_free=[None, None], plg_free=None,
                         pcum_free=None, carry_tok=ld[-1])
                x1t_free = [None] * 5; h2_free = [None] * 5
                h2Tf_free = [None] * 5
                h2d_w = []

                def p1_tile(i):
                    s = gseq[i]
                    if s != T['cur_seq']:
                        T['cur_seq'] = s
                        T['vec_ready'] = load_vecs(s, [rdy1, T['vec_readers']])
                        T['vec_readers'] = []
                    vec_ready = T['vec_ready']
                    geff2 = geff2_[s % 2]; sh2r = sh2r_[s % 2]
                    ti = i % 5
                    tok0 = i * 128
                    st_ = stt[ti]
                    lx = DMA('bx%d' % ti, [x1t_free[ti], rdy1], lambda ti=ti, tok0=tok0: nc.sync.dma_start(out=x1t[ti][:], in_=x1_d[tok0:tok0 + 128, :]))
                    a1 = SQ(lx, lambda ti=ti, st_=st_: nc.scalar.activation(out=junk[:], in_=x1t[ti][:], func=AF.Square, accum_out=st_[:, 0:1]))
                    yield
                    r1a = ACT(a1, lambda st_=st_: nc.scalar.activation(out=st_[:, 1:2], in_=st_[:, 0:1], func=AF.Sqrt, bias=EPS, scale=1.0 / D))
                    yield
                    r1 = DVE(r1a, lambda st_=st_: nc.vector.reciprocal(out=st_[:, 1:2], in_=st_[:, 1:2]))
                    d1 = DVE([r1, T['hm_free'], vec_ready], lambda ti=ti, st_=st_: nc.vector.scalar_tensor_tensor(
                        out=hm[:], in0=x1t[ti][:], scalar=st_[:, 1:2], in1=geff2[:], op0=ALU.mult, op1=ALU.mult))
                    x1t_free[ti] = d1
                    p1_ = POOL([d1, h2_free[ti], vec_ready], lambda ti=ti: nc.gpsimd.tensor_tensor(out=h2[ti][:], in0=hm[:], in1=sh2r[:], op=ALU.add))
                    T['hm_free'] = p1_
                    T['vec_readers'] = [p1_, d1]
                    wr = DMA('h2w%d' % ti, p1_, lambda ti=ti, tok0=tok0: nc.gpsimd.dma_start(out=h2_d[tok0:tok0 + 128, :], in_=h2[ti][:]), q='pool')
                    h2d_w.append(wr)
                    yield
                    tp = None
                    for k in range(8):
                        bank = PH[k // 4]
                        tp = PE([p1_, T['PH_free']] if k == 0 else None,
                                lambda k=k, ti=ti, bank=bank: nc.tensor.transpose(bank[:, (k % 4) * 128:(k % 4 + 1) * 128],
                                                                                  h2[ti][:, k * 128:(k + 1) * 128], ident_f[:]),
                                sig=(k == 7))
                    h2_free[ti] = [tp, wr]
                    yield
                    e1 = ACT([tp, h2Tf_free[ti]], lambda ti=ti: nc.scalar.copy(out=h2Tf[ti][:, 0:4, :], in_=PH[0][:, :].rearrange("p (k c) -> p k c", k=4)))
                    e2 = DVE([tp, h2Tf_free[ti]], lambda ti=ti: nc.vector.tensor_copy(out=h2Tf[ti][:, 4:8, :], in_=PH[1][:, :].rearrange("p (k c) -> p k c", k=4)))
                    T['PH_free'] = [e1, e2]
                    yield
                    plg = PS[4][:, 0:36]
                    m_l = mmg(plg, [(h2Tf[ti][:, k, :], w_r[:, k, :]) for k in range(8)], [e1, e2, T['plg_free'], rdy1])
                    h2Tf_free[ti] = m_l
                    yield
                    L = lg[ti]; W = wk[ti]
                    v1 = DVE([m_l], lambda L=L: nc.vector.tensor_tensor(out=L[:], in0=plg, in1=brr[:], op=ALU.add))
                    T['plg_free'] = v1
                    v2 = DVE(v1, lambda L=L, W=W: nc.vector.tensor_reduce(out=W[:, 0:1], in_=L[:, 0:4], axis=AX.X, op=ALU.max))
                    v3 = DVE(v2, lambda W=W: nc.vector.tensor_scalar(out=W[:, 1:2], in0=W[:, 0:1], scalar1=-1.0, scalar2=None, op0=ALU.mult))
                    v4 = DVE(v2, lambda L=L, W=W: nc.vector.tensor_scalar(out=W[:, 4:8], in0=L[:, 0:4], scalar1=W[:, 0:1], scalar2=None, op0=ALU.is_equal))
                    s1 = ACT([v3], lambda L=L, W=W: nc.scalar.activation(out=W[:, 8:12], in_=L[:, 0:4], func=AF.Exp, bias=W[:, 1:2], scale=1.0,
                                                                         accum_out=W[:, 2:3]))
                    yield
                    v5 = DVE(s1, lambda W=W: nc.vector.reciprocal(out=W[:, 3:4], in_=W[:, 2:3]))
                    v6 = DVE(v4, lambda L=L, W=W: nc.vector.tensor_tensor(
                        out=W[:, 16:48].rearrange("p (g e) -> p g e", g=4), in0=L[:, 4:36].rearrange("p (g e) -> p g e", g=4),
                        in1=W[:, 4:8].unsqueeze(2).to_broadcast([128, 4, 8]), op=ALU.mult))
                    v7 = DVE(v6, lambda W=W: nc.vector.tensor_reduce(out=W[:, 48:56], in_=W[:, 16:48].rearrange("p (g e) -> p e g", g=4),
                                                                    axis=AX.X, op=ALU.add))
                    v8 = DVE(v7, lambda W=W: nc.vector.tensor_reduce(out=W[:, 12:13], in_=W[:, 48:56], axis=AX.X, op=ALU.max))
                    v9 = DVE(v8, lambda W=W: nc.vector.tensor_scalar(out=W[:, 56:64], in0=W[:, 48:56], scalar1=W[:, 12:13], scalar2=None,
                                                                    op0=ALU.is_equal))
                    v10 = DVE(v9, lambda W=W: nc.vector.scalar_tensor_tensor(out=W[:, 64:72], in0=W[:, 56:64], scalar=-1e30, in1=W[:, 48:56],
                                                                            op0=ALU.mult, op1=ALU.add))
                    v11 = DVE(v10, lambda W=W: nc.vector.tensor_reduce(out=W[:, 13:14], in_=W[:, 64:72], axis=AX.X, op=ALU.max))
                    v12 = DVE(v11, lambda W=W: nc.vector.tensor_scalar(out=W[:, 72:80], in0=W[:, 64:72], scalar1=W[:, 13:14], scalar2=None,
                                                                      op0=ALU.is_equal))
                    v13 = DVE(v11, lambda W=W: nc.vector.tensor_scalar(out=W[:, 14:15], in0=W[:, 12:13], scalar1=-1.0, scalar2=None, op0=ALU.mult))
                    s2 = ACT([v13], lambda W=W: nc.scalar.activation(out=W[:, 15:16], in_=W[:, 13:14], func=AF.Exp, bias=W[:, 14:15], scale=1.0))
                    yield
                    v14 = DVE(s2, lambda W=W: nc.vector.tensor_scalar(out=W[:, 80:81], in0=W[:, 15:16], scalar1=1.0, scalar2=None, op0=ALU.add))
                    v15 = DVE(v14, lambda W=W: nc.vector.reciprocal(out=W[:, 81:82], in_=W[:, 80:81]))
                    v16 = DVE([v15, v5], lambda W=W, i=i: nc.vector.tensor_tensor(out=W1a[:, i:i + 1], in0=W[:, 81:82], in1=W[:, 3:4], op=ALU.mult))
                    v17 = DVE(v16, lambda W=W, i=i: nc.vector.tensor_tensor(out=W2a[:, i:i + 1], in0=W1a[:, i:i + 1], in1=W[:, 15:16], op=ALU.mult))
                    v18 = DVE([v9, v4], lambda W=W, i=i: nc.vector.tensor_tensor(
                        out=M1a[:, i, :].rearrange("p (g e) -> p g e", g=4), in0=W[:, 4:8].unsqueeze(2).to_broadcast([128, 4, 8]),
                        in1=W[:, 56:64].unsqueeze(1).to_broadcast([128, 4, 8]), op=ALU.mult))
                    v19 = DVE([v12], lambda W=W, i=i: nc.vector.tensor_tensor(
                        out=M2a[:, i, :].rearrange("p (g e) -> p g e", g=4), in0=W[:, 4:8].unsqueeze(2).to_broadcast([128, 4, 8]),
                        in1=W[:, 72:80].unsqueeze(1).to_broadcast([128, 4, 8]), op=ALU.mult))
                    OH = ohb[ti]
                    v20 = DVE([v18, v19, T['pcum_free']], lambda OH=OH, i=i: nc.vector.tensor_tensor(out=OH[:], in0=M1a[:, i, :], in1=M2a[:, i, :], op=ALU.add))
                    pcum = PS[5][:, 0:32]
                    ptot = PS[5][:, 32:64]
                    PE([v20, T['pcum_free'], rdy1], lambda OH=OH: nc.tensor.matmul(pcum, lhsT=utri[:], rhs=OH[:], start=True, stop=True), sig=False)
                    mc = PE(None, lambda OH=OH: nc.tensor.matmul(ptot, lhsT=onesb[:], rhs=OH[:], start=True, stop=True))
                    yield
                    v21 = DVE([mc, T['carry_tok']], lambda W=W: nc.vector.tensor_tensor(out=W[:, 96:128], in0=carry[:], in1=pcum, op=ALU.add))
                    v22 = DVE(v21, lambda: nc.vector.tensor_tensor(out=carry[:], in0=carry[:], in1=ptot, op=ALU.add))
                    T['carry_tok'] = v22
                    T['pcum_free'] = v22
                    v23 = DVE(v22, lambda W=W, i=i: nc.vector.tensor_tensor(out=W[:, 128:160], in0=W[:, 96:128], in1=M1a[:, i, :], op=ALU.mult))
                    v24 = DVE(v23, lambda W=W, i=i: nc.vector.tensor_reduce(out=R1a[:, i:i + 1], in_=W[:, 128:160], axis=AX.X, op=ALU.add))
                    v25 = DVE(v24, lambda W=W, i=i: nc.vector.tensor_tensor(out=W[:, 160:192], in0=W[:, 96:128], in1=M2a[:, i, :], op=ALU.mult))
                    v26 = DVE(v25, lambda W=W, i=i: nc.vector.tensor_reduce(out=R2a[:, i:i + 1], in_=W[:, 160:192], axis=AX.X, op=ALU.add))
                interleave((p1_tile(i) for i in range(NTT)), 5)
                dp.retire_since(mkb1)
                b1_bar = dp.last() + [h2d_w]

            with ExitStack() as b2:
                def sb2(name, shape, dt): return b2.enter_context(nc.sbuf_tensor(U(name), shape, dt))
                jv = sb2("jv", [128, NSL], F32)
                pidx = sb2("pidx", [128, 1], F32)
                tri32 = sb2("tri32", [32, 32], F32)
                cmp_ = sb2("cmp", [128, NSL * 32], F32)
                tmpM = sb2("tmpM", [128, NTT, 32], F32)
                nblk = sb2("nblk", [128, 32], F32)
                pc = sb2("pc", [128, 32], F32)
                pcT = sb2("pcT", [32, 128], F32)
                sst = sb2("sst", [128, 32], F32)
                send = sb2("send", [128, 32], F32)
                te = sb2("te", [128, NSL], F32)
                sf = sb2("sf", [128, NTT], F32)
                l = [DMA('i0', b1_bar, lambda: nc.sync.dma_start(out=jv[:], in_=jv_in)),
                     DMA('i0', b1_bar, lambda: nc.sync.dma_start(out=pidx[:], in_=pidx_in)),
                     DMA('i0', b1_bar, lambda: nc.sync.dma_start(out=tri32[:], in_=tri32_in))]
                c3 = cmp_[:].rearrange("p (e m) -> p e m", e=32)
                q1 = DVE([l, b1_bar], lambda: nc.vector.tensor_tensor(out=c3, in0=jv[:].unsqueeze(1).to_broadcast([128, 32, NSL]),
                                                                      in1=carry[:].unsqueeze(2).to_broadcast([128, 32, NSL]), op=ALU.is_lt))
                q2 = DVE(q1, lambda: nc.vector.tensor_reduce(out=nblk[:], in_=c3, axis=AX.X, op=ALU.add))
                q3 = DVE(q2, lambda: nc.vector.tensor_scalar(out=pc[:], in0=nblk[:], scalar1=128.0, scalar2=None, op0=ALU.mult))
                q4 = PE(q3, lambda: nc.tensor.transpose(PS[0][0:32, 0:128], pc[:, :], ident_f[:]))
                q5 = ACT(q4, lambda: nc.scalar.copy(out=pcT[:], in_=PS[0][0:32, 0:128]))
                q6 = PE([q5, l], lambda: nc.tensor.matmul(PS[1][:, 0:32], lhsT=pcT[:, :], rhs=tri32[:, :], start=True, stop=True))
                q7 = DVE(q6, lambda: nc.vector.tensor_copy(out=sst[:], in_=PS[1][:, 0:32]))
                q8 = DVE(q7, lambda: nc.vector.tensor_tensor(out=send[:], in0=sst[:], in1=pc[:], op=ALU.add))
                c4 = cmp_[:].rearrange("p (m e) -> p m e", e=32)
                q9 = DVE(q8, lambda: nc.vector.tensor_tensor(out=c4, in0=send[:].unsqueeze(1).to_broadcast([128, NSL, 32]),
                                                             in1=jv[:].unsqueeze(2).to_broadcast([128, NSL, 32]), op=ALU.is_le))
                q10 = DVE(q9, lambda: nc.vector.tensor_reduce(out=te[:], in_=c4, axis=AX.X, op=ALU.add))
                q11 = DVE(q10, lambda: nc.vector.tensor_scalar(out=te[:], in0=te[:], scalar1=31.0, scalar2=128.0, op0=ALU.min, op1=ALU.mult))
                q12 = DVE(q11, lambda: nc.vector.tensor_scalar(out=te[:], in0=te[:], scalar1=pidx[:, 0:1], scalar2=None, op0=ALU.add))
                q13 = DVE(q12, lambda: nc.vector.tensor_copy(out=idxw[:], in_=te[:]))
                q14 = DVE(q7, lambda: nc.vector.tensor_tensor(out=tmpM[:], in0=M1a[:], in1=sst[:].unsqueeze(1).to_broadcast([128, NTT, 32]), op=ALU.mult))
                q15 = DVE(q14, lambda: nc.vector.tensor_reduce(out=sf[:], in_=tmpM[:], axis=AX.X, op=ALU.add))
                q16 = DVE(q15, lambda: nc.vector.tensor_tensor(out=sf[:], in0=sf[:], in1=R1a[:], op=ALU.add))
                q17 = DVE(q16, lambda: nc.vector.tensor_copy(out=slot0[:], in_=sf[:]))
                q18 = DVE(q17, lambda: nc.vector.tensor_tensor(out=tmpM[:], in0=M2a[:], in1=sst[:].unsqueeze(1).to_broadcast([128, NTT, 32]), op=ALU.mult))
                q19 = DVE(q18, lambda: nc.vector.tensor_reduce(out=sf[:], in_=tmpM[:], axis=AX.X, op=ALU.add))
                q20 = DVE(q19, lambda: nc.vector.tensor_tensor(out=sf[:], in0=sf[:], in1=R2a[:], op=ALU.add))
                q21 = DVE(q20, lambda: nc.vector.tensor_copy(out=slot1[:], in_=sf[:]))
                b2_bar = dp.last()

            mkb3 = dp.mark()
            with ExitStack() as b3:
                def sb3(name, shape, dt): return b3.enter_context(nc.sbuf_tensor(U(name), shape, dt))
                hsc = [sb3("hsc%d" % i, [128, D], BF16) for i in range(3)]
                hsc_free = [None] * 3
                sc_toks = []
                for i in range(NTT):
                    si = i % 3
                    tok0 = i * 128
                    lh = DMA('hl%d' % si, [hsc_free[si], b2_bar], lambda si=si, tok0=tok0: nc.sync.dma_start(out=hsc[si][:], in_=h2_d[tok0:tok0 + 128, :]))
                    s0 = DMA('sc%d' % si, [lh, b2_bar], lambda si=si, i=i: nc.gpsimd.indirect_dma_start(
                        out=xs_d[:, :], out_offset=bass.IndirectOffsetOnAxis(ap=slot0[:, i:i + 1], axis=0), in_=hsc[si][:, :], in_offset=None), q='pool')
                    s1_ = DMA('sc%d' % si, [lh], lambda si=si, i=i: nc.gpsimd.indirect_dma_start(
                        out=xs_d[:, :], out_offset=bass.IndirectOffsetOnAxis(ap=slot1[:, i:i + 1], axis=0), in_=hsc[si][:, :], in_offset=None), q='pool')
                    hsc_free[si] = [s0, s1_]
                    sc_toks += [s0, s1_]
                scat_done = [sc_toks[-6:], b2_bar]

                PF = 3
                NW = PF + 3
                ND = PF + 5
                NX = PF + 2
                wgu = [sb3("wgu%d" % i, [128, 8, 512], BF16) for i in range(NW)]
                wdb = [sb3("wdb%d" % i, [128, 2, D], BF16) for i in range(ND)]
                xsb = [sb3("xsb%d" % i, [128, D], BF16) for i in range(NX)]
                xT = [sb3("xT%d" % i, [128, 8, 128], BF16) for i in range(2)]
                sgs = [sb3("sgs%d" % i, [128, 256], F32) for i in range(2)]
                hid = [sb3("hid%d" % i, [128, 256], BF16) for i in range(2)]
                hT = [sb3("hT%d" % i, [128, 2, 128], BF16) for i in range(2)]
                ysb = [sb3("ysb%d" % i, [128, D], F32) for i in range(2)]
                pX = [PS[0][:, :].bitcast(BF16), PS[1][:, :].bitcast(BF16)]
                pH = [PS[2], PS[3]]
                pHT = [PS[4][:, 0:128].bitcast(BF16), PS[5][:, 0:128].bitcast(BF16)]
                pY = [PS[6], PS[7]]
                wgu_free = [None] * NW; wdb_free = [None] * ND; xsb_free = [None] * NX
                pX_free = [None, None]; xT_free = [None, None]; pH_free = [None, None]; sgs_free = [None, None]
                hid_free = [None, None]; pHT_free = [None, None]; hT_free = [None, None]
                pY_free = [None, None]; ysb_free = [None, None]
                st0 = {}; st1 = {}; st2 = {}; ldt = {}
                ys_w = []

                def issue_loads(a):
                    wi = a % NW; di = a % ND; xj = a % NX
                    lw = DMA('wgl%d' % wi, [wgu_free[wi], scat_done], lambda wi=wi, a=a: nc.gpsimd.indirect_dma_start(
                        out=wgu[wi][:].rearrange("p k c -> p (k c)"), out_offset=None, in_=wgu_r[:, :],
                        in_offset=bass.IndirectOffsetOnAxis(ap=idxw[:, a:a + 1], axis=0)), q='pool')
                    lwd = DMA('wdl%d' % di, [wdb_free[di], scat_done], lambda di=di, a=a: nc.gpsimd.indirect_dma_start(
                        out=wdb[di][:].rearrange("p k c -> p (k c)"), out_offset=None, in_=wd_r[:, :],
                        in_offset=bass.IndirectOffsetOnAxis(ap=idxw[:, a:a + 1], axis=0)), q='pool')
                    lxs = DMA('xsl%d' % xj, [xsb_free[xj], scat_done, sc_toks], lambda xj=xj, a=a: nc.sync.dma_start(
                        out=xsb[xj][:], in_=xs_d[a * 128:(a + 1) * 128, :]))
                    ldt[a] = (lw, lwd, lxs)

                for a in range(min(PF, NSL)):
                    issue_loads(a)
                for it in range(NSL + 3):
                    if it + PF < NSL:
                        issue_loads(it + PF)
                    a = it
                    if a < NSL:
                        xi = a % 2; xj = a % NX
                        lw, lwd, lxs = ldt[a]
                        tp = None
                        for k in range(8):
                            tp = PE([lxs, pX_free[xi]] if k == 0 else None,
                                    lambda k=k, xi=xi, xj=xj: nc.tensor.transpose(pX[xi][:, k * 128:(k + 1) * 128], xsb[xj][:, k * 128:(k + 1) * 128], ident_b[:]),
                                    sig=(k == 7))
                        xsb_free[xj] = tp
                        if a % 2 == 0:
                            ev = ACT([tp, xT_free[xi]], lambda xi=xi: nc.scalar.copy(out=xT[xi][:].rearrange("p k c -> p (k c)"), in_=pX[xi]))
                        else:
                            ev = DVE([tp, xT_free[xi]], lambda xi=xi: nc.vector.tensor_copy(out=xT[xi][:].rearrange("p k c -> p (k c)"), in_=pX[xi]))
                        pX_free[xi] = ev
                        st0[a] = (ev, lw, lwd)
                    a = it - 1
                    if 0 <= a < NSL:
                        wi = a % NW; xi = a % 2
                        ev, lw, lwd = st0[a]
                        mh = mmg(pH[xi][:, :], [(xT[xi][:, k, :], wgu[wi][:, k, :]) for k in range(8)], [ev, lw, pH_free[xi]])
                        wgu_free[wi] = mh
                        xT_free[xi] = mh
                        a_s = ACT([mh, sgs_free[xi]], lambda xi=xi: nc.scalar.activation(out=sgs[xi][:], in_=pH[xi][:, 0:256], func=AF.Silu))
                        d_h = DVE([a_s, hid_free[xi]], lambda xi=xi: nc.vector.tensor_tensor(out=hid[xi][:], in0=sgs[xi][:], in1=pH[xi][:, 256:512], op=ALU.mult))
                        pH_free[xi] = d_h
                        sgs_free[xi] = d_h
                        st1[a] = (d_h, lwd)
                    a = it - 2
                    if 0 <= a < NSL:
                        xi = a % 2
                        d_h, lwd = st1[a]
                        tp2 = None
                        for j in range(2):
                            tp2 = PE([d_h, pHT_free[xi]] if j == 0 else None,
                                     lambda j=j, xi=xi: nc.tensor.transpose(pHT[xi][:, j * 128:(j + 1) * 128], hid[xi][:, j * 128:(j + 1) * 128], ident_b[:]),
                                     sig=(j == 1))
                        hid_free[xi] = tp2
                        ev2 = ACT([tp2, hT_free[xi]], lambda xi=xi: nc.scalar.copy(out=hT[xi][:].rearrange("p k c -> p (k c)"), in_=pHT[xi]))
                        pHT_free[xi] = ev2
                        st2[a] = (ev2, lwd)
                    a = it - 3
                    if 0 <= a < NSL:
                        xi = a % 2; di = a % ND
                        ev2, lwd = st2[a]
                        my0 = mmg(pY[0][:, :], [(hT[xi][:, j, :], wdb[di][:, j, 0:512]) for j in range(2)], [ev2, lwd, pY_free[0]])
                        my1 = mmg(pY[1][:, :], [(hT[xi][:, j, :], wdb[di][:, j, 512:1024]) for j in range(2)], [pY_free[1]])
                        wdb_free[di] = my1
                        hT_free[xi] = my1
                        c0 = ACT([my0, ysb_free[xi]], lambda xi=xi: nc.scalar.copy(out=ysb[xi][:, 0:512], in_=pY[0][:, :]))
                        c1 = DVE([my1, ysb_free[xi]], lambda xi=xi: nc.vector.tensor_copy(out=ysb[xi][:, 512:1024], in_=pY[1][:, :]))
                        pY_free = [c0, c1]
                        ysb_free[xi] = DMA('ysw%d' % xi, [c0, c1], lambda xi=xi, a=a: nc.sync.dma_start(out=ys_d[a * 128:(a + 1) * 128, :], in_=ysb[xi][:]))
                        ys_w.append(ysb_free[xi])
                dp.retire_since(mkb3)
                b3_bar = dp.last() + [ys_w[-2:]]

            with ExitStack() as b4:
                def sb4(name, shape, dt): return b4.enter_context(nc.sbuf_tensor(U(name), shape, dt))
                ya = [sb4("ya%d" % i, [128, D], F32) for i in range(5)]
                yb = [sb4("yb%d" % i, [128, D], F32) for i in range(5)]
                x1c = [sb4("x1c%d" % i, [128, D], F32) for i in range(5)]
                mm_ = [sb4("mm_%d" % i, [128, D], F32) for i in range(5)]
                ytmp = [sb4("ytmp%d" % i, [128, D], F32) for i in range(5)]
                yo = [sb4("yo%d" % i, [128, D], F32) for i in range(5)]
                junk = sb4("junkC", [128, D], BF16)
                stt = [sb4("stC%d" % i, [128, 8], F32) for i in range(5)]
                ya_free = [None] * 5; yb_free = [None] * 5; x1c_free = [None] * 5; mm_free = [None] * 5
                ytmp_free = [None] * 5; yo_free = [None] * 5
                T = dict(cur_seq=-1, vec_ready=None, vec_readers=[])
                out_toks = []

                def cmb_tile(i):
                    s = gseq[i]
                    if s != T['cur_seq']:
                        T['cur_seq'] = s
                        T['vec_ready'] = load_vecs(s, [b3_bar, T['vec_readers']])
                        T['vec_readers'] = []
                    vec_ready = T['vec_ready']
                    gvec2 = gvec2_[s % 2]
                    ti = i % 5
                    tok0 = i * 128
                    st_ = stt[ti]
                    ga = DMA('ga%d' % ti, [ya_free[ti], b3_bar, ys_w], lambda ti=ti, i=i: nc.gpsimd.indirect_dma_start(
                        out=ya[ti][:, :], out_offset=None, in_=ys_d[:, :], in_offset=bass.IndirectOffsetOnAxis(ap=slot0[:, i:i + 1], axis=0)), q='pool')
                    gb_ = DMA('gb%d' % ti, [yb_free[ti], b3_bar], lambda ti=ti, i=i: nc.gpsimd.indirect_dma_start(
                        out=yb[ti][:, :], out_offset=None, in_=ys_d[:, :], in_offset=bass.IndirectOffsetOnAxis(ap=slot1[:, i:i + 1], axis=0)), q='pool')
                    lx = DMA('cx%d' % ti, [x1c_free[ti], b3_bar], lambda ti=ti, tok0=tok0: nc.sync.dma_start(out=x1c[ti][:], in_=x1_d[tok0:tok0 + 128, :]))
                    yield
                    d1 = DVE([ga, mm_free[ti]], lambda ti=ti, i=i: nc.vector.tensor_scalar(out=mm_[ti][:], in0=ya[ti][:], scalar1=W1a[:, i:i + 1], scalar2=None, op0=ALU.mult))
                    ya_free[ti] = d1
                    d2 = DVE([gb_, d1], lambda ti=ti, i=i: nc.vector.scalar_tensor_tensor(out=mm_[ti][:], in0=yb[ti][:], scalar=W2a[:, i:i + 1], in1=mm_[ti][:],
                                                                                         op0=ALU.mult, op1=ALU.add))
                    yb_free[ti] = d2
                    a1 = SQ(d2, lambda ti=ti, st_=st_: nc.scalar.activation(out=junk[:], in_=mm_[ti][:], func=AF.Square, accum_out=st_[:, 0:1]))
                    yield
                    r1a = ACT(a1, lambda st_=st_: nc.scalar.activation(out=st_[:, 1:2], in_=st_[:, 0:1], func=AF.Sqrt, bias=EPS, scale=1.0 / D))
                    yield
                    r1 = DVE(r1a, lambda st_=st_: nc.vector.reciprocal(out=st_[:, 1:2], in_=st_[:, 1:2]))
                    d3 = DVE([r1, ytmp_free[ti], vec_ready], lambda ti=ti, st_=st_: nc.vector.scalar_tensor_tensor(
                        out=ytmp[ti][:], in0=mm_[ti][:], scalar=st_[:, 1:2], in1=gvec2[:], op0=ALU.mult, op1=ALU.mult))
                    mm_free[ti] = d3
                    T['vec_readers'] = [d3]
                    pp = POOL([d3, lx, yo_free[ti]], lambda ti=ti: nc.gpsimd.tensor_tensor(out=yo[ti][:], in0=ytmp[ti][:], in1=x1c[ti][:], op=ALU.add))
                    ytmp_free[ti] = pp
                    x1c_free[ti] = pp
                    yo_free[ti] = DMA('yo%d' % ti, pp, lambda ti=ti, tok0=tok0: nc.sync.dma_start(out=y[tok0:tok0 + 128, :], in_=yo[ti][:]))
                    out_toks.append(yo_free[ti])
                interleave((cmb_tile(i) for i in range(NTT)), 5)
            dp.wait('sp', [yo_free, out_toks[-5:]])
            for e in ('pe', 'act', 'dve', 'pool'):
                dp.wait('sp', [(e, dp.cnt[e])])
    return nc


def _rope_tables(pos):
    half = 16
    inv = (10000.0 ** (-np.arange(half, dtype=np.float32) / half)).astype(np.float32)
    ang = pos.astype(np.float32)[:, None] * inv[None, :]
    cos = np.cos(ang).astype(np.float32)
    sin = np.sin(ang).astype(np.float32)
    c = np.concatenate([cos, cos], axis=1).T
    s_ = np.concatenate([sin, sin], axis=1).T
    return np.ascontiguousarray(c), np.ascontiguousarray(s_)


def _consts(NSL):
    ident = np.eye(128, dtype=np.float32)
    egrp = np.zeros((8, 512), np.float32)
    for g in range(8):
        egrp[g, g * 64:(g + 1) * 64] = 1.0
    utri = np.triu(np.ones((128, 128), np.float32), k=1)
    tri32 = np.triu(np.ones((32, 32), np.float32), k=1)
    jv = np.tile((np.arange(NSL, dtype=np.float32) * 128.0)[None, :], (128, 1))
    pidx = np.arange(128, dtype=np.float32).reshape(128, 1)
    return dict(ident=ident, egrp=egrp, utri=utri, tri32=tri32, jv=np.ascontiguousarray(jv), pidx=pidx,
                zeros=np.zeros((128, 8192), np.float32))


def _nt(cfg):
    nqt = sum(cfg['NQG']) * 512
    nt = (2 * nqt + 32 * 127 + 127) // 128
    return ((nt + 7) // 8) * 8


WEIGHT_KEYS = ['w_ada', 'b_ada', 'g_pre1', 'g_post1', 'g_pre2', 'g_post2', 'w_in', 'g_q', 'w_uq', 'g_kv', 'w_ukv',
               'g_v_gmlp', 'w_spatial', 'b_spatial', 'g_attn_out', 'g_gmlp_out', 'w_out', 'w_router_group',
               'b_router_group', 'w_router_expert', 'b_router_expert', 'w_gate', 'w_up', 'w_down']

_NC_CACHE = {}


def kernel(**inputs):
    S = 4096
    x_all = np.concatenate([np.asarray(inputs['x_prompt'], np.float32), np.asarray(inputs['x_sample'], np.float32)], axis=0)
    c_all = np.concatenate([np.asarray(inputs['c_prompt'], np.float32), np.asarray(inputs['c_sample'], np.float32)], axis=0)
    weights = {k: np.ascontiguousarray(np.asarray(inputs[k], np.float32)) for k in WEIGHT_KEYS}
    consts = _consts(_nt(FULL_CFG))
    pos_nat = np.arange(S)
    in_maps = []
    plans = []
    for c in range(8):
        if c % 2 == 0:
            s0 = (5 * c) // 2
            A, B, Cq, qhalf = s0, s0 + 1, s0 + 2, 0
        else:
            s0 = (5 * c - 1) // 2
            Cq, qhalf, A, B = s0, 1, s0 + 1, s0 + 2
        if qhalf == 0:
            posC = pos_nat
        else:
            posC = np.concatenate([pos_nat[S // 2:], pos_nat[:S // 2]])
        xs = np.concatenate([x_all[A], x_all[B], x_all[Cq][posC]], axis=0)
        cv = np.stack([c_all[A], c_all[B], c_all[Cq]], axis=0)
        rc = np.zeros((3, 32, S), np.float32)
        rs = np.zeros((3, 32, S), np.float32)
        for i, p in enumerate([pos_nat, pos_nat, posC]):
            rc[i], rs[i] = _rope_tables(p)
        m = dict(weights)
        m.update(xs=np.ascontiguousarray(xs), cvec=np.ascontiguousarray(cv), rope_c=rc, rope_s=rs)
        m.update(consts)
        in_maps.append(m)
        plans.append((A, B, Cq, qhalf))
    if 'full' not in _NC_CACHE:
        _NC_CACHE['full'] = build(FULL_CFG)
    nc = _NC_CACHE['full']
    res = run_bass_kernel_spmd(nc, in_maps, core_ids=list(range(8)))
    y_all = np.zeros((20, S, D), np.float32)
    for c in range(8):
        yc = res.results[c]['y']
        A, B, Cq, qhalf = plans[c]
        y_all[A] = yc[0:S]
        y_all[B] = yc[S:2 * S]
        if qhalf == 0:
            y_all[Cq, 0:S // 2] = yc[2 * S:2 * S + S // 2]
        else:
            y_all[Cq, S // 2:] = yc[2 * S:2 * S + S // 2]
    return (np.ascontiguousarray(y_all[0:4]), np.ascontiguousarray(y_all[4:20]))
```

```python
import numpy as np
import concourse.bass as bass
import concourse.mybir as mybir
from concourse.bass_utils import run_bass_kernel_spmd
from contextlib import ExitStack

F32, BF16 = mybir.dt.float32, mybir.dt.bfloat16
I32 = mybir.dt.int32
AF = mybir.ActivationFunctionType
ALU = mybir.AluOpType
AX = mybir.AxisListType
D = 1024
EPS = 1e-6
NE = 32
QSCALE = 96.0 ** -0.5

FULL_CFG = dict(S=4096, NSEQ=3, NQG=[8, 8, 4])


class Dep:
    def __init__(self, nc, es):
        self.nc = nc
        self.es = es
        self.eng = {'pe': nc.tensor, 'act': nc.scalar, 'dve': nc.vector, 'pool': nc.gpsimd, 'sp': nc.sync}
        self.sem = {e: es.enter_context(nc.semaphore('s_' + e)) for e in self.eng}
        self.cnt = {e: 0 for e in self.eng}
        self.waited = {e: {} for e in self.eng}
        self.dsem = {}
        self.entries = {}
        self.free = []
        self.sw_names = set()

    def semof(self, k):
        return self.sem[k] if k in self.sem else self.entries[k][0]

    def wait(self, e, deps):
        mx = {}
        for k, v in _flat(deps):
            if v > mx.get(k, 0):
                mx[k] = v
        for k, v in mx.items():
            if self.waited[e].get(k, 0) < v:
                self.eng[e].wait_ge(self.semof(k), v)
                self.waited[e][k] = v

    def op(self, e, deps, fn, sig=True):
        self.wait(e, deps)
        ins = fn()
        if sig:
            ins.then_inc(self.sem[e], 1)
            self.cnt[e] += 1
            return (e, self.cnt[e])
        return None

    def dma(self, q, name, deps, fn):
        if name not in self.dsem:
            if q != 'pool' and self.free:
                key = self.free.pop()
            else:
                key = 'D%d' % len(self.entries)
                self.entries[key] = [self.es.enter_context(self.nc.semaphore('d_' + key)), 0]
            self.dsem[name] = key
            if q == 'pool':
                self.sw_names.add(name)
        assert (name in self.sw_names) == (q == 'pool'), name
        key = self.dsem[name]
        self.wait(q, deps)
        ins = fn()
        ent = self.entries[key]
        ins.then_inc(ent[0], 16)
        ent[1] += 16
        return (key, ent[1])

    def reserve(self, name):
        key = 'D%d' % len(self.entries)
        self.entries[key] = [self.es.enter_context(self.nc.semaphore('d_' + key)), 0]
        self.dsem[name] = key
        self.sw_names.add(name)

    def mark(self):
        return set(self.dsem.keys())

    def retire_since(self, mark, keep=()):
        for n in list(self.dsem.keys()):
            if n in mark or n in keep:
                continue
            key = self.dsem[n]
            self.wait('sp', (key, self.entries[key][1]))
            if n in self.sw_names:
                continue
            del self.dsem[n]
            self.free.append(key)
        return self.op('sp', None, lambda: self.nc.sync.nop())

    def last(self):
        return [(e, self.cnt[e]) for e in self.eng if self.cnt[e] > 0]


def _flat(deps):
    out = []
    if deps is None:
        return out
    if isinstance(deps, tuple) and len(deps) == 2 and isinstance(deps[0], str):
        return [deps]
    for d in deps:
        out.extend(_flat(d))
    return out


def interleave(gens, depth):
    active = []
    it = iter(gens)
    done = False
    while True:
        if len(active) < depth and not done:
            try:
                active.append(next(it))
            except StopIteration:
                done = True
        if not active:
            break
        nxt = []
        for g in active:
            try:
                next(g)
                nxt.append(g)
            except StopIteration:
                pass
        active = nxt


def build(cfg):
    S = cfg['S']
    NSEQ = cfg['NSEQ']
    NQG = cfg['NQG']
    NG = S // 512
    KB = S // 128
    NT = NSEQ * S
    NQT = sum(NQG) * 512
    NGB = sum(NQG)
    NSL = (2 * NQT + 32 * 127 + 127) // 128
    NSL = ((NSL + 7) // 8) * 8

    nc = bass.Bass("TRN2", target_bir_lowering=False)

    def din(name, shape, dt=F32):
        return nc.dram_tensor(name, list(shape), dt, kind="ExternalInput").ap()

    def dscr(name, shape, dt):
        return nc.dram_tensor(name, list(shape), dt, kind="Internal").ap()

    xs = din("xs", [NT, D])
    cvec = din("cvec", [NSEQ, D])
    rope_c = din("rope_c", [NSEQ, 32, S])
    rope_s = din("rope_s", [NSEQ, 32, S])
    w_ada = din("w_ada", [D, 6 * D])
    b_ada = din("b_ada", [6 * D])
    g_pre1 = din("g_pre1", [D]); g_post1 = din("g_post1", [D])
    g_pre2 = din("g_pre2", [D]); g_post2 = din("g_post2", [D])
    w_in = din("w_in", [D, 1440])
    g_q = din("g_q", [256]); w_uq = din("w_uq", [256, 768])
    g_kv = din("g_kv", [128]); w_ukv = din("w_ukv", [128, 1024])
    g_v_gmlp = din("g_v_gmlp", [512])
    w_spatial = din("w_spatial", [8, 128, 128]); b_spatial = din("b_spatial", [8, 128])
    g_attn_out = din("g_attn_out", [512]); g_gmlp_out = din("g_gmlp_out", [512])
    w_out = din("w_out", [D, D])
    w_rg = din("w_router_group", [D, 4]); b_rg = din("b_router_group", [4])
    w_re = din("w_router_expert", [D, 32]); b_re = din("b_router_expert", [32])
    w_gate = din("w_gate", [NE, D, 256]); w_up = din("w_up", [NE, D, 256]); w_down = din("w_down", [NE, 256, D])
    ident_in = din("ident", [128, 128])
    egrp_in = din("egrp", [8, 512])
    utri_in = din("utri", [128, 128])
    tri32_in = din("tri32", [32, 32])
    jv_in = din("jv", [128, NSL])
    pidx_in = din("pidx", [128, 1])
    zeros_in = din("zeros", [128, 8192])
    y = nc.dram_tensor("y", [NQT, D], F32, kind="ExternalOutput").ap()

    mod_d = dscr("mod_d", [NSEQ, 6 * D], F32)
    sn_d = dscr("sn_d", [NT, 512], BF16)
    cq_d = dscr("cq_d", [NSEQ * NG, 128, 1024], BF16)
    x1_d = (nc.dram_tensor("x1_d", [NQT, D], F32, kind="ExternalOutput").ap() if cfg.get("dbg") else dscr("x1_d", [NQT, D], F32))
    wgu_r = dscr("wgu_r", [NE * 128, 8 * 512], BF16)
    wd_r = dscr("wd_r", [NE * 128, 2 * D], BF16)
    h2_d = dscr("h2_d", [NQT, D], BF16)
    xs_d = dscr("xs_d", [NSL * 128, D], BF16)
    ys_d = dscr("ys_d", [NSL * 128, D], F32)
    wkv_d = dscr("wkv_d", [128, 1024], BF16)
    wsp_d = dscr("wsp_d", [128, 1024], BF16)
    wq_d = dscr("wq_d", [128, 1536], BF16)
    wqsw_d = dscr("wqsw_d", [128, 1536], BF16)
    wo_d = dscr("wo_d", [128, 8192], BF16)

    _uid = [0]

    def U(name):
        _uid[0] += 1
        return "%s_u%d" % (name, _uid[0])

    top = ExitStack()
    with top:
        dp = Dep(nc, top)

        def PE(deps, fn, sig=True): return dp.op('pe', deps, fn, sig)
        def ACT(deps, fn, sig=True): return dp.op('act', deps, fn, sig)
        def DVE(deps, fn, sig=True): return dp.op('dve', deps, fn, sig)
        def POOL(deps, fn, sig=True): return dp.op('pool', deps, fn, sig)
        def DMA(name, deps, fn, q='sp'): return dp.dma(q, name, deps, fn)

        _jt = [None]

        def SQ(deps, fn):
            tok = ACT([deps, _jt[0]], fn)
            _jt[0] = tok
            return tok

        def mmg(out, pairs, deps, sig=True):
            n = len(pairs)
            tok = None
            for i, (l, r) in enumerate(pairs):
                tok = PE(deps if i == 0 else None,
                         lambda l=l, r=r, i=i: nc.tensor.matmul(out, lhsT=l, rhs=r, start=(i == 0), stop=(i == n - 1)),
                         sig=(sig and i == n - 1))
            return tok

        def rstd_chain(ss_ap, out_ap, inv_n, deps):
            t = ACT(deps, lambda: nc.scalar.activation(out=out_ap, in_=ss_ap, func=AF.Sqrt, bias=EPS, scale=inv_n))
            return DVE(t, lambda: nc.vector.reciprocal(out=out_ap, in_=out_ap))

        bg = []
        wcast = []
        zero_tok = []
        dp.reserve('wcast')
        dp.reserve('zero')
        for e in range(NE):
            bg.append(lambda e=e: wcast.append(DMA('wcast', None, lambda: nc.gpsimd.dma_start(
                out=wgu_r[e * 128:(e + 1) * 128, :].rearrange("p (k c) -> p k c", k=8)[:, :, 0:256],
                in_=w_gate[e].rearrange("(k p) c -> p k c", p=128)), q='pool')))
            bg.append(lambda e=e: wcast.append(DMA('wcast', None, lambda: nc.gpsimd.dma_start(
                out=wgu_r[e * 128:(e + 1) * 128, :].rearrange("p (k c) -> p k c", k=8)[:, :, 256:512],
                in_=w_up[e].rearrange("(k p) c -> p k c", p=128)), q='pool')))
            bg.append(lambda e=e: wcast.append(DMA('wcast', None, lambda: nc.gpsimd.dma_start(
                out=wd_r[e * 128:(e + 1) * 128, :].rearrange("p (j c) -> p j c", j=2),
                in_=w_down[e].rearrange("(j p) c -> p j c", p=128)), q='pool')))
        nz = (NSL * 128 * D) // (128 * 8192)
        xs_flat = xs_d.rearrange("(n p r) c -> n p (r c)", p=128, r=8)
        for zi in range(nz):
            bg.append(lambda zi=zi: zero_tok.append(DMA('zero', None, lambda: nc.gpsimd.dma_start(out=xs_flat[zi], in_=zeros_in), q='pool')))

        ident_f = top.enter_context(nc.sbuf_tensor(U("ident_f"), [128, 128], F32))
        ident_b = top.enter_context(nc.sbuf_tensor(U("ident_b"), [128, 128], BF16))
        ones_f = top.enter_context(nc.sbuf_tensor(U("ones_f"), [128, 64], F32))
        t_id = DMA('c0', None, lambda: nc.sync.dma_start(out=ident_f[:], in_=ident_in))
        t_idb = DVE(t_id, lambda: nc.vector.tensor_copy(out=ident_b[:], in_=ident_f[:]))
        t_ones = DVE(None, lambda: nc.vector.memset(ones_f[:], 1.0))

        mk0 = dp.mark()
        with ExitStack() as pes:
            def sb(name, shape, dt): return pes.enter_context(nc.sbuf_tensor(U(name), shape, dt))
            def ps(name, shape, dt): return pes.enter_context(nc.psum_tensor(U(name), shape, dt))
            csT = sb("csT", [128, 8, NSEQ], F32)
            csS = sb("csS", [128, 8, NSEQ], F32)
            wblk = [sb("wblk%d" % i, [128, 8, 512], F32) for i in range(2)]
            brep = sb("brep", [NSEQ, 6 * D], F32)
            modsb = sb("modsb", [NSEQ, 6 * D], F32)
            pmod = [ps("pmod%d" % i, [128, 512], F32) for i in range(2)]
            t_c = [DMA('p0', None, lambda q=q: nc.sync.dma_start(out=csT[:, :, q], in_=cvec[q].rearrange("(k p) -> p k", p=128),
                                                                 allow_slow_non_contiguous=True)) for q in range(NSEQ)]
            t_b = DMA('p1', None, lambda: nc.sync.dma_start(out=brep[:], in_=b_ada.partition_broadcast(NSEQ)))
            t_cs = ACT(t_c, lambda: nc.scalar.activation(out=csS[:], in_=csT[:], func=AF.Silu))
            wfree = [None, None]
            pfree = [None, None]
            ev = None
            for blk in range(12):
                i = blk % 2
                t_w = DMA('pw%d' % i, wfree[i], lambda blk=blk, i=i: nc.sync.dma_start(
                    out=wblk[i][:], in_=w_ada[:, blk * 512:(blk + 1) * 512].rearrange("(k p) c -> p k c", p=128)))
                t_m = mmg(pmod[i][0:NSEQ, :], [(csS[:, k, :], wblk[i][:, k, :]) for k in range(8)], [t_w, t_cs, pfree[i]])
                wfree[i] = t_m
                ev = DVE([t_m, t_b], lambda blk=blk, i=i: nc.vector.tensor_tensor(
                    out=modsb[:, blk * 512:(blk + 1) * 512], in0=pmod[i][0:NSEQ, :],
                    in1=brep[:, blk * 512:(blk + 1) * 512], op=ALU.add))
                pfree[i] = ev
            t_mod = DMA('p2', ev, lambda: nc.sync.dma_start(out=mod_d, in_=modsb[:]))

            tmpq = sb("tmpq", [128, 2, 768], F32)
            gq = sb("gq", [128, 2], F32)
            wq_t = sb("wq_t", [128, 2, 768], BF16)
            wqsw_t = sb("wqsw_t", [128, 2, 768], BF16)
            t1 = DMA('p3', None, lambda: nc.sync.dma_start(out=tmpq[:], in_=w_uq.rearrange("(k p) c -> p k c", p=128)))
            t2 = DMA('p3', None, lambda: nc.sync.dma_start(out=gq[:], in_=g_q.rearrange("(k p) -> p k", p=128),
                                                          allow_slow_non_contiguous=True))
            tq = None
            for k in range(2):
                tq = DVE([t1, t2], lambda k=k: nc.vector.tensor_scalar(
                    out=wq_t[:, k, :], in0=tmpq[:, k, :], scalar1=gq[:, k:k + 1], scalar2=QSCALE,
                    op0=ALU.mult, op1=ALU.mult))
            tz = POOL(None, lambda: nc.gpsimd.memset(wqsw_t[:], 0.0))
            wq4 = wq_t[:].rearrange("p k (h c) -> p k h c", h=8)
            wqs4 = wqsw_t[:].rearrange("p k (h c) -> p k h c", h=8)
            ta = DVE([tq, tz], lambda: nc.vector.tensor_scalar(out=wqs4[:, :, :, 64:80], in0=wq4[:, :, :, 80:96],
                                                              scalar1=-1.0, scalar2=None, op0=ALU.mult))
            tb = DVE(None, lambda: nc.vector.tensor_copy(out=wqs4[:, :, :, 80:96], in_=wq4[:, :, :, 64:80]))
            t_wq = DMA('p4', tq, lambda: nc.sync.dma_start(out=wq_d, in_=wq_t[:].rearrange("p k c -> p (k c)")))
            t_wqsw = DMA('p4', [ta, tb], lambda: nc.sync.dma_start(out=wqsw_d, in_=wqsw_t[:].rearrange("p k c -> p (k c)")))

            tmpkv = sb("tmpkv", [128, 1024], F32)
            gkv = sb("gkv", [128, 1], F32)
            wkv_t = sb("wkv_t", [128, 1024], BF16)
            t1 = DMA('p5', None, lambda: nc.sync.dma_start(out=tmpkv[:], in_=w_ukv))
            t2 = DMA('p5', None, lambda: nc.sync.dma_start(out=gkv[:], in_=g_kv.rearrange("(p o) -> p o", o=1)))
            tk = DVE([t1, t2], lambda: nc.vector.tensor_scalar(out=wkv_t[:], in0=tmpkv[:], scalar1=gkv[:, 0:1],
                                                              scalar2=None, op0=ALU.mult))
            t_wkv = DMA('p6', tk, lambda: nc.sync.dma_start(out=wkv_d, in_=wkv_t[:]))

            tmpo = sb("tmpo", [128, 8, 1024], F32)
            gcat = sb("gcat", [128, 8], F32)
            wo_t = sb("wo_t", [128, 8, 1024], BF16)
            t1 = DMA('p7', None, lambda: nc.sync.dma_start(out=tmpo[:], in_=w_out.rearrange("(k p) c -> p k c", p=128)))
            t2 = DMA('p7', None, lambda: nc.sync.dma_start(out=gcat[:, 0:4], in_=g_attn_out.rearrange("(k p) -> p k", p=128),
                                                          allow_slow_non_contiguous=True))
            t3 = DMA('p7', None, lambda: nc.sync.dma_start(out=gcat[:, 4:8], in_=g_gmlp_out.rearrange("(k p) -> p k", p=128),
                                                          allow_slow_non_contiguous=True))
            two = None
            for k in range(8):
                two = DVE([t1, t2, t3], lambda k=k: nc.vector.tensor_scalar(
                    out=wo_t[:, k, :], in0=tmpo[:, k, :], scalar1=gcat[:, k:k + 1], scalar2=None, op0=ALU.mult))
            t_wo = DMA('p8', two, lambda: nc.sync.dma_start(out=wo_d, in_=wo_t[:].rearrange("p k c -> p (k c)")))

            tmps = sb("tmps", [128, 8, 128], F32)
            wsp_t = sb("wsp_t", [128, 8, 128], BF16)
            psp = ps("psp", [128, 1024], F32)
            t1 = DMA('p9', None, lambda: nc.sync.dma_start(out=tmps[:], in_=w_spatial.rearrange("g t s -> t g s")))
            tt = None
            for g in range(8):
                tt = PE([t1, t_id], lambda g=g: nc.tensor.transpose(psp[:, g * 128:(g + 1) * 128], tmps[:, g, :], ident_f[:]),
                        sig=(g == 7))
            tc_ = DVE(tt, lambda: nc.vector.tensor_copy(out=wsp_t[:].rearrange("p g t -> p (g t)"), in_=psp[:]))
            t_wsp = DMA('p10', tc_, lambda: nc.sync.dma_start(out=wsp_d, in_=wsp_t[:].rearrange("p g t -> p (g t)")))
            prep_done = [t_mod, t_wq, t_wqsw, t_wkv, t_wo, t_wsp]
            dp.retire_since(mk0, keep=('wcast', 'zero', 'c0'))
            prep_bar = dp.last()

        with ExitStack() as aes:
            def sbA(name, shape, dt): return aes.enter_context(nc.sbuf_tensor(U(name), shape, dt))
            KT = sbA("KT", [128, 8, S], BF16)
            VA = sbA("VA", [128, KB, 8, 65], BF16)
            geff1 = sbA("geff1", [128, D], F32)
            sh1r = sbA("sh1r", [128, D], F32)
            gvec1 = sbA("gvec1", [128, D], F32)
            gvrep = sbA("gvrep", [128, 512], F32)
            PA = aes.enter_context(nc.psum_tensor(U("PA"), [128, 1024], F32))
            PB = aes.enter_context(nc.psum_tensor(U("PB"), [128, 1024], F32))
            PC = aes.enter_context(nc.psum_tensor(U("PC"), [128, 1024], F32))
            PD = aes.enter_context(nc.psum_tensor(U("PD"), [128, 1024], F32))

            t_va1 = POOL(prep_bar, lambda: nc.gpsimd.memset(VA[:, :, :, 64:65], 1.0))
            t_gv = DMA('a0', prep_bar, lambda: nc.sync.dma_start(out=gvrep[:], in_=g_v_gmlp.partition_broadcast(128)))
            seq_bar = [prep_bar, prep_done, t_va1, t_gv, t_idb, t_ones]
            qbase = 0
            for s in range(NSEQ):
                mk1 = dp.mark()
                with ExitStack() as p1:
                    def sb1(name, shape, dt): return p1.enter_context(nc.sbuf_tensor(U(name), shape, dt))
                    wAs = sb1("wAs", [128, 8, 384], BF16)
                    wAuv = sb1("wAuv", [128, 8, 1024], BF16)
                    wAkr = sb1("wAkr", [128, 8, 96], BF16)
                    wAks = sb1("wAks", [128, 8, 96], BF16)
                    wkv = sb1("wkv", [128, 1024], BF16)
                    wsp = sb1("wsp", [128, 8, 128], BF16)
                    bsp = sb1("bsp", [8, 128], F32)
                    egrp = sb1("egrp", [8, 512], F32)
                    vt = [sb1("vt%d" % i, [128, D], F32) for i in range(2)]
                    xt = [sb1("xt%d" % i, [128, D], F32) for i in range(2)]
                    junk = sb1("junk", [128, D], BF16)
                    hm = sb1("hm", [128, D], F32)
                    hb = [sb1("hb%d" % i, [128, D], BF16) for i in range(2)]
                    hT = sb1("hT", [128, 8, 512], BF16)
                    zsb = [sb1("zsb%d" % i, [128, 384], BF16) for i in range(2)]
                    cqnT = [sb1("cqnT%d" % i, [128, 2, 512], BF16) for i in range(2)]
                    ckvnT = [sb1("ckvnT%d" % i, [128, 512], BF16) for i in range(2)]
                    gu = [sb1("gu%d" % i, [128, 512], BF16) for i in range(2)]
                    gv = [sb1("gv%d" % i, [128, 512], F32) for i in range(2)]
                    zraw = [sb1("zraw%d" % i, [128, 384], F32) for i in range(2)]
                    vn = [sb1("vn%d" % i, [128, 512], BF16) for i in range(2)]
                    sraw = [sb1("sraw%d" % i, [128, 512], F32) for i in range(2)]
                    sn = [sb1("sn%d" % i, [128, 512], BF16) for i in range(2)]
                    stt = [sb1("stt%d" % i, [128, 16], F32) for i in range(2)]
                    Ctt = [sb1("Ctt%d" % i, [128, 128], F32) for i in range(2)]
                    Stt = [sb1("Stt%d" % i, [128, 128], F32) for i in range(2)]
                    kt1 = [sb1("kt1_%d" % i, [128, 128], F32) for i in range(2)]
                    kt2 = [sb1("kt2_%d" % i, [128, 128], F32) for i in range(2)]
                    krr = [sb1("krr%d" % i, [128, 128], BF16) for i in range(2)]

                    pT = PA[:, 0:512].bitcast(BF16)
                    pT2 = PA[:, 512:1024].bitcast(BF16)
                    pzs = PB[:, 0:384]
                    pss = PB[:, 512:1024]
                    pu = PC[:, 0:512]
                    pv = PC[:, 512:1024]
                    pkr = PD[:, 0:512]
                    pks = PD[:, 512:1024]

                    sb_ = seq_bar
                    wl = []
                    wl.append(DMA('a1', sb_, lambda: nc.gpsimd.dma_start(
                        out=wAs[:], in_=w_in[:, 0:384].rearrange("(k p) c -> p k c", p=128)), q='pool'))
                    wl.append(DMA('a1', sb_, lambda: nc.gpsimd.dma_start(
                        out=wAuv[:], in_=w_in[:, 416:1440].rearrange("(k p) c -> p k c", p=128)), q='pool'))
                    tz1 = POOL(sb_, lambda: nc.gpsimd.memset(wAkr[:], 0.0))
                    tz2 = POOL(sb_, lambda: nc.gpsimd.memset(wAks[:], 0.0))
                    wl.append(DMA('a1', [tz1], lambda: nc.gpsimd.dma_start(
                        out=wAkr[:, :, 64:96], in_=w_in[:, 384:416].rearrange("(k p) c -> p k c", p=128)), q='pool'))
                    tn = DMA('a2', [tz2], lambda: nc.gpsimd.dma_start(
                        out=wAks[:, :, 64:80], in_=w_in[:, 400:416].rearrange("(k p) c -> p k c", p=128)), q='pool')
                    wl.append(DMA('a1', [tz2], lambda: nc.gpsimd.dma_start(
                        out=wAks[:, :, 80:96], in_=w_in[:, 384:400].rearrange("(k p) c -> p k c", p=128)), q='pool'))
                    wl.append(POOL(tn, lambda: nc.gpsimd.tensor_scalar(out=wAks[:, :, 64:80], in0=wAks[:, :, 64:80],
                                                                      scalar1=-1.0, scalar2=None, op0=ALU.mult)))
                    wl.append(DMA('a3', sb_, lambda: nc.sync.dma_start(out=wkv[:], in_=wkv_d)))
                    wl.append(DMA('a3', sb_, lambda: nc.sync.dma_start(out=wsp[:].rearrange("p g t -> p (g t)"), in_=wsp_d)))
                    wl.append(DMA('a3', sb_, lambda: nc.sync.dma_start(out=bsp[:], in_=b_spatial)))
                    wl.append(DMA('a3', sb_, lambda: nc.sync.dma_start(out=egrp[:], in_=egrp_in)))
                    l1 = DMA('a4_0', sb_, lambda: nc.sync.dma_start(out=vt[0][:], in_=mod_d[s, 1024:2048].partition_broadcast(128)))
                    l2 = DMA('a4_1', sb_, lambda: nc.sync.dma_start(out=vt[1][:], in_=g_pre1.partition_broadcast(128)))
                    l3 = DMA('a4_2', sb_, lambda: nc.sync.dma_start(out=sh1r[:], in_=mod_d[s, 0:1024].partition_broadcast(128)))
                    tg = DVE([l1, l2], lambda: nc.vector.scalar_tensor_tensor(out=geff1[:], in0=vt[0][:], scalar=1.0, in1=vt[1][:],
                                                                               op0=ALU.add, op1=ALU.mult))
                    l4 = DMA('a4_3', [tg], lambda: nc.sync.dma_start(out=vt[0][:], in_=mod_d[s, 2048:3072].partition_broadcast(128)))
                    l5 = DMA('a4_4', [tg], lambda: nc.sync.dma_start(out=vt[1][:], in_=g_post1.partition_broadcast(128)))
                    tg2 = DVE([l4, l5], lambda: nc.vector.tensor_tensor(out=gvec1[:], in0=vt[0][:], in1=vt[1][:], op=ALU.mult))
                    ready = [wl, l3, tg, tg2]

                    xt_free = [None, None]; hb_free = [None, None]
                    hT_free = [None] * 4
                    zraw_free = [None, None]; zsb_free = [None, None]; cq_free = [None, None]; ckv_free = [None, None]
                    gu_free = [None, None]; gv_free = [None, None]; vn_free = [None, None]; sn_free = [None, None]
                    sraw_free = [None, None]; ct_free = [None, None]; kt_free = [None, None]; krr_free = [None, None]
                    P = dict(hm_free=None, pT_free=None, pT2_free=None, pzs_free=None, pu_free=None, pv_free=None, pss_free=None,
                             pkr_free=None, pks_free=None)
                    grp = {}

                    def p1_tile(g, t):
                        gi = g % 2
                        ti = t % 2
                        if t == 0:
                            grp[g] = dict(cq_w=[], ckv_w=[])
                        G = grp[g]
                        tok0 = s * S + g * 512 + t * 128
                        ts_ = slice(t * 128, (t + 1) * 128)
                        gts = slice(g * 512 + t * 128, g * 512 + (t + 1) * 128)
                        st_ = stt[ti]
                        lx = DMA('x%d' % ti, [xt_free[ti], ready], lambda: nc.sync.dma_start(out=xt[ti][:], in_=xs[tok0:tok0 + 128, :]))
                        lc = DMA('rc%d' % ti, [ct_free[ti], ready], lambda: nc.sync.dma_start(out=Ctt[ti][64:96, :], in_=rope_c[s, :, gts]))
                        ls = DMA('rs%d' % ti, [ct_free[ti], ready], lambda: nc.sync.dma_start(out=Stt[ti][64:96, :], in_=rope_s[s, :, gts]))
                        a1 = SQ(lx, lambda: nc.scalar.activation(out=junk[:], in_=xt[ti][:], func=AF.Square, accum_out=st_[:, 0:1]))
                        yield
                        r1a = ACT(a1, lambda: nc.scalar.activation(out=st_[:, 1:2], in_=st_[:, 0:1], func=AF.Sqrt, bias=EPS, scale=1.0 / D))
                        yield
                        r1 = DVE(r1a, lambda: nc.vector.reciprocal(out=st_[:, 1:2], in_=st_[:, 1:2]))
                        d1 = DVE([r1, P['hm_free']], lambda: nc.vector.scalar_tensor_tensor(
                            out=hm[:], in0=xt[ti][:], scalar=st_[:, 1:2], in1=geff1[:], op0=ALU.mult, op1=ALU.mult))
                        xt_free[ti] = d1
                        p1_ = POOL([d1, hb_free[ti]], lambda: nc.gpsimd.tensor_tensor(out=hb[ti][:], in0=hm[:], in1=sh1r[:], op=ALU.add))
                        P['hm_free'] = p1_
                        yield
                        tp = None
                        for k in range(8):
                            tp = PE([p1_, P['pT_free']] if k == 0 else None,
                                    lambda k=k: nc.tensor.transpose(pT[:, k * 128:(k + 1) * 128], hb[ti][:, k * 128:(k + 1) * 128], ident_b[:]),
                                    sig=(k == 7))
                        hb_free[ti] = tp
                        yield
                        ev = ACT([tp, hT_free[t]], lambda: nc.scalar.copy(out=hT[:, :, ts_], in_=pT.rearrange("p (k c) -> p k c", k=8)))
                        P['pT_free'] = ev
                        yield
                        m_zs = mmg(pzs, [(hT[:, k, ts_], wAs[:, k, :]) for k in range(8)], [ev, P['pzs_free']])
                        m_u = mmg(pu, [(hT[:, k, ts_], wAuv[:, k, 0:512]) for k in range(8)], [P['pu_free']])
                        m_v = mmg(pv, [(hT[:, k, ts_], wAuv[:, k, 512:1024]) for k in range(8)], [P['pv_free']])
                        m_kr = mmg(pkr[0:96, 0:128], [(wAkr[:, k, :], hT[:, k, ts_]) for k in range(8)], [P['pkr_free']])
                        m_ks = mmg(pks[0:96, 0:128], [(wAks[:, k, :], hT[:, k, ts_]) for k in range(8)], [P['pks_free']])
                        hT_free[t] = m_ks
                        yield
                        zr = DVE([m_zs, zraw_free[ti]], lambda: nc.vector.tensor_copy(out=zraw[ti][:], in_=pzs))
                        P['pzs_free'] = zr
                        g1 = ACT([m_u, gu_free[ti]], lambda: nc.scalar.activation(out=gu[ti][:], in_=pu, func=AF.Gelu_apprx_tanh))
                        P['pu_free'] = g1
                        g2 = ACT([m_v, gv_free[ti]], lambda: nc.scalar.activation(out=gv[ti][:], in_=pv, func=AF.Gelu_apprx_tanh))
                        P['pv_free'] = g2
                        k1 = DVE([m_kr, lc, kt_free[ti]], lambda: nc.vector.tensor_tensor(out=kt1[ti][64:96, :], in0=pkr[64:96, 0:128], in1=Ctt[ti][64:96, :], op=ALU.mult))
                        P['pkr_free'] = k1
                        k2 = DVE([m_ks, ls], lambda: nc.vector.tensor_tensor(out=kt2[ti][64:96, :], in0=pks[64:96, 0:128], in1=Stt[ti][64:96, :], op=ALU.mult))
                        P['pks_free'] = k2
                        ct_free[ti] = k2
                        yield
                        a2 = SQ(zr, lambda: nc.scalar.activation(out=junk[:, 0:256], in_=zraw[ti][:, 0:256], func=AF.Square, accum_out=st_[:, 2:3]))
                        a3 = SQ(None, lambda: nc.scalar.activation(out=junk[:, 256:384], in_=zraw[ti][:, 256:384], func=AF.Square, accum_out=st_[:, 3:4]))
                        g3 = SQ(g2, lambda: nc.scalar.activation(out=junk[:, 0:512], in_=gv[ti][:], func=AF.Square, accum_out=st_[:, 6:7]))
                        k3 = DVE([k1, k2, krr_free[ti]], lambda: nc.vector.tensor_tensor(out=krr[ti][64:96, :], in0=kt1[ti][64:96, :], in1=kt2[ti][64:96, :], op=ALU.add))
                        kt_free[ti] = k3
                        kc = None
                        for h in range(8):
                            kc = POOL(k3, lambda h=h: nc.gpsimd.tensor_copy(out=KT[64:96, h, gts], in_=krr[ti][64:96, :]))
                        krr_free[ti] = kc
                        yield
                        q1 = ACT([a2, a3], lambda: nc.scalar.activation(out=st_[:, 4:5], in_=st_[:, 2:3], func=AF.Sqrt, bias=EPS, scale=1.0 / 256))
                        q2 = ACT(None, lambda: nc.scalar.activation(out=st_[:, 5:6], in_=st_[:, 3:4], func=AF.Sqrt, bias=EPS, scale=1.0 / 128))
                        q3 = ACT(g3, lambda: nc.scalar.activation(out=st_[:, 7:8], in_=st_[:, 6:7], func=AF.Sqrt, bias=EPS, scale=1.0 / 512))
                        yield
                        r2 = DVE([q1, q2], lambda: nc.vector.reciprocal(out=st_[:, 4:6], in_=st_[:, 4:6]))
                        r4 = DVE(q3, lambda: nc.vector.reciprocal(out=st_[:, 7:8], in_=st_[:, 7:8]))
                        d2 = DVE([r4, vn_free[ti]], lambda: nc.vector.scalar_tensor_tensor(
                            out=vn[ti][:], in0=gv[ti][:], scalar=st_[:, 7:8], in1=gvrep[:], op0=ALU.mult, op1=ALU.mult))
                        gv_free[ti] = d2
                        c1 = ACT([r2, zsb_free[ti]], lambda: nc.scalar.activation(
                            out=zsb[ti][:, 0:256], in_=zraw[ti][:, 0:256], func=AF.Copy, scale=st_[:, 4:5]))
                        c2 = ACT(None, lambda: nc.scalar.activation(
                            out=zsb[ti][:, 256:384], in_=zraw[ti][:, 256:384], func=AF.Copy, scale=st_[:, 5:6]))
                        zraw_free[ti] = c2
                        yield
                        tp2 = None
                        for k in range(3):
                            tp2 = PE([c1, c2, P['pT2_free']] if k == 0 else None,
                                     lambda k=k: nc.tensor.transpose(pT2[:, k * 128:(k + 1) * 128], zsb[ti][:, k * 128:(k + 1) * 128], ident_b[:]),
                                     sig=(k == 2))
                        zsb_free[ti] = tp2
                        PE([P['pss_free'], ready], lambda: nc.tensor.matmul(pss, lhsT=bsp[:, :], rhs=egrp[:, :], start=True, stop=False), sig=False)
                        m_s = None
                        for gg in range(8):
                            m_s = PE(d2 if gg == 0 else None,
                                     lambda gg=gg: nc.tensor.matmul(pss[:, gg * 64:(gg + 1) * 64], lhsT=wsp[:, gg, :],
                                                                    rhs=vn[ti][:, gg * 64:(gg + 1) * 64], start=False, stop=(gg == 7)),
                                     sig=(gg == 7))
                        vn_free[ti] = m_s
                        yield
                        e1 = DVE([tp2, cq_free[gi] if t == 0 else None], lambda: nc.vector.tensor_copy(
                            out=cqnT[gi][:, :, ts_], in_=pT2[:, 0:256].rearrange("p (k c) -> p k c", k=2)))
                        e2 = DVE([ckv_free[gi] if t == 0 else None], lambda: nc.vector.tensor_copy(
                            out=ckvnT[gi][:, ts_], in_=pT2[:, 256:384]))
                        P['pT2_free'] = e2
                        G['cq_w'].append(e1)
                        G['ckv_w'].append(e2)
                        d3 = DVE([m_s, g1, sraw_free[ti]], lambda: nc.vector.tensor_tensor(out=sraw[ti][:], in0=gu[ti][:], in1=pss, op=ALU.mult))
                        P['pss_free'] = d3
                        gu_free[ti] = d3
                        yield
                        a4 = SQ(d3, lambda: nc.scalar.activation(out=junk[:, 0:512], in_=sraw[ti][:], func=AF.Square, accum_out=st_[:, 8:9]))
                        yield
                        q4 = ACT(a4, lambda: nc.scalar.activation(out=st_[:, 9:10], in_=st_[:, 8:9], func=AF.Sqrt, bias=EPS, scale=1.0 / 512))
                        yield
                        r5 = DVE(q4, lambda: nc.vector.reciprocal(out=st_[:, 9:10], in_=st_[:, 9:10]))
                        yield
                        c3 = ACT([r5, sn_free[ti]], lambda: nc.scalar.activation(out=sn[ti][:], in_=sraw[ti][:], func=AF.Copy, scale=st_[:, 9:10]))
                        sraw_free[ti] = c3
                        sn_free[ti] = DMA('sn%d' % ti, c3, lambda: nc.sync.dma_start(out=sn_d[tok0:tok0 + 128, :], in_=sn[ti][:]))
                        if t != 3:
                            return
                        yield
                        gs = slice(g * 512, (g + 1) * 512)
                        cq_free[gi] = DMA('cq%d' % gi, G['cq_w'], lambda: nc.sync.dma_start(
                            out=cq_d[s * NG + g], in_=cqnT[gi][:].rearrange("p k c -> p (k c)")))
                        bank_free = [P['pkr_free'], P['pks_free']]
                        banks = [pkr, pks]
                        for h in range(8):
                            bi = h % 2
                            mk = mmg(banks[bi][0:64, :], [(wkv[:, h * 128:h * 128 + 64], ckvnT[gi][:, :])], [G['ckv_w'], bank_free[bi]])
                            if h % 2 == 0:
                                bank_free[bi] = ACT(mk, lambda h=h, bi=bi: nc.scalar.copy(out=KT[0:64, h, gs], in_=banks[bi][0:64, :]))
                            else:
                                bank_free[bi] = DVE(mk, lambda h=h, bi=bi: nc.vector.tensor_copy(out=KT[0:64, h, gs], in_=banks[bi][0:64, :]))
                        wkv3 = wkv[:].rearrange("p (h c) -> p h c", h=8)[:, :, 64:128]
                        mv = None
                        for tt in range(4):
                            bi = tt % 2
                            kb = g * 4 + tt
                            mv = mmg(banks[bi][:, :].rearrange("p (h c) -> p h c", h=8), [(ckvnT[gi][:, tt * 128:(tt + 1) * 128], wkv3)], [bank_free[bi]])
                            bank_free[bi] = DVE(mv, lambda kb=kb, bi=bi: nc.vector.tensor_copy(
                                out=VA[:, kb, :, 0:64], in_=banks[bi][:, :].rearrange("p (h c) -> p h c", h=8)))
                        ckv_free[gi] = mv
                        P['pkr_free'] = bank_free[0]
                        P['pks_free'] = bank_free[1]

                    interleave((p1_tile(g, t) for g in range(NG) for t in range(4)), 2)
                    dp.retire_since(mk1)
                    p1_bar = dp.last() + [sn_free, cq_free]

                mk2 = dp.mark()
                with ExitStack() as p2:
                    def sb2(name, shape, dt): return p2.enter_context(nc.sbuf_tensor(U(name), shape, dt))
                    wq = sb2("wq", [128, 2, 768], BF16)
                    wqs = sb2("wqs", [128, 2, 768], BF16)
                    wo = sb2("wo", [128, 8, 1024], BF16)
                    cqT = [sb2("cqT%d" % i, [128, 2, 512], BF16) for i in range(2)]
                    Ct = sb2("Ct2", [128, 512], F32)
                    St = sb2("St2", [128, 512], F32)
                    qt1 = [sb2("qt1_%d" % i, [128, 512], F32) for i in range(2)]
                    qt2 = [sb2("qt2_%d" % i, [128, 512], F32) for i in range(2)]
                    qt_free = [None, None]
                    QT = sb2("QT", [128, 8, 512], BF16)
                    pTs = [sb2("pTs%d" % i, [128, 1024], BF16) for i in range(3)]
                    osb = [sb2("osb%d" % i, [128, 512], F32) for i in range(2)]
                    rcp = [sb2("rcp%d" % i, [128, 4], F32) for i in range(2)]
                    a_tok = sb2("a_tok", [128, 4, 512], BF16)
                    merged = [sb2("merged%d" % i, [128, D], BF16) for i in range(4)]
                    mT = [sb2("mT%d" % i, [128, 8, 128], BF16) for i in range(2)]
                    otmp = sb2("otmp", [128, D], F32)
                    xr = [sb2("xr%d" % i, [128, D], F32) for i in range(2)]
                    x1 = [sb2("x1_%d" % i, [128, D], F32) for i in range(2)]
                    junk = sb2("junk2", [128, D], BF16)
                    stt = [sb2("stq%d" % i, [128, 16], F32) for i in range(2)]

                    po = [PC[:, 0:512], PD[:, 0:512]]
                    ptr3 = PC[:, 512:512 + 260].rearrange("p (t c) -> p t c", t=4)
                    pmT = PC[:, 512:1024].bitcast(BF16)
                    scT = [PA, PB]

                    wl2 = [DMA('b1', p1_bar, lambda: nc.sync.dma_start(out=wq[:].rearrange("p k c -> p (k c)"), in_=wq_d)),
                           DMA('b1', p1_bar, lambda: nc.sync.dma_start(out=wqs[:].rearrange("p k c -> p (k c)"), in_=wqsw_d)),
                           DMA('b1', p1_bar, lambda: nc.sync.dma_start(out=wo[:].rearrange("p k c -> p (k c)"), in_=wo_d))]
                    ready2 = [p1_bar, wl2]
                    cq_free2 = [None, None]; rope_free = None; QT_free = []
                    sc_free = [None, None]; pTs_free = [None, None, None]; po_free = [None, None]; osb_free = [None, None]
                    rinv_free = [None]; prb_free = None; aT_free = [None, None]; pa_free = []
                    merged_free = [None] * 4; mT_free = [None, None]; pmT_free = None
                    otmp_free = None; xr_free = [None, None]; x1_free = [None, None]
                    step = 0
                    tcount = 0
                    for qg in range(NQG[s]):
                        gi = qg % 2
                        gs = slice(qg * 512, (qg + 1) * 512)
                        lq = DMA('cql%d' % gi, [cq_free2[gi], ready2], lambda gi=gi, qg=qg: nc.sync.dma_start(
                            out=cqT[gi][:].rearrange("p k c -> p (k c)"), in_=cq_d[s * NG + qg]))
                        lr1 = DMA('rp2c', [rope_free, ready2], lambda gs=gs: nc.sync.dma_start(out=Ct[64:96, :], in_=rope_c[s, :, gs]))
                        lr2 = DMA('rp2s', [rope_free, ready2], lambda gs=gs: nc.sync.dma_start(out=St[64:96, :], in_=rope_s[s, :, gs]))
                        pre_ld = []
                        for t in range(4):
                            tok0_ = s * S + qg * 512 + t * 128
                            l_sn = DMA('snl%d' % t, [merged_free[t], ready2], lambda t=t, tok0_=tok0_: nc.sync.dma_start(
                                out=merged[t][:, 512:1024], in_=sn_d[tok0_:tok0_ + 128, :]))
                            pre_ld.append(l_sn)
                        wq4 = wq[:].rearrange("p k (h c) -> p k h c", h=8)
                        wqs4 = wqs[:].rearrange("p k (h c) -> p k h c", h=8)
                        QT_w = []
                        Qs = dict(mqs=None, qd=None)

                        def q_head(h):
                            T = scT[h % 2]
                            hi = h % 2
                            mq = mmg(T[0:96, 0:512], [(wq4[:, k, h, :], cqT[gi][:, k, :]) for k in range(2)], [lq, sc_free[h % 2]])
                            mqs = mmg(T[0:96, 512:1024], [(wqs4[:, k, h, :], cqT[gi][:, k, :]) for k in range(2)], None)
                            Qs['mqs'] = mqs
                            yield
                            c0 = ACT([mq, QT_free if h == 0 else None], lambda: nc.scalar.copy(out=QT[0:64, h, :], in_=T[0:64, 0:512]))
                            q1 = DVE([mq, lr1, qt_free[hi]], lambda: nc.vector.tensor_tensor(out=qt1[hi][64:96, :], in0=T[64:96, 0:512], in1=Ct[64:96, :], op=ALU.mult))
                            q2 = DVE([mqs, lr2], lambda: nc.vector.tensor_tensor(out=qt2[hi][64:96, :], in0=T[64:96, 512:1024], in1=St[64:96, :], op=ALU.mult))
                            sc_free[h % 2] = [c0, q2]
                            yield
                            qd = DVE([q1, q2, QT_free if h == 0 else None], lambda: nc.vector.tensor_tensor(
                                out=QT[64:96, h, :], in0=qt1[hi][64:96, :], in1=qt2[hi][64:96, :], op=ALU.add))
                            qt_free[hi] = qd
                            Qs['qd'] = qd
                            QT_w.extend([c0, qd])
                        interleave((q_head(h) for h in range(8)), 2)
                        mqs = Qs['mqs']
                        qd = Qs['qd']
                        cq_free2[gi] = mqs
                        rope_free = qd
                        NP = KB // 2
                        steps = [(h, j) for h in range(8) for j in range(NP)]
                        qk_tok = {}

                        def emit_qk(idx):
                            h, j = steps[idx]
                            T = scT[idx % 2]
                            tk = None
                            for u in range(2):
                                kb = 2 * j + u
                                tk = PE([sc_free[idx % 2], QT_w] if u == 0 else None,
                                        lambda h=h, kb=kb, u=u, T=T: nc.tensor.matmul(
                                            T[:, u * 512:(u + 1) * 512], lhsT=KT[0:96, h, kb * 128:(kb + 1) * 128], rhs=QT[0:96, h, :],
                                            start=True, stop=True), sig=(u == 1))
                            qk_tok[idx] = tk

                        emit_qk(0)
                        pa_w = []
                        QT_readers = []
                        pending = []
                        A = dict(prb_free=prb_free)
                        for idx, (h, j) in enumerate(steps):
                            if idx + 1 < len(steps):
                                emit_qk(idx + 1)
                            T = scT[idx % 2]
                            sl = step % 3
                            step += 1
                            ex = ACT([qk_tok[idx], pTs_free[sl]], lambda T=T, sl=sl: nc.scalar.activation(out=pTs[sl][:], in_=T[:, :], func=AF.Exp))
                            sc_free[idx % 2] = ex
                            pvt = None
                            for u in range(2):
                                kb = 2 * j + u
                                pvt = PE([ex, po_free[h % 2] if (j == 0 and u == 0) else None],
                                         lambda h=h, kb=kb, u=u, sl=sl: nc.tensor.matmul(
                                             po[h % 2][0:65, :], lhsT=VA[:, kb, h, :], rhs=pTs[sl][:, u * 512:(u + 1) * 512],
                                             start=(kb == 0), stop=(kb == KB - 1)), sig=(u == 1))
                            pTs_free[sl] = pvt
                            for pend in list(pending):
                                pend[0] -= 1
                                if pend[0] <= 0:
                                    pend[1]()
                                    pending.remove(pend)
                            if j == min(1, NP - 1) and bg:
                                bg.pop(0)()
                            if j == NP - 1:
                                for pend in list(pending):
                                    pend[1]()
                                    pending.remove(pend)
                                oi = h % 2
                                QT_readers.append(pvt)
                                o1 = DVE([pvt, osb_free[oi]], lambda oi=oi: nc.vector.tensor_copy(out=osb[oi][0:65, :], in_=po[oi][0:65, :]))
                                po_free[oi] = o1
                                hs = dict(o1=o1, oi=oi, h=h)

                                def part_a(hs=hs):
                                    oi = hs['oi']
                                    o3 = None
                                    for t in range(4):
                                        o3 = PE([hs['o1'], A['prb_free']] if t == 0 else None,
                                                lambda t=t, oi=oi: nc.tensor.transpose(ptr3[:, t, :], osb[oi][0:65, t * 128:(t + 1) * 128], ident_f[0:65, 0:65]),
                                                sig=(t == 3))
                                    osb_free[oi] = o3
                                    hs['o3'] = o3

                                def part_b(hs=hs):
                                    oi = hs['oi']; h = hs['h']
                                    rv = DVE([hs['o3']], lambda oi=oi: nc.vector.reciprocal(out=rcp[oi][:, :], in_=ptr3[:, :, 64]))
                                    o4 = DVE([rv, pa_free if h == 0 else None], lambda oi=oi, h=h: nc.vector.tensor_tensor(
                                        out=a_tok[:, :, h * 64:(h + 1) * 64], in0=ptr3[:, :, 0:64],
                                        in1=rcp[oi][:, :].unsqueeze(2).to_broadcast([128, 4, 64]), op=ALU.mult))
                                    A['prb_free'] = o4
                                    pa_w.append(o4)
                                pending.append([2, part_a])
                                pending.append([3, part_b])
                        for pend in list(pending):
                            pend[1]()
                            pending.remove(pend)
                        prb_free = A['prb_free']
                        QT_free = QT_readers
                        pa_r = []
                        M = dict(pmT_free=pmT_free, prb_free=prb_free, otmp_free=otmp_free)

                        def mg_tile(t, ti):
                            tok0 = s * S + qg * 512 + t * 128
                            otok0 = qbase + qg * 512 + t * 128
                            st_ = stt[ti]
                            lsn = pre_ld[t]
                            lxr = DMA('xr%d' % ti, [xr_free[ti], ready2], lambda: nc.sync.dma_start(
                                out=xr[ti][:], in_=xs[tok0:tok0 + 128, :]))
                            a1 = SQ(pa_w, lambda: nc.scalar.activation(out=junk[:, 0:512], in_=a_tok[:, t, :], func=AF.Square,
                                                                        accum_out=st_[:, 0:1]))
                            yield
                            r1a = ACT(a1, lambda: nc.scalar.activation(out=st_[:, 1:2], in_=st_[:, 0:1], func=AF.Sqrt, bias=EPS, scale=1.0 / 512))
                            yield
                            r1 = DVE(r1a, lambda: nc.vector.reciprocal(out=st_[:, 1:2], in_=st_[:, 1:2]))
                            c1 = ACT([r1, merged_free[t]], lambda: nc.scalar.activation(
                                out=merged[t][:, 0:512], in_=a_tok[:, t, :], func=AF.Copy, scale=st_[:, 1:2]))
                            pa_r.append(c1)
                            yield
                            tp = None
                            for k in range(8):
                                tp = PE([c1, lsn, M['pmT_free'], M['prb_free']] if k == 0 else None,
                                        lambda k=k: nc.tensor.transpose(pmT[:, k * 128:(k + 1) * 128], merged[t][:, k * 128:(k + 1) * 128], ident_b[:]),
                                        sig=(k == 7))
                            merged_free[t] = tp
                            yield
                            ev = DVE([tp, mT_free[ti]], lambda: nc.vector.tensor_copy(out=mT[ti][:].rearrange("p k c -> p (k c)"), in_=pmT))
                            M['pmT_free'] = ev
                            M['prb_free'] = ev
                            yield
                            T = scT[t % 2]
                            mo1 = mmg(T[:, 0:512], [(mT[ti][:, k, :], wo[:, k, 0:512]) for k in range(8)], [ev, sc_free[t % 2]])
                            mo2 = mmg(T[:, 512:1024], [(mT[ti][:, k, :], wo[:, k, 512:1024]) for k in range(8)], None)
                            mT_free[ti] = mo2
                            yield
                            a2 = SQ(mo2, lambda: nc.scalar.activation(out=junk[:], in_=T[:, :], func=AF.Square, accum_out=st_[:, 2:3]))
                            yield
                            r2a = ACT(a2, lambda: nc.scalar.activation(out=st_[:, 3:4], in_=st_[:, 2:3], func=AF.Sqrt, bias=EPS, scale=1.0 / D))
                            yield
                            r2 = DVE(r2a, lambda: nc.vector.reciprocal(out=st_[:, 3:4], in_=st_[:, 3:4]))
                            d1 = DVE([r2, M['otmp_free']], lambda: nc.vector.scalar_tensor_tensor(
                                out=otmp[:], in0=T[:, :], scalar=st_[:, 3:4], in1=gvec1[:], op0=ALU.mult, op1=ALU.mult))
                            sc_free[t % 2] = d1
                            pp = DVE([d1, lxr, x1_free[ti]], lambda: nc.vector.tensor_tensor(out=x1[ti][:], in0=otmp[:], in1=xr[ti][:], op=ALU.add))
                            M['otmp_free'] = pp
                            xr_free[ti] = pp
                            x1_free[ti] = DMA('x1s%d' % ti, pp, lambda: nc.sync.dma_start(
                                out=x1_d[otok0:otok0 + 128, :], in_=x1[ti][:]))
                        interleave((mg_tile(t, (tcount + t) % 2) for t in range(4)), 2)
                        tcount += 4
                        pmT_free = M['pmT_free']; prb_free = M['prb_free']; otmp_free = M['otmp_free']
                        pa_free = pa_r
                    dp.retire_since(mk2)
                    p2_bar = dp.last() + [x1_free]
                seq_bar = p2_bar
                qbase += NQG[s] * 512
            stageA_bar = seq_bar
        while bg:
            bg.pop(0)()
        wcast_tok = wcast

        NTT = NQT // 128
        gseq = []
        for s in range(NSEQ):
            gseq += [s] * (NQG[s] * 4)
        with ExitStack() as bes:
            def sbB(name, shape, dt): return bes.enter_context(nc.sbuf_tensor(U(name), shape, dt))
            geff2_ = [sbB("geff2_%d" % i, [128, D], F32) for i in range(2)]
            sh2r_ = [sbB("sh2r_%d" % i, [128, D], F32) for i in range(2)]
            gvec2_ = [sbB("gvec2_%d" % i, [128, D], F32) for i in range(2)]
            vt = [sbB("vtB%d" % i, [128, D], F32) for i in range(2)]
            M1a = sbB("M1a", [128, NTT, 32], F32)
            M2a = sbB("M2a", [128, NTT, 32], F32)
            W1a = sbB("W1a", [128, NTT], F32)
            W2a = sbB("W2a", [128, NTT], F32)
            R1a = sbB("R1a", [128, NTT], F32)
            R2a = sbB("R2a", [128, NTT], F32)
            slot0 = sbB("slot0", [128, NTT], I32)
            slot1 = sbB("slot1", [128, NTT], I32)
            idxw = sbB("idxw", [128, NSL], I32)
            carry = sbB("carry", [128, 32], F32)
            PS = [bes.enter_context(nc.psum_tensor(U("PS%d" % i), [128, 512], F32)) for i in range(8)]
            bb = stageA_bar
            readyB = [bb, wcast_tok, zero_tok]

            def load_vecs(s, deps):
                geff2 = geff2_[s % 2]; sh2r = sh2r_[s % 2]; gvec2 = gvec2_[s % 2]
                l1 = DMA('v0_0', deps, lambda s=s: nc.sync.dma_start(out=vt[0][:], in_=mod_d[s, 4096:5120].partition_broadcast(128)))
                l2 = DMA('v0_1', deps, lambda: nc.sync.dma_start(out=vt[1][:], in_=g_pre2.partition_broadcast(128)))
                l3 = DMA('v0_2', deps, lambda s=s: nc.sync.dma_start(out=sh2r[:], in_=mod_d[s, 3072:4096].partition_broadcast(128)))
                tg = DVE([l1, l2], lambda: nc.vector.scalar_tensor_tensor(out=geff2[:], in0=vt[0][:], scalar=1.0, in1=vt[1][:],
                                                                           op0=ALU.add, op1=ALU.mult))
                l4 = DMA('v0_3', [tg], lambda s=s: nc.sync.dma_start(out=vt[0][:], in_=mod_d[s, 5120:6144].partition_broadcast(128)))
                l5 = DMA('v0_4', [tg], lambda: nc.sync.dma_start(out=vt[1][:], in_=g_post2.partition_broadcast(128)))
                tg2 = DVE([l4, l5], lambda: nc.vector.tensor_tensor(out=gvec2[:], in0=vt[0][:], in1=vt[1][:], op=ALU.mult))
                return [l3, tg, tg2]

            mkb1 = dp.mark()
            with ExitStack() as b1:
                def sb1(name, shape, dt): return b1.enter_context(nc.sbuf_tensor(U(name), shape, dt))
                w_r = sb1("w_r", [128, 8, 36], F32)
                brr = sb1("brr", [128, 36], F32)
                utri = sb1("utri", [128, 128], BF16)
                onesb = sb1("onesb", [128, 128], BF16)
                x1t = [sb1("x1t%d" % i, [128, D], F32) for i in range(5)]
                junk = sb1("junkB", [128, D], BF16)
                hm = sb1("hmB", [128, D], F32)
                h2 = [sb1("h2_%d" % i, [128, D], F32) for i in range(5)]
                h2Tf = [sb1("h2Tf%d" % i, [128, 8, 128], F32) for i in range(5)]
                stt = [sb1("stB%d" % i, [128, 8], F32) for i in range(5)]
                lg = [sb1("lg%d" % i, [128, 36], F32) for i in range(5)]
                wk = [sb1("wk%d" % i, [128, 192], F32) for i in range(5)]
                ohb = [sb1("ohb%d" % i, [128, 32], BF16) for i in range(5)]
                PH = [PS[6], PS[7]]
                ld = [DMA('s0', bb, lambda: nc.sync.dma_start(out=w_r[:, :, 0:4], in_=w_rg.rearrange("(k p) c -> p k c", p=128))),
                      DMA('s0', bb, lambda: nc.sync.dma_start(out=w_r[:, :, 4:36], in_=w_re.rearrange("(k p) c -> p k c", p=128))),
                      DMA('s0', bb, lambda: nc.sync.dma_start(out=brr[:, 0:4], in_=b_rg.partition_broadcast(128))),
                      DMA('s0', bb, lambda: nc.sync.dma_start(out=brr[:, 4:36], in_=b_re.partition_broadcast(128))),
                      DMA('s1', bb, lambda: nc.gpsimd.dma_start(out=utri[:], in_=utri_in), q='pool'),
                      POOL(bb, lambda: nc.gpsimd.memset(onesb[:], 1.0)),
                      POOL(bb, lambda: nc.gpsimd.memset(carry[:], 0.0))]
                rdy1 = [readyB, ld]
                T = dict(cur_seq=-1, vec_ready=None, vec_readers=[], hm_free=None, PH_free=[None, None], plg_free=None,
                         pcum_free=None, carry_tok=ld[-1])
                x1t_free = [None] * 5; h2_free = [None] * 5
                h2Tf_free = [None] * 5
                h2d_w = []

                def p1_tile(i):
                    s = gseq[i]
                    if s != T['cur_seq']:
                        T['cur_seq'] = s
                        T['vec_ready'] = load_vecs(s, [rdy1, T['vec_readers']])
                        T['vec_readers'] = []
                    vec_ready = T['vec_ready']
                    geff2 = geff2_[s % 2]; sh2r = sh2r_[s % 2]
                    ti = i % 5
                    tok0 = i * 128
                    st_ = stt[ti]
                    lx = DMA('bx%d' % ti, [x1t_free[ti], rdy1], lambda ti=ti, tok0=tok0: nc.sync.dma_start(out=x1t[ti][:], in_=x1_d[tok0:tok0 + 128, :]))
                    a1 = SQ(lx, lambda ti=ti, st_=st_: nc.scalar.activation(out=junk[:], in_=x1t[ti][:], func=AF.Square, accum_out=st_[:, 0:1]))
                    yield
                    r1a = ACT(a1, lambda st_=st_: nc.scalar.activation(out=st_[:, 1:2], in_=st_[:, 0:1], func=AF.Sqrt, bias=EPS, scale=1.0 / D))
                    yield
                    r1 = DVE(r1a, lambda st_=st_: nc.vector.reciprocal(out=st_[:, 1:2], in_=st_[:, 1:2]))
                    d1 = DVE([r1, T['hm_free'], vec_ready], lambda ti=ti, st_=st_: nc.vector.scalar_tensor_tensor(
                        out=hm[:], in0=x1t[ti][:], scalar=st_[:, 1:2], in1=geff2[:], op0=ALU.mult, op1=ALU.mult))
                    x1t_free[ti] = d1
                    p1_ = POOL([d1, h2_free[ti], vec_ready], lambda ti=ti: nc.gpsimd.tensor_tensor(out=h2[ti][:], in0=hm[:], in1=sh2r[:], op=ALU.add))
                    T['hm_free'] = p1_
                    T['vec_readers'] = [p1_, d1]
                    wr = DMA('h2w%d' % ti, p1_, lambda ti=ti, tok0=tok0: nc.gpsimd.dma_start(out=h2_d[tok0:tok0 + 128, :], in_=h2[ti][:]), q='pool')
                    h2d_w.append(wr)
                    yield
                    tp = None
                    for k in range(8):
                        bank = PH[k // 4]
                        tp = PE([p1_, T['PH_free']] if k == 0 else None,
                                lambda k=k, ti=ti, bank=bank: nc.tensor.transpose(bank[:, (k % 4) * 128:(k % 4 + 1) * 128],
                                                                                  h2[ti][:, k * 128:(k + 1) * 128], ident_f[:]),
                                sig=(k == 7))
                    h2_free[ti] = [tp, wr]
                    yield
                    e1 = ACT([tp, h2Tf_free[ti]], lambda ti=ti: nc.scalar.copy(out=h2Tf[ti][:, 0:4, :], in_=PH[0][:, :].rearrange("p (k c) -> p k c", k=4)))
                    e2 = DVE([tp, h2Tf_free[ti]], lambda ti=ti: nc.vector.tensor_copy(out=h2Tf[ti][:, 4:8, :], in_=PH[1][:, :].rearrange("p (k c) -> p k c", k=4)))
                    T['PH_free'] = [e1, e2]
                    yield
                    plg = PS[4][:, 0:36]
                    m_l = mmg(plg, [(h2Tf[ti][:, k, :], w_r[:, k, :]) for k in range(8)], [e1, e2, T['plg_free'], rdy1])
                    h2Tf_free[ti] = m_l
                    yield
                    L = lg[ti]; W = wk[ti]
                    v1 = DVE([m_l], lambda L=L: nc.vector.tensor_tensor(out=L[:], in0=plg, in1=brr[:], op=ALU.add))
                    T['plg_free'] = v1
                    v2 = DVE(v1, lambda L=L, W=W: nc.vector.tensor_reduce(out=W[:, 0:1], in_=L[:, 0:4], axis=AX.X, op=ALU.max))
                    v3 = DVE(v2, lambda W=W: nc.vector.tensor_scalar(out=W[:, 1:2], in0=W[:, 0:1], scalar1=-1.0, scalar2=None, op0=ALU.mult))
                    v4 = DVE(v2, lambda L=L, W=W: nc.vector.tensor_scalar(out=W[:, 4:8], in0=L[:, 0:4], scalar1=W[:, 0:1], scalar2=None, op0=ALU.is_equal))
                    s1 = ACT([v3], lambda L=L, W=W: nc.scalar.activation(out=W[:, 8:12], in_=L[:, 0:4], func=AF.Exp, bias=W[:, 1:2], scale=1.0,
                                                                         accum_out=W[:, 2:3]))
                    yield
                    v5 = DVE(s1, lambda W=W: nc.vector.reciprocal(out=W[:, 3:4], in_=W[:, 2:3]))
                    v6 = DVE(v4, lambda L=L, W=W: nc.vector.tensor_tensor(
                        out=W[:, 16:48].rearrange("p (g e) -> p g e", g=4), in0=L[:, 4:36].rearrange("p (g e) -> p g e", g=4),
                        in1=W[:, 4:8].unsqueeze(2).to_broadcast([128, 4, 8]), op=ALU.mult))
                    v7 = DVE(v6, lambda W=W: nc.vector.tensor_reduce(out=W[:, 48:56], in_=W[:, 16:48].rearrange("p (g e) -> p e g", g=4),
                                                                    axis=AX.X, op=ALU.add))
                    v8 = DVE(v7, lambda W=W: nc.vector.tensor_reduce(out=W[:, 12:13], in_=W[:, 48:56], axis=AX.X, op=ALU.max))
                    v9 = DVE(v8, lambda W=W: nc.vector.tensor_scalar(out=W[:, 56:64], in0=W[:, 48:56], scalar1=W[:, 12:13], scalar2=None,
                                                                    op0=ALU.is_equal))
                    v10 = DVE(v9, lambda W=W: nc.vector.scalar_tensor_tensor(out=W[:, 64:72], in0=W[:, 56:64], scalar=-1e30, in1=W[:, 48:56],
                                                                            op0=ALU.mult, op1=ALU.add))
                    v11 = DVE(v10, lambda W=W: nc.vector.tensor_reduce(out=W[:, 13:14], in_=W[:, 64:72], axis=AX.X, op=ALU.max))
                    v12 = DVE(v11, lambda W=W: nc.vector.tensor_scalar(out=W[:, 72:80], in0=W[:, 64:72], scalar1=W[:, 13:14], scalar2=None,
                                                                      op0=ALU.is_equal))
                    v13 = DVE(v11, lambda W=W: nc.vector.tensor_scalar(out=W[:, 14:15], in0=W[:, 12:13], scalar1=-1.0, scalar2=None, op0=ALU.mult))
                    s2 = ACT([v13], lambda W=W: nc.scalar.activation(out=W[:, 15:16], in_=W[:, 13:14], func=AF.Exp, bias=W[:, 14:15], scale=1.0))
                    yield
                    v14 = DVE(s2, lambda W=W: nc.vector.tensor_scalar(out=W[:, 80:81], in0=W[:, 15:16], scalar1=1.0, scalar2=None, op0=ALU.add))
                    v15 = DVE(v14, lambda W=W: nc.vector.reciprocal(out=W[:, 81:82], in_=W[:, 80:81]))
                    v16 = DVE([v15, v5], lambda W=W, i=i: nc.vector.tensor_tensor(out=W1a[:, i:i + 1], in0=W[:, 81:82], in1=W[:, 3:4], op=ALU.mult))
                    v17 = DVE(v16, lambda W=W, i=i: nc.vector.tensor_tensor(out=W2a[:, i:i + 1], in0=W1a[:, i:i + 1], in1=W[:, 15:16], op=ALU.mult))
                    v18 = DVE([v9, v4], lambda W=W, i=i: nc.vector.tensor_tensor(
                        out=M1a[:, i, :].rearrange("p (g e) -> p g e", g=4), in0=W[:, 4:8].unsqueeze(2).to_broadcast([128, 4, 8]),
                        in1=W[:, 56:64].unsqueeze(1).to_broadcast([128, 4, 8]), op=ALU.mult))
                    v19 = DVE([v12], lambda W=W, i=i: nc.vector.tensor_tensor(
                        out=M2a[:, i, :].rearrange("p (g e) -> p g e", g=4), in0=W[:, 4:8].unsqueeze(2).to_broadcast([128, 4, 8]),
                        in1=W[:, 72:80].unsqueeze(1).to_broadcast([128, 4, 8]), op=ALU.mult))
                    OH = ohb[ti]
                    v20 = DVE([v18, v19, T['pcum_free']], lambda OH=OH, i=i: nc.vector.tensor_tensor(out=OH[:], in0=M1a[:, i, :], in1=M2a[:, i, :], op=ALU.add))
                    pcum = PS[5][:, 0:32]
                    ptot = PS[5][:, 32:64]
                    PE([v20, T['pcum_free'], rdy1], lambda OH=OH: nc.tensor.matmul(pcum, lhsT=utri[:], rhs=OH[:], start=True, stop=True), sig=False)
                    mc = PE(None, lambda OH=OH: nc.tensor.matmul(ptot, lhsT=onesb[:], rhs=OH[:], start=True, stop=True))
                    yield
                    v21 = DVE([mc, T['carry_tok']], lambda W=W: nc.vector.tensor_tensor(out=W[:, 96:128], in0=carry[:], in1=pcum, op=ALU.add))
                    v22 = DVE(v21, lambda: nc.vector.tensor_tensor(out=carry[:], in0=carry[:], in1=ptot, op=ALU.add))
                    T['carry_tok'] = v22
                    T['pcum_free'] = v22
                    v23 = DVE(v22, lambda W=W, i=i: nc.vector.tensor_tensor(out=W[:, 128:160], in0=W[:, 96:128], in1=M1a[:, i, :], op=ALU.mult))
                    v24 = DVE(v23, lambda W=W, i=i: nc.vector.tensor_reduce(out=R1a[:, i:i + 1], in_=W[:, 128:160], axis=AX.X, op=ALU.add))
                    v25 = DVE(v24, lambda W=W, i=i: nc.vector.tensor_tensor(out=W[:, 160:192], in0=W[:, 96:128], in1=M2a[:, i, :], op=ALU.mult))
                    v26 = DVE(v25, lambda W=W, i=i: nc.vector.tensor_reduce(out=R2a[:, i:i + 1], in_=W[:, 160:192], axis=AX.X, op=ALU.add))
                interleave((p1_tile(i) for i in range(NTT)), 5)
                dp.retire_since(mkb1)
                b1_bar = dp.last() + [h2d_w]

            with ExitStack() as b2:
                def sb2(name, shape, dt): return b2.enter_context(nc.sbuf_tensor(U(name), shape, dt))
                jv = sb2("jv", [128, NSL], F32)
                pidx = sb2("pidx", [128, 1], F32)
                tri32 = sb2("tri32", [32, 32], F32)
                cmp_ = sb2("cmp", [128, NSL * 32], F32)
                tmpM = sb2("tmpM", [128, NTT, 32], F32)
                nblk = sb2("nblk", [128, 32], F32)
                pc = sb2("pc", [128, 32], F32)
                pcT = sb2("pcT", [32, 128], F32)
                sst = sb2("sst", [128, 32], F32)
                send = sb2("send", [128, 32], F32)
                te = sb2("te", [128, NSL], F32)
                sf = sb2("sf", [128, NTT], F32)
                l = [DMA('i0', b1_bar, lambda: nc.sync.dma_start(out=jv[:], in_=jv_in)),
                     DMA('i0', b1_bar, lambda: nc.sync.dma_start(out=pidx[:], in_=pidx_in)),
                     DMA('i0', b1_bar, lambda: nc.sync.dma_start(out=tri32[:], in_=tri32_in))]
                c3 = cmp_[:].rearrange("p (e m) -> p e m", e=32)
                q1 = DVE([l, b1_bar], lambda: nc.vector.tensor_tensor(out=c3, in0=jv[:].unsqueeze(1).to_broadcast([128, 32, NSL]),
                                                                      in1=carry[:].unsqueeze(2).to_broadcast([128, 32, NSL]), op=ALU.is_lt))
                q2 = DVE(q1, lambda: nc.vector.tensor_reduce(out=nblk[:], in_=c3, axis=AX.X, op=ALU.add))
                q3 = DVE(q2, lambda: nc.vector.tensor_scalar(out=pc[:], in0=nblk[:], scalar1=128.0, scalar2=None, op0=ALU.mult))
                q4 = PE(q3, lambda: nc.tensor.transpose(PS[0][0:32, 0:128], pc[:, :], ident_f[:]))
                q5 = ACT(q4, lambda: nc.scalar.copy(out=pcT[:], in_=PS[0][0:32, 0:128]))
                q6 = PE([q5, l], lambda: nc.tensor.matmul(PS[1][:, 0:32], lhsT=pcT[:, :], rhs=tri32[:, :], start=True, stop=True))
                q7 = DVE(q6, lambda: nc.vector.tensor_copy(out=sst[:], in_=PS[1][:, 0:32]))
                q8 = DVE(q7, lambda: nc.vector.tensor_tensor(out=send[:], in0=sst[:], in1=pc[:], op=ALU.add))
                c4 = cmp_[:].rearrange("p (m e) -> p m e", e=32)
                q9 = DVE(q8, lambda: nc.vector.tensor_tensor(out=c4, in0=send[:].unsqueeze(1).to_broadcast([128, NSL, 32]),
                                                             in1=jv[:].unsqueeze(2).to_broadcast([128, NSL, 32]), op=ALU.is_le))
                q10 = DVE(q9, lambda: nc.vector.tensor_reduce(out=te[:], in_=c4, axis=AX.X, op=ALU.add))
                q11 = DVE(q10, lambda: nc.vector.tensor_scalar(out=te[:], in0=te[:], scalar1=31.0, scalar2=128.0, op0=ALU.min, op1=ALU.mult))
                q12 = DVE(q11, lambda: nc.vector.tensor_scalar(out=te[:], in0=te[:], scalar1=pidx[:, 0:1], scalar2=None, op0=ALU.add))
                q13 = DVE(q12, lambda: nc.vector.tensor_copy(out=idxw[:], in_=te[:]))
                q14 = DVE(q7, lambda: nc.vector.tensor_tensor(out=tmpM[:], in0=M1a[:], in1=sst[:].unsqueeze(1).to_broadcast([128, NTT, 32]), op=ALU.mult))
                q15 = DVE(q14, lambda: nc.vector.tensor_reduce(out=sf[:], in_=tmpM[:], axis=AX.X, op=ALU.add))
                q16 = DVE(q15, lambda: nc.vector.tensor_tensor(out=sf[:], in0=sf[:], in1=R1a[:], op=ALU.add))
                q17 = DVE(q16, lambda: nc.vector.tensor_copy(out=slot0[:], in_=sf[:]))
                q18 = DVE(q17, lambda: nc.vector.tensor_tensor(out=tmpM[:], in0=M2a[:], in1=sst[:].unsqueeze(1).to_broadcast([128, NTT, 32]), op=ALU.mult))
                q19 = DVE(q18, lambda: nc.vector.tensor_reduce(out=sf[:], in_=tmpM[:], axis=AX.X, op=ALU.add))
                q20 = DVE(q19, lambda: nc.vector.tensor_tensor(out=sf[:], in0=sf[:], in1=R2a[:], op=ALU.add))
                q21 = DVE(q20, lambda: nc.vector.tensor_copy(out=slot1[:], in_=sf[:]))
                b2_bar = dp.last()

            mkb3 = dp.mark()
            with ExitStack() as b3:
                def sb3(name, shape, dt): return b3.enter_context(nc.sbuf_tensor(U(name), shape, dt))
                hsc = [sb3("hsc%d" % i, [128, D], BF16) for i in range(3)]
                hsc_free = [None] * 3
                sc_toks = []
                for i in range(NTT):
                    si = i % 3
                    tok0 = i * 128
                    lh = DMA('hl%d' % si, [hsc_free[si], b2_bar], lambda si=si, tok0=tok0: nc.sync.dma_start(out=hsc[si][:], in_=h2_d[tok0:tok0 + 128, :]))
                    s0 = DMA('sc%d' % si, [lh, b2_bar], lambda si=si, i=i: nc.gpsimd.indirect_dma_start(
                        out=xs_d[:, :], out_offset=bass.IndirectOffsetOnAxis(ap=slot0[:, i:i + 1], axis=0), in_=hsc[si][:, :], in_offset=None), q='pool')
                    s1_ = DMA('sc%d' % si, [lh], lambda si=si, i=i: nc.gpsimd.indirect_dma_start(
                        out=xs_d[:, :], out_offset=bass.IndirectOffsetOnAxis(ap=slot1[:, i:i + 1], axis=0), in_=hsc[si][:, :], in_offset=None), q='pool')
                    hsc_free[si] = [s0, s1_]
                    sc_toks += [s0, s1_]
                scat_done = [sc_toks[-6:], b2_bar]

                PF = 3
                NW = PF + 3
                ND = PF + 5
                NX = PF + 2
                wgu = [sb3("wgu%d" % i, [128, 8, 512], BF16) for i in range(NW)]
                wdb = [sb3("wdb%d" % i, [128, 2, D], BF16) for i in range(ND)]
                xsb = [sb3("xsb%d" % i, [128, D], BF16) for i in range(NX)]
                xT = [sb3("xT%d" % i, [128, 8, 128], BF16) for i in range(2)]
                sgs = [sb3("sgs%d" % i, [128, 256], F32) for i in range(2)]
                hid = [sb3("hid%d" % i, [128, 256], BF16) for i in range(2)]
                hT = [sb3("hT%d" % i, [128, 2, 128], BF16) for i in range(2)]
                ysb = [sb3("ysb%d" % i, [128, D], F32) for i in range(2)]
                pX = [PS[0][:, :].bitcast(BF16), PS[1][:, :].bitcast(BF16)]
                pH = [PS[2], PS[3]]
                pHT = [PS[4][:, 0:128].bitcast(BF16), PS[5][:, 0:128].bitcast(BF16)]
                pY = [PS[6], PS[7]]
                wgu_free = [None] * NW; wdb_free = [None] * ND; xsb_free = [None] * NX
                pX_free = [None, None]; xT_free = [None, None]; pH_free = [None, None]; sgs_free = [None, None]
                hid_free = [None, None]; pHT_free = [None, None]; hT_free = [None, None]
                pY_free = [None, None]; ysb_free = [None, None]
                st0 = {}; st1 = {}; st2 = {}; ldt = {}
                ys_w = []

                def issue_loads(a):
                    wi = a % NW; di = a % ND; xj = a % NX
                    lw = DMA('wgl%d' % wi, [wgu_free[wi], scat_done], lambda wi=wi, a=a: nc.gpsimd.indirect_dma_start(
                        out=wgu[wi][:].rearrange("p k c -> p (k c)"), out_offset=None, in_=wgu_r[:, :],
                        in_offset=bass.IndirectOffsetOnAxis(ap=idxw[:, a:a + 1], axis=0)), q='pool')
                    lwd = DMA('wdl%d' % di, [wdb_free[di], scat_done], lambda di=di, a=a: nc.gpsimd.indirect_dma_start(
                        out=wdb[di][:].rearrange("p k c -> p (k c)"), out_offset=None, in_=wd_r[:, :],
                        in_offset=bass.IndirectOffsetOnAxis(ap=idxw[:, a:a + 1], axis=0)), q='pool')
                    lxs = DMA('xsl%d' % xj, [xsb_free[xj], scat_done, sc_toks], lambda xj=xj, a=a: nc.sync.dma_start(
                        out=xsb[xj][:], in_=xs_d[a * 128:(a + 1) * 128, :]))
                    ldt[a] = (lw, lwd, lxs)

                for a in range(min(PF, NSL)):
                    issue_loads(a)
                for it in range(NSL + 3):
                    if it + PF < NSL:
                        issue_loads(it + PF)
                    a = it
                    if a < NSL:
                        xi = a % 2; xj = a % NX
                        lw, lwd, lxs = ldt[a]
                        tp = None
                        for k in range(8):
                            tp = PE([lxs, pX_free[xi]] if k == 0 else None,
                                    lambda k=k, xi=xi, xj=xj: nc.tensor.transpose(pX[xi][:, k * 128:(k + 1) * 128], xsb[xj][:, k * 128:(k + 1) * 128], ident_b[:]),
                                    sig=(k == 7))
                        xsb_free[xj] = tp
                        if a % 2 == 0:
                            ev = ACT([tp, xT_free[xi]], lambda xi=xi: nc.scalar.copy(out=xT[xi][:].rearrange("p k c -> p (k c)"), in_=pX[xi]))
                        else:
                            ev = DVE([tp, xT_free[xi]], lambda xi=xi: nc.vector.tensor_copy(out=xT[xi][:].rearrange("p k c -> p (k c)"), in_=pX[xi]))
                        pX_free[xi] = ev
                        st0[a] = (ev, lw, lwd)
                    a = it - 1
                    if 0 <= a < NSL:
                        wi = a % NW; xi = a % 2
                        ev, lw, lwd = st0[a]
                        mh = mmg(pH[xi][:, :], [(xT[xi][:, k, :], wgu[wi][:, k, :]) for k in range(8)], [ev, lw, pH_free[xi]])
                        wgu_free[wi] = mh
                        xT_free[xi] = mh
                        a_s = ACT([mh, sgs_free[xi]], lambda xi=xi: nc.scalar.activation(out=sgs[xi][:], in_=pH[xi][:, 0:256], func=AF.Silu))
                        d_h = DVE([a_s, hid_free[xi]], lambda xi=xi: nc.vector.tensor_tensor(out=hid[xi][:], in0=sgs[xi][:], in1=pH[xi][:, 256:512], op=ALU.mult))
                        pH_free[xi] = d_h
                        sgs_free[xi] = d_h
                        st1[a] = (d_h, lwd)
                    a = it - 2
                    if 0 <= a < NSL:
                        xi = a % 2
                        d_h, lwd = st1[a]
                        tp2 = None
                        for j in range(2):
                            tp2 = PE([d_h, pHT_free[xi]] if j == 0 else None,
                                     lambda j=j, xi=xi: nc.tensor.transpose(pHT[xi][:, j * 128:(j + 1) * 128], hid[xi][:, j * 128:(j + 1) * 128], ident_b[:]),
                                     sig=(j == 1))
                        hid_free[xi] = tp2
                        ev2 = ACT([tp2, hT_free[xi]], lambda xi=xi: nc.scalar.copy(out=hT[xi][:].rearrange("p k c -> p (k c)"), in_=pHT[xi]))
                        pHT_free[xi] = ev2
                        st2[a] = (ev2, lwd)
                    a = it - 3
                    if 0 <= a < NSL:
                        xi = a % 2; di = a % ND
                        ev2, lwd = st2[a]
                        my0 = mmg(pY[0][:, :], [(hT[xi][:, j, :], wdb[di][:, j, 0:512]) for j in range(2)], [ev2, lwd, pY_free[0]])
                        my1 = mmg(pY[1][:, :], [(hT[xi][:, j, :], wdb[di][:, j, 512:1024]) for j in range(2)], [pY_free[1]])
                        wdb_free[di] = my1
                        hT_free[xi] = my1
                        c0 = ACT([my0, ysb_free[xi]], lambda xi=xi: nc.scalar.copy(out=ysb[xi][:, 0:512], in_=pY[0][:, :]))
                        c1 = DVE([my1, ysb_free[xi]], lambda xi=xi: nc.vector.tensor_copy(out=ysb[xi][:, 512:1024], in_=pY[1][:, :]))
                        pY_free = [c0, c1]
                        ysb_free[xi] = DMA('ysw%d' % xi, [c0, c1], lambda xi=xi, a=a: nc.sync.dma_start(out=ys_d[a * 128:(a + 1) * 128, :], in_=ysb[xi][:]))
                        ys_w.append(ysb_free[xi])
                dp.retire_since(mkb3)
                b3_bar = dp.last() + [ys_w[-2:]]

            with ExitStack() as b4:
                def sb4(name, shape, dt): return b4.enter_context(nc.sbuf_tensor(U(name), shape, dt))
                ya = [sb4("ya%d" % i, [128, D], F32) for i in range(5)]
                yb = [sb4("yb%d" % i, [128, D], F32) for i in range(5)]
                x1c = [sb4("x1c%d" % i, [128, D], F32) for i in range(5)]
                mm_ = [sb4("mm_%d" % i, [128, D], F32) for i in range(5)]
                ytmp = [sb4("ytmp%d" % i, [128, D], F32) for i in range(5)]
                yo = [sb4("yo%d" % i, [128, D], F32) for i in range(5)]
                junk = sb4("junkC", [128, D], BF16)
                stt = [sb4("stC%d" % i, [128, 8], F32) for i in range(5)]
                ya_free = [None] * 5; yb_free = [None] * 5; x1c_free = [None] * 5; mm_free = [None] * 5
                ytmp_free = [None] * 5; yo_free = [None] * 5
                T = dict(cur_seq=-1, vec_ready=None, vec_readers=[])
                out_toks = []

                def cmb_tile(i):
                    s = gseq[i]
                    if s != T['cur_seq']:
                        T['cur_seq'] = s
                        T['vec_ready'] = load_vecs(s, [b3_bar, T['vec_readers']])
                        T['vec_readers'] = []
                    vec_ready = T['vec_ready']
                    gvec2 = gvec2_[s % 2]
                    ti = i % 5
                    tok0 = i * 128
                    st_ = stt[ti]
                    ga = DMA('ga%d' % ti, [ya_free[ti], b3_bar, ys_w], lambda ti=ti, i=i: nc.gpsimd.indirect_dma_start(
                        out=ya[ti][:, :], out_offset=None, in_=ys_d[:, :], in_offset=bass.IndirectOffsetOnAxis(ap=slot0[:, i:i + 1], axis=0)), q='pool')
                    gb_ = DMA('gb%d' % ti, [yb_free[ti], b3_bar], lambda ti=ti, i=i: nc.gpsimd.indirect_dma_start(
                        out=yb[ti][:, :], out_offset=None, in_=ys_d[:, :], in_offset=bass.IndirectOffsetOnAxis(ap=slot1[:, i:i + 1], axis=0)), q='pool')
                    lx = DMA('cx%d' % ti, [x1c_free[ti], b3_bar], lambda ti=ti, tok0=tok0: nc.sync.dma_start(out=x1c[ti][:], in_=x1_d[tok0:tok0 + 128, :]))
                    yield
                    d1 = DVE([ga, mm_free[ti]], lambda ti=ti, i=i: nc.vector.tensor_scalar(out=mm_[ti][:], in0=ya[ti][:], scalar1=W1a[:, i:i + 1], scalar2=None, op0=ALU.mult))
                    ya_free[ti] = d1
                    d2 = DVE([gb_, d1], lambda ti=ti, i=i: nc.vector.scalar_tensor_tensor(out=mm_[ti][:], in0=yb[ti][:], scalar=W2a[:, i:i + 1], in1=mm_[ti][:],
                                                                                         op0=ALU.mult, op1=ALU.add))
                    yb_free[ti] = d2
                    a1 = SQ(d2, lambda ti=ti, st_=st_: nc.scalar.activation(out=junk[:], in_=mm_[ti][:], func=AF.Square, accum_out=st_[:, 0:1]))
                    yield
                    r1a = ACT(a1, lambda st_=st_: nc.scalar.activation(out=st_[:, 1:2], in_=st_[:, 0:1], func=AF.Sqrt, bias=EPS, scale=1.0 / D))
                    yield
                    r1 = DVE(r1a, lambda st_=st_: nc.vector.reciprocal(out=st_[:, 1:2], in_=st_[:, 1:2]))
                    d3 = DVE([r1, ytmp_free[ti], vec_ready], lambda ti=ti, st_=st_: nc.vector.scalar_tensor_tensor(
                        out=ytmp[ti][:], in0=mm_[ti][:], scalar=st_[:, 1:2], in1=gvec2[:], op0=ALU.mult, op1=ALU.mult))
                    mm_free[ti] = d3
                    T['vec_readers'] = [d3]
                    pp = POOL([d3, lx, yo_free[ti]], lambda ti=ti: nc.gpsimd.tensor_tensor(out=yo[ti][:], in0=ytmp[ti][:], in1=x1c[ti][:], op=ALU.add))
                    ytmp_free[ti] = pp
                    x1c_free[ti] = pp
                    yo_free[ti] = DMA('yo%d' % ti, pp, lambda ti=ti, tok0=tok0: nc.sync.dma_start(out=y[tok0:tok0 + 128, :], in_=yo[ti][:]))
                    out_toks.append(yo_free[ti])
                interleave((cmb_tile(i) for i in range(NTT)), 5)
            dp.wait('sp', [yo_free, out_toks[-5:]])
            for e in ('pe', 'act', 'dve', 'pool'):
                dp.wait('sp', [(e, dp.cnt[e])])
    return nc


def _rope_tables(pos):
    half = 16
    inv = (10000.0 ** (-np.arange(half, dtype=np.float32) / half)).astype(np.float32)
    ang = pos.astype(np.float32)[:, None] * inv[None, :]
    cos = np.cos(ang).astype(np.float32)
    sin = np.sin(ang).astype(np.float32)
    c = np.concatenate([cos, cos], axis=1).T
    s_ = np.concatenate([sin, sin], axis=1).T
    return np.ascontiguousarray(c), np.ascontiguousarray(s_)


def _consts(NSL):
    ident = np.eye(128, dtype=np.float32)
    egrp = np.zeros((8, 512), np.float32)
    for g in range(8):
        egrp[g, g * 64:(g + 1) * 64] = 1.0
    utri = np.triu(np.ones((128, 128), np.float32), k=1)
    tri32 = np.triu(np.ones((32, 32), np.float32), k=1)
    jv = np.tile((np.arange(NSL, dtype=np.float32) * 128.0)[None, :], (128, 1))
    pidx = np.arange(128, dtype=np.float32).reshape(128, 1)
    return dict(ident=ident, egrp=egrp, utri=utri, tri32=tri32, jv=np.ascontiguousarray(jv), pidx=pidx,
                zeros=np.zeros((128, 8192), np.float32))


def _nt(cfg):
    nqt = sum(cfg['NQG']) * 512
    nt = (2 * nqt + 32 * 127 + 127) // 128
    return ((nt + 7) // 8) * 8


WEIGHT_KEYS = ['w_ada', 'b_ada', 'g_pre1', 'g_post1', 'g_pre2', 'g_post2', 'w_in', 'g_q', 'w_uq', 'g_kv', 'w_ukv',
               'g_v_gmlp', 'w_spatial', 'b_spatial', 'g_attn_out', 'g_gmlp_out', 'w_out', 'w_router_group',
               'b_router_group', 'w_router_expert', 'b_router_expert', 'w_gate', 'w_up', 'w_down']

_NC_CACHE = {}


def kernel(**inputs):
    S = 4096
    x_all = np.concatenate([np.asarray(inputs['x_prompt'], np.float32), np.asarray(inputs['x_sample'], np.float32)], axis=0)
    c_all = np.concatenate([np.asarray(inputs['c_prompt'], np.float32), np.asarray(inputs['c_sample'], np.float32)], axis=0)
    weights = {k: np.ascontiguousarray(np.asarray(inputs[k], np.float32)) for k in WEIGHT_KEYS}
    consts = _consts(_nt(FULL_CFG))
    pos_nat = np.arange(S)
    in_maps = []
    plans = []
    for c in range(8):
        if c % 2 == 0:
            s0 = (5 * c) // 2
            A, B, Cq, qhalf = s0, s0 + 1, s0 + 2, 0
        else:
            s0 = (5 * c - 1) // 2
            Cq, qhalf, A, B = s0, 1, s0 + 1, s0 + 2
        if qhalf == 0:
            posC = pos_nat
        else:
            posC = np.concatenate([pos_nat[S // 2:], pos_nat[:S // 2]])
        xs = np.concatenate([x_all[A], x_all[B], x_all[Cq][posC]], axis=0)
        cv = np.stack([c_all[A], c_all[B], c_all[Cq]], axis=0)
        rc = np.zeros((3, 32, S), np.float32)
        rs = np.zeros((3, 32, S), np.float32)
        for i, p in enumerate([pos_nat, pos_nat, posC]):
            rc[i], rs[i] = _rope_tables(p)
        m = dict(weights)
        m.update(xs=np.ascontiguousarray(xs), cvec=np.ascontiguousarray(cv), rope_c=rc, rope_s=rs)
        m.update(consts)
        in_maps.append(m)
        plans.append((A, B, Cq, qhalf))
    if 'full' not in _NC_CACHE:
        _NC_CACHE['full'] = build(FULL_CFG)
    nc = _NC_CACHE['full']
    res = run_bass_kernel_spmd(nc, in_maps, core_ids=list(range(8)))
    y_all = np.zeros((20, S, D), np.float32)
    for c in range(8):
        yc = res.results[c]['y']
        A, B, Cq, qhalf = plans[c]
        y_all[A] = yc[0:S]
        y_all[B] = yc[S:2 * S]
        if qhalf == 0:
            y_all[Cq, 0:S // 2] = yc[2 * S:2 * S + S // 2]
        else:
            y_all[Cq, S // 2:] = yc[2 * S:2 * S + S // 2]
    return (np.ascontiguousarray(y_all[0:4]), np.ascontiguousarray(y_all[4:20]))
```
